# Optimizing a Trainium2 kernel written in Bass

```python
import math
import jax, jax.numpy as jnp
from jax import lax
import numpy as np

D_MODEL = 1024
BATCH = 16
SEQ = 4096
DEPTH = 2

HEAD_DIM = 64
H_SB = 4
H_FOX = 4
H_MLA = 4
H_DIL = 4
N_HEADS_OUT = H_SB + H_FOX + H_MLA + H_DIL
D_MIX = N_HEADS_OUT * HEAD_DIM
Q_BLOCK = 128
ROPE_THETA = 10000.0
MLA_Q_LORA = 256
MLA_KV_LORA = 128
MLA_NOPE = 64
MLA_ROPE = 32
MLA_V = HEAD_DIM
DIL_BRANCHES = ((128, 1), (512, 4), (2048, 16))
N_BR = len(DIL_BRANCHES)
N_SB = 3 * H_SB * HEAD_DIM
N_FOX = 3 * H_FOX * HEAD_DIM + H_FOX
N_MLA = MLA_Q_LORA + MLA_KV_LORA + MLA_ROPE
N_DIL = 3 * N_BR * H_DIL * HEAD_DIM
N_IN = N_SB + N_FOX + N_MLA + N_DIL
N_GROUPS = 4
EXPERTS_PER_GROUP = 4
N_EXPERTS = N_GROUPS * EXPERTS_PER_GROUP
TOP_K = 2
D_EXPERT = 512
MOE_BLOCK = 256
LN_EPS = 1e-5
RMS_EPS = 1e-6

kernel_name = 'hybrid_sb_fox_mla_dilated_hmoe'


def _layernorm(x, g, b):
    xf = x.astype(jnp.float32)
    mu = jnp.mean(xf, -1, keepdims=True)
    var = jnp.mean(jnp.square(xf - mu), -1, keepdims=True)
    return ((xf - mu) * lax.rsqrt(var + LN_EPS) * g + b).astype(x.dtype)


def _rmsnorm(x, g):
    xf = x.astype(jnp.float32)
    return (xf * lax.rsqrt(jnp.mean(jnp.square(xf), -1, keepdims=True) + RMS_EPS) * g).astype(x.dtype)


def _rope(x, pos):
    half = x.shape[-1] // 2
    inv_freq = ROPE_THETA ** (-jnp.arange(half, dtype=jnp.float32) / half)
    ang = pos.astype(jnp.float32)[:, None] * inv_freq[None, :]
    cos = jnp.cos(ang)[None, :, None, :]
    sin = jnp.sin(ang)[None, :, None, :]
    xf = x.astype(jnp.float32)
    x1, x2 = xf[..., :half], xf[..., half:]
    return jnp.concatenate([x1 * cos - x2 * sin, x2 * cos + x1 * sin], -1).astype(x.dtype)


def _sweep(block_fn, seq):
    out = lax.map(block_fn, jnp.arange(seq // Q_BLOCK, dtype=jnp.int32) * Q_BLOCK)
    nb, b, h, blk, dv = out.shape
    return out.transpose(1, 2, 0, 3, 4).reshape(b, h, nb * blk, dv)


def _causal_softmax_sweep(logits_fn, v):
    seq = v.shape[2]
    kpos = jnp.arange(seq)

    def block(t0):
        logits = logits_fn(t0)
        qpos = t0 + jnp.arange(Q_BLOCK)
        mask = kpos[None, :] <= qpos[:, None]
        p = jax.nn.softmax(jnp.where(mask, logits, -jnp.inf), axis=-1)
        return jnp.einsum('bhqk,bhkd->bhqd', p.astype(v.dtype), v)

    return _sweep(block, seq)


def _stick_breaking_attention(q, k, v):
    seq = q.shape[2]
    scale = q.shape[-1] ** -0.5
    kpos = jnp.arange(seq)

    def block(t0):
        qb = lax.dynamic_slice_in_dim(q, t0, Q_BLOCK, axis=2)
        z = jnp.einsum('bhqd,bhkd->bhqk', qb, k).astype(jnp.float32) * scale
        qpos = t0 + jnp.arange(Q_BLOCK)
        before = kpos[None, :] < qpos[:, None]
        log_keep = jnp.where(before, jax.nn.log_sigmoid(-z), 0.0)
        log_after = lax.cumsum(log_keep, axis=3, reverse=True) - log_keep
        w = jnp.where(before, jnp.exp(jax.nn.log_sigmoid(z) + log_after), 0.0)
        return jnp.einsum('bhqk,bhkd->bhqd', w.astype(v.dtype), v)

    return _sweep(block, seq)


def _forgetting_attention(q, k, v, log_f):
    scale = q.shape[-1] ** -0.5
    c = jnp.cumsum(log_f, axis=-1)

    def logits(t0):
        qb = lax.dynamic_slice_in_dim(q, t0, Q_BLOCK, axis=2)
        cb = lax.dynamic_slice_in_dim(c, t0, Q_BLOCK, axis=2)
        s = jnp.einsum('bhqd,bhkd->bhqk', qb, k).astype(jnp.float32) * scale
        return s + cb[..., :, None] - c[..., None, :]

    return _causal_softmax_sweep(logits, v)


def _mla_attention(q_nope, q_rope, k_nope, k_rope, v):
    scale = (q_nope.shape[-1] + q_rope.shape[-1]) ** -0.5

    def logits(t0):
        qn = lax.dynamic_slice_in_dim(q_nope, t0, Q_BLOCK, axis=2)
        qr = lax.dynamic_slice_in_dim(q_rope, t0, Q_BLOCK, axis=2)
        s = (jnp.einsum('bhqd,bhkd->bhqk', qn, k_nope).astype(jnp.float32)
             + jnp.einsum('bhqr,bkr->bhqk', qr, k_rope).astype(jnp.float32))
        return s * scale

    return _causal_softmax_sweep(logits, v)


def _dilated_attention(qs, ks, vs):
    seq = qs[0].shape[1]
    scale = qs[0].shape[-1] ** -0.5

    def block(t0):
        qpos = t0 + jnp.arange(Q_BLOCK)
        outs, lses = [], []
        for (window, dil), q, k, v in zip(DIL_BRANCHES, qs, ks, vs):
            n_keys = window // dil + 1
            idx = qpos[:, None] - dil * jnp.arange(n_keys)[None, :]
            valid = idx >= 0
            idx = jnp.maximum(idx, 0)
            qb = lax.dynamic_slice_in_dim(q, t0, Q_BLOCK, axis=1)
            kb = jnp.take(k, idx, axis=1)
            vb = jnp.take(v, idx, axis=1)
            z = jnp.einsum('bqhd,bqnhd->bhqn', qb, kb).astype(jnp.float32) * scale
            z = jnp.where(valid[None, None], z, -jnp.inf)
            m = jnp.max(z, -1, keepdims=True)
            p = jnp.exp(z - m)
            den = jnp.sum(p, -1, keepdims=True)
            outs.append(jnp.einsum('bhqn,bqnhd->bhqd', (p / den).astype(v.dtype), vb).astype(jnp.float32))
            lses.append(m + jnp.log(den))
        mix = jax.nn.softmax(jnp.stack(lses), axis=0)
        return jnp.sum(mix * jnp.stack(outs), axis=0).astype(vs[0].dtype)

    return _sweep(block, seq)


def _mixing(x, w_in, b_forget, g_cq, g_ckv, w_uq, w_ukv, g_head, w_out):
    bsz, seq, _ = x.shape
    pos = jnp.arange(seq)
    proj = x @ w_in
    p_sb, p_fox, p_mla, p_dil = jnp.split(proj, [N_SB, N_SB + N_FOX, N_SB + N_FOX + N_MLA], axis=-1)

    def heads(t, n):
        return t.reshape(bsz, seq, n, -1).transpose(0, 2, 1, 3)

    q, k, v = jnp.split(p_sb, 3, axis=-1)
    o_sb = _stick_breaking_attention(heads(q, H_SB), heads(k, H_SB), heads(v, H_SB))

    hd = H_FOX * HEAD_DIM
    q, k, v, f = jnp.split(p_fox, [hd, 2 * hd, 3 * hd], axis=-1)
    log_f = jax.nn.log_sigmoid((f + b_forget).astype(jnp.float32)).transpose(0, 2, 1)
    o_fox = _forgetting_attention(heads(q, H_FOX), heads(k, H_FOX), heads(v, H_FOX), log_f)

    c_q, c_kv, k_r = jnp.split(p_mla, [MLA_Q_LORA, MLA_Q_LORA + MLA_KV_LORA], axis=-1)
    q = (_rmsnorm(c_q, g_cq) @ w_uq).reshape(bsz, seq, H_MLA, MLA_NOPE + MLA_ROPE)
    kv = (_rmsnorm(c_kv, g_ckv) @ w_ukv).reshape(bsz, seq, H_MLA, MLA_NOPE + MLA_V)
    q_rope = _rope(q[..., MLA_NOPE:], pos)
    k_rope = _rope(k_r[:, :, None, :], pos)[:, :, 0]
    tr = lambda a: a.transpose(0, 2, 1, 3)
    o_mla = _mla_attention(tr(q[..., :MLA_NOPE]), tr(q_rope), tr(kv[..., :MLA_NOPE]), k_rope,
                           tr(kv[..., MLA_NOPE:]))

    q, k, v = jnp.split(p_dil, 3, axis=-1)
    br = lambda a: a.reshape(bsz, seq, N_BR, H_DIL, HEAD_DIM)
    flat = lambda a: a.reshape(bsz, seq, N_BR * H_DIL, HEAD_DIM)
    q = br(_rope(flat(q), pos))
    k = br(_rope(flat(k), pos))
    v = br(v)
    o_dil = _dilated_attention([q[:, :, i] for i in range(N_BR)],
                               [k[:, :, i] for i in range(N_BR)],
                               [v[:, :, i] for i in range(N_BR)])

    o = jnp.concatenate([o_sb, o_fox, o_mla, o_dil], axis=1)
    o = _rmsnorm(o, g_head[None, :, None, :])
    return o.transpose(0, 2, 1, 3).reshape(bsz, seq, D_MIX) @ w_out


def _hier_moe(x, w_group, b_group, w_expert, b_expert, w1, w3, w2):
    n_tok, d = x.shape
    xf = x.astype(jnp.float32)
    g_logits = xf @ w_group.astype(jnp.float32) + b_group.astype(jnp.float32)
    g_sel = jnp.argmax(g_logits, axis=-1)
    g_w = jnp.take_along_axis(jax.nn.softmax(g_logits, -1), g_sel[:, None], axis=-1)
    e_logits = (xf @ w_expert.astype(jnp.float32) + b_expert.astype(jnp.float32)).reshape(
        n_tok, N_GROUPS, EXPERTS_PER_GROUP)
    e_in_group = jnp.take_along_axis(e_logits, g_sel[:, None, None], axis=1)[:, 0]
    top_logit, top_idx = lax.top_k(e_in_group, TOP_K)
    gate = jax.nn.softmax(top_logit, -1) * g_w
    expert_id = (g_sel[:, None] * EXPERTS_PER_GROUP + top_idx).reshape(-1).astype(jnp.int32)

    n_assign = n_tok * TOP_K
    n_slots = n_assign + N_EXPERTS * MOE_BLOCK
    n_chunks = n_slots // MOE_BLOCK
    tok_id = jnp.repeat(jnp.arange(n_tok, dtype=jnp.int32), TOP_K)
    order = jnp.argsort(expert_id)
    e_sorted = expert_id[order]
    counts = jnp.bincount(expert_id, length=N_EXPERTS)
    start = jnp.cumsum(counts) - counts
    padded = (counts + MOE_BLOCK - 1) // MOE_BLOCK * MOE_BLOCK
    pad_end = jnp.cumsum(padded)
    pad_start = pad_end - padded
    dest = pad_start[e_sorted] + jnp.arange(n_assign) - start[e_sorted]
    slot_tok = jnp.full((n_slots,), n_tok, jnp.int32).at[dest].set(tok_id[order])
    slot_gate = jnp.zeros((n_slots,), jnp.float32).at[dest].set(gate.reshape(-1)[order])
    chunk_expert = jnp.minimum(
        jnp.searchsorted(pad_end, jnp.arange(n_chunks) * MOE_BLOCK, side='right'), N_EXPERTS - 1)
    x_pad = jnp.concatenate([x, jnp.zeros((1, d), x.dtype)], axis=0)
    xs = x_pad[slot_tok].reshape(n_chunks, MOE_BLOCK, d)

    def expert_block(args):
        xb, e = args
        h = jax.nn.silu(xb @ w1[e]) * (xb @ w3[e])
        return h @ w2[e]

    ys = lax.map(expert_block, (xs, chunk_expert)).reshape(n_slots, d)
    ys = ys * slot_gate[:, None].astype(ys.dtype)
    return jax.ops.segment_sum(ys, slot_tok, num_segments=n_tok + 1)[:n_tok]


def setup_inputs(seed: int = 0) -> dict:
    key = jax.random.key(seed)
    ks = jax.random.split(key, 20)
    L = DEPTH
    beta = (8.0 * DEPTH) ** -0.25
    f32 = jnp.float32

    def normal(k, shape, scale):
        return jax.random.normal(k, shape, f32) * scale

    return {
        'x': normal(ks[0], (BATCH, SEQ, D_MODEL), 1.0),
        'w_in': normal(ks[1], (L, D_MODEL, N_IN), D_MODEL ** -0.5),
        'b_forget': normal(ks[2], (L, H_FOX), 0.1),
        'g_cq': 1.0 + normal(ks[3], (L, MLA_Q_LORA), 0.02),
        'g_ckv': 1.0 + normal(ks[4], (L, MLA_KV_LORA), 0.02),
        'w_uq': normal(ks[5], (L, MLA_Q_LORA, H_MLA * (MLA_NOPE + MLA_ROPE)), MLA_Q_LORA ** -0.5),
        'w_ukv': normal(ks[6], (L, MLA_KV_LORA, H_MLA * (MLA_NOPE + MLA_V)), MLA_KV_LORA ** -0.5),
        'g_head': 1.0 + normal(ks[7], (L, N_HEADS_OUT, HEAD_DIM), 0.02),
        'w_out': normal(ks[8], (L, D_MIX, D_MODEL), beta * D_MIX ** -0.5),
        'ln1_g': 1.0 + normal(ks[9], (L, D_MODEL), 0.02),
        'ln1_b': normal(ks[10], (L, D_MODEL), 0.02),
        'w_group': normal(ks[11], (L, D_MODEL, N_GROUPS), D_MODEL ** -0.5),
        'b_group': normal(ks[12], (L, N_GROUPS), 0.01),
        'w_expert': normal(ks[13], (L, D_MODEL, N_EXPERTS), D_MODEL ** -0.5),
        'b_expert': normal(ks[14], (L, N_EXPERTS), 0.01),
        'w1': normal(ks[15], (L, N_EXPERTS, D_MODEL, D_EXPERT), D_MODEL ** -0.5),
        'w3': normal(ks[16], (L, N_EXPERTS, D_MODEL, D_EXPERT), D_MODEL ** -0.5),
        'w2': normal(ks[17], (L, N_EXPERTS, D_EXPERT, D_MODEL), beta * D_EXPERT ** -0.5),
        'ln2_g': 1.0 + normal(ks[18], (L, D_MODEL), 0.02),
        'ln2_b': normal(ks[19], (L, D_MODEL), 0.02),
    }


def reference(x, w_in, b_forget, g_cq, g_ckv, w_uq, w_ukv, g_head, w_out, ln1_g, ln1_b,
              w_group, b_group, w_expert, b_expert, w1, w3, w2, ln2_g, ln2_b):
    alpha = (2.0 * DEPTH) ** 0.25
    bsz, seq, d = x.shape
    for l in range(DEPTH):
        h = _mixing(x, w_in[l], b_forget[l], g_cq[l], g_ckv[l], w_uq[l], w_ukv[l], g_head[l], w_out[l])
        x = _layernorm(alpha * x + h, ln1_g[l], ln1_b[l])
        m = _hier_moe(x.reshape(bsz * seq, d), w_group[l], b_group[l], w_expert[l], b_expert[l],
                      w1[l], w3[l], w2[l]).reshape(bsz, seq, d)
        x = _layernorm(alpha * x + m, ln2_g[l], ln2_b[l])
    return x
```

```python
import numpy as np
import concourse.bass as bass
import concourse.mybir as mybir
from concourse.bass_utils import run_bass_kernel_spmd

F32 = mybir.dt.float32
BF16 = mybir.dt.bfloat16
AF = mybir.ActivationFunctionType
ALU = mybir.AluOpType
AX = mybir.AxisListType

SAME_ENGINE_SYNC = True
NDMA_SLOTS = 6

SEQ = 4096
DM = 1024
NB = SEQ // 128
NT = SEQ // 512
DEPTH = 2
ALPHA = (2.0 * DEPTH) ** 0.25
LN_EPS = 1e-5
RMS_EPS = 1e-6
N_IN = 4260
C_SB, C_FOX, C_MLA, C_DIL = 0, 768, 1540, 1956
MLA_SCALE = 96.0 ** -0.5
DIL_R = (1, 4, 16)
NEXP = 16
DEXP = 512


class Res:
    __slots__ = ("w", "r", "excl")

    def __init__(self, excl=False):
        self.w = None
        self.r = []
        self.excl = excl


class Sched:
    ENG = ("pe", "act", "dve", "pool", "sp")

    def __init__(self, nc):
        self.nc = nc
        self.ops = {e: [] for e in self.ENG}
        self.cnt = {e: 0 for e in self.ENG}
        self.seen = {e: {} for e in self.ENG}
        self.sems = {}
        self.dma_slots = {}
        self.dma_rr = {}
        self.final_waits = []
        self._stack = []
        self.nops = 0

    def sem(self, key):
        if key not in self.sems:
            cm = self.nc.semaphore("s_" + "_".join(str(k) for k in (key if isinstance(key, tuple) else (key,))))
            s = cm.__enter__()
            self._stack.append(cm)
            self.sems[key] = s
        return self.sems[key]

    def _deps(self, eng, reads, writes):
        deps = {}

        def add(t):
            if t is None:
                return
            k, v = t
            if deps.get(k, 0) < v:
                deps[k] = v
        for r in reads:
            add(r.w)
            if r.excl:
                for t in r.r:
                    if t[0] != eng:
                        add(t)
        for w in writes:
            add(w.w)
            for t in w.r:
                add(t)
        waits = []
        seen = self.seen[eng]
        for k, v in deps.items():
            if k == eng and (eng == "pe" or not SAME_ENGINE_SYNC):
                continue
            if seen.get(k, 0) >= v:
                continue
            seen[k] = v
            waits.append((k, v))
        return waits

    def _commit(self, ticket, reads, writes):
        for r in reads:
            if len(r.r) > 16:
                m = {}
                for k, v in r.r:
                    if m.get(k, 0) < v:
                        m[k] = v
                r.r = list(m.items())
            r.r.append(ticket)
        for w in writes:
            w.w = ticket
            w.r = []

    def op(self, eng, fn, reads=(), writes=()):
        waits = self._deps(eng, reads, writes)
        self.cnt[eng] += 1
        ticket = (eng, self.cnt[eng])
        self.ops[eng].append((waits, fn, (eng, 1)))
        self._commit(ticket, reads, writes)
        self.nops += 1
        return ticket

    def dma(self, q, out, in_, reads=(), writes=(), final=False, **kw):
        waits = self._deps(q, reads, writes)
        if q not in self.dma_slots:
            self.dma_slots[q] = [[("d", q, i), 0] for i in range(NDMA_SLOTS)]
            self.dma_rr[q] = 0
        i = self.dma_rr[q]
        self.dma_rr[q] = (i + 1) % NDMA_SLOTS
        slot = self.dma_slots[q][i]
        key, tot = slot
        if tot > 0 and self.seen[q].get(key, 0) < tot:
            self.seen[q][key] = tot
            waits.append((key, tot))
        slot[1] = tot + 16
        ticket = (key, tot + 16)
        fn = lambda e, out=out, in_=in_, kw=kw: e.dma_start(out=out, in_=in_, **kw)
        self.ops[q].append((waits, fn, (key, 16)))
        self._commit(ticket, reads, writes)
        self.nops += 1
        if final:
            self.final_waits.append(ticket)
        return ticket

    def flush(self):
        totals = [(e, self.cnt[e]) for e in self.ENG if self.cnt[e] > 0]
        for q, slots in self.dma_slots.items():
            for key, tot in slots:
                if tot > 0:
                    totals.append((key, tot))
        for e in self.ENG:
            self.sem(e)
            for waits, fn, inc in self.ops[e]:
                for k, v in waits:
                    self.sem(k)
                self.sem(inc[0])
        ops = self.ops
        self.ops = {e: [] for e in self.ENG}
        for e in self.ENG:
            for k, v in totals:
                self.seen[e][k] = max(self.seen[e].get(k, 0), v)
        with self.nc.Block() as block:
            def run(engname):
                def body(e):
                    for waits, fn, inc in ops[engname]:
                        for k, v in waits:
                            e.wait_ge(self.sems[k], v)
                        fn(e).then_inc(self.sems[inc[0]], inc[1])
                    for k, v in totals:
                        e.wait_ge(self.sems[k], v)
                return body
            block.sync(run("sp"))
            block.tensor(run("pe"))
            block.scalar(run("act"))
            block.vector(run("dve"))
            block.gpsimd(run("pool"))

    def close(self):
        for cm in reversed(self._stack):
            cm.__exit__(None, None, None)
        self._stack = []


_UID = [0]


class Alloc:
    def __init__(self, nc):
        self.nc = nc
        self.stack = []

    @property
    def n(self):
        _UID[0] += 1
        return _UID[0]

    def sb(self, name, shape, dt):
        cm = self.nc.sbuf_tensor("%s_%d" % (name, self.n), list(shape), dt)
        t = cm.__enter__()
        self.stack.append(cm)
        return t

    def ps(self, name, shape, dt):
        cm = self.nc.psum_tensor("%s_%d" % (name, self.n), list(shape), dt)
        t = cm.__enter__()
        self.stack.append(cm)
        return t

    def close(self):
        for cm in reversed(self.stack):
            cm.__exit__(None, None, None)
        self.stack = []


class Buf:
    def __init__(self, t, excl=False):
        self.t = t
        self.r = Res(excl)


def rot(A, kind, name, shape, dt, n):
    f = A.sb if kind == "sb" else A.ps
    return [Buf(f(name + str(i), shape, dt), excl=(kind == "ps")) for i in range(n)]


def make_ident(A, S, dt):
    b = Buf(A.sb("ident", [128, 128], dt))
    S.op("pool", lambda e: e.memset(b.t[:], 1.0), writes=[b.r])
    S.op("pool", lambda e: e.affine_select(out=b.t[:], in_=b.t[:], pattern=[[-1, 128]], compare_op=ALU.is_equal,
                                           fill=0.0, base=0, channel_multiplier=1), reads=[b.r], writes=[b.r])
    return b


def perm_view(ap2d, r, t0, n):
    if r == 1:
        return ap2d[:, t0:t0 + n]
    sc = SEQ // r
    v = ap2d.rearrange("p (i c) -> p c i", c=r)
    c0, i0 = t0 // sc, t0 % sc
    if i0 + n <= sc:
        return v[:, c0, i0:i0 + n]
    assert i0 == 0 and n % sc == 0
    return v[:, c0:c0 + n // sc, :]


import os as _os
_P1SEC = _os.environ.get("P1SEC", "abcd")
_MSUB = _os.environ.get("MSUB", "qkrvptc")
_MC = _os.environ.get("MC", "1234")


def phase1(nc, S, T, xin, r_xin, l, sc):
    A = Alloc(nc)
    ident = make_ident(A, S, BF16)
    xT = Buf(A.sb("xT", [128, 8, SEQ], BF16))
    xs = rot(A, "sb", "xs", [128, DM], F32, 2)
    xb = rot(A, "sb", "xb", [128, DM], BF16, 2)
    pst = rot(A, "ps", "pst", [128, DM], BF16, 2)
    pj = rot(A, "ps", "pj", [128, 512], F32, 4)
    pv = rot(A, "ps", "pv", [128, 512], F32, 2)
    Win = T["w_in"][l].rearrange("(c p) n -> p c n", p=128)

    for b in range(NB):
        s, c_, p = xs[b % 2], xb[b % 2], pst[b % 2]
        S.dma("sp", s.t[:], xin[b * 128:(b + 1) * 128, :], reads=[r_xin], writes=[s.r])
        S.op("act", lambda e, s=s, c_=c_: e.activation(out=c_.t[:], in_=s.t[:], func=AF.Copy), reads=[s.r], writes=[c_.r])
        for c in range(8):
            S.op("pe", lambda e, c=c, c_=c_, p=p: e.transpose(out=p.t[:, c * 128:(c + 1) * 128], in_=c_.t[:, c * 128:(c + 1) * 128],
                                                              identity=ident.t[:]), reads=[c_.r, ident.r], writes=[p.r])
        S.op("dve", lambda e, b=b, p=p: e.tensor_copy(out=xT.t[:, :, b * 128:(b + 1) * 128],
                                                      in_=p.t[:, :].rearrange("p (c t) -> p c t", c=8)), reads=[p.r], writes=[xT.r])

    wts = rot(A, "sb", "wt", [128, 8, 416], BF16, 2)
    wsw = rot(A, "sb", "wsw", [128, 8, 256], BF16, 2)
    stg = rot(A, "sb", "stg", [128, 512], BF16, 4)
    vst = rot(A, "sb", "vst", [128, NB, 2, 65], BF16, 2)
    tmp1 = rot(A, "sb", "tmp1", [128, 512], F32, 2)
    tmp2 = rot(A, "sb", "tmp2", [128, 512], F32, 2)
    CT = Buf(A.sb("ropeC", [128, SEQ], BF16))
    ST = Buf(A.sb("ropeS", [128, SEQ], BF16))
    for v in vst:
        S.op("pool", lambda e, v=v: e.memset(v.t[:], 1.0), writes=[v.r])
    state = {"w": 0, "pj": 0, "stg": 0, "v": 0, "pv": 0}

    def load_w(col_ranges):
        w = wts[state["w"] % 2]
        state["w"] += 1
        o = 0
        for (c0, n) in col_ranges:
            S.dma("pool", w.t[:, :, o:o + n], Win[:, :, c0:c0 + n], writes=[w.r])
            o += n
        return w

    def nxt(key, lst):
        b = lst[state[key] % len(lst)]
        state[key] += 1
        return b

    def proj_fm(w, o, M, r, j, wtile=None):
        p = nxt("pj", pj)
        wt_ = w if wtile is None else wtile
        for c in range(8):
            S.op("pe", lambda e, c=c, p=p, wt_=wt_: e.matmul(p.t[0:M, :], lhsT=wt_.t[:, c, o:o + M],
                                                             rhs=perm_view(xT.t[:, c, :], r, j * 512, 512),
                                                             start=(c == 0), stop=(c == 7)),
                 reads=[wt_.r, xT.r], writes=[p.r])
        return p

    def store_rows(st, rows, dst, r_dst, j):
        S.dma("sp", dst[:, j * 512:(j + 1) * 512], st.t[rows[0]:rows[1], :], reads=[st.r], writes=[r_dst])

    def v_proj(w, o, r, vt, ncol=128):
        for b4 in range(NB // 4):
            p = nxt("pv", pv)
            for bb in range(4):
                b = b4 * 4 + bb
                for c in range(8):
                    S.op("pe", lambda e, c=c, b=b, bb=bb, p=p: e.matmul(p.t[:, bb * 128:(bb + 1) * 128],
                                                                      lhsT=perm_view(xT.t[:, c, :], r, b * 128, 128),
                                                                      rhs=w.t[:, c, o:o + ncol], start=(c == 0), stop=(c == 7)),
                         reads=[w.r, xT.r], writes=[p.r])
            S.op("dve", lambda e, b4=b4, p=p: e.tensor_copy(
                out=vt.t[:, b4 * 4:(b4 + 1) * 4, :, 0:64],
                in_=p.t[:, :].rearrange("p (b h d) -> p b h d", b=4, h=2)), reads=[p.r], writes=[vt.r])

    def scale_q(w):
        S.op("dve", lambda e: e.tensor_scalar(out=w.t[:, :, 0:128], in0=w.t[:, :, 0:128], scalar1=0.125, scalar2=None,
                                              op0=ALU.mult), reads=[w.r], writes=[w.r])

    for kind, cbase, hbase, pbase in (("sb", C_SB, 0, 0), ("fox", C_FOX, 4, 2)):
        for hp in range(2):
            w = load_w([(cbase + hp * 128, 128), (cbase + 256 + hp * 128, 128), (cbase + 512 + hp * 128, 128)])
            scale_q(w)
            for qk, dst, rd in ((0, sc["QS"], sc["r_QS"]), (1, sc["KS"], sc["r_KS"])):
                for j in range(NT):
                    p = proj_fm(w, qk * 128, 128, 1, j)
                    st = nxt("stg", stg)
                    S.op("act", lambda e, p=p, st=st: e.activation(out=st.t[:], in_=p.t[:], func=AF.Copy), reads=[p.r], writes=[st.r])
                    for hh in range(2):
                        h = hbase + hp * 2 + hh
                        store_rows(st, (hh * 64, hh * 64 + 64), dst[h][0:64, :], rd[h], j)
            vt = nxt("v", vst)
            v_proj(w, 256, 1, vt)
            S.dma("sp", sc["VS"][pbase + hp], vt.t[:, :, :, :].rearrange("p b h d -> p (b h d)"), reads=[vt.r], writes=[sc["r_VS"][pbase + hp]])

    S.flush()
    if "b" not in _P1SEC:
        A.close()
        return
    A2 = Alloc(nc)
    wf = Buf(A2.sb("wf", [128, 8, 4], BF16))
    S.dma("pool", wf.t[:], Win[:, :, C_FOX + 768:C_FOX + 772], writes=[wf.r])
    bfg = Buf(A2.sb("bfg", [4, 1], F32))
    S.dma("sp", bfg.t[:], T["b_forget"][l].rearrange("(h o) -> h o", o=1), writes=[bfg.r])
    S.op("dve", lambda e: e.tensor_scalar(out=bfg.t[:], in0=bfg.t[:], scalar1=-1.0, scalar2=None, op0=ALU.mult), reads=[bfg.r], writes=[bfg.r])
    nlf = Buf(A2.sb("nlf", [4, SEQ], F32))
    ncum = Buf(A2.sb("ncum", [4, SEQ], F32))
    ones4 = Buf(A2.sb("ones4", [4, 512], F32))
    S.op("pool", lambda e: e.memset(ones4.t[:], 1.0), writes=[ones4.r])
    for j in range(NT):
        p = proj_fm(wf, 0, 4, 1, j)
        S.op("act", lambda e, p=p, j=j: e.activation(out=nlf.t[:, j * 512:(j + 1) * 512], in_=p.t[0:4, :], func=AF.Exp,
                                                     bias=bfg.t[:, 0:1], scale=-1.0), reads=[p.r, bfg.r], writes=[nlf.r])
    S.op("act", lambda e: e.activation(out=nlf.t[:], in_=nlf.t[:], func=AF.Ln, bias=1.0), reads=[nlf.r], writes=[nlf.r])
    for j in range(NT):
        sl = slice(j * 512, (j + 1) * 512)
        init = 0.0 if j == 0 else ncum.t[:, j * 512 - 1:j * 512]
        S.op("dve", lambda e, sl=sl, init=init: e.tensor_tensor_scan(out=ncum.t[:, sl], data0=ones4.t[:, :], data1=nlf.t[:, sl],
                                                                     initial=init, op0=ALU.mult, op1=ALU.add),
             reads=[ones4.r, nlf.r, ncum.r], writes=[ncum.r])
    parts = [Buf(A2.sb("cpart%d" % i, [4, SEQ], BF16)) for i in range(3)]
    for i in range(3):
        S.op("dve", lambda e, i=i: e.tensor_copy(out=parts[i].t[:], in_=ncum.t[:]), reads=[ncum.r], writes=[parts[i].r])
        if i < 2:
            S.op("dve", lambda e, i=i: e.tensor_tensor(out=ncum.t[:], in0=ncum.t[:], in1=parts[i].t[:], op=ALU.subtract),
                 reads=[ncum.r, parts[i].r], writes=[ncum.r])
    ones3 = Buf(A2.sb("ones3", [35, SEQ], BF16))
    S.op("pool", lambda e: e.memset(ones3.t[0:3, :], 1.0), writes=[ones3.r])
    S.op("pool", lambda e: e.memset(ones3.t[32:35, :], -1.0), writes=[ones3.r])
    for h in range(4):
        H = 4 + h
        S.dma("sp", sc["KS"][H][64:67, :], ones3.t[32:35, :], reads=[ones3.r], writes=[sc["r_KS"][H]])
        S.dma("sp", sc["QS"][H][67:70, :], ones3.t[0:3, :], reads=[ones3.r], writes=[sc["r_QS"][H]])
        for i in range(3):
            S.dma("sp", sc["KS"][H][67 + i:68 + i, :], parts[i].t[h:h + 1, :], reads=[parts[i].r], writes=[sc["r_KS"][H]])
            S.dma("sp", sc["QS"][H][64 + i:65 + i, :], parts[i].t[h:h + 1, :], reads=[parts[i].r], writes=[sc["r_QS"][H]])
    S.flush()
    A2.close()

    if "c" not in _P1SEC:
        A.close()
        return
    w = load_w([(C_MLA, 416)])
    wkrs = nxt("w", wsw) if False else wsw[0]
    if "p" in _MSUB:
        S.op("pool", lambda e: e.tensor_copy(out=wkrs.t[:, :, 0:16], in_=w.t[:, :, 400:416]), reads=[w.r], writes=[wkrs.r])
        S.op("pool", lambda e: e.tensor_copy(out=wkrs.t[:, :, 16:32], in_=w.t[:, :, 384:400]), reads=[w.r], writes=[wkrs.r])
    A3 = Alloc(nc)
    wuq = Buf(A3.sb("wuq", [128, 2, 384], BF16))
    wuqs = Buf(A3.sb("wuqs", [128, 2, 384], BF16))
    wukv = Buf(A3.sb("wukv", [128, 512], BF16))
    S.dma("pool", wuq.t[:], T["w_uq"][l].rearrange("(c p) n -> p c n", p=128), writes=[wuq.r])
    S.dma("pool", wukv.t[:], T["w_ukv"][l], writes=[wukv.r])
    S.op("pool", lambda e: e.tensor_copy(out=wuqs.t[:], in_=wuq.t[:]), reads=[wuq.r], writes=[wuqs.r])
    for c2 in (range(2) if "p" in _MSUB else []):
        v4o = wuqs.t[:, c2, :].rearrange("p (h d) -> p h d", h=4)
        v4i = wuq.t[:, c2, :].rearrange("p (h d) -> p h d", h=4)
        S.op("pool", lambda e, v4o=v4o, v4i=v4i: e.tensor_copy(out=v4o[:, :, 64:80], in_=v4i[:, :, 80:96]), reads=[wuq.r, wuqs.r], writes=[wuqs.r])
        S.op("pool", lambda e, v4o=v4o, v4i=v4i: e.tensor_copy(out=v4o[:, :, 80:96], in_=v4i[:, :, 64:80]), reads=[wuq.r, wuqs.r], writes=[wuqs.r])
    gcq = Buf(A3.sb("gcq", [128, 2], F32))
    gckv = Buf(A3.sb("gckv", [128, 1], F32))
    for c2 in range(2):
        S.dma("sp", gcq.t[:, c2:c2 + 1], T["g_cq"][l][c2 * 128:(c2 + 1) * 128].rearrange("(p o) -> p o", o=1), writes=[gcq.r])
    S.dma("sp", gckv.t[:], T["g_ckv"][l].rearrange("(p o) -> p o", o=1), writes=[gckv.r])
    for tb, nm in (((CT, "rope32c"), (ST, "rope32s")) if "t" in _MSUB else []):
        S.dma("pool", tb.t[0:32, :], T[nm], writes=[tb.r])
        S.dma("pool", tb.t[64:96, :], T[nm], writes=[tb.r])
    onesq = Buf(A3.sb("onesq", [128, 128], BF16))
    oneskv = Buf(A3.sb("oneskv", [128, 128], BF16))
    epst = Buf(A3.sb("epst", [128, 1], F32))
    S.op("pool", lambda e: e.memset(epst.t[:], RMS_EPS), writes=[epst.r])
    S.op("pool", lambda e: e.memset(onesq.t[:], 1.0 / 256.0), writes=[onesq.r])
    S.op("pool", lambda e: e.memset(oneskv.t[:], 1.0 / 128.0), writes=[oneskv.r])
    cqg = rot(A3, "sb", "cqg", [128, 2, 512], BF16, 2)
    cq2 = rot(A3, "sb", "cq2", [128, 2, 512], BF16, 2)
    ckg = rot(A3, "sb", "ckg", [128, 512], BF16, 2)
    ck2 = rot(A3, "sb", "ck2", [128, 512], BF16, 2)
    rq = rot(A3, "sb", "rq", [128, 512], F32, 2)
    rkv = rot(A3, "sb", "rkv", [128, 512], F32, 2)
    rtok = rot(A3, "sb", "rtok", [128, 1], F32, 2)
    vstm = Buf(A3.sb("vstm", [128, NB, 4, 65], BF16))
    S.op("pool", lambda e: e.memset(vstm.t[:], 1.0), writes=[vstm.r])
    wukv_v = wukv.t[:, :].rearrange("p (h x) -> p h x", h=4)[:, :, 64:128]
    for j in (range(NT) if "c" in _MSUB else []):
        tc = slice(j * 512, (j + 1) * 512)
        a, a2, kg, k2, rq_, rkv_ = cqg[j % 2], cq2[j % 2], ckg[j % 2], ck2[j % 2], rq[j % 2], rkv[j % 2]
        for c2 in (range(2) if "1" in _MC else []):
            p = proj_fm(w, c2 * 128, 128, 1, j)
            S.op("dve", lambda e, p=p, c2=c2, a=a: e.tensor_scalar(out=a.t[:, c2, :], in0=p.t[:], scalar1=gcq.t[:, c2:c2 + 1], scalar2=None,
                                                                  op0=ALU.mult), reads=[p.r, gcq.r], writes=[a.r])
            S.op("act", lambda e, p=p, c2=c2, a2=a2: e.activation(out=a2.t[:, c2, :], in_=p.t[:], func=AF.Square), reads=[p.r], writes=[a2.r])
        if "2" in _MC:
            p = proj_fm(w, 256, 128, 1, j)
            if "5" not in _MC:
                S.op("dve", lambda e, p=p, kg=kg: e.tensor_scalar(out=kg.t[:], in0=p.t[:], scalar1=gckv.t[:, 0:1], scalar2=None, op0=ALU.mult),
                     reads=[p.r, gckv.r], writes=[kg.r])
            if "6" not in _MC:
                S.op("act", lambda e, p=p, k2=k2: e.activation(out=k2.t[:], in_=p.t[:], func=AF.Square), reads=[p.r], writes=[k2.r])
        if "3" in _MC:
            p = nxt("pj", pj)
            for c2 in range(2):
                S.op("pe", lambda e, p=p, c2=c2, a2=a2: e.matmul(p.t[:], lhsT=onesq.t[:], rhs=a2.t[:, c2, :], start=(c2 == 0), stop=(c2 == 1)),
                     reads=[onesq.r, a2.r], writes=[p.r])
            S.op("act", lambda e, p=p, rq_=rq_: e.activation(out=rq_.t[:], in_=p.t[:], func=AF.Sqrt, bias=epst.t[:, 0:1]), reads=[p.r, epst.r], writes=[rq_.r])
            S.op("dve", lambda e, rq_=rq_: e.reciprocal(out=rq_.t[:], in_=rq_.t[:]), reads=[rq_.r], writes=[rq_.r])
        if "4" in _MC:
            p = nxt("pj", pj)
            S.op("pe", lambda e, p=p, k2=k2: e.matmul(p.t[:], lhsT=oneskv.t[:], rhs=k2.t[:], start=True, stop=True), reads=[oneskv.r, k2.r], writes=[p.r])
            S.op("act", lambda e, p=p, rkv_=rkv_: e.activation(out=rkv_.t[:], in_=p.t[:], func=AF.Sqrt, bias=epst.t[:, 0:1]), reads=[p.r, epst.r], writes=[rkv_.r])
            S.op("dve", lambda e, rkv_=rkv_: e.reciprocal(out=rkv_.t[:], in_=rkv_.t[:]), reads=[rkv_.r], writes=[rkv_.r])
        for h in (range(4) if "q" in _MSUB else []):
            H = 8 + h
            pa, pb = nxt("pj", pj), nxt("pj", pj)
            for pp, ww in ((pa, wuq), (pb, wuqs)):
                for c2 in range(2):
                    S.op("pe", lambda e, pp=pp, ww=ww, c2=c2, h=h, a=a: e.matmul(pp.t[0:96, :], lhsT=ww.t[:, c2, h * 96:(h + 1) * 96], rhs=a.t[:, c2, :],
                                                                             start=(c2 == 0), stop=(c2 == 1)), reads=[ww.r, a.r], writes=[pp.r])
            st = nxt("stg", stg)
            t1, t2 = tmp1[h % 2], tmp2[h % 2]
            S.op("dve", lambda e, pa=pa, st=st, rq_=rq_: e.scalar_tensor_tensor(out=st.t[0:64, :], in0=pa.t[0:64, :], scalar=MLA_SCALE, in1=rq_.t[0:64, :],
                                                                             op0=ALU.mult, op1=ALU.mult), reads=[pa.r, rq_.r], writes=[st.r])
            S.op("dve", lambda e, pa=pa, t1=t1, tc=tc: e.tensor_tensor(out=t1.t[64:96, :], in0=pa.t[64:96, :], in1=CT.t[64:96, tc], op=ALU.mult),
                 reads=[pa.r, CT.r], writes=[t1.r])
            S.op("dve", lambda e, pb=pb, t2=t2, tc=tc: e.tensor_tensor(out=t2.t[64:96, :], in0=pb.t[64:96, :], in1=ST.t[64:96, tc], op=ALU.mult),
                 reads=[pb.r, ST.r], writes=[t2.r])
            S.op("pool", lambda e, t1=t1, t2=t2: e.tensor_tensor(out=t1.t[64:96, :], in0=t1.t[64:96, :], in1=t2.t[64:96, :], op=ALU.add),
                 reads=[t1.r, t2.r], writes=[t1.r])
            S.op("dve", lambda e, t1=t1, st=st, rq_=rq_: e.scalar_tensor_tensor(out=st.t[64:96, :], in0=t1.t[64:96, :], scalar=MLA_SCALE, in1=rq_.t[64:96, :],
                                                                             op0=ALU.mult, op1=ALU.mult), reads=[t1.r, rq_.r, st.r], writes=[st.r])
            store_rows(st, (0, 96), sc["QS"][H][0:96, :], sc["r_QS"][H], j)
        for h in (range(4) if "k" in _MSUB else []):
            H = 8 + h
            p = nxt("pj", pj)
            S.op("pe", lambda e, p=p, h=h, kg=kg: e.matmul(p.t[0:64, :], lhsT=wukv.t[:, h * 128:h * 128 + 64], rhs=kg.t[:], start=True, stop=True),
                 reads=[wukv.r, kg.r], writes=[p.r])
            st = nxt("stg", stg)
            S.op("dve", lambda e, p=p, st=st, rkv_=rkv_: e.tensor_tensor(out=st.t[0:64, :], in0=p.t[0:64, :], in1=rkv_.t[0:64, :], op=ALU.mult),
                 reads=[p.r, rkv_.r], writes=[st.r])
            store_rows(st, (0, 64), sc["KS"][H][0:64, :], sc["r_KS"][H], j)
        if "r" not in _MSUB:
            continue
        pa = proj_fm(w, 384, 32, 1, j)
        pb = proj_fm(wkrs, 0, 32, 1, j)
        t1, t2 = tmp1[0], tmp2[0]
        st = nxt("stg", stg)
        S.op("dve", lambda e, pa=pa, t1=t1, tc=tc: e.tensor_tensor(out=t1.t[0:32, :], in0=pa.t[0:32, :], in1=CT.t[0:32, tc], op=ALU.mult),
             reads=[pa.r, CT.r], writes=[t1.r])
        S.op("dve", lambda e, pb=pb, t2=t2, tc=tc: e.tensor_tensor(out=t2.t[0:32, :], in0=pb.t[0:32, :], in1=ST.t[0:32, tc], op=ALU.mult),
             reads=[pb.r, ST.r], writes=[t2.r])
        S.op("pool", lambda e, t1=t1, t2=t2, st=st: e.tensor_tensor(out=st.t[0:32, :], in0=t1.t[0:32, :], in1=t2.t[0:32, :], op=ALU.add),
             reads=[t1.r, t2.r], writes=[st.r])
        for h in range(4):
            store_rows(st, (0, 32), sc["KS"][8 + h][64:96, :], sc["r_KS"][8 + h], j)
        for bb in (range(4) if "v" in _MSUB else []):
            b = j * 4 + bb
            p = nxt("pv", pv)
            S.op("pe", lambda e, p=p, bb=bb, kg=kg: e.matmul(p.t[:, 0:256], lhsT=kg.t[:, bb * 128:(bb + 1) * 128], rhs=wukv_v, start=True, stop=True),
                 reads=[wukv.r, kg.r], writes=[p.r])
            S.op("pe", lambda e, p=p, bb=bb, k2=k2: e.matmul(p.t[:, 256:257], lhsT=k2.t[:, bb * 128:(bb + 1) * 128], rhs=oneskv.t[:, 0:1], start=True, stop=True),
                 reads=[oneskv.r, k2.r], writes=[p.r])
            rt = rtok[b % 2]
            S.op("act", lambda e, p=p, rt=rt: e.activation(out=rt.t[:], in_=p.t[:, 256:257], func=AF.Sqrt, bias=epst.t[:, 0:1]), reads=[p.r, epst.r], writes=[rt.r])
            S.op("dve", lambda e, rt=rt: e.reciprocal(out=rt.t[:], in_=rt.t[:]), reads=[rt.r], writes=[rt.r])
            S.op("dve", lambda e, p=p, b=b, rt=rt: e.tensor_scalar(out=vstm.t[:, b, :, 0:64], in0=p.t[:, 0:256].rearrange("p (h d) -> p h d", h=4),
                                                                  scalar1=rt.t[:, 0:1], scalar2=None, op0=ALU.mult), reads=[p.r, rt.r], writes=[vstm.r])
    for hp in range(2):
        S.dma("sp", sc["VS"][4 + hp].rearrange("p (b h d) -> p b h d", b=NB, h=2), vstm.t[:, :, 2 * hp:2 * hp + 2, :], reads=[vstm.r], writes=[sc["r_VS"][4 + hp]])

    S.flush()
    A3.close()
    if "d" not in _P1SEC:
        A.close()
        return
    for tb, nm in ((CT, "rope64c"), (ST, "rope64s")):
        S.dma("pool", tb.t[0:64, :], T[nm], writes=[tb.r])
        S.dma("pool", tb.t[64:128, :], T[nm], writes=[tb.r])
    for g in range(3):
        r = DIL_R[g]
        for hp in range(2):
            o = g * 256 + hp * 128
            w = load_w([(C_DIL + o, 128), (C_DIL + 768 + o, 128), (C_DIL + 1536 + o, 128)])
            scale_q(w)
            ws = wsw[(g * 2 + hp) % 2]
            for c in range(8):
                vo = ws.t[:, c, :].rearrange("p (h f d) -> p h f d", h=4, f=2)
                vi = w.t[:, c, 0:256].rearrange("p (h f d) -> p h f d", h=4, f=2)
                S.op("pool", lambda e, vo=vo, vi=vi: e.tensor_copy(out=vo[:, :, 0, :], in_=vi[:, :, 1, :]), reads=[w.r], writes=[ws.r])
                S.op("pool", lambda e, vo=vo, vi=vi: e.tensor_copy(out=vo[:, :, 1, :], in_=vi[:, :, 0, :]), reads=[w.r], writes=[ws.r])
            for qk, dst, rd in ((0, sc["QD"], sc["r_QD"]), (1, sc["KD"], sc["r_KD"])):
                for j in range(NT):
                    pa = proj_fm(w, qk * 128, 128, r, j)
                    pb = proj_fm(ws, qk * 128, 128, r, j)
                    t1, t2 = tmp1[j % 2], tmp2[j % 2]
                    st = nxt("stg", stg)
                    cv = perm_view(CT.t[:, :], r, j * 512, 512)
                    sv = perm_view(ST.t[:, :], r, j * 512, 512)
                    shp = None if len(cv.shape) == 2 else cv.shape

                    def v3(ap):
                        return ap if shp is None else ap.rearrange("p (a b) -> p a b", a=shp[1])
                    S.op("dve", lambda e, pa=pa, t1=t1, cv=cv, v3=v3: e.tensor_tensor(out=v3(t1.t[:]), in0=v3(pa.t[:]), in1=cv, op=ALU.mult),
                         reads=[pa.r, CT.r], writes=[t1.r])
                    S.op("dve", lambda e, pb=pb, t2=t2, sv=sv, v3=v3: e.tensor_tensor(out=v3(t2.t[:]), in0=v3(pb.t[:]), in1=sv, op=ALU.mult),
                         reads=[pb.r, ST.r], writes=[t2.r])
                    S.op("pool", lambda e, t1=t1, t2=t2, st=st: e.tensor_tensor(out=st.t[:], in0=t1.t[:], in1=t2.t[:], op=ALU.add),
                         reads=[t1.r, t2.r], writes=[st.r])
                    for hh in range(2):
                        store_rows(st, (hh * 64, hh * 64 + 64), dst[g][hp * 2 + hh], rd[g][hp * 2 + hh], j)
            vt = nxt("v", vst)
            v_proj(w, 256, r, vt)
            S.dma("sp", sc["VD"][g][hp], vt.t[:, :, :, :].rearrange("p b h d -> p (b h d)"), reads=[vt.r], writes=[sc["r_VD"][g][hp]])
    S.flush()
    A.close()


def phase2(nc, S, T, l, sc, heads=None):
    A = Alloc(nc)
    negtri = Buf(A.sb("negtri", [128, 128], BF16))
    S.op("pool", lambda e: e.memset(negtri.t[:], -1.0), writes=[negtri.r])
    S.op("pool", lambda e: e.affine_select(out=negtri.t[:], in_=negtri.t[:], pattern=[[-1, 128]], compare_op=ALU.is_ge, fill=0.0, base=0,
                                           channel_multiplier=1), reads=[negtri.r], writes=[negtri.r])
    ones = Buf(A.sb("ones", [128, 128], BF16))
    S.op("pool", lambda e: e.memset(ones.t[:], 1.0), writes=[ones.r])
    wn = Buf(A.sb("wn", [65, 64], BF16))
    wnsb = Buf(A.sb("wnsb", [65, 64], BF16))
    for t_, v_ in ((wn, RMS_EPS), (wnsb, 0.0)):
        S.op("pool", lambda e, t_=t_: e.memset(t_.t[:], 1.0 / 64.0), writes=[t_.r])
        S.op("pool", lambda e, t_=t_, v_=v_: e.memset(t_.t[64:65, :], v_), reads=[t_.r], writes=[t_.r])
    gh = Buf(A.sb("gh", [64, 16], F32))
    for h_ in range(16):
        S.dma("sp", gh.t[:, h_:h_ + 1], T["g_head"][l][h_].rearrange("(d o) -> d o", o=1), writes=[gh.r])
    eps2 = Buf(A.sb("eps2", [64, 2], F32))
    S.op("pool", lambda e: e.memset(eps2.t[:, 0:1], RMS_EPS), writes=[eps2.r])
    S.op("pool", lambda e: e.memset(eps2.t[:, 1:2], 0.0), reads=[eps2.r], writes=[eps2.r])

    Qt = rot(A, "sb", "Qt", [128, SEQ], BF16, 2)
    Kt = rot(A, "sb", "Kt", [128, SEQ], BF16, 2)
    Vt = rot(A, "sb", "Vt", [128, NB, 2, 65], BF16, 2)
    pz = rot(A, "ps", "pz", [128, 512], F32, 3)
    po = rot(A, "ps", "po", [128, 512], F32, 2)
    pc = rot(A, "ps", "pc", [128, 512], F32, 2)
    pss = rot(A, "ps", "pss", [128, 512], F32, 1)
    Pb = rot(A, "sb", "Pb", [128, 512], BF16, 4)
    eb = rot(A, "sb", "eb", [128, 512], F32, 2)
    spb = rot(A, "sb", "spb", [128, 512], BF16, 3)
    lw = rot(A, "sb", "lw", [128, 512], F32, 2)
    Rsb = Buf(A.sb("Rsb", [128, 512], F32))
    sqb = rot(A, "sb", "sqb", [65, 512], BF16, 2)
    stb = rot(A, "sb", "stb", [64, 512], F32, 2)
    yb = rot(A, "sb", "yb", [64, 512], BF16, 2)
    acc = rot(A, "sb", "acc", [65, SEQ], F32, 2)
    st = {"fin": 0, "ld": 0, "vld": 0, "o": 0}

    def finish(src_ap, r_src, h, t0, n, is_sb):
        i = st["fin"]
        st["fin"] += 1
        sq, s_, y, ps_ = sqb[i % 2], stb[i % 2], yb[i % 2], pss[0]
        S.op("act", lambda e: e.activation(out=sq.t[:, 0:n], in_=src_ap, func=AF.Square), reads=[r_src], writes=[sq.r])
        wn_ = wnsb if is_sb else wn
        S.op("pe", lambda e: e.matmul(ps_.t[0:64, 0:n], lhsT=wn_.t[:, :], rhs=sq.t[:, 0:n], start=True, stop=True), reads=[wn_.r, sq.r], writes=[ps_.r])
        S.op("act", lambda e: e.activation(out=s_.t[:, 0:n], in_=ps_.t[0:64, 0:n], func=AF.Sqrt, bias=(eps2.t[:, 0:1] if is_sb else eps2.t[:, 1:2])),
             reads=[ps_.r, eps2.r], writes=[s_.r])
        S.op("dve", lambda e: e.reciprocal(out=s_.t[:, 0:n], in_=s_.t[:, 0:n]), reads=[s_.r], writes=[s_.r])
        S.op("dve", lambda e: e.scalar_tensor_tensor(out=y.t[:, 0:n], in0=src_ap[0:64], scalar=gh.t[:, h:h + 1], in1=s_.t[:, 0:n],
                                                     op0=ALU.mult, op1=ALU.mult), reads=[r_src, gh.r, s_.r], writes=[y.r])
        S.dma("sp", sc["OnT"][h // 2, (h % 2) * 64:(h % 2) * 64 + 64, t0:t0 + n], y.t[:, 0:n], reads=[y.r], writes=[sc["r_OnT"]])

    def load_qk(qsrc, r_q, ksrc, r_k, kd):
        i = st["ld"]
        st["ld"] += 1
        q, k = Qt[i % 2], Kt[i % 2]
        S.dma("sp", q.t[0:kd, :], qsrc, reads=[r_q], writes=[q.r])
        S.dma("sp", k.t[0:kd, :], ksrc, reads=[r_k], writes=[k.r])
        return q, k

    def load_v(vsrc, r_v):
        i = st["vld"]
        st["vld"] += 1
        v = Vt[i % 2]
        S.dma("sp", v.t[:, :, :, :].rearrange("p b h d -> p (b h d)"), vsrc, reads=[r_v], writes=[v.r])
        return v

    def run_steps(steps, q, k, kd, v, hh, kind, done_cb):
        n_ = len(steps)
        ctx = [dict() for _ in range(n_)]

        def s1(i):
            sp_ = steps[i]
            z = pz[i % 3] if kind != "sb" else pz[i % 2]
            q0, n, kb = sp_["q0"], sp_["n"], sp_["kb"]
            S.op("pe", lambda e: e.matmul(z.t[:, 0:n], lhsT=k.t[0:kd, kb * 128:(kb + 1) * 128], rhs=q.t[0:kd, q0:q0 + n], start=True, stop=True),
                 reads=[k.r, q.r], writes=[z.r])
            if kind == "sb":
                e_, s_ = eb[i % 2], spb[i % 3]
                S.op("act", lambda e: e.activation(out=e_.t[:, 0:n], in_=z.t[:, 0:n], func=AF.Exp), reads=[z.r], writes=[e_.r])
                S.op("act", lambda e: e.activation(out=s_.t[:, 0:n], in_=e_.t[:, 0:n], func=AF.Ln, bias=1.0), reads=[e_.r], writes=[s_.r])
                if sp_["mA"] is not None:
                    S.op("pool", lambda e: e.affine_select(out=s_.t[:, 0:n], in_=s_.t[:, 0:n], pattern=[[1, n]], compare_op=ALU.is_ge, fill=0.0,
                                                           base=sp_["mA"], channel_multiplier=-1), reads=[s_.r], writes=[s_.r])
                ctx[i]["sp"] = s_
            else:
                p_ = Pb[i % 4]
                if kind == "fox" and sp_["mA"] is not None:
                    l_ = lw[i % 2]
                    S.op("dve", lambda e: e.tensor_scalar(out=l_.t[:, 0:n], in0=z.t[:, 0:n], scalar1=60.0, scalar2=None, op0=ALU.min), reads=[z.r], writes=[l_.r])
                    S.op("act", lambda e: e.activation(out=p_.t[:, 0:n], in_=l_.t[:, 0:n], func=AF.Exp), reads=[l_.r], writes=[p_.r])
                else:
                    S.op("act", lambda e: e.activation(out=p_.t[:, 0:n], in_=z.t[:, 0:n], func=AF.Exp), reads=[z.r], writes=[p_.r])
                if sp_["mA"] is not None:
                    S.op("pool", lambda e: e.affine_select(out=p_.t[:, 0:n], in_=p_.t[:, 0:n], pattern=[[1, n]], compare_op=ALU.is_ge, fill=0.0,
                                                           base=sp_["mA"], channel_multiplier=-1), reads=[p_.r], writes=[p_.r])
                if sp_["mB"] is not None:
                    S.op("pool", lambda e: e.affine_select(out=p_.t[:, 0:n], in_=p_.t[:, 0:n], pattern=[[-1, n]], compare_op=ALU.is_ge, fill=0.0,
                                                           base=sp_["mB"], channel_multiplier=1), reads=[p_.r], writes=[p_.r])
                ctx[i]["P"] = p_

        def s2(i):
            if kind != "sb":
                return
            sp_ = steps[i]
            q0, n, kb = sp_["q0"], sp_["n"], sp_["kb"]
            s_ = ctx[i]["sp"]
            c_, rc, l_, p_ = pc[i % 2], pz[2], lw[i % 2], Pb[i % 4]
            S.op("pe", lambda e: e.matmul(c_.t[:, 0:n], lhsT=k.t[0:kd, kb * 128:(kb + 1) * 128], rhs=q.t[0:kd, q0:q0 + n], start=True, stop=False),
                 reads=[k.r, q.r], writes=[c_.r])
            S.op("pe", lambda e: e.matmul(c_.t[:, 0:n], lhsT=negtri.t[:], rhs=s_.t[:, 0:n], start=False, stop=True), reads=[negtri.r, s_.r], writes=[c_.r])
            S.op("pe", lambda e: e.matmul(rc.t[:, 0:n], lhsT=ones.t[:], rhs=s_.t[:, 0:n], start=True, stop=True), reads=[ones.r, s_.r], writes=[rc.r])
            if sp_["first"]:
                S.op("dve", lambda e: e.tensor_copy(out=l_.t[:, 0:n], in_=c_.t[:, 0:n]), reads=[c_.r], writes=[l_.r])
                S.op("dve", lambda e: e.tensor_copy(out=Rsb.t[:, 0:n], in_=rc.t[:, 0:n]), reads=[rc.r], writes=[Rsb.r])
            else:
                S.op("dve", lambda e: e.tensor_tensor(out=l_.t[:, 0:n], in0=c_.t[:, 0:n], in1=Rsb.t[:, 0:n], op=ALU.subtract), reads=[c_.r, Rsb.r], writes=[l_.r])
                S.op("dve", lambda e: e.tensor_tensor(out=Rsb.t[:, 0:n], in0=rc.t[:, 0:n], in1=Rsb.t[:, 0:n], op=ALU.add), reads=[rc.r, Rsb.r], writes=[Rsb.r])
            S.op("act", lambda e: e.activation(out=p_.t[:, 0:n], in_=l_.t[:, 0:n], func=AF.Exp), reads=[l_.r], writes=[p_.r])
            if sp_["mA"] is not None:
                S.op("pool", lambda e: e.affine_select(out=p_.t[:, 0:n], in_=p_.t[:, 0:n], pattern=[[1, n]], compare_op=ALU.is_ge, fill=0.0,
                                                       base=sp_["mA"], channel_multiplier=-1), reads=[p_.r], writes=[p_.r])
            ctx[i]["P"] = p_

        def s3(i):
            sp_ = steps[i]
            n, kb = sp_["n"], sp_["kb"]
            if sp_["first"]:
                st["o"] += 1
            o_ = po[st["o"] % 2]
            p_ = ctx[i]["P"]
            S.op("pe", lambda e: e.matmul(o_.t[0:65, 0:n], lhsT=v.t[:, kb, hh, :], rhs=p_.t[:, 0:n], start=sp_["first"], stop=sp_["last"]),
                 reads=[v.r, p_.r], writes=[o_.r])
            if sp_["last"]:
                done_cb(o_, sp_)

        for i in range(n_ + 2):
            if i < n_:
                s1(i)
            if 0 <= i - 1 < n_:
                s2(i - 1)
            if 0 <= i - 2 < n_:
                s3(i - 2)

    def causal_steps(strict, descending):
        steps = []
        for qt in range(NT):
            q0 = qt * 512
            kbs = list(range(0, 4 * qt + 4))
            if descending:
                kbs = kbs[::-1]
            for ii, kb in enumerate(kbs):
                base = q0 - 128 * kb - (1 if strict else 0)
                steps.append(dict(q0=q0, n=512, kb=kb, mA=(base if base - 127 < 0 else None), mB=None, first=(ii == 0), last=(ii == len(kbs) - 1)))
        return steps

    def dil_steps(r):
        sc_ = SEQ // r
        n = min(512, sc_)
        steps = []
        for q0 in range(0, SEQ, n):
            cs = (q0 // sc_) * sc_
            k_lo = max(cs, q0 - 128)
            kbs = list(range(k_lo // 128, (q0 + n) // 128))
            for ii, kb in enumerate(kbs):
                bA = q0 - 128 * kb
                bB = 128 + 128 * kb - q0
                steps.append(dict(q0=q0, n=n, kb=kb, mA=(bA if bA - 127 < 0 else None), mB=(bB if bB - (n - 1) < 0 else None),
                                  first=(ii == 0), last=(ii == len(kbs) - 1)))
        return steps

    hsel = (lambda h: True) if heads is None else (lambda h: h in heads)
    for kind, hbase, pbase, kd in (("sb", 0, 0, 64), ("fox", 4, 2, 70), ("mla", 8, 4, 96)):
        steps = causal_steps(strict=(kind == "sb"), descending=(kind == "sb"))
        for hp in range(2):
            if not (hsel(hbase + 2 * hp) or hsel(hbase + 2 * hp + 1)):
                continue
            v = load_v(sc["VS"][pbase + hp], sc["r_VS"][pbase + hp])
            for hh in range(2):
                h = hbase + hp * 2 + hh
                if not hsel(h):
                    continue
                q, k = load_qk(sc["QS"][h][0:kd, :], sc["r_QS"][h], sc["KS"][h][0:kd, :], sc["r_KS"][h], kd)

                def done(o_, sp_, h=h, kind=kind):
                    finish(o_.t[0:65, 0:sp_["n"]], o_.r, h, sp_["q0"], sp_["n"], kind == "sb")
                run_steps(steps, q, k, kd, v, hh, kind, done)
    for hp in range(2):
        if not (hsel(12 + 2 * hp) or hsel(13 + 2 * hp)):
            continue
        for g in range(3):
            r = DIL_R[g]
            steps = dil_steps(r)
            v = load_v(sc["VD"][g][hp], sc["r_VD"][g][hp])
            for hh in range(2):
                hd = hp * 2 + hh
                q, k = load_qk(sc["QD"][g][hd], sc["r_QD"][g][hd], sc["KD"][g][hd], sc["r_KD"][g][hd], 64)
                a_ = acc[hh]

                def done(o_, sp_, a_=a_, r=r, g=g):
                    n, q0 = sp_["n"], sp_["q0"]
                    dst = perm_view(a_.t[:, :], r, q0, n)
                    if g == 0:
                        S.op("act", lambda e: e.activation(out=dst, in_=o_.t[0:65, 0:n], func=AF.Copy), reads=[o_.r], writes=[a_.r])
                    else:
                        S.op("dve", lambda e: e.tensor_tensor(out=dst, in0=o_.t[0:65, 0:n], in1=dst, op=ALU.add), reads=[o_.r, a_.r], writes=[a_.r])
                run_steps(steps, q, k, 64, v, hh, "dil", done)
        for hh in range(2):
            for qt in range(NT):
                finish(acc[hh].t[:, qt * 512:(qt + 1) * 512], acc[hh].r, 12 + hp * 2 + hh, qt * 512, 512, False)
    S.flush()
    A.close()


def layernorm_block(S, y, g_b, b_b, small, out):
    st6, mv, rs = small["st6"], small["mv"], small["rs"]
    for hf in range(2):
        S.op("dve", lambda e, hf=hf: e.bn_stats(out=st6.t[:, hf, :], in_=y.t[:, hf * 512:(hf + 1) * 512]), reads=[y.r], writes=[st6.r])
    S.op("dve", lambda e: e.bn_aggr(out=mv.t[:], in_=st6.t[:, :, :].rearrange("p a b -> p (a b)")), reads=[st6.r], writes=[mv.r])
    S.op("act", lambda e: e.activation(out=rs.t[:], in_=mv.t[:, 1:2], func=AF.Sqrt, bias=small["eps"].t[:, 0:1]), reads=[mv.r, small["eps"].r], writes=[rs.r])
    S.op("dve", lambda e: e.reciprocal(out=rs.t[:], in_=rs.t[:]), reads=[rs.r], writes=[rs.r])
    S.op("dve", lambda e: e.tensor_scalar(out=y.t[:], in0=y.t[:], scalar1=mv.t[:, 0:1], scalar2=rs.t[:, 0:1], op0=ALU.subtract, op1=ALU.mult),
         reads=[y.r, mv.r, rs.r], writes=[y.r])
    S.op("pool", lambda e: e.tensor_tensor(out=y.t[:], in0=y.t[:], in1=g_b.t[:], op=ALU.mult), reads=[y.r, g_b.r], writes=[y.r])
    S.op("pool", lambda e: e.tensor_tensor(out=out.t[:], in0=y.t[:], in1=b_b.t[:], op=ALU.add), reads=[y.r, b_b.r], writes=[out.r])


def bcast_row(S, A, name, src1d, n):
    b = Buf(A.sb(name, [128, n], F32))
    S.dma("sp", b.t[:], src1d.rearrange("(o n) -> o n", o=1).partition_broadcast(128), writes=[b.r])
    return b


def phase3(nc, S, T, xin, r_xin, l, sc):
    A = Alloc(nc)
    identf = make_ident(A, S, F32)
    wout = Buf(A.sb("wout", [128, 8, DM], BF16))
    S.dma("pool", wout.t[:], T["w_out"][l].rearrange("(c p) n -> p c n", p=128), writes=[wout.r])
    wr = Buf(A.sb("wr", [128, 8, 20], F32))
    S.dma("sp", wr.t[:, :, 0:4], T["w_group"][l].rearrange("(c p) n -> p c n", p=128), writes=[wr.r])
    S.dma("sp", wr.t[:, :, 4:20], T["w_expert"][l].rearrange("(c p) n -> p c n", p=128), writes=[wr.r])
    brt = Buf(A.sb("brt", [128, 20], F32))
    S.dma("sp", brt.t[:, 0:4], T["b_group"][l].rearrange("(o n) -> o n", o=1).partition_broadcast(128), writes=[brt.r])
    S.dma("sp", brt.t[:, 4:20], T["b_expert"][l].rearrange("(o n) -> o n", o=1).partition_broadcast(128), writes=[brt.r])
    g_b = bcast_row(S, A, "ln1g", T["ln1_g"][l], DM)
    b_b = bcast_row(S, A, "ln1b", T["ln1_b"][l], DM)
    on = rot(A, "sb", "on", [128, 8, 512], BF16, 2)
    xs = rot(A, "sb", "xs3", [128, DM], F32, 2)
    y = rot(A, "sb", "y3", [128, DM], F32, 2)
    x1 = rot(A, "sb", "x1o", [128, DM], F32, 2)
    xtf = rot(A, "sb", "xtf", [128, 8, 128], F32, 2)
    xtb = rot(A, "sb", "xtb", [128, 8, 512], BF16, 2)
    gate = Buf(A.sb("gate", [128, NB, 16], F32))
    ph = rot(A, "ps", "ph", [128, DM], F32, 2)
    ptr = rot(A, "ps", "ptr", [128, DM], F32, 1)
    plg = rot(A, "ps", "plg", [128, 512], F32, 2)
    epsl = Buf(A.sb("epsl", [128, 1], F32))
    S.op("pool", lambda e: e.memset(epsl.t[:], LN_EPS), writes=[epsl.r])
    small = [dict(st6=Buf(A.sb("st6", [128, 2, 6], F32)), mv=Buf(A.sb("mv", [128, 2], F32)), rs=Buf(A.sb("rs", [128, 1], F32)), eps=epsl) for _ in range(2)]
    gt = [{k: Buf(A.sb("g_" + k, shp, F32)) for k, shp in (("lg", [128, 20]), ("m", [128, 1]), ("goh", [128, 4]), ("ex", [128, 4]), ("se", [128, 1]),
                                                           ("t44", [128, 4, 4]), ("es", [128, 4]), ("m1", [128, 1]), ("oh1", [128, 4]), ("es2", [128, 4]),
                                                           ("m2", [128, 1]), ("oh2", [128, 4]), ("d", [128, 1]), ("p1", [128, 1]), ("p2", [128, 1]),
                                                           ("gi", [128, 4]), ("nm", [128, 1]))} for _ in range(2)]
    for j in range(NT):
        o_ = on[j % 2]
        S.dma("sp", o_.t[:], sc["OnT"][:, :, j * 512:(j + 1) * 512].rearrange("c p t -> p c t"), reads=[sc["r_OnT"]], writes=[o_.r])
        xb_ = xtb[j % 2]
        for bb in range(4):
            b = j * 4 + bb
            s_, y_, x1_, xf_, p_, sm, G = xs[b % 2], y[b % 2], x1[b % 2], xtf[b % 2], ph[b % 2], small[b % 2], gt[b % 2]
            S.dma("sp", s_.t[:], xin[b * 128:(b + 1) * 128, :], reads=[r_xin], writes=[s_.r])
            for hf in range(2):
                for c in range(8):
                    S.op("pe", lambda e, hf=hf, c=c, bb=bb, o_=o_, p_=p_: e.matmul(p_.t[:, hf * 512:(hf + 1) * 512], lhsT=o_.t[:, c, bb * 128:(bb + 1) * 128],
                                                                            rhs=wout.t[:, c, hf * 512:(hf + 1) * 512], start=(c == 0), stop=(c == 7)),
                         reads=[o_.r, wout.r], writes=[p_.r])
            S.op("dve", lambda e, s_=s_, y_=y_, p_=p_: e.scalar_tensor_tensor(out=y_.t[:], in0=s_.t[:], scalar=ALPHA, in1=p_.t[:], op0=ALU.mult, op1=ALU.add),
                 reads=[s_.r, p_.r], writes=[y_.r])
            layernorm_block(S, y_, g_b, b_b, sm, x1_)
            S.dma("sp", sc["X1"][b * 128:(b + 1) * 128, :], x1_.t[:], reads=[x1_.r], writes=[sc["r_X1"]])
            pt = ptr[0]
            for c in range(8):
                S.op("pe", lambda e, c=c, x1_=x1_, pt=pt: e.transpose(out=pt.t[:, c * 128:(c + 1) * 128], in_=x1_.t[:, c * 128:(c + 1) * 128], identity=identf.t[:]),
                     reads=[x1_.r, identf.r], writes=[pt.r])
            S.op("act", lambda e, pt=pt, xf_=xf_: e.activation(out=xf_.t[:, :, :], in_=pt.t[:, :].rearrange("p (c t) -> p c t", c=8), func=AF.Copy),
                 reads=[pt.r], writes=[xf_.r])
            S.op("dve", lambda e, pt=pt, xb_=xb_, bb=bb: e.tensor_copy(out=xb_.t[:, :, bb * 128:(bb + 1) * 128], in_=pt.t[:, :].rearrange("p (c t) -> p c t", c=8)),
                 reads=[pt.r], writes=[xb_.r])
            pl = plg[b % 2]
            for c in range(8):
                S.op("pe", lambda e, c=c, xf_=xf_, pl=pl: e.matmul(pl.t[:, 0:20], lhsT=xf_.t[:, c, :], rhs=wr.t[:, c, :], start=(c == 0), stop=(c == 7)),
                     reads=[xf_.r, wr.r], writes=[pl.r])
            def V(name, fn, reads, writes):
                S.op("dve", fn, reads=[G[k].r if isinstance(k, str) else k for k in reads], writes=[G[k].r if isinstance(k, str) else k for k in writes])
            lg = G["lg"].t
            V("lg", lambda e, pl=pl, lg=lg: e.tensor_tensor(out=lg[:], in0=pl.t[:, 0:20], in1=brt.t[:], op=ALU.add), [pl.r, brt.r], ["lg"])
            V("m", lambda e, G=G, lg=lg: e.tensor_reduce(out=G["m"].t[:], in_=lg[:, 0:4], axis=AX.X, op=ALU.max), ["lg"], ["m"])
            V("goh", lambda e, G=G, lg=lg: e.tensor_scalar(out=G["goh"].t[:], in0=lg[:, 0:4], scalar1=G["m"].t[:, 0:1], scalar2=None, op0=ALU.is_equal), ["lg", "m"], ["goh"])
            V("nm", lambda e, G=G: e.tensor_scalar(out=G["nm"].t[:], in0=G["m"].t[:], scalar1=-1.0, scalar2=None, op0=ALU.mult), ["m"], ["nm"])
            S.op("act", lambda e, G=G, lg=lg: e.activation(out=G["ex"].t[:], in_=lg[:, 0:4], func=AF.Exp, bias=G["nm"].t[:, 0:1]), reads=[G["lg"].r, G["nm"].r], writes=[G["ex"].r])
            V("se", lambda e, G=G: e.tensor_reduce(out=G["se"].t[:], in_=G["ex"].t[:], axis=AX.X, op=ALU.add), ["ex"], ["se"])
            V("se2", lambda e, G=G: e.reciprocal(out=G["se"].t[:], in_=G["se"].t[:]), ["se"], ["se"])
            V("t44", lambda e, G=G, lg=lg: e.tensor_tensor(out=G["t44"].t[:], in0=lg[:, 4:20].rearrange("p (g x) -> p g x", g=4),
                                                         in1=G["goh"].t[:, :].unsqueeze(2).to_broadcast([128, 4, 4]), op=ALU.mult), ["lg", "goh"], ["t44"])
            V("es", lambda e, G=G: e.tensor_reduce(out=G["es"].t[:], in_=G["t44"].t[:, :, :].rearrange("p g x -> p x g"), axis=AX.X, op=ALU.add), ["t44"], ["es"])
            V("m1", lambda e, G=G: e.tensor_reduce(out=G["m1"].t[:], in_=G["es"].t[:], axis=AX.X, op=ALU.max), ["es"], ["m1"])
            V("oh1", lambda e, G=G: e.tensor_scalar(out=G["oh1"].t[:], in0=G["es"].t[:], scalar1=G["m1"].t[:, 0:1], scalar2=None, op0=ALU.is_equal), ["es", "m1"], ["oh1"])
            V("es2", lambda e, G=G: e.scalar_tensor_tensor(out=G["es2"].t[:], in0=G["oh1"].t[:], scalar=-1e30, in1=G["es"].t[:], op0=ALU.mult, op1=ALU.add),
              ["oh1", "es"], ["es2"])
            V("m2", lambda e, G=G: e.tensor_reduce(out=G["m2"].t[:], in_=G["es2"].t[:], axis=AX.X, op=ALU.max), ["es2"], ["m2"])
            V("oh2", lambda e, G=G: e.tensor_scalar(out=G["oh2"].t[:], in0=G["es2"].t[:], scalar1=G["m2"].t[:, 0:1], scalar2=None, op0=ALU.is_equal), ["es2", "m2"], ["oh2"])
            V("d", lambda e, G=G: e.tensor_tensor(out=G["d"].t[:], in0=G["m2"].t[:], in1=G["m1"].t[:], op=ALU.subtract), ["m1", "m2"], ["d"])
            S.op("act", lambda e, G=G: e.activation(out=G["d"].t[:], in_=G["d"].t[:], func=AF.Exp), reads=[G["d"].r], writes=[G["d"].r])
            V("p1", lambda e, G=G: e.tensor_scalar(out=G["p1"].t[:], in0=G["d"].t[:], scalar1=1.0, scalar2=None, op0=ALU.add), ["d"], ["p1"])
            V("p1r", lambda e, G=G: e.reciprocal(out=G["p1"].t[:], in_=G["p1"].t[:]), ["p1"], ["p1"])
            V("p2", lambda e, G=G: e.tensor_tensor(out=G["p2"].t[:], in0=G["d"].t[:], in1=G["p1"].t[:], op=ALU.mult), ["d", "p1"], ["p2"])
            V("gi", lambda e, G=G: e.tensor_scalar(out=G["gi"].t[:], in0=G["oh1"].t[:], scalar1=G["p1"].t[:, 0:1], scalar2=None, op0=ALU.mult), ["oh1", "p1"], ["gi"])
            V("gi2", lambda e, G=G: e.scalar_tensor_tensor(out=G["gi"].t[:], in0=G["oh2"].t[:], scalar=G["p2"].t[:, 0:1], in1=G["gi"].t[:], op0=ALU.mult, op1=ALU.add),
              ["oh2", "p2", "gi"], ["gi"])
            V("gi3", lambda e, G=G: e.tensor_scalar(out=G["gi"].t[:], in0=G["gi"].t[:], scalar1=G["se"].t[:, 0:1], scalar2=None, op0=ALU.mult), ["gi", "se"], ["gi"])
            V("gate", lambda e, G=G, b=b: e.tensor_tensor(out=gate.t[:, b, :].rearrange("p (g x) -> p g x", g=4),
                                                        in0=G["goh"].t[:, :].unsqueeze(2).to_broadcast([128, 4, 4]),
                                                        in1=G["gi"].t[:, :].unsqueeze(1).to_broadcast([128, 4, 4]), op=ALU.mult), ["goh", "gi"], [gate.r])
        S.dma("sp", sc["X1T"][:, :, j * 512:(j + 1) * 512], xb_.t[:], reads=[xb_.r], writes=[sc["r_X1T"]])
    S.dma("sp", sc["GATE"], gate.t[:, :, :].rearrange("p b e -> p (b e)"), reads=[gate.r], writes=[sc["r_GATE"]])
    S.flush()
    A.close()


def phase4(nc, S, T, l, sc, xout, r_xout, final, TG=1024):
    A = Alloc(nc)
    g_b = bcast_row(S, A, "ln2g", T["ln2_g"][l], DM)
    b_b = bcast_row(S, A, "ln2b", T["ln2_b"][l], DM)
    gate = Buf(A.sb("gate4", [128, NB, 16], F32))
    S.dma("sp", gate.t[:, :, :].rearrange("p b e -> p (b e)"), sc["GATE"], reads=[sc["r_GATE"]], writes=[gate.r])
    xT = Buf(A.sb("x1T", [128, 8, TG], BF16))
    accb = Buf(A.sb("accm", [128, TG // 128, DM], F32))
    w1 = rot(A, "sb", "w1", [128, 8, DEXP], BF16, 2)
    w3 = rot(A, "sb", "w3", [128, 8, DEXP], BF16, 2)
    w2 = rot(A, "sb", "w2", [128, 4, DM], BF16, 2)
    sa = rot(A, "sb", "sa", [128, 512], F32, 2)
    hT = rot(A, "sb", "hT", [128, 4, 512], BF16, 2)
    xs = rot(A, "sb", "xs4", [128, DM], F32, 2)
    yo = rot(A, "sb", "yo4", [128, DM], F32, 2)
    epsl = Buf(A.sb("epsl4", [128, 1], F32))
    S.op("pool", lambda e: e.memset(epsl.t[:], LN_EPS), writes=[epsl.r])
    small = [dict(st6=Buf(A.sb("st6b", [128, 2, 6], F32)), mv=Buf(A.sb("mvb", [128, 2], F32)), rs=Buf(A.sb("rsb", [128, 1], F32)), eps=epsl) for _ in range(2)]
    pa = rot(A, "ps", "pa", [128, 512], F32, 2)
    pb = rot(A, "ps", "pb", [128, 512], F32, 2)
    py = rot(A, "ps", "py", [128, 512], F32, 3)
    cnt = {"y": 0, "ab": 0, "w": 0}
    W1 = T["w1"][l]
    W3 = T["w3"][l]
    W2 = T["w2"][l]
    for gi in range(SEQ // TG):
        t0 = gi * TG
        S.dma("sp", xT.t[:], sc["X1T"][:, :, t0:t0 + TG], reads=[sc["r_X1T"]], writes=[xT.r])
        for ex in range(NEXP):
            i = cnt["w"]
            cnt["w"] += 1
            a1, a3, a2 = w1[i % 2], w3[i % 2], w2[i % 2]
            S.dma("pool", a1.t[:], W1[ex].rearrange("(c p) n -> p c n", p=128), writes=[a1.r])
            S.dma("pool", a3.t[:], W3[ex].rearrange("(c p) n -> p c n", p=128), writes=[a3.r])
            S.dma("pool", a2.t[:], W2[ex].rearrange("(c p) n -> p c n", p=128), writes=[a2.r])
            for tt in range(TG // 512):
                tc = slice(tt * 512, (tt + 1) * 512)
                h_ = hT[(ex * (TG // 512) + tt) % 2]
                for jc in range(4):
                    k_ = cnt["ab"]
                    cnt["ab"] += 1
                    pa_, pb_, sa_ = pa[k_ % 2], pb[k_ % 2], sa[k_ % 2]
                    for c in range(8):
                        S.op("pe", lambda e, c=c, jc=jc, pa_=pa_, a1=a1, tc=tc: e.matmul(pa_.t[:], lhsT=a1.t[:, c, jc * 128:(jc + 1) * 128], rhs=xT.t[:, c, tc],
                                                                                start=(c == 0), stop=(c == 7)), reads=[a1.r, xT.r], writes=[pa_.r])
                    for c in range(8):
                        S.op("pe", lambda e, c=c, jc=jc, pb_=pb_, a3=a3, tc=tc: e.matmul(pb_.t[:], lhsT=a3.t[:, c, jc * 128:(jc + 1) * 128], rhs=xT.t[:, c, tc],
                                                                                start=(c == 0), stop=(c == 7)), reads=[a3.r, xT.r], writes=[pb_.r])
                    S.op("act", lambda e, pa_=pa_, sa_=sa_: e.activation(out=sa_.t[:], in_=pa_.t[:], func=AF.Silu), reads=[pa_.r], writes=[sa_.r])
                    S.op("dve", lambda e, pb_=pb_, sa_=sa_, h_=h_, jc=jc: e.tensor_tensor(out=h_.t[:, jc, :], in0=pb_.t[:], in1=sa_.t[:], op=ALU.mult),
                         reads=[pb_.r, sa_.r], writes=[h_.r])
                for tb in range(4):
                    blk = tt * 4 + tb
                    gb = (t0 // 128) + blk
                    for hf in range(2):
                        y_ = py[cnt["y"] % 3]
                        cnt["y"] += 1
                        for jc in range(4):
                            S.op("pe", lambda e, jc=jc, tb=tb, hf=hf, y_=y_, h_=h_, a2=a2: e.matmul(y_.t[:], lhsT=h_.t[:, jc, tb * 128:(tb + 1) * 128],
                                                                                           rhs=a2.t[:, jc, hf * 512:(hf + 1) * 512], start=(jc == 0), stop=(jc == 3)),
                                 reads=[h_.r, a2.r], writes=[y_.r])
                        dst = accb.t[:, blk, hf * 512:(hf + 1) * 512]
                        if ex == 0:
                            S.op("dve", lambda e, y_=y_, dst=dst, gb=gb, ex=ex: e.tensor_scalar(out=dst, in0=y_.t[:], scalar1=gate.t[:, gb, ex:ex + 1], scalar2=None, op0=ALU.mult),
                                 reads=[y_.r, gate.r], writes=[accb.r])
                        else:
                            S.op("dve", lambda e, y_=y_, dst=dst, gb=gb, ex=ex: e.scalar_tensor_tensor(out=dst, in0=y_.t[:], scalar=gate.t[:, gb, ex:ex + 1], in1=dst,
                                                                                              op0=ALU.mult, op1=ALU.add), reads=[y_.r, gate.r, accb.r], writes=[accb.r])
        for blk in range(TG // 128):
            gb = (t0 // 128) + blk
            s_, y_, sm = xs[blk % 2], yo[blk % 2], small[blk % 2]
            S.dma("sp", s_.t[:], sc["X1"][gb * 128:(gb + 1) * 128, :], reads=[sc["r_X1"]], writes=[s_.r])
            S.op("dve", lambda e, s_=s_, blk=blk: e.scalar_tensor_tensor(out=s_.t[:], in0=s_.t[:], scalar=ALPHA, in1=accb.t[:, blk, :], op0=ALU.mult, op1=ALU.add),
                 reads=[s_.r, accb.r], writes=[s_.r])
            layernorm_block(S, s_, g_b, b_b, sm, y_)
            S.dma("sp", xout[gb * 128:(gb + 1) * 128, :], y_.t[:], reads=[y_.r], writes=[r_xout], final=final)
    S.flush()
    A.close()


def rope_tables():
    pos = np.arange(SEQ, dtype=np.float32)
    out = {}
    for dim, nm in ((64, "rope64"), (32, "rope32")):
        half = dim // 2
        inv = (10000.0 ** (-np.arange(half, dtype=np.float32) / half)).astype(np.float32)
        ang = pos[None, :] * inv[:, None]
        c = np.cos(ang).astype(np.float32)
        s = np.sin(ang).astype(np.float32)
        out[nm + "c"] = np.ascontiguousarray(np.concatenate([c, c], 0))
        out[nm + "s"] = np.ascontiguousarray(np.concatenate([-s, s], 0))
    return out


W_SPECS = [("w_in", [DEPTH, DM, N_IN]), ("b_forget", [DEPTH, 4]), ("g_cq", [DEPTH, 256]), ("g_ckv", [DEPTH, 128]), ("w_uq", [DEPTH, 256, 384]),
           ("w_ukv", [DEPTH, 128, 512]), ("g_head", [DEPTH, 16, 64]), ("w_out", [DEPTH, DM, DM]), ("ln1_g", [DEPTH, DM]), ("ln1_b", [DEPTH, DM]),
           ("w_group", [DEPTH, DM, 4]), ("b_group", [DEPTH, 4]), ("w_expert", [DEPTH, DM, 16]), ("b_expert", [DEPTH, 16]),
           ("w1", [DEPTH, NEXP, DM, DEXP]), ("w3", [DEPTH, NEXP, DM, DEXP]), ("w2", [DEPTH, NEXP, DEXP, DM]), ("ln2_g", [DEPTH, DM]), ("ln2_b", [DEPTH, DM])]


def build_program(nseq=2, layers=(0, 1), phases=(1, 2, 3, 4), debug=False, heads=None, TG=1024):
    nc = bass.Bass("TRN2", target_bir_lowering=False)
    T = {}
    T["x"] = nc.dram_tensor("x", [nseq, SEQ, DM], F32, kind="ExternalInput").ap()
    for nm, shp in W_SPECS:
        T[nm] = nc.dram_tensor(nm, shp, F32, kind="ExternalInput").ap()
    for nm, rows in (("rope64c", 64), ("rope64s", 64), ("rope32c", 32), ("rope32s", 32)):
        T[nm] = nc.dram_tensor(nm, [rows, SEQ], F32, kind="ExternalInput").ap()
    out = nc.dram_tensor("out", [nseq, SEQ, DM], F32, kind="ExternalOutput").ap()
    dk = "ExternalOutput" if debug else "Internal"

    def scratch(name, shape, dt):
        return nc.dram_tensor(name, shape, dt, kind=dk).ap()
    sc = {}
    qs = scratch("QS", [12, 96, SEQ], BF16)
    ks = scratch("KS", [12, 96, SEQ], BF16)
    sc["QS"] = [qs[h] for h in range(12)]
    sc["KS"] = [ks[h] for h in range(12)]
    vs = scratch("VS", [6, 128, NB * 2 * 65], BF16)
    sc["VS"] = [vs[p] for p in range(6)]
    qd = scratch("QD", [3, 4, 64, SEQ], BF16)
    kd = scratch("KD", [3, 4, 64, SEQ], BF16)
    sc["QD"] = [[qd[g, h] for h in range(4)] for g in range(3)]
    sc["KD"] = [[kd[g, h] for h in range(4)] for g in range(3)]
    vd = scratch("VD", [3, 2, 128, NB * 2 * 65], BF16)
    sc["VD"] = [[vd[g, p] for p in range(2)] for g in range(3)]
    sc["OnT"] = scratch("OnT", [8, 128, SEQ], BF16)
    sc["X1"] = scratch("X1", [SEQ, DM], F32)
    sc["X1T"] = scratch("X1T", [128, 8, SEQ], BF16)
    sc["GATE"] = scratch("GATE", [128, NB * 16], F32)
    xmid = scratch("XMID", [SEQ, DM], F32)
    sc["r_QS"] = [Res() for _ in range(12)]
    sc["r_KS"] = [Res() for _ in range(12)]
    sc["r_VS"] = [Res() for _ in range(6)]
    sc["r_QD"] = [[Res() for _ in range(4)] for _ in range(3)]
    sc["r_KD"] = [[Res() for _ in range(4)] for _ in range(3)]
    sc["r_VD"] = [[Res() for _ in range(2)] for _ in range(3)]
    for k in ("OnT", "X1", "X1T", "GATE"):
        sc["r_" + k] = Res()
    r_xmid = Res()
    r_x = Res()
    r_out = Res()
    S = Sched(nc)
    for s in range(nseq):
        for li, l in enumerate(layers):
            xin, r_xin = (T["x"][s], r_x) if li == 0 else (xmid, r_xmid)
            last = li == len(layers) - 1
            xo, r_xo = (out[s], r_out) if last else (xmid, r_xmid)
            if 1 in phases:
                phase1(nc, S, T, xin, r_xin, l, sc)
            if 2 in phases:
                phase2(nc, S, T, l, sc, heads=heads)
            if 3 in phases:
                phase3(nc, S, T, xin, r_xin, l, sc)
            if 4 in phases:
                phase4(nc, S, T, l, sc, xo, r_xo, final=last, TG=TG)
    S.close()
    return nc, S


_CACHE = {}


def kernel(**inputs):
    n = 8
    nseq = 2
    x = np.ascontiguousarray(np.asarray(inputs["x"], dtype=np.float32))
    tabs = rope_tables()
    if "nc" not in _CACHE:
        _CACHE["nc"] = build_program(nseq=nseq)[0]
    nc = _CACHE["nc"]
    base = {nm: np.ascontiguousarray(np.asarray(inputs[nm], dtype=np.float32)) for nm, _ in W_SPECS}
    base.update(tabs)
    in_maps = []
    for c in range(n):
        m = dict(base)
        m["x"] = x[c * nseq:(c + 1) * nseq]
        in_maps.append(m)
    res = run_bass_kernel_spmd(nc, in_maps, core_ids=list(range(n)))
    return np.concatenate([r["out"] for r in res.results], axis=0).astype(np.float32)
```

```python
import numpy as np
from os import environ as _os_env
import concourse.bass as bass
import concourse.mybir as mybir
from concourse.bass_utils import run_bass_kernel_spmd

F32 = mybir.dt.float32
BF16 = mybir.dt.bfloat16
I32 = mybir.dt.int32
AF = mybir.ActivationFunctionType
ALU = mybir.AluOpType
AX = mybir.AxisListType

SAME_ENGINE_SYNC = bool(int(_os_env.get("SES", "1")))
NDMA_SLOTS = 6

SEQ = 4096
DM = 1024
NB = SEQ // 128
NT = SEQ // 512
DEPTH = 2
ALPHA = (2.0 * DEPTH) ** 0.25
LN_EPS = 1e-5
RMS_EPS = 1e-6
N_IN = 4260
C_SB, C_FOX, C_MLA, C_DIL = 0, 768, 1540, 1956
MLA_SCALE = 96.0 ** -0.5
DIL_R = (1, 4, 16)
NEXP = 16
DEXP = 512
NTILE = 11
NSLOT = NTILE * 512
ROUTED = bool(int(_os_env.get("ROUTED", "1")))


class Res:
    __slots__ = ("w", "r", "excl")

    def __init__(self, excl=False):
        self.w = None
        self.r = []
        self.excl = excl


class Sched:
    ENG = ("pe", "act", "dve", "pool", "sp")

    def __init__(self, nc):
        self.nc = nc
        self.ops = {e: [] for e in self.ENG}
        self.cnt = {e: 0 for e in self.ENG}
        self.seen = {e: {} for e in self.ENG}
        self.sems = {}
        self.dma_slots = {}
        self.dma_rr = {}
        self.final_waits = []
        self._stack = []
        self.nops = 0

    def sem(self, key):
        if key not in self.sems:
            cm = self.nc.semaphore("s_" + "_".join(str(k) for k in (key if isinstance(key, tuple) else (key,))))
            s = cm.__enter__()
            self._stack.append(cm)
            self.sems[key] = s
        return self.sems[key]

    def _deps(self, eng, reads, writes):
        deps = {}

        def add(t):
            if t is None:
                return
            k, v = t
            if deps.get(k, 0) < v:
                deps[k] = v
        for r in reads:
            add(r.w)
            if r.excl:
                for t in r.r:
                    if t[0] != eng:
                        add(t)
        for w in writes:
            add(w.w)
            for t in w.r:
                add(t)
        waits = []
        seen = self.seen[eng]
        for k, v in deps.items():
            if k == eng and (eng == "pe" or not SAME_ENGINE_SYNC):
                continue
            if seen.get(k, 0) >= v:
                continue
            seen[k] = v
            waits.append((k, v))
        return waits

    def _commit(self, ticket, reads, writes):
        for r in reads:
            if len(r.r) > 16:
                m = {}
                for k, v in r.r:
                    if m.get(k, 0) < v:
                        m[k] = v
                r.r = list(m.items())
            r.r.append(ticket)
        for w in writes:
            w.w = ticket
            w.r = []

    def op(self, eng, fn, reads=(), writes=()):
        waits = self._deps(eng, reads, writes)
        self.cnt[eng] += 1
        ticket = (eng, self.cnt[eng])
        self.ops[eng].append((waits, fn, (eng, 1)))
        self._commit(ticket, reads, writes)
        self.nops += 1
        return ticket

    def dma(self, q, out, in_, reads=(), writes=(), final=False, **kw):
        fn = lambda e, out=out, in_=in_, kw=kw: e.dma_start(out=out, in_=in_, **kw)
        return self.dma_fn(q, fn, reads, writes, final)

    def dma_fn(self, q, fn, reads=(), writes=(), final=False):
        waits = self._deps(q, reads, writes)
        if q not in self.dma_slots:
            self.dma_slots[q] = [[("d", q, i), 0] for i in range(NDMA_SLOTS)]
            self.dma_rr[q] = 0
        i = self.dma_rr[q]
        self.dma_rr[q] = (i + 1) % NDMA_SLOTS
        slot = self.dma_slots[q][i]
        key, tot = slot
        if tot > 0 and self.seen[q].get(key, 0) < tot:
            self.seen[q][key] = tot
            waits.append((key, tot))
        slot[1] = tot + 16
        ticket = (key, tot + 16)
        self.ops[q].append((waits, fn, (key, 16)))
        self._commit(ticket, reads, writes)
        self.nops += 1
        if final:
            self.final_waits.append(ticket)
        return ticket

    def flush(self):
        totals = [(e, self.cnt[e]) for e in self.ENG if self.cnt[e] > 0]
        for q, slots in self.dma_slots.items():
            for key, tot in slots:
                if tot > 0:
                    totals.append((key, tot))
        for e in self.ENG:
            self.sem(e)
            for waits, fn, inc in self.ops[e]:
                for k, v in waits:
                    self.sem(k)
                self.sem(inc[0])
        ops = self.ops
        self.ops = {e: [] for e in self.ENG}
        for e in self.ENG:
            for k, v in totals:
                self.seen[e][k] = max(self.seen[e].get(k, 0), v)
        with self.nc.Block() as block:
            def run(engname):
                def body(e):
                    for waits, fn, inc in ops[engname]:
                        for k, v in waits:
                            e.wait_ge(self.sems[k], v)
                        fn(e).then_inc(self.sems[inc[0]], inc[1])
                    for k, v in totals:
                        e.wait_ge(self.sems[k], v)
                return body
            block.sync(run("sp"))
            block.tensor(run("pe"))
            block.scalar(run("act"))
            block.vector(run("dve"))
            block.gpsimd(run("pool"))

    def close(self):
        for cm in reversed(self._stack):
            cm.__exit__(None, None, None)
        self._stack = []


_UID = [0]


class Alloc:
    def __init__(self, nc):
        self.nc = nc
        self.stack = []

    @property
    def n(self):
        _UID[0] += 1
        return _UID[0]

    def sb(self, name, shape, dt):
        cm = self.nc.sbuf_tensor("%s_%d" % (name, self.n), list(shape), dt)
        t = cm.__enter__()
        self.stack.append(cm)
        return t

    def ps(self, name, shape, dt):
        cm = self.nc.psum_tensor("%s_%d" % (name, self.n), list(shape), dt)
        t = cm.__enter__()
        self.stack.append(cm)
        return t

    def close(self):
        for cm in reversed(self.stack):
            cm.__exit__(None, None, None)
        self.stack = []


class Buf:
    def __init__(self, t, excl=False):
        self.t = t
        self.r = Res(excl)


def rot(A, kind, name, shape, dt, n):
    f = A.sb if kind == "sb" else A.ps
    return [Buf(f(name + str(i), shape, dt), excl=(kind == "ps")) for i in range(n)]


def make_ident(A, S, dt):
    b = Buf(A.sb("ident", [128, 128], dt))
    S.op("pool", lambda e: e.memset(b.t[:], 1.0), writes=[b.r])
    S.op("pool", lambda e: e.affine_select(out=b.t[:], in_=b.t[:], pattern=[[-1, 128]], compare_op=ALU.is_equal,
                                           fill=0.0, base=0, channel_multiplier=1), reads=[b.r], writes=[b.r])
    return b


def perm_view(ap2d, r, t0, n):
    if r == 1:
        return ap2d[:, t0:t0 + n]
    sc = SEQ // r
    v = ap2d.rearrange("p (i c) -> p c i", c=r)
    c0, i0 = t0 // sc, t0 % sc
    if i0 + n <= sc:
        return v[:, c0, i0:i0 + n]
    assert i0 == 0 and n % sc == 0
    return v[:, c0:c0 + n // sc, :]


import os as _os
_P1SEC = _os.environ.get("P1SEC", "abcd")
_MSUB = _os.environ.get("MSUB", "qkrvptc")
_MC = _os.environ.get("MC", "1234")


def phase1(nc, S, T, xin, r_xin, l, sc):
    A = Alloc(nc)
    ident = make_ident(A, S, BF16)
    xT = Buf(A.sb("xT", [128, 8, SEQ], BF16))
    xs = rot(A, "sb", "xs", [128, DM], F32, 2)
    xb = rot(A, "sb", "xb", [128, DM], BF16, 2)
    pst = rot(A, "ps", "pst", [128, DM], BF16, 2)
    pj = rot(A, "ps", "pj", [128, 512], F32, 4)
    pv = rot(A, "ps", "pv", [128, 512], F32, 2)
    Win = T["w_in"][l].rearrange("(c p) n -> p c n", p=128)

    for b in range(NB):
        s, c_, p = xs[b % 2], xb[b % 2], pst[b % 2]
        S.dma("sp", s.t[:], xin[b * 128:(b + 1) * 128, :], reads=[r_xin], writes=[s.r])
        S.op("act", lambda e, s=s, c_=c_: e.activation(out=c_.t[:], in_=s.t[:], func=AF.Copy), reads=[s.r], writes=[c_.r])
        for c in range(8):
            S.op("pe", lambda e, c=c, c_=c_, p=p: e.transpose(out=p.t[:, c * 128:(c + 1) * 128], in_=c_.t[:, c * 128:(c + 1) * 128],
                                                              identity=ident.t[:]), reads=[c_.r, ident.r], writes=[p.r])
        S.op("dve", lambda e, b=b, p=p: e.tensor_copy(out=xT.t[:, :, b * 128:(b + 1) * 128],
                                                      in_=p.t[:, :].rearrange("p (c t) -> p c t", c=8)), reads=[p.r], writes=[xT.r])

    wts = rot(A, "sb", "wt", [128, 8, 416], BF16, 2)
    wsw = rot(A, "sb", "wsw", [128, 8, 256], BF16, 2)
    stg = rot(A, "sb", "stg", [128, 512], BF16, 4)
    vst = rot(A, "sb", "vst", [128, NB, 2, 65], BF16, 2)
    tmp1 = rot(A, "sb", "tmp1", [128, 512], F32, 2)
    tmp2 = rot(A, "sb", "tmp2", [128, 512], F32, 2)
    CT = Buf(A.sb("ropeC", [128, SEQ], BF16))
    ST = Buf(A.sb("ropeS", [128, SEQ], BF16))
    for v in vst:
        S.op("pool", lambda e, v=v: e.memset(v.t[:], 1.0), writes=[v.r])
    state = {"w": 0, "pj": 0, "stg": 0, "v": 0, "pv": 0}

    def load_w(col_ranges):
        w = wts[state["w"] % 2]
        state["w"] += 1
        o = 0
        for (c0, n) in col_ranges:
            S.dma("pool", w.t[:, :, o:o + n], Win[:, :, c0:c0 + n], writes=[w.r])
            o += n
        return w

    def nxt(key, lst):
        b = lst[state[key] % len(lst)]
        state[key] += 1
        return b

    def proj_fm(w, o, M, r, j, wtile=None):
        p = nxt("pj", pj)
        wt_ = w if wtile is None else wtile
        for c in range(8):
            S.op("pe", lambda e, c=c, p=p, wt_=wt_: e.matmul(p.t[0:M, :], lhsT=wt_.t[:, c, o:o + M],
                                                             rhs=perm_view(xT.t[:, c, :], r, j * 512, 512),
                                                             start=(c == 0), stop=(c == 7)),
                 reads=[wt_.r, xT.r], writes=[p.r])
        return p

    def store_rows(st, rows, dst, r_dst, j):
        S.dma("sp", dst[:, j * 512:(j + 1) * 512], st.t[rows[0]:rows[1], :], reads=[st.r], writes=[r_dst])

    def v_proj(w, o, r, vt, ncol=128):
        for b4 in range(NB // 4):
            p = nxt("pv", pv)
            for bb in range(4):
                b = b4 * 4 + bb
                for c in range(8):
                    S.op("pe", lambda e, c=c, b=b, bb=bb, p=p: e.matmul(p.t[:, bb * 128:(bb + 1) * 128],
                                                                      lhsT=perm_view(xT.t[:, c, :], r, b * 128, 128),
                                                                      rhs=w.t[:, c, o:o + ncol], start=(c == 0), stop=(c == 7)),
                         reads=[w.r, xT.r], writes=[p.r])
            S.op("dve", lambda e, b4=b4, p=p: e.tensor_copy(
                out=vt.t[:, b4 * 4:(b4 + 1) * 4, :, 0:64],
                in_=p.t[:, :].rearrange("p (b h d) -> p b h d", b=4, h=2)), reads=[p.r], writes=[vt.r])

    def scale_q(w):
        S.op("dve", lambda e: e.tensor_scalar(out=w.t[:, :, 0:128], in0=w.t[:, :, 0:128], scalar1=0.125, scalar2=None,
                                              op0=ALU.mult), reads=[w.r], writes=[w.r])

    for kind, cbase, hbase, pbase in (("sb", C_SB, 0, 0), ("fox", C_FOX, 4, 2)):
        for hp in range(2):
            w = load_w([(cbase + hp * 128, 128), (cbase + 256 + hp * 128, 128), (cbase + 512 + hp * 128, 128)])
            scale_q(w)
            for qk, dst, rd in ((0, sc["QS"], sc["r_QS"]), (1, sc["KS"], sc["r_KS"])):
                for j in range(NT):
                    p = proj_fm(w, qk * 128, 128, 1, j)
                    st = nxt("stg", stg)
                    S.op("act", lambda e, p=p, st=st: e.activation(out=st.t[:], in_=p.t[:], func=AF.Copy), reads=[p.r], writes=[st.r])
                    for hh in range(2):
                        h = hbase + hp * 2 + hh
                        store_rows(st, (hh * 64, hh * 64 + 64), dst[h][0:64, :], rd[h], j)
            vt = nxt("v", vst)
            v_proj(w, 256, 1, vt)
            S.dma("sp", sc["VS"][pbase + hp], vt.t[:, :, :, :].rearrange("p b h d -> p (b h d)"), reads=[vt.r], writes=[sc["r_VS"][pbase + hp]])

    S.flush()
    if "b" not in _P1SEC:
        A.close()
        return
    A2 = Alloc(nc)
    wf = Buf(A2.sb("wf", [128, 8, 4], BF16))
    S.dma("pool", wf.t[:], Win[:, :, C_FOX + 768:C_FOX + 772], writes=[wf.r])
    bfg = Buf(A2.sb("bfg", [4, 1], F32))
    S.dma("sp", bfg.t[:], T["b_forget"][l].rearrange("(h o) -> h o", o=1), writes=[bfg.r])
    S.op("dve", lambda e: e.tensor_scalar(out=bfg.t[:], in0=bfg.t[:], scalar1=-1.0, scalar2=None, op0=ALU.mult), reads=[bfg.r], writes=[bfg.r])
    nlf = Buf(A2.sb("nlf", [4, SEQ], F32))
    ncum = Buf(A2.sb("ncum", [4, SEQ], F32))
    ones4 = Buf(A2.sb("ones4", [4, 512], F32))
    S.op("pool", lambda e: e.memset(ones4.t[:], 1.0), writes=[ones4.r])
    for j in range(NT):
        p = proj_fm(wf, 0, 4, 1, j)
        S.op("act", lambda e, p=p, j=j: e.activation(out=nlf.t[:, j * 512:(j + 1) * 512], in_=p.t[0:4, :], func=AF.Exp,
                                                     bias=bfg.t[:, 0:1], scale=-1.0), reads=[p.r, bfg.r], writes=[nlf.r])
    S.op("act", lambda e: e.activation(out=nlf.t[:], in_=nlf.t[:], func=AF.Ln, bias=1.0), reads=[nlf.r], writes=[nlf.r])
    for j in range(NT):
        sl = slice(j * 512, (j + 1) * 512)
        init = 0.0 if j == 0 else ncum.t[:, j * 512 - 1:j * 512]
        S.op("dve", lambda e, sl=sl, init=init: e.tensor_tensor_scan(out=ncum.t[:, sl], data0=ones4.t[:, :], data1=nlf.t[:, sl],
                                                                     initial=init, op0=ALU.mult, op1=ALU.add),
             reads=[ones4.r, nlf.r, ncum.r], writes=[ncum.r])
    parts = [Buf(A2.sb("cpart%d" % i, [4, SEQ], BF16)) for i in range(3)]
    for i in range(3):
        S.op("dve", lambda e, i=i: e.tensor_copy(out=parts[i].t[:], in_=ncum.t[:]), reads=[ncum.r], writes=[parts[i].r])
        if i < 2:
            S.op("dve", lambda e, i=i: e.tensor_tensor(out=ncum.t[:], in0=ncum.t[:], in1=parts[i].t[:], op=ALU.subtract),
                 reads=[ncum.r, parts[i].r], writes=[ncum.r])
    ones3 = Buf(A2.sb("ones3", [35, SEQ], BF16))
    S.op("pool", lambda e: e.memset(ones3.t[0:3, :], 1.0), writes=[ones3.r])
    S.op("pool", lambda e: e.memset(ones3.t[32:35, :], -1.0), writes=[ones3.r])
    for h in range(4):
        H = 4 + h
        S.dma("sp", sc["KS"][H][64:67, :], ones3.t[32:35, :], reads=[ones3.r], writes=[sc["r_KS"][H]])
        S.dma("sp", sc["QS"][H][67:70, :], ones3.t[0:3, :], reads=[ones3.r], writes=[sc["r_QS"][H]])
        for i in range(3):
            S.dma("sp", sc["KS"][H][67 + i:68 + i, :], parts[i].t[h:h + 1, :], reads=[parts[i].r], writes=[sc["r_KS"][H]])
            S.dma("sp", sc["QS"][H][64 + i:65 + i, :], parts[i].t[h:h + 1, :], reads=[parts[i].r], writes=[sc["r_QS"][H]])
    S.flush()
    A2.close()

    if "c" not in _P1SEC:
        A.close()
        return
    w = load_w([(C_MLA, 416)])
    wkrs = nxt("w", wsw) if False else wsw[0]
    if "p" in _MSUB:
        S.op("pool", lambda e: e.tensor_copy(out=wkrs.t[:, :, 0:16], in_=w.t[:, :, 400:416]), reads=[w.r], writes=[wkrs.r])
        S.op("pool", lambda e: e.tensor_copy(out=wkrs.t[:, :, 16:32], in_=w.t[:, :, 384:400]), reads=[w.r], writes=[wkrs.r])
    A3 = Alloc(nc)
    wuq = Buf(A3.sb("wuq", [128, 2, 384], BF16))
    wuqs = Buf(A3.sb("wuqs", [128, 2, 384], BF16))
    wukv = Buf(A3.sb("wukv", [128, 512], BF16))
    S.dma("pool", wuq.t[:], T["w_uq"][l].rearrange("(c p) n -> p c n", p=128), writes=[wuq.r])
    S.dma("pool", wukv.t[:], T["w_ukv"][l], writes=[wukv.r])
    S.op("pool", lambda e: e.tensor_copy(out=wuqs.t[:], in_=wuq.t[:]), reads=[wuq.r], writes=[wuqs.r])
    for c2 in (range(2) if "p" in _MSUB else []):
        v4o = wuqs.t[:, c2, :].rearrange("p (h d) -> p h d", h=4)
        v4i = wuq.t[:, c2, :].rearrange("p (h d) -> p h d", h=4)
        S.op("pool", lambda e, v4o=v4o, v4i=v4i: e.tensor_copy(out=v4o[:, :, 64:80], in_=v4i[:, :, 80:96]), reads=[wuq.r, wuqs.r], writes=[wuqs.r])
        S.op("pool", lambda e, v4o=v4o, v4i=v4i: e.tensor_copy(out=v4o[:, :, 80:96], in_=v4i[:, :, 64:80]), reads=[wuq.r, wuqs.r], writes=[wuqs.r])
    gcq = Buf(A3.sb("gcq", [128, 2], F32))
    gckv = Buf(A3.sb("gckv", [128, 1], F32))
    for c2 in range(2):
        S.dma("sp", gcq.t[:, c2:c2 + 1], T["g_cq"][l][c2 * 128:(c2 + 1) * 128].rearrange("(p o) -> p o", o=1), writes=[gcq.r])
    S.dma("sp", gckv.t[:], T["g_ckv"][l].rearrange("(p o) -> p o", o=1), writes=[gckv.r])
    for tb, nm in (((CT, "rope32c"), (ST, "rope32s")) if "t" in _MSUB else []):
        S.dma("pool", tb.t[0:32, :], T[nm], writes=[tb.r])
        S.dma("pool", tb.t[64:96, :], T[nm], writes=[tb.r])
    onesq = Buf(A3.sb("onesq", [128, 128], BF16))
    oneskv = Buf(A3.sb("oneskv", [128, 128], BF16))
    epst = Buf(A3.sb("epst", [128, 1], F32))
    S.op("pool", lambda e: e.memset(epst.t[:], RMS_EPS), writes=[epst.r])
    S.op("pool", lambda e: e.memset(onesq.t[:], 1.0 / 256.0), writes=[onesq.r])
    S.op("pool", lambda e: e.memset(oneskv.t[:], 1.0 / 128.0), writes=[oneskv.r])
    cqg = rot(A3, "sb", "cqg", [128, 2, 512], BF16, 2)
    cq2 = rot(A3, "sb", "cq2", [128, 2, 512], BF16, 2)
    ckg = rot(A3, "sb", "ckg", [128, 512], BF16, 2)
    ck2 = rot(A3, "sb", "ck2", [128, 512], BF16, 2)
    rq = rot(A3, "sb", "rq", [128, 512], F32, 2)
    rkv = rot(A3, "sb", "rkv", [128, 512], F32, 2)
    rtok = rot(A3, "sb", "rtok", [128, 1], F32, 2)
    vstm = Buf(A3.sb("vstm", [128, NB, 4, 65], BF16))
    S.op("pool", lambda e: e.memset(vstm.t[:], 1.0), writes=[vstm.r])
    wukv_v = wukv.t[:, :].rearrange("p (h x) -> p h x", h=4)[:, :, 64:128]
    for j in (range(NT) if "c" in _MSUB else []):
        tc = slice(j * 512, (j + 1) * 512)
        a, a2, kg, k2, rq_, rkv_ = cqg[j % 2], cq2[j % 2], ckg[j % 2], ck2[j % 2], rq[j % 2], rkv[j % 2]
        for c2 in (range(2) if "1" in _MC else []):
            p = proj_fm(w, c2 * 128, 128, 1, j)
            S.op("dve", lambda e, p=p, c2=c2, a=a: e.tensor_scalar(out=a.t[:, c2, :], in0=p.t[:], scalar1=gcq.t[:, c2:c2 + 1], scalar2=None,
                                                                  op0=ALU.mult), reads=[p.r, gcq.r], writes=[a.r])
            S.op("act", lambda e, p=p, c2=c2, a2=a2: e.activation(out=a2.t[:, c2, :], in_=p.t[:], func=AF.Square), reads=[p.r], writes=[a2.r])
        if "2" in _MC:
            p = proj_fm(w, 256, 128, 1, j)
            if "5" not in _MC:
                S.op("dve", lambda e, p=p, kg=kg: e.tensor_scalar(out=kg.t[:], in0=p.t[:], scalar1=gckv.t[:, 0:1], scalar2=None, op0=ALU.mult),
                     reads=[p.r, gckv.r], writes=[kg.r])
            if "6" not in _MC:
                S.op("act", lambda e, p=p, k2=k2: e.activation(out=k2.t[:], in_=p.t[:], func=AF.Square), reads=[p.r], writes=[k2.r])
        if "3" in _MC:
            p = nxt("pj", pj)
            for c2 in range(2):
                S.op("pe", lambda e, p=p, c2=c2, a2=a2: e.matmul(p.t[:], lhsT=onesq.t[:], rhs=a2.t[:, c2, :], start=(c2 == 0), stop=(c2 == 1)),
                     reads=[onesq.r, a2.r], writes=[p.r])
            S.op("act", lambda e, p=p, rq_=rq_: e.activation(out=rq_.t[:], in_=p.t[:], func=AF.Sqrt, bias=epst.t[:, 0:1]), reads=[p.r, epst.r], writes=[rq_.r])
            S.op("dve", lambda e, rq_=rq_: e.reciprocal(out=rq_.t[:], in_=rq_.t[:]), reads=[rq_.r], writes=[rq_.r])
        if "4" in _MC:
            p = nxt("pj", pj)
            S.op("pe", lambda e, p=p, k2=k2: e.matmul(p.t[:], lhsT=oneskv.t[:], rhs=k2.t[:], start=True, stop=True), reads=[oneskv.r, k2.r], writes=[p.r])
            S.op("act", lambda e, p=p, rkv_=rkv_: e.activation(out=rkv_.t[:], in_=p.t[:], func=AF.Sqrt, bias=epst.t[:, 0:1]), reads=[p.r, epst.r], writes=[rkv_.r])
            S.op("dve", lambda e, rkv_=rkv_: e.reciprocal(out=rkv_.t[:], in_=rkv_.t[:]), reads=[rkv_.r], writes=[rkv_.r])
        for h in (range(4) if "q" in _MSUB else []):
            H = 8 + h
            pa, pb = nxt("pj", pj), nxt("pj", pj)
            for pp, ww in ((pa, wuq), (pb, wuqs)):
                for c2 in range(2):
                    S.op("pe", lambda e, pp=pp, ww=ww, c2=c2, h=h, a=a: e.matmul(pp.t[0:96, :], lhsT=ww.t[:, c2, h * 96:(h + 1) * 96], rhs=a.t[:, c2, :],
                                                                             start=(c2 == 0), stop=(c2 == 1)), reads=[ww.r, a.r], writes=[pp.r])
            st = nxt("stg", stg)
            t1, t2 = tmp1[h % 2], tmp2[h % 2]
            S.op("dve", lambda e, pa=pa, st=st, rq_=rq_: e.scalar_tensor_tensor(out=st.t[0:64, :], in0=pa.t[0:64, :], scalar=MLA_SCALE, in1=rq_.t[0:64, :],
                                                                             op0=ALU.mult, op1=ALU.mult), reads=[pa.r, rq_.r], writes=[st.r])
            S.op("dve", lambda e, pa=pa, t1=t1, tc=tc: e.tensor_tensor(out=t1.t[64:96, :], in0=pa.t[64:96, :], in1=CT.t[64:96, tc], op=ALU.mult),
                 reads=[pa.r, CT.r], writes=[t1.r])
            S.op("dve", lambda e, pb=pb, t2=t2, tc=tc: e.tensor_tensor(out=t2.t[64:96, :], in0=pb.t[64:96, :], in1=ST.t[64:96, tc], op=ALU.mult),
                 reads=[pb.r, ST.r], writes=[t2.r])
            S.op("pool", lambda e, t1=t1, t2=t2: e.tensor_tensor(out=t1.t[64:96, :], in0=t1.t[64:96, :], in1=t2.t[64:96, :], op=ALU.add),
                 reads=[t1.r, t2.r], writes=[t1.r])
            S.op("dve", lambda e, t1=t1, st=st, rq_=rq_: e.scalar_tensor_tensor(out=st.t[64:96, :], in0=t1.t[64:96, :], scalar=MLA_SCALE, in1=rq_.t[64:96, :],
                                                                             op0=ALU.mult, op1=ALU.mult), reads=[t1.r, rq_.r, st.r], writes=[st.r])
            store_rows(st, (0, 96), sc["QS"][H][0:96, :], sc["r_QS"][H], j)
        for h in (range(4) if "k" in _MSUB else []):
            H = 8 + h
            p = nxt("pj", pj)
            S.op("pe", lambda e, p=p, h=h, kg=kg: e.matmul(p.t[0:64, :], lhsT=wukv.t[:, h * 128:h * 128 + 64], rhs=kg.t[:], start=True, stop=True),
                 reads=[wukv.r, kg.r], writes=[p.r])
            st = nxt("stg", stg)
            S.op("dve", lambda e, p=p, st=st, rkv_=rkv_: e.tensor_tensor(out=st.t[0:64, :], in0=p.t[0:64, :], in1=rkv_.t[0:64, :], op=ALU.mult),
                 reads=[p.r, rkv_.r], writes=[st.r])
            store_rows(st, (0, 64), sc["KS"][H][0:64, :], sc["r_KS"][H], j)
        if "r" not in _MSUB:
            continue
        pa = proj_fm(w, 384, 32, 1, j)
        pb = proj_fm(wkrs, 0, 32, 1, j)
        t1, t2 = tmp1[0], tmp2[0]
        st = nxt("stg", stg)
        S.op("dve", lambda e, pa=pa, t1=t1, tc=tc: e.tensor_tensor(out=t1.t[0:32, :], in0=pa.t[0:32, :], in1=CT.t[0:32, tc], op=ALU.mult),
             reads=[pa.r, CT.r], writes=[t1.r])
        S.op("dve", lambda e, pb=pb, t2=t2, tc=tc: e.tensor_tensor(out=t2.t[0:32, :], in0=pb.t[0:32, :], in1=ST.t[0:32, tc], op=ALU.mult),
             reads=[pb.r, ST.r], writes=[t2.r])
        S.op("pool", lambda e, t1=t1, t2=t2, st=st: e.tensor_tensor(out=st.t[0:32, :], in0=t1.t[0:32, :], in1=t2.t[0:32, :], op=ALU.add),
             reads=[t1.r, t2.r], writes=[st.r])
        for h in range(4):
            store_rows(st, (0, 32), sc["KS"][8 + h][64:96, :], sc["r_KS"][8 + h], j)
        for bb in (range(4) if "v" in _MSUB else []):
            b = j * 4 + bb
            p = nxt("pv", pv)
            S.op("pe", lambda e, p=p, bb=bb, kg=kg: e.matmul(p.t[:, 0:256], lhsT=kg.t[:, bb * 128:(bb + 1) * 128], rhs=wukv_v, start=True, stop=True),
                 reads=[wukv.r, kg.r], writes=[p.r])
            S.op("pe", lambda e, p=p, bb=bb, k2=k2: e.matmul(p.t[:, 256:257], lhsT=k2.t[:, bb * 128:(bb + 1) * 128], rhs=oneskv.t[:, 0:1], start=True, stop=True),
                 reads=[oneskv.r, k2.r], writes=[p.r])
            rt = rtok[b % 2]
            S.op("act", lambda e, p=p, rt=rt: e.activation(out=rt.t[:], in_=p.t[:, 256:257], func=AF.Sqrt, bias=epst.t[:, 0:1]), reads=[p.r, epst.r], writes=[rt.r])
            S.op("dve", lambda e, rt=rt: e.reciprocal(out=rt.t[:], in_=rt.t[:]), reads=[rt.r], writes=[rt.r])
            S.op("dve", lambda e, p=p, b=b, rt=rt: e.tensor_scalar(out=vstm.t[:, b, :, 0:64], in0=p.t[:, 0:256].rearrange("p (h d) -> p h d", h=4),
                                                                  scalar1=rt.t[:, 0:1], scalar2=None, op0=ALU.mult), reads=[p.r, rt.r], writes=[vstm.r])
    for hp in range(2):
        S.dma("sp", sc["VS"][4 + hp].rearrange("p (b h d) -> p b h d", b=NB, h=2), vstm.t[:, :, 2 * hp:2 * hp + 2, :], reads=[vstm.r], writes=[sc["r_VS"][4 + hp]])

    S.flush()
    A3.close()
    if "d" not in _P1SEC:
        A.close()
        return
    for tb, nm in ((CT, "rope64c"), (ST, "rope64s")):
        S.dma("pool", tb.t[0:64, :], T[nm], writes=[tb.r])
        S.dma("pool", tb.t[64:128, :], T[nm], writes=[tb.r])
    for g in range(3):
        r = DIL_R[g]
        for hp in range(2):
            o = g * 256 + hp * 128
            w = load_w([(C_DIL + o, 128), (C_DIL + 768 + o, 128), (C_DIL + 1536 + o, 128)])
            scale_q(w)
            ws = wsw[(g * 2 + hp) % 2]
            for c in range(8):
                vo = ws.t[:, c, :].rearrange("p (h f d) -> p h f d", h=4, f=2)
                vi = w.t[:, c, 0:256].rearrange("p (h f d) -> p h f d", h=4, f=2)
                S.op("pool", lambda e, vo=vo, vi=vi: e.tensor_copy(out=vo[:, :, 0, :], in_=vi[:, :, 1, :]), reads=[w.r], writes=[ws.r])
                S.op("pool", lambda e, vo=vo, vi=vi: e.tensor_copy(out=vo[:, :, 1, :], in_=vi[:, :, 0, :]), reads=[w.r], writes=[ws.r])
            for qk, dst, rd in ((0, sc["QD"], sc["r_QD"]), (1, sc["KD"], sc["r_KD"])):
                for j in range(NT):
                    pa = proj_fm(w, qk * 128, 128, r, j)
                    pb = proj_fm(ws, qk * 128, 128, r, j)
                    t1, t2 = tmp1[j % 2], tmp2[j % 2]
                    st = nxt("stg", stg)
                    cv = perm_view(CT.t[:, :], r, j * 512, 512)
                    sv = perm_view(ST.t[:, :], r, j * 512, 512)
                    shp = None if len(cv.shape) == 2 else cv.shape

                    def v3(ap):
                        return ap if shp is None else ap.rearrange("p (a b) -> p a b", a=shp[1])
                    S.op("dve", lambda e, pa=pa, t1=t1, cv=cv, v3=v3: e.tensor_tensor(out=v3(t1.t[:]), in0=v3(pa.t[:]), in1=cv, op=ALU.mult),
                         reads=[pa.r, CT.r], writes=[t1.r])
                    S.op("dve", lambda e, pb=pb, t2=t2, sv=sv, v3=v3: e.tensor_tensor(out=v3(t2.t[:]), in0=v3(pb.t[:]), in1=sv, op=ALU.mult),
                         reads=[pb.r, ST.r], writes=[t2.r])
                    S.op("pool", lambda e, t1=t1, t2=t2, st=st: e.tensor_tensor(out=st.t[:], in0=t1.t[:], in1=t2.t[:], op=ALU.add),
                         reads=[t1.r, t2.r], writes=[st.r])
                    for hh in range(2):
                        store_rows(st, (hh * 64, hh * 64 + 64), dst[g][hp * 2 + hh], rd[g][hp * 2 + hh], j)
            vt = nxt("v", vst)
            v_proj(w, 256, r, vt)
            S.dma("sp", sc["VD"][g][hp], vt.t[:, :, :, :].rearrange("p b h d -> p (b h d)"), reads=[vt.r], writes=[sc["r_VD"][g][hp]])
    S.flush()
    A.close()


def phase2(nc, S, T, l, sc, heads=None):
    A = Alloc(nc)
    negtri = Buf(A.sb("negtri", [128, 128], BF16))
    S.op("pool", lambda e: e.memset(negtri.t[:], -1.0), writes=[negtri.r])
    S.op("pool", lambda e: e.affine_select(out=negtri.t[:], in_=negtri.t[:], pattern=[[-1, 128]], compare_op=ALU.is_ge, fill=0.0, base=0,
                                           channel_multiplier=1), reads=[negtri.r], writes=[negtri.r])
    ones = Buf(A.sb("ones", [128, 128], BF16))
    S.op("pool", lambda e: e.memset(ones.t[:], 1.0), writes=[ones.r])
    wn = Buf(A.sb("wn", [65, 64], BF16))
    wnsb = Buf(A.sb("wnsb", [65, 64], BF16))
    for t_, v_ in ((wn, RMS_EPS), (wnsb, 0.0)):
        S.op("pool", lambda e, t_=t_: e.memset(t_.t[:], 1.0 / 64.0), writes=[t_.r])
        S.op("pool", lambda e, t_=t_, v_=v_: e.memset(t_.t[64:65, :], v_), reads=[t_.r], writes=[t_.r])
    gh = Buf(A.sb("gh", [64, 16], F32))
    for h_ in range(16):
        S.dma("sp", gh.t[:, h_:h_ + 1], T["g_head"][l][h_].rearrange("(d o) -> d o", o=1), writes=[gh.r])
    eps2 = Buf(A.sb("eps2", [64, 2], F32))
    S.op("pool", lambda e: e.memset(eps2.t[:, 0:1], RMS_EPS), writes=[eps2.r])
    S.op("pool", lambda e: e.memset(eps2.t[:, 1:2], 0.0), reads=[eps2.r], writes=[eps2.r])

    Qt = rot(A, "sb", "Qt", [128, SEQ], BF16, 2)
    Kt = rot(A, "sb", "Kt", [128, SEQ], BF16, 2)
    Vt = rot(A, "sb", "Vt", [128, NB, 2, 65], BF16, 2)
    pz = rot(A, "ps", "pz", [128, 512], F32, 3)
    po = rot(A, "ps", "po", [128, 512], F32, 2)
    pc = rot(A, "ps", "pc", [128, 512], F32, 2)
    pss = rot(A, "ps", "pss", [128, 512], F32, 1)
    Pb = rot(A, "sb", "Pb", [128, 512], BF16, 4)
    eb = rot(A, "sb", "eb", [128, 512], F32, 2)
    spb = rot(A, "sb", "spb", [128, 512], BF16, 3)
    lw = rot(A, "sb", "lw", [128, 512], F32, 2)
    Rsb = Buf(A.sb("Rsb", [128, 512], F32))
    sqb = rot(A, "sb", "sqb", [65, 512], BF16, 2)
    stb = rot(A, "sb", "stb", [64, 512], F32, 2)
    yb = rot(A, "sb", "yb", [64, 512], BF16, 2)
    acc = rot(A, "sb", "acc", [65, SEQ], F32, 2)
    st = {"fin": 0, "ld": 0, "vld": 0, "o": 0}

    def finish(src_ap, r_src, h, t0, n, is_sb):
        i = st["fin"]
        st["fin"] += 1
        sq, s_, y, ps_ = sqb[i % 2], stb[i % 2], yb[i % 2], pss[0]
        S.op("act", lambda e: e.activation(out=sq.t[:, 0:n], in_=src_ap, func=AF.Square), reads=[r_src], writes=[sq.r])
        wn_ = wnsb if is_sb else wn
        S.op("pe", lambda e: e.matmul(ps_.t[0:64, 0:n], lhsT=wn_.t[:, :], rhs=sq.t[:, 0:n], start=True, stop=True), reads=[wn_.r, sq.r], writes=[ps_.r])
        S.op("act", lambda e: e.activation(out=s_.t[:, 0:n], in_=ps_.t[0:64, 0:n], func=AF.Sqrt, bias=(eps2.t[:, 0:1] if is_sb else eps2.t[:, 1:2])),
             reads=[ps_.r, eps2.r], writes=[s_.r])
        S.op("dve", lambda e: e.reciprocal(out=s_.t[:, 0:n], in_=s_.t[:, 0:n]), reads=[s_.r], writes=[s_.r])
        S.op("dve", lambda e: e.scalar_tensor_tensor(out=y.t[:, 0:n], in0=src_ap[0:64], scalar=gh.t[:, h:h + 1], in1=s_.t[:, 0:n],
                                                     op0=ALU.mult, op1=ALU.mult), reads=[r_src, gh.r, s_.r], writes=[y.r])
        S.dma("sp", sc["OnT"][h // 2, (h % 2) * 64:(h % 2) * 64 + 64, t0:t0 + n], y.t[:, 0:n], reads=[y.r], writes=[sc["r_OnT"]])

    def load_qk(qsrc, r_q, ksrc, r_k, kd):
        i = st["ld"]
        st["ld"] += 1
        q, k = Qt[i % 2], Kt[i % 2]
        S.dma("sp", q.t[0:kd, :], qsrc, reads=[r_q], writes=[q.r])
        S.dma("sp", k.t[0:kd, :], ksrc, reads=[r_k], writes=[k.r])
        return q, k

    def load_v(vsrc, r_v):
        i = st["vld"]
        st["vld"] += 1
        v = Vt[i % 2]
        S.dma("sp", v.t[:, :, :, :].rearrange("p b h d -> p (b h d)"), vsrc, reads=[r_v], writes=[v.r])
        return v

    def run_steps(steps, q, k, kd, v, hh, kind, done_cb):
        n_ = len(steps)
        ctx = [dict() for _ in range(n_)]

        def s1(i):
            sp_ = steps[i]
            z = pz[i % 3] if kind != "sb" else pz[i % 2]
            q0, n, kb = sp_["q0"], sp_["n"], sp_["kb"]
            S.op("pe", lambda e: e.matmul(z.t[:, 0:n], lhsT=k.t[0:kd, kb * 128:(kb + 1) * 128], rhs=q.t[0:kd, q0:q0 + n], start=True, stop=True),
                 reads=[k.r, q.r], writes=[z.r])
            if kind == "sb":
                e_, s_ = eb[i % 2], spb[i % 3]
                S.op("act", lambda e: e.activation(out=e_.t[:, 0:n], in_=z.t[:, 0:n], func=AF.Exp), reads=[z.r], writes=[e_.r])
                S.op("act", lambda e: e.activation(out=s_.t[:, 0:n], in_=e_.t[:, 0:n], func=AF.Ln, bias=1.0), reads=[e_.r], writes=[s_.r])
                if sp_["mA"] is not None:
                    S.op("pool", lambda e: e.affine_select(out=s_.t[:, 0:n], in_=s_.t[:, 0:n], pattern=[[1, n]], compare_op=ALU.is_ge, fill=0.0,
                                                           base=sp_["mA"], channel_multiplier=-1), reads=[s_.r], writes=[s_.r])
                ctx[i]["sp"] = s_
            else:
                p_ = Pb[i % 4]
                if kind == "fox" and sp_["mA"] is not None:
                    l_ = lw[i % 2]
                    S.op("dve", lambda e: e.tensor_scalar(out=l_.t[:, 0:n], in0=z.t[:, 0:n], scalar1=60.0, scalar2=None, op0=ALU.min), reads=[z.r], writes=[l_.r])
                    S.op("act", lambda e: e.activation(out=p_.t[:, 0:n], in_=l_.t[:, 0:n], func=AF.Exp), reads=[l_.r], writes=[p_.r])
                else:
                    S.op("act", lambda e: e.activation(out=p_.t[:, 0:n], in_=z.t[:, 0:n], func=AF.Exp), reads=[z.r], writes=[p_.r])
                if sp_["mA"] is not None:
                    S.op("pool", lambda e: e.affine_select(out=p_.t[:, 0:n], in_=p_.t[:, 0:n], pattern=[[1, n]], compare_op=ALU.is_ge, fill=0.0,
                                                           base=sp_["mA"], channel_multiplier=-1), reads=[p_.r], writes=[p_.r])
                if sp_["mB"] is not None:
                    S.op("pool", lambda e: e.affine_select(out=p_.t[:, 0:n], in_=p_.t[:, 0:n], pattern=[[-1, n]], compare_op=ALU.is_ge, fill=0.0,
                                                           base=sp_["mB"], channel_multiplier=1), reads=[p_.r], writes=[p_.r])
                ctx[i]["P"] = p_

        def s2(i):
            if kind != "sb":
                return
            sp_ = steps[i]
            q0, n, kb = sp_["q0"], sp_["n"], sp_["kb"]
            s_ = ctx[i]["sp"]
            c_, rc, l_, p_ = pc[i % 2], pz[2], lw[i % 2], Pb[i % 4]
            S.op("pe", lambda e: e.matmul(c_.t[:, 0:n], lhsT=k.t[0:kd, kb * 128:(kb + 1) * 128], rhs=q.t[0:kd, q0:q0 + n], start=True, stop=False),
                 reads=[k.r, q.r], writes=[c_.r])
            S.op("pe", lambda e: e.matmul(c_.t[:, 0:n], lhsT=negtri.t[:], rhs=s_.t[:, 0:n], start=False, stop=True), reads=[negtri.r, s_.r], writes=[c_.r])
            S.op("pe", lambda e: e.matmul(rc.t[:, 0:n], lhsT=ones.t[:], rhs=s_.t[:, 0:n], start=True, stop=True), reads=[ones.r, s_.r], writes=[rc.r])
            if sp_["first"]:
                S.op("dve", lambda e: e.tensor_copy(out=l_.t[:, 0:n], in_=c_.t[:, 0:n]), reads=[c_.r], writes=[l_.r])
                S.op("dve", lambda e: e.tensor_copy(out=Rsb.t[:, 0:n], in_=rc.t[:, 0:n]), reads=[rc.r], writes=[Rsb.r])
            else:
                S.op("dve", lambda e: e.tensor_tensor(out=l_.t[:, 0:n], in0=c_.t[:, 0:n], in1=Rsb.t[:, 0:n], op=ALU.subtract), reads=[c_.r, Rsb.r], writes=[l_.r])
                S.op("dve", lambda e: e.tensor_tensor(out=Rsb.t[:, 0:n], in0=rc.t[:, 0:n], in1=Rsb.t[:, 0:n], op=ALU.add), reads=[rc.r, Rsb.r], writes=[Rsb.r])
            S.op("act", lambda e: e.activation(out=p_.t[:, 0:n], in_=l_.t[:, 0:n], func=AF.Exp), reads=[l_.r], writes=[p_.r])
            if sp_["mA"] is not None:
                S.op("pool", lambda e: e.affine_select(out=p_.t[:, 0:n], in_=p_.t[:, 0:n], pattern=[[1, n]], compare_op=ALU.is_ge, fill=0.0,
                                                       base=sp_["mA"], channel_multiplier=-1), reads=[p_.r], writes=[p_.r])
            ctx[i]["P"] = p_

        def s3(i):
            sp_ = steps[i]
            n, kb = sp_["n"], sp_["kb"]
            if sp_["first"]:
                st["o"] += 1
            o_ = po[st["o"] % 2]
            p_ = ctx[i]["P"]
            S.op("pe", lambda e: e.matmul(o_.t[0:65, 0:n], lhsT=v.t[:, kb, hh, :], rhs=p_.t[:, 0:n], start=sp_["first"], stop=sp_["last"]),
                 reads=[v.r, p_.r], writes=[o_.r])
            if sp_["last"]:
                done_cb(o_, sp_)

        for i in range(n_ + 2):
            if i < n_:
                s1(i)
            if 0 <= i - 1 < n_:
                s2(i - 1)
            if 0 <= i - 2 < n_:
                s3(i - 2)

    def causal_steps(strict, descending):
        steps = []
        for qt in range(NT):
            q0 = qt * 512
            kbs = list(range(0, 4 * qt + 4))
            if descending:
                kbs = kbs[::-1]
            for ii, kb in enumerate(kbs):
                base = q0 - 128 * kb - (1 if strict else 0)
                steps.append(dict(q0=q0, n=512, kb=kb, mA=(base if base - 127 < 0 else None), mB=None, first=(ii == 0), last=(ii == len(kbs) - 1)))
        return steps

    def dil_steps(r):
        sc_ = SEQ // r
        n = min(512, sc_)
        steps = []
        for q0 in range(0, SEQ, n):
            cs = (q0 // sc_) * sc_
            k_lo = max(cs, q0 - 128)
            kbs = list(range(k_lo // 128, (q0 + n) // 128))
            for ii, kb in enumerate(kbs):
                bA = q0 - 128 * kb
                bB = 128 + 128 * kb - q0
                steps.append(dict(q0=q0, n=n, kb=kb, mA=(bA if bA - 127 < 0 else None), mB=(bB if bB - (n - 1) < 0 else None),
                                  first=(ii == 0), last=(ii == len(kbs) - 1)))
        return steps

    hsel = (lambda h: True) if heads is None else (lambda h: h in heads)
    for kind, hbase, pbase, kd in (("sb", 0, 0, 64), ("fox", 4, 2, 70), ("mla", 8, 4, 96)):
        steps = causal_steps(strict=(kind == "sb"), descending=(kind == "sb"))
        for hp in range(2):
            if not (hsel(hbase + 2 * hp) or hsel(hbase + 2 * hp + 1)):
                continue
            v = load_v(sc["VS"][pbase + hp], sc["r_VS"][pbase + hp])
            for hh in range(2):
                h = hbase + hp * 2 + hh
                if not hsel(h):
                    continue
                q, k = load_qk(sc["QS"][h][0:kd, :], sc["r_QS"][h], sc["KS"][h][0:kd, :], sc["r_KS"][h], kd)

                def done(o_, sp_, h=h, kind=kind):
                    finish(o_.t[0:65, 0:sp_["n"]], o_.r, h, sp_["q0"], sp_["n"], kind == "sb")
                run_steps(steps, q, k, kd, v, hh, kind, done)
    for hp in range(2):
        if not (hsel(12 + 2 * hp) or hsel(13 + 2 * hp)):
            continue
        for g in range(3):
            r = DIL_R[g]
            steps = dil_steps(r)
            v = load_v(sc["VD"][g][hp], sc["r_VD"][g][hp])
            for hh in range(2):
                hd = hp * 2 + hh
                q, k = load_qk(sc["QD"][g][hd], sc["r_QD"][g][hd], sc["KD"][g][hd], sc["r_KD"][g][hd], 64)
                a_ = acc[hh]

                def done(o_, sp_, a_=a_, r=r, g=g):
                    n, q0 = sp_["n"], sp_["q0"]
                    dst = perm_view(a_.t[:, :], r, q0, n)
                    if g == 0:
                        S.op("act", lambda e: e.activation(out=dst, in_=o_.t[0:65, 0:n], func=AF.Copy), reads=[o_.r], writes=[a_.r])
                    else:
                        S.op("dve", lambda e: e.tensor_tensor(out=dst, in0=o_.t[0:65, 0:n], in1=dst, op=ALU.add), reads=[o_.r, a_.r], writes=[a_.r])
                run_steps(steps, q, k, 64, v, hh, "dil", done)
        for hh in range(2):
            for qt in range(NT):
                finish(acc[hh].t[:, qt * 512:(qt + 1) * 512], acc[hh].r, 12 + hp * 2 + hh, qt * 512, 512, False)
    S.flush()
    A.close()


def layernorm_block(S, y, g_b, b_b, small, out):
    st6, mv, rs = small["st6"], small["mv"], small["rs"]
    for hf in range(2):
        S.op("dve", lambda e, hf=hf: e.bn_stats(out=st6.t[:, hf, :], in_=y.t[:, hf * 512:(hf + 1) * 512]), reads=[y.r], writes=[st6.r])
    S.op("dve", lambda e: e.bn_aggr(out=mv.t[:], in_=st6.t[:, :, :].rearrange("p a b -> p (a b)")), reads=[st6.r], writes=[mv.r])
    S.op("act", lambda e: e.activation(out=rs.t[:], in_=mv.t[:, 1:2], func=AF.Sqrt, bias=small["eps"].t[:, 0:1]), reads=[mv.r, small["eps"].r], writes=[rs.r])
    S.op("dve", lambda e: e.reciprocal(out=rs.t[:], in_=rs.t[:]), reads=[rs.r], writes=[rs.r])
    S.op("dve", lambda e: e.tensor_scalar(out=y.t[:], in0=y.t[:], scalar1=mv.t[:, 0:1], scalar2=rs.t[:, 0:1], op0=ALU.subtract, op1=ALU.mult),
         reads=[y.r, mv.r, rs.r], writes=[y.r])
    S.op("pool", lambda e: e.tensor_tensor(out=y.t[:], in0=y.t[:], in1=g_b.t[:], op=ALU.mult), reads=[y.r, g_b.r], writes=[y.r])
    S.op("pool", lambda e: e.tensor_tensor(out=out.t[:], in0=y.t[:], in1=b_b.t[:], op=ALU.add), reads=[y.r, b_b.r], writes=[out.r])


def bcast_row(S, A, name, src1d, n):
    b = Buf(A.sb(name, [128, n], F32))
    S.dma("sp", b.t[:], src1d.rearrange("(o n) -> o n", o=1).partition_broadcast(128), writes=[b.r])
    return b


def phase3(nc, S, T, xin, r_xin, l, sc):
    A = Alloc(nc)
    identf = make_ident(A, S, F32)
    wout = Buf(A.sb("wout", [128, 8, DM], BF16))
    S.dma("pool", wout.t[:], T["w_out"][l].rearrange("(c p) n -> p c n", p=128), writes=[wout.r])
    wr = Buf(A.sb("wr", [128, 8, 20], F32))
    S.dma("sp", wr.t[:, :, 0:4], T["w_group"][l].rearrange("(c p) n -> p c n", p=128), writes=[wr.r])
    S.dma("sp", wr.t[:, :, 4:20], T["w_expert"][l].rearrange("(c p) n -> p c n", p=128), writes=[wr.r])
    brt = Buf(A.sb("brt", [128, 20], F32))
    S.dma("sp", brt.t[:, 0:4], T["b_group"][l].rearrange("(o n) -> o n", o=1).partition_broadcast(128), writes=[brt.r])
    S.dma("sp", brt.t[:, 4:20], T["b_expert"][l].rearrange("(o n) -> o n", o=1).partition_broadcast(128), writes=[brt.r])
    g_b = bcast_row(S, A, "ln1g", T["ln1_g"][l], DM)
    b_b = bcast_row(S, A, "ln1b", T["ln1_b"][l], DM)
    on = rot(A, "sb", "on", [128, 8, 512], BF16, 2)
    xs = rot(A, "sb", "xs3", [128, DM], F32, 2)
    y = rot(A, "sb", "y3", [128, DM], F32, 2)
    x1 = rot(A, "sb", "x1o", [128, DM], F32, 2)
    xtf = rot(A, "sb", "xtf", [128, 8, 128], F32, 2)
    xtb = rot(A, "sb", "xtb", [128, 8, 512], BF16, 2)
    gate = Buf(A.sb("gate", [128, NB, 16], F32))
    if ROUTED:
        x1b = Buf(A.sb("x1b", [128, NB, DM], BF16))
        gohall = Buf(A.sb("gohall", [128, NB, 4], F32))
    ph = rot(A, "ps", "ph", [128, DM], F32, 2)
    ptr = rot(A, "ps", "ptr", [128, DM], F32, 1)
    plg = rot(A, "ps", "plg", [128, 512], F32, 2)
    epsl = Buf(A.sb("epsl", [128, 1], F32))
    S.op("pool", lambda e: e.memset(epsl.t[:], LN_EPS), writes=[epsl.r])
    small = [dict(st6=Buf(A.sb("st6", [128, 2, 6], F32)), mv=Buf(A.sb("mv", [128, 2], F32)), rs=Buf(A.sb("rs", [128, 1], F32)), eps=epsl) for _ in range(2)]
    gt = [{k: Buf(A.sb("g_" + k, shp, F32)) for k, shp in (("lg", [128, 20]), ("m", [128, 1]), ("goh", [128, 4]), ("ex", [128, 4]), ("se", [128, 1]),
                                                           ("t44", [128, 4, 4]), ("es", [128, 4]), ("m1", [128, 1]), ("oh1", [128, 4]), ("es2", [128, 4]),
                                                           ("m2", [128, 1]), ("oh2", [128, 4]), ("d", [128, 1]), ("p1", [128, 1]), ("p2", [128, 1]),
                                                           ("gi", [128, 4]), ("nm", [128, 1]))} for _ in range(2)]
    for j in range(NT):
        o_ = on[j % 2]
        S.dma("sp", o_.t[:], sc["OnT"][:, :, j * 512:(j + 1) * 512].rearrange("c p t -> p c t"), reads=[sc["r_OnT"]], writes=[o_.r])
        xb_ = xtb[j % 2]
        for bb in range(4):
            b = j * 4 + bb
            s_, y_, x1_, xf_, p_, sm, G = xs[b % 2], y[b % 2], x1[b % 2], xtf[b % 2], ph[b % 2], small[b % 2], gt[b % 2]
            S.dma("sp", s_.t[:], xin[b * 128:(b + 1) * 128, :], reads=[r_xin], writes=[s_.r])
            for hf in range(2):
                for c in range(8):
                    S.op("pe", lambda e, hf=hf, c=c, bb=bb, o_=o_, p_=p_: e.matmul(p_.t[:, hf * 512:(hf + 1) * 512], lhsT=o_.t[:, c, bb * 128:(bb + 1) * 128],
                                                                            rhs=wout.t[:, c, hf * 512:(hf + 1) * 512], start=(c == 0), stop=(c == 7)),
                         reads=[o_.r, wout.r], writes=[p_.r])
            S.op("dve", lambda e, s_=s_, y_=y_, p_=p_: e.scalar_tensor_tensor(out=y_.t[:], in0=s_.t[:], scalar=ALPHA, in1=p_.t[:], op0=ALU.mult, op1=ALU.add),
                 reads=[s_.r, p_.r], writes=[y_.r])
            layernorm_block(S, y_, g_b, b_b, sm, x1_)
            S.dma("sp", sc["X1"][b * 128:(b + 1) * 128, :], x1_.t[:], reads=[x1_.r], writes=[sc["r_X1"]])
            if ROUTED:
                S.op("pool", lambda e, b=b, x1_=x1_: e.tensor_copy(out=x1b.t[:, b, :], in_=x1_.t[:]), reads=[x1_.r], writes=[x1b.r])
            pt = ptr[0]
            for c in range(8):
                S.op("pe", lambda e, c=c, x1_=x1_, pt=pt: e.transpose(out=pt.t[:, c * 128:(c + 1) * 128], in_=x1_.t[:, c * 128:(c + 1) * 128], identity=identf.t[:]),
                     reads=[x1_.r, identf.r], writes=[pt.r])
            S.op("act", lambda e, pt=pt, xf_=xf_: e.activation(out=xf_.t[:, :, :], in_=pt.t[:, :].rearrange("p (c t) -> p c t", c=8), func=AF.Copy),
                 reads=[pt.r], writes=[xf_.r])
            if not ROUTED:
                S.op("dve", lambda e, pt=pt, xb_=xb_, bb=bb: e.tensor_copy(out=xb_.t[:, :, bb * 128:(bb + 1) * 128], in_=pt.t[:, :].rearrange("p (c t) -> p c t", c=8)),
                     reads=[pt.r], writes=[xb_.r])
            pl = plg[b % 2]
            for c in range(8):
                S.op("pe", lambda e, c=c, xf_=xf_, pl=pl: e.matmul(pl.t[:, 0:20], lhsT=xf_.t[:, c, :], rhs=wr.t[:, c, :], start=(c == 0), stop=(c == 7)),
                     reads=[xf_.r, wr.r], writes=[pl.r])
            def V(name, fn, reads, writes):
                S.op("dve", fn, reads=[G[k].r if isinstance(k, str) else k for k in reads], writes=[G[k].r if isinstance(k, str) else k for k in writes])
            lg = G["lg"].t
            V("lg", lambda e, pl=pl, lg=lg: e.tensor_tensor(out=lg[:], in0=pl.t[:, 0:20], in1=brt.t[:], op=ALU.add), [pl.r, brt.r], ["lg"])
            V("m", lambda e, G=G, lg=lg: e.tensor_reduce(out=G["m"].t[:], in_=lg[:, 0:4], axis=AX.X, op=ALU.max), ["lg"], ["m"])
            V("goh", lambda e, G=G, lg=lg: e.tensor_scalar(out=G["goh"].t[:], in0=lg[:, 0:4], scalar1=G["m"].t[:, 0:1], scalar2=None, op0=ALU.is_equal), ["lg", "m"], ["goh"])
            if ROUTED:
                V("gohc", lambda e, G=G, b=b: e.tensor_copy(out=gohall.t[:, b, :], in_=G["goh"].t[:]), ["goh"], [gohall.r])
            V("nm", lambda e, G=G: e.tensor_scalar(out=G["nm"].t[:], in0=G["m"].t[:], scalar1=-1.0, scalar2=None, op0=ALU.mult), ["m"], ["nm"])
            S.op("act", lambda e, G=G, lg=lg: e.activation(out=G["ex"].t[:], in_=lg[:, 0:4], func=AF.Exp, bias=G["nm"].t[:, 0:1]), reads=[G["lg"].r, G["nm"].r], writes=[G["ex"].r])
            V("se", lambda e, G=G: e.tensor_reduce(out=G["se"].t[:], in_=G["ex"].t[:], axis=AX.X, op=ALU.add), ["ex"], ["se"])
            V("se2", lambda e, G=G: e.reciprocal(out=G["se"].t[:], in_=G["se"].t[:]), ["se"], ["se"])
            V("t44", lambda e, G=G, lg=lg: e.tensor_tensor(out=G["t44"].t[:], in0=lg[:, 4:20].rearrange("p (g x) -> p g x", g=4),
                                                         in1=G["goh"].t[:, :].unsqueeze(2).to_broadcast([128, 4, 4]), op=ALU.mult), ["lg", "goh"], ["t44"])
            V("es", lambda e, G=G: e.tensor_reduce(out=G["es"].t[:], in_=G["t44"].t[:, :, :].rearrange("p g x -> p x g"), axis=AX.X, op=ALU.add), ["t44"], ["es"])
            V("m1", lambda e, G=G: e.tensor_reduce(out=G["m1"].t[:], in_=G["es"].t[:], axis=AX.X, op=ALU.max), ["es"], ["m1"])
            V("oh1", lambda e, G=G: e.tensor_scalar(out=G["oh1"].t[:], in0=G["es"].t[:], scalar1=G["m1"].t[:, 0:1], scalar2=None, op0=ALU.is_equal), ["es", "m1"], ["oh1"])
            V("es2", lambda e, G=G: e.scalar_tensor_tensor(out=G["es2"].t[:], in0=G["oh1"].t[:], scalar=-1e30, in1=G["es"].t[:], op0=ALU.mult, op1=ALU.add),
              ["oh1", "es"], ["es2"])
            V("m2", lambda e, G=G: e.tensor_reduce(out=G["m2"].t[:], in_=G["es2"].t[:], axis=AX.X, op=ALU.max), ["es2"], ["m2"])
            V("oh2", lambda e, G=G: e.tensor_scalar(out=G["oh2"].t[:], in0=G["es2"].t[:], scalar1=G["m2"].t[:, 0:1], scalar2=None, op0=ALU.is_equal), ["es2", "m2"], ["oh2"])
            V("d", lambda e, G=G: e.tensor_tensor(out=G["d"].t[:], in0=G["m2"].t[:], in1=G["m1"].t[:], op=ALU.subtract), ["m1", "m2"], ["d"])
            S.op("act", lambda e, G=G: e.activation(out=G["d"].t[:], in_=G["d"].t[:], func=AF.Exp), reads=[G["d"].r], writes=[G["d"].r])
            V("p1", lambda e, G=G: e.tensor_scalar(out=G["p1"].t[:], in0=G["d"].t[:], scalar1=1.0, scalar2=None, op0=ALU.add), ["d"], ["p1"])
            V("p1r", lambda e, G=G: e.reciprocal(out=G["p1"].t[:], in_=G["p1"].t[:]), ["p1"], ["p1"])
            V("p2", lambda e, G=G: e.tensor_tensor(out=G["p2"].t[:], in0=G["d"].t[:], in1=G["p1"].t[:], op=ALU.mult), ["d", "p1"], ["p2"])
            V("gi", lambda e, G=G: e.tensor_scalar(out=G["gi"].t[:], in0=G["oh1"].t[:], scalar1=G["p1"].t[:, 0:1], scalar2=None, op0=ALU.mult), ["oh1", "p1"], ["gi"])
            V("gi2", lambda e, G=G: e.scalar_tensor_tensor(out=G["gi"].t[:], in0=G["oh2"].t[:], scalar=G["p2"].t[:, 0:1], in1=G["gi"].t[:], op0=ALU.mult, op1=ALU.add),
              ["oh2", "p2", "gi"], ["gi"])
            V("gi3", lambda e, G=G: e.tensor_scalar(out=G["gi"].t[:], in0=G["gi"].t[:], scalar1=G["se"].t[:, 0:1], scalar2=None, op0=ALU.mult), ["gi", "se"], ["gi"])
            V("gate", lambda e, G=G, b=b: e.tensor_tensor(out=gate.t[:, b, :].rearrange("p (g x) -> p g x", g=4),
                                                        in0=G["goh"].t[:, :].unsqueeze(2).to_broadcast([128, 4, 4]),
                                                        in1=G["gi"].t[:, :].unsqueeze(1).to_broadcast([128, 4, 4]), op=ALU.mult), ["goh", "gi"], [gate.r])
        if not ROUTED:
            S.dma("sp", sc["X1T"][:, :, j * 512:(j + 1) * 512], xb_.t[:], reads=[xb_.r], writes=[sc["r_X1T"]])
    if not ROUTED:
        S.dma("sp", sc["GATE"], gate.t[:, :, :].rearrange("p b e -> p (b e)"), reads=[gate.r], writes=[sc["r_GATE"]])
    else:
        route_epilogue(S, A, sc, x1b, gate, gohall, plg, l)
    S.flush()
    A.close()


def route_epilogue(S, A, sc, x1b, gate, gohall, plg, l):
    def T_(name, shape, dt=F32):
        return Buf(A.sb(name, shape, dt))
    onesf = T_("onesf", [128, 128])
    tris = T_("tris", [128, 128])
    S.op("pool", lambda e: e.memset(onesf.t[:], 1.0), writes=[onesf.r])
    S.op("pool", lambda e: e.memset(tris.t[:], 1.0), writes=[tris.r])
    S.op("pool", lambda e: e.affine_select(out=tris.t[:], in_=tris.t[:], pattern=[[1, 128]], compare_op=ALU.is_ge, fill=0.0, base=-1,
                                           channel_multiplier=-1), reads=[tris.r], writes=[tris.r])
    pt, pr = plg[0], plg[1]
    for b in range(NB):
        S.op("pe", lambda e, b=b: e.matmul(pt.t[:, b * 4:(b + 1) * 4], lhsT=onesf.t[:], rhs=gohall.t[:, b, :], start=True, stop=True),
             reads=[onesf.r, gohall.r], writes=[pt.r])
        S.op("pe", lambda e, b=b: e.matmul(pr.t[:, b * 4:(b + 1) * 4], lhsT=tris.t[:], rhs=gohall.t[:, b, :], start=True, stop=True),
             reads=[tris.r, gohall.r], writes=[pr.r])
    totb = T_("totb", [128, NB, 4])
    cum = T_("cumb", [128, NB, 4])
    ones32 = T_("ones32", [128, NB])
    S.op("pool", lambda e: e.memset(ones32.t[:], 1.0), writes=[ones32.r])
    S.op("dve", lambda e: e.tensor_copy(out=totb.t[:, :, :], in_=pt.t[:, 0:NB * 4].rearrange("p (b g) -> p b g", g=4)), reads=[pt.r], writes=[totb.r])
    for g in range(4):
        S.op("dve", lambda e, g=g: e.tensor_tensor_scan(out=cum.t[:, :, g], data0=ones32.t[:, :], data1=totb.t[:, :, g], initial=0.0,
                                                        op0=ALU.mult, op1=ALU.add), reads=[ones32.r, totb.r, cum.r], writes=[cum.r])
    boffx = T_("boffx", [128, NB, 4])
    S.op("dve", lambda e: e.tensor_tensor(out=boffx.t[:], in0=cum.t[:], in1=totb.t[:], op=ALU.subtract), reads=[cum.r, totb.r], writes=[boffx.r])
    thr_i = T_("thri", [128, 16], I32)
    thr = T_("thr", [128, 16])
    S.op("pool", lambda e: e.iota(thr_i.t[:], pattern=[[512, 16]], base=0, channel_multiplier=0), writes=[thr_i.r])
    S.op("dve", lambda e: e.tensor_copy(out=thr.t[:], in_=thr_i.t[:]), reads=[thr_i.r], writes=[thr.r])
    cmp = T_("cmp", [128, 4, 8])
    ntl = T_("ntl", [128, 4])
    S.op("dve", lambda e: e.tensor_tensor(out=cmp.t[:], in0=cum.t[:, NB - 1, :].unsqueeze(2).to_broadcast([128, 4, 8]),
                                          in1=thr.t[:, 0:8].unsqueeze(1).to_broadcast([128, 4, 8]), op=ALU.is_gt), reads=[cum.r, thr.r], writes=[cmp.r])
    S.op("dve", lambda e: e.tensor_reduce(out=ntl.t[:], in_=cmp.t[:], axis=AX.X, op=ALU.add), reads=[cmp.r], writes=[ntl.r])
    S.op("dve", lambda e: e.tensor_scalar(out=ntl.t[:], in0=ntl.t[:], scalar1=512.0, scalar2=None, op0=ALU.mult), reads=[ntl.r], writes=[ntl.r])
    pst = T_("pst", [128, 4])
    pen = T_("pen", [128, 4])
    S.op("pool", lambda e: e.memset(pst.t[:], 0.0), writes=[pst.r])
    for g in range(1, 4):
        S.op("dve", lambda e, g=g: e.tensor_tensor(out=pst.t[:, g:g + 1], in0=pst.t[:, g - 1:g], in1=ntl.t[:, g - 1:g], op=ALU.add),
             reads=[pst.r, ntl.r], writes=[pst.r])
    S.op("dve", lambda e: e.tensor_tensor(out=pen.t[:], in0=pst.t[:], in1=ntl.t[:], op=ALU.add), reads=[pst.r, ntl.r], writes=[pen.r])
    v = T_("vdest", [128, NB, 4])
    S.op("dve", lambda e: e.tensor_tensor(out=v.t[:], in0=pr.t[:, 0:NB * 4].rearrange("p (b g) -> p b g", g=4), in1=boffx.t[:], op=ALU.add),
         reads=[pr.r, boffx.r], writes=[v.r])
    S.op("dve", lambda e: e.tensor_tensor(out=v.t[:], in0=v.t[:], in1=pst.t[:, :].unsqueeze(1).to_broadcast([128, NB, 4]), op=ALU.add),
         reads=[v.r, pst.r], writes=[v.r])
    S.op("dve", lambda e: e.tensor_tensor(out=v.t[:], in0=v.t[:], in1=gohall.t[:], op=ALU.mult), reads=[v.r, gohall.r], writes=[v.r])
    destf = T_("destf", [128, NB])
    desti = T_("desti", [128, NB], I32)
    S.op("dve", lambda e: e.tensor_reduce(out=destf.t[:], in_=v.t[:], axis=AX.X, op=ALU.add), reads=[v.r], writes=[destf.r])
    S.op("dve", lambda e: e.tensor_copy(out=desti.t[:], in_=destf.t[:]), reads=[destf.r], writes=[desti.r])
    S.dma("sp", sc["DEST"], desti.t[:], reads=[desti.r], writes=[sc["r_DEST"]])
    cmp2 = T_("cmp2", [128, NTILE, 4])
    gk = T_("gk", [128, NTILE])
    S.op("dve", lambda e: e.tensor_tensor(out=cmp2.t[:], in0=pen.t[:, :].unsqueeze(1).to_broadcast([128, NTILE, 4]),
                                          in1=thr.t[:, 0:NTILE].unsqueeze(2).to_broadcast([128, NTILE, 4]), op=ALU.is_le), reads=[pen.r, thr.r], writes=[cmp2.r])
    S.op("dve", lambda e: e.tensor_reduce(out=gk.t[:], in_=cmp2.t[:], axis=AX.X, op=ALU.add), reads=[cmp2.r], writes=[gk.r])
    S.op("dve", lambda e: e.tensor_scalar(out=gk.t[:], in0=gk.t[:], scalar1=3.0, scalar2=1024.0, op0=ALU.min, op1=ALU.mult), reads=[gk.r], writes=[gk.r])
    S.op("dve", lambda e: e.tensor_scalar(out=gk.t[:], in0=gk.t[:], scalar1=float(l * 4096), scalar2=None, op0=ALU.add), reads=[gk.r], writes=[gk.r])
    cw_i = T_("cwi", [128, 8], I32)
    cw = T_("cw", [128, 8])
    S.op("pool", lambda e: e.iota(cw_i.t[:], pattern=[[256, 4], [1, 2]], base=0, channel_multiplier=2), writes=[cw_i.r])
    S.op("dve", lambda e: e.tensor_copy(out=cw.t[:], in_=cw_i.t[:]), reads=[cw_i.r], writes=[cw.r])
    idxf = T_("idxf", [128, NTILE, 8])
    idxi = T_("idxi", [128, NTILE, 8], I32)
    S.op("dve", lambda e: e.tensor_tensor(out=idxf.t[:], in0=cw.t[:, :].unsqueeze(1).to_broadcast([128, NTILE, 8]),
                                          in1=gk.t[:, :].unsqueeze(2).to_broadcast([128, NTILE, 8]), op=ALU.add), reads=[cw.r, gk.r], writes=[idxf.r])
    S.op("dve", lambda e: e.tensor_copy(out=idxi.t[:], in_=idxf.t[:]), reads=[idxf.r], writes=[idxi.r])
    S.dma("sp", sc["IDXW"], idxi.t[:, :, :].rearrange("p k j -> p (k j)"), reads=[idxi.r], writes=[sc["r_IDXW"]])
    zt = Buf(A.sb("zt", [128, 4 * DM], BF16))
    zg = T_("zg", [128, NTILE * 4 * 16])
    S.op("pool", lambda e: e.memset(zt.t[:], 0.0), writes=[zt.r])
    S.op("pool", lambda e: e.memset(zg.t[:], 0.0), writes=[zg.r])
    for k in range(NTILE):
        S.dma("sp", sc["XS"][k * 512:(k + 1) * 512, :].rearrange("(p r) n -> p (r n)", p=128), zt.t[:], reads=[zt.r], writes=[sc["r_XS"]])
    S.dma("sp", sc["GS"].rearrange("(p r) n -> p (r n)", p=128), zg.t[:], reads=[zg.r], writes=[sc["r_GS"]])
    for b in range(NB):
        S.dma_fn("pool", lambda e, b=b: e.indirect_dma_start(out=sc["XS"][:, :], out_offset=bass.IndirectOffsetOnAxis(ap=desti.t[:, b:b + 1], axis=0),
                                                            in_=x1b.t[:, b, :], in_offset=None), reads=[desti.r, x1b.r], writes=[sc["r_XS"]])
        S.dma_fn("pool", lambda e, b=b: e.indirect_dma_start(out=sc["GS"][:, :], out_offset=bass.IndirectOffsetOnAxis(ap=desti.t[:, b:b + 1], axis=0),
                                                            in_=gate.t[:, b, :], in_offset=None), reads=[desti.r, gate.r], writes=[sc["r_GS"]])


def phase4(nc, S, T, l, sc, xout, r_xout, final, TG=1024):
    A = Alloc(nc)
    g_b = bcast_row(S, A, "ln2g", T["ln2_g"][l], DM)
    b_b = bcast_row(S, A, "ln2b", T["ln2_b"][l], DM)
    gate = Buf(A.sb("gate4", [128, NB, 16], F32))
    S.dma("sp", gate.t[:, :, :].rearrange("p b e -> p (b e)"), sc["GATE"], reads=[sc["r_GATE"]], writes=[gate.r])
    xT = Buf(A.sb("x1T", [128, 8, TG], BF16))
    accb = Buf(A.sb("accm", [128, TG // 128, DM], F32))
    w1 = rot(A, "sb", "w1", [128, 8, DEXP], BF16, 2)
    w3 = rot(A, "sb", "w3", [128, 8, DEXP], BF16, 2)
    w2 = rot(A, "sb", "w2", [128, 4, DM], BF16, 2)
    sa = rot(A, "sb", "sa", [128, 512], F32, 2)
    hT = rot(A, "sb", "hT", [128, 4, 512], BF16, 2)
    xs = rot(A, "sb", "xs4", [128, DM], F32, 2)
    yo = rot(A, "sb", "yo4", [128, DM], F32, 2)
    epsl = Buf(A.sb("epsl4", [128, 1], F32))
    S.op("pool", lambda e: e.memset(epsl.t[:], LN_EPS), writes=[epsl.r])
    small = [dict(st6=Buf(A.sb("st6b", [128, 2, 6], F32)), mv=Buf(A.sb("mvb", [128, 2], F32)), rs=Buf(A.sb("rsb", [128, 1], F32)), eps=epsl) for _ in range(2)]
    pa = rot(A, "ps", "pa", [128, 512], F32, 2)
    pb = rot(A, "ps", "pb", [128, 512], F32, 2)
    py = rot(A, "ps", "py", [128, 512], F32, 3)
    cnt = {"y": 0, "ab": 0, "w": 0}
    W1 = T["w1"][l]
    W3 = T["w3"][l]
    W2 = T["w2"][l]
    for gi in range(SEQ // TG):
        t0 = gi * TG
        S.dma("sp", xT.t[:], sc["X1T"][:, :, t0:t0 + TG], reads=[sc["r_X1T"]], writes=[xT.r])
        for ex in range(NEXP):
            i = cnt["w"]
            cnt["w"] += 1
            a1, a3, a2 = w1[i % 2], w3[i % 2], w2[i % 2]
            S.dma("pool", a1.t[:], W1[ex].rearrange("(c p) n -> p c n", p=128), writes=[a1.r])
            S.dma("pool", a3.t[:], W3[ex].rearrange("(c p) n -> p c n", p=128), writes=[a3.r])
            S.dma("pool", a2.t[:], W2[ex].rearrange("(c p) n -> p c n", p=128), writes=[a2.r])
            for tt in range(TG // 512):
                tc = slice(tt * 512, (tt + 1) * 512)
                h_ = hT[(ex * (TG // 512) + tt) % 2]
                for jc in range(4):
                    k_ = cnt["ab"]
                    cnt["ab"] += 1
                    pa_, pb_, sa_ = pa[k_ % 2], pb[k_ % 2], sa[k_ % 2]
                    for c in range(8):
                        S.op("pe", lambda e, c=c, jc=jc, pa_=pa_, a1=a1, tc=tc: e.matmul(pa_.t[:], lhsT=a1.t[:, c, jc * 128:(jc + 1) * 128], rhs=xT.t[:, c, tc],
                                                                                start=(c == 0), stop=(c == 7)), reads=[a1.r, xT.r], writes=[pa_.r])
                    for c in range(8):
                        S.op("pe", lambda e, c=c, jc=jc, pb_=pb_, a3=a3, tc=tc: e.matmul(pb_.t[:], lhsT=a3.t[:, c, jc * 128:(jc + 1) * 128], rhs=xT.t[:, c, tc],
                                                                                start=(c == 0), stop=(c == 7)), reads=[a3.r, xT.r], writes=[pb_.r])
                    S.op("act", lambda e, pa_=pa_, sa_=sa_: e.activation(out=sa_.t[:], in_=pa_.t[:], func=AF.Silu), reads=[pa_.r], writes=[sa_.r])
                    S.op("dve", lambda e, pb_=pb_, sa_=sa_, h_=h_, jc=jc: e.tensor_tensor(out=h_.t[:, jc, :], in0=pb_.t[:], in1=sa_.t[:], op=ALU.mult),
                         reads=[pb_.r, sa_.r], writes=[h_.r])
                for tb in range(4):
                    blk = tt * 4 + tb
                    gb = (t0 // 128) + blk
                    for hf in range(2):
                        y_ = py[cnt["y"] % 3]
                        cnt["y"] += 1
                        for jc in range(4):
                            S.op("pe", lambda e, jc=jc, tb=tb, hf=hf, y_=y_, h_=h_, a2=a2: e.matmul(y_.t[:], lhsT=h_.t[:, jc, tb * 128:(tb + 1) * 128],
                                                                                           rhs=a2.t[:, jc, hf * 512:(hf + 1) * 512], start=(jc == 0), stop=(jc == 3)),
                                 reads=[h_.r, a2.r], writes=[y_.r])
                        dst = accb.t[:, blk, hf * 512:(hf + 1) * 512]
                        if ex == 0:
                            S.op("dve", lambda e, y_=y_, dst=dst, gb=gb, ex=ex: e.tensor_scalar(out=dst, in0=y_.t[:], scalar1=gate.t[:, gb, ex:ex + 1], scalar2=None, op0=ALU.mult),
                                 reads=[y_.r, gate.r], writes=[accb.r])
                        else:
                            S.op("dve", lambda e, y_=y_, dst=dst, gb=gb, ex=ex: e.scalar_tensor_tensor(out=dst, in0=y_.t[:], scalar=gate.t[:, gb, ex:ex + 1], in1=dst,
                                                                                              op0=ALU.mult, op1=ALU.add), reads=[y_.r, gate.r, accb.r], writes=[accb.r])
        for blk in range(TG // 128):
            gb = (t0 // 128) + blk
            s_, y_, sm = xs[blk % 2], yo[blk % 2], small[blk % 2]
            S.dma("sp", s_.t[:], sc["X1"][gb * 128:(gb + 1) * 128, :], reads=[sc["r_X1"]], writes=[s_.r])
            S.op("dve", lambda e, s_=s_, blk=blk: e.scalar_tensor_tensor(out=s_.t[:], in0=s_.t[:], scalar=ALPHA, in1=accb.t[:, blk, :], op0=ALU.mult, op1=ALU.add),
                 reads=[s_.r, accb.r], writes=[s_.r])
            layernorm_block(S, s_, g_b, b_b, sm, y_)
            S.dma("sp", xout[gb * 128:(gb + 1) * 128, :], y_.t[:], reads=[y_.r], writes=[r_xout], final=final)
    S.flush()
    A.close()


def phase4r(nc, S, T, l, sc, xout, r_xout, final):
    A = Alloc(nc)
    ident = make_ident(A, S, BF16)
    g_b = bcast_row(S, A, "ln2g", T["ln2_g"][l], DM)
    b_b = bcast_row(S, A, "ln2b", T["ln2_b"][l], DM)
    dest = Buf(A.sb("dest4", [128, NB], I32))
    idxw = Buf(A.sb("idxw4", [128, NTILE * 8], I32))
    S.dma("sp", dest.t[:], sc["DEST"], reads=[sc["r_DEST"]], writes=[dest.r])
    S.dma("sp", idxw.t[:], sc["IDXW"], reads=[sc["r_IDXW"]], writes=[idxw.r])
    xs = rot(A, "sb", "xs4r", [128, 4, DM], BF16, 2)
    gs = rot(A, "sb", "gs4r", [128, 4, 16], F32, 2)
    gsel = rot(A, "sb", "gsel", [128, 4, 4], F32, 2)
    xT = rot(A, "sb", "xT4r", [128, 8, 512], BF16, 2)
    accs = rot(A, "sb", "acc4r", [128, 4, DM], F32, 2)
    w1 = rot(A, "sb", "w1r", [128, 8 * DEXP], BF16, 2)
    w3 = rot(A, "sb", "w3r", [128, 8 * DEXP], BF16, 2)
    w2 = rot(A, "sb", "w2r", [128, 4 * DM], BF16, 2)
    sa = rot(A, "sb", "sar", [128, 512], F32, 2)
    hT = rot(A, "sb", "hTr", [128, 4, 512], BF16, 2)
    mt = rot(A, "sb", "mt4", [128, DM], F32, 2)
    xo = rot(A, "sb", "xo4", [128, DM], F32, 2)
    yo = rot(A, "sb", "yo4r", [128, DM], F32, 2)
    epsl = Buf(A.sb("epsl4r", [128, 1], F32))
    S.op("pool", lambda e: e.memset(epsl.t[:], LN_EPS), writes=[epsl.r])
    small = [dict(st6=Buf(A.sb("st6r", [128, 2, 6], F32)), mv=Buf(A.sb("mvr", [128, 2], F32)), rs=Buf(A.sb("rsr", [128, 1], F32)), eps=epsl) for _ in range(2)]
    ptp = rot(A, "ps", "ptp", [128, DM], BF16, 1)
    pa = rot(A, "ps", "par", [128, 512], F32, 2)
    pb = rot(A, "ps", "pbr", [128, 512], F32, 2)
    py = rot(A, "ps", "pyr", [128, 512], F32, 3)
    Wv = [T[nm].rearrange("l e k n -> (l e k n)").rearrange("(r x) -> r x", x=2048) for nm in ("w1", "w3", "w2")]
    cnt = {"y": 0, "ab": 0}
    steps = [(k, j) for k in range(NTILE) for j in range(4)]
    st = {}

    def front(si):
        k, j = steps[si]
        if j == 0:
            x_, g_, gl_, xT_, ac_ = xs[k % 2], gs[k % 2], gsel[k % 2], xT[k % 2], accs[k % 2]
            S.dma("sp", x_.t[:], sc["XS"][k * 512:(k + 1) * 512, :].rearrange("(b p) n -> p b n", p=128), reads=[sc["r_XS"]], writes=[x_.r])
            S.dma("sp", g_.t[:], sc["GS"][k * 512:(k + 1) * 512, :].rearrange("(b p) n -> p b n", p=128), reads=[sc["r_GS"]], writes=[g_.r])
            S.op("dve", lambda e: e.tensor_reduce(out=gl_.t[:], in_=g_.t[:, :, :].rearrange("p b (g j) -> p b j g", g=4), axis=AX.X, op=ALU.add),
                 reads=[g_.r], writes=[gl_.r])
            for blk in range(4):
                p = ptp[0]
                for c in range(8):
                    S.op("pe", lambda e, c=c, blk=blk: e.transpose(out=p.t[:, c * 128:(c + 1) * 128],
                                                                   in_=x_.t[:, blk, :].rearrange("p (pp c) -> p c pp", c=8)[:, c, :], identity=ident.t[:]),
                         reads=[x_.r, ident.r], writes=[p.r])
                S.op("act", lambda e, blk=blk: e.activation(out=xT_.t[:, :, blk * 128:(blk + 1) * 128], in_=p.t[:, :].rearrange("p (c t) -> p c t", c=8), func=AF.Copy),
                     reads=[p.r], writes=[xT_.r])
        xT_, ac_, gl_ = xT[k % 2], accs[k % 2], gsel[k % 2]
        a1, a3, a2, h_ = w1[si % 2], w3[si % 2], w2[si % 2], hT[si % 2]
        for wt_, src in ((a1, Wv[0]), (a3, Wv[1]), (a2, Wv[2])):
            for half in range(2):
                col = k * 8 + j * 2 + half
                S.dma_fn("pool", lambda e, wt_=wt_, src=src, half=half, col=col: e.indirect_dma_start(
                    out=wt_.t[:, half * 2048:(half + 1) * 2048], out_offset=None, in_=src,
                    in_offset=bass.IndirectOffsetOnAxis(ap=idxw.t[:, col:col + 1], axis=0)), reads=[idxw.r], writes=[wt_.r])
        w1v = a1.t[:, :].rearrange("p (c pp q) -> p c q pp", c=8, q=4)
        w3v = a3.t[:, :].rearrange("p (c pp q) -> p c q pp", c=8, q=4)
        for jc in range(4):
            k_ = cnt["ab"]
            cnt["ab"] += 1
            pa_, pb_, sa_ = pa[k_ % 2], pb[k_ % 2], sa[k_ % 2]
            for c in range(8):
                S.op("pe", lambda e, c=c, jc=jc, pa_=pa_: e.matmul(pa_.t[:], lhsT=w1v[:, c, jc, :], rhs=xT_.t[:, c, :], start=(c == 0), stop=(c == 7)),
                     reads=[a1.r, xT_.r], writes=[pa_.r])
            for c in range(8):
                S.op("pe", lambda e, c=c, jc=jc, pb_=pb_: e.matmul(pb_.t[:], lhsT=w3v[:, c, jc, :], rhs=xT_.t[:, c, :], start=(c == 0), stop=(c == 7)),
                     reads=[a3.r, xT_.r], writes=[pb_.r])
            S.op("act", lambda e, pa_=pa_, sa_=sa_: e.activation(out=sa_.t[:], in_=pa_.t[:], func=AF.Silu), reads=[pa_.r], writes=[sa_.r])
            S.op("dve", lambda e, pb_=pb_, sa_=sa_, jc=jc: e.tensor_tensor(out=h_.t[:, jc, :], in0=pb_.t[:], in1=sa_.t[:], op=ALU.mult),
                 reads=[pb_.r, sa_.r], writes=[h_.r])

    def back(si):
        k, j = steps[si]
        ac_, gl_, a2, h_ = accs[k % 2], gsel[k % 2], w2[si % 2], hT[si % 2]
        w2v = a2.t[:, :].rearrange("p (c n) -> p c n", c=4)
        for blk in range(4):
            for hf in range(2):
                y_ = py[cnt["y"] % 3]
                cnt["y"] += 1
                for jc in range(4):
                    S.op("pe", lambda e, jc=jc, blk=blk, hf=hf, y_=y_: e.matmul(y_.t[:], lhsT=h_.t[:, jc, blk * 128:(blk + 1) * 128],
                                                                               rhs=w2v[:, jc, hf * 512:(hf + 1) * 512], start=(jc == 0), stop=(jc == 3)),
                         reads=[h_.r, a2.r], writes=[y_.r])
                dst = ac_.t[:, blk, hf * 512:(hf + 1) * 512]
                if j == 0:
                    S.op("dve", lambda e, y_=y_, dst=dst, blk=blk: e.tensor_scalar(out=dst, in0=y_.t[:], scalar1=gl_.t[:, blk, j:j + 1], scalar2=None, op0=ALU.mult),
                         reads=[y_.r, gl_.r], writes=[ac_.r])
                else:
                    S.op("dve", lambda e, y_=y_, dst=dst, blk=blk: e.scalar_tensor_tensor(out=dst, in0=y_.t[:], scalar=gl_.t[:, blk, j:j + 1], in1=dst,
                                                                                         op0=ALU.mult, op1=ALU.add), reads=[y_.r, gl_.r, ac_.r], writes=[ac_.r])
        if j == 3:
            S.dma("sp", sc["YS"][k * 512:(k + 1) * 512, :].rearrange("(b p) n -> p b n", p=128), ac_.t[:], reads=[ac_.r], writes=[sc["r_YS"]])

    for si in range(len(steps) + 1):
        if si < len(steps):
            front(si)
        if si >= 1:
            back(si - 1)
    for b in range(NB):
        m_, x_, y_, sm = mt[b % 2], xo[b % 2], yo[b % 2], small[b % 2]
        S.dma_fn("pool", lambda e, b=b, m_=m_: e.indirect_dma_start(out=m_.t[:], out_offset=None, in_=sc["YS"][:, :],
                                                                   in_offset=bass.IndirectOffsetOnAxis(ap=dest.t[:, b:b + 1], axis=0)),
                 reads=[dest.r, sc["r_YS"]], writes=[m_.r])
        S.dma("sp", x_.t[:], sc["X1"][b * 128:(b + 1) * 128, :], reads=[sc["r_X1"]], writes=[x_.r])
        S.op("dve", lambda e, m_=m_, x_=x_: e.scalar_tensor_tensor(out=x_.t[:], in0=x_.t[:], scalar=ALPHA, in1=m_.t[:], op0=ALU.mult, op1=ALU.add),
             reads=[x_.r, m_.r], writes=[x_.r])
        layernorm_block(S, x_, g_b, b_b, sm, y_)
        S.dma("sp", xout[b * 128:(b + 1) * 128, :], y_.t[:], reads=[y_.r], writes=[r_xout], final=final)
    S.flush()
    A.close()


def rope_tables():
    pos = np.arange(SEQ, dtype=np.float32)
    out = {}
    for dim, nm in ((64, "rope64"), (32, "rope32")):
        half = dim // 2
        inv = (10000.0 ** (-np.arange(half, dtype=np.float32) / half)).astype(np.float32)
        ang = pos[None, :] * inv[:, None]
        c = np.cos(ang).astype(np.float32)
        s = np.sin(ang).astype(np.float32)
        out[nm + "c"] = np.ascontiguousarray(np.concatenate([c, c], 0))
        out[nm + "s"] = np.ascontiguousarray(np.concatenate([-s, s], 0))
    return out


W_SPECS = [("w_in", [DEPTH, DM, N_IN]), ("b_forget", [DEPTH, 4]), ("g_cq", [DEPTH, 256]), ("g_ckv", [DEPTH, 128]), ("w_uq", [DEPTH, 256, 384]),
           ("w_ukv", [DEPTH, 128, 512]), ("g_head", [DEPTH, 16, 64]), ("w_out", [DEPTH, DM, DM]), ("ln1_g", [DEPTH, DM]), ("ln1_b", [DEPTH, DM]),
           ("w_group", [DEPTH, DM, 4]), ("b_group", [DEPTH, 4]), ("w_expert", [DEPTH, DM, 16]), ("b_expert", [DEPTH, 16]),
           ("w1", [DEPTH, NEXP, DM, DEXP]), ("w3", [DEPTH, NEXP, DM, DEXP]), ("w2", [DEPTH, NEXP, DEXP, DM]), ("ln2_g", [DEPTH, DM]), ("ln2_b", [DEPTH, DM])]


def build_program(nseq=2, layers=(0, 1), phases=(1, 2, 3, 4), debug=False, heads=None, TG=1024):
    nc = bass.Bass("TRN2", target_bir_lowering=False)
    T = {}
    T["x"] = nc.dram_tensor("x", [nseq, SEQ, DM], F32, kind="ExternalInput").ap()
    for nm, shp in W_SPECS:
        T[nm] = nc.dram_tensor(nm, shp, F32, kind="ExternalInput").ap()
    for nm, rows in (("rope64c", 64), ("rope64s", 64), ("rope32c", 32), ("rope32s", 32)):
        T[nm] = nc.dram_tensor(nm, [rows, SEQ], F32, kind="ExternalInput").ap()
    out = nc.dram_tensor("out", [nseq, SEQ, DM], F32, kind="ExternalOutput").ap()
    dk = "ExternalOutput" if debug else "Internal"

    def scratch(name, shape, dt):
        return nc.dram_tensor(name, shape, dt, kind=dk).ap()
    sc = {}
    qs = scratch("QS", [12, 96, SEQ], BF16)
    ks = scratch("KS", [12, 96, SEQ], BF16)
    sc["QS"] = [qs[h] for h in range(12)]
    sc["KS"] = [ks[h] for h in range(12)]
    vs = scratch("VS", [6, 128, NB * 2 * 65], BF16)
    sc["VS"] = [vs[p] for p in range(6)]
    qd = scratch("QD", [3, 4, 64, SEQ], BF16)
    kd = scratch("KD", [3, 4, 64, SEQ], BF16)
    sc["QD"] = [[qd[g, h] for h in range(4)] for g in range(3)]
    sc["KD"] = [[kd[g, h] for h in range(4)] for g in range(3)]
    vd = scratch("VD", [3, 2, 128, NB * 2 * 65], BF16)
    sc["VD"] = [[vd[g, p] for p in range(2)] for g in range(3)]
    sc["OnT"] = scratch("OnT", [8, 128, SEQ], BF16)
    sc["X1"] = scratch("X1", [SEQ, DM], F32)
    sc["X1T"] = scratch("X1T", [128, 8, SEQ], BF16)
    sc["GATE"] = scratch("GATE", [128, NB * 16], F32)
    sc["XS"] = scratch("XS", [NSLOT, DM], BF16)
    sc["GS"] = scratch("GS", [NSLOT, 16], F32)
    sc["YS"] = scratch("YS", [NSLOT, DM], F32)
    sc["DEST"] = scratch("DEST", [128, NB], I32)
    sc["IDXW"] = scratch("IDXW", [128, NTILE * 8], I32)
    xmid = scratch("XMID", [SEQ, DM], F32)
    sc["r_QS"] = [Res() for _ in range(12)]
    sc["r_KS"] = [Res() for _ in range(12)]
    sc["r_VS"] = [Res() for _ in range(6)]
    sc["r_QD"] = [[Res() for _ in range(4)] for _ in range(3)]
    sc["r_KD"] = [[Res() for _ in range(4)] for _ in range(3)]
    sc["r_VD"] = [[Res() for _ in range(2)] for _ in range(3)]
    for k in ("OnT", "X1", "X1T", "GATE", "XS", "GS", "YS", "DEST", "IDXW"):
        sc["r_" + k] = Res()
    r_xmid = Res()
    r_x = Res()
    r_out = Res()
    S = Sched(nc)
    for s in range(nseq):
        for li, l in enumerate(layers):
            xin, r_xin = (T["x"][s], r_x) if li == 0 else (xmid, r_xmid)
            last = li == len(layers) - 1
            xo, r_xo = (out[s], r_out) if last else (xmid, r_xmid)
            if 1 in phases:
                phase1(nc, S, T, xin, r_xin, l, sc)
            if 2 in phases:
                phase2(nc, S, T, l, sc, heads=heads)
            if 3 in phases:
                phase3(nc, S, T, xin, r_xin, l, sc)
            if 4 in phases:
                if ROUTED:
                    phase4r(nc, S, T, l, sc, xo, r_xo, final=last)
                else:
                    phase4(nc, S, T, l, sc, xo, r_xo, final=last, TG=TG)
    S.close()
    return nc, S


_CACHE = {}


def kernel(**inputs):
    n = 8
    nseq = 2
    x = np.ascontiguousarray(np.asarray(inputs["x"], dtype=np.float32))
    tabs = rope_tables()
    if "nc" not in _CACHE:
        _CACHE["nc"] = build_program(nseq=nseq)[0]
    nc = _CACHE["nc"]
    base = {nm: np.ascontiguousarray(np.asarray(inputs[nm], dtype=np.float32)) for nm, _ in W_SPECS}
    base.update(tabs)
    in_maps = []
    for c in range(n):
        m = dict(base)
        m["x"] = x[c * nseq:(c + 1) * nseq]
        in_maps.append(m)
    res = run_bass_kernel_spmd(nc, in_maps, core_ids=list(range(n)))
    return np.concatenate([r["out"] for r in res.results], axis=0).astype(np.float32)
```

```python
import numpy as np
from os import environ as _os_env
import concourse.bass as bass
import concourse.mybir as mybir
from concourse.bass_utils import run_bass_kernel_spmd

F32 = mybir.dt.float32
BF16 = mybir.dt.bfloat16
I32 = mybir.dt.int32
AF = mybir.ActivationFunctionType
ALU = mybir.AluOpType
AX = mybir.AxisListType

SAME_ENGINE_SYNC = bool(int(_os_env.get("SES", "1")))
NDMA_SLOTS = 6

SEQ = 4096
DM = 1024
NB = SEQ // 128
NT = SEQ // 512
DEPTH = 2
ALPHA = (2.0 * DEPTH) ** 0.25
LN_EPS = 1e-5
RMS_EPS = 1e-6
N_IN = 4260
C_SB, C_FOX, C_MLA, C_DIL = 0, 768, 1540, 1956
MLA_SCALE = 96.0 ** -0.5
DIL_R = (1, 4, 16)
NEXP = 16
DEXP = 512
NTILE = 11
NSLOT = NTILE * 512
ROUTED = bool(int(_os_env.get("ROUTED", "1")))


class Res:
    __slots__ = ("w", "r", "excl")

    def __init__(self, excl=False):
        self.w = None
        self.r = []
        self.excl = excl


class Sched:
    ENG = ("pe", "act", "dve", "pool", "sp")

    def __init__(self, nc):
        self.nc = nc
        self.ops = {e: [] for e in self.ENG}
        self.cnt = {e: 0 for e in self.ENG}
        self.seen = {e: {} for e in self.ENG}
        self.sems = {}
        self.dma_slots = {}
        self.dma_rr = {}
        self.final_waits = []
        self._stack = []
        self.nops = 0

    def sem(self, key):
        if key not in self.sems:
            cm = self.nc.semaphore("s_" + "_".join(str(k) for k in (key if isinstance(key, tuple) else (key,))))
            s = cm.__enter__()
            self._stack.append(cm)
            self.sems[key] = s
        return self.sems[key]

    def _deps(self, eng, reads, writes):
        deps = {}

        def add(t):
            if t is None:
                return
            k, v = t
            if deps.get(k, 0) < v:
                deps[k] = v
        for r in reads:
            add(r.w)
            if r.excl:
                for t in r.r:
                    if t[0] != eng:
                        add(t)
        for w in writes:
            add(w.w)
            for t in w.r:
                add(t)
        waits = []
        seen = self.seen[eng]
        for k, v in deps.items():
            if k == eng and (eng == "pe" or not SAME_ENGINE_SYNC):
                continue
            if seen.get(k, 0) >= v:
                continue
            seen[k] = v
            waits.append((k, v))
        return waits

    def _commit(self, ticket, reads, writes):
        for r in reads:
            if len(r.r) > 16:
                m = {}
                for k, v in r.r:
                    if m.get(k, 0) < v:
                        m[k] = v
                r.r = list(m.items())
            r.r.append(ticket)
        for w in writes:
            w.w = ticket
            w.r = []

    def op(self, eng, fn, reads=(), writes=()):
        waits = self._deps(eng, reads, writes)
        self.cnt[eng] += 1
        ticket = (eng, self.cnt[eng])
        self.ops[eng].append((waits, fn, (eng, 1)))
        self._commit(ticket, reads, writes)
        self.nops += 1
        return ticket

    def dma(self, q, out, in_, reads=(), writes=(), final=False, **kw):
        fn = lambda e, out=out, in_=in_, kw=kw: e.dma_start(out=out, in_=in_, **kw)
        return self.dma_fn(q, fn, reads, writes, final)

    def dma_fn(self, q, fn, reads=(), writes=(), final=False):
        waits = self._deps(q, reads, writes)
        if q not in self.dma_slots:
            self.dma_slots[q] = [[("d", q, i), 0] for i in range(NDMA_SLOTS)]
            self.dma_rr[q] = 0
        i = self.dma_rr[q]
        self.dma_rr[q] = (i + 1) % NDMA_SLOTS
        slot = self.dma_slots[q][i]
        key, tot = slot
        if tot > 0 and self.seen[q].get(key, 0) < tot:
            self.seen[q][key] = tot
            waits.append((key, tot))
        slot[1] = tot + 16
        ticket = (key, tot + 16)
        self.ops[q].append((waits, fn, (key, 16)))
        self._commit(ticket, reads, writes)
        self.nops += 1
        if final:
            self.final_waits.append(ticket)
        return ticket

    def flush(self):
        totals = [(e, self.cnt[e]) for e in self.ENG if self.cnt[e] > 0]
        for q, slots in self.dma_slots.items():
            for key, tot in slots:
                if tot > 0:
                    totals.append((key, tot))
        for e in self.ENG:
            self.sem(e)
            for waits, fn, inc in self.ops[e]:
                for k, v in waits:
                    self.sem(k)
                self.sem(inc[0])
        ops = self.ops
        self.ops = {e: [] for e in self.ENG}
        for e in self.ENG:
            for k, v in totals:
                self.seen[e][k] = max(self.seen[e].get(k, 0), v)
        with self.nc.Block() as block:
            def run(engname):
                def body(e):
                    for waits, fn, inc in ops[engname]:
                        for k, v in waits:
                            e.wait_ge(self.sems[k], v)
                        fn(e).then_inc(self.sems[inc[0]], inc[1])
                    for k, v in totals:
                        e.wait_ge(self.sems[k], v)
                return body
            block.sync(run("sp"))
            block.tensor(run("pe"))
            block.scalar(run("act"))
            block.vector(run("dve"))
            block.gpsimd(run("pool"))

    def close(self):
        for cm in reversed(self._stack):
            cm.__exit__(None, None, None)
        self._stack = []


_UID = [0]


class Alloc:
    def __init__(self, nc):
        self.nc = nc
        self.stack = []

    @property
    def n(self):
        _UID[0] += 1
        return _UID[0]

    def sb(self, name, shape, dt):
        cm = self.nc.sbuf_tensor("%s_%d" % (name, self.n), list(shape), dt)
        t = cm.__enter__()
        self.stack.append(cm)
        return t

    def ps(self, name, shape, dt):
        cm = self.nc.psum_tensor("%s_%d" % (name, self.n), list(shape), dt)
        t = cm.__enter__()
        self.stack.append(cm)
        return t

    def close(self):
        for cm in reversed(self.stack):
            cm.__exit__(None, None, None)
        self.stack = []


class Buf:
    def __init__(self, t, excl=False):
        self.t = t
        self.r = Res(excl)


def rot(A, kind, name, shape, dt, n):
    f = A.sb if kind == "sb" else A.ps
    return [Buf(f(name + str(i), shape, dt), excl=(kind == "ps")) for i in range(n)]


def make_ident(A, S, dt):
    b = Buf(A.sb("ident", [128, 128], dt))
    S.op("pool", lambda e: e.memset(b.t[:], 1.0), writes=[b.r])
    S.op("pool", lambda e: e.affine_select(out=b.t[:], in_=b.t[:], pattern=[[-1, 128]], compare_op=ALU.is_equal,
                                           fill=0.0, base=0, channel_multiplier=1), reads=[b.r], writes=[b.r])
    return b


def perm_view(ap2d, r, t0, n):
    if r == 1:
        return ap2d[:, t0:t0 + n]
    sc = SEQ // r
    v = ap2d.rearrange("p (i c) -> p c i", c=r)
    c0, i0 = t0 // sc, t0 % sc
    if i0 + n <= sc:
        return v[:, c0, i0:i0 + n]
    assert i0 == 0 and n % sc == 0
    return v[:, c0:c0 + n // sc, :]


import os as _os
_P1SEC = _os.environ.get("P1SEC", "abcd")
_MSUB = _os.environ.get("MSUB", "qkrvptc")
_MC = _os.environ.get("MC", "1234")


def phase1(nc, S, T, xin, r_xin, l, sc):
    A = Alloc(nc)
    ident = make_ident(A, S, BF16)
    xT = Buf(A.sb("xT", [128, 8, SEQ], BF16))
    xs = rot(A, "sb", "xs", [128, DM], F32, 2)
    xb = rot(A, "sb", "xb", [128, DM], BF16, 2)
    pst = rot(A, "ps", "pst", [128, DM], BF16, 2)
    pj = rot(A, "ps", "pj", [128, 512], F32, 4)
    pv = rot(A, "ps", "pv", [128, 512], F32, 2)
    Win = T["w_in"][l].rearrange("(c p) n -> p c n", p=128)

    for b in range(NB):
        s, c_, p = xs[b % 2], xb[b % 2], pst[b % 2]
        S.dma("sp", s.t[:], xin[b * 128:(b + 1) * 128, :], reads=[r_xin], writes=[s.r])
        S.op("act", lambda e, s=s, c_=c_: e.activation(out=c_.t[:], in_=s.t[:], func=AF.Copy), reads=[s.r], writes=[c_.r])
        for c in range(8):
            S.op("pe", lambda e, c=c, c_=c_, p=p: e.transpose(out=p.t[:, c * 128:(c + 1) * 128], in_=c_.t[:, c * 128:(c + 1) * 128],
                                                              identity=ident.t[:]), reads=[c_.r, ident.r], writes=[p.r])
        S.op("dve", lambda e, b=b, p=p: e.tensor_copy(out=xT.t[:, :, b * 128:(b + 1) * 128],
                                                      in_=p.t[:, :].rearrange("p (c t) -> p c t", c=8)), reads=[p.r], writes=[xT.r])

    wts = rot(A, "sb", "wt", [128, 8, 416], BF16, 2)
    wsw = rot(A, "sb", "wsw", [128, 8, 256], BF16, 2)
    stg = rot(A, "sb", "stg", [128, 512], BF16, 4)
    vst = rot(A, "sb", "vst", [128, NB, 2, 65], BF16, 2)
    tmp1 = rot(A, "sb", "tmp1", [128, 512], F32, 2)
    tmp2 = rot(A, "sb", "tmp2", [128, 512], F32, 2)
    CT = Buf(A.sb("ropeC", [128, SEQ], BF16))
    ST = Buf(A.sb("ropeS", [128, SEQ], BF16))
    for v in vst:
        S.op("pool", lambda e, v=v: e.memset(v.t[:], 1.0), writes=[v.r])
    state = {"w": 0, "pj": 0, "stg": 0, "v": 0, "pv": 0}

    def load_w(col_ranges):
        w = wts[state["w"] % 2]
        state["w"] += 1
        o = 0
        for (c0, n) in col_ranges:
            S.dma("pool", w.t[:, :, o:o + n], Win[:, :, c0:c0 + n], writes=[w.r])
            o += n
        return w

    def nxt(key, lst):
        b = lst[state[key] % len(lst)]
        state[key] += 1
        return b

    def proj_fm(w, o, M, r, j, wtile=None):
        p = nxt("pj", pj)
        wt_ = w if wtile is None else wtile
        for c in range(8):
            S.op("pe", lambda e, c=c, p=p, wt_=wt_: e.matmul(p.t[0:M, :], lhsT=wt_.t[:, c, o:o + M],
                                                             rhs=perm_view(xT.t[:, c, :], r, j * 512, 512),
                                                             start=(c == 0), stop=(c == 7)),
                 reads=[wt_.r, xT.r], writes=[p.r])
        return p

    def store_rows(st, rows, dst, r_dst, j):
        S.dma("sp", dst[:, j * 512:(j + 1) * 512], st.t[rows[0]:rows[1], :], reads=[st.r], writes=[r_dst])

    def v_proj(w, o, r, vt, ncol=128):
        for b4 in range(NB // 4):
            p = nxt("pv", pv)
            for bb in range(4):
                b = b4 * 4 + bb
                for c in range(8):
                    S.op("pe", lambda e, c=c, b=b, bb=bb, p=p: e.matmul(p.t[:, bb * 128:(bb + 1) * 128],
                                                                      lhsT=perm_view(xT.t[:, c, :], r, b * 128, 128),
                                                                      rhs=w.t[:, c, o:o + ncol], start=(c == 0), stop=(c == 7)),
                         reads=[w.r, xT.r], writes=[p.r])
            S.op("dve", lambda e, b4=b4, p=p: e.tensor_copy(
                out=vt.t[:, b4 * 4:(b4 + 1) * 4, :, 0:64],
                in_=p.t[:, :].rearrange("p (b h d) -> p b h d", b=4, h=2)), reads=[p.r], writes=[vt.r])

    def scale_q(w):
        S.op("dve", lambda e: e.tensor_scalar(out=w.t[:, :, 0:128], in0=w.t[:, :, 0:128], scalar1=0.125, scalar2=None,
                                              op0=ALU.mult), reads=[w.r], writes=[w.r])

    for kind, cbase, hbase, pbase in (("sb", C_SB, 0, 0), ("fox", C_FOX, 4, 2)):
        for hp in range(2):
            w = load_w([(cbase + hp * 128, 128), (cbase + 256 + hp * 128, 128), (cbase + 512 + hp * 128, 128)])
            scale_q(w)
            for qk, dst, rd in ((0, sc["QS"], sc["r_QS"]), (1, sc["KS"], sc["r_KS"])):
                for j in range(NT):
                    p = proj_fm(w, qk * 128, 128, 1, j)
                    st = nxt("stg", stg)
                    S.op("act", lambda e, p=p, st=st: e.activation(out=st.t[:], in_=p.t[:], func=AF.Copy), reads=[p.r], writes=[st.r])
                    for hh in range(2):
                        h = hbase + hp * 2 + hh
                        store_rows(st, (hh * 64, hh * 64 + 64), dst[h][0:64, :], rd[h], j)
            vt = nxt("v", vst)
            v_proj(w, 256, 1, vt)
            S.dma("sp", sc["VS"][pbase + hp], vt.t[:, :, :, :].rearrange("p b h d -> p (b h d)"), reads=[vt.r], writes=[sc["r_VS"][pbase + hp]])

    S.flush()
    if "b" not in _P1SEC:
        A.close()
        return
    A2 = Alloc(nc)
    wf = Buf(A2.sb("wf", [128, 8, 4], BF16))
    S.dma("pool", wf.t[:], Win[:, :, C_FOX + 768:C_FOX + 772], writes=[wf.r])
    bfg = Buf(A2.sb("bfg", [4, 1], F32))
    S.dma("sp", bfg.t[:], T["b_forget"][l].rearrange("(h o) -> h o", o=1), writes=[bfg.r])
    S.op("dve", lambda e: e.tensor_scalar(out=bfg.t[:], in0=bfg.t[:], scalar1=-1.0, scalar2=None, op0=ALU.mult), reads=[bfg.r], writes=[bfg.r])
    nlf = Buf(A2.sb("nlf", [4, SEQ], F32))
    ncum = Buf(A2.sb("ncum", [4, SEQ], F32))
    ones4 = Buf(A2.sb("ones4", [4, 512], F32))
    S.op("pool", lambda e: e.memset(ones4.t[:], 1.0), writes=[ones4.r])
    for j in range(NT):
        p = proj_fm(wf, 0, 4, 1, j)
        S.op("act", lambda e, p=p, j=j: e.activation(out=nlf.t[:, j * 512:(j + 1) * 512], in_=p.t[0:4, :], func=AF.Exp,
                                                     bias=bfg.t[:, 0:1], scale=-1.0), reads=[p.r, bfg.r], writes=[nlf.r])
    S.op("act", lambda e: e.activation(out=nlf.t[:], in_=nlf.t[:], func=AF.Ln, bias=1.0), reads=[nlf.r], writes=[nlf.r])
    for j in range(NT):
        sl = slice(j * 512, (j + 1) * 512)
        init = 0.0 if j == 0 else ncum.t[:, j * 512 - 1:j * 512]
        S.op("dve", lambda e, sl=sl, init=init: e.tensor_tensor_scan(out=ncum.t[:, sl], data0=ones4.t[:, :], data1=nlf.t[:, sl],
                                                                     initial=init, op0=ALU.mult, op1=ALU.add),
             reads=[ones4.r, nlf.r, ncum.r], writes=[ncum.r])
    parts = [Buf(A2.sb("cpart%d" % i, [4, SEQ], BF16)) for i in range(3)]
    for i in range(3):
        S.op("dve", lambda e, i=i: e.tensor_copy(out=parts[i].t[:], in_=ncum.t[:]), reads=[ncum.r], writes=[parts[i].r])
        if i < 2:
            S.op("dve", lambda e, i=i: e.tensor_tensor(out=ncum.t[:], in0=ncum.t[:], in1=parts[i].t[:], op=ALU.subtract),
                 reads=[ncum.r, parts[i].r], writes=[ncum.r])
    ones3 = Buf(A2.sb("ones3", [35, SEQ], BF16))
    S.op("pool", lambda e: e.memset(ones3.t[0:3, :], 1.0), writes=[ones3.r])
    S.op("pool", lambda e: e.memset(ones3.t[32:35, :], -1.0), writes=[ones3.r])
    for h in range(4):
        H = 4 + h
        S.dma("sp", sc["KS"][H][64:67, :], ones3.t[32:35, :], reads=[ones3.r], writes=[sc["r_KS"][H]])
        S.dma("sp", sc["QS"][H][67:70, :], ones3.t[0:3, :], reads=[ones3.r], writes=[sc["r_QS"][H]])
        for i in range(3):
            S.dma("sp", sc["KS"][H][67 + i:68 + i, :], parts[i].t[h:h + 1, :], reads=[parts[i].r], writes=[sc["r_KS"][H]])
            S.dma("sp", sc["QS"][H][64 + i:65 + i, :], parts[i].t[h:h + 1, :], reads=[parts[i].r], writes=[sc["r_QS"][H]])
    S.flush()
    A2.close()

    if "c" not in _P1SEC:
        A.close()
        return
    w = load_w([(C_MLA, 416)])
    wkrs = nxt("w", wsw) if False else wsw[0]
    if "p" in _MSUB:
        S.op("pool", lambda e: e.tensor_copy(out=wkrs.t[:, :, 0:16], in_=w.t[:, :, 400:416]), reads=[w.r], writes=[wkrs.r])
        S.op("pool", lambda e: e.tensor_copy(out=wkrs.t[:, :, 16:32], in_=w.t[:, :, 384:400]), reads=[w.r], writes=[wkrs.r])
    A3 = Alloc(nc)
    wuq = Buf(A3.sb("wuq", [128, 2, 384], BF16))
    wuqs = Buf(A3.sb("wuqs", [128, 2, 384], BF16))
    wukv = Buf(A3.sb("wukv", [128, 512], BF16))
    S.dma("pool", wuq.t[:], T["w_uq"][l].rearrange("(c p) n -> p c n", p=128), writes=[wuq.r])
    S.dma("pool", wukv.t[:], T["w_ukv"][l], writes=[wukv.r])
    S.op("pool", lambda e: e.tensor_copy(out=wuqs.t[:], in_=wuq.t[:]), reads=[wuq.r], writes=[wuqs.r])
    for c2 in (range(2) if "p" in _MSUB else []):
        v4o = wuqs.t[:, c2, :].rearrange("p (h d) -> p h d", h=4)
        v4i = wuq.t[:, c2, :].rearrange("p (h d) -> p h d", h=4)
        S.op("pool", lambda e, v4o=v4o, v4i=v4i: e.tensor_copy(out=v4o[:, :, 64:80], in_=v4i[:, :, 80:96]), reads=[wuq.r, wuqs.r], writes=[wuqs.r])
        S.op("pool", lambda e, v4o=v4o, v4i=v4i: e.tensor_copy(out=v4o[:, :, 80:96], in_=v4i[:, :, 64:80]), reads=[wuq.r, wuqs.r], writes=[wuqs.r])
    gcq = Buf(A3.sb("gcq", [128, 2], F32))
    gckv = Buf(A3.sb("gckv", [128, 1], F32))
    for c2 in range(2):
        S.dma("sp", gcq.t[:, c2:c2 + 1], T["g_cq"][l][c2 * 128:(c2 + 1) * 128].rearrange("(p o) -> p o", o=1), writes=[gcq.r])
    S.dma("sp", gckv.t[:], T["g_ckv"][l].rearrange("(p o) -> p o", o=1), writes=[gckv.r])
    for tb, nm in (((CT, "rope32c"), (ST, "rope32s")) if "t" in _MSUB else []):
        S.dma("pool", tb.t[0:32, :], T[nm], writes=[tb.r])
        S.dma("pool", tb.t[64:96, :], T[nm], writes=[tb.r])
    onesq = Buf(A3.sb("onesq", [128, 128], BF16))
    oneskv = Buf(A3.sb("oneskv", [128, 128], BF16))
    epst = Buf(A3.sb("epst", [128, 1], F32))
    S.op("pool", lambda e: e.memset(epst.t[:], RMS_EPS), writes=[epst.r])
    S.op("pool", lambda e: e.memset(onesq.t[:], 1.0 / 256.0), writes=[onesq.r])
    S.op("pool", lambda e: e.memset(oneskv.t[:], 1.0 / 128.0), writes=[oneskv.r])
    cqg = rot(A3, "sb", "cqg", [128, 2, 512], BF16, 2)
    cq2 = rot(A3, "sb", "cq2", [128, 2, 512], BF16, 2)
    ckg = rot(A3, "sb", "ckg", [128, 512], BF16, 2)
    ck2 = rot(A3, "sb", "ck2", [128, 512], BF16, 2)
    rq = rot(A3, "sb", "rq", [128, 512], F32, 2)
    rkv = rot(A3, "sb", "rkv", [128, 512], F32, 2)
    rtok = rot(A3, "sb", "rtok", [128, 1], F32, 2)
    vstm = Buf(A3.sb("vstm", [128, NB, 4, 65], BF16))
    S.op("pool", lambda e: e.memset(vstm.t[:], 1.0), writes=[vstm.r])
    wukv_v = wukv.t[:, :].rearrange("p (h x) -> p h x", h=4)[:, :, 64:128]
    for j in (range(NT) if "c" in _MSUB else []):
        tc = slice(j * 512, (j + 1) * 512)
        a, a2, kg, k2, rq_, rkv_ = cqg[j % 2], cq2[j % 2], ckg[j % 2], ck2[j % 2], rq[j % 2], rkv[j % 2]
        for c2 in (range(2) if "1" in _MC else []):
            p = proj_fm(w, c2 * 128, 128, 1, j)
            S.op("dve", lambda e, p=p, c2=c2, a=a: e.tensor_scalar(out=a.t[:, c2, :], in0=p.t[:], scalar1=gcq.t[:, c2:c2 + 1], scalar2=None,
                                                                  op0=ALU.mult), reads=[p.r, gcq.r], writes=[a.r])
            S.op("act", lambda e, p=p, c2=c2, a2=a2: e.activation(out=a2.t[:, c2, :], in_=p.t[:], func=AF.Square), reads=[p.r], writes=[a2.r])
        if "2" in _MC:
            p = proj_fm(w, 256, 128, 1, j)
            if "5" not in _MC:
                S.op("dve", lambda e, p=p, kg=kg: e.tensor_scalar(out=kg.t[:], in0=p.t[:], scalar1=gckv.t[:, 0:1], scalar2=None, op0=ALU.mult),
                     reads=[p.r, gckv.r], writes=[kg.r])
            if "6" not in _MC:
                S.op("act", lambda e, p=p, k2=k2: e.activation(out=k2.t[:], in_=p.t[:], func=AF.Square), reads=[p.r], writes=[k2.r])
        if "3" in _MC:
            p = nxt("pj", pj)
            for c2 in range(2):
                S.op("pe", lambda e, p=p, c2=c2, a2=a2: e.matmul(p.t[:], lhsT=onesq.t[:], rhs=a2.t[:, c2, :], start=(c2 == 0), stop=(c2 == 1)),
                     reads=[onesq.r, a2.r], writes=[p.r])
            S.op("act", lambda e, p=p, rq_=rq_: e.activation(out=rq_.t[:], in_=p.t[:], func=AF.Sqrt, bias=epst.t[:, 0:1]), reads=[p.r, epst.r], writes=[rq_.r])
            S.op("dve", lambda e, rq_=rq_: e.reciprocal(out=rq_.t[:], in_=rq_.t[:]), reads=[rq_.r], writes=[rq_.r])
        if "4" in _MC:
            p = nxt("pj", pj)
            S.op("pe", lambda e, p=p, k2=k2: e.matmul(p.t[:], lhsT=oneskv.t[:], rhs=k2.t[:], start=True, stop=True), reads=[oneskv.r, k2.r], writes=[p.r])
            S.op("act", lambda e, p=p, rkv_=rkv_: e.activation(out=rkv_.t[:], in_=p.t[:], func=AF.Sqrt, bias=epst.t[:, 0:1]), reads=[p.r, epst.r], writes=[rkv_.r])
            S.op("dve", lambda e, rkv_=rkv_: e.reciprocal(out=rkv_.t[:], in_=rkv_.t[:]), reads=[rkv_.r], writes=[rkv_.r])
        for h in (range(4) if "q" in _MSUB else []):
            H = 8 + h
            pa, pb = nxt("pj", pj), nxt("pj", pj)
            for pp, ww in ((pa, wuq), (pb, wuqs)):
                for c2 in range(2):
                    S.op("pe", lambda e, pp=pp, ww=ww, c2=c2, h=h, a=a: e.matmul(pp.t[0:96, :], lhsT=ww.t[:, c2, h * 96:(h + 1) * 96], rhs=a.t[:, c2, :],
                                                                             start=(c2 == 0), stop=(c2 == 1)), reads=[ww.r, a.r], writes=[pp.r])
            st = nxt("stg", stg)
            t1, t2 = tmp1[h % 2], tmp2[h % 2]
            S.op("dve", lambda e, pa=pa, st=st, rq_=rq_: e.scalar_tensor_tensor(out=st.t[0:64, :], in0=pa.t[0:64, :], scalar=MLA_SCALE, in1=rq_.t[0:64, :],
                                                                             op0=ALU.mult, op1=ALU.mult), reads=[pa.r, rq_.r], writes=[st.r])
            S.op("dve", lambda e, pa=pa, t1=t1, tc=tc: e.tensor_tensor(out=t1.t[64:96, :], in0=pa.t[64:96, :], in1=CT.t[64:96, tc], op=ALU.mult),
                 reads=[pa.r, CT.r], writes=[t1.r])
            S.op("dve", lambda e, pb=pb, t2=t2, tc=tc: e.tensor_tensor(out=t2.t[64:96, :], in0=pb.t[64:96, :], in1=ST.t[64:96, tc], op=ALU.mult),
                 reads=[pb.r, ST.r], writes=[t2.r])
            S.op("pool", lambda e, t1=t1, t2=t2: e.tensor_tensor(out=t1.t[64:96, :], in0=t1.t[64:96, :], in1=t2.t[64:96, :], op=ALU.add),
                 reads=[t1.r, t2.r], writes=[t1.r])
            S.op("dve", lambda e, t1=t1, st=st, rq_=rq_: e.scalar_tensor_tensor(out=st.t[64:96, :], in0=t1.t[64:96, :], scalar=MLA_SCALE, in1=rq_.t[64:96, :],
                                                                             op0=ALU.mult, op1=ALU.mult), reads=[t1.r, rq_.r, st.r], writes=[st.r])
            store_rows(st, (0, 96), sc["QS"][H][0:96, :], sc["r_QS"][H], j)
        for h in (range(4) if "k" in _MSUB else []):
            H = 8 + h
            p = nxt("pj", pj)
            S.op("pe", lambda e, p=p, h=h, kg=kg: e.matmul(p.t[0:64, :], lhsT=wukv.t[:, h * 128:h * 128 + 64], rhs=kg.t[:], start=True, stop=True),
                 reads=[wukv.r, kg.r], writes=[p.r])
            st = nxt("stg", stg)
            S.op("dve", lambda e, p=p, st=st, rkv_=rkv_: e.tensor_tensor(out=st.t[0:64, :], in0=p.t[0:64, :], in1=rkv_.t[0:64, :], op=ALU.mult),
                 reads=[p.r, rkv_.r], writes=[st.r])
            store_rows(st, (0, 64), sc["KS"][H][0:64, :], sc["r_KS"][H], j)
        if "r" not in _MSUB:
            continue
        pa = proj_fm(w, 384, 32, 1, j)
        pb = proj_fm(wkrs, 0, 32, 1, j)
        t1, t2 = tmp1[0], tmp2[0]
        st = nxt("stg", stg)
        S.op("dve", lambda e, pa=pa, t1=t1, tc=tc: e.tensor_tensor(out=t1.t[0:32, :], in0=pa.t[0:32, :], in1=CT.t[0:32, tc], op=ALU.mult),
             reads=[pa.r, CT.r], writes=[t1.r])
        S.op("dve", lambda e, pb=pb, t2=t2, tc=tc: e.tensor_tensor(out=t2.t[0:32, :], in0=pb.t[0:32, :], in1=ST.t[0:32, tc], op=ALU.mult),
             reads=[pb.r, ST.r], writes=[t2.r])
        S.op("pool", lambda e, t1=t1, t2=t2, st=st: e.tensor_tensor(out=st.t[0:32, :], in0=t1.t[0:32, :], in1=t2.t[0:32, :], op=ALU.add),
             reads=[t1.r, t2.r], writes=[st.r])
        for h in range(4):
            store_rows(st, (0, 32), sc["KS"][8 + h][64:96, :], sc["r_KS"][8 + h], j)
        for bb in (range(4) if "v" in _MSUB else []):
            b = j * 4 + bb
            p = nxt("pv", pv)
            S.op("pe", lambda e, p=p, bb=bb, kg=kg: e.matmul(p.t[:, 0:256], lhsT=kg.t[:, bb * 128:(bb + 1) * 128], rhs=wukv_v, start=True, stop=True),
                 reads=[wukv.r, kg.r], writes=[p.r])
            S.op("pe", lambda e, p=p, bb=bb, k2=k2: e.matmul(p.t[:, 256:257], lhsT=k2.t[:, bb * 128:(bb + 1) * 128], rhs=oneskv.t[:, 0:1], start=True, stop=True),
                 reads=[oneskv.r, k2.r], writes=[p.r])
            rt = rtok[b % 2]
            S.op("act", lambda e, p=p, rt=rt: e.activation(out=rt.t[:], in_=p.t[:, 256:257], func=AF.Sqrt, bias=epst.t[:, 0:1]), reads=[p.r, epst.r], writes=[rt.r])
            S.op("dve", lambda e, rt=rt: e.reciprocal(out=rt.t[:], in_=rt.t[:]), reads=[rt.r], writes=[rt.r])
            S.op("dve", lambda e, p=p, b=b, rt=rt: e.tensor_scalar(out=vstm.t[:, b, :, 0:64], in0=p.t[:, 0:256].rearrange("p (h d) -> p h d", h=4),
                                                                  scalar1=rt.t[:, 0:1], scalar2=None, op0=ALU.mult), reads=[p.r, rt.r], writes=[vstm.r])
    for hp in range(2):
        S.dma("sp", sc["VS"][4 + hp].rearrange("p (b h d) -> p b h d", b=NB, h=2), vstm.t[:, :, 2 * hp:2 * hp + 2, :], reads=[vstm.r], writes=[sc["r_VS"][4 + hp]])

    S.flush()
    A3.close()
    if "d" not in _P1SEC:
        A.close()
        return
    for tb, nm in ((CT, "rope64c"), (ST, "rope64s")):
        S.dma("pool", tb.t[0:64, :], T[nm], writes=[tb.r])
        S.dma("pool", tb.t[64:128, :], T[nm], writes=[tb.r])
    for g in range(3):
        r = DIL_R[g]
        for hp in range(2):
            o = g * 256 + hp * 128
            w = load_w([(C_DIL + o, 128), (C_DIL + 768 + o, 128), (C_DIL + 1536 + o, 128)])
            scale_q(w)
            ws = wsw[(g * 2 + hp) % 2]
            for c in range(8):
                vo = ws.t[:, c, :].rearrange("p (h f d) -> p h f d", h=4, f=2)
                vi = w.t[:, c, 0:256].rearrange("p (h f d) -> p h f d", h=4, f=2)
                S.op("pool", lambda e, vo=vo, vi=vi: e.tensor_copy(out=vo[:, :, 0, :], in_=vi[:, :, 1, :]), reads=[w.r], writes=[ws.r])
                S.op("pool", lambda e, vo=vo, vi=vi: e.tensor_copy(out=vo[:, :, 1, :], in_=vi[:, :, 0, :]), reads=[w.r], writes=[ws.r])
            for qk, dst, rd in ((0, sc["QD"], sc["r_QD"]), (1, sc["KD"], sc["r_KD"])):
                for j in range(NT):
                    pa = proj_fm(w, qk * 128, 128, r, j)
                    pb = proj_fm(ws, qk * 128, 128, r, j)
                    t1, t2 = tmp1[j % 2], tmp2[j % 2]
                    st = nxt("stg", stg)
                    cv = perm_view(CT.t[:, :], r, j * 512, 512)
                    sv = perm_view(ST.t[:, :], r, j * 512, 512)
                    shp = None if len(cv.shape) == 2 else cv.shape

                    def v3(ap):
                        return ap if shp is None else ap.rearrange("p (a b) -> p a b", a=shp[1])
                    S.op("dve", lambda e, pa=pa, t1=t1, cv=cv, v3=v3: e.tensor_tensor(out=v3(t1.t[:]), in0=v3(pa.t[:]), in1=cv, op=ALU.mult),
                         reads=[pa.r, CT.r], writes=[t1.r])
                    S.op("dve", lambda e, pb=pb, t2=t2, sv=sv, v3=v3: e.tensor_tensor(out=v3(t2.t[:]), in0=v3(pb.t[:]), in1=sv, op=ALU.mult),
                         reads=[pb.r, ST.r], writes=[t2.r])
                    S.op("pool", lambda e, t1=t1, t2=t2, st=st: e.tensor_tensor(out=st.t[:], in0=t1.t[:], in1=t2.t[:], op=ALU.add),
                         reads=[t1.r, t2.r], writes=[st.r])
                    for hh in range(2):
                        store_rows(st, (hh * 64, hh * 64 + 64), dst[g][hp * 2 + hh], rd[g][hp * 2 + hh], j)
            vt = nxt("v", vst)
            v_proj(w, 256, r, vt)
            S.dma("sp", sc["VD"][g][hp], vt.t[:, :, :, :].rearrange("p b h d -> p (b h d)"), reads=[vt.r], writes=[sc["r_VD"][g][hp]])
    S.flush()
    A.close()


def phase2(nc, S, T, l, sc, heads=None):
    A = Alloc(nc)
    negtri = Buf(A.sb("negtri", [128, 128], BF16))
    S.op("pool", lambda e: e.memset(negtri.t[:], -1.0), writes=[negtri.r])
    S.op("pool", lambda e: e.affine_select(out=negtri.t[:], in_=negtri.t[:], pattern=[[-1, 128]], compare_op=ALU.is_ge, fill=0.0, base=0,
                                           channel_multiplier=1), reads=[negtri.r], writes=[negtri.r])
    ones = Buf(A.sb("ones", [128, 128], BF16))
    S.op("pool", lambda e: e.memset(ones.t[:], 1.0), writes=[ones.r])
    wn = Buf(A.sb("wn", [65, 64], BF16))
    wnsb = Buf(A.sb("wnsb", [65, 64], BF16))
    for t_, v_ in ((wn, RMS_EPS), (wnsb, 0.0)):
        S.op("pool", lambda e, t_=t_: e.memset(t_.t[:], 1.0 / 64.0), writes=[t_.r])
        S.op("pool", lambda e, t_=t_, v_=v_: e.memset(t_.t[64:65, :], v_), reads=[t_.r], writes=[t_.r])
    gh = Buf(A.sb("gh", [64, 16], F32))
    for h_ in range(16):
        S.dma("sp", gh.t[:, h_:h_ + 1], T["g_head"][l][h_].rearrange("(d o) -> d o", o=1), writes=[gh.r])
    eps2 = Buf(A.sb("eps2", [64, 2], F32))
    S.op("pool", lambda e: e.memset(eps2.t[:, 0:1], RMS_EPS), writes=[eps2.r])
    S.op("pool", lambda e: e.memset(eps2.t[:, 1:2], 0.0), reads=[eps2.r], writes=[eps2.r])

    Qt = rot(A, "sb", "Qt", [128, SEQ], BF16, 2)
    Kt = rot(A, "sb", "Kt", [128, SEQ], BF16, 2)
    Vt = rot(A, "sb", "Vt", [128, NB, 2, 65], BF16, 2)
    pz = rot(A, "ps", "pz", [128, 512], F32, 3)
    po = rot(A, "ps", "po", [128, 512], F32, 2)
    pc = rot(A, "ps", "pc", [128, 512], F32, 2)
    pss = rot(A, "ps", "pss", [128, 512], F32, 1)
    Pb = rot(A, "sb", "Pb", [128, 512], BF16, 4)
    eb = rot(A, "sb", "eb", [128, 512], F32, 2)
    spb = rot(A, "sb", "spb", [128, 512], BF16, 3)
    lw = rot(A, "sb", "lw", [128, 512], F32, 2)
    Rsb = Buf(A.sb("Rsb", [128, 512], F32))
    sqb = rot(A, "sb", "sqb", [65, 512], BF16, 2)
    osb = rot(A, "sb", "osb", [65, 512], F32, 2)
    def mk_mask(name, n, conds):
        m = Buf(A.sb(name, [128, n], BF16))
        S.op("pool", lambda e: e.memset(m.t[:], 1.0), writes=[m.r])
        for (step, base, cm) in conds:
            S.op("pool", lambda e, step=step, base=base, cm=cm: e.affine_select(out=m.t[:], in_=m.t[:], pattern=[[step, n]], compare_op=ALU.is_ge, fill=0.0,
                                                                                base=base, channel_multiplier=cm), reads=[m.r], writes=[m.r])
        return m
    maskC = [mk_mask("mc%d" % o, 512, [(1, -128 * o, -1)]) for o in range(4)]
    maskS = [mk_mask("ms%d" % o, 512, [(1, -128 * o - 1, -1)]) for o in range(4)]
    maskD = {512: {o: mk_mask("md%d" % (o + 1), 512, [(1, -128 * o, -1), (-1, 128 + 128 * o, 1)]) for o in range(-1, 4)},
             256: {o: mk_mask("me%d" % o, 256, [(1, -128 * o, -1), (-1, 128 + 128 * o, 1)]) for o in range(0, 2)}}
    stb = rot(A, "sb", "stb", [64, 512], F32, 2)
    yb = rot(A, "sb", "yb", [64, 512], BF16, 2)
    acc = rot(A, "sb", "acc", [65, SEQ], F32, 2)
    st = {"fin": 0, "ld": 0, "vld": 0, "o": 0}

    def finish(src_ap, r_src, h, t0, n, is_sb, in_sbuf=False):
        i = st["fin"]
        st["fin"] += 1
        sq, s_, y, ps_ = sqb[i % 2], stb[i % 2], yb[i % 2], pss[0]
        if in_sbuf:
            o_ap, r_o = src_ap, r_src
        else:
            ob = osb[i % 2]
            S.op("act", lambda e: e.activation(out=ob.t[:, 0:n], in_=src_ap, func=AF.Copy), reads=[r_src], writes=[ob.r])
            o_ap, r_o = ob.t[:, 0:n], ob.r
        S.op("dve", lambda e: e.tensor_tensor(out=sq.t[:, 0:n], in0=o_ap, in1=o_ap, op=ALU.mult), reads=[r_o], writes=[sq.r])
        wn_ = wnsb if is_sb else wn
        S.op("pe", lambda e: e.matmul(ps_.t[0:64, 0:n], lhsT=wn_.t[:, :], rhs=sq.t[:, 0:n], start=True, stop=True), reads=[wn_.r, sq.r], writes=[ps_.r])
        S.op("act", lambda e: e.activation(out=s_.t[:, 0:n], in_=ps_.t[0:64, 0:n], func=AF.Ln, bias=(eps2.t[:, 0:1] if is_sb else eps2.t[:, 1:2])),
             reads=[ps_.r, eps2.r], writes=[s_.r])
        S.op("act", lambda e: e.activation(out=s_.t[:, 0:n], in_=s_.t[:, 0:n], func=AF.Exp, scale=-0.5), reads=[s_.r], writes=[s_.r])
        S.op("dve", lambda e: e.scalar_tensor_tensor(out=y.t[:, 0:n], in0=o_ap[0:64], scalar=gh.t[:, h:h + 1], in1=s_.t[:, 0:n],
                                                     op0=ALU.mult, op1=ALU.mult), reads=[r_o, gh.r, s_.r], writes=[y.r])
        S.dma("sp", sc["OnT"][h // 2, (h % 2) * 64:(h % 2) * 64 + 64, t0:t0 + n], y.t[:, 0:n], reads=[y.r], writes=[sc["r_OnT"]])

    def load_qk(qsrc, r_q, ksrc, r_k, kd):
        i = st["ld"]
        st["ld"] += 1
        q, k = Qt[i % 2], Kt[i % 2]
        S.dma("sp", q.t[0:kd, :], qsrc, reads=[r_q], writes=[q.r])
        S.dma("sp", k.t[0:kd, :], ksrc, reads=[r_k], writes=[k.r])
        return q, k

    def load_v(vsrc, r_v):
        i = st["vld"]
        st["vld"] += 1
        v = Vt[i % 2]
        S.dma("sp", v.t[:, :, :, :].rearrange("p b h d -> p (b h d)"), vsrc, reads=[r_v], writes=[v.r])
        return v

    def run_steps(steps, q, k, kd, v, hh, kind, done_cb):
        n_ = len(steps)
        ctx = [dict() for _ in range(n_)]

        def s1(i):
            sp_ = steps[i]
            z = pz[i % 3] if kind != "sb" else pz[i % 2]
            q0, n, kb = sp_["q0"], sp_["n"], sp_["kb"]
            S.op("pe", lambda e: e.matmul(z.t[:, 0:n], lhsT=k.t[0:kd, kb * 128:(kb + 1) * 128], rhs=q.t[0:kd, q0:q0 + n], start=True, stop=True),
                 reads=[k.r, q.r], writes=[z.r])
            if kind == "sb":
                e_, s_ = eb[i % 2], spb[i % 3]
                S.op("act", lambda e: e.activation(out=e_.t[:, 0:n], in_=z.t[:, 0:n], func=AF.Exp), reads=[z.r], writes=[e_.r])
                S.op("act", lambda e: e.activation(out=s_.t[:, 0:n], in_=e_.t[:, 0:n], func=AF.Ln, bias=1.0), reads=[e_.r], writes=[s_.r])
                if sp_["mask"] is not None:
                    mk = sp_["mask"]
                    S.op("pool", lambda e: e.tensor_tensor(out=s_.t[:, 0:n], in0=s_.t[:, 0:n], in1=mk.t[:, 0:n], op=ALU.mult), reads=[s_.r, mk.r], writes=[s_.r])
                ctx[i]["sp"] = s_
            else:
                p_ = Pb[i % 4]
                if kind == "fox" and sp_["mask"] is not None:
                    l_ = lw[i % 2]
                    S.op("dve", lambda e: e.tensor_scalar(out=l_.t[:, 0:n], in0=z.t[:, 0:n], scalar1=60.0, scalar2=None, op0=ALU.min), reads=[z.r], writes=[l_.r])
                    S.op("act", lambda e: e.activation(out=p_.t[:, 0:n], in_=l_.t[:, 0:n], func=AF.Exp), reads=[l_.r], writes=[p_.r])
                else:
                    S.op("act", lambda e: e.activation(out=p_.t[:, 0:n], in_=z.t[:, 0:n], func=AF.Exp), reads=[z.r], writes=[p_.r])
                if sp_["mask"] is not None:
                    mk = sp_["mask"]
                    S.op("dve", lambda e: e.tensor_tensor(out=p_.t[:, 0:n], in0=p_.t[:, 0:n], in1=mk.t[:, 0:n], op=ALU.mult), reads=[p_.r, mk.r], writes=[p_.r])
                ctx[i]["P"] = p_

        def s2(i):
            if kind != "sb":
                return
            sp_ = steps[i]
            q0, n, kb = sp_["q0"], sp_["n"], sp_["kb"]
            s_ = ctx[i]["sp"]
            c_, rc, l_, p_ = pc[i % 2], pz[2], lw[i % 2], Pb[i % 4]
            S.op("pe", lambda e: e.matmul(c_.t[:, 0:n], lhsT=k.t[0:kd, kb * 128:(kb + 1) * 128], rhs=q.t[0:kd, q0:q0 + n], start=True, stop=False),
                 reads=[k.r, q.r], writes=[c_.r])
            S.op("pe", lambda e: e.matmul(c_.t[:, 0:n], lhsT=negtri.t[:], rhs=s_.t[:, 0:n], start=False, stop=True), reads=[negtri.r, s_.r], writes=[c_.r])
            S.op("pe", lambda e: e.matmul(rc.t[:, 0:n], lhsT=ones.t[:], rhs=s_.t[:, 0:n], start=True, stop=True), reads=[ones.r, s_.r], writes=[rc.r])
            if sp_["first"]:
                S.op("dve", lambda e: e.tensor_copy(out=l_.t[:, 0:n], in_=c_.t[:, 0:n]), reads=[c_.r], writes=[l_.r])
                S.op("dve", lambda e: e.tensor_copy(out=Rsb.t[:, 0:n], in_=rc.t[:, 0:n]), reads=[rc.r], writes=[Rsb.r])
            else:
                S.op("dve", lambda e: e.tensor_tensor(out=l_.t[:, 0:n], in0=c_.t[:, 0:n], in1=Rsb.t[:, 0:n], op=ALU.subtract), reads=[c_.r, Rsb.r], writes=[l_.r])
                S.op("dve", lambda e: e.tensor_tensor(out=Rsb.t[:, 0:n], in0=rc.t[:, 0:n], in1=Rsb.t[:, 0:n], op=ALU.add), reads=[rc.r, Rsb.r], writes=[Rsb.r])
            S.op("act", lambda e: e.activation(out=p_.t[:, 0:n], in_=l_.t[:, 0:n], func=AF.Exp), reads=[l_.r], writes=[p_.r])
            if sp_["mask"] is not None:
                mk = sp_["mask"]
                S.op("pool", lambda e: e.tensor_tensor(out=p_.t[:, 0:n], in0=p_.t[:, 0:n], in1=mk.t[:, 0:n], op=ALU.mult), reads=[p_.r, mk.r], writes=[p_.r])
            ctx[i]["P"] = p_

        def s3(i):
            sp_ = steps[i]
            n, kb = sp_["n"], sp_["kb"]
            if sp_["first"]:
                st["o"] += 1
            o_ = po[st["o"] % 2]
            p_ = ctx[i]["P"]
            S.op("pe", lambda e: e.matmul(o_.t[0:65, 0:n], lhsT=v.t[:, kb, hh, :], rhs=p_.t[:, 0:n], start=sp_["first"], stop=sp_["last"]),
                 reads=[v.r, p_.r], writes=[o_.r])
            if sp_["last"]:
                done_cb(o_, sp_)

        for i in range(n_ + 2):
            if i < n_:
                s1(i)
            if 0 <= i - 1 < n_:
                s2(i - 1)
            if 0 <= i - 2 < n_:
                s3(i - 2)

    def causal_steps(strict, descending):
        steps = []
        for qt in range(NT):
            q0 = qt * 512
            kbs = list(range(0, 4 * qt + 4))
            if descending:
                kbs = kbs[::-1]
            for ii, kb in enumerate(kbs):
                o = kb - 4 * qt
                steps.append(dict(q0=q0, n=512, kb=kb, mask=((maskS if strict else maskC)[o] if o >= 0 else None), first=(ii == 0), last=(ii == len(kbs) - 1)))
        return steps

    def dil_steps(r):
        sc_ = SEQ // r
        n = min(512, sc_)
        steps = []
        for q0 in range(0, SEQ, n):
            cs = (q0 // sc_) * sc_
            k_lo = max(cs, q0 - 128)
            kbs = list(range(k_lo // 128, (q0 + n) // 128))
            for ii, kb in enumerate(kbs):
                steps.append(dict(q0=q0, n=n, kb=kb, mask=maskD[n][kb - q0 // 128], first=(ii == 0), last=(ii == len(kbs) - 1)))
        return steps

    hsel = (lambda h: True) if heads is None else (lambda h: h in heads)
    jobs = []
    for kind, hbase, pbase, kd in (("sb", 0, 0, 64), ("fox", 4, 2, 70), ("mla", 8, 4, 96)):
        for hp in range(2):
            for hh in range(2):
                h = hbase + hp * 2 + hh
                if hsel(h):
                    jobs.append((kind, h, pbase + hp, hh, kd))
    step_cache = {"sb": causal_steps(True, True), "fox": causal_steps(False, False)}
    step_cache["mla"] = step_cache["fox"]
    loaded = {}
    vcur = {}

    def prefetch(job):
        kind, h, pr_, hh, kd = job
        if pr_ not in vcur:
            vcur.clear()
            vcur[pr_] = load_v(sc["VS"][pr_], sc["r_VS"][pr_])
        loaded[h] = load_qk(sc["QS"][h][0:kd, :], sc["r_QS"][h], sc["KS"][h][0:kd, :], sc["r_KS"][h], kd) + (vcur[pr_],)
    if jobs:
        prefetch(jobs[0])
    for ji, job in enumerate(jobs):
        kind, h, pr_, hh, kd = job
        q, k, v = loaded.pop(h)
        if ji + 1 < len(jobs):
            prefetch(jobs[ji + 1])

        def done(o_, sp_, h=h, kind=kind):
            finish(o_.t[0:65, 0:sp_["n"]], o_.r, h, sp_["q0"], sp_["n"], kind == "sb")
        run_steps(step_cache[kind], q, k, kd, v, hh, kind, done)
    for hp in range(2):
        if not (hsel(12 + 2 * hp) or hsel(13 + 2 * hp)):
            continue
        for g in range(3):
            r = DIL_R[g]
            steps = dil_steps(r)
            v = load_v(sc["VD"][g][hp], sc["r_VD"][g][hp])
            for hh in range(2):
                hd = hp * 2 + hh
                q, k = load_qk(sc["QD"][g][hd], sc["r_QD"][g][hd], sc["KD"][g][hd], sc["r_KD"][g][hd], 64)
                a_ = acc[hh]

                def done(o_, sp_, a_=a_, r=r, g=g):
                    n, q0 = sp_["n"], sp_["q0"]
                    dst = perm_view(a_.t[:, :], r, q0, n)
                    if g == 0:
                        S.op("act", lambda e: e.activation(out=dst, in_=o_.t[0:65, 0:n], func=AF.Copy), reads=[o_.r], writes=[a_.r])
                    else:
                        S.op("dve", lambda e: e.tensor_tensor(out=dst, in0=o_.t[0:65, 0:n], in1=dst, op=ALU.add), reads=[o_.r, a_.r], writes=[a_.r])
                run_steps(steps, q, k, 64, v, hh, "dil", done)
        for hh in range(2):
            for qt in range(NT):
                finish(acc[hh].t[:, qt * 512:(qt + 1) * 512], acc[hh].r, 12 + hp * 2 + hh, qt * 512, 512, False, in_sbuf=True)
    S.flush()
    A.close()


def layernorm_block(S, y, g_b, b_b, small, out):
    st6, mv, rs = small["st6"], small["mv"], small["rs"]
    for hf in range(2):
        S.op("dve", lambda e, hf=hf: e.bn_stats(out=st6.t[:, hf, :], in_=y.t[:, hf * 512:(hf + 1) * 512]), reads=[y.r], writes=[st6.r])
    S.op("dve", lambda e: e.bn_aggr(out=mv.t[:], in_=st6.t[:, :, :].rearrange("p a b -> p (a b)")), reads=[st6.r], writes=[mv.r])
    S.op("act", lambda e: e.activation(out=rs.t[:], in_=mv.t[:, 1:2], func=AF.Ln, bias=small["eps"].t[:, 0:1]), reads=[mv.r, small["eps"].r], writes=[rs.r])
    S.op("act", lambda e: e.activation(out=rs.t[:], in_=rs.t[:], func=AF.Exp, scale=-0.5), reads=[rs.r], writes=[rs.r])
    S.op("dve", lambda e: e.scalar_tensor_tensor(out=y.t[:], in0=y.t[:], scalar=mv.t[:, 0:1], in1=g_b.t[:], op0=ALU.subtract, op1=ALU.mult),
         reads=[y.r, mv.r, g_b.r], writes=[y.r])
    S.op("dve", lambda e: e.scalar_tensor_tensor(out=out.t[:], in0=y.t[:], scalar=rs.t[:, 0:1], in1=b_b.t[:], op0=ALU.mult, op1=ALU.add),
         reads=[y.r, rs.r, b_b.r], writes=[out.r])


def bcast_row(S, A, name, src1d, n):
    b = Buf(A.sb(name, [128, n], F32))
    S.dma("sp", b.t[:], src1d.rearrange("(o n) -> o n", o=1).partition_broadcast(128), writes=[b.r])
    return b


def phase3(nc, S, T, xin, r_xin, l, sc):
    A = Alloc(nc)
    identf = make_ident(A, S, F32)
    wout = Buf(A.sb("wout", [128, 8, DM], BF16))
    S.dma("pool", wout.t[:], T["w_out"][l].rearrange("(c p) n -> p c n", p=128), writes=[wout.r])
    wr = Buf(A.sb("wr", [128, 8, 20], F32))
    S.dma("sp", wr.t[:, :, 0:4], T["w_group"][l].rearrange("(c p) n -> p c n", p=128), writes=[wr.r])
    S.dma("sp", wr.t[:, :, 4:20], T["w_expert"][l].rearrange("(c p) n -> p c n", p=128), writes=[wr.r])
    brt = Buf(A.sb("brt", [128, 20], F32))
    S.dma("sp", brt.t[:, 0:4], T["b_group"][l].rearrange("(o n) -> o n", o=1).partition_broadcast(128), writes=[brt.r])
    S.dma("sp", brt.t[:, 4:20], T["b_expert"][l].rearrange("(o n) -> o n", o=1).partition_broadcast(128), writes=[brt.r])
    g_b = bcast_row(S, A, "ln1g", T["ln1_g"][l], DM)
    b_b = bcast_row(S, A, "ln1b", T["ln1_b"][l], DM)
    on = rot(A, "sb", "on", [128, 8, 512], BF16, 2)
    xs = rot(A, "sb", "xs3", [128, DM], F32, 3)
    y = rot(A, "sb", "y3", [128, DM], F32, 3)
    x1 = rot(A, "sb", "x1o", [128, DM], F32, 6)
    xtf = rot(A, "sb", "xtf", [128, 8, 128], F32, 2)
    xtb = rot(A, "sb", "xtb", [128, 8, 512], BF16, 2)
    gate = Buf(A.sb("gate", [128, NB, 16], F32))
    lgall = Buf(A.sb("lgall", [128, NB, 20], F32))
    if ROUTED:
        x1b = Buf(A.sb("x1b", [128, NB, DM], BF16))
        gohall = Buf(A.sb("gohall", [128, NB, 4], F32))
    ph = rot(A, "ps", "ph", [128, DM], F32, 2)
    ptr = rot(A, "ps", "ptr", [128, DM], F32, 1)
    plg = rot(A, "ps", "plg", [128, 512], F32, 2)
    epsl = Buf(A.sb("epsl", [128, 1], F32))
    S.op("pool", lambda e: e.memset(epsl.t[:], LN_EPS), writes=[epsl.r])
    small = [dict(st6=Buf(A.sb("st6", [128, 2, 6], F32)), mv=Buf(A.sb("mv", [128, 2], F32)), rs=Buf(A.sb("rs", [128, 1], F32)), eps=epsl) for _ in range(3)]
    pend = []
    for j in range(NT):
        o_ = on[j % 2]
        S.dma("sp", o_.t[:], sc["OnT"][:, :, j * 512:(j + 1) * 512].rearrange("c p t -> p c t"), reads=[sc["r_OnT"]], writes=[o_.r])
        xb_ = xtb[j % 2]
        for bb in range(4):
            b = j * 4 + bb
            s_, y_, x1_, xf_, p_, sm = xs[b % 3], y[b % 3], x1[b % 6], xtf[b % 2], ph[b % 2], small[b % 3]
            S.dma("sp", s_.t[:], xin[b * 128:(b + 1) * 128, :], reads=[r_xin], writes=[s_.r])
            for hf in range(2):
                for c in range(8):
                    S.op("pe", lambda e, hf=hf, c=c, bb=bb, o_=o_, p_=p_: e.matmul(p_.t[:, hf * 512:(hf + 1) * 512], lhsT=o_.t[:, c, bb * 128:(bb + 1) * 128],
                                                                            rhs=wout.t[:, c, hf * 512:(hf + 1) * 512], start=(c == 0), stop=(c == 7)),
                         reads=[o_.r, wout.r], writes=[p_.r])
            S.op("dve", lambda e, s_=s_, y_=y_, p_=p_: e.scalar_tensor_tensor(out=y_.t[:], in0=s_.t[:], scalar=ALPHA, in1=p_.t[:], op0=ALU.mult, op1=ALU.add),
                 reads=[s_.r, p_.r], writes=[y_.r])
            layernorm_block(S, y_, g_b, b_b, sm, x1_)
            S.dma("sp", sc["X1"][b * 128:(b + 1) * 128, :], x1_.t[:], reads=[x1_.r], writes=[sc["r_X1"]])
            if ROUTED:
                S.op("act", lambda e, b=b, x1_=x1_: e.activation(out=x1b.t[:, b, :], in_=x1_.t[:], func=AF.Copy), reads=[x1_.r], writes=[x1b.r])
            def stage_b(b=b, bb=bb, x1_=x1_, xf_=xf_, xb_=xb_):
                pt = ptr[0]
                for c in range(8):
                    S.op("pe", lambda e, c=c: e.transpose(out=pt.t[:, c * 128:(c + 1) * 128], in_=x1_.t[:, c * 128:(c + 1) * 128], identity=identf.t[:]),
                         reads=[x1_.r, identf.r], writes=[pt.r])
                S.op("act", lambda e: e.activation(out=xf_.t[:, :, :], in_=pt.t[:, :].rearrange("p (c t) -> p c t", c=8), func=AF.Copy),
                     reads=[pt.r], writes=[xf_.r])
                if not ROUTED:
                    S.op("dve", lambda e: e.tensor_copy(out=xb_.t[:, :, bb * 128:(bb + 1) * 128], in_=pt.t[:, :].rearrange("p (c t) -> p c t", c=8)),
                         reads=[pt.r], writes=[xb_.r])
                pl = plg[b % 2]
                for c in range(8):
                    S.op("pe", lambda e, c=c: e.matmul(pl.t[:, 0:20], lhsT=xf_.t[:, c, :], rhs=wr.t[:, c, :], start=(c == 0), stop=(c == 7)),
                         reads=[xf_.r, wr.r], writes=[pl.r])
                S.op("dve", lambda e: e.tensor_tensor(out=lgall.t[:, b, :], in0=pl.t[:, 0:20], in1=brt.t[:], op=ALU.add), reads=[pl.r, brt.r], writes=[lgall.r])
            pend.append(stage_b)
            if len(pend) > 3:
                pend.pop(0)()
            if (not ROUTED) and bb == 3:
                while pend:
                    pend.pop(0)()
        if not ROUTED:
            S.dma("sp", sc["X1T"][:, :, j * 512:(j + 1) * 512], xb_.t[:], reads=[xb_.r], writes=[sc["r_X1T"]])
    while pend:
        pend.pop(0)()
    def GT(name, shape):
        return Buf(A.sb("gv_" + name, shape, F32))
    B3 = [128, NB, 4]
    gl = lgall.t[:, :, 0:4]
    el = lgall.t[:, :, 4:20].rearrange("p b (g x) -> p b g x", g=4)
    m_, goh, tmp, se = GT("m", [128, NB]), (gohall if ROUTED else GT("goh", B3)), GT("tmp", B3), GT("se", [128, NB])
    t44, es, m1, oh1, es2, m2, oh2 = GT("t44", [128, NB, 4, 4]), GT("es", B3), GT("m1", [128, NB]), GT("oh1", B3), GT("es2", B3), GT("m2", [128, NB]), GT("oh2", B3)
    d_, p1, p2, gi = GT("d", [128, NB]), GT("p1", [128, NB]), GT("p2", [128, NB]), GT("gi", B3)

    def bc(t2):
        return t2.t[:, :].unsqueeze(2).to_broadcast(B3)

    def D(fn, reads, writes):
        S.op("dve", fn, reads=[x.r for x in reads], writes=[x.r for x in writes])
    D(lambda e: e.tensor_reduce(out=m_.t[:], in_=gl, axis=AX.X, op=ALU.max), [lgall], [m_])
    D(lambda e: e.tensor_tensor(out=goh.t[:], in0=gl, in1=bc(m_), op=ALU.is_equal), [lgall, m_], [goh])
    D(lambda e: e.tensor_tensor(out=tmp.t[:], in0=gl, in1=bc(m_), op=ALU.subtract), [lgall, m_], [tmp])
    S.op("act", lambda e: e.activation(out=tmp.t[:], in_=tmp.t[:], func=AF.Exp), reads=[tmp.r], writes=[tmp.r])
    D(lambda e: e.tensor_reduce(out=se.t[:], in_=tmp.t[:], axis=AX.X, op=ALU.add), [tmp], [se])
    D(lambda e: e.reciprocal(out=se.t[:], in_=se.t[:]), [se], [se])
    D(lambda e: e.tensor_tensor(out=t44.t[:], in0=el, in1=goh.t[:, :, :].unsqueeze(3).to_broadcast([128, NB, 4, 4]), op=ALU.mult), [lgall, goh], [t44])
    D(lambda e: e.tensor_reduce(out=es.t[:], in_=t44.t[:, :, :, :].rearrange("p b g x -> p b x g"), axis=AX.X, op=ALU.add), [t44], [es])
    D(lambda e: e.tensor_reduce(out=m1.t[:], in_=es.t[:], axis=AX.X, op=ALU.max), [es], [m1])
    D(lambda e: e.tensor_tensor(out=oh1.t[:], in0=es.t[:], in1=bc(m1), op=ALU.is_equal), [es, m1], [oh1])
    D(lambda e: e.scalar_tensor_tensor(out=es2.t[:], in0=oh1.t[:], scalar=-1e30, in1=es.t[:], op0=ALU.mult, op1=ALU.add), [oh1, es], [es2])
    D(lambda e: e.tensor_reduce(out=m2.t[:], in_=es2.t[:], axis=AX.X, op=ALU.max), [es2], [m2])
    D(lambda e: e.tensor_tensor(out=oh2.t[:], in0=es2.t[:], in1=bc(m2), op=ALU.is_equal), [es2, m2], [oh2])
    D(lambda e: e.tensor_tensor(out=d_.t[:], in0=m2.t[:], in1=m1.t[:], op=ALU.subtract), [m1, m2], [d_])
    S.op("act", lambda e: e.activation(out=d_.t[:], in_=d_.t[:], func=AF.Exp), reads=[d_.r], writes=[d_.r])
    D(lambda e: e.tensor_scalar(out=p1.t[:], in0=d_.t[:], scalar1=1.0, scalar2=None, op0=ALU.add), [d_], [p1])
    D(lambda e: e.reciprocal(out=p1.t[:], in_=p1.t[:]), [p1], [p1])
    D(lambda e: e.tensor_tensor(out=p2.t[:], in0=d_.t[:], in1=p1.t[:], op=ALU.mult), [d_, p1], [p2])
    D(lambda e: e.tensor_tensor(out=gi.t[:], in0=oh1.t[:], in1=bc(p1), op=ALU.mult), [oh1, p1], [gi])
    D(lambda e: e.tensor_tensor(out=oh2.t[:], in0=oh2.t[:], in1=bc(p2), op=ALU.mult), [oh2, p2], [oh2])
    D(lambda e: e.tensor_tensor(out=gi.t[:], in0=gi.t[:], in1=oh2.t[:], op=ALU.add), [gi, oh2], [gi])
    D(lambda e: e.tensor_tensor(out=gi.t[:], in0=gi.t[:], in1=bc(se), op=ALU.mult), [gi, se], [gi])
    D(lambda e: e.tensor_tensor(out=gate.t[:, :, :].rearrange("p b (g x) -> p b g x", g=4), in0=goh.t[:, :, :].unsqueeze(3).to_broadcast([128, NB, 4, 4]),
                                in1=gi.t[:, :, :].unsqueeze(2).to_broadcast([128, NB, 4, 4]), op=ALU.mult), [goh, gi], [gate])
    if not ROUTED:
        S.dma("sp", sc["GATE"], gate.t[:, :, :].rearrange("p b e -> p (b e)"), reads=[gate.r], writes=[sc["r_GATE"]])
    else:
        route_epilogue(S, A, sc, x1b, gate, gohall, plg, l)
    S.flush()
    A.close()


def route_epilogue(S, A, sc, x1b, gate, gohall, plg, l):
    def T_(name, shape, dt=F32):
        return Buf(A.sb(name, shape, dt))
    onesf = T_("onesf", [128, 128])
    tris = T_("tris", [128, 128])
    S.op("pool", lambda e: e.memset(onesf.t[:], 1.0), writes=[onesf.r])
    S.op("pool", lambda e: e.memset(tris.t[:], 1.0), writes=[tris.r])
    S.op("pool", lambda e: e.affine_select(out=tris.t[:], in_=tris.t[:], pattern=[[1, 128]], compare_op=ALU.is_ge, fill=0.0, base=-1,
                                           channel_multiplier=-1), reads=[tris.r], writes=[tris.r])
    pt, pr = plg[0], plg[1]
    for b in range(NB):
        S.op("pe", lambda e, b=b: e.matmul(pt.t[:, b * 4:(b + 1) * 4], lhsT=onesf.t[:], rhs=gohall.t[:, b, :], start=True, stop=True),
             reads=[onesf.r, gohall.r], writes=[pt.r])
        S.op("pe", lambda e, b=b: e.matmul(pr.t[:, b * 4:(b + 1) * 4], lhsT=tris.t[:], rhs=gohall.t[:, b, :], start=True, stop=True),
             reads=[tris.r, gohall.r], writes=[pr.r])
    totb = T_("totb", [128, NB, 4])
    cum = T_("cumb", [128, NB, 4])
    ones32 = T_("ones32", [128, NB])
    S.op("pool", lambda e: e.memset(ones32.t[:], 1.0), writes=[ones32.r])
    S.op("dve", lambda e: e.tensor_copy(out=totb.t[:, :, :], in_=pt.t[:, 0:NB * 4].rearrange("p (b g) -> p b g", g=4)), reads=[pt.r], writes=[totb.r])
    for g in range(4):
        S.op("dve", lambda e, g=g: e.tensor_tensor_scan(out=cum.t[:, :, g], data0=ones32.t[:, :], data1=totb.t[:, :, g], initial=0.0,
                                                        op0=ALU.mult, op1=ALU.add), reads=[ones32.r, totb.r, cum.r], writes=[cum.r])
    boffx = T_("boffx", [128, NB, 4])
    S.op("dve", lambda e: e.tensor_tensor(out=boffx.t[:], in0=cum.t[:], in1=totb.t[:], op=ALU.subtract), reads=[cum.r, totb.r], writes=[boffx.r])
    thr_i = T_("thri", [128, 16], I32)
    thr = T_("thr", [128, 16])
    S.op("pool", lambda e: e.iota(thr_i.t[:], pattern=[[512, 16]], base=0, channel_multiplier=0), writes=[thr_i.r])
    S.op("dve", lambda e: e.tensor_copy(out=thr.t[:], in_=thr_i.t[:]), reads=[thr_i.r], writes=[thr.r])
    cmp = T_("cmp", [128, 4, 8])
    ntl = T_("ntl", [128, 4])
    S.op("dve", lambda e: e.tensor_tensor(out=cmp.t[:], in0=cum.t[:, NB - 1, :].unsqueeze(2).to_broadcast([128, 4, 8]),
                                          in1=thr.t[:, 0:8].unsqueeze(1).to_broadcast([128, 4, 8]), op=ALU.is_gt), reads=[cum.r, thr.r], writes=[cmp.r])
    S.op("dve", lambda e: e.tensor_reduce(out=ntl.t[:], in_=cmp.t[:], axis=AX.X, op=ALU.add), reads=[cmp.r], writes=[ntl.r])
    S.op("dve", lambda e: e.tensor_scalar(out=ntl.t[:], in0=ntl.t[:], scalar1=512.0, scalar2=None, op0=ALU.mult), reads=[ntl.r], writes=[ntl.r])
    pst = T_("pst", [128, 4])
    pen = T_("pen", [128, 4])
    S.op("pool", lambda e: e.memset(pst.t[:], 0.0), writes=[pst.r])
    for g in range(1, 4):
        S.op("dve", lambda e, g=g: e.tensor_tensor(out=pst.t[:, g:g + 1], in0=pst.t[:, g - 1:g], in1=ntl.t[:, g - 1:g], op=ALU.add),
             reads=[pst.r, ntl.r], writes=[pst.r])
    S.op("dve", lambda e: e.tensor_tensor(out=pen.t[:], in0=pst.t[:], in1=ntl.t[:], op=ALU.add), reads=[pst.r, ntl.r], writes=[pen.r])
    v = T_("vdest", [128, NB, 4])
    S.op("dve", lambda e: e.tensor_tensor(out=v.t[:], in0=pr.t[:, 0:NB * 4].rearrange("p (b g) -> p b g", g=4), in1=boffx.t[:], op=ALU.add),
         reads=[pr.r, boffx.r], writes=[v.r])
    S.op("dve", lambda e: e.tensor_tensor(out=v.t[:], in0=v.t[:], in1=pst.t[:, :].unsqueeze(1).to_broadcast([128, NB, 4]), op=ALU.add),
         reads=[v.r, pst.r], writes=[v.r])
    S.op("dve", lambda e: e.tensor_tensor(out=v.t[:], in0=v.t[:], in1=gohall.t[:], op=ALU.mult), reads=[v.r, gohall.r], writes=[v.r])
    destf = T_("destf", [128, NB])
    desti = T_("desti", [128, NB], I32)
    S.op("dve", lambda e: e.tensor_reduce(out=destf.t[:], in_=v.t[:], axis=AX.X, op=ALU.add), reads=[v.r], writes=[destf.r])
    S.op("dve", lambda e: e.tensor_copy(out=desti.t[:], in_=destf.t[:]), reads=[destf.r], writes=[desti.r])
    S.dma("sp", sc["DEST"], desti.t[:], reads=[desti.r], writes=[sc["r_DEST"]])
    cmp2 = T_("cmp2", [128, NTILE, 4])
    gk = T_("gk", [128, NTILE])
    S.op("dve", lambda e: e.tensor_tensor(out=cmp2.t[:], in0=pen.t[:, :].unsqueeze(1).to_broadcast([128, NTILE, 4]),
                                          in1=thr.t[:, 0:NTILE].unsqueeze(2).to_broadcast([128, NTILE, 4]), op=ALU.is_le), reads=[pen.r, thr.r], writes=[cmp2.r])
    S.op("dve", lambda e: e.tensor_reduce(out=gk.t[:], in_=cmp2.t[:], axis=AX.X, op=ALU.add), reads=[cmp2.r], writes=[gk.r])
    S.op("dve", lambda e: e.tensor_scalar(out=gk.t[:], in0=gk.t[:], scalar1=3.0, scalar2=1024.0, op0=ALU.min, op1=ALU.mult), reads=[gk.r], writes=[gk.r])
    S.op("dve", lambda e: e.tensor_scalar(out=gk.t[:], in0=gk.t[:], scalar1=float(l * 4096), scalar2=None, op0=ALU.add), reads=[gk.r], writes=[gk.r])
    cw_i = T_("cwi", [128, 8], I32)
    cw = T_("cw", [128, 8])
    S.op("pool", lambda e: e.iota(cw_i.t[:], pattern=[[256, 4], [1, 2]], base=0, channel_multiplier=2), writes=[cw_i.r])
    S.op("dve", lambda e: e.tensor_copy(out=cw.t[:], in_=cw_i.t[:]), reads=[cw_i.r], writes=[cw.r])
    idxf = T_("idxf", [128, NTILE, 8])
    idxi = T_("idxi", [128, NTILE, 8], I32)
    S.op("dve", lambda e: e.tensor_tensor(out=idxf.t[:], in0=cw.t[:, :].unsqueeze(1).to_broadcast([128, NTILE, 8]),
                                          in1=gk.t[:, :].unsqueeze(2).to_broadcast([128, NTILE, 8]), op=ALU.add), reads=[cw.r, gk.r], writes=[idxf.r])
    S.op("dve", lambda e: e.tensor_copy(out=idxi.t[:], in_=idxf.t[:]), reads=[idxf.r], writes=[idxi.r])
    S.dma("sp", sc["IDXW"], idxi.t[:, :, :].rearrange("p k j -> p (k j)"), reads=[idxi.r], writes=[sc["r_IDXW"]])
    zt = Buf(A.sb("zt", [128, 4 * DM], BF16))
    zg = T_("zg", [128, NTILE * 4 * 16])
    S.op("pool", lambda e: e.memset(zt.t[:], 0.0), writes=[zt.r])
    S.op("pool", lambda e: e.memset(zg.t[:], 0.0), writes=[zg.r])
    for k in range(NTILE):
        S.dma("sp", sc["XS"][k * 512:(k + 1) * 512, :].rearrange("(p r) n -> p (r n)", p=128), zt.t[:], reads=[zt.r], writes=[sc["r_XS"]])
    S.dma("sp", sc["GS"].rearrange("(p r) n -> p (r n)", p=128), zg.t[:], reads=[zg.r], writes=[sc["r_GS"]])
    for b in range(NB):
        S.dma_fn("pool", lambda e, b=b: e.indirect_dma_start(out=sc["XS"][:, :], out_offset=bass.IndirectOffsetOnAxis(ap=desti.t[:, b:b + 1], axis=0),
                                                            in_=x1b.t[:, b, :], in_offset=None), reads=[desti.r, x1b.r], writes=[sc["r_XS"]])
        S.dma_fn("pool", lambda e, b=b: e.indirect_dma_start(out=sc["GS"][:, :], out_offset=bass.IndirectOffsetOnAxis(ap=desti.t[:, b:b + 1], axis=0),
                                                            in_=gate.t[:, b, :], in_offset=None), reads=[desti.r, gate.r], writes=[sc["r_GS"]])


def phase4(nc, S, T, l, sc, xout, r_xout, final, TG=1024):
    A = Alloc(nc)
    g_b = bcast_row(S, A, "ln2g", T["ln2_g"][l], DM)
    b_b = bcast_row(S, A, "ln2b", T["ln2_b"][l], DM)
    gate = Buf(A.sb("gate4", [128, NB, 16], F32))
    S.dma("sp", gate.t[:, :, :].rearrange("p b e -> p (b e)"), sc["GATE"], reads=[sc["r_GATE"]], writes=[gate.r])
    xT = Buf(A.sb("x1T", [128, 8, TG], BF16))
    accb = Buf(A.sb("accm", [128, TG // 128, DM], F32))
    w1 = rot(A, "sb", "w1", [128, 8, DEXP], BF16, 2)
    w3 = rot(A, "sb", "w3", [128, 8, DEXP], BF16, 2)
    w2 = rot(A, "sb", "w2", [128, 4, DM], BF16, 2)
    sa = rot(A, "sb", "sa", [128, 512], F32, 2)
    hT = rot(A, "sb", "hT", [128, 4, 512], BF16, 2)
    xs = rot(A, "sb", "xs4", [128, DM], F32, 2)
    yo = rot(A, "sb", "yo4", [128, DM], F32, 2)
    epsl = Buf(A.sb("epsl4", [128, 1], F32))
    S.op("pool", lambda e: e.memset(epsl.t[:], LN_EPS), writes=[epsl.r])
    small = [dict(st6=Buf(A.sb("st6b", [128, 2, 6], F32)), mv=Buf(A.sb("mvb", [128, 2], F32)), rs=Buf(A.sb("rsb", [128, 1], F32)), eps=epsl) for _ in range(2)]
    pa = rot(A, "ps", "pa", [128, 512], F32, 2)
    pb = rot(A, "ps", "pb", [128, 512], F32, 2)
    py = rot(A, "ps", "py", [128, 512], F32, 3)
    cnt = {"y": 0, "ab": 0, "w": 0}
    W1 = T["w1"][l]
    W3 = T["w3"][l]
    W2 = T["w2"][l]
    for gi in range(SEQ // TG):
        t0 = gi * TG
        S.dma("sp", xT.t[:], sc["X1T"][:, :, t0:t0 + TG], reads=[sc["r_X1T"]], writes=[xT.r])
        for ex in range(NEXP):
            i = cnt["w"]
            cnt["w"] += 1
            a1, a3, a2 = w1[i % 2], w3[i % 2], w2[i % 2]
            S.dma("pool", a1.t[:], W1[ex].rearrange("(c p) n -> p c n", p=128), writes=[a1.r])
            S.dma("pool", a3.t[:], W3[ex].rearrange("(c p) n -> p c n", p=128), writes=[a3.r])
            S.dma("pool", a2.t[:], W2[ex].rearrange("(c p) n -> p c n", p=128), writes=[a2.r])
            for tt in range(TG // 512):
                tc = slice(tt * 512, (tt + 1) * 512)
                h_ = hT[(ex * (TG // 512) + tt) % 2]
                for jc in range(4):
                    k_ = cnt["ab"]
                    cnt["ab"] += 1
                    pa_, pb_, sa_ = pa[k_ % 2], pb[k_ % 2], sa[k_ % 2]
                    for c in range(8):
                        S.op("pe", lambda e, c=c, jc=jc, pa_=pa_, a1=a1, tc=tc: e.matmul(pa_.t[:], lhsT=a1.t[:, c, jc * 128:(jc + 1) * 128], rhs=xT.t[:, c, tc],
                                                                                start=(c == 0), stop=(c == 7)), reads=[a1.r, xT.r], writes=[pa_.r])
                    for c in range(8):
                        S.op("pe", lambda e, c=c, jc=jc, pb_=pb_, a3=a3, tc=tc: e.matmul(pb_.t[:], lhsT=a3.t[:, c, jc * 128:(jc + 1) * 128], rhs=xT.t[:, c, tc],
                                                                                start=(c == 0), stop=(c == 7)), reads=[a3.r, xT.r], writes=[pb_.r])
                    S.op("act", lambda e, pa_=pa_, sa_=sa_: e.activation(out=sa_.t[:], in_=pa_.t[:], func=AF.Silu), reads=[pa_.r], writes=[sa_.r])
                    S.op("dve", lambda e, pb_=pb_, sa_=sa_, h_=h_, jc=jc: e.tensor_tensor(out=h_.t[:, jc, :], in0=pb_.t[:], in1=sa_.t[:], op=ALU.mult),
                         reads=[pb_.r, sa_.r], writes=[h_.r])
                for tb in range(4):
                    blk = tt * 4 + tb
                    gb = (t0 // 128) + blk
                    for hf in range(2):
                        y_ = py[cnt["y"] % 3]
                        cnt["y"] += 1
                        for jc in range(4):
                            S.op("pe", lambda e, jc=jc, tb=tb, hf=hf, y_=y_, h_=h_, a2=a2: e.matmul(y_.t[:], lhsT=h_.t[:, jc, tb * 128:(tb + 1) * 128],
                                                                                           rhs=a2.t[:, jc, hf * 512:(hf + 1) * 512], start=(jc == 0), stop=(jc == 3)),
                                 reads=[h_.r, a2.r], writes=[y_.r])
                        dst = accb.t[:, blk, hf * 512:(hf + 1) * 512]
                        if ex == 0:
                            S.op("dve", lambda e, y_=y_, dst=dst, gb=gb, ex=ex: e.tensor_scalar(out=dst, in0=y_.t[:], scalar1=gate.t[:, gb, ex:ex + 1], scalar2=None, op0=ALU.mult),
                                 reads=[y_.r, gate.r], writes=[accb.r])
                        else:
                            S.op("dve", lambda e, y_=y_, dst=dst, gb=gb, ex=ex: e.scalar_tensor_tensor(out=dst, in0=y_.t[:], scalar=gate.t[:, gb, ex:ex + 1], in1=dst,
                                                                                              op0=ALU.mult, op1=ALU.add), reads=[y_.r, gate.r, accb.r], writes=[accb.r])
        for blk in range(TG // 128):
            gb = (t0 // 128) + blk
            s_, y_, sm = xs[blk % 2], yo[blk % 2], small[blk % 2]
            S.dma("sp", s_.t[:], sc["X1"][gb * 128:(gb + 1) * 128, :], reads=[sc["r_X1"]], writes=[s_.r])
            S.op("dve", lambda e, s_=s_, blk=blk: e.scalar_tensor_tensor(out=s_.t[:], in0=s_.t[:], scalar=ALPHA, in1=accb.t[:, blk, :], op0=ALU.mult, op1=ALU.add),
                 reads=[s_.r, accb.r], writes=[s_.r])
            layernorm_block(S, s_, g_b, b_b, sm, y_)
            S.dma("sp", xout[gb * 128:(gb + 1) * 128, :], y_.t[:], reads=[y_.r], writes=[r_xout], final=final)
    S.flush()
    A.close()


def phase4r(nc, S, T, l, sc, xout, r_xout, final):
    A = Alloc(nc)
    ident = make_ident(A, S, BF16)
    g_b = bcast_row(S, A, "ln2g", T["ln2_g"][l], DM)
    b_b = bcast_row(S, A, "ln2b", T["ln2_b"][l], DM)
    dest = Buf(A.sb("dest4", [128, NB], I32))
    idxw = Buf(A.sb("idxw4", [128, NTILE * 8], I32))
    S.dma("sp", dest.t[:], sc["DEST"], reads=[sc["r_DEST"]], writes=[dest.r])
    S.dma("sp", idxw.t[:], sc["IDXW"], reads=[sc["r_IDXW"]], writes=[idxw.r])
    xs = rot(A, "sb", "xs4r", [128, 4, DM], BF16, 2)
    gs = rot(A, "sb", "gs4r", [128, 4, 16], F32, 2)
    gsel = rot(A, "sb", "gsel", [128, 4, 4], F32, 2)
    xT = rot(A, "sb", "xT4r", [128, 8, 512], BF16, 2)
    accs = rot(A, "sb", "acc4r", [128, 4, DM], F32, 2)
    w1 = rot(A, "sb", "w1r", [128, 8 * DEXP], BF16, 2)
    w3 = rot(A, "sb", "w3r", [128, 8 * DEXP], BF16, 2)
    w2 = rot(A, "sb", "w2r", [128, 4 * DM], BF16, 2)
    sa = rot(A, "sb", "sar", [128, 512], F32, 2)
    hT = rot(A, "sb", "hTr", [128, 4, 512], BF16, 2)
    mt = rot(A, "sb", "mt4", [128, DM], F32, 4)
    xo = rot(A, "sb", "xo4", [128, DM], F32, 4)
    yo = rot(A, "sb", "yo4r", [128, DM], F32, 4)
    epsl = Buf(A.sb("epsl4r", [128, 1], F32))
    S.op("pool", lambda e: e.memset(epsl.t[:], LN_EPS), writes=[epsl.r])
    small = [dict(st6=Buf(A.sb("st6r", [128, 2, 6], F32)), mv=Buf(A.sb("mvr", [128, 2], F32)), rs=Buf(A.sb("rsr", [128, 1], F32)), eps=epsl) for _ in range(4)]
    ptp = rot(A, "ps", "ptp", [128, DM], BF16, 1)
    pa = rot(A, "ps", "par", [128, 512], F32, 2)
    pb = rot(A, "ps", "pbr", [128, 512], F32, 2)
    py = rot(A, "ps", "pyr", [128, 512], F32, 3)
    Wv = [T[nm].rearrange("l e k n -> (l e k n)").rearrange("(r x) -> r x", x=2048) for nm in ("w1", "w3", "w2")]
    cnt = {"y": 0, "ab": 0}
    steps = [(k, j) for k in range(NTILE) for j in range(4)]
    st = {}

    def front(si):
        k, j = steps[si]
        if j == 0:
            x_, g_, gl_, xT_, ac_ = xs[k % 2], gs[k % 2], gsel[k % 2], xT[k % 2], accs[k % 2]
            S.dma("sp", x_.t[:], sc["XS"][k * 512:(k + 1) * 512, :].rearrange("(b p) n -> p b n", p=128), reads=[sc["r_XS"]], writes=[x_.r])
            S.dma("sp", g_.t[:], sc["GS"][k * 512:(k + 1) * 512, :].rearrange("(b p) n -> p b n", p=128), reads=[sc["r_GS"]], writes=[g_.r])
            S.op("dve", lambda e: e.tensor_reduce(out=gl_.t[:], in_=g_.t[:, :, :].rearrange("p b (g j) -> p b j g", g=4), axis=AX.X, op=ALU.add),
                 reads=[g_.r], writes=[gl_.r])
            for blk in range(4):
                p = ptp[0]
                for c in range(8):
                    S.op("pe", lambda e, c=c, blk=blk: e.transpose(out=p.t[:, c * 128:(c + 1) * 128],
                                                                   in_=x_.t[:, blk, :].rearrange("p (pp c) -> p c pp", c=8)[:, c, :], identity=ident.t[:]),
                         reads=[x_.r, ident.r], writes=[p.r])
                S.op("act", lambda e, blk=blk: e.activation(out=xT_.t[:, :, blk * 128:(blk + 1) * 128], in_=p.t[:, :].rearrange("p (c t) -> p c t", c=8), func=AF.Copy),
                     reads=[p.r], writes=[xT_.r])
        xT_, ac_, gl_ = xT[k % 2], accs[k % 2], gsel[k % 2]
        a1, a3, a2, h_ = w1[si % 2], w3[si % 2], w2[si % 2], hT[si % 2]
        for wt_, src in ((a1, Wv[0]), (a3, Wv[1]), (a2, Wv[2])):
            for half in range(2):
                col = k * 8 + j * 2 + half
                S.dma_fn("pool", lambda e, wt_=wt_, src=src, half=half, col=col: e.indirect_dma_start(
                    out=wt_.t[:, half * 2048:(half + 1) * 2048], out_offset=None, in_=src,
                    in_offset=bass.IndirectOffsetOnAxis(ap=idxw.t[:, col:col + 1], axis=0)), reads=[idxw.r], writes=[wt_.r])
        w1v = a1.t[:, :].rearrange("p (c pp q) -> p c q pp", c=8, q=4)
        w3v = a3.t[:, :].rearrange("p (c pp q) -> p c q pp", c=8, q=4)
        for jc in range(4):
            k_ = cnt["ab"]
            cnt["ab"] += 1
            pa_, pb_, sa_ = pa[k_ % 2], pb[k_ % 2], sa[k_ % 2]
            for c in range(8):
                S.op("pe", lambda e, c=c, jc=jc, pa_=pa_: e.matmul(pa_.t[:], lhsT=w1v[:, c, jc, :], rhs=xT_.t[:, c, :], start=(c == 0), stop=(c == 7)),
                     reads=[a1.r, xT_.r], writes=[pa_.r])
            for c in range(8):
                S.op("pe", lambda e, c=c, jc=jc, pb_=pb_: e.matmul(pb_.t[:], lhsT=w3v[:, c, jc, :], rhs=xT_.t[:, c, :], start=(c == 0), stop=(c == 7)),
                     reads=[a3.r, xT_.r], writes=[pb_.r])
            S.op("act", lambda e, pa_=pa_, sa_=sa_: e.activation(out=sa_.t[:], in_=pa_.t[:], func=AF.Silu), reads=[pa_.r], writes=[sa_.r])
            S.op("dve", lambda e, pb_=pb_, sa_=sa_, jc=jc: e.tensor_tensor(out=h_.t[:, jc, :], in0=pb_.t[:], in1=sa_.t[:], op=ALU.mult),
                 reads=[pb_.r, sa_.r], writes=[h_.r])

    def back(si):
        k, j = steps[si]
        ac_, gl_, a2, h_ = accs[k % 2], gsel[k % 2], w2[si % 2], hT[si % 2]
        w2v = a2.t[:, :].rearrange("p (c n) -> p c n", c=4)
        for blk in range(4):
            for hf in range(2):
                y_ = py[cnt["y"] % 3]
                cnt["y"] += 1
                for jc in range(4):
                    S.op("pe", lambda e, jc=jc, blk=blk, hf=hf, y_=y_: e.matmul(y_.t[:], lhsT=h_.t[:, jc, blk * 128:(blk + 1) * 128],
                                                                               rhs=w2v[:, jc, hf * 512:(hf + 1) * 512], start=(jc == 0), stop=(jc == 3)),
                         reads=[h_.r, a2.r], writes=[y_.r])
                dst = ac_.t[:, blk, hf * 512:(hf + 1) * 512]
                if j == 0:
                    S.op("dve", lambda e, y_=y_, dst=dst, blk=blk: e.tensor_scalar(out=dst, in0=y_.t[:], scalar1=gl_.t[:, blk, j:j + 1], scalar2=None, op0=ALU.mult),
                         reads=[y_.r, gl_.r], writes=[ac_.r])
                else:
                    S.op("dve", lambda e, y_=y_, dst=dst, blk=blk: e.scalar_tensor_tensor(out=dst, in0=y_.t[:], scalar=gl_.t[:, blk, j:j + 1], in1=dst,
                                                                                         op0=ALU.mult, op1=ALU.add), reads=[y_.r, gl_.r, ac_.r], writes=[ac_.r])
        if j == 3:
            S.dma("sp", sc["YS"][k * 512:(k + 1) * 512, :].rearrange("(b p) n -> p b n", p=128), ac_.t[:], reads=[ac_.r], writes=[sc["r_YS"]])

    for si in range(len(steps) + 1):
        if si < len(steps):
            front(si)
        if si >= 1:
            back(si - 1)
    def comb_load(b):
        m_, x_ = mt[b % 4], xo[b % 4]
        S.dma_fn("pool", lambda e: e.indirect_dma_start(out=m_.t[:], out_offset=None, in_=sc["YS"][:, :],
                                                        in_offset=bass.IndirectOffsetOnAxis(ap=dest.t[:, b:b + 1], axis=0)),
                 reads=[dest.r, sc["r_YS"]], writes=[m_.r])
        S.dma("sp", x_.t[:], sc["X1"][b * 128:(b + 1) * 128, :], reads=[sc["r_X1"]], writes=[x_.r])
    for b in range(min(3, NB)):
        comb_load(b)
    for b in range(NB):
        m_, x_, y_, sm = mt[b % 4], xo[b % 4], yo[b % 4], small[b % 4]
        S.op("dve", lambda e, m_=m_, x_=x_: e.scalar_tensor_tensor(out=x_.t[:], in0=x_.t[:], scalar=ALPHA, in1=m_.t[:], op0=ALU.mult, op1=ALU.add),
             reads=[x_.r, m_.r], writes=[x_.r])
        layernorm_block(S, x_, g_b, b_b, sm, y_)
        if b + 3 < NB:
            comb_load(b + 3)
        S.dma("sp", xout[b * 128:(b + 1) * 128, :], y_.t[:], reads=[y_.r], writes=[r_xout], final=final)
    S.flush()
    A.close()


def rope_tables():
    pos = np.arange(SEQ, dtype=np.float32)
    out = {}
    for dim, nm in ((64, "rope64"), (32, "rope32")):
        half = dim // 2
        inv = (10000.0 ** (-np.arange(half, dtype=np.float32) / half)).astype(np.float32)
        ang = pos[None, :] * inv[:, None]
        c = np.cos(ang).astype(np.float32)
        s = np.sin(ang).astype(np.float32)
        out[nm + "c"] = np.ascontiguousarray(np.concatenate([c, c], 0))
        out[nm + "s"] = np.ascontiguousarray(np.concatenate([-s, s], 0))
    return out


W_SPECS = [("w_in", [DEPTH, DM, N_IN]), ("b_forget", [DEPTH, 4]), ("g_cq", [DEPTH, 256]), ("g_ckv", [DEPTH, 128]), ("w_uq", [DEPTH, 256, 384]),
           ("w_ukv", [DEPTH, 128, 512]), ("g_head", [DEPTH, 16, 64]), ("w_out", [DEPTH, DM, DM]), ("ln1_g", [DEPTH, DM]), ("ln1_b", [DEPTH, DM]),
           ("w_group", [DEPTH, DM, 4]), ("b_group", [DEPTH, 4]), ("w_expert", [DEPTH, DM, 16]), ("b_expert", [DEPTH, 16]),
           ("w1", [DEPTH, NEXP, DM, DEXP]), ("w3", [DEPTH, NEXP, DM, DEXP]), ("w2", [DEPTH, NEXP, DEXP, DM]), ("ln2_g", [DEPTH, DM]), ("ln2_b", [DEPTH, DM])]


def build_program(nseq=2, layers=(0, 1), phases=(1, 2, 3, 4), debug=False, heads=None, TG=1024):
    nc = bass.Bass("TRN2", target_bir_lowering=False)
    T = {}
    T["x"] = nc.dram_tensor("x", [nseq, SEQ, DM], F32, kind="ExternalInput").ap()
    for nm, shp in W_SPECS:
        T[nm] = nc.dram_tensor(nm, shp, F32, kind="ExternalInput").ap()
    for nm, rows in (("rope64c", 64), ("rope64s", 64), ("rope32c", 32), ("rope32s", 32)):
        T[nm] = nc.dram_tensor(nm, [rows, SEQ], F32, kind="ExternalInput").ap()
    out = nc.dram_tensor("out", [nseq, SEQ, DM], F32, kind="ExternalOutput").ap()
    dk = "ExternalOutput" if debug else "Internal"

    def scratch(name, shape, dt):
        return nc.dram_tensor(name, shape, dt, kind=dk).ap()
    sc = {}
    qs = scratch("QS", [12, 96, SEQ], BF16)
    ks = scratch("KS", [12, 96, SEQ], BF16)
    sc["QS"] = [qs[h] for h in range(12)]
    sc["KS"] = [ks[h] for h in range(12)]
    vs = scratch("VS", [6, 128, NB * 2 * 65], BF16)
    sc["VS"] = [vs[p] for p in range(6)]
    qd = scratch("QD", [3, 4, 64, SEQ], BF16)
    kd = scratch("KD", [3, 4, 64, SEQ], BF16)
    sc["QD"] = [[qd[g, h] for h in range(4)] for g in range(3)]
    sc["KD"] = [[kd[g, h] for h in range(4)] for g in range(3)]
    vd = scratch("VD", [3, 2, 128, NB * 2 * 65], BF16)
    sc["VD"] = [[vd[g, p] for p in range(2)] for g in range(3)]
    sc["OnT"] = scratch("OnT", [8, 128, SEQ], BF16)
    sc["X1"] = scratch("X1", [SEQ, DM], F32)
    sc["X1T"] = scratch("X1T", [128, 8, SEQ], BF16)
    sc["GATE"] = scratch("GATE", [128, NB * 16], F32)
    sc["XS"] = scratch("XS", [NSLOT, DM], BF16)
    sc["GS"] = scratch("GS", [NSLOT, 16], F32)
    sc["YS"] = scratch("YS", [NSLOT, DM], F32)
    sc["DEST"] = scratch("DEST", [128, NB], I32)
    sc["IDXW"] = scratch("IDXW", [128, NTILE * 8], I32)
    xmid = scratch("XMID", [SEQ, DM], F32)
    sc["r_QS"] = [Res() for _ in range(12)]
    sc["r_KS"] = [Res() for _ in range(12)]
    sc["r_VS"] = [Res() for _ in range(6)]
    sc["r_QD"] = [[Res() for _ in range(4)] for _ in range(3)]
    sc["r_KD"] = [[Res() for _ in range(4)] for _ in range(3)]
    sc["r_VD"] = [[Res() for _ in range(2)] for _ in range(3)]
    for k in ("OnT", "X1", "X1T", "GATE", "XS", "GS", "YS", "DEST", "IDXW"):
        sc["r_" + k] = Res()
    r_xmid = Res()
    r_x = Res()
    r_out = Res()
    S = Sched(nc)
    for s in range(nseq):
        for li, l in enumerate(layers):
            xin, r_xin = (T["x"][s], r_x) if li == 0 else (xmid, r_xmid)
            last = li == len(layers) - 1
            xo, r_xo = (out[s], r_out) if last else (xmid, r_xmid)
            if 1 in phases:
                phase1(nc, S, T, xin, r_xin, l, sc)
            if 2 in phases:
                phase2(nc, S, T, l, sc, heads=heads)
            if 3 in phases:
                phase3(nc, S, T, xin, r_xin, l, sc)
            if 4 in phases:
                if ROUTED:
                    phase4r(nc, S, T, l, sc, xo, r_xo, final=last)
                else:
                    phase4(nc, S, T, l, sc, xo, r_xo, final=last, TG=TG)
    S.close()
    return nc, S


_CACHE = {}


def kernel(**inputs):
    n = 8
    nseq = 2
    x = np.ascontiguousarray(np.asarray(inputs["x"], dtype=np.float32))
    tabs = rope_tables()
    if "nc" not in _CACHE:
        _CACHE["nc"] = build_program(nseq=nseq)[0]
    nc = _CACHE["nc"]
    base = {nm: np.ascontiguousarray(np.asarray(inputs[nm], dtype=np.float32)) for nm, _ in W_SPECS}
    base.update(tabs)
    in_maps = []
    for c in range(n):
        m = dict(base)
        m["x"] = x[c * nseq:(c + 1) * nseq]
        in_maps.append(m)
    res = run_bass_kernel_spmd(nc, in_maps, core_ids=list(range(n)))
    return np.concatenate([r["out"] for r in res.results], axis=0).astype(np.float32)
```

```python
import numpy as np
from os import environ as _os_env
import concourse.bass as bass
import concourse.mybir as mybir
from concourse.bass_utils import run_bass_kernel_spmd

F32 = mybir.dt.float32
BF16 = mybir.dt.bfloat16
I32 = mybir.dt.int32
AF = mybir.ActivationFunctionType
ALU = mybir.AluOpType
AX = mybir.AxisListType

SAME_ENGINE_SYNC = bool(int(_os_env.get("SES", "1")))
NDMA_SLOTS = 6

SEQ = 4096
DM = 1024
NB = SEQ // 128
NT = SEQ // 512
DEPTH = 2
ALPHA = (2.0 * DEPTH) ** 0.25
LN_EPS = 1e-5
RMS_EPS = 1e-6
N_IN = 4260
C_SB, C_FOX, C_MLA, C_DIL = 0, 768, 1540, 1956
MLA_SCALE = 96.0 ** -0.5
DIL_R = (1, 4, 16)
NEXP = 16
DEXP = 512
NTILE = 11
NSLOT = NTILE * 512
ROUTED = bool(int(_os_env.get("ROUTED", "1")))


class Res:
    __slots__ = ("w", "r", "excl")

    def __init__(self, excl=False):
        self.w = None
        self.r = []
        self.excl = excl


class Sched:
    ENG = ("pe", "act", "dve", "pool", "sp")

    def __init__(self, nc):
        self.nc = nc
        self.ops = {e: [] for e in self.ENG}
        self.cnt = {e: 0 for e in self.ENG}
        self.seen = {e: {} for e in self.ENG}
        self.sems = {}
        self.dma_slots = {}
        self.dma_rr = {}
        self.final_waits = []
        self._stack = []
        self.nops = 0

    def sem(self, key):
        if key not in self.sems:
            cm = self.nc.semaphore("s_" + "_".join(str(k) for k in (key if isinstance(key, tuple) else (key,))))
            s = cm.__enter__()
            self._stack.append(cm)
            self.sems[key] = s
        return self.sems[key]

    def _deps(self, eng, reads, writes):
        deps = {}

        def add(t):
            if t is None:
                return
            k, v = t
            if deps.get(k, 0) < v:
                deps[k] = v
        for r in reads:
            add(r.w)
            if r.excl:
                for t in r.r:
                    if t[0] != eng:
                        add(t)
        for w in writes:
            add(w.w)
            for t in w.r:
                add(t)
        waits = []
        seen = self.seen[eng]
        for k, v in deps.items():
            if k == eng and (eng == "pe" or not SAME_ENGINE_SYNC):
                continue
            if seen.get(k, 0) >= v:
                continue
            seen[k] = v
            waits.append((k, v))
        return waits

    def _commit(self, ticket, reads, writes):
        for r in reads:
            if len(r.r) > 16:
                m = {}
                for k, v in r.r:
                    if m.get(k, 0) < v:
                        m[k] = v
                r.r = list(m.items())
            r.r.append(ticket)
        for w in writes:
            w.w = ticket
            w.r = []

    def op(self, eng, fn, reads=(), writes=()):
        waits = self._deps(eng, reads, writes)
        self.cnt[eng] += 1
        ticket = (eng, self.cnt[eng])
        self.ops[eng].append((waits, fn, (eng, 1)))
        self._commit(ticket, reads, writes)
        self.nops += 1
        return ticket

    def dma(self, q, out, in_, reads=(), writes=(), final=False, **kw):
        fn = lambda e, out=out, in_=in_, kw=kw: e.dma_start(out=out, in_=in_, **kw)
        return self.dma_fn(q, fn, reads, writes, final)

    def dma_fn(self, q, fn, reads=(), writes=(), final=False):
        waits = self._deps(q, reads, writes)
        if q not in self.dma_slots:
            self.dma_slots[q] = [[("d", q, i), 0] for i in range(NDMA_SLOTS)]
            self.dma_rr[q] = 0
        i = self.dma_rr[q]
        self.dma_rr[q] = (i + 1) % NDMA_SLOTS
        slot = self.dma_slots[q][i]
        key, tot = slot
        if tot > 0 and self.seen[q].get(key, 0) < tot:
            self.seen[q][key] = tot
            waits.append((key, tot))
        slot[1] = tot + 16
        ticket = (key, tot + 16)
        self.ops[q].append((waits, fn, (key, 16)))
        self._commit(ticket, reads, writes)
        self.nops += 1
        if final:
            self.final_waits.append(ticket)
        return ticket

    def flush(self):
        totals = [(e, self.cnt[e]) for e in self.ENG if self.cnt[e] > 0]
        for q, slots in self.dma_slots.items():
            for key, tot in slots:
                if tot > 0:
                    totals.append((key, tot))
        for e in self.ENG:
            self.sem(e)
            for waits, fn, inc in self.ops[e]:
                for k, v in waits:
                    self.sem(k)
                self.sem(inc[0])
        ops = self.ops
        self.ops = {e: [] for e in self.ENG}
        for e in self.ENG:
            for k, v in totals:
                self.seen[e][k] = max(self.seen[e].get(k, 0), v)
        with self.nc.Block() as block:
            def run(engname):
                def body(e):
                    for waits, fn, inc in ops[engname]:
                        for k, v in waits:
                            e.wait_ge(self.sems[k], v)
                        fn(e).then_inc(self.sems[inc[0]], inc[1])
                    for k, v in totals:
                        e.wait_ge(self.sems[k], v)
                return body
            block.sync(run("sp"))
            block.tensor(run("pe"))
            block.scalar(run("act"))
            block.vector(run("dve"))
            block.gpsimd(run("pool"))

    def close(self):
        for cm in reversed(self._stack):
            cm.__exit__(None, None, None)
        self._stack = []


_UID = [0]


class Alloc:
    def __init__(self, nc):
        self.nc = nc
        self.stack = []

    @property
    def n(self):
        _UID[0] += 1
        return _UID[0]

    def sb(self, name, shape, dt):
        cm = self.nc.sbuf_tensor("%s_%d" % (name, self.n), list(shape), dt)
        t = cm.__enter__()
        self.stack.append(cm)
        return t

    def ps(self, name, shape, dt):
        cm = self.nc.psum_tensor("%s_%d" % (name, self.n), list(shape), dt)
        t = cm.__enter__()
        self.stack.append(cm)
        return t

    def close(self):
        for cm in reversed(self.stack):
            cm.__exit__(None, None, None)
        self.stack = []


class Buf:
    def __init__(self, t, excl=False):
        self.t = t
        self.r = Res(excl)


def rot(A, kind, name, shape, dt, n):
    f = A.sb if kind == "sb" else A.ps
    return [Buf(f(name + str(i), shape, dt), excl=(kind == "ps")) for i in range(n)]


def make_ident(A, S, dt):
    b = Buf(A.sb("ident", [128, 128], dt))
    S.op("pool", lambda e: e.memset(b.t[:], 1.0), writes=[b.r])
    S.op("pool", lambda e: e.affine_select(out=b.t[:], in_=b.t[:], pattern=[[-1, 128]], compare_op=ALU.is_equal,
                                           fill=0.0, base=0, channel_multiplier=1), reads=[b.r], writes=[b.r])
    return b


def perm_view(ap2d, r, t0, n):
    if r == 1:
        return ap2d[:, t0:t0 + n]
    sc = SEQ // r
    v = ap2d.rearrange("p (i c) -> p c i", c=r)
    c0, i0 = t0 // sc, t0 % sc
    if i0 + n <= sc:
        return v[:, c0, i0:i0 + n]
    assert i0 == 0 and n % sc == 0
    return v[:, c0:c0 + n // sc, :]


import os as _os
_P1SEC = _os.environ.get("P1SEC", "abcd")
_MSUB = _os.environ.get("MSUB", "qkrvptc")
_MC = _os.environ.get("MC", "1234")


def phase1(nc, S, T, xin, r_xin, l, sc):
    A = Alloc(nc)
    ident = make_ident(A, S, BF16)
    xT = Buf(A.sb("xT", [128, 8, SEQ], BF16))
    xs = rot(A, "sb", "xs", [128, DM], F32, 4)
    xb = rot(A, "sb", "xb", [128, DM], BF16, 3)
    pst = rot(A, "ps", "pst", [128, DM], BF16, 2)
    pj = rot(A, "ps", "pj", [128, 512], F32, 4)
    pv = rot(A, "ps", "pv", [128, 512], F32, 2)
    Win = T["w_in"][l].rearrange("(c p) n -> p c n", p=128)

    for b in range(NB):
        s, c_, p = xs[b % 4], xb[b % 3], pst[b % 2]
        S.dma("sp", s.t[:], xin[b * 128:(b + 1) * 128, :], reads=[r_xin], writes=[s.r])
        S.op("act", lambda e, s=s, c_=c_: e.activation(out=c_.t[:], in_=s.t[:], func=AF.Copy), reads=[s.r], writes=[c_.r])
        for c in range(8):
            S.op("pe", lambda e, c=c, c_=c_, p=p: e.transpose(out=p.t[:, c * 128:(c + 1) * 128], in_=c_.t[:, c * 128:(c + 1) * 128],
                                                              identity=ident.t[:]), reads=[c_.r, ident.r], writes=[p.r])
        S.op("dve", lambda e, b=b, p=p: e.tensor_copy(out=xT.t[:, :, b * 128:(b + 1) * 128],
                                                      in_=p.t[:, :].rearrange("p (c t) -> p c t", c=8)), reads=[p.r], writes=[xT.r])

    wts = rot(A, "sb", "wt", [128, 8, 416], BF16, 2)
    wsw = rot(A, "sb", "wsw", [128, 8, 256], BF16, 2)
    stg = rot(A, "sb", "stg", [128, 512], BF16, 4)
    vst = rot(A, "sb", "vst", [128, NB, 2, 65], BF16, 2)
    tmp1 = rot(A, "sb", "tmp1", [128, 512], F32, 2)
    tmp2 = rot(A, "sb", "tmp2", [128, 512], F32, 2)
    CT = Buf(A.sb("ropeC", [128, SEQ], BF16))
    ST = Buf(A.sb("ropeS", [128, SEQ], BF16))
    for v in vst:
        S.op("pool", lambda e, v=v: e.memset(v.t[:], 1.0), writes=[v.r])
    state = {"w": 0, "pj": 0, "stg": 0, "v": 0, "pv": 0}

    def load_w(col_ranges):
        w = wts[state["w"] % 2]
        state["w"] += 1
        o = 0
        for (c0, n) in col_ranges:
            S.dma("pool", w.t[:, :, o:o + n], Win[:, :, c0:c0 + n], writes=[w.r])
            o += n
        return w

    def nxt(key, lst):
        b = lst[state[key] % len(lst)]
        state[key] += 1
        return b

    def proj_fm(w, o, M, r, j, wtile=None):
        p = nxt("pj", pj)
        wt_ = w if wtile is None else wtile
        for c in range(8):
            S.op("pe", lambda e, c=c, p=p, wt_=wt_: e.matmul(p.t[0:M, :], lhsT=wt_.t[:, c, o:o + M],
                                                             rhs=perm_view(xT.t[:, c, :], r, j * 512, 512),
                                                             start=(c == 0), stop=(c == 7)),
                 reads=[wt_.r, xT.r], writes=[p.r])
        return p

    def store_rows(st, rows, dst, r_dst, j):
        S.dma("sp", dst[:, j * 512:(j + 1) * 512], st.t[rows[0]:rows[1], :], reads=[st.r], writes=[r_dst])

    def v_proj(w, o, r, vt, ncol=128):
        for b4 in range(NB // 4):
            p = nxt("pv", pv)
            for bb in range(4):
                b = b4 * 4 + bb
                for c in range(8):
                    S.op("pe", lambda e, c=c, b=b, bb=bb, p=p: e.matmul(p.t[:, bb * 128:(bb + 1) * 128],
                                                                      lhsT=perm_view(xT.t[:, c, :], r, b * 128, 128),
                                                                      rhs=w.t[:, c, o:o + ncol], start=(c == 0), stop=(c == 7)),
                         reads=[w.r, xT.r], writes=[p.r])
            S.op("dve", lambda e, b4=b4, p=p: e.tensor_copy(
                out=vt.t[:, b4 * 4:(b4 + 1) * 4, :, 0:64],
                in_=p.t[:, :].rearrange("p (b h d) -> p b h d", b=4, h=2)), reads=[p.r], writes=[vt.r])

    def scale_q(w):
        S.op("dve", lambda e: e.tensor_scalar(out=w.t[:, :, 0:128], in0=w.t[:, :, 0:128], scalar1=0.125, scalar2=None,
                                              op0=ALU.mult), reads=[w.r], writes=[w.r])

    for kind, cbase, hbase, pbase in (("sb", C_SB, 0, 0), ("fox", C_FOX, 4, 2)):
        for hp in range(2):
            w = load_w([(cbase + hp * 128, 128), (cbase + 256 + hp * 128, 128), (cbase + 512 + hp * 128, 128)])
            scale_q(w)
            for qk, dst, rd in ((0, sc["QS"], sc["r_QS"]), (1, sc["KS"], sc["r_KS"])):
                for j in range(NT):
                    p = proj_fm(w, qk * 128, 128, 1, j)
                    st = nxt("stg", stg)
                    S.op("act", lambda e, p=p, st=st: e.activation(out=st.t[:], in_=p.t[:], func=AF.Copy), reads=[p.r], writes=[st.r])
                    for hh in range(2):
                        h = hbase + hp * 2 + hh
                        store_rows(st, (hh * 64, hh * 64 + 64), dst[h][0:64, :], rd[h], j)
            vt = nxt("v", vst)
            v_proj(w, 256, 1, vt)
            S.dma("sp", sc["VS"][pbase + hp], vt.t[:, :, :, :].rearrange("p b h d -> p (b h d)"), reads=[vt.r], writes=[sc["r_VS"][pbase + hp]])

    S.flush()
    if "b" not in _P1SEC:
        A.close()
        return
    A2 = Alloc(nc)
    wf = Buf(A2.sb("wf", [128, 8, 4], BF16))
    S.dma("pool", wf.t[:], Win[:, :, C_FOX + 768:C_FOX + 772], writes=[wf.r])
    bfg = Buf(A2.sb("bfg", [4, 1], F32))
    S.dma("sp", bfg.t[:], T["b_forget"][l].rearrange("(h o) -> h o", o=1), writes=[bfg.r])
    S.op("dve", lambda e: e.tensor_scalar(out=bfg.t[:], in0=bfg.t[:], scalar1=-1.0, scalar2=None, op0=ALU.mult), reads=[bfg.r], writes=[bfg.r])
    nlf = Buf(A2.sb("nlf", [4, SEQ], F32))
    ncum = Buf(A2.sb("ncum", [4, SEQ], F32))
    ones4 = Buf(A2.sb("ones4", [4, 512], F32))
    S.op("pool", lambda e: e.memset(ones4.t[:], 1.0), writes=[ones4.r])
    for j in range(NT):
        p = proj_fm(wf, 0, 4, 1, j)
        S.op("act", lambda e, p=p, j=j: e.activation(out=nlf.t[:, j * 512:(j + 1) * 512], in_=p.t[0:4, :], func=AF.Exp,
                                                     bias=bfg.t[:, 0:1], scale=-1.0), reads=[p.r, bfg.r], writes=[nlf.r])
    S.op("act", lambda e: e.activation(out=nlf.t[:], in_=nlf.t[:], func=AF.Ln, bias=1.0), reads=[nlf.r], writes=[nlf.r])
    for j in range(NT):
        sl = slice(j * 512, (j + 1) * 512)
        init = 0.0 if j == 0 else ncum.t[:, j * 512 - 1:j * 512]
        S.op("dve", lambda e, sl=sl, init=init: e.tensor_tensor_scan(out=ncum.t[:, sl], data0=ones4.t[:, :], data1=nlf.t[:, sl],
                                                                     initial=init, op0=ALU.mult, op1=ALU.add),
             reads=[ones4.r, nlf.r, ncum.r], writes=[ncum.r])
    class _Alias:
        def __init__(self, t, r):
            self.t, self.r = t, r
    nlf_b = nlf.t[:, :].bitcast(BF16)
    parts = [_Alias(nlf_b[:, 0:SEQ], nlf.r), _Alias(nlf_b[:, SEQ:2 * SEQ], nlf.r), Buf(A2.sb("cpart2", [4, SEQ], BF16))]
    for i in range(3):
        S.op("dve", lambda e, i=i: e.tensor_copy(out=parts[i].t[:], in_=ncum.t[:]), reads=[ncum.r], writes=[parts[i].r])
        if i < 2:
            S.op("dve", lambda e, i=i: e.tensor_tensor(out=ncum.t[:], in0=ncum.t[:], in1=parts[i].t[:], op=ALU.subtract),
                 reads=[ncum.r, parts[i].r], writes=[ncum.r])
    ones3 = Buf(A2.sb("ones3", [35, SEQ], BF16))
    S.op("pool", lambda e: e.memset(ones3.t[0:3, :], 1.0), writes=[ones3.r])
    S.op("pool", lambda e: e.memset(ones3.t[32:35, :], -1.0), writes=[ones3.r])
    for h in range(4):
        H = 4 + h
        S.dma("sp", sc["KS"][H][64:67, :], ones3.t[32:35, :], reads=[ones3.r], writes=[sc["r_KS"][H]])
        S.dma("sp", sc["QS"][H][67:70, :], ones3.t[0:3, :], reads=[ones3.r], writes=[sc["r_QS"][H]])
        for i in range(3):
            S.dma("sp", sc["KS"][H][67 + i:68 + i, :], parts[i].t[h:h + 1, :], reads=[parts[i].r], writes=[sc["r_KS"][H]])
            S.dma("sp", sc["QS"][H][64 + i:65 + i, :], parts[i].t[h:h + 1, :], reads=[parts[i].r], writes=[sc["r_QS"][H]])
    S.flush()
    A2.close()

    if "c" not in _P1SEC:
        A.close()
        return
    w = load_w([(C_MLA, 416)])
    wkrs = nxt("w", wsw) if False else wsw[0]
    if "p" in _MSUB:
        S.op("pool", lambda e: e.tensor_copy(out=wkrs.t[:, :, 0:16], in_=w.t[:, :, 400:416]), reads=[w.r], writes=[wkrs.r])
        S.op("pool", lambda e: e.tensor_copy(out=wkrs.t[:, :, 16:32], in_=w.t[:, :, 384:400]), reads=[w.r], writes=[wkrs.r])
    A3 = Alloc(nc)
    wuq = Buf(A3.sb("wuq", [128, 2, 384], BF16))
    wuqs = Buf(A3.sb("wuqs", [128, 2, 384], BF16))
    wukv = Buf(A3.sb("wukv", [128, 512], BF16))
    S.dma("pool", wuq.t[:], T["w_uq"][l].rearrange("(c p) n -> p c n", p=128), writes=[wuq.r])
    S.dma("pool", wukv.t[:], T["w_ukv"][l], writes=[wukv.r])
    S.op("pool", lambda e: e.tensor_copy(out=wuqs.t[:], in_=wuq.t[:]), reads=[wuq.r], writes=[wuqs.r])
    for c2 in (range(2) if "p" in _MSUB else []):
        v4o = wuqs.t[:, c2, :].rearrange("p (h d) -> p h d", h=4)
        v4i = wuq.t[:, c2, :].rearrange("p (h d) -> p h d", h=4)
        S.op("pool", lambda e, v4o=v4o, v4i=v4i: e.tensor_copy(out=v4o[:, :, 64:80], in_=v4i[:, :, 80:96]), reads=[wuq.r, wuqs.r], writes=[wuqs.r])
        S.op("pool", lambda e, v4o=v4o, v4i=v4i: e.tensor_copy(out=v4o[:, :, 80:96], in_=v4i[:, :, 64:80]), reads=[wuq.r, wuqs.r], writes=[wuqs.r])
    gcq = Buf(A3.sb("gcq", [128, 2], F32))
    gckv = Buf(A3.sb("gckv", [128, 1], F32))
    for c2 in range(2):
        S.dma("sp", gcq.t[:, c2:c2 + 1], T["g_cq"][l][c2 * 128:(c2 + 1) * 128].rearrange("(p o) -> p o", o=1), writes=[gcq.r])
    S.dma("sp", gckv.t[:], T["g_ckv"][l].rearrange("(p o) -> p o", o=1), writes=[gckv.r])
    for tb, nm in (((CT, "rope32c"), (ST, "rope32s")) if "t" in _MSUB else []):
        S.dma("pool", tb.t[0:32, :], T[nm], writes=[tb.r])
        S.dma("pool", tb.t[64:96, :], T[nm], writes=[tb.r])
    onesq = Buf(A3.sb("onesq", [128, 128], BF16))
    oneskv = Buf(A3.sb("oneskv", [128, 128], BF16))
    epst = Buf(A3.sb("epst", [128, 1], F32))
    S.op("pool", lambda e: e.memset(epst.t[:], RMS_EPS), writes=[epst.r])
    S.op("pool", lambda e: e.memset(onesq.t[:], 1.0 / 256.0), writes=[onesq.r])
    S.op("pool", lambda e: e.memset(oneskv.t[:], 1.0 / 128.0), writes=[oneskv.r])
    cqg = rot(A3, "sb", "cqg", [128, 2, 512], BF16, 2)
    cq2 = rot(A3, "sb", "cq2", [128, 2, 512], BF16, 2)
    ckg = rot(A3, "sb", "ckg", [128, 512], BF16, 2)
    ck2 = rot(A3, "sb", "ck2", [128, 512], BF16, 2)
    rq = rot(A3, "sb", "rq", [128, 512], F32, 2)
    rkv = rot(A3, "sb", "rkv", [128, 512], F32, 2)
    rtok = rot(A3, "sb", "rtok", [128, 1], F32, 2)
    vstm = Buf(A3.sb("vstm", [128, NB, 4, 65], BF16))
    S.op("pool", lambda e: e.memset(vstm.t[:], 1.0), writes=[vstm.r])
    wukv_v = wukv.t[:, :].rearrange("p (h x) -> p h x", h=4)[:, :, 64:128]
    for j in (range(NT) if "c" in _MSUB else []):
        tc = slice(j * 512, (j + 1) * 512)
        a, a2, kg, k2, rq_, rkv_ = cqg[j % 2], cq2[j % 2], ckg[j % 2], ck2[j % 2], rq[j % 2], rkv[j % 2]
        for c2 in (range(2) if "1" in _MC else []):
            p = proj_fm(w, c2 * 128, 128, 1, j)
            S.op("dve", lambda e, p=p, c2=c2, a=a: e.tensor_scalar(out=a.t[:, c2, :], in0=p.t[:], scalar1=gcq.t[:, c2:c2 + 1], scalar2=None,
                                                                  op0=ALU.mult), reads=[p.r, gcq.r], writes=[a.r])
            S.op("act", lambda e, p=p, c2=c2, a2=a2: e.activation(out=a2.t[:, c2, :], in_=p.t[:], func=AF.Square), reads=[p.r], writes=[a2.r])
        if "2" in _MC:
            p = proj_fm(w, 256, 128, 1, j)
            if "5" not in _MC:
                S.op("dve", lambda e, p=p, kg=kg: e.tensor_scalar(out=kg.t[:], in0=p.t[:], scalar1=gckv.t[:, 0:1], scalar2=None, op0=ALU.mult),
                     reads=[p.r, gckv.r], writes=[kg.r])
            if "6" not in _MC:
                S.op("act", lambda e, p=p, k2=k2: e.activation(out=k2.t[:], in_=p.t[:], func=AF.Square), reads=[p.r], writes=[k2.r])
        if "3" in _MC:
            p = nxt("pj", pj)
            for c2 in range(2):
                S.op("pe", lambda e, p=p, c2=c2, a2=a2: e.matmul(p.t[:], lhsT=onesq.t[:], rhs=a2.t[:, c2, :], start=(c2 == 0), stop=(c2 == 1)),
                     reads=[onesq.r, a2.r], writes=[p.r])
            S.op("act", lambda e, p=p, rq_=rq_: e.activation(out=rq_.t[:], in_=p.t[:], func=AF.Sqrt, bias=epst.t[:, 0:1]), reads=[p.r, epst.r], writes=[rq_.r])
            S.op("dve", lambda e, rq_=rq_: e.reciprocal(out=rq_.t[:], in_=rq_.t[:]), reads=[rq_.r], writes=[rq_.r])
        if "4" in _MC:
            p = nxt("pj", pj)
            S.op("pe", lambda e, p=p, k2=k2: e.matmul(p.t[:], lhsT=oneskv.t[:], rhs=k2.t[:], start=True, stop=True), reads=[oneskv.r, k2.r], writes=[p.r])
            S.op("act", lambda e, p=p, rkv_=rkv_: e.activation(out=rkv_.t[:], in_=p.t[:], func=AF.Sqrt, bias=epst.t[:, 0:1]), reads=[p.r, epst.r], writes=[rkv_.r])
            S.op("dve", lambda e, rkv_=rkv_: e.reciprocal(out=rkv_.t[:], in_=rkv_.t[:]), reads=[rkv_.r], writes=[rkv_.r])
        for h in (range(4) if "q" in _MSUB else []):
            H = 8 + h
            pa, pb = nxt("pj", pj), nxt("pj", pj)
            for pp, ww in ((pa, wuq), (pb, wuqs)):
                for c2 in range(2):
                    S.op("pe", lambda e, pp=pp, ww=ww, c2=c2, h=h, a=a: e.matmul(pp.t[0:96, :], lhsT=ww.t[:, c2, h * 96:(h + 1) * 96], rhs=a.t[:, c2, :],
                                                                             start=(c2 == 0), stop=(c2 == 1)), reads=[ww.r, a.r], writes=[pp.r])
            st = nxt("stg", stg)
            t1, t2 = tmp1[h % 2], tmp2[h % 2]
            S.op("dve", lambda e, pa=pa, st=st, rq_=rq_: e.scalar_tensor_tensor(out=st.t[0:64, :], in0=pa.t[0:64, :], scalar=MLA_SCALE, in1=rq_.t[0:64, :],
                                                                             op0=ALU.mult, op1=ALU.mult), reads=[pa.r, rq_.r], writes=[st.r])
            S.op("dve", lambda e, pa=pa, t1=t1, tc=tc: e.tensor_tensor(out=t1.t[64:96, :], in0=pa.t[64:96, :], in1=CT.t[64:96, tc], op=ALU.mult),
                 reads=[pa.r, CT.r], writes=[t1.r])
            S.op("dve", lambda e, pb=pb, t2=t2, tc=tc: e.tensor_tensor(out=t2.t[64:96, :], in0=pb.t[64:96, :], in1=ST.t[64:96, tc], op=ALU.mult),
                 reads=[pb.r, ST.r], writes=[t2.r])
            S.op("pool", lambda e, t1=t1, t2=t2: e.tensor_tensor(out=t1.t[64:96, :], in0=t1.t[64:96, :], in1=t2.t[64:96, :], op=ALU.add),
                 reads=[t1.r, t2.r], writes=[t1.r])
            S.op("dve", lambda e, t1=t1, st=st, rq_=rq_: e.scalar_tensor_tensor(out=st.t[64:96, :], in0=t1.t[64:96, :], scalar=MLA_SCALE, in1=rq_.t[64:96, :],
                                                                             op0=ALU.mult, op1=ALU.mult), reads=[t1.r, rq_.r, st.r], writes=[st.r])
            store_rows(st, (0, 96), sc["QS"][H][0:96, :], sc["r_QS"][H], j)
        for h in (range(4) if "k" in _MSUB else []):
            H = 8 + h
            p = nxt("pj", pj)
            S.op("pe", lambda e, p=p, h=h, kg=kg: e.matmul(p.t[0:64, :], lhsT=wukv.t[:, h * 128:h * 128 + 64], rhs=kg.t[:], start=True, stop=True),
                 reads=[wukv.r, kg.r], writes=[p.r])
            st = nxt("stg", stg)
            S.op("dve", lambda e, p=p, st=st, rkv_=rkv_: e.tensor_tensor(out=st.t[0:64, :], in0=p.t[0:64, :], in1=rkv_.t[0:64, :], op=ALU.mult),
                 reads=[p.r, rkv_.r], writes=[st.r])
            store_rows(st, (0, 64), sc["KS"][H][0:64, :], sc["r_KS"][H], j)
        if "r" not in _MSUB:
            continue
        pa = proj_fm(w, 384, 32, 1, j)
        pb = proj_fm(wkrs, 0, 32, 1, j)
        t1, t2 = tmp1[0], tmp2[0]
        st = nxt("stg", stg)
        S.op("dve", lambda e, pa=pa, t1=t1, tc=tc: e.tensor_tensor(out=t1.t[0:32, :], in0=pa.t[0:32, :], in1=CT.t[0:32, tc], op=ALU.mult),
             reads=[pa.r, CT.r], writes=[t1.r])
        S.op("dve", lambda e, pb=pb, t2=t2, tc=tc: e.tensor_tensor(out=t2.t[0:32, :], in0=pb.t[0:32, :], in1=ST.t[0:32, tc], op=ALU.mult),
             reads=[pb.r, ST.r], writes=[t2.r])
        S.op("pool", lambda e, t1=t1, t2=t2, st=st: e.tensor_tensor(out=st.t[0:32, :], in0=t1.t[0:32, :], in1=t2.t[0:32, :], op=ALU.add),
             reads=[t1.r, t2.r], writes=[st.r])
        for h in range(4):
            store_rows(st, (0, 32), sc["KS"][8 + h][64:96, :], sc["r_KS"][8 + h], j)
        for bb in (range(4) if "v" in _MSUB else []):
            b = j * 4 + bb
            p = nxt("pv", pv)
            S.op("pe", lambda e, p=p, bb=bb, kg=kg: e.matmul(p.t[:, 0:256], lhsT=kg.t[:, bb * 128:(bb + 1) * 128], rhs=wukv_v, start=True, stop=True),
                 reads=[wukv.r, kg.r], writes=[p.r])
            S.op("pe", lambda e, p=p, bb=bb, k2=k2: e.matmul(p.t[:, 256:257], lhsT=k2.t[:, bb * 128:(bb + 1) * 128], rhs=oneskv.t[:, 0:1], start=True, stop=True),
                 reads=[oneskv.r, k2.r], writes=[p.r])
            rt = rtok[b % 2]
            S.op("act", lambda e, p=p, rt=rt: e.activation(out=rt.t[:], in_=p.t[:, 256:257], func=AF.Sqrt, bias=epst.t[:, 0:1]), reads=[p.r, epst.r], writes=[rt.r])
            S.op("dve", lambda e, rt=rt: e.reciprocal(out=rt.t[:], in_=rt.t[:]), reads=[rt.r], writes=[rt.r])
            S.op("dve", lambda e, p=p, b=b, rt=rt: e.tensor_scalar(out=vstm.t[:, b, :, 0:64], in0=p.t[:, 0:256].rearrange("p (h d) -> p h d", h=4),
                                                                  scalar1=rt.t[:, 0:1], scalar2=None, op0=ALU.mult), reads=[p.r, rt.r], writes=[vstm.r])
    for hp in range(2):
        S.dma("sp", sc["VS"][4 + hp].rearrange("p (b h d) -> p b h d", b=NB, h=2), vstm.t[:, :, 2 * hp:2 * hp + 2, :], reads=[vstm.r], writes=[sc["r_VS"][4 + hp]])

    S.flush()
    A3.close()
    if "d" not in _P1SEC:
        A.close()
        return
    for tb, nm in ((CT, "rope64c"), (ST, "rope64s")):
        S.dma("pool", tb.t[0:64, :], T[nm], writes=[tb.r])
        S.dma("pool", tb.t[64:128, :], T[nm], writes=[tb.r])
    for g in range(3):
        r = DIL_R[g]
        for hp in range(2):
            o = g * 256 + hp * 128
            w = load_w([(C_DIL + o, 128), (C_DIL + 768 + o, 128), (C_DIL + 1536 + o, 128)])
            scale_q(w)
            ws = wsw[(g * 2 + hp) % 2]
            for c in range(8):
                vo = ws.t[:, c, :].rearrange("p (h f d) -> p h f d", h=4, f=2)
                vi = w.t[:, c, 0:256].rearrange("p (h f d) -> p h f d", h=4, f=2)
                S.op("pool", lambda e, vo=vo, vi=vi: e.tensor_copy(out=vo[:, :, 0, :], in_=vi[:, :, 1, :]), reads=[w.r], writes=[ws.r])
                S.op("pool", lambda e, vo=vo, vi=vi: e.tensor_copy(out=vo[:, :, 1, :], in_=vi[:, :, 0, :]), reads=[w.r], writes=[ws.r])
            for qk, dst, rd in ((0, sc["QD"], sc["r_QD"]), (1, sc["KD"], sc["r_KD"])):
                for j in range(NT):
                    pa = proj_fm(w, qk * 128, 128, r, j)
                    pb = proj_fm(ws, qk * 128, 128, r, j)
                    t1, t2 = tmp1[j % 2], tmp2[j % 2]
                    st = nxt("stg", stg)
                    cv = perm_view(CT.t[:, :], r, j * 512, 512)
                    sv = perm_view(ST.t[:, :], r, j * 512, 512)
                    shp = None if len(cv.shape) == 2 else cv.shape

                    def v3(ap):
                        return ap if shp is None else ap.rearrange("p (a b) -> p a b", a=shp[1])
                    S.op("dve", lambda e, pa=pa, t1=t1, cv=cv, v3=v3: e.tensor_tensor(out=v3(t1.t[:]), in0=v3(pa.t[:]), in1=cv, op=ALU.mult),
                         reads=[pa.r, CT.r], writes=[t1.r])
                    S.op("dve", lambda e, pb=pb, t2=t2, sv=sv, v3=v3: e.tensor_tensor(out=v3(t2.t[:]), in0=v3(pb.t[:]), in1=sv, op=ALU.mult),
                         reads=[pb.r, ST.r], writes=[t2.r])
                    S.op("pool", lambda e, t1=t1, t2=t2, st=st: e.tensor_tensor(out=st.t[:], in0=t1.t[:], in1=t2.t[:], op=ALU.add),
                         reads=[t1.r, t2.r], writes=[st.r])
                    for hh in range(2):
                        store_rows(st, (hh * 64, hh * 64 + 64), dst[g][hp * 2 + hh], rd[g][hp * 2 + hh], j)
            vt = nxt("v", vst)
            v_proj(w, 256, r, vt)
            S.dma("sp", sc["VD"][g][hp], vt.t[:, :, :, :].rearrange("p b h d -> p (b h d)"), reads=[vt.r], writes=[sc["r_VD"][g][hp]])
    S.flush()
    A.close()


def phase2(nc, S, T, l, sc, heads=None, after_sb=None):
    A = Alloc(nc)
    negtri = Buf(A.sb("negtri", [128, 128], BF16))
    S.op("pool", lambda e: e.memset(negtri.t[:], -1.0), writes=[negtri.r])
    S.op("pool", lambda e: e.affine_select(out=negtri.t[:], in_=negtri.t[:], pattern=[[-1, 128]], compare_op=ALU.is_ge, fill=0.0, base=0,
                                           channel_multiplier=1), reads=[negtri.r], writes=[negtri.r])
    ones = Buf(A.sb("ones", [128, 128], BF16))
    S.op("pool", lambda e: e.memset(ones.t[:], 1.0), writes=[ones.r])
    wn = Buf(A.sb("wn", [65, 64], BF16))
    wnsb = Buf(A.sb("wnsb", [65, 64], BF16))
    for t_, v_ in ((wn, RMS_EPS), (wnsb, 0.0)):
        S.op("pool", lambda e, t_=t_: e.memset(t_.t[:], 1.0 / 64.0), writes=[t_.r])
        S.op("pool", lambda e, t_=t_, v_=v_: e.memset(t_.t[64:65, :], v_), reads=[t_.r], writes=[t_.r])
    gh = Buf(A.sb("gh", [64, 16], F32))
    for h_ in range(16):
        S.dma("sp", gh.t[:, h_:h_ + 1], T["g_head"][l][h_].rearrange("(d o) -> d o", o=1), writes=[gh.r])
    eps2 = Buf(A.sb("eps2", [64, 2], F32))
    S.op("pool", lambda e: e.memset(eps2.t[:, 0:1], RMS_EPS), writes=[eps2.r])
    S.op("pool", lambda e: e.memset(eps2.t[:, 1:2], 0.0), reads=[eps2.r], writes=[eps2.r])

    Qt = rot(A, "sb", "Qt", [128, SEQ], BF16, 2)
    Kt = rot(A, "sb", "Kt", [128, SEQ], BF16, 2)
    Vt = rot(A, "sb", "Vt", [128, NB, 2, 65], BF16, 2)
    pz = rot(A, "ps", "pz", [128, 512], F32, 3)
    po = rot(A, "ps", "po", [128, 512], F32, 2)
    pc = rot(A, "ps", "pc", [128, 512], F32, 2)
    pss = rot(A, "ps", "pss", [128, 512], F32, 1)
    Pb = rot(A, "sb", "Pb", [128, 512], BF16, 4)
    eb = rot(A, "sb", "eb", [128, 512], F32, 2)
    spb = rot(A, "sb", "spb", [128, 512], BF16, 3)
    lw = rot(A, "sb", "lw", [128, 512], F32, 2)
    Rsb = Buf(A.sb("Rsb", [128, 512], F32))
    sqb = rot(A, "sb", "sqb", [65, 512], BF16, 2)
    osb = rot(A, "sb", "osb", [65, 512], F32, 2)
    def mk_mask(name, n, conds):
        m = Buf(A.sb(name, [128, n], BF16))
        S.op("pool", lambda e: e.memset(m.t[:], 1.0), writes=[m.r])
        for (step, base, cm) in conds:
            S.op("pool", lambda e, step=step, base=base, cm=cm: e.affine_select(out=m.t[:], in_=m.t[:], pattern=[[step, n]], compare_op=ALU.is_ge, fill=0.0,
                                                                                base=base, channel_multiplier=cm), reads=[m.r], writes=[m.r])
        return m
    maskS = [mk_mask("ms%d" % o, 512, [(1, -128 * o - 1, -1)]) for o in (3, 2, 1, 0)][::-1]
    maskC = [mk_mask("mc%d" % o, 512, [(1, -128 * o, -1)]) for o in range(4)]
    maskD = {512: {o: mk_mask("md%d" % (o + 1), 512, [(1, -128 * o, -1), (-1, 128 + 128 * o, 1)]) for o in range(-1, 4)},
             256: {o: mk_mask("me%d" % o, 256, [(1, -128 * o, -1), (-1, 128 + 128 * o, 1)]) for o in range(0, 2)}}
    stb = rot(A, "sb", "stb", [64, 512], F32, 2)
    yb = rot(A, "sb", "yb", [64, 512], BF16, 2)
    acc = rot(A, "sb", "acc", [65, SEQ], F32, 2)
    st = {"fin": 0, "ld": 0, "vld": 0, "o": 0}

    def finish(src_ap, r_src, h, t0, n, is_sb, in_sbuf=False):
        i = st["fin"]
        st["fin"] += 1
        sq, s_, y, ps_ = sqb[i % 2], stb[i % 2], yb[i % 2], pss[0]
        if in_sbuf:
            o_ap, r_o = src_ap, r_src
        else:
            ob = osb[i % 2]
            S.op("act", lambda e: e.activation(out=ob.t[:, 0:n], in_=src_ap, func=AF.Copy), reads=[r_src], writes=[ob.r])
            o_ap, r_o = ob.t[:, 0:n], ob.r
        S.op("dve", lambda e: e.tensor_tensor(out=sq.t[:, 0:n], in0=o_ap, in1=o_ap, op=ALU.mult), reads=[r_o], writes=[sq.r])
        wn_ = wnsb if is_sb else wn
        S.op("pe", lambda e: e.matmul(ps_.t[0:64, 0:n], lhsT=wn_.t[:, :], rhs=sq.t[:, 0:n], start=True, stop=True), reads=[wn_.r, sq.r], writes=[ps_.r])
        S.op("act", lambda e: e.activation(out=s_.t[:, 0:n], in_=ps_.t[0:64, 0:n], func=AF.Ln, bias=(eps2.t[:, 0:1] if is_sb else eps2.t[:, 1:2])),
             reads=[ps_.r, eps2.r], writes=[s_.r])
        S.op("act", lambda e: e.activation(out=s_.t[:, 0:n], in_=s_.t[:, 0:n], func=AF.Exp, scale=-0.5), reads=[s_.r], writes=[s_.r])
        S.op("dve", lambda e: e.scalar_tensor_tensor(out=y.t[:, 0:n], in0=o_ap[0:64], scalar=gh.t[:, h:h + 1], in1=s_.t[:, 0:n],
                                                     op0=ALU.mult, op1=ALU.mult), reads=[r_o, gh.r, s_.r], writes=[y.r])
        S.dma("sp", sc["OnT"][h // 2, (h % 2) * 64:(h % 2) * 64 + 64, t0:t0 + n], y.t[:, 0:n], reads=[y.r], writes=[sc["r_OnT"]])

    def load_qk(qsrc, r_q, ksrc, r_k, kd):
        i = st["ld"]
        st["ld"] += 1
        q, k = Qt[i % 2], Kt[i % 2]
        S.dma("sp", q.t[0:kd, :], qsrc, reads=[r_q], writes=[q.r])
        S.dma("sp", k.t[0:kd, :], ksrc, reads=[r_k], writes=[k.r])
        return q, k

    def load_v(vsrc, r_v):
        i = st["vld"]
        st["vld"] += 1
        v = Vt[i % 2]
        S.dma("sp", v.t[:, :, :, :].rearrange("p b h d -> p (b h d)"), vsrc, reads=[r_v], writes=[v.r])
        return v

    def run_steps(steps, q, k, kd, v, hh, kind, done_cb):
        n_ = len(steps)
        ctx = [dict() for _ in range(n_)]

        def s1(i):
            sp_ = steps[i]
            z = pz[i % 3] if kind != "sb" else pz[i % 2]
            q0, n, kb = sp_["q0"], sp_["n"], sp_["kb"]
            S.op("pe", lambda e: e.matmul(z.t[:, 0:n], lhsT=k.t[0:kd, kb * 128:(kb + 1) * 128], rhs=q.t[0:kd, q0:q0 + n], start=True, stop=True),
                 reads=[k.r, q.r], writes=[z.r])
            if kind == "sb":
                e_, s_ = eb[i % 2], spb[i % 3]
                S.op("act", lambda e: e.activation(out=e_.t[:, 0:n], in_=z.t[:, 0:n], func=AF.Exp), reads=[z.r], writes=[e_.r])
                S.op("act", lambda e: e.activation(out=s_.t[:, 0:n], in_=e_.t[:, 0:n], func=AF.Ln, bias=1.0), reads=[e_.r], writes=[s_.r])
                if sp_["mask"] is not None:
                    mk = sp_["mask"]
                    S.op("pool", lambda e: e.tensor_tensor(out=s_.t[:, 0:n], in0=s_.t[:, 0:n], in1=mk.t[:, 0:n], op=ALU.mult), reads=[s_.r, mk.r], writes=[s_.r])
                ctx[i]["sp"] = s_
            else:
                p_ = Pb[i % 4]
                if kind == "fox" and sp_["mask"] is not None:
                    l_ = lw[i % 2]
                    S.op("dve", lambda e: e.tensor_scalar(out=l_.t[:, 0:n], in0=z.t[:, 0:n], scalar1=60.0, scalar2=None, op0=ALU.min), reads=[z.r], writes=[l_.r])
                    S.op("act", lambda e: e.activation(out=p_.t[:, 0:n], in_=l_.t[:, 0:n], func=AF.Exp), reads=[l_.r], writes=[p_.r])
                else:
                    S.op("act", lambda e: e.activation(out=p_.t[:, 0:n], in_=z.t[:, 0:n], func=AF.Exp), reads=[z.r], writes=[p_.r])
                if sp_["mask"] is not None:
                    mk = sp_["mask"]
                    S.op("dve", lambda e: e.tensor_tensor(out=p_.t[:, 0:n], in0=p_.t[:, 0:n], in1=mk.t[:, 0:n], op=ALU.mult), reads=[p_.r, mk.r], writes=[p_.r])
                ctx[i]["P"] = p_

        def s2(i):
            if kind != "sb":
                return
            sp_ = steps[i]
            q0, n, kb = sp_["q0"], sp_["n"], sp_["kb"]
            s_ = ctx[i]["sp"]
            c_, rc, l_, p_ = pc[i % 2], pz[2], lw[i % 2], Pb[i % 4]
            S.op("pe", lambda e: e.matmul(c_.t[:, 0:n], lhsT=k.t[0:kd, kb * 128:(kb + 1) * 128], rhs=q.t[0:kd, q0:q0 + n], start=True, stop=False),
                 reads=[k.r, q.r], writes=[c_.r])
            S.op("pe", lambda e: e.matmul(c_.t[:, 0:n], lhsT=negtri.t[:], rhs=s_.t[:, 0:n], start=False, stop=True), reads=[negtri.r, s_.r], writes=[c_.r])
            S.op("pe", lambda e: e.matmul(rc.t[:, 0:n], lhsT=ones.t[:], rhs=s_.t[:, 0:n], start=True, stop=True), reads=[ones.r, s_.r], writes=[rc.r])
            if sp_["first"]:
                S.op("dve", lambda e: e.tensor_copy(out=l_.t[:, 0:n], in_=c_.t[:, 0:n]), reads=[c_.r], writes=[l_.r])
                S.op("dve", lambda e: e.tensor_copy(out=Rsb.t[:, 0:n], in_=rc.t[:, 0:n]), reads=[rc.r], writes=[Rsb.r])
            else:
                S.op("dve", lambda e: e.tensor_tensor(out=l_.t[:, 0:n], in0=c_.t[:, 0:n], in1=Rsb.t[:, 0:n], op=ALU.subtract), reads=[c_.r, Rsb.r], writes=[l_.r])
                S.op("dve", lambda e: e.tensor_tensor(out=Rsb.t[:, 0:n], in0=rc.t[:, 0:n], in1=Rsb.t[:, 0:n], op=ALU.add), reads=[rc.r, Rsb.r], writes=[Rsb.r])
            S.op("act", lambda e: e.activation(out=p_.t[:, 0:n], in_=l_.t[:, 0:n], func=AF.Exp), reads=[l_.r], writes=[p_.r])
            if sp_["mask"] is not None:
                mk = sp_["mask"]
                S.op("pool", lambda e: e.tensor_tensor(out=p_.t[:, 0:n], in0=p_.t[:, 0:n], in1=mk.t[:, 0:n], op=ALU.mult), reads=[p_.r, mk.r], writes=[p_.r])
            ctx[i]["P"] = p_

        def s3(i):
            sp_ = steps[i]
            n, kb = sp_["n"], sp_["kb"]
            if sp_["first"]:
                st["o"] += 1
            o_ = po[st["o"] % 2]
            p_ = ctx[i]["P"]
            S.op("pe", lambda e: e.matmul(o_.t[0:65, 0:n], lhsT=v.t[:, kb, hh, :], rhs=p_.t[:, 0:n], start=sp_["first"], stop=sp_["last"]),
                 reads=[v.r, p_.r], writes=[o_.r])
            if sp_["last"]:
                done_cb(o_, sp_)

        for i in range(n_ + 2):
            if i < n_:
                s1(i)
            if 0 <= i - 1 < n_:
                s2(i - 1)
            if 0 <= i - 2 < n_:
                s3(i - 2)

    def causal_steps(strict, descending):
        steps = []
        for qt in range(NT):
            q0 = qt * 512
            kbs = list(range(0, 4 * qt + 4))
            if descending:
                kbs = kbs[::-1]
            for ii, kb in enumerate(kbs):
                o = kb - 4 * qt
                steps.append(dict(q0=q0, n=512, kb=kb, mask=((maskS if strict else maskC)[o] if o >= 0 else None), first=(ii == 0), last=(ii == len(kbs) - 1)))
        return steps

    def dil_steps(r):
        sc_ = SEQ // r
        n = min(512, sc_)
        steps = []
        for q0 in range(0, SEQ, n):
            cs = (q0 // sc_) * sc_
            k_lo = max(cs, q0 - 128)
            kbs = list(range(k_lo // 128, (q0 + n) // 128))
            for ii, kb in enumerate(kbs):
                steps.append(dict(q0=q0, n=n, kb=kb, mask=maskD[n][kb - q0 // 128], first=(ii == 0), last=(ii == len(kbs) - 1)))
        return steps

    hsel = (lambda h: True) if heads is None else (lambda h: h in heads)
    jobs = []
    for kind, hbase, pbase, kd in (("sb", 0, 0, 64), ("fox", 4, 2, 70), ("mla", 8, 4, 96)):
        for hp in range(2):
            for hh in range(2):
                h = hbase + hp * 2 + hh
                if hsel(h):
                    jobs.append((kind, h, pbase + hp, hh, kd))
    step_cache = {"sb": causal_steps(True, True), "fox": causal_steps(False, False)}
    step_cache["mla"] = step_cache["fox"]
    loaded = {}
    vcur = {}

    def prefetch(job):
        kind, h, pr_, hh, kd = job
        if pr_ not in vcur:
            vcur.clear()
            vcur[pr_] = load_v(sc["VS"][pr_], sc["r_VS"][pr_])
        loaded[h] = load_qk(sc["QS"][h][0:kd, :], sc["r_QS"][h], sc["KS"][h][0:kd, :], sc["r_KS"][h], kd) + (vcur[pr_],)
    if jobs:
        prefetch(jobs[0])
    for ji, job in enumerate(jobs):
        kind, h, pr_, hh, kd = job
        if after_sb is not None and kind != "sb":
            after_sb()
            after_sb = None
        q, k, v = loaded.pop(h)
        if ji + 1 < len(jobs):
            prefetch(jobs[ji + 1])

        def done(o_, sp_, h=h, kind=kind):
            finish(o_.t[0:65, 0:sp_["n"]], o_.r, h, sp_["q0"], sp_["n"], kind == "sb")
        run_steps(step_cache[kind], q, k, kd, v, hh, kind, done)
    if after_sb is not None:
        after_sb()
    for hp in range(2):
        if not (hsel(12 + 2 * hp) or hsel(13 + 2 * hp)):
            continue
        for g in range(3):
            r = DIL_R[g]
            steps = dil_steps(r)
            v = load_v(sc["VD"][g][hp], sc["r_VD"][g][hp])
            for hh in range(2):
                hd = hp * 2 + hh
                q, k = load_qk(sc["QD"][g][hd], sc["r_QD"][g][hd], sc["KD"][g][hd], sc["r_KD"][g][hd], 64)
                a_ = acc[hh]

                def done(o_, sp_, a_=a_, r=r, g=g):
                    n, q0 = sp_["n"], sp_["q0"]
                    dst = perm_view(a_.t[:, :], r, q0, n)
                    if g == 0:
                        S.op("act", lambda e: e.activation(out=dst, in_=o_.t[0:65, 0:n], func=AF.Copy), reads=[o_.r], writes=[a_.r])
                    else:
                        S.op("dve", lambda e: e.tensor_tensor(out=dst, in0=o_.t[0:65, 0:n], in1=dst, op=ALU.add), reads=[o_.r, a_.r], writes=[a_.r])
                run_steps(steps, q, k, 64, v, hh, "dil", done)
        for hh in range(2):
            for qt in range(NT):
                finish(acc[hh].t[:, qt * 512:(qt + 1) * 512], acc[hh].r, 12 + hp * 2 + hh, qt * 512, 512, False, in_sbuf=True)
    S.flush()
    A.close()


def layernorm_block(S, y, g_b, b_b, small, out):
    st6, mv, rs = small["st6"], small["mv"], small["rs"]
    for hf in range(2):
        S.op("dve", lambda e, hf=hf: e.bn_stats(out=st6.t[:, hf, :], in_=y.t[:, hf * 512:(hf + 1) * 512]), reads=[y.r], writes=[st6.r])
    S.op("dve", lambda e: e.bn_aggr(out=mv.t[:], in_=st6.t[:, :, :].rearrange("p a b -> p (a b)")), reads=[st6.r], writes=[mv.r])
    S.op("act", lambda e: e.activation(out=rs.t[:], in_=mv.t[:, 1:2], func=AF.Ln, bias=small["eps"].t[:, 0:1]), reads=[mv.r, small["eps"].r], writes=[rs.r])
    S.op("act", lambda e: e.activation(out=rs.t[:], in_=rs.t[:], func=AF.Exp, scale=-0.5), reads=[rs.r], writes=[rs.r])
    S.op("dve", lambda e: e.scalar_tensor_tensor(out=y.t[:], in0=y.t[:], scalar=mv.t[:, 0:1], in1=g_b.t[:], op0=ALU.subtract, op1=ALU.mult),
         reads=[y.r, mv.r, g_b.r], writes=[y.r])
    S.op("dve", lambda e: e.scalar_tensor_tensor(out=out.t[:], in0=y.t[:], scalar=rs.t[:, 0:1], in1=b_b.t[:], op0=ALU.mult, op1=ALU.add),
         reads=[y.r, rs.r, b_b.r], writes=[out.r])


def bcast_row(S, A, name, src1d, n):
    b = Buf(A.sb(name, [128, n], F32))
    S.dma("sp", b.t[:], src1d.rearrange("(o n) -> o n", o=1).partition_broadcast(128), writes=[b.r])
    return b


def phase3(nc, S, T, xin, r_xin, l, sc):
    A = Alloc(nc)
    identf = make_ident(A, S, F32)
    wout = Buf(A.sb("wout", [128, 8, DM], BF16))
    S.dma("pool", wout.t[:], T["w_out"][l].rearrange("(c p) n -> p c n", p=128), writes=[wout.r])
    wr = Buf(A.sb("wr", [128, 8, 20], F32))
    S.dma("sp", wr.t[:, :, 0:4], T["w_group"][l].rearrange("(c p) n -> p c n", p=128), writes=[wr.r])
    S.dma("sp", wr.t[:, :, 4:20], T["w_expert"][l].rearrange("(c p) n -> p c n", p=128), writes=[wr.r])
    brt = Buf(A.sb("brt", [128, 20], F32))
    S.dma("sp", brt.t[:, 0:4], T["b_group"][l].rearrange("(o n) -> o n", o=1).partition_broadcast(128), writes=[brt.r])
    S.dma("sp", brt.t[:, 4:20], T["b_expert"][l].rearrange("(o n) -> o n", o=1).partition_broadcast(128), writes=[brt.r])
    g_b = bcast_row(S, A, "ln1g", T["ln1_g"][l], DM)
    b_b = bcast_row(S, A, "ln1b", T["ln1_b"][l], DM)
    on = rot(A, "sb", "on", [128, 8, 512], BF16, 2)
    xs = rot(A, "sb", "xs3", [128, DM], F32, 3)
    y = rot(A, "sb", "y3", [128, DM], F32, 3)
    x1 = rot(A, "sb", "x1o", [128, DM], F32, 6)
    xtf = rot(A, "sb", "xtf", [128, 8, 128], F32, 3)
    xtb = rot(A, "sb", "xtb", [128, 8, 512], BF16, 2)
    gate = Buf(A.sb("gate", [128, NB, 16], F32))
    lgall = Buf(A.sb("lgall", [128, NB, 20], F32))
    if ROUTED:
        x1b = Buf(A.sb("x1b", [128, NB, DM], BF16))
        gohall = Buf(A.sb("gohall", [128, NB, 4], F32))
        S.op("pool", lambda e: e.memset(x1b.t[:, 0:4, :], 0.0), writes=[x1b.r])
        S.op("pool", lambda e: e.memset(gate.t[:], 0.0), writes=[gate.r])
        for k in (range(NTILE) if int(_os_env.get("ZF", "1")) else []):
            S.dma("sp", sc["XS"][k * 512:(k + 1) * 512, :].rearrange("(p r) n -> p (r n)", p=128), x1b.t[:, 0:4, :].rearrange("p b n -> p (b n)"),
                  reads=[x1b.r], writes=[sc["r_XS"]])
        for k in (range(NTILE) if int(_os_env.get("ZF", "1")) else []):
            S.dma("sp", sc["GS"][k * 512:(k + 1) * 512, :].rearrange("(p r) n -> p (r n)", p=128), gate.t[:, 0:4, :].rearrange("p b n -> p (b n)"),
                  reads=[gate.r], writes=[sc["r_GS"]])
    ph = rot(A, "ps", "ph", [128, DM], F32, 2)
    ptr = rot(A, "ps", "ptr", [128, DM], F32, 1)
    plg = rot(A, "ps", "plg", [128, 512], F32, 2)
    epsl = Buf(A.sb("epsl", [128, 1], F32))
    S.op("pool", lambda e: e.memset(epsl.t[:], LN_EPS), writes=[epsl.r])
    small = [dict(st6=Buf(A.sb("st6", [128, 2, 6], F32)), mv=Buf(A.sb("mv", [128, 2], F32)), rs=Buf(A.sb("rs", [128, 1], F32)), eps=epsl) for _ in range(3)]
    pend = []
    pend2 = []
    for j in range(NT):
        o_ = on[j % 2]
        S.dma("sp", o_.t[:], sc["OnT"][:, :, j * 512:(j + 1) * 512].rearrange("c p t -> p c t"), reads=[sc["r_OnT"]], writes=[o_.r])
        xb_ = xtb[j % 2]
        for bb in range(4):
            b = j * 4 + bb
            s_, y_, x1_, xf_, p_, sm = xs[b % 3], y[b % 3], x1[b % 6], xtf[b % 3], ph[b % 2], small[b % 3]
            S.dma("sp", s_.t[:], xin[b * 128:(b + 1) * 128, :], reads=[r_xin], writes=[s_.r])
            for hf in range(2):
                for c in range(8):
                    S.op("pe", lambda e, hf=hf, c=c, bb=bb, o_=o_, p_=p_: e.matmul(p_.t[:, hf * 512:(hf + 1) * 512], lhsT=o_.t[:, c, bb * 128:(bb + 1) * 128],
                                                                            rhs=wout.t[:, c, hf * 512:(hf + 1) * 512], start=(c == 0), stop=(c == 7)),
                         reads=[o_.r, wout.r], writes=[p_.r])
            S.op("dve", lambda e, s_=s_, y_=y_, p_=p_: e.scalar_tensor_tensor(out=y_.t[:], in0=s_.t[:], scalar=ALPHA, in1=p_.t[:], op0=ALU.mult, op1=ALU.add),
                 reads=[s_.r, p_.r], writes=[y_.r])
            layernorm_block(S, y_, g_b, b_b, sm, x1_)
            S.dma("sp", sc["X1"][b * 128:(b + 1) * 128, :], x1_.t[:], reads=[x1_.r], writes=[sc["r_X1"]])
            if ROUTED:
                S.op("act", lambda e, b=b, x1_=x1_: e.activation(out=x1b.t[:, b, :], in_=x1_.t[:], func=AF.Copy), reads=[x1_.r], writes=[x1b.r])
            def stage_b(b=b, bb=bb, x1_=x1_, xf_=xf_, xb_=xb_):
                pt = ptr[0]
                for c in range(8):
                    S.op("pe", lambda e, c=c: e.transpose(out=pt.t[:, c * 128:(c + 1) * 128], in_=x1_.t[:, c * 128:(c + 1) * 128], identity=identf.t[:]),
                         reads=[x1_.r, identf.r], writes=[pt.r])
                S.op("act", lambda e: e.activation(out=xf_.t[:, :, :], in_=pt.t[:, :].rearrange("p (c t) -> p c t", c=8), func=AF.Copy),
                     reads=[pt.r], writes=[xf_.r])
                if not ROUTED:
                    S.op("dve", lambda e: e.tensor_copy(out=xb_.t[:, :, bb * 128:(bb + 1) * 128], in_=pt.t[:, :].rearrange("p (c t) -> p c t", c=8)),
                         reads=[pt.r], writes=[xb_.r])
                def stage_c():
                    pl = plg[b % 2]
                    for c in range(8):
                        S.op("pe", lambda e, c=c: e.matmul(pl.t[:, 0:20], lhsT=xf_.t[:, c, :], rhs=wr.t[:, c, :], start=(c == 0), stop=(c == 7)),
                             reads=[xf_.r, wr.r], writes=[pl.r])
                    S.op("dve", lambda e: e.tensor_tensor(out=lgall.t[:, b, :], in0=pl.t[:, 0:20], in1=brt.t[:], op=ALU.add), reads=[pl.r, brt.r], writes=[lgall.r])
                if int(_os_env.get("INL", "0")):
                    stage_c()
                else:
                    pend2.append(stage_c)
            pend.append(stage_b)
            if len(pend2) > int(_os_env.get("LAG2", "1")):
                pend2.pop(0)()
            if len(pend) > 3:
                pend.pop(0)()
            if (not ROUTED) and bb == 3:
                while pend:
                    pend.pop(0)()
                while pend2:
                    pend2.pop(0)()
        if not ROUTED:
            S.dma("sp", sc["X1T"][:, :, j * 512:(j + 1) * 512], xb_.t[:], reads=[xb_.r], writes=[sc["r_X1T"]])
    while pend:
        pend.pop(0)()
        while len(pend2) > 1:
            pend2.pop(0)()
    while pend2:
        pend2.pop(0)()
    def GT(name, shape):
        return Buf(A.sb("gv_" + name, shape, F32))
    B3 = [128, NB, 4]
    gl = lgall.t[:, :, 0:4]
    el = lgall.t[:, :, 4:20].rearrange("p b (g x) -> p b g x", g=4)
    m_, goh, tmp, se = GT("m", [128, NB]), (gohall if ROUTED else GT("goh", B3)), GT("tmp", B3), GT("se", [128, NB])
    t44, es, m1, oh1, es2, m2, oh2 = GT("t44", [128, NB, 4, 4]), GT("es", B3), GT("m1", [128, NB]), GT("oh1", B3), GT("es2", B3), GT("m2", [128, NB]), GT("oh2", B3)
    d_, p1, p2, gi = GT("d", [128, NB]), GT("p1", [128, NB]), GT("p2", [128, NB]), GT("gi", B3)

    def bc(t2):
        return t2.t[:, :].unsqueeze(2).to_broadcast(B3)

    def D(fn, reads, writes):
        S.op("dve", fn, reads=[x.r for x in reads], writes=[x.r for x in writes])
    D(lambda e: e.tensor_reduce(out=m_.t[:], in_=gl, axis=AX.X, op=ALU.max), [lgall], [m_])
    D(lambda e: e.tensor_tensor(out=goh.t[:], in0=gl, in1=bc(m_), op=ALU.is_equal), [lgall, m_], [goh])
    D(lambda e: e.tensor_tensor(out=tmp.t[:], in0=gl, in1=bc(m_), op=ALU.subtract), [lgall, m_], [tmp])
    S.op("act", lambda e: e.activation(out=tmp.t[:], in_=tmp.t[:], func=AF.Exp), reads=[tmp.r], writes=[tmp.r])
    D(lambda e: e.tensor_reduce(out=se.t[:], in_=tmp.t[:], axis=AX.X, op=ALU.add), [tmp], [se])
    D(lambda e: e.reciprocal(out=se.t[:], in_=se.t[:]), [se], [se])
    D(lambda e: e.tensor_tensor(out=t44.t[:], in0=el, in1=goh.t[:, :, :].unsqueeze(3).to_broadcast([128, NB, 4, 4]), op=ALU.mult), [lgall, goh], [t44])
    D(lambda e: e.tensor_reduce(out=es.t[:], in_=t44.t[:, :, :, :].rearrange("p b g x -> p b x g"), axis=AX.X, op=ALU.add), [t44], [es])
    D(lambda e: e.tensor_reduce(out=m1.t[:], in_=es.t[:], axis=AX.X, op=ALU.max), [es], [m1])
    D(lambda e: e.tensor_tensor(out=oh1.t[:], in0=es.t[:], in1=bc(m1), op=ALU.is_equal), [es, m1], [oh1])
    D(lambda e: e.scalar_tensor_tensor(out=es2.t[:], in0=oh1.t[:], scalar=-1e30, in1=es.t[:], op0=ALU.mult, op1=ALU.add), [oh1, es], [es2])
    D(lambda e: e.tensor_reduce(out=m2.t[:], in_=es2.t[:], axis=AX.X, op=ALU.max), [es2], [m2])
    D(lambda e: e.tensor_tensor(out=oh2.t[:], in0=es2.t[:], in1=bc(m2), op=ALU.is_equal), [es2, m2], [oh2])
    D(lambda e: e.tensor_tensor(out=d_.t[:], in0=m2.t[:], in1=m1.t[:], op=ALU.subtract), [m1, m2], [d_])
    S.op("act", lambda e: e.activation(out=d_.t[:], in_=d_.t[:], func=AF.Exp), reads=[d_.r], writes=[d_.r])
    D(lambda e: e.tensor_scalar(out=p1.t[:], in0=d_.t[:], scalar1=1.0, scalar2=None, op0=ALU.add), [d_], [p1])
    D(lambda e: e.reciprocal(out=p1.t[:], in_=p1.t[:]), [p1], [p1])
    D(lambda e: e.tensor_tensor(out=p2.t[:], in0=d_.t[:], in1=p1.t[:], op=ALU.mult), [d_, p1], [p2])
    D(lambda e: e.tensor_tensor(out=gi.t[:], in0=oh1.t[:], in1=bc(p1), op=ALU.mult), [oh1, p1], [gi])
    D(lambda e: e.tensor_tensor(out=oh2.t[:], in0=oh2.t[:], in1=bc(p2), op=ALU.mult), [oh2, p2], [oh2])
    D(lambda e: e.tensor_tensor(out=gi.t[:], in0=gi.t[:], in1=oh2.t[:], op=ALU.add), [gi, oh2], [gi])
    D(lambda e: e.tensor_tensor(out=gi.t[:], in0=gi.t[:], in1=bc(se), op=ALU.mult), [gi, se], [gi])
    D(lambda e: e.tensor_tensor(out=gate.t[:, :, :].rearrange("p b (g x) -> p b g x", g=4), in0=goh.t[:, :, :].unsqueeze(3).to_broadcast([128, NB, 4, 4]),
                                in1=gi.t[:, :, :].unsqueeze(2).to_broadcast([128, NB, 4, 4]), op=ALU.mult), [goh, gi], [gate])
    if not ROUTED:
        S.dma("sp", sc["GATE"], gate.t[:, :, :].rearrange("p b e -> p (b e)"), reads=[gate.r], writes=[sc["r_GATE"]])
    else:
        route_epilogue(S, A, sc, x1b, gate, gohall, plg, l)
    S.flush()
    A.close()


def route_epilogue(S, A, sc, x1b, gate, gohall, plg, l):
    def T_(name, shape, dt=F32):
        return Buf(A.sb(name, shape, dt))
    onesf = T_("onesf", [128, 128])
    tris = T_("tris", [128, 128])
    S.op("pool", lambda e: e.memset(onesf.t[:], 1.0), writes=[onesf.r])
    S.op("pool", lambda e: e.memset(tris.t[:], 1.0), writes=[tris.r])
    S.op("pool", lambda e: e.affine_select(out=tris.t[:], in_=tris.t[:], pattern=[[1, 128]], compare_op=ALU.is_ge, fill=0.0, base=-1,
                                           channel_multiplier=-1), reads=[tris.r], writes=[tris.r])
    pt, pr = plg[0], plg[1]
    for b in range(NB):
        S.op("pe", lambda e, b=b: e.matmul(pt.t[:, b * 4:(b + 1) * 4], lhsT=onesf.t[:], rhs=gohall.t[:, b, :], start=True, stop=True),
             reads=[onesf.r, gohall.r], writes=[pt.r])
        S.op("pe", lambda e, b=b: e.matmul(pr.t[:, b * 4:(b + 1) * 4], lhsT=tris.t[:], rhs=gohall.t[:, b, :], start=True, stop=True),
             reads=[tris.r, gohall.r], writes=[pr.r])
    totb = T_("totb", [128, NB, 4])
    cum = T_("cumb", [128, NB, 4])
    ones32 = T_("ones32", [128, NB])
    S.op("pool", lambda e: e.memset(ones32.t[:], 1.0), writes=[ones32.r])
    S.op("dve", lambda e: e.tensor_copy(out=totb.t[:, :, :], in_=pt.t[:, 0:NB * 4].rearrange("p (b g) -> p b g", g=4)), reads=[pt.r], writes=[totb.r])
    for g in range(4):
        S.op("dve", lambda e, g=g: e.tensor_tensor_scan(out=cum.t[:, :, g], data0=ones32.t[:, :], data1=totb.t[:, :, g], initial=0.0,
                                                        op0=ALU.mult, op1=ALU.add), reads=[ones32.r, totb.r, cum.r], writes=[cum.r])
    boffx = T_("boffx", [128, NB, 4])
    S.op("dve", lambda e: e.tensor_tensor(out=boffx.t[:], in0=cum.t[:], in1=totb.t[:], op=ALU.subtract), reads=[cum.r, totb.r], writes=[boffx.r])
    thr_i = T_("thri", [128, 16], I32)
    thr = T_("thr", [128, 16])
    S.op("pool", lambda e: e.iota(thr_i.t[:], pattern=[[512, 16]], base=0, channel_multiplier=0), writes=[thr_i.r])
    S.op("dve", lambda e: e.tensor_copy(out=thr.t[:], in_=thr_i.t[:]), reads=[thr_i.r], writes=[thr.r])
    cmp = T_("cmp", [128, 4, 8])
    ntl = T_("ntl", [128, 4])
    S.op("dve", lambda e: e.tensor_tensor(out=cmp.t[:], in0=cum.t[:, NB - 1, :].unsqueeze(2).to_broadcast([128, 4, 8]),
                                          in1=thr.t[:, 0:8].unsqueeze(1).to_broadcast([128, 4, 8]), op=ALU.is_gt), reads=[cum.r, thr.r], writes=[cmp.r])
    S.op("dve", lambda e: e.tensor_reduce(out=ntl.t[:], in_=cmp.t[:], axis=AX.X, op=ALU.add), reads=[cmp.r], writes=[ntl.r])
    S.op("dve", lambda e: e.tensor_scalar(out=ntl.t[:], in0=ntl.t[:], scalar1=512.0, scalar2=None, op0=ALU.mult), reads=[ntl.r], writes=[ntl.r])
    pst = T_("pst", [128, 4])
    pen = T_("pen", [128, 4])
    S.op("pool", lambda e: e.memset(pst.t[:], 0.0), writes=[pst.r])
    for g in range(1, 4):
        S.op("dve", lambda e, g=g: e.tensor_tensor(out=pst.t[:, g:g + 1], in0=pst.t[:, g - 1:g], in1=ntl.t[:, g - 1:g], op=ALU.add),
             reads=[pst.r, ntl.r], writes=[pst.r])
    S.op("dve", lambda e: e.tensor_tensor(out=pen.t[:], in0=pst.t[:], in1=ntl.t[:], op=ALU.add), reads=[pst.r, ntl.r], writes=[pen.r])
    v = T_("vdest", [128, NB, 4])
    S.op("dve", lambda e: e.tensor_tensor(out=v.t[:], in0=pr.t[:, 0:NB * 4].rearrange("p (b g) -> p b g", g=4), in1=boffx.t[:], op=ALU.add),
         reads=[pr.r, boffx.r], writes=[v.r])
    S.op("dve", lambda e: e.tensor_tensor(out=v.t[:], in0=v.t[:], in1=pst.t[:, :].unsqueeze(1).to_broadcast([128, NB, 4]), op=ALU.add),
         reads=[v.r, pst.r], writes=[v.r])
    S.op("dve", lambda e: e.tensor_tensor(out=v.t[:], in0=v.t[:], in1=gohall.t[:], op=ALU.mult), reads=[v.r, gohall.r], writes=[v.r])
    destf = T_("destf", [128, NB])
    desti = T_("desti", [128, NB], I32)
    S.op("dve", lambda e: e.tensor_reduce(out=destf.t[:], in_=v.t[:], axis=AX.X, op=ALU.add), reads=[v.r], writes=[destf.r])
    S.op("dve", lambda e: e.tensor_copy(out=desti.t[:], in_=destf.t[:]), reads=[destf.r], writes=[desti.r])
    S.dma("sp", sc["DEST"], desti.t[:], reads=[desti.r], writes=[sc["r_DEST"]])
    cmp2 = T_("cmp2", [128, NTILE, 4])
    gk = T_("gk", [128, NTILE])
    S.op("dve", lambda e: e.tensor_tensor(out=cmp2.t[:], in0=pen.t[:, :].unsqueeze(1).to_broadcast([128, NTILE, 4]),
                                          in1=thr.t[:, 0:NTILE].unsqueeze(2).to_broadcast([128, NTILE, 4]), op=ALU.is_le), reads=[pen.r, thr.r], writes=[cmp2.r])
    S.op("dve", lambda e: e.tensor_reduce(out=gk.t[:], in_=cmp2.t[:], axis=AX.X, op=ALU.add), reads=[cmp2.r], writes=[gk.r])
    S.op("dve", lambda e: e.tensor_scalar(out=gk.t[:], in0=gk.t[:], scalar1=3.0, scalar2=1024.0, op0=ALU.min, op1=ALU.mult), reads=[gk.r], writes=[gk.r])
    S.op("dve", lambda e: e.tensor_scalar(out=gk.t[:], in0=gk.t[:], scalar1=float(l * 4096), scalar2=None, op0=ALU.add), reads=[gk.r], writes=[gk.r])
    cw_i = T_("cwi", [128, 8], I32)
    cw = T_("cw", [128, 8])
    S.op("pool", lambda e: e.iota(cw_i.t[:], pattern=[[256, 4], [1, 2]], base=0, channel_multiplier=2), writes=[cw_i.r])
    S.op("dve", lambda e: e.tensor_copy(out=cw.t[:], in_=cw_i.t[:]), reads=[cw_i.r], writes=[cw.r])
    idxf = T_("idxf", [128, NTILE, 8])
    idxi = T_("idxi", [128, NTILE, 8], I32)
    S.op("dve", lambda e: e.tensor_tensor(out=idxf.t[:], in0=cw.t[:, :].unsqueeze(1).to_broadcast([128, NTILE, 8]),
                                          in1=gk.t[:, :].unsqueeze(2).to_broadcast([128, NTILE, 8]), op=ALU.add), reads=[cw.r, gk.r], writes=[idxf.r])
    S.op("dve", lambda e: e.tensor_copy(out=idxi.t[:], in_=idxf.t[:]), reads=[idxf.r], writes=[idxi.r])
    S.dma("sp", sc["IDXW"], idxi.t[:, :, :].rearrange("p k j -> p (k j)"), reads=[idxi.r], writes=[sc["r_IDXW"]])
    for b in range(NB):
        S.dma_fn("pool", lambda e, b=b: e.indirect_dma_start(out=sc["XS"][:, :], out_offset=bass.IndirectOffsetOnAxis(ap=desti.t[:, b:b + 1], axis=0),
                                                            in_=x1b.t[:, b, :], in_offset=None), reads=[desti.r, x1b.r], writes=[sc["r_XS"]])
        S.dma_fn("pool", lambda e, b=b: e.indirect_dma_start(out=sc["GS"][:, :], out_offset=bass.IndirectOffsetOnAxis(ap=desti.t[:, b:b + 1], axis=0),
                                                            in_=gate.t[:, b, :], in_offset=None), reads=[desti.r, gate.r], writes=[sc["r_GS"]])


def phase4(nc, S, T, l, sc, xout, r_xout, final, TG=1024):
    A = Alloc(nc)
    g_b = bcast_row(S, A, "ln2g", T["ln2_g"][l], DM)
    b_b = bcast_row(S, A, "ln2b", T["ln2_b"][l], DM)
    gate = Buf(A.sb("gate4", [128, NB, 16], F32))
    S.dma("sp", gate.t[:, :, :].rearrange("p b e -> p (b e)"), sc["GATE"], reads=[sc["r_GATE"]], writes=[gate.r])
    xT = Buf(A.sb("x1T", [128, 8, TG], BF16))
    accb = Buf(A.sb("accm", [128, TG // 128, DM], F32))
    w1 = rot(A, "sb", "w1", [128, 8, DEXP], BF16, 2)
    w3 = rot(A, "sb", "w3", [128, 8, DEXP], BF16, 2)
    w2 = rot(A, "sb", "w2", [128, 4, DM], BF16, 2)
    sa = rot(A, "sb", "sa", [128, 512], F32, 2)
    hT = rot(A, "sb", "hT", [128, 4, 512], BF16, 2)
    xs = rot(A, "sb", "xs4", [128, DM], F32, 2)
    yo = rot(A, "sb", "yo4", [128, DM], F32, 2)
    epsl = Buf(A.sb("epsl4", [128, 1], F32))
    S.op("pool", lambda e: e.memset(epsl.t[:], LN_EPS), writes=[epsl.r])
    small = [dict(st6=Buf(A.sb("st6b", [128, 2, 6], F32)), mv=Buf(A.sb("mvb", [128, 2], F32)), rs=Buf(A.sb("rsb", [128, 1], F32)), eps=epsl) for _ in range(2)]
    pa = rot(A, "ps", "pa", [128, 512], F32, 2)
    pb = rot(A, "ps", "pb", [128, 512], F32, 2)
    py = rot(A, "ps", "py", [128, 512], F32, 3)
    cnt = {"y": 0, "ab": 0, "w": 0}
    W1 = T["w1"][l]
    W3 = T["w3"][l]
    W2 = T["w2"][l]
    for gi in range(SEQ // TG):
        t0 = gi * TG
        S.dma("sp", xT.t[:], sc["X1T"][:, :, t0:t0 + TG], reads=[sc["r_X1T"]], writes=[xT.r])
        for ex in range(NEXP):
            i = cnt["w"]
            cnt["w"] += 1
            a1, a3, a2 = w1[i % 2], w3[i % 2], w2[i % 2]
            S.dma("pool", a1.t[:], W1[ex].rearrange("(c p) n -> p c n", p=128), writes=[a1.r])
            S.dma("pool", a3.t[:], W3[ex].rearrange("(c p) n -> p c n", p=128), writes=[a3.r])
            S.dma("pool", a2.t[:], W2[ex].rearrange("(c p) n -> p c n", p=128), writes=[a2.r])
            for tt in range(TG // 512):
                tc = slice(tt * 512, (tt + 1) * 512)
                h_ = hT[(ex * (TG // 512) + tt) % 2]
                for jc in range(4):
                    k_ = cnt["ab"]
                    cnt["ab"] += 1
                    pa_, pb_, sa_ = pa[k_ % 2], pb[k_ % 2], sa[k_ % 2]
                    for c in range(8):
                        S.op("pe", lambda e, c=c, jc=jc, pa_=pa_, a1=a1, tc=tc: e.matmul(pa_.t[:], lhsT=a1.t[:, c, jc * 128:(jc + 1) * 128], rhs=xT.t[:, c, tc],
                                                                                start=(c == 0), stop=(c == 7)), reads=[a1.r, xT.r], writes=[pa_.r])
                    for c in range(8):
                        S.op("pe", lambda e, c=c, jc=jc, pb_=pb_, a3=a3, tc=tc: e.matmul(pb_.t[:], lhsT=a3.t[:, c, jc * 128:(jc + 1) * 128], rhs=xT.t[:, c, tc],
                                                                                start=(c == 0), stop=(c == 7)), reads=[a3.r, xT.r], writes=[pb_.r])
                    S.op("act", lambda e, pa_=pa_, sa_=sa_: e.activation(out=sa_.t[:], in_=pa_.t[:], func=AF.Silu), reads=[pa_.r], writes=[sa_.r])
                    S.op("dve", lambda e, pb_=pb_, sa_=sa_, h_=h_, jc=jc: e.tensor_tensor(out=h_.t[:, jc, :], in0=pb_.t[:], in1=sa_.t[:], op=ALU.mult),
                         reads=[pb_.r, sa_.r], writes=[h_.r])
                for tb in range(4):
                    blk = tt * 4 + tb
                    gb = (t0 // 128) + blk
                    for hf in range(2):
                        y_ = py[cnt["y"] % 3]
                        cnt["y"] += 1
                        for jc in range(4):
                            S.op("pe", lambda e, jc=jc, tb=tb, hf=hf, y_=y_, h_=h_, a2=a2: e.matmul(y_.t[:], lhsT=h_.t[:, jc, tb * 128:(tb + 1) * 128],
                                                                                           rhs=a2.t[:, jc, hf * 512:(hf + 1) * 512], start=(jc == 0), stop=(jc == 3)),
                                 reads=[h_.r, a2.r], writes=[y_.r])
                        dst = accb.t[:, blk, hf * 512:(hf + 1) * 512]
                        if ex == 0:
                            S.op("dve", lambda e, y_=y_, dst=dst, gb=gb, ex=ex: e.tensor_scalar(out=dst, in0=y_.t[:], scalar1=gate.t[:, gb, ex:ex + 1], scalar2=None, op0=ALU.mult),
                                 reads=[y_.r, gate.r], writes=[accb.r])
                        else:
                            S.op("dve", lambda e, y_=y_, dst=dst, gb=gb, ex=ex: e.scalar_tensor_tensor(out=dst, in0=y_.t[:], scalar=gate.t[:, gb, ex:ex + 1], in1=dst,
                                                                                              op0=ALU.mult, op1=ALU.add), reads=[y_.r, gate.r, accb.r], writes=[accb.r])
        for blk in range(TG // 128):
            gb = (t0 // 128) + blk
            s_, y_, sm = xs[blk % 2], yo[blk % 2], small[blk % 2]
            S.dma("sp", s_.t[:], sc["X1"][gb * 128:(gb + 1) * 128, :], reads=[sc["r_X1"]], writes=[s_.r])
            S.op("dve", lambda e, s_=s_, blk=blk: e.scalar_tensor_tensor(out=s_.t[:], in0=s_.t[:], scalar=ALPHA, in1=accb.t[:, blk, :], op0=ALU.mult, op1=ALU.add),
                 reads=[s_.r, accb.r], writes=[s_.r])
            layernorm_block(S, s_, g_b, b_b, sm, y_)
            S.dma("sp", xout[gb * 128:(gb + 1) * 128, :], y_.t[:], reads=[y_.r], writes=[r_xout], final=final)
    S.flush()
    A.close()


def phase4r(nc, S, T, l, sc, xout, r_xout, final):
    A = Alloc(nc)
    ident = make_ident(A, S, BF16)
    g_b = bcast_row(S, A, "ln2g", T["ln2_g"][l], DM)
    b_b = bcast_row(S, A, "ln2b", T["ln2_b"][l], DM)
    dest = Buf(A.sb("dest4", [128, NB], I32))
    idxw = Buf(A.sb("idxw4", [128, NTILE * 8], I32))
    S.dma("sp", dest.t[:], sc["DEST"], reads=[sc["r_DEST"]], writes=[dest.r])
    S.dma("sp", idxw.t[:], sc["IDXW"], reads=[sc["r_IDXW"]], writes=[idxw.r])
    xs = rot(A, "sb", "xs4r", [128, 4, DM], BF16, 2)
    gs = rot(A, "sb", "gs4r", [128, 4, 16], F32, 2)
    gsel = rot(A, "sb", "gsel", [128, 4, 4], F32, 2)
    xT = rot(A, "sb", "xT4r", [128, 8, 512], BF16, 2)
    accs = rot(A, "sb", "acc4r", [128, 4, DM], F32, 2)
    w1 = rot(A, "sb", "w1r", [128, 8 * DEXP], BF16, 2)
    w3 = rot(A, "sb", "w3r", [128, 8 * DEXP], BF16, 2)
    w2 = rot(A, "sb", "w2r", [128, 4 * DM], BF16, 2)
    sa = rot(A, "sb", "sar", [128, 512], F32, 2)
    hT = rot(A, "sb", "hTr", [128, 4, 512], BF16, 2)
    mt = rot(A, "sb", "mt4", [128, DM], F32, 4)
    xo = rot(A, "sb", "xo4", [128, DM], F32, 4)
    yo = rot(A, "sb", "yo4r", [128, DM], F32, 4)
    epsl = Buf(A.sb("epsl4r", [128, 1], F32))
    S.op("pool", lambda e: e.memset(epsl.t[:], LN_EPS), writes=[epsl.r])
    small = [dict(st6=Buf(A.sb("st6r", [128, 2, 6], F32)), mv=Buf(A.sb("mvr", [128, 2], F32)), rs=Buf(A.sb("rsr", [128, 1], F32)), eps=epsl) for _ in range(4)]
    ptp = rot(A, "ps", "ptp", [128, DM], BF16, 1)
    pa = rot(A, "ps", "par", [128, 512], F32, 2)
    pb = rot(A, "ps", "pbr", [128, 512], F32, 2)
    py = rot(A, "ps", "pyr", [128, 512], F32, 3)
    Wv = [sc["WB"][i][:, :] for i in range(3)]
    cnt = {"y": 0, "ab": 0}
    steps = [(k, j) for k in range(NTILE) for j in range(4)]
    st = {}

    def front(si):
        k, j = steps[si]
        if j == 0:
            x_, g_, gl_, xT_, ac_ = xs[k % 2], gs[k % 2], gsel[k % 2], xT[k % 2], accs[k % 2]
            S.dma("sp", x_.t[:], sc["XS"][k * 512:(k + 1) * 512, :].rearrange("(b p) n -> p b n", p=128), reads=[sc["r_XS"]], writes=[x_.r])
            S.dma("sp", g_.t[:], sc["GS"][k * 512:(k + 1) * 512, :].rearrange("(b p) n -> p b n", p=128), reads=[sc["r_GS"]], writes=[g_.r])
            S.op("dve", lambda e: e.tensor_reduce(out=gl_.t[:], in_=g_.t[:, :, :].rearrange("p b (g j) -> p b j g", g=4), axis=AX.X, op=ALU.add),
                 reads=[g_.r], writes=[gl_.r])
            for blk in range(4):
                p = ptp[0]
                for c in range(8):
                    S.op("pe", lambda e, c=c, blk=blk: e.transpose(out=p.t[:, c * 128:(c + 1) * 128],
                                                                   in_=x_.t[:, blk, :].rearrange("p (pp c) -> p c pp", c=8)[:, c, :], identity=ident.t[:]),
                         reads=[x_.r, ident.r], writes=[p.r])
                S.op("act", lambda e, blk=blk: e.activation(out=xT_.t[:, :, blk * 128:(blk + 1) * 128], in_=p.t[:, :].rearrange("p (c t) -> p c t", c=8), func=AF.Copy),
                     reads=[p.r], writes=[xT_.r])
        xT_, ac_, gl_ = xT[k % 2], accs[k % 2], gsel[k % 2]
        a1, a3, a2, h_ = w1[si % 2], w3[si % 2], w2[si % 2], hT[si % 2]
        for wt_, src in ((a1, Wv[0]), (a3, Wv[1]), (a2, Wv[2])):
            for half in range(2):
                col = k * 8 + j * 2 + half
                S.dma_fn("pool", lambda e, wt_=wt_, src=src, half=half, col=col: e.indirect_dma_start(
                    out=wt_.t[:, half * 2048:(half + 1) * 2048], out_offset=None, in_=src,
                    in_offset=bass.IndirectOffsetOnAxis(ap=idxw.t[:, col:col + 1], axis=0)), reads=[idxw.r, sc["r_WB"][l]], writes=[wt_.r])
        w1v = a1.t[:, :].rearrange("p (c pp q) -> p c q pp", c=8, q=4)
        w3v = a3.t[:, :].rearrange("p (c pp q) -> p c q pp", c=8, q=4)
        for jc in range(4):
            k_ = cnt["ab"]
            cnt["ab"] += 1
            pa_, pb_, sa_ = pa[k_ % 2], pb[k_ % 2], sa[k_ % 2]
            for c in range(8):
                S.op("pe", lambda e, c=c, jc=jc, pa_=pa_: e.matmul(pa_.t[:], lhsT=w1v[:, c, jc, :], rhs=xT_.t[:, c, :], start=(c == 0), stop=(c == 7)),
                     reads=[a1.r, xT_.r], writes=[pa_.r])
            for c in range(8):
                S.op("pe", lambda e, c=c, jc=jc, pb_=pb_: e.matmul(pb_.t[:], lhsT=w3v[:, c, jc, :], rhs=xT_.t[:, c, :], start=(c == 0), stop=(c == 7)),
                     reads=[a3.r, xT_.r], writes=[pb_.r])
            S.op("act", lambda e, pa_=pa_, sa_=sa_: e.activation(out=sa_.t[:], in_=pa_.t[:], func=AF.Silu), reads=[pa_.r], writes=[sa_.r])
            S.op("dve", lambda e, pb_=pb_, sa_=sa_, jc=jc: e.tensor_tensor(out=h_.t[:, jc, :], in0=pb_.t[:], in1=sa_.t[:], op=ALU.mult),
                 reads=[pb_.r, sa_.r], writes=[h_.r])

    def back(si):
        k, j = steps[si]
        ac_, gl_, a2, h_ = accs[k % 2], gsel[k % 2], w2[si % 2], hT[si % 2]
        w2v = a2.t[:, :].rearrange("p (c n) -> p c n", c=4)
        for blk in range(4):
            for hf in range(2):
                y_ = py[cnt["y"] % 3]
                cnt["y"] += 1
                for jc in range(4):
                    S.op("pe", lambda e, jc=jc, blk=blk, hf=hf, y_=y_: e.matmul(y_.t[:], lhsT=h_.t[:, jc, blk * 128:(blk + 1) * 128],
                                                                               rhs=w2v[:, jc, hf * 512:(hf + 1) * 512], start=(jc == 0), stop=(jc == 3)),
                         reads=[h_.r, a2.r], writes=[y_.r])
                dst = ac_.t[:, blk, hf * 512:(hf + 1) * 512]
                if j == 0:
                    S.op("dve", lambda e, y_=y_, dst=dst, blk=blk: e.tensor_scalar(out=dst, in0=y_.t[:], scalar1=gl_.t[:, blk, j:j + 1], scalar2=None, op0=ALU.mult),
                         reads=[y_.r, gl_.r], writes=[ac_.r])
                else:
                    S.op("dve", lambda e, y_=y_, dst=dst, blk=blk: e.scalar_tensor_tensor(out=dst, in0=y_.t[:], scalar=gl_.t[:, blk, j:j + 1], in1=dst,
                                                                                         op0=ALU.mult, op1=ALU.add), reads=[y_.r, gl_.r, ac_.r], writes=[ac_.r])
        if j == 3:
            S.dma("sp", sc["YS"][k * 512:(k + 1) * 512, :].rearrange("(b p) n -> p b n", p=128), ac_.t[:], reads=[ac_.r], writes=[sc["r_YS"]])

    for si in range(len(steps) + 1):
        if si < len(steps):
            front(si)
        if si >= 1:
            back(si - 1)
    def comb_load(b):
        m_, x_ = mt[b % 4], xo[b % 4]
        S.dma_fn("pool", lambda e: e.indirect_dma_start(out=m_.t[:], out_offset=None, in_=sc["YS"][:, :],
                                                        in_offset=bass.IndirectOffsetOnAxis(ap=dest.t[:, b:b + 1], axis=0)),
                 reads=[dest.r, sc["r_YS"]], writes=[m_.r])
        S.dma("sp", x_.t[:], sc["X1"][b * 128:(b + 1) * 128, :], reads=[sc["r_X1"]], writes=[x_.r])
    for b in range(min(3, NB)):
        comb_load(b)
    for b in range(NB):
        m_, x_, y_, sm = mt[b % 4], xo[b % 4], yo[b % 4], small[b % 4]
        S.op("dve", lambda e, m_=m_, x_=x_: e.scalar_tensor_tensor(out=x_.t[:], in0=x_.t[:], scalar=ALPHA, in1=m_.t[:], op0=ALU.mult, op1=ALU.add),
             reads=[x_.r, m_.r], writes=[x_.r])
        layernorm_block(S, x_, g_b, b_b, sm, y_)
        if b + 3 < NB:
            comb_load(b + 3)
        S.dma("sp", xout[b * 128:(b + 1) * 128, :], y_.t[:], reads=[y_.r], writes=[r_xout], final=final)
    S.flush()
    A.close()


def rope_tables():
    pos = np.arange(SEQ, dtype=np.float32)
    out = {}
    for dim, nm in ((64, "rope64"), (32, "rope32")):
        half = dim // 2
        inv = (10000.0 ** (-np.arange(half, dtype=np.float32) / half)).astype(np.float32)
        ang = pos[None, :] * inv[:, None]
        c = np.cos(ang).astype(np.float32)
        s = np.sin(ang).astype(np.float32)
        out[nm + "c"] = np.ascontiguousarray(np.concatenate([c, c], 0))
        out[nm + "s"] = np.ascontiguousarray(np.concatenate([-s, s], 0))
    return out


W_SPECS = [("w_in", [DEPTH, DM, N_IN]), ("b_forget", [DEPTH, 4]), ("g_cq", [DEPTH, 256]), ("g_ckv", [DEPTH, 128]), ("w_uq", [DEPTH, 256, 384]),
           ("w_ukv", [DEPTH, 128, 512]), ("g_head", [DEPTH, 16, 64]), ("w_out", [DEPTH, DM, DM]), ("ln1_g", [DEPTH, DM]), ("ln1_b", [DEPTH, DM]),
           ("w_group", [DEPTH, DM, 4]), ("b_group", [DEPTH, 4]), ("w_expert", [DEPTH, DM, 16]), ("b_expert", [DEPTH, 16]),
           ("w1", [DEPTH, NEXP, DM, DEXP]), ("w3", [DEPTH, NEXP, DM, DEXP]), ("w2", [DEPTH, NEXP, DEXP, DM]), ("ln2_g", [DEPTH, DM]), ("ln2_b", [DEPTH, DM])]


def build_program(nseq=2, layers=(0, 1), phases=(1, 2, 3, 4), debug=False, heads=None, TG=1024):
    nc = bass.Bass("TRN2", target_bir_lowering=False)
    T = {}
    T["x"] = nc.dram_tensor("x", [nseq, SEQ, DM], F32, kind="ExternalInput").ap()
    for nm, shp in W_SPECS:
        T[nm] = nc.dram_tensor(nm, shp, F32, kind="ExternalInput").ap()
    for nm, rows in (("rope64c", 64), ("rope64s", 64), ("rope32c", 32), ("rope32s", 32)):
        T[nm] = nc.dram_tensor(nm, [rows, SEQ], F32, kind="ExternalInput").ap()
    out = nc.dram_tensor("out", [nseq, SEQ, DM], F32, kind="ExternalOutput").ap()
    dk = "ExternalOutput" if debug else "Internal"

    def scratch(name, shape, dt):
        return nc.dram_tensor(name, shape, dt, kind=dk).ap()
    sc = {}
    qs = scratch("QS", [12, 96, SEQ], BF16)
    ks = scratch("KS", [12, 96, SEQ], BF16)
    sc["QS"] = [qs[h] for h in range(12)]
    sc["KS"] = [ks[h] for h in range(12)]
    vs = scratch("VS", [6, 128, NB * 2 * 65], BF16)
    sc["VS"] = [vs[p] for p in range(6)]
    qd = scratch("QD", [3, 4, 64, SEQ], BF16)
    kd = scratch("KD", [3, 4, 64, SEQ], BF16)
    sc["QD"] = [[qd[g, h] for h in range(4)] for g in range(3)]
    sc["KD"] = [[kd[g, h] for h in range(4)] for g in range(3)]
    vd = scratch("VD", [3, 2, 128, NB * 2 * 65], BF16)
    sc["VD"] = [[vd[g, p] for p in range(2)] for g in range(3)]
    sc["OnT"] = scratch("OnT", [8, 128, SEQ], BF16)
    sc["X1"] = scratch("X1", [SEQ, DM], F32)
    sc["X1T"] = scratch("X1T", [128, 8, SEQ], BF16)
    sc["GATE"] = scratch("GATE", [128, NB * 16], F32)
    sc["XS"] = scratch("XS", [NSLOT, DM], BF16)
    sc["GS"] = scratch("GS", [NSLOT, 16], F32)
    sc["YS"] = scratch("YS", [NSLOT, DM], F32)
    sc["DEST"] = scratch("DEST", [128, NB], I32)
    sc["IDXW"] = scratch("IDXW", [128, NTILE * 8], I32)
    sc["WB"] = [scratch("WB%d" % i, [DEPTH * 4096, 2048], BF16) for i in range(3)]
    sc["r_WB"] = [Res() for _ in range(DEPTH)]
    xmid = scratch("XMID", [SEQ, DM], F32)
    sc["r_QS"] = [Res() for _ in range(12)]
    sc["r_KS"] = [Res() for _ in range(12)]
    sc["r_VS"] = [Res() for _ in range(6)]
    sc["r_QD"] = [[Res() for _ in range(4)] for _ in range(3)]
    sc["r_KD"] = [[Res() for _ in range(4)] for _ in range(3)]
    sc["r_VD"] = [[Res() for _ in range(2)] for _ in range(3)]
    for k in ("OnT", "X1", "X1T", "GATE", "XS", "GS", "YS", "DEST", "IDXW"):
        sc["r_" + k] = Res()
    r_xmid = Res()
    r_x = Res()
    r_out = Res()
    S = Sched(nc)
    for s in range(nseq):
        for li, l in enumerate(layers):
            xin, r_xin = (T["x"][s], r_x) if li == 0 else (xmid, r_xmid)
            last = li == len(layers) - 1
            xo, r_xo = (out[s], r_out) if last else (xmid, r_xmid)
            if 1 in phases:
                phase1(nc, S, T, xin, r_xin, l, sc)
            if 2 in phases:
                cv = (lambda l=l: convert_expert_weights(S, T, sc, l)) if (ROUTED and s == 0) else None
                phase2(nc, S, T, l, sc, heads=heads, after_sb=cv)
            if 3 in phases:
                phase3(nc, S, T, xin, r_xin, l, sc)
            if 4 in phases:
                if ROUTED:
                    phase4r(nc, S, T, l, sc, xo, r_xo, final=last)
                else:
                    phase4(nc, S, T, l, sc, xo, r_xo, final=last, TG=TG)
    S.close()
    return nc, S


def convert_expert_weights(S, T, sc, l):
    for i, nm in enumerate(("w1", "w3", "w2")):
        src = T[nm][l].rearrange("e k n -> (e k n)").rearrange("(r x) -> r x", x=2048)
        for ch in range(8):
            S.dma("pool", sc["WB"][i][l * 4096 + ch * 512:l * 4096 + (ch + 1) * 512, :], src[ch * 512:(ch + 1) * 512, :], writes=[sc["r_WB"][l]])


_CACHE = {}


def kernel(**inputs):
    n = 8
    nseq = 2
    x = np.ascontiguousarray(np.asarray(inputs["x"], dtype=np.float32))
    tabs = rope_tables()
    if "nc" not in _CACHE:
        _CACHE["nc"] = build_program(nseq=nseq)[0]
    nc = _CACHE["nc"]
    base = {nm: np.ascontiguousarray(np.asarray(inputs[nm], dtype=np.float32)) for nm, _ in W_SPECS}
    base.update(tabs)
    in_maps = []
    for c in range(n):
        m = dict(base)
        m["x"] = x[c * nseq:(c + 1) * nseq]
        in_maps.append(m)
    res = run_bass_kernel_spmd(nc, in_maps, core_ids=list(range(n)))
    return np.concatenate([r["out"] for r in res.results], axis=0).astype(np.float32)
```

```python
import numpy as np
from os import environ as _os_env
import concourse.bass as bass
import concourse.mybir as mybir
from concourse.bass_utils import run_bass_kernel_spmd

F32 = mybir.dt.float32
BF16 = mybir.dt.bfloat16
I32 = mybir.dt.int32
AF = mybir.ActivationFunctionType
ALU = mybir.AluOpType
AX = mybir.AxisListType

SAME_ENGINE_SYNC = bool(int(_os_env.get("SES", "1")))
NDMA_SLOTS = int(_os_env.get("NSLOTS", "8"))

SEQ = 4096
DM = 1024
NB = SEQ // 128
NT = SEQ // 512
DEPTH = 2
ALPHA = (2.0 * DEPTH) ** 0.25
LN_EPS = 1e-5
RMS_EPS = 1e-6
N_IN = 4260
C_SB, C_FOX, C_MLA, C_DIL = 0, 768, 1540, 1956
MLA_SCALE = 96.0 ** -0.5
DIL_R = (1, 4, 16)
NEXP = 16
DEXP = 512
NTILE = 11
NSLOT = NTILE * 512
ROUTED = bool(int(_os_env.get("ROUTED", "1")))


class Res:
    __slots__ = ("w", "r", "excl")

    def __init__(self, excl=False):
        self.w = None
        self.r = []
        self.excl = excl


class Sched:
    ENG = ("pe", "act", "dve", "pool", "sp")

    def __init__(self, nc):
        self.nc = nc
        self.ops = {e: [] for e in self.ENG}
        self.cnt = {e: 0 for e in self.ENG}
        self.seen = {e: {} for e in self.ENG}
        self.sems = {}
        self.dma_slots = {}
        self.dma_rr = {}
        self.final_waits = []
        self._stack = []
        self.nops = 0

    def sem(self, key):
        if key not in self.sems:
            cm = self.nc.semaphore("s_" + "_".join(str(k) for k in (key if isinstance(key, tuple) else (key,))))
            s = cm.__enter__()
            self._stack.append(cm)
            self.sems[key] = s
        return self.sems[key]

    def _deps(self, eng, reads, writes):
        deps = {}

        def add(t):
            if t is None:
                return
            k, v = t
            if deps.get(k, 0) < v:
                deps[k] = v
        for r in reads:
            add(r.w)
            if r.excl:
                for t in r.r:
                    if t[0] != eng:
                        add(t)
        for w in writes:
            add(w.w)
            for t in w.r:
                add(t)
        waits = []
        seen = self.seen[eng]
        for k, v in deps.items():
            if k == eng and (eng == "pe" or not SAME_ENGINE_SYNC):
                continue
            if seen.get(k, 0) >= v:
                continue
            seen[k] = v
            waits.append((k, v))
        return waits

    def _commit(self, ticket, reads, writes):
        for r in reads:
            if len(r.r) > 16:
                m = {}
                for k, v in r.r:
                    if m.get(k, 0) < v:
                        m[k] = v
                r.r = list(m.items())
            r.r.append(ticket)
        for w in writes:
            w.w = ticket
            w.r = []

    def op(self, eng, fn, reads=(), writes=()):
        waits = self._deps(eng, reads, writes)
        self.cnt[eng] += 1
        ticket = (eng, self.cnt[eng])
        self.ops[eng].append((waits, fn, (eng, 1)))
        self._commit(ticket, reads, writes)
        self.nops += 1
        return ticket

    def dma(self, q, out, in_, reads=(), writes=(), final=False, **kw):
        fn = lambda e, out=out, in_=in_, kw=kw: e.dma_start(out=out, in_=in_, **kw)
        return self.dma_fn(q, fn, reads, writes, final)

    def dma_fn(self, q, fn, reads=(), writes=(), final=False):
        waits = self._deps(q, reads, writes)
        if q not in self.dma_slots:
            self.dma_slots[q] = [[("d", q, i), 0] for i in range(NDMA_SLOTS)]
            self.dma_rr[q] = 0
        i = self.dma_rr[q]
        self.dma_rr[q] = (i + 1) % NDMA_SLOTS
        slot = self.dma_slots[q][i]
        key, tot = slot
        if tot > 0 and self.seen[q].get(key, 0) < tot:
            self.seen[q][key] = tot
            waits.append((key, tot))
        slot[1] = tot + 16
        ticket = (key, tot + 16)
        self.ops[q].append((waits, fn, (key, 16)))
        self._commit(ticket, reads, writes)
        self.nops += 1
        if final:
            self.final_waits.append(ticket)
        return ticket

    def flush(self):
        totals = [(e, self.cnt[e]) for e in self.ENG if self.cnt[e] > 0]
        for q, slots in self.dma_slots.items():
            for key, tot in slots:
                if tot > 0:
                    totals.append((key, tot))
        for e in self.ENG:
            self.sem(e)
            for waits, fn, inc in self.ops[e]:
                for k, v in waits:
                    self.sem(k)
                self.sem(inc[0])
        ops = self.ops
        self.ops = {e: [] for e in self.ENG}
        for e in self.ENG:
            for k, v in totals:
                self.seen[e][k] = max(self.seen[e].get(k, 0), v)
        with self.nc.Block() as block:
            def run(engname):
                def body(e):
                    for waits, fn, inc in ops[engname]:
                        for k, v in waits:
                            e.wait_ge(self.sems[k], v)
                        fn(e).then_inc(self.sems[inc[0]], inc[1])
                    for k, v in totals:
                        e.wait_ge(self.sems[k], v)
                return body
            block.sync(run("sp"))
            block.tensor(run("pe"))
            block.scalar(run("act"))
            block.vector(run("dve"))
            block.gpsimd(run("pool"))

    def close(self):
        for cm in reversed(self._stack):
            cm.__exit__(None, None, None)
        self._stack = []


_UID = [0]


class Alloc:
    def __init__(self, nc):
        self.nc = nc
        self.stack = []

    @property
    def n(self):
        _UID[0] += 1
        return _UID[0]

    def sb(self, name, shape, dt):
        cm = self.nc.sbuf_tensor("%s_%d" % (name, self.n), list(shape), dt)
        t = cm.__enter__()
        self.stack.append(cm)
        return t

    def ps(self, name, shape, dt):
        cm = self.nc.psum_tensor("%s_%d" % (name, self.n), list(shape), dt)
        t = cm.__enter__()
        self.stack.append(cm)
        return t

    def close(self):
        for cm in reversed(self.stack):
            cm.__exit__(None, None, None)
        self.stack = []


class Buf:
    def __init__(self, t, excl=False):
        self.t = t
        self.r = Res(excl)


def rot(A, kind, name, shape, dt, n):
    f = A.sb if kind == "sb" else A.ps
    return [Buf(f(name + str(i), shape, dt), excl=(kind == "ps")) for i in range(n)]


def make_ident(A, S, dt):
    b = Buf(A.sb("ident", [128, 128], dt))
    S.op("pool", lambda e: e.memset(b.t[:], 1.0), writes=[b.r])
    S.op("pool", lambda e: e.affine_select(out=b.t[:], in_=b.t[:], pattern=[[-1, 128]], compare_op=ALU.is_equal,
                                           fill=0.0, base=0, channel_multiplier=1), reads=[b.r], writes=[b.r])
    return b


def perm_view(ap2d, r, t0, n):
    if r == 1:
        return ap2d[:, t0:t0 + n]
    sc = SEQ // r
    v = ap2d.rearrange("p (i c) -> p c i", c=r)
    c0, i0 = t0 // sc, t0 % sc
    if i0 + n <= sc:
        return v[:, c0, i0:i0 + n]
    assert i0 == 0 and n % sc == 0
    return v[:, c0:c0 + n // sc, :]


import os as _os
_P1SEC = _os.environ.get("P1SEC", "abcd")
_MSUB = _os.environ.get("MSUB", "qkrvptc")
_MC = _os.environ.get("MC", "1234")


def phase1(nc, S, T, xin, r_xin, l, sc):
    A = Alloc(nc)
    ident = make_ident(A, S, BF16)
    xT = Buf(A.sb("xT", [128, 8, SEQ], BF16))
    xs = rot(A, "sb", "xs", [128, DM], F32, 4)
    xb = rot(A, "sb", "xb", [128, DM], BF16, 3)
    pst = rot(A, "ps", "pst", [128, DM], BF16, 2)
    pj = rot(A, "ps", "pj", [128, 512], F32, 4)
    pv = rot(A, "ps", "pv", [128, 512], F32, 2)
    Win = T["w_in"][l].rearrange("(c p) n -> p c n", p=128)

    for b in range(NB):
        s, c_, p = xs[b % 4], xb[b % 3], pst[b % 2]
        S.dma("sp", s.t[:], xin[b * 128:(b + 1) * 128, :], reads=[r_xin], writes=[s.r])
        S.op("act", lambda e, s=s, c_=c_: e.activation(out=c_.t[:], in_=s.t[:], func=AF.Copy), reads=[s.r], writes=[c_.r])
        for c in range(8):
            S.op("pe", lambda e, c=c, c_=c_, p=p: e.transpose(out=p.t[:, c * 128:(c + 1) * 128], in_=c_.t[:, c * 128:(c + 1) * 128],
                                                              identity=ident.t[:]), reads=[c_.r, ident.r], writes=[p.r])
        S.op("dve", lambda e, b=b, p=p: e.tensor_copy(out=xT.t[:, :, b * 128:(b + 1) * 128],
                                                      in_=p.t[:, :].rearrange("p (c t) -> p c t", c=8)), reads=[p.r], writes=[xT.r])

    wts = rot(A, "sb", "wt", [128, 8, 416], BF16, 2)
    wsw = rot(A, "sb", "wsw", [128, 8, 256], BF16, 2)
    stg = rot(A, "sb", "stg", [128, 512], BF16, 4)
    vst = rot(A, "sb", "vst", [128, NB, 2, 65], BF16, 2)
    tmp1 = rot(A, "sb", "tmp1", [128, 512], F32, 2)
    tmp2 = rot(A, "sb", "tmp2", [128, 512], F32, 2)
    CT = Buf(A.sb("ropeC", [128, SEQ], BF16))
    ST = Buf(A.sb("ropeS", [128, SEQ], BF16))
    for v in vst:
        S.op("pool", lambda e, v=v: e.memset(v.t[:], 1.0), writes=[v.r])
    state = {"w": 0, "pj": 0, "stg": 0, "v": 0, "pv": 0}

    def load_w(col_ranges):
        w = wts[state["w"] % 2]
        state["w"] += 1
        o = 0
        for (c0, n) in col_ranges:
            S.dma("pool", w.t[:, :, o:o + n], Win[:, :, c0:c0 + n], writes=[w.r])
            o += n
        return w

    def nxt(key, lst):
        b = lst[state[key] % len(lst)]
        state[key] += 1
        return b

    def proj_fm(w, o, M, r, j, wtile=None):
        p = nxt("pj", pj)
        wt_ = w if wtile is None else wtile
        for c in range(8):
            S.op("pe", lambda e, c=c, p=p, wt_=wt_: e.matmul(p.t[0:M, :], lhsT=wt_.t[:, c, o:o + M],
                                                             rhs=perm_view(xT.t[:, c, :], r, j * 512, 512),
                                                             start=(c == 0), stop=(c == 7)),
                 reads=[wt_.r, xT.r], writes=[p.r])
        return p

    def store_rows(st, rows, dst, r_dst, j):
        S.dma("sp", dst[:, j * 512:(j + 1) * 512], st.t[rows[0]:rows[1], :], reads=[st.r], writes=[r_dst])

    def v_proj(w, o, r, vt, ncol=128):
        for b4 in range(NB // 4):
            p = nxt("pv", pv)
            for bb in range(4):
                b = b4 * 4 + bb
                for c in range(8):
                    S.op("pe", lambda e, c=c, b=b, bb=bb, p=p: e.matmul(p.t[:, bb * 128:(bb + 1) * 128],
                                                                      lhsT=perm_view(xT.t[:, c, :], r, b * 128, 128),
                                                                      rhs=w.t[:, c, o:o + ncol], start=(c == 0), stop=(c == 7)),
                         reads=[w.r, xT.r], writes=[p.r])
            S.op("dve", lambda e, b4=b4, p=p: e.tensor_copy(
                out=vt.t[:, b4 * 4:(b4 + 1) * 4, :, 0:64],
                in_=p.t[:, :].rearrange("p (b h d) -> p b h d", b=4, h=2)), reads=[p.r], writes=[vt.r])

    def scale_q(w):
        S.op("dve", lambda e: e.tensor_scalar(out=w.t[:, :, 0:128], in0=w.t[:, :, 0:128], scalar1=0.125, scalar2=None,
                                              op0=ALU.mult), reads=[w.r], writes=[w.r])

    for kind, cbase, hbase, pbase in (("sb", C_SB, 0, 0), ("fox", C_FOX, 4, 2)):
        for hp in range(2):
            w = load_w([(cbase + hp * 128, 128), (cbase + 256 + hp * 128, 128), (cbase + 512 + hp * 128, 128)])
            scale_q(w)
            for qk, dst, rd in ((0, sc["QS"], sc["r_QS"]), (1, sc["KS"], sc["r_KS"])):
                for j in range(NT):
                    p = proj_fm(w, qk * 128, 128, 1, j)
                    st = nxt("stg", stg)
                    S.op("act", lambda e, p=p, st=st: e.activation(out=st.t[:], in_=p.t[:], func=AF.Copy), reads=[p.r], writes=[st.r])
                    for hh in range(2):
                        h = hbase + hp * 2 + hh
                        store_rows(st, (hh * 64, hh * 64 + 64), dst[h][0:64, :], rd[h], j)
            vt = nxt("v", vst)
            v_proj(w, 256, 1, vt)
            S.dma("sp", sc["VS"][pbase + hp], vt.t[:, :, :, :].rearrange("p b h d -> p (b h d)"), reads=[vt.r], writes=[sc["r_VS"][pbase + hp]])

    S.flush()
    if "b" not in _P1SEC:
        A.close()
        return
    A2 = Alloc(nc)
    wf = Buf(A2.sb("wf", [128, 8, 4], BF16))
    S.dma("pool", wf.t[:], Win[:, :, C_FOX + 768:C_FOX + 772], writes=[wf.r])
    bfg = Buf(A2.sb("bfg", [4, 1], F32))
    S.dma("sp", bfg.t[:], T["b_forget"][l].rearrange("(h o) -> h o", o=1), writes=[bfg.r])
    S.op("dve", lambda e: e.tensor_scalar(out=bfg.t[:], in0=bfg.t[:], scalar1=-1.0, scalar2=None, op0=ALU.mult), reads=[bfg.r], writes=[bfg.r])
    nlf = Buf(A2.sb("nlf", [4, SEQ], F32))
    ncum = Buf(A2.sb("ncum", [4, SEQ], F32))
    ones4 = Buf(A2.sb("ones4", [4, 512], F32))
    S.op("pool", lambda e: e.memset(ones4.t[:], 1.0), writes=[ones4.r])
    for j in range(NT):
        p = proj_fm(wf, 0, 4, 1, j)
        S.op("act", lambda e, p=p, j=j: e.activation(out=nlf.t[:, j * 512:(j + 1) * 512], in_=p.t[0:4, :], func=AF.Exp,
                                                     bias=bfg.t[:, 0:1], scale=-1.0), reads=[p.r, bfg.r], writes=[nlf.r])
    S.op("act", lambda e: e.activation(out=nlf.t[:], in_=nlf.t[:], func=AF.Ln, bias=1.0), reads=[nlf.r], writes=[nlf.r])
    for j in range(NT):
        sl = slice(j * 512, (j + 1) * 512)
        init = 0.0 if j == 0 else ncum.t[:, j * 512 - 1:j * 512]
        S.op("dve", lambda e, sl=sl, init=init: e.tensor_tensor_scan(out=ncum.t[:, sl], data0=ones4.t[:, :], data1=nlf.t[:, sl],
                                                                     initial=init, op0=ALU.mult, op1=ALU.add),
             reads=[ones4.r, nlf.r, ncum.r], writes=[ncum.r])
    class _Alias:
        def __init__(self, t, r):
            self.t, self.r = t, r
    nlf_b = nlf.t[:, :].bitcast(BF16)
    parts = [_Alias(nlf_b[:, 0:SEQ], nlf.r), _Alias(nlf_b[:, SEQ:2 * SEQ], nlf.r), Buf(A2.sb("cpart2", [4, SEQ], BF16))]
    for i in range(3):
        S.op("dve", lambda e, i=i: e.tensor_copy(out=parts[i].t[:], in_=ncum.t[:]), reads=[ncum.r], writes=[parts[i].r])
        if i < 2:
            S.op("dve", lambda e, i=i: e.tensor_tensor(out=ncum.t[:], in0=ncum.t[:], in1=parts[i].t[:], op=ALU.subtract),
                 reads=[ncum.r, parts[i].r], writes=[ncum.r])
    ones3 = Buf(A2.sb("ones3", [35, SEQ], BF16))
    S.op("pool", lambda e: e.memset(ones3.t[0:3, :], 1.0), writes=[ones3.r])
    S.op("pool", lambda e: e.memset(ones3.t[32:35, :], -1.0), writes=[ones3.r])
    for h in range(4):
        H = 4 + h
        S.dma("sp", sc["KS"][H][64:67, :], ones3.t[32:35, :], reads=[ones3.r], writes=[sc["r_KS"][H]])
        S.dma("sp", sc["QS"][H][67:70, :], ones3.t[0:3, :], reads=[ones3.r], writes=[sc["r_QS"][H]])
        for i in range(3):
            S.dma("sp", sc["KS"][H][67 + i:68 + i, :], parts[i].t[h:h + 1, :], reads=[parts[i].r], writes=[sc["r_KS"][H]])
            S.dma("sp", sc["QS"][H][64 + i:65 + i, :], parts[i].t[h:h + 1, :], reads=[parts[i].r], writes=[sc["r_QS"][H]])
    S.flush()
    A2.close()

    if "c" not in _P1SEC:
        A.close()
        return
    w = load_w([(C_MLA, 416)])
    wkrs = nxt("w", wsw) if False else wsw[0]
    if "p" in _MSUB:
        S.op("pool", lambda e: e.tensor_copy(out=wkrs.t[:, :, 0:16], in_=w.t[:, :, 400:416]), reads=[w.r], writes=[wkrs.r])
        S.op("pool", lambda e: e.tensor_copy(out=wkrs.t[:, :, 16:32], in_=w.t[:, :, 384:400]), reads=[w.r], writes=[wkrs.r])
    A3 = Alloc(nc)
    wuq = Buf(A3.sb("wuq", [128, 2, 384], BF16))
    wuqs = Buf(A3.sb("wuqs", [128, 2, 384], BF16))
    wukv = Buf(A3.sb("wukv", [128, 512], BF16))
    S.dma("pool", wuq.t[:], T["w_uq"][l].rearrange("(c p) n -> p c n", p=128), writes=[wuq.r])
    S.dma("pool", wukv.t[:], T["w_ukv"][l], writes=[wukv.r])
    S.op("pool", lambda e: e.tensor_copy(out=wuqs.t[:], in_=wuq.t[:]), reads=[wuq.r], writes=[wuqs.r])
    for c2 in (range(2) if "p" in _MSUB else []):
        v4o = wuqs.t[:, c2, :].rearrange("p (h d) -> p h d", h=4)
        v4i = wuq.t[:, c2, :].rearrange("p (h d) -> p h d", h=4)
        S.op("pool", lambda e, v4o=v4o, v4i=v4i: e.tensor_copy(out=v4o[:, :, 64:80], in_=v4i[:, :, 80:96]), reads=[wuq.r, wuqs.r], writes=[wuqs.r])
        S.op("pool", lambda e, v4o=v4o, v4i=v4i: e.tensor_copy(out=v4o[:, :, 80:96], in_=v4i[:, :, 64:80]), reads=[wuq.r, wuqs.r], writes=[wuqs.r])
    gcq = Buf(A3.sb("gcq", [128, 2], F32))
    gckv = Buf(A3.sb("gckv", [128, 1], F32))
    for c2 in range(2):
        S.dma("sp", gcq.t[:, c2:c2 + 1], T["g_cq"][l][c2 * 128:(c2 + 1) * 128].rearrange("(p o) -> p o", o=1), writes=[gcq.r])
    S.dma("sp", gckv.t[:], T["g_ckv"][l].rearrange("(p o) -> p o", o=1), writes=[gckv.r])
    for tb, nm in (((CT, "rope32c"), (ST, "rope32s")) if "t" in _MSUB else []):
        S.dma("pool", tb.t[0:32, :], T[nm], writes=[tb.r])
        S.dma("pool", tb.t[64:96, :], T[nm], writes=[tb.r])
    onesq = Buf(A3.sb("onesq", [128, 128], BF16))
    oneskv = Buf(A3.sb("oneskv", [128, 128], BF16))
    epst = Buf(A3.sb("epst", [128, 1], F32))
    S.op("pool", lambda e: e.memset(epst.t[:], RMS_EPS), writes=[epst.r])
    S.op("pool", lambda e: e.memset(onesq.t[:], 1.0 / 256.0), writes=[onesq.r])
    S.op("pool", lambda e: e.memset(oneskv.t[:], 1.0 / 128.0), writes=[oneskv.r])
    cqg = rot(A3, "sb", "cqg", [128, 2, 512], BF16, 2)
    cq2 = rot(A3, "sb", "cq2", [128, 2, 512], BF16, 2)
    ckg = rot(A3, "sb", "ckg", [128, 512], BF16, 2)
    ck2 = rot(A3, "sb", "ck2", [128, 512], BF16, 2)
    rq = rot(A3, "sb", "rq", [128, 512], F32, 2)
    rkv = rot(A3, "sb", "rkv", [128, 512], F32, 2)
    rtok = rot(A3, "sb", "rtok", [128, 1], F32, 2)
    vstm = Buf(A3.sb("vstm", [128, NB, 4, 65], BF16))
    S.op("pool", lambda e: e.memset(vstm.t[:], 1.0), writes=[vstm.r])
    wukv_v = wukv.t[:, :].rearrange("p (h x) -> p h x", h=4)[:, :, 64:128]
    for j in (range(NT) if "c" in _MSUB else []):
        tc = slice(j * 512, (j + 1) * 512)
        a, a2, kg, k2, rq_, rkv_ = cqg[j % 2], cq2[j % 2], ckg[j % 2], ck2[j % 2], rq[j % 2], rkv[j % 2]
        for c2 in (range(2) if "1" in _MC else []):
            p = proj_fm(w, c2 * 128, 128, 1, j)
            S.op("dve", lambda e, p=p, c2=c2, a=a: e.tensor_scalar(out=a.t[:, c2, :], in0=p.t[:], scalar1=gcq.t[:, c2:c2 + 1], scalar2=None,
                                                                  op0=ALU.mult), reads=[p.r, gcq.r], writes=[a.r])
            S.op("act", lambda e, p=p, c2=c2, a2=a2: e.activation(out=a2.t[:, c2, :], in_=p.t[:], func=AF.Square), reads=[p.r], writes=[a2.r])
        if "2" in _MC:
            p = proj_fm(w, 256, 128, 1, j)
            if "5" not in _MC:
                S.op("dve", lambda e, p=p, kg=kg: e.tensor_scalar(out=kg.t[:], in0=p.t[:], scalar1=gckv.t[:, 0:1], scalar2=None, op0=ALU.mult),
                     reads=[p.r, gckv.r], writes=[kg.r])
            if "6" not in _MC:
                S.op("act", lambda e, p=p, k2=k2: e.activation(out=k2.t[:], in_=p.t[:], func=AF.Square), reads=[p.r], writes=[k2.r])
        if "3" in _MC:
            p = nxt("pj", pj)
            for c2 in range(2):
                S.op("pe", lambda e, p=p, c2=c2, a2=a2: e.matmul(p.t[:], lhsT=onesq.t[:], rhs=a2.t[:, c2, :], start=(c2 == 0), stop=(c2 == 1)),
                     reads=[onesq.r, a2.r], writes=[p.r])
            S.op("act", lambda e, p=p, rq_=rq_: e.activation(out=rq_.t[:], in_=p.t[:], func=AF.Sqrt, bias=epst.t[:, 0:1]), reads=[p.r, epst.r], writes=[rq_.r])
            S.op("dve", lambda e, rq_=rq_: e.reciprocal(out=rq_.t[:], in_=rq_.t[:]), reads=[rq_.r], writes=[rq_.r])
        if "4" in _MC:
            p = nxt("pj", pj)
            S.op("pe", lambda e, p=p, k2=k2: e.matmul(p.t[:], lhsT=oneskv.t[:], rhs=k2.t[:], start=True, stop=True), reads=[oneskv.r, k2.r], writes=[p.r])
            S.op("act", lambda e, p=p, rkv_=rkv_: e.activation(out=rkv_.t[:], in_=p.t[:], func=AF.Sqrt, bias=epst.t[:, 0:1]), reads=[p.r, epst.r], writes=[rkv_.r])
            S.op("dve", lambda e, rkv_=rkv_: e.reciprocal(out=rkv_.t[:], in_=rkv_.t[:]), reads=[rkv_.r], writes=[rkv_.r])
        for h in (range(4) if "q" in _MSUB else []):
            H = 8 + h
            pa, pb = nxt("pj", pj), nxt("pj", pj)
            for pp, ww in ((pa, wuq), (pb, wuqs)):
                for c2 in range(2):
                    S.op("pe", lambda e, pp=pp, ww=ww, c2=c2, h=h, a=a: e.matmul(pp.t[0:96, :], lhsT=ww.t[:, c2, h * 96:(h + 1) * 96], rhs=a.t[:, c2, :],
                                                                             start=(c2 == 0), stop=(c2 == 1)), reads=[ww.r, a.r], writes=[pp.r])
            st = nxt("stg", stg)
            t1, t2 = tmp1[h % 2], tmp2[h % 2]
            S.op("dve", lambda e, pa=pa, st=st, rq_=rq_: e.scalar_tensor_tensor(out=st.t[0:64, :], in0=pa.t[0:64, :], scalar=MLA_SCALE, in1=rq_.t[0:64, :],
                                                                             op0=ALU.mult, op1=ALU.mult), reads=[pa.r, rq_.r], writes=[st.r])
            S.op("dve", lambda e, pa=pa, t1=t1, tc=tc: e.tensor_tensor(out=t1.t[64:96, :], in0=pa.t[64:96, :], in1=CT.t[64:96, tc], op=ALU.mult),
                 reads=[pa.r, CT.r], writes=[t1.r])
            S.op("dve", lambda e, pb=pb, t2=t2, tc=tc: e.tensor_tensor(out=t2.t[64:96, :], in0=pb.t[64:96, :], in1=ST.t[64:96, tc], op=ALU.mult),
                 reads=[pb.r, ST.r], writes=[t2.r])
            S.op("pool", lambda e, t1=t1, t2=t2: e.tensor_tensor(out=t1.t[64:96, :], in0=t1.t[64:96, :], in1=t2.t[64:96, :], op=ALU.add),
                 reads=[t1.r, t2.r], writes=[t1.r])
            S.op("dve", lambda e, t1=t1, st=st, rq_=rq_: e.scalar_tensor_tensor(out=st.t[64:96, :], in0=t1.t[64:96, :], scalar=MLA_SCALE, in1=rq_.t[64:96, :],
                                                                             op0=ALU.mult, op1=ALU.mult), reads=[t1.r, rq_.r, st.r], writes=[st.r])
            store_rows(st, (0, 96), sc["QS"][H][0:96, :], sc["r_QS"][H], j)
        for h in (range(4) if "k" in _MSUB else []):
            H = 8 + h
            p = nxt("pj", pj)
            S.op("pe", lambda e, p=p, h=h, kg=kg: e.matmul(p.t[0:64, :], lhsT=wukv.t[:, h * 128:h * 128 + 64], rhs=kg.t[:], start=True, stop=True),
                 reads=[wukv.r, kg.r], writes=[p.r])
            st = nxt("stg", stg)
            S.op("dve", lambda e, p=p, st=st, rkv_=rkv_: e.tensor_tensor(out=st.t[0:64, :], in0=p.t[0:64, :], in1=rkv_.t[0:64, :], op=ALU.mult),
                 reads=[p.r, rkv_.r], writes=[st.r])
            store_rows(st, (0, 64), sc["KS"][H][0:64, :], sc["r_KS"][H], j)
        if "r" not in _MSUB:
            continue
        pa = proj_fm(w, 384, 32, 1, j)
        pb = proj_fm(wkrs, 0, 32, 1, j)
        t1, t2 = tmp1[0], tmp2[0]
        st = nxt("stg", stg)
        S.op("dve", lambda e, pa=pa, t1=t1, tc=tc: e.tensor_tensor(out=t1.t[0:32, :], in0=pa.t[0:32, :], in1=CT.t[0:32, tc], op=ALU.mult),
             reads=[pa.r, CT.r], writes=[t1.r])
        S.op("dve", lambda e, pb=pb, t2=t2, tc=tc: e.tensor_tensor(out=t2.t[0:32, :], in0=pb.t[0:32, :], in1=ST.t[0:32, tc], op=ALU.mult),
             reads=[pb.r, ST.r], writes=[t2.r])
        S.op("pool", lambda e, t1=t1, t2=t2, st=st: e.tensor_tensor(out=st.t[0:32, :], in0=t1.t[0:32, :], in1=t2.t[0:32, :], op=ALU.add),
             reads=[t1.r, t2.r], writes=[st.r])
        for h in range(4):
            store_rows(st, (0, 32), sc["KS"][8 + h][64:96, :], sc["r_KS"][8 + h], j)
        for bb in (range(4) if "v" in _MSUB else []):
            b = j * 4 + bb
            p = nxt("pv", pv)
            S.op("pe", lambda e, p=p, bb=bb, kg=kg: e.matmul(p.t[:, 0:256], lhsT=kg.t[:, bb * 128:(bb + 1) * 128], rhs=wukv_v, start=True, stop=True),
                 reads=[wukv.r, kg.r], writes=[p.r])
            S.op("pe", lambda e, p=p, bb=bb, k2=k2: e.matmul(p.t[:, 256:257], lhsT=k2.t[:, bb * 128:(bb + 1) * 128], rhs=oneskv.t[:, 0:1], start=True, stop=True),
                 reads=[oneskv.r, k2.r], writes=[p.r])
            rt = rtok[b % 2]
            S.op("act", lambda e, p=p, rt=rt: e.activation(out=rt.t[:], in_=p.t[:, 256:257], func=AF.Sqrt, bias=epst.t[:, 0:1]), reads=[p.r, epst.r], writes=[rt.r])
            S.op("dve", lambda e, rt=rt: e.reciprocal(out=rt.t[:], in_=rt.t[:]), reads=[rt.r], writes=[rt.r])
            S.op("dve", lambda e, p=p, b=b, rt=rt: e.tensor_scalar(out=vstm.t[:, b, :, 0:64], in0=p.t[:, 0:256].rearrange("p (h d) -> p h d", h=4),
                                                                  scalar1=rt.t[:, 0:1], scalar2=None, op0=ALU.mult), reads=[p.r, rt.r], writes=[vstm.r])
    for hp in range(2):
        S.dma("sp", sc["VS"][4 + hp].rearrange("p (b h d) -> p b h d", b=NB, h=2), vstm.t[:, :, 2 * hp:2 * hp + 2, :], reads=[vstm.r], writes=[sc["r_VS"][4 + hp]])

    S.flush()
    A3.close()
    if "d" not in _P1SEC:
        A.close()
        return
    for tb, nm in ((CT, "rope64c"), (ST, "rope64s")):
        S.dma("pool", tb.t[0:64, :], T[nm], writes=[tb.r])
        S.dma("pool", tb.t[64:128, :], T[nm], writes=[tb.r])
    for g in range(3):
        r = DIL_R[g]
        for hp in range(2):
            o = g * 256 + hp * 128
            w = load_w([(C_DIL + o, 128), (C_DIL + 768 + o, 128), (C_DIL + 1536 + o, 128)])
            scale_q(w)
            ws = wsw[(g * 2 + hp) % 2]
            for c in range(8):
                vo = ws.t[:, c, :].rearrange("p (h f d) -> p h f d", h=4, f=2)
                vi = w.t[:, c, 0:256].rearrange("p (h f d) -> p h f d", h=4, f=2)
                S.op("pool", lambda e, vo=vo, vi=vi: e.tensor_copy(out=vo[:, :, 0, :], in_=vi[:, :, 1, :]), reads=[w.r], writes=[ws.r])
                S.op("pool", lambda e, vo=vo, vi=vi: e.tensor_copy(out=vo[:, :, 1, :], in_=vi[:, :, 0, :]), reads=[w.r], writes=[ws.r])
            for qk, dst, rd in ((0, sc["QD"], sc["r_QD"]), (1, sc["KD"], sc["r_KD"])):
                for j in range(NT):
                    pa = proj_fm(w, qk * 128, 128, r, j)
                    pb = proj_fm(ws, qk * 128, 128, r, j)
                    t1, t2 = tmp1[j % 2], tmp2[j % 2]
                    st = nxt("stg", stg)
                    cv = perm_view(CT.t[:, :], r, j * 512, 512)
                    sv = perm_view(ST.t[:, :], r, j * 512, 512)
                    shp = None if len(cv.shape) == 2 else cv.shape

                    def v3(ap):
                        return ap if shp is None else ap.rearrange("p (a b) -> p a b", a=shp[1])
                    S.op("dve", lambda e, pa=pa, t1=t1, cv=cv, v3=v3: e.tensor_tensor(out=v3(t1.t[:]), in0=v3(pa.t[:]), in1=cv, op=ALU.mult),
                         reads=[pa.r, CT.r], writes=[t1.r])
                    S.op("dve", lambda e, pb=pb, t2=t2, sv=sv, v3=v3: e.tensor_tensor(out=v3(t2.t[:]), in0=v3(pb.t[:]), in1=sv, op=ALU.mult),
                         reads=[pb.r, ST.r], writes=[t2.r])
                    S.op("pool", lambda e, t1=t1, t2=t2, st=st: e.tensor_tensor(out=st.t[:], in0=t1.t[:], in1=t2.t[:], op=ALU.add),
                         reads=[t1.r, t2.r], writes=[st.r])
                    for hh in range(2):
                        store_rows(st, (hh * 64, hh * 64 + 64), dst[g][hp * 2 + hh], rd[g][hp * 2 + hh], j)
            vt = nxt("v", vst)
            v_proj(w, 256, r, vt)
            S.dma("sp", sc["VD"][g][hp], vt.t[:, :, :, :].rearrange("p b h d -> p (b h d)"), reads=[vt.r], writes=[sc["r_VD"][g][hp]])
    S.flush()
    A.close()


def phase2(nc, S, T, l, sc, heads=None, after_sb=None):
    A = Alloc(nc)
    negtri = Buf(A.sb("negtri", [128, 128], BF16))
    S.op("pool", lambda e: e.memset(negtri.t[:], -1.0), writes=[negtri.r])
    S.op("pool", lambda e: e.affine_select(out=negtri.t[:], in_=negtri.t[:], pattern=[[-1, 128]], compare_op=ALU.is_ge, fill=0.0, base=0,
                                           channel_multiplier=1), reads=[negtri.r], writes=[negtri.r])
    ones = Buf(A.sb("ones", [128, 128], BF16))
    S.op("pool", lambda e: e.memset(ones.t[:], 1.0), writes=[ones.r])
    wn = Buf(A.sb("wn", [65, 64], BF16))
    wnsb = Buf(A.sb("wnsb", [65, 64], BF16))
    for t_, v_ in ((wn, RMS_EPS), (wnsb, 0.0)):
        S.op("pool", lambda e, t_=t_: e.memset(t_.t[:], 1.0 / 64.0), writes=[t_.r])
        S.op("pool", lambda e, t_=t_, v_=v_: e.memset(t_.t[64:65, :], v_), reads=[t_.r], writes=[t_.r])
    gh = Buf(A.sb("gh", [64, 16], F32))
    for h_ in range(16):
        S.dma("sp", gh.t[:, h_:h_ + 1], T["g_head"][l][h_].rearrange("(d o) -> d o", o=1), writes=[gh.r])
    eps2 = Buf(A.sb("eps2", [64, 2], F32))
    S.op("pool", lambda e: e.memset(eps2.t[:, 0:1], RMS_EPS), writes=[eps2.r])
    S.op("pool", lambda e: e.memset(eps2.t[:, 1:2], 0.0), reads=[eps2.r], writes=[eps2.r])

    Qt = rot(A, "sb", "Qt", [128, SEQ], BF16, 4)
    Kt = rot(A, "sb", "Kt", [128, SEQ], BF16, 4)
    Vt = rot(A, "sb", "Vt", [128, NB, 2, 65], BF16, 2)
    pz = rot(A, "ps", "pz", [128, 512], F32, 3)
    po = rot(A, "ps", "po", [128, 512], F32, 2)
    pc = rot(A, "ps", "pc", [128, 512], F32, 2)
    pss = rot(A, "ps", "pss", [128, 512], F32, 1)
    Pb = rot(A, "sb", "Pb", [128, 512], BF16, 8)
    eb = rot(A, "sb", "eb", [128, 512], F32, 2)
    spb = rot(A, "sb", "spb", [128, 512], BF16, 3)
    lw = rot(A, "sb", "lw", [128, 512], F32, 4)
    Rsb = Buf(A.sb("Rsb", [128, 512], F32))
    sqb = rot(A, "sb", "sqb", [65, 512], BF16, 2)
    osb = rot(A, "sb", "osb", [65, 512], F32, 3)
    def mk_mask(name, n, conds):
        m = Buf(A.sb(name, [128, n], BF16))
        S.op("pool", lambda e: e.memset(m.t[:], 1.0), writes=[m.r])
        for (step, base, cm) in conds:
            S.op("pool", lambda e, step=step, base=base, cm=cm: e.affine_select(out=m.t[:], in_=m.t[:], pattern=[[step, n]], compare_op=ALU.is_ge, fill=0.0,
                                                                                base=base, channel_multiplier=cm), reads=[m.r], writes=[m.r])
        return m
    maskS = [mk_mask("ms%d" % o, 512, [(1, -128 * o - 1, -1)]) for o in (3, 2, 1, 0)][::-1]
    maskC = [mk_mask("mc%d" % o, 512, [(1, -128 * o, -1)]) for o in range(4)]
    maskD = {512: {o: mk_mask("md%d" % (o + 1), 512, [(1, -128 * o, -1), (-1, 128 + 128 * o, 1)]) for o in range(-1, 4)},
             256: {o: mk_mask("me%d" % o, 256, [(1, -128 * o, -1), (-1, 128 + 128 * o, 1)]) for o in range(0, 2)}}
    stb = rot(A, "sb", "stb", [64, 512], F32, 2)
    yb = rot(A, "sb", "yb", [64, 512], BF16, 2)
    acc = rot(A, "sb", "acc", [65, SEQ], F32, 2)
    st = {"fin": 0, "ld": 0, "vld": 0, "o": 0}

    def finish(src_ap, r_src, h, t0, n, is_sb, in_sbuf=False):
        i = st["fin"]
        st["fin"] += 1
        sq, s_, y, ps_ = sqb[i % 2], stb[i % 2], yb[i % 2], pss[0]
        if in_sbuf:
            o_ap, r_o = src_ap, r_src
        else:
            ob = osb[i % 3]
            S.op("act", lambda e: e.activation(out=ob.t[:, 0:n], in_=src_ap, func=AF.Copy), reads=[r_src], writes=[ob.r])
            o_ap, r_o = ob.t[:, 0:n], ob.r
        S.op("dve", lambda e: e.tensor_tensor(out=sq.t[:, 0:n], in0=o_ap, in1=o_ap, op=ALU.mult), reads=[r_o], writes=[sq.r])
        wn_ = wnsb if is_sb else wn
        S.op("pe", lambda e: e.matmul(ps_.t[0:64, 0:n], lhsT=wn_.t[:, :], rhs=sq.t[:, 0:n], start=True, stop=True), reads=[wn_.r, sq.r], writes=[ps_.r])
        S.op("act", lambda e: e.activation(out=s_.t[:, 0:n], in_=ps_.t[0:64, 0:n], func=AF.Ln, bias=(eps2.t[:, 0:1] if is_sb else eps2.t[:, 1:2])),
             reads=[ps_.r, eps2.r], writes=[s_.r])
        S.op("act", lambda e: e.activation(out=s_.t[:, 0:n], in_=s_.t[:, 0:n], func=AF.Exp, scale=-0.5), reads=[s_.r], writes=[s_.r])
        S.op("dve", lambda e: e.scalar_tensor_tensor(out=y.t[:, 0:n], in0=o_ap[0:64], scalar=gh.t[:, h:h + 1], in1=s_.t[:, 0:n],
                                                     op0=ALU.mult, op1=ALU.mult), reads=[r_o, gh.r, s_.r], writes=[y.r])
        S.dma("sp", sc["OnT"][h // 2, (h % 2) * 64:(h % 2) * 64 + 64, t0:t0 + n], y.t[:, 0:n], reads=[y.r], writes=[sc["r_OnT"]])

    def load_qk(qsrc, r_q, ksrc, r_k, kd):
        i = st["ld"]
        st["ld"] += 1
        q, k = Qt[i % 4], Kt[i % 4]
        S.dma("sp", q.t[0:kd, :], qsrc, reads=[r_q], writes=[q.r])
        S.dma("sp", k.t[0:kd, :], ksrc, reads=[r_k], writes=[k.r])
        return q, k

    def load_v(vsrc, r_v):
        i = st["vld"]
        st["vld"] += 1
        v = Vt[i % 2]
        S.dma("sp", v.t[:, :, :, :].rearrange("p b h d -> p (b h d)"), vsrc, reads=[r_v], writes=[v.r])
        return v

    def make_stages(steps, q, k, kd, v, hh, kind, done_cb, u=0):
        n_ = len(steps)
        ctx = [dict() for _ in range(n_)]
        zsel = [[pz[0], pz[1]], [pz[2], pc[0]]][u]

        def s1(i):
            sp_ = steps[i]
            z = zsel[i % 2]
            q0, n, kb = sp_["q0"], sp_["n"], sp_["kb"]
            S.op("pe", lambda e: e.matmul(z.t[:, 0:n], lhsT=k.t[0:kd, kb * 128:(kb + 1) * 128], rhs=q.t[0:kd, q0:q0 + n], start=True, stop=True),
                 reads=[k.r, q.r], writes=[z.r])
            if kind == "sb":
                e_, s_ = eb[i % 2], spb[i % 3]
                S.op("act", lambda e: e.activation(out=e_.t[:, 0:n], in_=z.t[:, 0:n], func=AF.Exp), reads=[z.r], writes=[e_.r])
                S.op("act", lambda e: e.activation(out=s_.t[:, 0:n], in_=e_.t[:, 0:n], func=AF.Ln, bias=1.0), reads=[e_.r], writes=[s_.r])
                if sp_["mask"] is not None:
                    mk = sp_["mask"]
                    S.op("pool", lambda e: e.tensor_tensor(out=s_.t[:, 0:n], in0=s_.t[:, 0:n], in1=mk.t[:, 0:n], op=ALU.mult), reads=[s_.r, mk.r], writes=[s_.r])
                ctx[i]["sp"] = s_
            else:
                p_ = Pb[u * 4 + i % 4]
                if kind == "fox" and sp_["mask"] is not None:
                    l_ = lw[u * 2 + i % 2]
                    S.op("dve", lambda e: e.tensor_scalar(out=l_.t[:, 0:n], in0=z.t[:, 0:n], scalar1=60.0, scalar2=None, op0=ALU.min), reads=[z.r], writes=[l_.r])
                    S.op("act", lambda e: e.activation(out=p_.t[:, 0:n], in_=l_.t[:, 0:n], func=AF.Exp), reads=[l_.r], writes=[p_.r])
                else:
                    S.op("act", lambda e: e.activation(out=p_.t[:, 0:n], in_=z.t[:, 0:n], func=AF.Exp), reads=[z.r], writes=[p_.r])
                if sp_["mask"] is not None:
                    mk = sp_["mask"]
                    S.op("dve", lambda e: e.tensor_tensor(out=p_.t[:, 0:n], in0=p_.t[:, 0:n], in1=mk.t[:, 0:n], op=ALU.mult), reads=[p_.r, mk.r], writes=[p_.r])
                ctx[i]["P"] = p_

        def s2(i):
            if kind != "sb":
                return
            sp_ = steps[i]
            q0, n, kb = sp_["q0"], sp_["n"], sp_["kb"]
            s_ = ctx[i]["sp"]
            c_, rc, l_, p_ = pc[i % 2], pz[2], lw[i % 2], Pb[i % 4]
            S.op("pe", lambda e: e.matmul(c_.t[:, 0:n], lhsT=k.t[0:kd, kb * 128:(kb + 1) * 128], rhs=q.t[0:kd, q0:q0 + n], start=True, stop=False),
                 reads=[k.r, q.r], writes=[c_.r])
            S.op("pe", lambda e: e.matmul(c_.t[:, 0:n], lhsT=negtri.t[:], rhs=s_.t[:, 0:n], start=False, stop=True), reads=[negtri.r, s_.r], writes=[c_.r])
            S.op("pe", lambda e: e.matmul(rc.t[:, 0:n], lhsT=ones.t[:], rhs=s_.t[:, 0:n], start=True, stop=True), reads=[ones.r, s_.r], writes=[rc.r])
            if sp_["first"]:
                S.op("dve", lambda e: e.tensor_copy(out=l_.t[:, 0:n], in_=c_.t[:, 0:n]), reads=[c_.r], writes=[l_.r])
                S.op("dve", lambda e: e.tensor_copy(out=Rsb.t[:, 0:n], in_=rc.t[:, 0:n]), reads=[rc.r], writes=[Rsb.r])
            else:
                S.op("dve", lambda e: e.tensor_tensor(out=l_.t[:, 0:n], in0=c_.t[:, 0:n], in1=Rsb.t[:, 0:n], op=ALU.subtract), reads=[c_.r, Rsb.r], writes=[l_.r])
                S.op("dve", lambda e: e.tensor_tensor(out=Rsb.t[:, 0:n], in0=rc.t[:, 0:n], in1=Rsb.t[:, 0:n], op=ALU.add), reads=[rc.r, Rsb.r], writes=[Rsb.r])
            S.op("act", lambda e: e.activation(out=p_.t[:, 0:n], in_=l_.t[:, 0:n], func=AF.Exp), reads=[l_.r], writes=[p_.r])
            if sp_["mask"] is not None:
                mk = sp_["mask"]
                S.op("pool", lambda e: e.tensor_tensor(out=p_.t[:, 0:n], in0=p_.t[:, 0:n], in1=mk.t[:, 0:n], op=ALU.mult), reads=[p_.r, mk.r], writes=[p_.r])
            ctx[i]["P"] = p_

        def s3(i):
            sp_ = steps[i]
            n, kb = sp_["n"], sp_["kb"]
            if sp_["first"] and u == 0:
                st["o"] += 1
            o_ = po[st["o"] % 2] if u == 0 else pc[1]
            p_ = ctx[i]["P"]
            S.op("pe", lambda e: e.matmul(o_.t[0:65, 0:n], lhsT=v.t[:, kb, hh, :], rhs=p_.t[:, 0:n], start=sp_["first"], stop=sp_["last"]),
                 reads=[v.r, p_.r], writes=[o_.r])
            if sp_["last"]:
                done_cb(o_, sp_)

        return n_, s1, s2, s3

    def drive(stage_sets):
        nmax = max(ss[0] for ss in stage_sets)
        for i in range(nmax + 2):
            for n_, s1, s2, s3 in stage_sets:
                if i < n_:
                    s1(i)
            for n_, s1, s2, s3 in stage_sets:
                if 0 <= i - 1 < n_:
                    s2(i - 1)
            for n_, s1, s2, s3 in stage_sets:
                if 0 <= i - 2 < n_:
                    s3(i - 2)

    def run_steps(steps, q, k, kd, v, hh, kind, done_cb):
        drive([make_stages(steps, q, k, kd, v, hh, kind, done_cb, 0)])

    def causal_steps(strict, descending):
        steps = []
        for qt in range(NT):
            q0 = qt * 512
            kbs = list(range(0, 4 * qt + 4))
            if descending:
                kbs = kbs[::-1]
            for ii, kb in enumerate(kbs):
                o = kb - 4 * qt
                steps.append(dict(q0=q0, n=512, kb=kb, mask=((maskS if strict else maskC)[o] if o >= 0 else None), first=(ii == 0), last=(ii == len(kbs) - 1)))
        return steps

    def dil_steps(r):
        sc_ = SEQ // r
        n = min(512, sc_)
        steps = []
        for q0 in range(0, SEQ, n):
            cs = (q0 // sc_) * sc_
            k_lo = max(cs, q0 - 128)
            kbs = list(range(k_lo // 128, (q0 + n) // 128))
            for ii, kb in enumerate(kbs):
                steps.append(dict(q0=q0, n=n, kb=kb, mask=maskD[n][kb - q0 // 128], first=(ii == 0), last=(ii == len(kbs) - 1)))
        return steps

    hsel = (lambda h: True) if heads is None else (lambda h: h in heads)
    groups = []
    for kind, hbase, pbase, kd in (("sb", 0, 0, 64), ("fox", 4, 2, 70), ("mla", 8, 4, 96)):
        for hp in range(2):
            js = [(kind, hbase + hp * 2 + hh, pbase + hp, hh, kd) for hh in range(2) if hsel(hbase + hp * 2 + hh)]
            if kind == "sb":
                groups += [[j] for j in js]
            elif js:
                groups.append(js)
    step_cache = {"sb": causal_steps(True, True), "fox": causal_steps(False, False)}
    step_cache["mla"] = step_cache["fox"]
    loaded = {}
    vcur = {}

    def prefetch(group):
        for job in group:
            kind, h, pr_, hh, kd = job
            if pr_ not in vcur:
                vcur.clear()
                vcur[pr_] = load_v(sc["VS"][pr_], sc["r_VS"][pr_])
            loaded[h] = load_qk(sc["QS"][h][0:kd, :], sc["r_QS"][h], sc["KS"][h][0:kd, :], sc["r_KS"][h], kd) + (vcur[pr_],)
    if groups:
        prefetch(groups[0])
    for gi, group in enumerate(groups):
        if after_sb is not None and group[0][0] != "sb":
            after_sb()
            after_sb = None
        cur = [loaded.pop(job[1]) for job in group]
        if gi + 1 < len(groups):
            prefetch(groups[gi + 1])
        sets = []
        for u, (job, (q, k, v)) in enumerate(zip(group, cur)):
            kind, h, pr_, hh, kd = job

            def done(o_, sp_, h=h, kind=kind):
                finish(o_.t[0:65, 0:sp_["n"]], o_.r, h, sp_["q0"], sp_["n"], kind == "sb")
            sets.append(make_stages(step_cache[kind], q, k, kd, v, hh, kind, done, u))
        drive(sets)
    if after_sb is not None:
        after_sb()
    dgroups = [(hp, g) for hp in range(2) if (hsel(12 + 2 * hp) or hsel(13 + 2 * hp)) for g in range(3)]
    dsteps = {g: dil_steps(DIL_R[g]) for g in range(3)}
    dl = {}

    def dprefetch(grp):
        hp, g = grp
        v = load_v(sc["VD"][g][hp], sc["r_VD"][g][hp])
        dl[grp] = [load_qk(sc["QD"][g][hp * 2 + hh], sc["r_QD"][g][hp * 2 + hh], sc["KD"][g][hp * 2 + hh], sc["r_KD"][g][hp * 2 + hh], 64) + (v,)
                   for hh in range(2)]
    if dgroups:
        dprefetch(dgroups[0])
    for gi, grp in enumerate(dgroups):
        hp, g = grp
        r = DIL_R[g]
        cur = dl.pop(grp)
        if gi + 1 < len(dgroups):
            dprefetch(dgroups[gi + 1])
        sets = []
        for hh in range(2):
            q, k, v = cur[hh]
            a_ = acc[hh]

            def done(o_, sp_, a_=a_, r=r, g=g):
                n, q0 = sp_["n"], sp_["q0"]
                dst = perm_view(a_.t[:, :], r, q0, n)
                if g == 0:
                    S.op("act", lambda e: e.activation(out=dst, in_=o_.t[0:65, 0:n], func=AF.Copy), reads=[o_.r], writes=[a_.r])
                else:
                    S.op("dve", lambda e: e.tensor_tensor(out=dst, in0=o_.t[0:65, 0:n], in1=dst, op=ALU.add), reads=[o_.r, a_.r], writes=[a_.r])
            sets.append(make_stages(dsteps[g], q, k, 64, v, hh, "dil", done, hh))
        drive(sets)
        if g == 2:
            for h2 in range(2):
                for qt in range(NT):
                    finish(acc[h2].t[:, qt * 512:(qt + 1) * 512], acc[h2].r, 12 + hp * 2 + h2, qt * 512, 512, False, in_sbuf=True)
    S.flush()
    A.close()


def layernorm_block(S, y, g_b, b_b, small, out):
    st6, mv, rs = small["st6"], small["mv"], small["rs"]
    for hf in range(2):
        S.op("dve", lambda e, hf=hf: e.bn_stats(out=st6.t[:, hf, :], in_=y.t[:, hf * 512:(hf + 1) * 512]), reads=[y.r], writes=[st6.r])
    S.op("dve", lambda e: e.bn_aggr(out=mv.t[:], in_=st6.t[:, :, :].rearrange("p a b -> p (a b)")), reads=[st6.r], writes=[mv.r])
    S.op("act", lambda e: e.activation(out=rs.t[:], in_=mv.t[:, 1:2], func=AF.Ln, bias=small["eps"].t[:, 0:1]), reads=[mv.r, small["eps"].r], writes=[rs.r])
    S.op("act", lambda e: e.activation(out=rs.t[:], in_=rs.t[:], func=AF.Exp, scale=-0.5), reads=[rs.r], writes=[rs.r])
    S.op("dve", lambda e: e.scalar_tensor_tensor(out=y.t[:], in0=y.t[:], scalar=mv.t[:, 0:1], in1=g_b.t[:], op0=ALU.subtract, op1=ALU.mult),
         reads=[y.r, mv.r, g_b.r], writes=[y.r])
    S.op("dve", lambda e: e.scalar_tensor_tensor(out=out.t[:], in0=y.t[:], scalar=rs.t[:, 0:1], in1=b_b.t[:], op0=ALU.mult, op1=ALU.add),
         reads=[y.r, rs.r, b_b.r], writes=[out.r])


def bcast_row(S, A, name, src1d, n):
    b = Buf(A.sb(name, [128, n], F32))
    S.dma("sp", b.t[:], src1d.rearrange("(o n) -> o n", o=1).partition_broadcast(128), writes=[b.r])
    return b


def phase3(nc, S, T, xin, r_xin, l, sc):
    A = Alloc(nc)
    identf = make_ident(A, S, F32)
    wout = Buf(A.sb("wout", [128, 8, DM], BF16))
    S.dma("pool", wout.t[:], T["w_out"][l].rearrange("(c p) n -> p c n", p=128), writes=[wout.r])
    wr = Buf(A.sb("wr", [128, 8, 20], F32))
    S.dma("sp", wr.t[:, :, 0:4], T["w_group"][l].rearrange("(c p) n -> p c n", p=128), writes=[wr.r])
    S.dma("sp", wr.t[:, :, 4:20], T["w_expert"][l].rearrange("(c p) n -> p c n", p=128), writes=[wr.r])
    brt = Buf(A.sb("brt", [128, 20], F32))
    S.dma("sp", brt.t[:, 0:4], T["b_group"][l].rearrange("(o n) -> o n", o=1).partition_broadcast(128), writes=[brt.r])
    S.dma("sp", brt.t[:, 4:20], T["b_expert"][l].rearrange("(o n) -> o n", o=1).partition_broadcast(128), writes=[brt.r])
    g_b = bcast_row(S, A, "ln1g", T["ln1_g"][l], DM)
    b_b = bcast_row(S, A, "ln1b", T["ln1_b"][l], DM)
    on = rot(A, "sb", "on", [128, 8, 512], BF16, 2)
    xs = rot(A, "sb", "xs3", [128, DM], F32, 4)
    y = rot(A, "sb", "y3", [128, DM], F32, 3)
    x1 = rot(A, "sb", "x1o", [128, DM], F32, 6)
    xtf = rot(A, "sb", "xtf", [128, 8, 128], F32, 3)
    xtb = rot(A, "sb", "xtb", [128, 8, 512], BF16, 2)
    gate = Buf(A.sb("gate", [128, NB, 16], F32))
    lgall = Buf(A.sb("lgall", [128, NB, 20], F32))
    if ROUTED:
        x1b = Buf(A.sb("x1b", [128, NB, DM], BF16))
        gohall = Buf(A.sb("gohall", [128, NB, 4], F32))
        S.op("pool", lambda e: e.memset(x1b.t[:, 0:4, :], 0.0), writes=[x1b.r])
        S.op("pool", lambda e: e.memset(gate.t[:], 0.0), writes=[gate.r])
        for k in (range(NTILE) if int(_os_env.get("ZF", "1")) else []):
            S.dma("sp", sc["XS"][k * 512:(k + 1) * 512, :].rearrange("(p r) n -> p (r n)", p=128), x1b.t[:, 0:4, :].rearrange("p b n -> p (b n)"),
                  reads=[x1b.r], writes=[sc["r_XS"]])
        for k in (range(NTILE) if int(_os_env.get("ZF", "1")) else []):
            S.dma("sp", sc["GS"][k * 512:(k + 1) * 512, :].rearrange("(p r) n -> p (r n)", p=128), gate.t[:, 0:4, :].rearrange("p b n -> p (b n)"),
                  reads=[gate.r], writes=[sc["r_GS"]])
    ph = rot(A, "ps", "ph", [128, DM], F32, 2)
    ptr = rot(A, "ps", "ptr", [128, DM], F32, 1)
    plg = rot(A, "ps", "plg", [128, 512], F32, 2)
    epsl = Buf(A.sb("epsl", [128, 1], F32))
    S.op("pool", lambda e: e.memset(epsl.t[:], LN_EPS), writes=[epsl.r])
    small = [dict(st6=Buf(A.sb("st6", [128, 2, 6], F32)), mv=Buf(A.sb("mv", [128, 2], F32)), rs=Buf(A.sb("rs", [128, 1], F32)), eps=epsl) for _ in range(3)]
    pend = []
    pend2 = []

    def ld_on(j):
        S.dma("sp", on[j % 2].t[:], sc["OnT"][:, :, j * 512:(j + 1) * 512].rearrange("c p t -> p c t"), reads=[sc["r_OnT"]], writes=[on[j % 2].r])

    def ld_x(b):
        S.dma("sp", xs[b % 4].t[:], xin[b * 128:(b + 1) * 128, :], reads=[r_xin], writes=[xs[b % 4].r])
    for j in range(NT):
        o_ = on[j % 2]
        if j == 0:
            ld_on(0)
        if j + 1 < NT:
            ld_on(j + 1)
        xb_ = xtb[j % 2]
        for bb in range(4):
            b = j * 4 + bb
            s_, y_, x1_, xf_, p_, sm = xs[b % 4], y[b % 3], x1[b % 6], xtf[b % 3], ph[b % 2], small[b % 3]
            if b == 0:
                ld_x(0)
                ld_x(1)
            if b + 2 < NB:
                ld_x(b + 2)
            for hf in range(2):
                for c in range(8):
                    S.op("pe", lambda e, hf=hf, c=c, bb=bb, o_=o_, p_=p_: e.matmul(p_.t[:, hf * 512:(hf + 1) * 512], lhsT=o_.t[:, c, bb * 128:(bb + 1) * 128],
                                                                            rhs=wout.t[:, c, hf * 512:(hf + 1) * 512], start=(c == 0), stop=(c == 7)),
                         reads=[o_.r, wout.r], writes=[p_.r])
            S.op("dve", lambda e, s_=s_, y_=y_, p_=p_: e.scalar_tensor_tensor(out=y_.t[:], in0=s_.t[:], scalar=ALPHA, in1=p_.t[:], op0=ALU.mult, op1=ALU.add),
                 reads=[s_.r, p_.r], writes=[y_.r])
            layernorm_block(S, y_, g_b, b_b, sm, x1_)
            S.dma("sp", sc["X1"][b * 128:(b + 1) * 128, :], x1_.t[:], reads=[x1_.r], writes=[sc["r_X1"]])
            if ROUTED:
                S.op("act", lambda e, b=b, x1_=x1_: e.activation(out=x1b.t[:, b, :], in_=x1_.t[:], func=AF.Copy), reads=[x1_.r], writes=[x1b.r])
            def stage_b(b=b, bb=bb, x1_=x1_, xf_=xf_, xb_=xb_):
                pt = ptr[0]
                for c in range(8):
                    S.op("pe", lambda e, c=c: e.transpose(out=pt.t[:, c * 128:(c + 1) * 128], in_=x1_.t[:, c * 128:(c + 1) * 128], identity=identf.t[:]),
                         reads=[x1_.r, identf.r], writes=[pt.r])
                S.op("act", lambda e: e.activation(out=xf_.t[:, :, :], in_=pt.t[:, :].rearrange("p (c t) -> p c t", c=8), func=AF.Copy),
                     reads=[pt.r], writes=[xf_.r])
                if not ROUTED:
                    S.op("dve", lambda e: e.tensor_copy(out=xb_.t[:, :, bb * 128:(bb + 1) * 128], in_=pt.t[:, :].rearrange("p (c t) -> p c t", c=8)),
                         reads=[pt.r], writes=[xb_.r])
                def stage_c():
                    pl = plg[b % 2]
                    for c in range(8):
                        S.op("pe", lambda e, c=c: e.matmul(pl.t[:, 0:20], lhsT=xf_.t[:, c, :], rhs=wr.t[:, c, :], start=(c == 0), stop=(c == 7)),
                             reads=[xf_.r, wr.r], writes=[pl.r])
                    S.op("dve", lambda e: e.tensor_tensor(out=lgall.t[:, b, :], in0=pl.t[:, 0:20], in1=brt.t[:], op=ALU.add), reads=[pl.r, brt.r], writes=[lgall.r])
                if int(_os_env.get("INL", "0")):
                    stage_c()
                else:
                    pend2.append(stage_c)
            pend.append(stage_b)
            if len(pend2) > int(_os_env.get("LAG2", "1")):
                pend2.pop(0)()
            if len(pend) > 3:
                pend.pop(0)()
            if (not ROUTED) and bb == 3:
                while pend:
                    pend.pop(0)()
                while pend2:
                    pend2.pop(0)()
        if not ROUTED:
            S.dma("sp", sc["X1T"][:, :, j * 512:(j + 1) * 512], xb_.t[:], reads=[xb_.r], writes=[sc["r_X1T"]])
    while pend:
        pend.pop(0)()
        while len(pend2) > 1:
            pend2.pop(0)()
    while pend2:
        pend2.pop(0)()
    def GT(name, shape):
        return Buf(A.sb("gv_" + name, shape, F32))
    B3 = [128, NB, 4]
    gl = lgall.t[:, :, 0:4]
    el = lgall.t[:, :, 4:20].rearrange("p b (g x) -> p b g x", g=4)
    m_, goh, tmp, se = GT("m", [128, NB]), (gohall if ROUTED else GT("goh", B3)), GT("tmp", B3), GT("se", [128, NB])
    t44, es, m1, oh1, es2, m2, oh2 = GT("t44", [128, NB, 4, 4]), GT("es", B3), GT("m1", [128, NB]), GT("oh1", B3), GT("es2", B3), GT("m2", [128, NB]), GT("oh2", B3)
    d_, p1, p2, gi = GT("d", [128, NB]), GT("p1", [128, NB]), GT("p2", [128, NB]), GT("gi", B3)

    def bc(t2):
        return t2.t[:, :].unsqueeze(2).to_broadcast(B3)

    def D(fn, reads, writes):
        S.op("dve", fn, reads=[x.r for x in reads], writes=[x.r for x in writes])
    D(lambda e: e.tensor_reduce(out=m_.t[:], in_=gl, axis=AX.X, op=ALU.max), [lgall], [m_])
    D(lambda e: e.tensor_tensor(out=goh.t[:], in0=gl, in1=bc(m_), op=ALU.is_equal), [lgall, m_], [goh])
    D(lambda e: e.tensor_tensor(out=tmp.t[:], in0=gl, in1=bc(m_), op=ALU.subtract), [lgall, m_], [tmp])
    S.op("act", lambda e: e.activation(out=tmp.t[:], in_=tmp.t[:], func=AF.Exp), reads=[tmp.r], writes=[tmp.r])
    D(lambda e: e.tensor_reduce(out=se.t[:], in_=tmp.t[:], axis=AX.X, op=ALU.add), [tmp], [se])
    D(lambda e: e.reciprocal(out=se.t[:], in_=se.t[:]), [se], [se])
    D(lambda e: e.tensor_tensor(out=t44.t[:], in0=el, in1=goh.t[:, :, :].unsqueeze(3).to_broadcast([128, NB, 4, 4]), op=ALU.mult), [lgall, goh], [t44])
    D(lambda e: e.tensor_reduce(out=es.t[:], in_=t44.t[:, :, :, :].rearrange("p b g x -> p b x g"), axis=AX.X, op=ALU.add), [t44], [es])
    D(lambda e: e.tensor_reduce(out=m1.t[:], in_=es.t[:], axis=AX.X, op=ALU.max), [es], [m1])
    D(lambda e: e.tensor_tensor(out=oh1.t[:], in0=es.t[:], in1=bc(m1), op=ALU.is_equal), [es, m1], [oh1])
    D(lambda e: e.scalar_tensor_tensor(out=es2.t[:], in0=oh1.t[:], scalar=-1e30, in1=es.t[:], op0=ALU.mult, op1=ALU.add), [oh1, es], [es2])
    D(lambda e: e.tensor_reduce(out=m2.t[:], in_=es2.t[:], axis=AX.X, op=ALU.max), [es2], [m2])
    D(lambda e: e.tensor_tensor(out=oh2.t[:], in0=es2.t[:], in1=bc(m2), op=ALU.is_equal), [es2, m2], [oh2])
    D(lambda e: e.tensor_tensor(out=d_.t[:], in0=m2.t[:], in1=m1.t[:], op=ALU.subtract), [m1, m2], [d_])
    S.op("act", lambda e: e.activation(out=d_.t[:], in_=d_.t[:], func=AF.Exp), reads=[d_.r], writes=[d_.r])
    D(lambda e: e.tensor_scalar(out=p1.t[:], in0=d_.t[:], scalar1=1.0, scalar2=None, op0=ALU.add), [d_], [p1])
    D(lambda e: e.reciprocal(out=p1.t[:], in_=p1.t[:]), [p1], [p1])
    D(lambda e: e.tensor_tensor(out=p2.t[:], in0=d_.t[:], in1=p1.t[:], op=ALU.mult), [d_, p1], [p2])
    D(lambda e: e.tensor_tensor(out=gi.t[:], in0=oh1.t[:], in1=bc(p1), op=ALU.mult), [oh1, p1], [gi])
    D(lambda e: e.tensor_tensor(out=oh2.t[:], in0=oh2.t[:], in1=bc(p2), op=ALU.mult), [oh2, p2], [oh2])
    D(lambda e: e.tensor_tensor(out=gi.t[:], in0=gi.t[:], in1=oh2.t[:], op=ALU.add), [gi, oh2], [gi])
    D(lambda e: e.tensor_tensor(out=gi.t[:], in0=gi.t[:], in1=bc(se), op=ALU.mult), [gi, se], [gi])
    D(lambda e: e.tensor_tensor(out=gate.t[:, :, :].rearrange("p b (g x) -> p b g x", g=4), in0=goh.t[:, :, :].unsqueeze(3).to_broadcast([128, NB, 4, 4]),
                                in1=gi.t[:, :, :].unsqueeze(2).to_broadcast([128, NB, 4, 4]), op=ALU.mult), [goh, gi], [gate])
    if not ROUTED:
        S.dma("sp", sc["GATE"], gate.t[:, :, :].rearrange("p b e -> p (b e)"), reads=[gate.r], writes=[sc["r_GATE"]])
    else:
        route_epilogue(S, A, sc, x1b, gate, gohall, plg, l)
    S.flush()
    A.close()


def route_epilogue(S, A, sc, x1b, gate, gohall, plg, l):
    def T_(name, shape, dt=F32):
        return Buf(A.sb(name, shape, dt))
    onesf = T_("onesf", [128, 128])
    tris = T_("tris", [128, 128])
    S.op("pool", lambda e: e.memset(onesf.t[:], 1.0), writes=[onesf.r])
    S.op("pool", lambda e: e.memset(tris.t[:], 1.0), writes=[tris.r])
    S.op("pool", lambda e: e.affine_select(out=tris.t[:], in_=tris.t[:], pattern=[[1, 128]], compare_op=ALU.is_ge, fill=0.0, base=-1,
                                           channel_multiplier=-1), reads=[tris.r], writes=[tris.r])
    pt, pr = plg[0], plg[1]
    for b in range(NB):
        S.op("pe", lambda e, b=b: e.matmul(pt.t[:, b * 4:(b + 1) * 4], lhsT=onesf.t[:], rhs=gohall.t[:, b, :], start=True, stop=True),
             reads=[onesf.r, gohall.r], writes=[pt.r])
        S.op("pe", lambda e, b=b: e.matmul(pr.t[:, b * 4:(b + 1) * 4], lhsT=tris.t[:], rhs=gohall.t[:, b, :], start=True, stop=True),
             reads=[tris.r, gohall.r], writes=[pr.r])
    totb = T_("totb", [128, NB, 4])
    cum = T_("cumb", [128, NB, 4])
    ones32 = T_("ones32", [128, NB])
    S.op("pool", lambda e: e.memset(ones32.t[:], 1.0), writes=[ones32.r])
    S.op("dve", lambda e: e.tensor_copy(out=totb.t[:, :, :], in_=pt.t[:, 0:NB * 4].rearrange("p (b g) -> p b g", g=4)), reads=[pt.r], writes=[totb.r])
    for g in range(4):
        S.op("dve", lambda e, g=g: e.tensor_tensor_scan(out=cum.t[:, :, g], data0=ones32.t[:, :], data1=totb.t[:, :, g], initial=0.0,
                                                        op0=ALU.mult, op1=ALU.add), reads=[ones32.r, totb.r, cum.r], writes=[cum.r])
    boffx = T_("boffx", [128, NB, 4])
    S.op("dve", lambda e: e.tensor_tensor(out=boffx.t[:], in0=cum.t[:], in1=totb.t[:], op=ALU.subtract), reads=[cum.r, totb.r], writes=[boffx.r])
    thr_i = T_("thri", [128, 16], I32)
    thr = T_("thr", [128, 16])
    S.op("pool", lambda e: e.iota(thr_i.t[:], pattern=[[512, 16]], base=0, channel_multiplier=0), writes=[thr_i.r])
    S.op("dve", lambda e: e.tensor_copy(out=thr.t[:], in_=thr_i.t[:]), reads=[thr_i.r], writes=[thr.r])
    cmp = T_("cmp", [128, 4, 8])
    ntl = T_("ntl", [128, 4])
    S.op("dve", lambda e: e.tensor_tensor(out=cmp.t[:], in0=cum.t[:, NB - 1, :].unsqueeze(2).to_broadcast([128, 4, 8]),
                                          in1=thr.t[:, 0:8].unsqueeze(1).to_broadcast([128, 4, 8]), op=ALU.is_gt), reads=[cum.r, thr.r], writes=[cmp.r])
    S.op("dve", lambda e: e.tensor_reduce(out=ntl.t[:], in_=cmp.t[:], axis=AX.X, op=ALU.add), reads=[cmp.r], writes=[ntl.r])
    S.op("dve", lambda e: e.tensor_scalar(out=ntl.t[:], in0=ntl.t[:], scalar1=512.0, scalar2=None, op0=ALU.mult), reads=[ntl.r], writes=[ntl.r])
    pst = T_("pst", [128, 4])
    pen = T_("pen", [128, 4])
    S.op("pool", lambda e: e.memset(pst.t[:], 0.0), writes=[pst.r])
    for g in range(1, 4):
        S.op("dve", lambda e, g=g: e.tensor_tensor(out=pst.t[:, g:g + 1], in0=pst.t[:, g - 1:g], in1=ntl.t[:, g - 1:g], op=ALU.add),
             reads=[pst.r, ntl.r], writes=[pst.r])
    S.op("dve", lambda e: e.tensor_tensor(out=pen.t[:], in0=pst.t[:], in1=ntl.t[:], op=ALU.add), reads=[pst.r, ntl.r], writes=[pen.r])
    v = T_("vdest", [128, NB, 4])
    S.op("dve", lambda e: e.tensor_tensor(out=v.t[:], in0=pr.t[:, 0:NB * 4].rearrange("p (b g) -> p b g", g=4), in1=boffx.t[:], op=ALU.add),
         reads=[pr.r, boffx.r], writes=[v.r])
    S.op("dve", lambda e: e.tensor_tensor(out=v.t[:], in0=v.t[:], in1=pst.t[:, :].unsqueeze(1).to_broadcast([128, NB, 4]), op=ALU.add),
         reads=[v.r, pst.r], writes=[v.r])
    S.op("dve", lambda e: e.tensor_tensor(out=v.t[:], in0=v.t[:], in1=gohall.t[:], op=ALU.mult), reads=[v.r, gohall.r], writes=[v.r])
    destf = T_("destf", [128, NB])
    desti = T_("desti", [128, NB], I32)
    S.op("dve", lambda e: e.tensor_reduce(out=destf.t[:], in_=v.t[:], axis=AX.X, op=ALU.add), reads=[v.r], writes=[destf.r])
    S.op("dve", lambda e: e.tensor_copy(out=desti.t[:], in_=destf.t[:]), reads=[destf.r], writes=[desti.r])
    S.dma("sp", sc["DEST"], desti.t[:], reads=[desti.r], writes=[sc["r_DEST"]])
    cmp2 = T_("cmp2", [128, NTILE, 4])
    gk = T_("gk", [128, NTILE])
    S.op("dve", lambda e: e.tensor_tensor(out=cmp2.t[:], in0=pen.t[:, :].unsqueeze(1).to_broadcast([128, NTILE, 4]),
                                          in1=thr.t[:, 0:NTILE].unsqueeze(2).to_broadcast([128, NTILE, 4]), op=ALU.is_le), reads=[pen.r, thr.r], writes=[cmp2.r])
    S.op("dve", lambda e: e.tensor_reduce(out=gk.t[:], in_=cmp2.t[:], axis=AX.X, op=ALU.add), reads=[cmp2.r], writes=[gk.r])
    S.op("dve", lambda e: e.tensor_scalar(out=gk.t[:], in0=gk.t[:], scalar1=3.0, scalar2=1024.0, op0=ALU.min, op1=ALU.mult), reads=[gk.r], writes=[gk.r])
    S.op("dve", lambda e: e.tensor_scalar(out=gk.t[:], in0=gk.t[:], scalar1=float(l * 4096), scalar2=None, op0=ALU.add), reads=[gk.r], writes=[gk.r])
    cw_i = T_("cwi", [128, 8], I32)
    cw = T_("cw", [128, 8])
    S.op("pool", lambda e: e.iota(cw_i.t[:], pattern=[[256, 4], [1, 2]], base=0, channel_multiplier=2), writes=[cw_i.r])
    S.op("dve", lambda e: e.tensor_copy(out=cw.t[:], in_=cw_i.t[:]), reads=[cw_i.r], writes=[cw.r])
    idxf = T_("idxf", [128, NTILE, 8])
    idxi = T_("idxi", [128, NTILE, 8], I32)
    S.op("dve", lambda e: e.tensor_tensor(out=idxf.t[:], in0=cw.t[:, :].unsqueeze(1).to_broadcast([128, NTILE, 8]),
                                          in1=gk.t[:, :].unsqueeze(2).to_broadcast([128, NTILE, 8]), op=ALU.add), reads=[cw.r, gk.r], writes=[idxf.r])
    S.op("dve", lambda e: e.tensor_copy(out=idxi.t[:], in_=idxf.t[:]), reads=[idxf.r], writes=[idxi.r])
    S.dma("sp", sc["IDXW"], idxi.t[:, :, :].rearrange("p k j -> p (k j)"), reads=[idxi.r], writes=[sc["r_IDXW"]])
    for b in range(NB):
        S.dma_fn("pool", lambda e, b=b: e.indirect_dma_start(out=sc["XS"][:, :], out_offset=bass.IndirectOffsetOnAxis(ap=desti.t[:, b:b + 1], axis=0),
                                                            in_=x1b.t[:, b, :], in_offset=None), reads=[desti.r, x1b.r], writes=[sc["r_XS"]])
        S.dma_fn("pool", lambda e, b=b: e.indirect_dma_start(out=sc["GS"][:, :], out_offset=bass.IndirectOffsetOnAxis(ap=desti.t[:, b:b + 1], axis=0),
                                                            in_=gate.t[:, b, :], in_offset=None), reads=[desti.r, gate.r], writes=[sc["r_GS"]])


def phase4(nc, S, T, l, sc, xout, r_xout, final, TG=1024):
    A = Alloc(nc)
    g_b = bcast_row(S, A, "ln2g", T["ln2_g"][l], DM)
    b_b = bcast_row(S, A, "ln2b", T["ln2_b"][l], DM)
    gate = Buf(A.sb("gate4", [128, NB, 16], F32))
    S.dma("sp", gate.t[:, :, :].rearrange("p b e -> p (b e)"), sc["GATE"], reads=[sc["r_GATE"]], writes=[gate.r])
    xT = Buf(A.sb("x1T", [128, 8, TG], BF16))
    accb = Buf(A.sb("accm", [128, TG // 128, DM], F32))
    w1 = rot(A, "sb", "w1", [128, 8, DEXP], BF16, 2)
    w3 = rot(A, "sb", "w3", [128, 8, DEXP], BF16, 2)
    w2 = rot(A, "sb", "w2", [128, 4, DM], BF16, 2)
    sa = rot(A, "sb", "sa", [128, 512], F32, 2)
    hT = rot(A, "sb", "hT", [128, 4, 512], BF16, 2)
    xs = rot(A, "sb", "xs4", [128, DM], F32, 2)
    yo = rot(A, "sb", "yo4", [128, DM], F32, 2)
    epsl = Buf(A.sb("epsl4", [128, 1], F32))
    S.op("pool", lambda e: e.memset(epsl.t[:], LN_EPS), writes=[epsl.r])
    small = [dict(st6=Buf(A.sb("st6b", [128, 2, 6], F32)), mv=Buf(A.sb("mvb", [128, 2], F32)), rs=Buf(A.sb("rsb", [128, 1], F32)), eps=epsl) for _ in range(2)]
    pa = rot(A, "ps", "pa", [128, 512], F32, 2)
    pb = rot(A, "ps", "pb", [128, 512], F32, 2)
    py = rot(A, "ps", "py", [128, 512], F32, 3)
    cnt = {"y": 0, "ab": 0, "w": 0}
    W1 = T["w1"][l]
    W3 = T["w3"][l]
    W2 = T["w2"][l]
    for gi in range(SEQ // TG):
        t0 = gi * TG
        S.dma("sp", xT.t[:], sc["X1T"][:, :, t0:t0 + TG], reads=[sc["r_X1T"]], writes=[xT.r])
        for ex in range(NEXP):
            i = cnt["w"]
            cnt["w"] += 1
            a1, a3, a2 = w1[i % 2], w3[i % 2], w2[i % 2]
            S.dma("pool", a1.t[:], W1[ex].rearrange("(c p) n -> p c n", p=128), writes=[a1.r])
            S.dma("pool", a3.t[:], W3[ex].rearrange("(c p) n -> p c n", p=128), writes=[a3.r])
            S.dma("pool", a2.t[:], W2[ex].rearrange("(c p) n -> p c n", p=128), writes=[a2.r])
            for tt in range(TG // 512):
                tc = slice(tt * 512, (tt + 1) * 512)
                h_ = hT[(ex * (TG // 512) + tt) % 2]
                for jc in range(4):
                    k_ = cnt["ab"]
                    cnt["ab"] += 1
                    pa_, pb_, sa_ = pa[k_ % 2], pb[k_ % 2], sa[k_ % 2]
                    for c in range(8):
                        S.op("pe", lambda e, c=c, jc=jc, pa_=pa_, a1=a1, tc=tc: e.matmul(pa_.t[:], lhsT=a1.t[:, c, jc * 128:(jc + 1) * 128], rhs=xT.t[:, c, tc],
                                                                                start=(c == 0), stop=(c == 7)), reads=[a1.r, xT.r], writes=[pa_.r])
                    for c in range(8):
                        S.op("pe", lambda e, c=c, jc=jc, pb_=pb_, a3=a3, tc=tc: e.matmul(pb_.t[:], lhsT=a3.t[:, c, jc * 128:(jc + 1) * 128], rhs=xT.t[:, c, tc],
                                                                                start=(c == 0), stop=(c == 7)), reads=[a3.r, xT.r], writes=[pb_.r])
                    S.op("act", lambda e, pa_=pa_, sa_=sa_: e.activation(out=sa_.t[:], in_=pa_.t[:], func=AF.Silu), reads=[pa_.r], writes=[sa_.r])
                    S.op("dve", lambda e, pb_=pb_, sa_=sa_, h_=h_, jc=jc: e.tensor_tensor(out=h_.t[:, jc, :], in0=pb_.t[:], in1=sa_.t[:], op=ALU.mult),
                         reads=[pb_.r, sa_.r], writes=[h_.r])
                for tb in range(4):
                    blk = tt * 4 + tb
                    gb = (t0 // 128) + blk
                    for hf in range(2):
                        y_ = py[cnt["y"] % 3]
                        cnt["y"] += 1
                        for jc in range(4):
                            S.op("pe", lambda e, jc=jc, tb=tb, hf=hf, y_=y_, h_=h_, a2=a2: e.matmul(y_.t[:], lhsT=h_.t[:, jc, tb * 128:(tb + 1) * 128],
                                                                                           rhs=a2.t[:, jc, hf * 512:(hf + 1) * 512], start=(jc == 0), stop=(jc == 3)),
                                 reads=[h_.r, a2.r], writes=[y_.r])
                        dst = accb.t[:, blk, hf * 512:(hf + 1) * 512]
                        if ex == 0:
                            S.op("dve", lambda e, y_=y_, dst=dst, gb=gb, ex=ex: e.tensor_scalar(out=dst, in0=y_.t[:], scalar1=gate.t[:, gb, ex:ex + 1], scalar2=None, op0=ALU.mult),
                                 reads=[y_.r, gate.r], writes=[accb.r])
                        else:
                            S.op("dve", lambda e, y_=y_, dst=dst, gb=gb, ex=ex: e.scalar_tensor_tensor(out=dst, in0=y_.t[:], scalar=gate.t[:, gb, ex:ex + 1], in1=dst,
                                                                                              op0=ALU.mult, op1=ALU.add), reads=[y_.r, gate.r, accb.r], writes=[accb.r])
        for blk in range(TG // 128):
            gb = (t0 // 128) + blk
            s_, y_, sm = xs[blk % 2], yo[blk % 2], small[blk % 2]
            S.dma("sp", s_.t[:], sc["X1"][gb * 128:(gb + 1) * 128, :], reads=[sc["r_X1"]], writes=[s_.r])
            S.op("dve", lambda e, s_=s_, blk=blk: e.scalar_tensor_tensor(out=s_.t[:], in0=s_.t[:], scalar=ALPHA, in1=accb.t[:, blk, :], op0=ALU.mult, op1=ALU.add),
                 reads=[s_.r, accb.r], writes=[s_.r])
            layernorm_block(S, s_, g_b, b_b, sm, y_)
            S.dma("sp", xout[gb * 128:(gb + 1) * 128, :], y_.t[:], reads=[y_.r], writes=[r_xout], final=final)
    S.flush()
    A.close()


def phase4r(nc, S, T, l, sc, xout, r_xout, final):
    A = Alloc(nc)
    ident = make_ident(A, S, BF16)
    g_b = bcast_row(S, A, "ln2g", T["ln2_g"][l], DM)
    b_b = bcast_row(S, A, "ln2b", T["ln2_b"][l], DM)
    dest = Buf(A.sb("dest4", [128, NB], I32))
    idxw = Buf(A.sb("idxw4", [128, NTILE * 8], I32))
    S.dma("sp", dest.t[:], sc["DEST"], reads=[sc["r_DEST"]], writes=[dest.r])
    S.dma("sp", idxw.t[:], sc["IDXW"], reads=[sc["r_IDXW"]], writes=[idxw.r])
    xs = rot(A, "sb", "xs4r", [128, 4, DM], BF16, 2)
    gs = rot(A, "sb", "gs4r", [128, 4, 16], F32, 2)
    gsel = rot(A, "sb", "gsel", [128, 4, 4], F32, 2)
    xT = rot(A, "sb", "xT4r", [128, 8, 512], BF16, 2)
    accs = rot(A, "sb", "acc4r", [128, 4, DM], F32, 2)
    w1 = rot(A, "sb", "w1r", [128, 8 * DEXP], BF16, 2)
    w3 = rot(A, "sb", "w3r", [128, 8 * DEXP], BF16, 2)
    w2 = rot(A, "sb", "w2r", [128, 4 * DM], BF16, 2)
    sa = rot(A, "sb", "sar", [128, 512], F32, 2)
    hT = rot(A, "sb", "hTr", [128, 4, 512], BF16, 2)
    mt = rot(A, "sb", "mt4", [128, DM], F32, 4)
    xo = rot(A, "sb", "xo4", [128, DM], F32, 4)
    yo = rot(A, "sb", "yo4r", [128, DM], F32, 4)
    epsl = Buf(A.sb("epsl4r", [128, 1], F32))
    S.op("pool", lambda e: e.memset(epsl.t[:], LN_EPS), writes=[epsl.r])
    small = [dict(st6=Buf(A.sb("st6r", [128, 2, 6], F32)), mv=Buf(A.sb("mvr", [128, 2], F32)), rs=Buf(A.sb("rsr", [128, 1], F32)), eps=epsl) for _ in range(4)]
    ptp = rot(A, "ps", "ptp", [128, DM], BF16, 1)
    pa = rot(A, "ps", "par", [128, 512], F32, 2)
    pb = rot(A, "ps", "pbr", [128, 512], F32, 2)
    py = rot(A, "ps", "pyr", [128, 512], F32, 3)
    Wv = [sc["WB"][i][:, :] for i in range(3)]
    cnt = {"y": 0, "ab": 0}
    steps = [(k, j) for k in range(NTILE) for j in range(4)]
    st = {}

    def front(si):
        k, j = steps[si]
        if j == 0:
            x_, g_, gl_, xT_, ac_ = xs[k % 2], gs[k % 2], gsel[k % 2], xT[k % 2], accs[k % 2]
            S.dma("sp", x_.t[:], sc["XS"][k * 512:(k + 1) * 512, :].rearrange("(b p) n -> p b n", p=128), reads=[sc["r_XS"]], writes=[x_.r])
            S.dma("sp", g_.t[:], sc["GS"][k * 512:(k + 1) * 512, :].rearrange("(b p) n -> p b n", p=128), reads=[sc["r_GS"]], writes=[g_.r])
            S.op("dve", lambda e: e.tensor_reduce(out=gl_.t[:], in_=g_.t[:, :, :].rearrange("p b (g j) -> p b j g", g=4), axis=AX.X, op=ALU.add),
                 reads=[g_.r], writes=[gl_.r])
            for blk in range(4):
                p = ptp[0]
                for c in range(8):
                    S.op("pe", lambda e, c=c, blk=blk: e.transpose(out=p.t[:, c * 128:(c + 1) * 128],
                                                                   in_=x_.t[:, blk, :].rearrange("p (pp c) -> p c pp", c=8)[:, c, :], identity=ident.t[:]),
                         reads=[x_.r, ident.r], writes=[p.r])
                S.op("act", lambda e, blk=blk: e.activation(out=xT_.t[:, :, blk * 128:(blk + 1) * 128], in_=p.t[:, :].rearrange("p (c t) -> p c t", c=8), func=AF.Copy),
                     reads=[p.r], writes=[xT_.r])
        xT_, ac_, gl_ = xT[k % 2], accs[k % 2], gsel[k % 2]
        a1, a3, a2, h_ = w1[si % 2], w3[si % 2], w2[si % 2], hT[si % 2]
        for wt_, src in ((a1, Wv[0]), (a3, Wv[1]), (a2, Wv[2])):
            for half in range(2):
                col = k * 8 + j * 2 + half
                S.dma_fn("pool", lambda e, wt_=wt_, src=src, half=half, col=col: e.indirect_dma_start(
                    out=wt_.t[:, half * 2048:(half + 1) * 2048], out_offset=None, in_=src,
                    in_offset=bass.IndirectOffsetOnAxis(ap=idxw.t[:, col:col + 1], axis=0)), reads=[idxw.r, sc["r_WB"][l]], writes=[wt_.r])
        w1v = a1.t[:, :].rearrange("p (c pp q) -> p c q pp", c=8, q=4)
        w3v = a3.t[:, :].rearrange("p (c pp q) -> p c q pp", c=8, q=4)
        for jc in range(4):
            k_ = cnt["ab"]
            cnt["ab"] += 1
            pa_, pb_, sa_ = pa[k_ % 2], pb[k_ % 2], sa[k_ % 2]
            for c in range(8):
                S.op("pe", lambda e, c=c, jc=jc, pa_=pa_: e.matmul(pa_.t[:], lhsT=w1v[:, c, jc, :], rhs=xT_.t[:, c, :], start=(c == 0), stop=(c == 7)),
                     reads=[a1.r, xT_.r], writes=[pa_.r])
            for c in range(8):
                S.op("pe", lambda e, c=c, jc=jc, pb_=pb_: e.matmul(pb_.t[:], lhsT=w3v[:, c, jc, :], rhs=xT_.t[:, c, :], start=(c == 0), stop=(c == 7)),
                     reads=[a3.r, xT_.r], writes=[pb_.r])
            S.op("act", lambda e, pa_=pa_, sa_=sa_: e.activation(out=sa_.t[:], in_=pa_.t[:], func=AF.Silu), reads=[pa_.r], writes=[sa_.r])
            S.op("dve", lambda e, pb_=pb_, sa_=sa_, jc=jc: e.tensor_tensor(out=h_.t[:, jc, :], in0=pb_.t[:], in1=sa_.t[:], op=ALU.mult),
                 reads=[pb_.r, sa_.r], writes=[h_.r])

    def back(si):
        k, j = steps[si]
        ac_, gl_, a2, h_ = accs[k % 2], gsel[k % 2], w2[si % 2], hT[si % 2]
        w2v = a2.t[:, :].rearrange("p (c n) -> p c n", c=4)
        for blk in range(4):
            for hf in range(2):
                y_ = py[cnt["y"] % 3]
                cnt["y"] += 1
                for jc in range(4):
                    S.op("pe", lambda e, jc=jc, blk=blk, hf=hf, y_=y_: e.matmul(y_.t[:], lhsT=h_.t[:, jc, blk * 128:(blk + 1) * 128],
                                                                               rhs=w2v[:, jc, hf * 512:(hf + 1) * 512], start=(jc == 0), stop=(jc == 3)),
                         reads=[h_.r, a2.r], writes=[y_.r])
                dst = ac_.t[:, blk, hf * 512:(hf + 1) * 512]
                if j == 0:
                    S.op("dve", lambda e, y_=y_, dst=dst, blk=blk: e.tensor_scalar(out=dst, in0=y_.t[:], scalar1=gl_.t[:, blk, j:j + 1], scalar2=None, op0=ALU.mult),
                         reads=[y_.r, gl_.r], writes=[ac_.r])
                else:
                    S.op("dve", lambda e, y_=y_, dst=dst, blk=blk: e.scalar_tensor_tensor(out=dst, in0=y_.t[:], scalar=gl_.t[:, blk, j:j + 1], in1=dst,
                                                                                         op0=ALU.mult, op1=ALU.add), reads=[y_.r, gl_.r, ac_.r], writes=[ac_.r])
        if j == 3:
            S.dma("sp", sc["YS"][k * 512:(k + 1) * 512, :].rearrange("(b p) n -> p b n", p=128), ac_.t[:], reads=[ac_.r], writes=[sc["r_YS"]])

    for si in range(len(steps) + 1):
        if si < len(steps):
            front(si)
        if si >= 1:
            back(si - 1)
    def comb_load(b):
        m_, x_ = mt[b % 4], xo[b % 4]
        S.dma_fn("pool", lambda e: e.indirect_dma_start(out=m_.t[:], out_offset=None, in_=sc["YS"][:, :],
                                                        in_offset=bass.IndirectOffsetOnAxis(ap=dest.t[:, b:b + 1], axis=0)),
                 reads=[dest.r, sc["r_YS"]], writes=[m_.r])
        S.dma("sp", x_.t[:], sc["X1"][b * 128:(b + 1) * 128, :], reads=[sc["r_X1"]], writes=[x_.r])
    for b in range(min(3, NB)):
        comb_load(b)
    for b in range(NB):
        m_, x_, y_, sm = mt[b % 4], xo[b % 4], yo[b % 4], small[b % 4]
        S.op("dve", lambda e, m_=m_, x_=x_: e.scalar_tensor_tensor(out=x_.t[:], in0=x_.t[:], scalar=ALPHA, in1=m_.t[:], op0=ALU.mult, op1=ALU.add),
             reads=[x_.r, m_.r], writes=[x_.r])
        layernorm_block(S, x_, g_b, b_b, sm, y_)
        if b + 3 < NB:
            comb_load(b + 3)
        S.dma("sp", xout[b * 128:(b + 1) * 128, :], y_.t[:], reads=[y_.r], writes=[r_xout], final=final)
    S.flush()
    A.close()


def rope_tables():
    pos = np.arange(SEQ, dtype=np.float32)
    out = {}
    for dim, nm in ((64, "rope64"), (32, "rope32")):
        half = dim // 2
        inv = (10000.0 ** (-np.arange(half, dtype=np.float32) / half)).astype(np.float32)
        ang = pos[None, :] * inv[:, None]
        c = np.cos(ang).astype(np.float32)
        s = np.sin(ang).astype(np.float32)
        out[nm + "c"] = np.ascontiguousarray(np.concatenate([c, c], 0))
        out[nm + "s"] = np.ascontiguousarray(np.concatenate([-s, s], 0))
    return out


W_SPECS = [("w_in", [DEPTH, DM, N_IN]), ("b_forget", [DEPTH, 4]), ("g_cq", [DEPTH, 256]), ("g_ckv", [DEPTH, 128]), ("w_uq", [DEPTH, 256, 384]),
           ("w_ukv", [DEPTH, 128, 512]), ("g_head", [DEPTH, 16, 64]), ("w_out", [DEPTH, DM, DM]), ("ln1_g", [DEPTH, DM]), ("ln1_b", [DEPTH, DM]),
           ("w_group", [DEPTH, DM, 4]), ("b_group", [DEPTH, 4]), ("w_expert", [DEPTH, DM, 16]), ("b_expert", [DEPTH, 16]),
           ("w1", [DEPTH, NEXP, DM, DEXP]), ("w3", [DEPTH, NEXP, DM, DEXP]), ("w2", [DEPTH, NEXP, DEXP, DM]), ("ln2_g", [DEPTH, DM]), ("ln2_b", [DEPTH, DM])]


def build_program(nseq=2, layers=(0, 1), phases=(1, 2, 3, 4), debug=False, heads=None, TG=1024):
    nc = bass.Bass("TRN2", target_bir_lowering=False)
    T = {}
    T["x"] = nc.dram_tensor("x", [nseq, SEQ, DM], F32, kind="ExternalInput").ap()
    for nm, shp in W_SPECS:
        T[nm] = nc.dram_tensor(nm, shp, F32, kind="ExternalInput").ap()
    for nm, rows in (("rope64c", 64), ("rope64s", 64), ("rope32c", 32), ("rope32s", 32)):
        T[nm] = nc.dram_tensor(nm, [rows, SEQ], F32, kind="ExternalInput").ap()
    out = nc.dram_tensor("out", [nseq, SEQ, DM], F32, kind="ExternalOutput").ap()
    dk = "ExternalOutput" if debug else "Internal"

    def scratch(name, shape, dt):
        return nc.dram_tensor(name, shape, dt, kind=dk).ap()
    sc = {}
    qs = scratch("QS", [12, 96, SEQ], BF16)
    ks = scratch("KS", [12, 96, SEQ], BF16)
    sc["QS"] = [qs[h] for h in range(12)]
    sc["KS"] = [ks[h] for h in range(12)]
    vs = scratch("VS", [6, 128, NB * 2 * 65], BF16)
    sc["VS"] = [vs[p] for p in range(6)]
    qd = scratch("QD", [3, 4, 64, SEQ], BF16)
    kd = scratch("KD", [3, 4, 64, SEQ], BF16)
    sc["QD"] = [[qd[g, h] for h in range(4)] for g in range(3)]
    sc["KD"] = [[kd[g, h] for h in range(4)] for g in range(3)]
    vd = scratch("VD", [3, 2, 128, NB * 2 * 65], BF16)
    sc["VD"] = [[vd[g, p] for p in range(2)] for g in range(3)]
    sc["OnT"] = scratch("OnT", [8, 128, SEQ], BF16)
    sc["X1"] = scratch("X1", [SEQ, DM], F32)
    sc["X1T"] = scratch("X1T", [128, 8, SEQ], BF16)
    sc["GATE"] = scratch("GATE", [128, NB * 16], F32)
    sc["XS"] = scratch("XS", [NSLOT, DM], BF16)
    sc["GS"] = scratch("GS", [NSLOT, 16], F32)
    sc["YS"] = scratch("YS", [NSLOT, DM], F32)
    sc["DEST"] = scratch("DEST", [128, NB], I32)
    sc["IDXW"] = scratch("IDXW", [128, NTILE * 8], I32)
    sc["WB"] = [scratch("WB%d" % i, [DEPTH * 4096, 2048], BF16) for i in range(3)]
    sc["r_WB"] = [Res() for _ in range(DEPTH)]
    xmid = scratch("XMID", [SEQ, DM], F32)
    sc["r_QS"] = [Res() for _ in range(12)]
    sc["r_KS"] = [Res() for _ in range(12)]
    sc["r_VS"] = [Res() for _ in range(6)]
    sc["r_QD"] = [[Res() for _ in range(4)] for _ in range(3)]
    sc["r_KD"] = [[Res() for _ in range(4)] for _ in range(3)]
    sc["r_VD"] = [[Res() for _ in range(2)] for _ in range(3)]
    for k in ("OnT", "X1", "X1T", "GATE", "XS", "GS", "YS", "DEST", "IDXW"):
        sc["r_" + k] = Res()
    r_xmid = Res()
    r_x = Res()
    r_out = Res()
    S = Sched(nc)
    for s in range(nseq):
        for li, l in enumerate(layers):
            xin, r_xin = (T["x"][s], r_x) if li == 0 else (xmid, r_xmid)
            last = li == len(layers) - 1
            xo, r_xo = (out[s], r_out) if last else (xmid, r_xmid)
            if 1 in phases:
                phase1(nc, S, T, xin, r_xin, l, sc)
            if 2 in phases:
                cv = (lambda l=l: convert_expert_weights(S, T, sc, l)) if (ROUTED and s == 0) else None
                phase2(nc, S, T, l, sc, heads=heads, after_sb=cv)
            if 3 in phases:
                phase3(nc, S, T, xin, r_xin, l, sc)
            if 4 in phases:
                if ROUTED:
                    phase4r(nc, S, T, l, sc, xo, r_xo, final=last)
                else:
                    phase4(nc, S, T, l, sc, xo, r_xo, final=last, TG=TG)
    S.close()
    return nc, S


def convert_expert_weights(S, T, sc, l):
    for i, nm in enumerate(("w1", "w3", "w2")):
        src = T[nm][l].rearrange("e k n -> (e k n)").rearrange("(r x) -> r x", x=2048)
        for ch in range(8):
            S.dma("pool", sc["WB"][i][l * 4096 + ch * 512:l * 4096 + (ch + 1) * 512, :], src[ch * 512:(ch + 1) * 512, :], writes=[sc["r_WB"][l]])


_CACHE = {}


def kernel(**inputs):
    n = 8
    nseq = 2
    x = np.ascontiguousarray(np.asarray(inputs["x"], dtype=np.float32))
    tabs = rope_tables()
    if "nc" not in _CACHE:
        _CACHE["nc"] = build_program(nseq=nseq)[0]
    nc = _CACHE["nc"]
    base = {nm: np.ascontiguousarray(np.asarray(inputs[nm], dtype=np.float32)) for nm, _ in W_SPECS}
    base.update(tabs)
    in_maps = []
    for c in range(n):
        m = dict(base)
        m["x"] = x[c * nseq:(c + 1) * nseq]
        in_maps.append(m)
    res = run_bass_kernel_spmd(nc, in_maps, core_ids=list(range(n)))
    return np.concatenate([r["out"] for r in res.results], axis=0).astype(np.float32)
```

```python
import numpy as np
from os import environ as _os_env
import concourse.bass as bass
import concourse.mybir as mybir
from concourse.bass_utils import run_bass_kernel_spmd

F32 = mybir.dt.float32
BF16 = mybir.dt.bfloat16
I32 = mybir.dt.int32
AF = mybir.ActivationFunctionType
ALU = mybir.AluOpType
AX = mybir.AxisListType

SAME_ENGINE_SYNC = bool(int(_os_env.get("SES", "1")))
NDMA_SLOTS = int(_os_env.get("NSLOTS", "8"))

SEQ = 4096
DM = 1024
NB = SEQ // 128
NT = SEQ // 512
DEPTH = 2
ALPHA = (2.0 * DEPTH) ** 0.25
LN_EPS = 1e-5
RMS_EPS = 1e-6
N_IN = 4260
C_SB, C_FOX, C_MLA, C_DIL = 0, 768, 1540, 1956
MLA_SCALE = 96.0 ** -0.5
DIL_R = (1, 4, 16)
NEXP = 16
DEXP = 512
NTILE = 11
NSLOT = NTILE * 512
ROUTED = bool(int(_os_env.get("ROUTED", "1")))


class Res:
    __slots__ = ("w", "r", "excl")

    def __init__(self, excl=False):
        self.w = None
        self.r = []
        self.excl = excl


class Sched:
    ENG = ("pe", "act", "dve", "pool", "sp")

    def __init__(self, nc):
        self.nc = nc
        self.ops = {e: [] for e in self.ENG}
        self.cnt = {e: 0 for e in self.ENG}
        self.seen = {e: {} for e in self.ENG}
        self.sems = {}
        self.dma_slots = {}
        self.dma_rr = {}
        self.final_waits = []
        self._stack = []
        self.nops = 0

    def sem(self, key):
        if key not in self.sems:
            cm = self.nc.semaphore("s_" + "_".join(str(k) for k in (key if isinstance(key, tuple) else (key,))))
            s = cm.__enter__()
            self._stack.append(cm)
            self.sems[key] = s
        return self.sems[key]

    def _deps(self, eng, reads, writes):
        deps = {}

        def add(t):
            if t is None:
                return
            k, v = t
            if deps.get(k, 0) < v:
                deps[k] = v
        for r in reads:
            add(r.w)
            if r.excl:
                for t in r.r:
                    if t[0] != eng:
                        add(t)
        for w in writes:
            add(w.w)
            for t in w.r:
                add(t)
        waits = []
        seen = self.seen[eng]
        for k, v in deps.items():
            if k == eng and (eng == "pe" or not SAME_ENGINE_SYNC):
                continue
            if seen.get(k, 0) >= v:
                continue
            seen[k] = v
            waits.append((k, v))
        return waits

    def _commit(self, ticket, reads, writes):
        for r in reads:
            if len(r.r) > 16:
                m = {}
                for k, v in r.r:
                    if m.get(k, 0) < v:
                        m[k] = v
                r.r = list(m.items())
            r.r.append(ticket)
        for w in writes:
            w.w = ticket
            w.r = []

    def op(self, eng, fn, reads=(), writes=()):
        waits = self._deps(eng, reads, writes)
        self.cnt[eng] += 1
        ticket = (eng, self.cnt[eng])
        self.ops[eng].append((waits, fn, (eng, 1)))
        self._commit(ticket, reads, writes)
        self.nops += 1
        return ticket

    def dma(self, q, out, in_, reads=(), writes=(), final=False, **kw):
        fn = lambda e, out=out, in_=in_, kw=kw: e.dma_start(out=out, in_=in_, **kw)
        return self.dma_fn(q, fn, reads, writes, final)

    def dma_fn(self, q, fn, reads=(), writes=(), final=False):
        waits = self._deps(q, reads, writes)
        if q not in self.dma_slots:
            self.dma_slots[q] = [[("d", q, i), 0] for i in range(NDMA_SLOTS)]
            self.dma_rr[q] = 0
        i = self.dma_rr[q]
        self.dma_rr[q] = (i + 1) % NDMA_SLOTS
        slot = self.dma_slots[q][i]
        key, tot = slot
        if tot > 0 and self.seen[q].get(key, 0) < tot:
            self.seen[q][key] = tot
            waits.append((key, tot))
        slot[1] = tot + 16
        ticket = (key, tot + 16)
        self.ops[q].append((waits, fn, (key, 16)))
        self._commit(ticket, reads, writes)
        self.nops += 1
        if final:
            self.final_waits.append(ticket)
        return ticket

    def flush(self):
        totals = [(e, self.cnt[e]) for e in self.ENG if self.cnt[e] > 0]
        for q, slots in self.dma_slots.items():
            for key, tot in slots:
                if tot > 0:
                    totals.append((key, tot))
        for e in self.ENG:
            self.sem(e)
            for waits, fn, inc in self.ops[e]:
                for k, v in waits:
                    self.sem(k)
                self.sem(inc[0])
        ops = self.ops
        self.ops = {e: [] for e in self.ENG}
        for e in self.ENG:
            for k, v in totals:
                self.seen[e][k] = max(self.seen[e].get(k, 0), v)
        with self.nc.Block() as block:
            def run(engname):
                def body(e):
                    for waits, fn, inc in ops[engname]:
                        for k, v in waits:
                            e.wait_ge(self.sems[k], v)
                        fn(e).then_inc(self.sems[inc[0]], inc[1])
                    for k, v in totals:
                        e.wait_ge(self.sems[k], v)
                return body
            block.sync(run("sp"))
            block.tensor(run("pe"))
            block.scalar(run("act"))
            block.vector(run("dve"))
            block.gpsimd(run("pool"))

    def close(self):
        for cm in reversed(self._stack):
            cm.__exit__(None, None, None)
        self._stack = []


_UID = [0]


class Alloc:
    def __init__(self, nc):
        self.nc = nc
        self.stack = []

    @property
    def n(self):
        _UID[0] += 1
        return _UID[0]

    def sb(self, name, shape, dt):
        cm = self.nc.sbuf_tensor("%s_%d" % (name, self.n), list(shape), dt)
        t = cm.__enter__()
        self.stack.append(cm)
        return t

    def ps(self, name, shape, dt):
        cm = self.nc.psum_tensor("%s_%d" % (name, self.n), list(shape), dt)
        t = cm.__enter__()
        self.stack.append(cm)
        return t

    def close(self):
        for cm in reversed(self.stack):
            cm.__exit__(None, None, None)
        self.stack = []


class Buf:
    def __init__(self, t, excl=False):
        self.t = t
        self.r = Res(excl)


def rot(A, kind, name, shape, dt, n):
    f = A.sb if kind == "sb" else A.ps
    return [Buf(f(name + str(i), shape, dt), excl=(kind == "ps")) for i in range(n)]


def make_ident(A, S, dt):
    b = Buf(A.sb("ident", [128, 128], dt))
    S.op("pool", lambda e: e.memset(b.t[:], 1.0), writes=[b.r])
    S.op("pool", lambda e: e.affine_select(out=b.t[:], in_=b.t[:], pattern=[[-1, 128]], compare_op=ALU.is_equal,
                                           fill=0.0, base=0, channel_multiplier=1), reads=[b.r], writes=[b.r])
    return b


def perm_view(ap2d, r, t0, n):
    if r == 1:
        return ap2d[:, t0:t0 + n]
    sc = SEQ // r
    v = ap2d.rearrange("p (i c) -> p c i", c=r)
    c0, i0 = t0 // sc, t0 % sc
    if i0 + n <= sc:
        return v[:, c0, i0:i0 + n]
    assert i0 == 0 and n % sc == 0
    return v[:, c0:c0 + n // sc, :]


import os as _os
_P1SEC = _os.environ.get("P1SEC", "abcd")
_MSUB = _os.environ.get("MSUB", "qkrvptc")
_MC = _os.environ.get("MC", "1234")


def phase1(nc, S, T, xin, r_xin, l, sc):
    A = Alloc(nc)
    ident = make_ident(A, S, BF16)
    xT = Buf(A.sb("xT", [128, 8, SEQ], BF16))
    xs = rot(A, "sb", "xs", [128, DM], F32, 4)
    xb = rot(A, "sb", "xb", [128, DM], BF16, 3)
    pst = rot(A, "ps", "pst", [128, DM], BF16, 2)
    pj = rot(A, "ps", "pj", [128, 512], F32, 4)
    pv = rot(A, "ps", "pv", [128, 512], F32, 2)
    Win = T["w_in"][l].rearrange("(c p) n -> p c n", p=128)

    for b in range(NB):
        s, c_, p = xs[b % 4], xb[b % 3], pst[b % 2]
        S.dma("sp", s.t[:], xin[b * 128:(b + 1) * 128, :], reads=[r_xin], writes=[s.r])
        S.op("act", lambda e, s=s, c_=c_: e.activation(out=c_.t[:], in_=s.t[:], func=AF.Copy), reads=[s.r], writes=[c_.r])
        for c in range(8):
            S.op("pe", lambda e, c=c, c_=c_, p=p: e.transpose(out=p.t[:, c * 128:(c + 1) * 128], in_=c_.t[:, c * 128:(c + 1) * 128],
                                                              identity=ident.t[:]), reads=[c_.r, ident.r], writes=[p.r])
        S.op("dve", lambda e, b=b, p=p: e.tensor_copy(out=xT.t[:, :, b * 128:(b + 1) * 128],
                                                      in_=p.t[:, :].rearrange("p (c t) -> p c t", c=8)), reads=[p.r], writes=[xT.r])

    wts = rot(A, "sb", "wt", [128, 8, 416], BF16, 2)
    wsw = rot(A, "sb", "wsw", [128, 8, 256], BF16, 2)
    stg = rot(A, "sb", "stg", [128, 512], BF16, 4)
    vst = rot(A, "sb", "vst", [128, NB, 2, 65], BF16, 2)
    tmp1 = rot(A, "sb", "tmp1", [128, 512], F32, 2)
    tmp2 = rot(A, "sb", "tmp2", [128, 512], F32, 2)
    CT = Buf(A.sb("ropeC", [128, SEQ], BF16))
    ST = Buf(A.sb("ropeS", [128, SEQ], BF16))
    for v in vst:
        S.op("pool", lambda e, v=v: e.memset(v.t[:], 1.0), writes=[v.r])
    state = {"w": 0, "pj": 0, "stg": 0, "v": 0, "pv": 0}

    def load_w(col_ranges):
        w = wts[state["w"] % 2]
        state["w"] += 1
        o = 0
        for (c0, n) in col_ranges:
            S.dma("pool", w.t[:, :, o:o + n], Win[:, :, c0:c0 + n], writes=[w.r])
            o += n
        return w

    def nxt(key, lst):
        b = lst[state[key] % len(lst)]
        state[key] += 1
        return b

    def proj_fm(w, o, M, r, j, wtile=None):
        p = nxt("pj", pj)
        wt_ = w if wtile is None else wtile
        for c in range(8):
            S.op("pe", lambda e, c=c, p=p, wt_=wt_: e.matmul(p.t[0:M, :], lhsT=wt_.t[:, c, o:o + M],
                                                             rhs=perm_view(xT.t[:, c, :], r, j * 512, 512),
                                                             start=(c == 0), stop=(c == 7)),
                 reads=[wt_.r, xT.r], writes=[p.r])
        return p

    def store_rows(st, rows, dst, r_dst, j):
        S.dma("sp", dst[:, j * 512:(j + 1) * 512], st.t[rows[0]:rows[1], :], reads=[st.r], writes=[r_dst])

    def v_proj(w, o, r, vt, ncol=128):
        for b4 in range(NB // 4):
            p = nxt("pv", pv)
            for bb in range(4):
                b = b4 * 4 + bb
                for c in range(8):
                    S.op("pe", lambda e, c=c, b=b, bb=bb, p=p: e.matmul(p.t[:, bb * 128:(bb + 1) * 128],
                                                                      lhsT=perm_view(xT.t[:, c, :], r, b * 128, 128),
                                                                      rhs=w.t[:, c, o:o + ncol], start=(c == 0), stop=(c == 7)),
                         reads=[w.r, xT.r], writes=[p.r])
            S.op("dve", lambda e, b4=b4, p=p: e.tensor_copy(
                out=vt.t[:, b4 * 4:(b4 + 1) * 4, :, 0:64],
                in_=p.t[:, :].rearrange("p (b h d) -> p b h d", b=4, h=2)), reads=[p.r], writes=[vt.r])

    def scale_q(w):
        S.op("dve", lambda e: e.tensor_scalar(out=w.t[:, :, 0:128], in0=w.t[:, :, 0:128], scalar1=0.125, scalar2=None,
                                              op0=ALU.mult), reads=[w.r], writes=[w.r])

    for kind, cbase, hbase, pbase in (("sb", C_SB, 0, 0), ("fox", C_FOX, 4, 2)):
        for hp in range(2):
            w = load_w([(cbase + hp * 128, 128), (cbase + 256 + hp * 128, 128), (cbase + 512 + hp * 128, 128)])
            scale_q(w)
            for qk, dst, rd in ((0, sc["QS"], sc["r_QS"]), (1, sc["KS"], sc["r_KS"])):
                for j in range(NT):
                    p = proj_fm(w, qk * 128, 128, 1, j)
                    st = nxt("stg", stg)
                    S.op("act", lambda e, p=p, st=st: e.activation(out=st.t[:], in_=p.t[:], func=AF.Copy), reads=[p.r], writes=[st.r])
                    for hh in range(2):
                        h = hbase + hp * 2 + hh
                        store_rows(st, (hh * 64, hh * 64 + 64), dst[h][0:64, :], rd[h], j)
            vt = nxt("v", vst)
            v_proj(w, 256, 1, vt)
            S.dma("sp", sc["VS"][pbase + hp], vt.t[:, :, :, :].rearrange("p b h d -> p (b h d)"), reads=[vt.r], writes=[sc["r_VS"][pbase + hp]])

    S.flush()
    if "b" not in _P1SEC:
        A.close()
        return
    A2 = Alloc(nc)
    wf = Buf(A2.sb("wf", [128, 8, 4], BF16))
    S.dma("pool", wf.t[:], Win[:, :, C_FOX + 768:C_FOX + 772], writes=[wf.r])
    bfg = Buf(A2.sb("bfg", [4, 1], F32))
    S.dma("sp", bfg.t[:], T["b_forget"][l].rearrange("(h o) -> h o", o=1), writes=[bfg.r])
    S.op("dve", lambda e: e.tensor_scalar(out=bfg.t[:], in0=bfg.t[:], scalar1=-1.0, scalar2=None, op0=ALU.mult), reads=[bfg.r], writes=[bfg.r])
    nlf = Buf(A2.sb("nlf", [4, SEQ], F32))
    ncum = Buf(A2.sb("ncum", [4, SEQ], F32))
    ones4 = Buf(A2.sb("ones4", [4, 512], F32))
    S.op("pool", lambda e: e.memset(ones4.t[:], 1.0), writes=[ones4.r])
    for j in range(NT):
        p = proj_fm(wf, 0, 4, 1, j)
        S.op("act", lambda e, p=p, j=j: e.activation(out=nlf.t[:, j * 512:(j + 1) * 512], in_=p.t[0:4, :], func=AF.Exp,
                                                     bias=bfg.t[:, 0:1], scale=-1.0), reads=[p.r, bfg.r], writes=[nlf.r])
    S.op("act", lambda e: e.activation(out=nlf.t[:], in_=nlf.t[:], func=AF.Ln, bias=1.0), reads=[nlf.r], writes=[nlf.r])
    for j in range(NT):
        sl = slice(j * 512, (j + 1) * 512)
        init = 0.0 if j == 0 else ncum.t[:, j * 512 - 1:j * 512]
        S.op("dve", lambda e, sl=sl, init=init: e.tensor_tensor_scan(out=ncum.t[:, sl], data0=ones4.t[:, :], data1=nlf.t[:, sl],
                                                                     initial=init, op0=ALU.mult, op1=ALU.add),
             reads=[ones4.r, nlf.r, ncum.r], writes=[ncum.r])
    class _Alias:
        def __init__(self, t, r):
            self.t, self.r = t, r
    nlf_b = nlf.t[:, :].bitcast(BF16)
    parts = [_Alias(nlf_b[:, 0:SEQ], nlf.r), _Alias(nlf_b[:, SEQ:2 * SEQ], nlf.r), Buf(A2.sb("cpart2", [4, SEQ], BF16))]
    for i in range(3):
        S.op("dve", lambda e, i=i: e.tensor_copy(out=parts[i].t[:], in_=ncum.t[:]), reads=[ncum.r], writes=[parts[i].r])
        if i < 2:
            S.op("dve", lambda e, i=i: e.tensor_tensor(out=ncum.t[:], in0=ncum.t[:], in1=parts[i].t[:], op=ALU.subtract),
                 reads=[ncum.r, parts[i].r], writes=[ncum.r])
    ones3 = Buf(A2.sb("ones3", [35, SEQ], BF16))
    S.op("pool", lambda e: e.memset(ones3.t[0:3, :], 1.0), writes=[ones3.r])
    S.op("pool", lambda e: e.memset(ones3.t[32:35, :], -1.0), writes=[ones3.r])
    for h in range(4):
        H = 4 + h
        S.dma("sp", sc["KS"][H][64:67, :], ones3.t[32:35, :], reads=[ones3.r], writes=[sc["r_KS"][H]])
        S.dma("sp", sc["QS"][H][67:70, :], ones3.t[0:3, :], reads=[ones3.r], writes=[sc["r_QS"][H]])
        for i in range(3):
            S.dma("sp", sc["KS"][H][67 + i:68 + i, :], parts[i].t[h:h + 1, :], reads=[parts[i].r], writes=[sc["r_KS"][H]])
            S.dma("sp", sc["QS"][H][64 + i:65 + i, :], parts[i].t[h:h + 1, :], reads=[parts[i].r], writes=[sc["r_QS"][H]])
    S.flush()
    A2.close()

    if "c" not in _P1SEC:
        A.close()
        return
    w = load_w([(C_MLA, 416)])
    wkrs = nxt("w", wsw) if False else wsw[0]
    if "p" in _MSUB:
        S.op("pool", lambda e: e.tensor_copy(out=wkrs.t[:, :, 0:16], in_=w.t[:, :, 400:416]), reads=[w.r], writes=[wkrs.r])
        S.op("pool", lambda e: e.tensor_copy(out=wkrs.t[:, :, 16:32], in_=w.t[:, :, 384:400]), reads=[w.r], writes=[wkrs.r])
    A3 = Alloc(nc)
    wuq = Buf(A3.sb("wuq", [128, 2, 384], BF16))
    wuqs = Buf(A3.sb("wuqs", [128, 2, 384], BF16))
    wukv = Buf(A3.sb("wukv", [128, 512], BF16))
    S.dma("pool", wuq.t[:], T["w_uq"][l].rearrange("(c p) n -> p c n", p=128), writes=[wuq.r])
    S.dma("pool", wukv.t[:], T["w_ukv"][l], writes=[wukv.r])
    S.op("pool", lambda e: e.tensor_copy(out=wuqs.t[:], in_=wuq.t[:]), reads=[wuq.r], writes=[wuqs.r])
    for c2 in (range(2) if "p" in _MSUB else []):
        v4o = wuqs.t[:, c2, :].rearrange("p (h d) -> p h d", h=4)
        v4i = wuq.t[:, c2, :].rearrange("p (h d) -> p h d", h=4)
        S.op("pool", lambda e, v4o=v4o, v4i=v4i: e.tensor_copy(out=v4o[:, :, 64:80], in_=v4i[:, :, 80:96]), reads=[wuq.r, wuqs.r], writes=[wuqs.r])
        S.op("pool", lambda e, v4o=v4o, v4i=v4i: e.tensor_copy(out=v4o[:, :, 80:96], in_=v4i[:, :, 64:80]), reads=[wuq.r, wuqs.r], writes=[wuqs.r])
    gcq = Buf(A3.sb("gcq", [128, 2], F32))
    gckv = Buf(A3.sb("gckv", [128, 1], F32))
    for c2 in range(2):
        S.dma("sp", gcq.t[:, c2:c2 + 1], T["g_cq"][l][c2 * 128:(c2 + 1) * 128].rearrange("(p o) -> p o", o=1), writes=[gcq.r])
    S.dma("sp", gckv.t[:], T["g_ckv"][l].rearrange("(p o) -> p o", o=1), writes=[gckv.r])
    for tb, nm in (((CT, "rope32c"), (ST, "rope32s")) if "t" in _MSUB else []):
        S.dma("pool", tb.t[0:32, :], T[nm], writes=[tb.r])
        S.dma("pool", tb.t[64:96, :], T[nm], writes=[tb.r])
    onesq = Buf(A3.sb("onesq", [128, 128], BF16))
    oneskv = Buf(A3.sb("oneskv", [128, 128], BF16))
    epst = Buf(A3.sb("epst", [128, 1], F32))
    S.op("pool", lambda e: e.memset(epst.t[:], RMS_EPS), writes=[epst.r])
    S.op("pool", lambda e: e.memset(onesq.t[:], 1.0 / 256.0), writes=[onesq.r])
    S.op("pool", lambda e: e.memset(oneskv.t[:], 1.0 / 128.0), writes=[oneskv.r])
    cqg = rot(A3, "sb", "cqg", [128, 2, 512], BF16, 2)
    cq2 = rot(A3, "sb", "cq2", [128, 2, 512], BF16, 2)
    ckg = rot(A3, "sb", "ckg", [128, 512], BF16, 2)
    ck2 = rot(A3, "sb", "ck2", [128, 512], BF16, 2)
    rq = rot(A3, "sb", "rq", [128, 512], F32, 2)
    rkv = rot(A3, "sb", "rkv", [128, 512], F32, 2)
    rtok = rot(A3, "sb", "rtok", [128, 1], F32, 2)
    vstm = Buf(A3.sb("vstm", [128, NB, 4, 65], BF16))
    S.op("pool", lambda e: e.memset(vstm.t[:], 1.0), writes=[vstm.r])
    wukv_v = wukv.t[:, :].rearrange("p (h x) -> p h x", h=4)[:, :, 64:128]
    for j in (range(NT) if "c" in _MSUB else []):
        tc = slice(j * 512, (j + 1) * 512)
        a, a2, kg, k2, rq_, rkv_ = cqg[j % 2], cq2[j % 2], ckg[j % 2], ck2[j % 2], rq[j % 2], rkv[j % 2]
        for c2 in (range(2) if "1" in _MC else []):
            p = proj_fm(w, c2 * 128, 128, 1, j)
            S.op("dve", lambda e, p=p, c2=c2, a=a: e.tensor_scalar(out=a.t[:, c2, :], in0=p.t[:], scalar1=gcq.t[:, c2:c2 + 1], scalar2=None,
                                                                  op0=ALU.mult), reads=[p.r, gcq.r], writes=[a.r])
            S.op("act", lambda e, p=p, c2=c2, a2=a2: e.activation(out=a2.t[:, c2, :], in_=p.t[:], func=AF.Square), reads=[p.r], writes=[a2.r])
        if "2" in _MC:
            p = proj_fm(w, 256, 128, 1, j)
            if "5" not in _MC:
                S.op("dve", lambda e, p=p, kg=kg: e.tensor_scalar(out=kg.t[:], in0=p.t[:], scalar1=gckv.t[:, 0:1], scalar2=None, op0=ALU.mult),
                     reads=[p.r, gckv.r], writes=[kg.r])
            if "6" not in _MC:
                S.op("act", lambda e, p=p, k2=k2: e.activation(out=k2.t[:], in_=p.t[:], func=AF.Square), reads=[p.r], writes=[k2.r])
        if "3" in _MC:
            p = nxt("pj", pj)
            for c2 in range(2):
                S.op("pe", lambda e, p=p, c2=c2, a2=a2: e.matmul(p.t[:], lhsT=onesq.t[:], rhs=a2.t[:, c2, :], start=(c2 == 0), stop=(c2 == 1)),
                     reads=[onesq.r, a2.r], writes=[p.r])
            S.op("act", lambda e, p=p, rq_=rq_: e.activation(out=rq_.t[:], in_=p.t[:], func=AF.Sqrt, bias=epst.t[:, 0:1]), reads=[p.r, epst.r], writes=[rq_.r])
            S.op("dve", lambda e, rq_=rq_: e.reciprocal(out=rq_.t[:], in_=rq_.t[:]), reads=[rq_.r], writes=[rq_.r])
        if "4" in _MC:
            p = nxt("pj", pj)
            S.op("pe", lambda e, p=p, k2=k2: e.matmul(p.t[:], lhsT=oneskv.t[:], rhs=k2.t[:], start=True, stop=True), reads=[oneskv.r, k2.r], writes=[p.r])
            S.op("act", lambda e, p=p, rkv_=rkv_: e.activation(out=rkv_.t[:], in_=p.t[:], func=AF.Sqrt, bias=epst.t[:, 0:1]), reads=[p.r, epst.r], writes=[rkv_.r])
            S.op("dve", lambda e, rkv_=rkv_: e.reciprocal(out=rkv_.t[:], in_=rkv_.t[:]), reads=[rkv_.r], writes=[rkv_.r])
        for h in (range(4) if "q" in _MSUB else []):
            H = 8 + h
            pa, pb = nxt("pj", pj), nxt("pj", pj)
            for pp, ww in ((pa, wuq), (pb, wuqs)):
                for c2 in range(2):
                    S.op("pe", lambda e, pp=pp, ww=ww, c2=c2, h=h, a=a: e.matmul(pp.t[0:96, :], lhsT=ww.t[:, c2, h * 96:(h + 1) * 96], rhs=a.t[:, c2, :],
                                                                             start=(c2 == 0), stop=(c2 == 1)), reads=[ww.r, a.r], writes=[pp.r])
            st = nxt("stg", stg)
            t1, t2 = tmp1[h % 2], tmp2[h % 2]
            S.op("dve", lambda e, pa=pa, st=st, rq_=rq_: e.scalar_tensor_tensor(out=st.t[0:64, :], in0=pa.t[0:64, :], scalar=MLA_SCALE, in1=rq_.t[0:64, :],
                                                                             op0=ALU.mult, op1=ALU.mult), reads=[pa.r, rq_.r], writes=[st.r])
            S.op("dve", lambda e, pa=pa, t1=t1, tc=tc: e.tensor_tensor(out=t1.t[64:96, :], in0=pa.t[64:96, :], in1=CT.t[64:96, tc], op=ALU.mult),
                 reads=[pa.r, CT.r], writes=[t1.r])
            S.op("dve", lambda e, pb=pb, t2=t2, tc=tc: e.tensor_tensor(out=t2.t[64:96, :], in0=pb.t[64:96, :], in1=ST.t[64:96, tc], op=ALU.mult),
                 reads=[pb.r, ST.r], writes=[t2.r])
            S.op("pool", lambda e, t1=t1, t2=t2: e.tensor_tensor(out=t1.t[64:96, :], in0=t1.t[64:96, :], in1=t2.t[64:96, :], op=ALU.add),
                 reads=[t1.r, t2.r], writes=[t1.r])
            S.op("dve", lambda e, t1=t1, st=st, rq_=rq_: e.scalar_tensor_tensor(out=st.t[64:96, :], in0=t1.t[64:96, :], scalar=MLA_SCALE, in1=rq_.t[64:96, :],
                                                                             op0=ALU.mult, op1=ALU.mult), reads=[t1.r, rq_.r, st.r], writes=[st.r])
            store_rows(st, (0, 96), sc["QS"][H][0:96, :], sc["r_QS"][H], j)
        for h in (range(4) if "k" in _MSUB else []):
            H = 8 + h
            p = nxt("pj", pj)
            S.op("pe", lambda e, p=p, h=h, kg=kg: e.matmul(p.t[0:64, :], lhsT=wukv.t[:, h * 128:h * 128 + 64], rhs=kg.t[:], start=True, stop=True),
                 reads=[wukv.r, kg.r], writes=[p.r])
            st = nxt("stg", stg)
            S.op("dve", lambda e, p=p, st=st, rkv_=rkv_: e.tensor_tensor(out=st.t[0:64, :], in0=p.t[0:64, :], in1=rkv_.t[0:64, :], op=ALU.mult),
                 reads=[p.r, rkv_.r], writes=[st.r])
            store_rows(st, (0, 64), sc["KS"][H][0:64, :], sc["r_KS"][H], j)
        if "r" not in _MSUB:
            continue
        pa = proj_fm(w, 384, 32, 1, j)
        pb = proj_fm(wkrs, 0, 32, 1, j)
        t1, t2 = tmp1[0], tmp2[0]
        st = nxt("stg", stg)
        S.op("dve", lambda e, pa=pa, t1=t1, tc=tc: e.tensor_tensor(out=t1.t[0:32, :], in0=pa.t[0:32, :], in1=CT.t[0:32, tc], op=ALU.mult),
             reads=[pa.r, CT.r], writes=[t1.r])
        S.op("dve", lambda e, pb=pb, t2=t2, tc=tc: e.tensor_tensor(out=t2.t[0:32, :], in0=pb.t[0:32, :], in1=ST.t[0:32, tc], op=ALU.mult),
             reads=[pb.r, ST.r], writes=[t2.r])
        S.op("pool", lambda e, t1=t1, t2=t2, st=st: e.tensor_tensor(out=st.t[0:32, :], in0=t1.t[0:32, :], in1=t2.t[0:32, :], op=ALU.add),
             reads=[t1.r, t2.r], writes=[st.r])
        for h in range(4):
            store_rows(st, (0, 32), sc["KS"][8 + h][64:96, :], sc["r_KS"][8 + h], j)
        for bb in (range(4) if "v" in _MSUB else []):
            b = j * 4 + bb
            p = nxt("pv", pv)
            S.op("pe", lambda e, p=p, bb=bb, kg=kg: e.matmul(p.t[:, 0:256], lhsT=kg.t[:, bb * 128:(bb + 1) * 128], rhs=wukv_v, start=True, stop=True),
                 reads=[wukv.r, kg.r], writes=[p.r])
            S.op("pe", lambda e, p=p, bb=bb, k2=k2: e.matmul(p.t[:, 256:257], lhsT=k2.t[:, bb * 128:(bb + 1) * 128], rhs=oneskv.t[:, 0:1], start=True, stop=True),
                 reads=[oneskv.r, k2.r], writes=[p.r])
            rt = rtok[b % 2]
            S.op("act", lambda e, p=p, rt=rt: e.activation(out=rt.t[:], in_=p.t[:, 256:257], func=AF.Sqrt, bias=epst.t[:, 0:1]), reads=[p.r, epst.r], writes=[rt.r])
            S.op("dve", lambda e, rt=rt: e.reciprocal(out=rt.t[:], in_=rt.t[:]), reads=[rt.r], writes=[rt.r])
            S.op("dve", lambda e, p=p, b=b, rt=rt: e.tensor_scalar(out=vstm.t[:, b, :, 0:64], in0=p.t[:, 0:256].rearrange("p (h d) -> p h d", h=4),
                                                                  scalar1=rt.t[:, 0:1], scalar2=None, op0=ALU.mult), reads=[p.r, rt.r], writes=[vstm.r])
    for hp in range(2):
        S.dma("sp", sc["VS"][4 + hp].rearrange("p (b h d) -> p b h d", b=NB, h=2), vstm.t[:, :, 2 * hp:2 * hp + 2, :], reads=[vstm.r], writes=[sc["r_VS"][4 + hp]])

    S.flush()
    A3.close()
    if "d" not in _P1SEC:
        A.close()
        return
    for tb, nm in ((CT, "rope64c"), (ST, "rope64s")):
        S.dma("pool", tb.t[0:64, :], T[nm], writes=[tb.r])
        S.dma("pool", tb.t[64:128, :], T[nm], writes=[tb.r])
    for g in range(3):
        r = DIL_R[g]
        for hp in range(2):
            o = g * 256 + hp * 128
            w = load_w([(C_DIL + o, 128), (C_DIL + 768 + o, 128), (C_DIL + 1536 + o, 128)])
            scale_q(w)
            ws = wsw[(g * 2 + hp) % 2]
            for c in range(8):
                vo = ws.t[:, c, :].rearrange("p (h f d) -> p h f d", h=4, f=2)
                vi = w.t[:, c, 0:256].rearrange("p (h f d) -> p h f d", h=4, f=2)
                S.op("pool", lambda e, vo=vo, vi=vi: e.tensor_copy(out=vo[:, :, 0, :], in_=vi[:, :, 1, :]), reads=[w.r], writes=[ws.r])
                S.op("pool", lambda e, vo=vo, vi=vi: e.tensor_copy(out=vo[:, :, 1, :], in_=vi[:, :, 0, :]), reads=[w.r], writes=[ws.r])
            for qk, dst, rd in ((0, sc["QD"], sc["r_QD"]), (1, sc["KD"], sc["r_KD"])):
                for j in range(NT):
                    pa = proj_fm(w, qk * 128, 128, r, j)
                    pb = proj_fm(ws, qk * 128, 128, r, j)
                    t1, t2 = tmp1[j % 2], tmp2[j % 2]
                    st = nxt("stg", stg)
                    cv = perm_view(CT.t[:, :], r, j * 512, 512)
                    sv = perm_view(ST.t[:, :], r, j * 512, 512)
                    shp = None if len(cv.shape) == 2 else cv.shape

                    def v3(ap):
                        return ap if shp is None else ap.rearrange("p (a b) -> p a b", a=shp[1])
                    S.op("dve", lambda e, pa=pa, t1=t1, cv=cv, v3=v3: e.tensor_tensor(out=v3(t1.t[:]), in0=v3(pa.t[:]), in1=cv, op=ALU.mult),
                         reads=[pa.r, CT.r], writes=[t1.r])
                    S.op("dve", lambda e, pb=pb, t2=t2, sv=sv, v3=v3: e.tensor_tensor(out=v3(t2.t[:]), in0=v3(pb.t[:]), in1=sv, op=ALU.mult),
                         reads=[pb.r, ST.r], writes=[t2.r])
                    S.op("pool", lambda e, t1=t1, t2=t2, st=st: e.tensor_tensor(out=st.t[:], in0=t1.t[:], in1=t2.t[:], op=ALU.add),
                         reads=[t1.r, t2.r], writes=[st.r])
                    for hh in range(2):
                        store_rows(st, (hh * 64, hh * 64 + 64), dst[g][hp * 2 + hh], rd[g][hp * 2 + hh], j)
            vt = nxt("v", vst)
            v_proj(w, 256, r, vt)
            S.dma("sp", sc["VD"][g][hp], vt.t[:, :, :, :].rearrange("p b h d -> p (b h d)"), reads=[vt.r], writes=[sc["r_VD"][g][hp]])
    S.flush()
    A.close()


def phase2(nc, S, T, l, sc, heads=None, after_sb=None):
    A = Alloc(nc)
    negtri = Buf(A.sb("negtri", [128, 128], BF16))
    S.op("pool", lambda e: e.memset(negtri.t[:], -1.0), writes=[negtri.r])
    S.op("pool", lambda e: e.affine_select(out=negtri.t[:], in_=negtri.t[:], pattern=[[-1, 128]], compare_op=ALU.is_ge, fill=0.0, base=0,
                                           channel_multiplier=1), reads=[negtri.r], writes=[negtri.r])
    ones = Buf(A.sb("ones", [128, 128], BF16))
    S.op("pool", lambda e: e.memset(ones.t[:], 1.0), writes=[ones.r])
    wn = Buf(A.sb("wn", [65, 64], BF16))
    wnsb = Buf(A.sb("wnsb", [65, 64], BF16))
    for t_, v_ in ((wn, RMS_EPS), (wnsb, 0.0)):
        S.op("pool", lambda e, t_=t_: e.memset(t_.t[:], 1.0 / 64.0), writes=[t_.r])
        S.op("pool", lambda e, t_=t_, v_=v_: e.memset(t_.t[64:65, :], v_), reads=[t_.r], writes=[t_.r])
    gh = Buf(A.sb("gh", [64, 16], F32))
    for h_ in range(16):
        S.dma("sp", gh.t[:, h_:h_ + 1], T["g_head"][l][h_].rearrange("(d o) -> d o", o=1), writes=[gh.r])
    eps2 = Buf(A.sb("eps2", [64, 2], F32))
    S.op("pool", lambda e: e.memset(eps2.t[:, 0:1], RMS_EPS), writes=[eps2.r])
    S.op("pool", lambda e: e.memset(eps2.t[:, 1:2], 0.0), reads=[eps2.r], writes=[eps2.r])

    Qt = rot(A, "sb", "Qt", [128, SEQ], BF16, 4)
    Kt = rot(A, "sb", "Kt", [128, SEQ], BF16, 4)
    Vt = rot(A, "sb", "Vt", [128, NB, 2, 65], BF16, 2)
    pz = rot(A, "ps", "pz", [128, 512], F32, 3)
    po = rot(A, "ps", "po", [128, 512], F32, 2)
    pc = rot(A, "ps", "pc", [128, 512], F32, 2)
    pss = rot(A, "ps", "pss", [128, 512], F32, 1)
    Pb = rot(A, "sb", "Pb", [128, 512], BF16, 8)
    eb = rot(A, "sb", "eb", [128, 512], F32, 2)
    spb = rot(A, "sb", "spb", [128, 512], BF16, 3)
    lw = rot(A, "sb", "lw", [128, 512], F32, 4)
    Rsb = Buf(A.sb("Rsb", [128, 512], F32))
    sqb = rot(A, "sb", "sqb", [65, 512], BF16, 2)
    osb = rot(A, "sb", "osb", [65, 512], F32, 3)
    def mk_mask(name, n, conds):
        m = Buf(A.sb(name, [128, n], BF16))
        S.op("pool", lambda e: e.memset(m.t[:], 1.0), writes=[m.r])
        for (step, base, cm) in conds:
            S.op("pool", lambda e, step=step, base=base, cm=cm: e.affine_select(out=m.t[:], in_=m.t[:], pattern=[[step, n]], compare_op=ALU.is_ge, fill=0.0,
                                                                                base=base, channel_multiplier=cm), reads=[m.r], writes=[m.r])
        return m
    maskS = [mk_mask("ms%d" % o, 512, [(1, -128 * o - 1, -1)]) for o in (3, 2, 1, 0)][::-1]
    maskC = [mk_mask("mc%d" % o, 512, [(1, -128 * o, -1)]) for o in range(4)]
    maskD = {512: {o: mk_mask("md%d" % (o + 1), 512, [(1, -128 * o, -1), (-1, 128 + 128 * o, 1)]) for o in range(-1, 4)},
             256: {o: mk_mask("me%d" % o, 256, [(1, -128 * o, -1), (-1, 128 + 128 * o, 1)]) for o in range(0, 2)}}
    stb = rot(A, "sb", "stb", [64, 512], F32, 2)
    yb = rot(A, "sb", "yb", [64, 512], BF16, 2)
    acc = rot(A, "sb", "acc", [65, SEQ], F32, 2)
    st = {"fin": 0, "ld": 0, "vld": 0, "o": 0}

    def finish(src_ap, r_src, h, t0, n, is_sb, in_sbuf=False):
        i = st["fin"]
        st["fin"] += 1
        sq, s_, y, ps_ = sqb[i % 2], stb[i % 2], yb[i % 2], pss[0]
        if in_sbuf:
            o_ap, r_o = src_ap, r_src
        else:
            ob = osb[i % 3]
            S.op("act", lambda e: e.activation(out=ob.t[:, 0:n], in_=src_ap, func=AF.Copy), reads=[r_src], writes=[ob.r])
            o_ap, r_o = ob.t[:, 0:n], ob.r
        S.op("dve", lambda e: e.tensor_tensor(out=sq.t[:, 0:n], in0=o_ap, in1=o_ap, op=ALU.mult), reads=[r_o], writes=[sq.r])
        wn_ = wnsb if is_sb else wn
        S.op("pe", lambda e: e.matmul(ps_.t[0:64, 0:n], lhsT=wn_.t[:, :], rhs=sq.t[:, 0:n], start=True, stop=True), reads=[wn_.r, sq.r], writes=[ps_.r])
        S.op("act", lambda e: e.activation(out=s_.t[:, 0:n], in_=ps_.t[0:64, 0:n], func=AF.Ln, bias=(eps2.t[:, 0:1] if is_sb else eps2.t[:, 1:2])),
             reads=[ps_.r, eps2.r], writes=[s_.r])
        S.op("act", lambda e: e.activation(out=s_.t[:, 0:n], in_=s_.t[:, 0:n], func=AF.Exp, scale=-0.5), reads=[s_.r], writes=[s_.r])
        S.op("dve", lambda e: e.scalar_tensor_tensor(out=y.t[:, 0:n], in0=o_ap[0:64], scalar=gh.t[:, h:h + 1], in1=s_.t[:, 0:n],
                                                     op0=ALU.mult, op1=ALU.mult), reads=[r_o, gh.r, s_.r], writes=[y.r])
        S.dma("sp", sc["OnT"][h // 2, (h % 2) * 64:(h % 2) * 64 + 64, t0:t0 + n], y.t[:, 0:n], reads=[y.r], writes=[sc["r_OnT"]])

    def load_qk(qsrc, r_q, ksrc, r_k, kd):
        i = st["ld"]
        st["ld"] += 1
        q, k = Qt[i % 4], Kt[i % 4]
        S.dma("sp", q.t[0:kd, :], qsrc, reads=[r_q], writes=[q.r])
        S.dma("sp", k.t[0:kd, :], ksrc, reads=[r_k], writes=[k.r])
        return q, k

    def load_v(vsrc, r_v):
        i = st["vld"]
        st["vld"] += 1
        v = Vt[i % 2]
        S.dma("sp", v.t[:, :, :, :].rearrange("p b h d -> p (b h d)"), vsrc, reads=[r_v], writes=[v.r])
        return v

    def make_stages(steps, q, k, kd, v, hh, kind, done_cb, u=0):
        n_ = len(steps)
        ctx = [dict() for _ in range(n_)]
        zsel = [[pz[0], pz[1]], [pz[2], pc[0]]][u]

        def s1(i):
            sp_ = steps[i]
            z = zsel[i % 2]
            q0, n, kb = sp_["q0"], sp_["n"], sp_["kb"]
            S.op("pe", lambda e: e.matmul(z.t[:, 0:n], lhsT=k.t[0:kd, kb * 128:(kb + 1) * 128], rhs=q.t[0:kd, q0:q0 + n], start=True, stop=True),
                 reads=[k.r, q.r], writes=[z.r])
            if kind == "sb":
                e_, s_ = eb[i % 2], spb[i % 3]
                S.op("act", lambda e: e.activation(out=e_.t[:, 0:n], in_=z.t[:, 0:n], func=AF.Exp), reads=[z.r], writes=[e_.r])
                S.op("act", lambda e: e.activation(out=s_.t[:, 0:n], in_=e_.t[:, 0:n], func=AF.Ln, bias=1.0), reads=[e_.r], writes=[s_.r])
                if sp_["mask"] is not None:
                    mk = sp_["mask"]
                    S.op("pool", lambda e: e.tensor_tensor(out=s_.t[:, 0:n], in0=s_.t[:, 0:n], in1=mk.t[:, 0:n], op=ALU.mult), reads=[s_.r, mk.r], writes=[s_.r])
                ctx[i]["sp"] = s_
            else:
                p_ = Pb[u * 4 + i % 4]
                if kind == "fox" and sp_["mask"] is not None:
                    l_ = lw[u * 2 + i % 2]
                    S.op("dve", lambda e: e.tensor_scalar(out=l_.t[:, 0:n], in0=z.t[:, 0:n], scalar1=60.0, scalar2=None, op0=ALU.min), reads=[z.r], writes=[l_.r])
                    S.op("act", lambda e: e.activation(out=p_.t[:, 0:n], in_=l_.t[:, 0:n], func=AF.Exp), reads=[l_.r], writes=[p_.r])
                else:
                    S.op("act", lambda e: e.activation(out=p_.t[:, 0:n], in_=z.t[:, 0:n], func=AF.Exp), reads=[z.r], writes=[p_.r])
                if sp_["mask"] is not None:
                    mk = sp_["mask"]
                    S.op("dve", lambda e: e.tensor_tensor(out=p_.t[:, 0:n], in0=p_.t[:, 0:n], in1=mk.t[:, 0:n], op=ALU.mult), reads=[p_.r, mk.r], writes=[p_.r])
                ctx[i]["P"] = p_

        def s2(i):
            if kind != "sb":
                return
            sp_ = steps[i]
            q0, n, kb = sp_["q0"], sp_["n"], sp_["kb"]
            s_ = ctx[i]["sp"]
            c_, rc, l_, p_ = pc[i % 2], pz[2], lw[i % 2], Pb[i % 4]
            S.op("pe", lambda e: e.matmul(c_.t[:, 0:n], lhsT=k.t[0:kd, kb * 128:(kb + 1) * 128], rhs=q.t[0:kd, q0:q0 + n], start=True, stop=False),
                 reads=[k.r, q.r], writes=[c_.r])
            S.op("pe", lambda e: e.matmul(c_.t[:, 0:n], lhsT=negtri.t[:], rhs=s_.t[:, 0:n], start=False, stop=True), reads=[negtri.r, s_.r], writes=[c_.r])
            S.op("pe", lambda e: e.matmul(rc.t[:, 0:n], lhsT=ones.t[:], rhs=s_.t[:, 0:n], start=True, stop=True), reads=[ones.r, s_.r], writes=[rc.r])
            if sp_["first"]:
                S.op("dve", lambda e: e.tensor_copy(out=l_.t[:, 0:n], in_=c_.t[:, 0:n]), reads=[c_.r], writes=[l_.r])
                S.op("dve", lambda e: e.tensor_copy(out=Rsb.t[:, 0:n], in_=rc.t[:, 0:n]), reads=[rc.r], writes=[Rsb.r])
            else:
                S.op("dve", lambda e: e.tensor_tensor(out=l_.t[:, 0:n], in0=c_.t[:, 0:n], in1=Rsb.t[:, 0:n], op=ALU.subtract), reads=[c_.r, Rsb.r], writes=[l_.r])
                S.op("dve", lambda e: e.tensor_tensor(out=Rsb.t[:, 0:n], in0=rc.t[:, 0:n], in1=Rsb.t[:, 0:n], op=ALU.add), reads=[rc.r, Rsb.r], writes=[Rsb.r])
            S.op("act", lambda e: e.activation(out=p_.t[:, 0:n], in_=l_.t[:, 0:n], func=AF.Exp), reads=[l_.r], writes=[p_.r])
            if sp_["mask"] is not None:
                mk = sp_["mask"]
                S.op("pool", lambda e: e.tensor_tensor(out=p_.t[:, 0:n], in0=p_.t[:, 0:n], in1=mk.t[:, 0:n], op=ALU.mult), reads=[p_.r, mk.r], writes=[p_.r])
            ctx[i]["P"] = p_

        def s3(i):
            sp_ = steps[i]
            n, kb = sp_["n"], sp_["kb"]
            if sp_["first"] and u == 0:
                st["o"] += 1
            o_ = po[st["o"] % 2] if u == 0 else pc[1]
            p_ = ctx[i]["P"]
            S.op("pe", lambda e: e.matmul(o_.t[0:65, 0:n], lhsT=v.t[:, kb, hh, :], rhs=p_.t[:, 0:n], start=sp_["first"], stop=sp_["last"]),
                 reads=[v.r, p_.r], writes=[o_.r])
            if sp_["last"]:
                done_cb(o_, sp_)

        return n_, s1, s2, s3

    def drive(stage_sets):
        nmax = max(ss[0] for ss in stage_sets)
        for i in range(nmax + 2):
            for n_, s1, s2, s3 in stage_sets:
                if i < n_:
                    s1(i)
            for n_, s1, s2, s3 in stage_sets:
                if 0 <= i - 1 < n_:
                    s2(i - 1)
            for n_, s1, s2, s3 in stage_sets:
                if 0 <= i - 2 < n_:
                    s3(i - 2)

    def run_steps(steps, q, k, kd, v, hh, kind, done_cb):
        drive([make_stages(steps, q, k, kd, v, hh, kind, done_cb, 0)])

    def causal_steps(strict, descending):
        steps = []
        for qt in range(NT):
            q0 = qt * 512
            kbs = list(range(0, 4 * qt + 4))
            if descending:
                kbs = kbs[::-1]
            for ii, kb in enumerate(kbs):
                o = kb - 4 * qt
                steps.append(dict(q0=q0, n=512, kb=kb, mask=((maskS if strict else maskC)[o] if o >= 0 else None), first=(ii == 0), last=(ii == len(kbs) - 1)))
        return steps

    def dil_steps(r):
        sc_ = SEQ // r
        n = min(512, sc_)
        steps = []
        for q0 in range(0, SEQ, n):
            cs = (q0 // sc_) * sc_
            k_lo = max(cs, q0 - 128)
            kbs = list(range(k_lo // 128, (q0 + n) // 128))
            for ii, kb in enumerate(kbs):
                steps.append(dict(q0=q0, n=n, kb=kb, mask=maskD[n][kb - q0 // 128], first=(ii == 0), last=(ii == len(kbs) - 1)))
        return steps

    hsel = (lambda h: True) if heads is None else (lambda h: h in heads)
    groups = []
    for kind, hbase, pbase, kd in (("sb", 0, 0, 64), ("fox", 4, 2, 70), ("mla", 8, 4, 96)):
        for hp in range(2):
            js = [(kind, hbase + hp * 2 + hh, pbase + hp, hh, kd) for hh in range(2) if hsel(hbase + hp * 2 + hh)]
            if kind == "sb":
                groups += [[j] for j in js]
            elif js:
                groups.append(js)
    step_cache = {"sb": causal_steps(True, True), "fox": causal_steps(False, False)}
    step_cache["mla"] = step_cache["fox"]
    loaded = {}
    vcur = {}

    def prefetch(group):
        for job in group:
            kind, h, pr_, hh, kd = job
            if pr_ not in vcur:
                vcur.clear()
                vcur[pr_] = load_v(sc["VS"][pr_], sc["r_VS"][pr_])
            loaded[h] = load_qk(sc["QS"][h][0:kd, :], sc["r_QS"][h], sc["KS"][h][0:kd, :], sc["r_KS"][h], kd) + (vcur[pr_],)
    if groups:
        prefetch(groups[0])
    for gi, group in enumerate(groups):
        if after_sb is not None and group[0][0] != "sb":
            after_sb()
            after_sb = None
        cur = [loaded.pop(job[1]) for job in group]
        if gi + 1 < len(groups):
            prefetch(groups[gi + 1])
        sets = []
        for u, (job, (q, k, v)) in enumerate(zip(group, cur)):
            kind, h, pr_, hh, kd = job

            def done(o_, sp_, h=h, kind=kind):
                finish(o_.t[0:65, 0:sp_["n"]], o_.r, h, sp_["q0"], sp_["n"], kind == "sb")
            sets.append(make_stages(step_cache[kind], q, k, kd, v, hh, kind, done, u))
        drive(sets)
    if after_sb is not None:
        after_sb()
    dgroups = [(hp, g) for hp in range(2) if (hsel(12 + 2 * hp) or hsel(13 + 2 * hp)) for g in range(3)]
    dsteps = {g: dil_steps(DIL_R[g]) for g in range(3)}
    dl = {}

    def dprefetch(grp):
        hp, g = grp
        v = load_v(sc["VD"][g][hp], sc["r_VD"][g][hp])
        dl[grp] = [load_qk(sc["QD"][g][hp * 2 + hh], sc["r_QD"][g][hp * 2 + hh], sc["KD"][g][hp * 2 + hh], sc["r_KD"][g][hp * 2 + hh], 64) + (v,)
                   for hh in range(2)]
    if dgroups:
        dprefetch(dgroups[0])
    for gi, grp in enumerate(dgroups):
        hp, g = grp
        r = DIL_R[g]
        cur = dl.pop(grp)
        if gi + 1 < len(dgroups):
            dprefetch(dgroups[gi + 1])
        sets = []
        for hh in range(2):
            q, k, v = cur[hh]
            a_ = acc[hh]

            def done(o_, sp_, a_=a_, r=r, g=g):
                n, q0 = sp_["n"], sp_["q0"]
                dst = perm_view(a_.t[:, :], r, q0, n)
                if g == 0:
                    S.op("act", lambda e: e.activation(out=dst, in_=o_.t[0:65, 0:n], func=AF.Copy), reads=[o_.r], writes=[a_.r])
                else:
                    S.op("dve", lambda e: e.tensor_tensor(out=dst, in0=o_.t[0:65, 0:n], in1=dst, op=ALU.add), reads=[o_.r, a_.r], writes=[a_.r])
            sets.append(make_stages(dsteps[g], q, k, 64, v, hh, "dil", done, hh))
        drive(sets)
        if g == 2:
            for h2 in range(2):
                for qt in range(NT):
                    finish(acc[h2].t[:, qt * 512:(qt + 1) * 512], acc[h2].r, 12 + hp * 2 + h2, qt * 512, 512, False, in_sbuf=True)
    S.flush()
    A.close()


def layernorm_block(S, y, g_b, b_b, small, out):
    st6, mv, rs = small["st6"], small["mv"], small["rs"]
    for hf in range(2):
        S.op("dve", lambda e, hf=hf: e.bn_stats(out=st6.t[:, hf, :], in_=y.t[:, hf * 512:(hf + 1) * 512]), reads=[y.r], writes=[st6.r])
    S.op("dve", lambda e: e.bn_aggr(out=mv.t[:], in_=st6.t[:, :, :].rearrange("p a b -> p (a b)")), reads=[st6.r], writes=[mv.r])
    S.op("act", lambda e: e.activation(out=rs.t[:], in_=mv.t[:, 1:2], func=AF.Ln, bias=small["eps"].t[:, 0:1]), reads=[mv.r, small["eps"].r], writes=[rs.r])
    S.op("act", lambda e: e.activation(out=rs.t[:], in_=rs.t[:], func=AF.Exp, scale=-0.5), reads=[rs.r], writes=[rs.r])
    S.op("dve", lambda e: e.scalar_tensor_tensor(out=y.t[:], in0=y.t[:], scalar=mv.t[:, 0:1], in1=g_b.t[:], op0=ALU.subtract, op1=ALU.mult),
         reads=[y.r, mv.r, g_b.r], writes=[y.r])
    S.op("dve", lambda e: e.scalar_tensor_tensor(out=out.t[:], in0=y.t[:], scalar=rs.t[:, 0:1], in1=b_b.t[:], op0=ALU.mult, op1=ALU.add),
         reads=[y.r, rs.r, b_b.r], writes=[out.r])


def bcast_row(S, A, name, src1d, n):
    b = Buf(A.sb(name, [128, n], F32))
    S.dma("sp", b.t[:], src1d.rearrange("(o n) -> o n", o=1).partition_broadcast(128), writes=[b.r])
    return b


def phase3(nc, S, T, xin, r_xin, l, sc):
    A = Alloc(nc)
    identf = make_ident(A, S, F32)
    wout = Buf(A.sb("wout", [128, 8, DM], BF16))
    S.dma("pool", wout.t[:], T["w_out"][l].rearrange("(c p) n -> p c n", p=128), writes=[wout.r])
    wr = Buf(A.sb("wr", [128, 8, 20], F32))
    S.dma("sp", wr.t[:, :, 0:4], T["w_group"][l].rearrange("(c p) n -> p c n", p=128), writes=[wr.r])
    S.dma("sp", wr.t[:, :, 4:20], T["w_expert"][l].rearrange("(c p) n -> p c n", p=128), writes=[wr.r])
    brt = Buf(A.sb("brt", [128, 20], F32))
    S.dma("sp", brt.t[:, 0:4], T["b_group"][l].rearrange("(o n) -> o n", o=1).partition_broadcast(128), writes=[brt.r])
    S.dma("sp", brt.t[:, 4:20], T["b_expert"][l].rearrange("(o n) -> o n", o=1).partition_broadcast(128), writes=[brt.r])
    g_b = bcast_row(S, A, "ln1g", T["ln1_g"][l], DM)
    b_b = bcast_row(S, A, "ln1b", T["ln1_b"][l], DM)
    on = rot(A, "sb", "on", [128, 8, 512], BF16, 2)
    xs = rot(A, "sb", "xs3", [128, DM], F32, 4)
    y = rot(A, "sb", "y3", [128, DM], F32, 3)
    x1 = rot(A, "sb", "x1o", [128, DM], F32, 6)
    xtf = rot(A, "sb", "xtf", [128, 8, 128], F32, 3)
    xtb = rot(A, "sb", "xtb", [128, 8, 512], BF16, 2)
    gate = Buf(A.sb("gate", [128, NB, 16], F32))
    lgall = Buf(A.sb("lgall", [128, NB, 20], F32))
    if ROUTED:
        x1b = Buf(A.sb("x1b", [128, NB, DM], BF16))
        gohall = Buf(A.sb("gohall", [128, NB, 4], F32))
        S.op("pool", lambda e: e.memset(x1b.t[:, 0:4, :], 0.0), writes=[x1b.r])
        S.op("pool", lambda e: e.memset(gate.t[:], 0.0), writes=[gate.r])
        for k in (range(NTILE) if int(_os_env.get("ZF", "1")) else []):
            S.dma("sp", sc["XS"][k * 512:(k + 1) * 512, :].rearrange("(p r) n -> p (r n)", p=128), x1b.t[:, 0:4, :].rearrange("p b n -> p (b n)"),
                  reads=[x1b.r], writes=[sc["r_XS"]])
        for k in (range(NTILE) if int(_os_env.get("ZF", "1")) else []):
            S.dma("sp", sc["GS"][k * 512:(k + 1) * 512, :].rearrange("(p r) n -> p (r n)", p=128), gate.t[:, 0:4, :].rearrange("p b n -> p (b n)"),
                  reads=[gate.r], writes=[sc["r_GS"]])
    ph = rot(A, "ps", "ph", [128, DM], F32, 2)
    ptr = rot(A, "ps", "ptr", [128, DM], F32, 1)
    plg = rot(A, "ps", "plg", [128, 512], F32, 2)
    epsl = Buf(A.sb("epsl", [128, 1], F32))
    S.op("pool", lambda e: e.memset(epsl.t[:], LN_EPS), writes=[epsl.r])
    small = [dict(st6=Buf(A.sb("st6", [128, 2, 6], F32)), mv=Buf(A.sb("mv", [128, 2], F32)), rs=Buf(A.sb("rs", [128, 1], F32)), eps=epsl) for _ in range(3)]
    pend = []
    pend2 = []

    def ld_on(j):
        S.dma("sp", on[j % 2].t[:], sc["OnT"][:, :, j * 512:(j + 1) * 512].rearrange("c p t -> p c t"), reads=[sc["r_OnT"]], writes=[on[j % 2].r])

    def ld_x(b):
        S.dma("sp", xs[b % 4].t[:], xin[b * 128:(b + 1) * 128, :], reads=[r_xin], writes=[xs[b % 4].r])
    for j in range(NT):
        o_ = on[j % 2]
        if j == 0:
            ld_on(0)
        if j + 1 < NT:
            ld_on(j + 1)
        xb_ = xtb[j % 2]
        for bb in range(4):
            b = j * 4 + bb
            s_, y_, x1_, xf_, p_, sm = xs[b % 4], y[b % 3], x1[b % 6], xtf[b % 3], ph[b % 2], small[b % 3]
            if b == 0:
                ld_x(0)
                ld_x(1)
            if b + 2 < NB:
                ld_x(b + 2)
            for hf in range(2):
                for c in range(8):
                    S.op("pe", lambda e, hf=hf, c=c, bb=bb, o_=o_, p_=p_: e.matmul(p_.t[:, hf * 512:(hf + 1) * 512], lhsT=o_.t[:, c, bb * 128:(bb + 1) * 128],
                                                                            rhs=wout.t[:, c, hf * 512:(hf + 1) * 512], start=(c == 0), stop=(c == 7)),
                         reads=[o_.r, wout.r], writes=[p_.r])
            S.op("dve", lambda e, s_=s_, y_=y_, p_=p_: e.scalar_tensor_tensor(out=y_.t[:], in0=s_.t[:], scalar=ALPHA, in1=p_.t[:], op0=ALU.mult, op1=ALU.add),
                 reads=[s_.r, p_.r], writes=[y_.r])
            layernorm_block(S, y_, g_b, b_b, sm, x1_)
            S.dma("sp", sc["X1"][b * 128:(b + 1) * 128, :], x1_.t[:], reads=[x1_.r], writes=[sc["r_X1"]])
            if ROUTED:
                S.op("act", lambda e, b=b, x1_=x1_: e.activation(out=x1b.t[:, b, :], in_=x1_.t[:], func=AF.Copy), reads=[x1_.r], writes=[x1b.r])
            def stage_b(b=b, bb=bb, x1_=x1_, xf_=xf_, xb_=xb_):
                pt = ptr[0]
                for c in range(8):
                    S.op("pe", lambda e, c=c: e.transpose(out=pt.t[:, c * 128:(c + 1) * 128], in_=x1_.t[:, c * 128:(c + 1) * 128], identity=identf.t[:]),
                         reads=[x1_.r, identf.r], writes=[pt.r])
                S.op("act", lambda e: e.activation(out=xf_.t[:, :, :], in_=pt.t[:, :].rearrange("p (c t) -> p c t", c=8), func=AF.Copy),
                     reads=[pt.r], writes=[xf_.r])
                if not ROUTED:
                    S.op("dve", lambda e: e.tensor_copy(out=xb_.t[:, :, bb * 128:(bb + 1) * 128], in_=pt.t[:, :].rearrange("p (c t) -> p c t", c=8)),
                         reads=[pt.r], writes=[xb_.r])
                def stage_c():
                    pl = plg[b % 2]
                    for c in range(8):
                        S.op("pe", lambda e, c=c: e.matmul(pl.t[:, 0:20], lhsT=xf_.t[:, c, :], rhs=wr.t[:, c, :], start=(c == 0), stop=(c == 7)),
                             reads=[xf_.r, wr.r], writes=[pl.r])
                    S.op("dve", lambda e: e.tensor_tensor(out=lgall.t[:, b, :], in0=pl.t[:, 0:20], in1=brt.t[:], op=ALU.add), reads=[pl.r, brt.r], writes=[lgall.r])
                if int(_os_env.get("INL", "0")):
                    stage_c()
                else:
                    pend2.append(stage_c)
            pend.append(stage_b)
            if len(pend2) > int(_os_env.get("LAG2", "1")):
                pend2.pop(0)()
            if len(pend) > 3:
                pend.pop(0)()
            if (not ROUTED) and bb == 3:
                while pend:
                    pend.pop(0)()
                while pend2:
                    pend2.pop(0)()
        if not ROUTED:
            S.dma("sp", sc["X1T"][:, :, j * 512:(j + 1) * 512], xb_.t[:], reads=[xb_.r], writes=[sc["r_X1T"]])
    while pend:
        pend.pop(0)()
        while len(pend2) > 1:
            pend2.pop(0)()
    while pend2:
        pend2.pop(0)()
    def GT(name, shape):
        return Buf(A.sb("gv_" + name, shape, F32))
    B3 = [128, NB, 4]
    gl = lgall.t[:, :, 0:4]
    el = lgall.t[:, :, 4:20].rearrange("p b (g x) -> p b g x", g=4)
    m_, goh, tmp, se = GT("m", [128, NB]), (gohall if ROUTED else GT("goh", B3)), GT("tmp", B3), GT("se", [128, NB])
    t44, es, m1, oh1, es2, m2, oh2 = GT("t44", [128, NB, 4, 4]), GT("es", B3), GT("m1", [128, NB]), GT("oh1", B3), GT("es2", B3), GT("m2", [128, NB]), GT("oh2", B3)
    d_, p1, p2, gi = GT("d", [128, NB]), GT("p1", [128, NB]), GT("p2", [128, NB]), GT("gi", B3)

    def bc(t2):
        return t2.t[:, :].unsqueeze(2).to_broadcast(B3)

    def D(fn, reads, writes):
        S.op("dve", fn, reads=[x.r for x in reads], writes=[x.r for x in writes])
    D(lambda e: e.tensor_reduce(out=m_.t[:], in_=gl, axis=AX.X, op=ALU.max), [lgall], [m_])
    D(lambda e: e.tensor_tensor(out=goh.t[:], in0=gl, in1=bc(m_), op=ALU.is_equal), [lgall, m_], [goh])
    D(lambda e: e.tensor_tensor(out=tmp.t[:], in0=gl, in1=bc(m_), op=ALU.subtract), [lgall, m_], [tmp])
    S.op("act", lambda e: e.activation(out=tmp.t[:], in_=tmp.t[:], func=AF.Exp), reads=[tmp.r], writes=[tmp.r])
    D(lambda e: e.tensor_reduce(out=se.t[:], in_=tmp.t[:], axis=AX.X, op=ALU.add), [tmp], [se])
    D(lambda e: e.reciprocal(out=se.t[:], in_=se.t[:]), [se], [se])
    D(lambda e: e.tensor_tensor(out=t44.t[:], in0=el, in1=goh.t[:, :, :].unsqueeze(3).to_broadcast([128, NB, 4, 4]), op=ALU.mult), [lgall, goh], [t44])
    D(lambda e: e.tensor_reduce(out=es.t[:], in_=t44.t[:, :, :, :].rearrange("p b g x -> p b x g"), axis=AX.X, op=ALU.add), [t44], [es])
    D(lambda e: e.tensor_reduce(out=m1.t[:], in_=es.t[:], axis=AX.X, op=ALU.max), [es], [m1])
    D(lambda e: e.tensor_tensor(out=oh1.t[:], in0=es.t[:], in1=bc(m1), op=ALU.is_equal), [es, m1], [oh1])
    D(lambda e: e.scalar_tensor_tensor(out=es2.t[:], in0=oh1.t[:], scalar=-1e30, in1=es.t[:], op0=ALU.mult, op1=ALU.add), [oh1, es], [es2])
    D(lambda e: e.tensor_reduce(out=m2.t[:], in_=es2.t[:], axis=AX.X, op=ALU.max), [es2], [m2])
    D(lambda e: e.tensor_tensor(out=oh2.t[:], in0=es2.t[:], in1=bc(m2), op=ALU.is_equal), [es2, m2], [oh2])
    D(lambda e: e.tensor_tensor(out=d_.t[:], in0=m2.t[:], in1=m1.t[:], op=ALU.subtract), [m1, m2], [d_])
    S.op("act", lambda e: e.activation(out=d_.t[:], in_=d_.t[:], func=AF.Exp), reads=[d_.r], writes=[d_.r])
    D(lambda e: e.tensor_scalar(out=p1.t[:], in0=d_.t[:], scalar1=1.0, scalar2=None, op0=ALU.add), [d_], [p1])
    D(lambda e: e.reciprocal(out=p1.t[:], in_=p1.t[:]), [p1], [p1])
    D(lambda e: e.tensor_tensor(out=p2.t[:], in0=d_.t[:], in1=p1.t[:], op=ALU.mult), [d_, p1], [p2])
    D(lambda e: e.tensor_tensor(out=gi.t[:], in0=oh1.t[:], in1=bc(p1), op=ALU.mult), [oh1, p1], [gi])
    D(lambda e: e.tensor_tensor(out=oh2.t[:], in0=oh2.t[:], in1=bc(p2), op=ALU.mult), [oh2, p2], [oh2])
    D(lambda e: e.tensor_tensor(out=gi.t[:], in0=gi.t[:], in1=oh2.t[:], op=ALU.add), [gi, oh2], [gi])
    D(lambda e: e.tensor_tensor(out=gi.t[:], in0=gi.t[:], in1=bc(se), op=ALU.mult), [gi, se], [gi])
    D(lambda e: e.tensor_tensor(out=gate.t[:, :, :].rearrange("p b (g x) -> p b g x", g=4), in0=goh.t[:, :, :].unsqueeze(3).to_broadcast([128, NB, 4, 4]),
                                in1=gi.t[:, :, :].unsqueeze(2).to_broadcast([128, NB, 4, 4]), op=ALU.mult), [goh, gi], [gate])
    if not ROUTED:
        S.dma("sp", sc["GATE"], gate.t[:, :, :].rearrange("p b e -> p (b e)"), reads=[gate.r], writes=[sc["r_GATE"]])
    else:
        route_epilogue(S, A, sc, x1b, gate, gohall, plg, l)
    S.flush()
    A.close()


def route_epilogue(S, A, sc, x1b, gate, gohall, plg, l):
    def T_(name, shape, dt=F32):
        return Buf(A.sb(name, shape, dt))
    onesf = T_("onesf", [128, 128])
    tris = T_("tris", [128, 128])
    S.op("pool", lambda e: e.memset(onesf.t[:], 1.0), writes=[onesf.r])
    S.op("pool", lambda e: e.memset(tris.t[:], 1.0), writes=[tris.r])
    S.op("pool", lambda e: e.affine_select(out=tris.t[:], in_=tris.t[:], pattern=[[1, 128]], compare_op=ALU.is_ge, fill=0.0, base=-1,
                                           channel_multiplier=-1), reads=[tris.r], writes=[tris.r])
    pt, pr = plg[0], plg[1]
    for b in range(NB):
        S.op("pe", lambda e, b=b: e.matmul(pt.t[:, b * 4:(b + 1) * 4], lhsT=onesf.t[:], rhs=gohall.t[:, b, :], start=True, stop=True),
             reads=[onesf.r, gohall.r], writes=[pt.r])
        S.op("pe", lambda e, b=b: e.matmul(pr.t[:, b * 4:(b + 1) * 4], lhsT=tris.t[:], rhs=gohall.t[:, b, :], start=True, stop=True),
             reads=[tris.r, gohall.r], writes=[pr.r])
    totb = T_("totb", [128, NB, 4])
    cum = T_("cumb", [128, NB, 4])
    ones32 = T_("ones32", [128, NB])
    S.op("pool", lambda e: e.memset(ones32.t[:], 1.0), writes=[ones32.r])
    S.op("dve", lambda e: e.tensor_copy(out=totb.t[:, :, :], in_=pt.t[:, 0:NB * 4].rearrange("p (b g) -> p b g", g=4)), reads=[pt.r], writes=[totb.r])
    for g in range(4):
        S.op("dve", lambda e, g=g: e.tensor_tensor_scan(out=cum.t[:, :, g], data0=ones32.t[:, :], data1=totb.t[:, :, g], initial=0.0,
                                                        op0=ALU.mult, op1=ALU.add), reads=[ones32.r, totb.r, cum.r], writes=[cum.r])
    boffx = T_("boffx", [128, NB, 4])
    S.op("dve", lambda e: e.tensor_tensor(out=boffx.t[:], in0=cum.t[:], in1=totb.t[:], op=ALU.subtract), reads=[cum.r, totb.r], writes=[boffx.r])
    thr_i = T_("thri", [128, 16], I32)
    thr = T_("thr", [128, 16])
    S.op("pool", lambda e: e.iota(thr_i.t[:], pattern=[[512, 16]], base=0, channel_multiplier=0), writes=[thr_i.r])
    S.op("dve", lambda e: e.tensor_copy(out=thr.t[:], in_=thr_i.t[:]), reads=[thr_i.r], writes=[thr.r])
    cmp = T_("cmp", [128, 4, 8])
    ntl = T_("ntl", [128, 4])
    S.op("dve", lambda e: e.tensor_tensor(out=cmp.t[:], in0=cum.t[:, NB - 1, :].unsqueeze(2).to_broadcast([128, 4, 8]),
                                          in1=thr.t[:, 0:8].unsqueeze(1).to_broadcast([128, 4, 8]), op=ALU.is_gt), reads=[cum.r, thr.r], writes=[cmp.r])
    S.op("dve", lambda e: e.tensor_reduce(out=ntl.t[:], in_=cmp.t[:], axis=AX.X, op=ALU.add), reads=[cmp.r], writes=[ntl.r])
    S.op("dve", lambda e: e.tensor_scalar(out=ntl.t[:], in0=ntl.t[:], scalar1=512.0, scalar2=None, op0=ALU.mult), reads=[ntl.r], writes=[ntl.r])
    pst = T_("pst", [128, 4])
    pen = T_("pen", [128, 4])
    S.op("pool", lambda e: e.memset(pst.t[:], 0.0), writes=[pst.r])
    for g in range(1, 4):
        S.op("dve", lambda e, g=g: e.tensor_tensor(out=pst.t[:, g:g + 1], in0=pst.t[:, g - 1:g], in1=ntl.t[:, g - 1:g], op=ALU.add),
             reads=[pst.r, ntl.r], writes=[pst.r])
    S.op("dve", lambda e: e.tensor_tensor(out=pen.t[:], in0=pst.t[:], in1=ntl.t[:], op=ALU.add), reads=[pst.r, ntl.r], writes=[pen.r])
    v = T_("vdest", [128, NB, 4])
    S.op("dve", lambda e: e.tensor_tensor(out=v.t[:], in0=pr.t[:, 0:NB * 4].rearrange("p (b g) -> p b g", g=4), in1=boffx.t[:], op=ALU.add),
         reads=[pr.r, boffx.r], writes=[v.r])
    S.op("dve", lambda e: e.tensor_tensor(out=v.t[:], in0=v.t[:], in1=pst.t[:, :].unsqueeze(1).to_broadcast([128, NB, 4]), op=ALU.add),
         reads=[v.r, pst.r], writes=[v.r])
    S.op("dve", lambda e: e.tensor_tensor(out=v.t[:], in0=v.t[:], in1=gohall.t[:], op=ALU.mult), reads=[v.r, gohall.r], writes=[v.r])
    destf = T_("destf", [128, NB])
    desti = T_("desti", [128, NB], I32)
    S.op("dve", lambda e: e.tensor_reduce(out=destf.t[:], in_=v.t[:], axis=AX.X, op=ALU.add), reads=[v.r], writes=[destf.r])
    S.op("dve", lambda e: e.tensor_copy(out=desti.t[:], in_=destf.t[:]), reads=[destf.r], writes=[desti.r])
    S.dma("sp", sc["DEST"], desti.t[:], reads=[desti.r], writes=[sc["r_DEST"]])
    cmp2 = T_("cmp2", [128, NTILE, 4])
    gk = T_("gk", [128, NTILE])
    S.op("dve", lambda e: e.tensor_tensor(out=cmp2.t[:], in0=pen.t[:, :].unsqueeze(1).to_broadcast([128, NTILE, 4]),
                                          in1=thr.t[:, 0:NTILE].unsqueeze(2).to_broadcast([128, NTILE, 4]), op=ALU.is_le), reads=[pen.r, thr.r], writes=[cmp2.r])
    S.op("dve", lambda e: e.tensor_reduce(out=gk.t[:], in_=cmp2.t[:], axis=AX.X, op=ALU.add), reads=[cmp2.r], writes=[gk.r])
    S.op("dve", lambda e: e.tensor_scalar(out=gk.t[:], in0=gk.t[:], scalar1=3.0, scalar2=1024.0, op0=ALU.min, op1=ALU.mult), reads=[gk.r], writes=[gk.r])
    S.op("dve", lambda e: e.tensor_scalar(out=gk.t[:], in0=gk.t[:], scalar1=float(l * 4096), scalar2=None, op0=ALU.add), reads=[gk.r], writes=[gk.r])
    cw_i = T_("cwi", [128, 8], I32)
    cw = T_("cw", [128, 8])
    S.op("pool", lambda e: e.iota(cw_i.t[:], pattern=[[256, 4], [1, 2]], base=0, channel_multiplier=2), writes=[cw_i.r])
    S.op("dve", lambda e: e.tensor_copy(out=cw.t[:], in_=cw_i.t[:]), reads=[cw_i.r], writes=[cw.r])
    idxf = T_("idxf", [128, NTILE, 8])
    idxi = T_("idxi", [128, NTILE, 8], I32)
    S.op("dve", lambda e: e.tensor_tensor(out=idxf.t[:], in0=cw.t[:, :].unsqueeze(1).to_broadcast([128, NTILE, 8]),
                                          in1=gk.t[:, :].unsqueeze(2).to_broadcast([128, NTILE, 8]), op=ALU.add), reads=[cw.r, gk.r], writes=[idxf.r])
    S.op("dve", lambda e: e.tensor_copy(out=idxi.t[:], in_=idxf.t[:]), reads=[idxf.r], writes=[idxi.r])
    S.dma("sp", sc["IDXW"], idxi.t[:, :, :].rearrange("p k j -> p (k j)"), reads=[idxi.r], writes=[sc["r_IDXW"]])
    for b in range(NB):
        S.dma_fn("pool", lambda e, b=b: e.indirect_dma_start(out=sc["XS"][:, :], out_offset=bass.IndirectOffsetOnAxis(ap=desti.t[:, b:b + 1], axis=0),
                                                            in_=x1b.t[:, b, :], in_offset=None), reads=[desti.r, x1b.r], writes=[sc["r_XS"]])
        S.dma_fn("pool", lambda e, b=b: e.indirect_dma_start(out=sc["GS"][:, :], out_offset=bass.IndirectOffsetOnAxis(ap=desti.t[:, b:b + 1], axis=0),
                                                            in_=gate.t[:, b, :], in_offset=None), reads=[desti.r, gate.r], writes=[sc["r_GS"]])


def phase4(nc, S, T, l, sc, xout, r_xout, final, TG=1024):
    A = Alloc(nc)
    g_b = bcast_row(S, A, "ln2g", T["ln2_g"][l], DM)
    b_b = bcast_row(S, A, "ln2b", T["ln2_b"][l], DM)
    gate = Buf(A.sb("gate4", [128, NB, 16], F32))
    S.dma("sp", gate.t[:, :, :].rearrange("p b e -> p (b e)"), sc["GATE"], reads=[sc["r_GATE"]], writes=[gate.r])
    xT = Buf(A.sb("x1T", [128, 8, TG], BF16))
    accb = Buf(A.sb("accm", [128, TG // 128, DM], F32))
    w1 = rot(A, "sb", "w1", [128, 8, DEXP], BF16, 2)
    w3 = rot(A, "sb", "w3", [128, 8, DEXP], BF16, 2)
    w2 = rot(A, "sb", "w2", [128, 4, DM], BF16, 2)
    sa = rot(A, "sb", "sa", [128, 512], F32, 2)
    hT = rot(A, "sb", "hT", [128, 4, 512], BF16, 2)
    xs = rot(A, "sb", "xs4", [128, DM], F32, 2)
    yo = rot(A, "sb", "yo4", [128, DM], F32, 2)
    epsl = Buf(A.sb("epsl4", [128, 1], F32))
    S.op("pool", lambda e: e.memset(epsl.t[:], LN_EPS), writes=[epsl.r])
    small = [dict(st6=Buf(A.sb("st6b", [128, 2, 6], F32)), mv=Buf(A.sb("mvb", [128, 2], F32)), rs=Buf(A.sb("rsb", [128, 1], F32)), eps=epsl) for _ in range(2)]
    pa = rot(A, "ps", "pa", [128, 512], F32, 2)
    pb = rot(A, "ps", "pb", [128, 512], F32, 2)
    py = rot(A, "ps", "py", [128, 512], F32, 3)
    cnt = {"y": 0, "ab": 0, "w": 0}
    W1 = T["w1"][l]
    W3 = T["w3"][l]
    W2 = T["w2"][l]
    for gi in range(SEQ // TG):
        t0 = gi * TG
        S.dma("sp", xT.t[:], sc["X1T"][:, :, t0:t0 + TG], reads=[sc["r_X1T"]], writes=[xT.r])
        for ex in range(NEXP):
            i = cnt["w"]
            cnt["w"] += 1
            a1, a3, a2 = w1[i % 2], w3[i % 2], w2[i % 2]
            S.dma("pool", a1.t[:], W1[ex].rearrange("(c p) n -> p c n", p=128), writes=[a1.r])
            S.dma("pool", a3.t[:], W3[ex].rearrange("(c p) n -> p c n", p=128), writes=[a3.r])
            S.dma("pool", a2.t[:], W2[ex].rearrange("(c p) n -> p c n", p=128), writes=[a2.r])
            for tt in range(TG // 512):
                tc = slice(tt * 512, (tt + 1) * 512)
                h_ = hT[(ex * (TG // 512) + tt) % 2]
                for jc in range(4):
                    k_ = cnt["ab"]
                    cnt["ab"] += 1
                    pa_, pb_, sa_ = pa[k_ % 2], pb[k_ % 2], sa[k_ % 2]
                    for c in range(8):
                        S.op("pe", lambda e, c=c, jc=jc, pa_=pa_, a1=a1, tc=tc: e.matmul(pa_.t[:], lhsT=a1.t[:, c, jc * 128:(jc + 1) * 128], rhs=xT.t[:, c, tc],
                                                                                start=(c == 0), stop=(c == 7)), reads=[a1.r, xT.r], writes=[pa_.r])
                    for c in range(8):
                        S.op("pe", lambda e, c=c, jc=jc, pb_=pb_, a3=a3, tc=tc: e.matmul(pb_.t[:], lhsT=a3.t[:, c, jc * 128:(jc + 1) * 128], rhs=xT.t[:, c, tc],
                                                                                start=(c == 0), stop=(c == 7)), reads=[a3.r, xT.r], writes=[pb_.r])
                    S.op("act", lambda e, pa_=pa_, sa_=sa_: e.activation(out=sa_.t[:], in_=pa_.t[:], func=AF.Silu), reads=[pa_.r], writes=[sa_.r])
                    S.op("dve", lambda e, pb_=pb_, sa_=sa_, h_=h_, jc=jc: e.tensor_tensor(out=h_.t[:, jc, :], in0=pb_.t[:], in1=sa_.t[:], op=ALU.mult),
                         reads=[pb_.r, sa_.r], writes=[h_.r])
                for tb in range(4):
                    blk = tt * 4 + tb
                    gb = (t0 // 128) + blk
                    for hf in range(2):
                        y_ = py[cnt["y"] % 3]
                        cnt["y"] += 1
                        for jc in range(4):
                            S.op("pe", lambda e, jc=jc, tb=tb, hf=hf, y_=y_, h_=h_, a2=a2: e.matmul(y_.t[:], lhsT=h_.t[:, jc, tb * 128:(tb + 1) * 128],
                                                                                           rhs=a2.t[:, jc, hf * 512:(hf + 1) * 512], start=(jc == 0), stop=(jc == 3)),
                                 reads=[h_.r, a2.r], writes=[y_.r])
                        dst = accb.t[:, blk, hf * 512:(hf + 1) * 512]
                        if ex == 0:
                            S.op("dve", lambda e, y_=y_, dst=dst, gb=gb, ex=ex: e.tensor_scalar(out=dst, in0=y_.t[:], scalar1=gate.t[:, gb, ex:ex + 1], scalar2=None, op0=ALU.mult),
                                 reads=[y_.r, gate.r], writes=[accb.r])
                        else:
                            S.op("dve", lambda e, y_=y_, dst=dst, gb=gb, ex=ex: e.scalar_tensor_tensor(out=dst, in0=y_.t[:], scalar=gate.t[:, gb, ex:ex + 1], in1=dst,
                                                                                              op0=ALU.mult, op1=ALU.add), reads=[y_.r, gate.r, accb.r], writes=[accb.r])
        for blk in range(TG // 128):
            gb = (t0 // 128) + blk
            s_, y_, sm = xs[blk % 2], yo[blk % 2], small[blk % 2]
            S.dma("sp", s_.t[:], sc["X1"][gb * 128:(gb + 1) * 128, :], reads=[sc["r_X1"]], writes=[s_.r])
            S.op("dve", lambda e, s_=s_, blk=blk: e.scalar_tensor_tensor(out=s_.t[:], in0=s_.t[:], scalar=ALPHA, in1=accb.t[:, blk, :], op0=ALU.mult, op1=ALU.add),
                 reads=[s_.r, accb.r], writes=[s_.r])
            layernorm_block(S, s_, g_b, b_b, sm, y_)
            S.dma("sp", xout[gb * 128:(gb + 1) * 128, :], y_.t[:], reads=[y_.r], writes=[r_xout], final=final)
    S.flush()
    A.close()


def phase4r(nc, S, T, l, sc, xout, r_xout, final):
    A = Alloc(nc)
    ident = make_ident(A, S, BF16)
    g_b = bcast_row(S, A, "ln2g", T["ln2_g"][l], DM)
    b_b = bcast_row(S, A, "ln2b", T["ln2_b"][l], DM)
    dest = Buf(A.sb("dest4", [128, NB], I32))
    idxw = Buf(A.sb("idxw4", [128, NTILE * 8], I32))
    S.dma("sp", dest.t[:], sc["DEST"], reads=[sc["r_DEST"]], writes=[dest.r])
    S.dma("sp", idxw.t[:], sc["IDXW"], reads=[sc["r_IDXW"]], writes=[idxw.r])
    xs = rot(A, "sb", "xs4r", [128, 4, DM], BF16, 2)
    gs = rot(A, "sb", "gs4r", [128, 4, 16], F32, 2)
    gsel = rot(A, "sb", "gsel", [128, 4, 4], F32, 2)
    xT = rot(A, "sb", "xT4r", [128, 8, 512], BF16, 2)
    accs = rot(A, "sb", "acc4r", [128, 4, DM], F32, 2)
    w1 = rot(A, "sb", "w1r", [128, 8 * DEXP], BF16, 3)
    w3 = rot(A, "sb", "w3r", [128, 8 * DEXP], BF16, 3)
    w2 = rot(A, "sb", "w2r", [128, 4 * DM], BF16, 3)
    sa = rot(A, "sb", "sar", [128, 512], F32, 2)
    hT = rot(A, "sb", "hTr", [128, 4, 512], BF16, 2)
    mt = rot(A, "sb", "mt4", [128, DM], F32, 4)
    xo = rot(A, "sb", "xo4", [128, DM], F32, 4)
    yo = rot(A, "sb", "yo4r", [128, DM], F32, 4)
    epsl = Buf(A.sb("epsl4r", [128, 1], F32))
    S.op("pool", lambda e: e.memset(epsl.t[:], LN_EPS), writes=[epsl.r])
    small = [dict(st6=Buf(A.sb("st6r", [128, 2, 6], F32)), mv=Buf(A.sb("mvr", [128, 2], F32)), rs=Buf(A.sb("rsr", [128, 1], F32)), eps=epsl) for _ in range(4)]
    ptp = rot(A, "ps", "ptp", [128, DM], BF16, 1)
    pa = rot(A, "ps", "par", [128, 512], F32, 2)
    pb = rot(A, "ps", "pbr", [128, 512], F32, 2)
    py = rot(A, "ps", "pyr", [128, 512], F32, 3)
    Wv = [sc["WB"][i][:, :] for i in range(3)]
    cnt = {"y": 0, "ab": 0}
    steps = [(k, j) for k in range(NTILE) for j in range(4)]
    st = {}

    def front(si):
        k, j = steps[si]
        if j == 0:
            x_, g_, gl_, xT_, ac_ = xs[k % 2], gs[k % 2], gsel[k % 2], xT[k % 2], accs[k % 2]
            S.dma("sp", x_.t[:], sc["XS"][k * 512:(k + 1) * 512, :].rearrange("(b p) n -> p b n", p=128), reads=[sc["r_XS"]], writes=[x_.r])
            S.dma("sp", g_.t[:], sc["GS"][k * 512:(k + 1) * 512, :].rearrange("(b p) n -> p b n", p=128), reads=[sc["r_GS"]], writes=[g_.r])
            S.op("dve", lambda e: e.tensor_reduce(out=gl_.t[:], in_=g_.t[:, :, :].rearrange("p b (g j) -> p b j g", g=4), axis=AX.X, op=ALU.add),
                 reads=[g_.r], writes=[gl_.r])
            for blk in range(4):
                p = ptp[0]
                for c in range(8):
                    S.op("pe", lambda e, c=c, blk=blk: e.transpose(out=p.t[:, c * 128:(c + 1) * 128],
                                                                   in_=x_.t[:, blk, :].rearrange("p (pp c) -> p c pp", c=8)[:, c, :], identity=ident.t[:]),
                         reads=[x_.r, ident.r], writes=[p.r])
                S.op("act", lambda e, blk=blk: e.activation(out=xT_.t[:, :, blk * 128:(blk + 1) * 128], in_=p.t[:, :].rearrange("p (c t) -> p c t", c=8), func=AF.Copy),
                     reads=[p.r], writes=[xT_.r])
        xT_, ac_, gl_ = xT[k % 2], accs[k % 2], gsel[k % 2]
        a1, a3, a2, h_ = w1[si % 3], w3[si % 3], w2[si % 3], hT[si % 2]
        for wt_, src in ((a1, Wv[0]), (a3, Wv[1]), (a2, Wv[2])):
            for half in range(2):
                col = k * 8 + j * 2 + half
                S.dma_fn("pool", lambda e, wt_=wt_, src=src, half=half, col=col: e.indirect_dma_start(
                    out=wt_.t[:, half * 2048:(half + 1) * 2048], out_offset=None, in_=src,
                    in_offset=bass.IndirectOffsetOnAxis(ap=idxw.t[:, col:col + 1], axis=0)), reads=[idxw.r, sc["r_WB"][l]], writes=[wt_.r])
        w1v = a1.t[:, :].rearrange("p (c pp q) -> p c q pp", c=8, q=4)
        w3v = a3.t[:, :].rearrange("p (c pp q) -> p c q pp", c=8, q=4)
        for jc in range(4):
            k_ = cnt["ab"]
            cnt["ab"] += 1
            pa_, pb_, sa_ = pa[k_ % 2], pb[k_ % 2], sa[k_ % 2]
            for c in range(8):
                S.op("pe", lambda e, c=c, jc=jc, pa_=pa_: e.matmul(pa_.t[:], lhsT=w1v[:, c, jc, :], rhs=xT_.t[:, c, :], start=(c == 0), stop=(c == 7)),
                     reads=[a1.r, xT_.r], writes=[pa_.r])
            for c in range(8):
                S.op("pe", lambda e, c=c, jc=jc, pb_=pb_: e.matmul(pb_.t[:], lhsT=w3v[:, c, jc, :], rhs=xT_.t[:, c, :], start=(c == 0), stop=(c == 7)),
                     reads=[a3.r, xT_.r], writes=[pb_.r])
            S.op("act", lambda e, pa_=pa_, sa_=sa_: e.activation(out=sa_.t[:], in_=pa_.t[:], func=AF.Silu), reads=[pa_.r], writes=[sa_.r])
            S.op("dve", lambda e, pb_=pb_, sa_=sa_, jc=jc: e.tensor_tensor(out=h_.t[:, jc, :], in0=pb_.t[:], in1=sa_.t[:], op=ALU.mult),
                 reads=[pb_.r, sa_.r], writes=[h_.r])

    def back(si):
        k, j = steps[si]
        ac_, gl_, a2, h_ = accs[k % 2], gsel[k % 2], w2[si % 3], hT[si % 2]
        w2v = a2.t[:, :].rearrange("p (c n) -> p c n", c=4)
        for blk in range(4):
            for hf in range(2):
                y_ = py[cnt["y"] % 3]
                cnt["y"] += 1
                for jc in range(4):
                    S.op("pe", lambda e, jc=jc, blk=blk, hf=hf, y_=y_: e.matmul(y_.t[:], lhsT=h_.t[:, jc, blk * 128:(blk + 1) * 128],
                                                                               rhs=w2v[:, jc, hf * 512:(hf + 1) * 512], start=(jc == 0), stop=(jc == 3)),
                         reads=[h_.r, a2.r], writes=[y_.r])
                dst = ac_.t[:, blk, hf * 512:(hf + 1) * 512]
                if j == 0:
                    S.op("dve", lambda e, y_=y_, dst=dst, blk=blk: e.tensor_scalar(out=dst, in0=y_.t[:], scalar1=gl_.t[:, blk, j:j + 1], scalar2=None, op0=ALU.mult),
                         reads=[y_.r, gl_.r], writes=[ac_.r])
                else:
                    S.op("dve", lambda e, y_=y_, dst=dst, blk=blk: e.scalar_tensor_tensor(out=dst, in0=y_.t[:], scalar=gl_.t[:, blk, j:j + 1], in1=dst,
                                                                                         op0=ALU.mult, op1=ALU.add), reads=[y_.r, gl_.r, ac_.r], writes=[ac_.r])
        if j == 3:
            S.dma("sp", sc["YS"][k * 512:(k + 1) * 512, :].rearrange("(b p) n -> p b n", p=128), ac_.t[:], reads=[ac_.r], writes=[sc["r_YS"]])

    for si in range(len(steps) + 1):
        if si < len(steps):
            front(si)
        if si >= 1:
            back(si - 1)
    def comb_load(b):
        m_, x_ = mt[b % 4], xo[b % 4]
        S.dma_fn("pool", lambda e: e.indirect_dma_start(out=m_.t[:], out_offset=None, in_=sc["YS"][:, :],
                                                        in_offset=bass.IndirectOffsetOnAxis(ap=dest.t[:, b:b + 1], axis=0)),
                 reads=[dest.r, sc["r_YS"]], writes=[m_.r])
        S.dma("sp", x_.t[:], sc["X1"][b * 128:(b + 1) * 128, :], reads=[sc["r_X1"]], writes=[x_.r])
    for b in range(min(3, NB)):
        comb_load(b)
    for b in range(NB):
        m_, x_, y_, sm = mt[b % 4], xo[b % 4], yo[b % 4], small[b % 4]
        S.op("dve", lambda e, m_=m_, x_=x_: e.scalar_tensor_tensor(out=x_.t[:], in0=x_.t[:], scalar=ALPHA, in1=m_.t[:], op0=ALU.mult, op1=ALU.add),
             reads=[x_.r, m_.r], writes=[x_.r])
        layernorm_block(S, x_, g_b, b_b, sm, y_)
        if b + 3 < NB:
            comb_load(b + 3)
        S.dma("sp", xout[b * 128:(b + 1) * 128, :], y_.t[:], reads=[y_.r], writes=[r_xout], final=final)
    S.flush()
    A.close()


def rope_tables():
    pos = np.arange(SEQ, dtype=np.float32)
    out = {}
    for dim, nm in ((64, "rope64"), (32, "rope32")):
        half = dim // 2
        inv = (10000.0 ** (-np.arange(half, dtype=np.float32) / half)).astype(np.float32)
        ang = pos[None, :] * inv[:, None]
        c = np.cos(ang).astype(np.float32)
        s = np.sin(ang).astype(np.float32)
        out[nm + "c"] = np.ascontiguousarray(np.concatenate([c, c], 0))
        out[nm + "s"] = np.ascontiguousarray(np.concatenate([-s, s], 0))
    return out


W_SPECS = [("w_in", [DEPTH, DM, N_IN]), ("b_forget", [DEPTH, 4]), ("g_cq", [DEPTH, 256]), ("g_ckv", [DEPTH, 128]), ("w_uq", [DEPTH, 256, 384]),
           ("w_ukv", [DEPTH, 128, 512]), ("g_head", [DEPTH, 16, 64]), ("w_out", [DEPTH, DM, DM]), ("ln1_g", [DEPTH, DM]), ("ln1_b", [DEPTH, DM]),
           ("w_group", [DEPTH, DM, 4]), ("b_group", [DEPTH, 4]), ("w_expert", [DEPTH, DM, 16]), ("b_expert", [DEPTH, 16]),
           ("w1", [DEPTH, NEXP, DM, DEXP]), ("w3", [DEPTH, NEXP, DM, DEXP]), ("w2", [DEPTH, NEXP, DEXP, DM]), ("ln2_g", [DEPTH, DM]), ("ln2_b", [DEPTH, DM])]


def build_program(nseq=2, layers=(0, 1), phases=(1, 2, 3, 4), debug=False, heads=None, TG=1024):
    nc = bass.Bass("TRN2", target_bir_lowering=False)
    T = {}
    T["x"] = nc.dram_tensor("x", [nseq, SEQ, DM], F32, kind="ExternalInput").ap()
    for nm, shp in W_SPECS:
        T[nm] = nc.dram_tensor(nm, shp, F32, kind="ExternalInput").ap()
    for nm, rows in (("rope64c", 64), ("rope64s", 64), ("rope32c", 32), ("rope32s", 32)):
        T[nm] = nc.dram_tensor(nm, [rows, SEQ], F32, kind="ExternalInput").ap()
    out = nc.dram_tensor("out", [nseq, SEQ, DM], F32, kind="ExternalOutput").ap()
    dk = "ExternalOutput" if debug else "Internal"

    def scratch(name, shape, dt):
        return nc.dram_tensor(name, shape, dt, kind=dk).ap()
    sc = {}
    qs = scratch("QS", [12, 96, SEQ], BF16)
    ks = scratch("KS", [12, 96, SEQ], BF16)
    sc["QS"] = [qs[h] for h in range(12)]
    sc["KS"] = [ks[h] for h in range(12)]
    vs = scratch("VS", [6, 128, NB * 2 * 65], BF16)
    sc["VS"] = [vs[p] for p in range(6)]
    qd = scratch("QD", [3, 4, 64, SEQ], BF16)
    kd = scratch("KD", [3, 4, 64, SEQ], BF16)
    sc["QD"] = [[qd[g, h] for h in range(4)] for g in range(3)]
    sc["KD"] = [[kd[g, h] for h in range(4)] for g in range(3)]
    vd = scratch("VD", [3, 2, 128, NB * 2 * 65], BF16)
    sc["VD"] = [[vd[g, p] for p in range(2)] for g in range(3)]
    sc["OnT"] = scratch("OnT", [8, 128, SEQ], BF16)
    sc["X1"] = scratch("X1", [SEQ, DM], F32)
    sc["X1T"] = scratch("X1T", [128, 8, SEQ], BF16)
    sc["GATE"] = scratch("GATE", [128, NB * 16], F32)
    sc["XS"] = scratch("XS", [NSLOT, DM], BF16)
    sc["GS"] = scratch("GS", [NSLOT, 16], F32)
    sc["YS"] = scratch("YS", [NSLOT, DM], F32)
    sc["DEST"] = scratch("DEST", [128, NB], I32)
    sc["IDXW"] = scratch("IDXW", [128, NTILE * 8], I32)
    sc["WB"] = [scratch("WB%d" % i, [DEPTH * 4096, 2048], BF16) for i in range(3)]
    sc["r_WB"] = [Res() for _ in range(DEPTH)]
    xmid = scratch("XMID", [SEQ, DM], F32)
    sc["r_QS"] = [Res() for _ in range(12)]
    sc["r_KS"] = [Res() for _ in range(12)]
    sc["r_VS"] = [Res() for _ in range(6)]
    sc["r_QD"] = [[Res() for _ in range(4)] for _ in range(3)]
    sc["r_KD"] = [[Res() for _ in range(4)] for _ in range(3)]
    sc["r_VD"] = [[Res() for _ in range(2)] for _ in range(3)]
    for k in ("OnT", "X1", "X1T", "GATE", "XS", "GS", "YS", "DEST", "IDXW"):
        sc["r_" + k] = Res()
    r_xmid = Res()
    r_x = Res()
    r_out = Res()
    S = Sched(nc)
    for s in range(nseq):
        for li, l in enumerate(layers):
            xin, r_xin = (T["x"][s], r_x) if li == 0 else (xmid, r_xmid)
            last = li == len(layers) - 1
            xo, r_xo = (out[s], r_out) if last else (xmid, r_xmid)
            if 1 in phases:
                phase1(nc, S, T, xin, r_xin, l, sc)
            if 2 in phases:
                cv = (lambda l=l: convert_expert_weights(S, T, sc, l)) if (ROUTED and s == 0) else None
                phase2(nc, S, T, l, sc, heads=heads, after_sb=cv)
            if 3 in phases:
                phase3(nc, S, T, xin, r_xin, l, sc)
            if 4 in phases:
                if ROUTED:
                    phase4r(nc, S, T, l, sc, xo, r_xo, final=last)
                else:
                    phase4(nc, S, T, l, sc, xo, r_xo, final=last, TG=TG)
    S.close()
    return nc, S


def convert_expert_weights(S, T, sc, l):
    for i, nm in enumerate(("w1", "w3", "w2")):
        src = T[nm][l].rearrange("e k n -> (e k n)").rearrange("(r x) -> r x", x=2048)
        for ch in range(8):
            S.dma("pool", sc["WB"][i][l * 4096 + ch * 512:l * 4096 + (ch + 1) * 512, :], src[ch * 512:(ch + 1) * 512, :], writes=[sc["r_WB"][l]])


_CACHE = {}


def kernel(**inputs):
    n = 8
    nseq = 2
    x = np.ascontiguousarray(np.asarray(inputs["x"], dtype=np.float32))
    tabs = rope_tables()
    if "nc" not in _CACHE:
        _CACHE["nc"] = build_program(nseq=nseq)[0]
    nc = _CACHE["nc"]
    base = {nm: np.ascontiguousarray(np.asarray(inputs[nm], dtype=np.float32)) for nm, _ in W_SPECS}
    base.update(tabs)
    in_maps = []
    for c in range(n):
        m = dict(base)
        m["x"] = x[c * nseq:(c + 1) * nseq]
        in_maps.append(m)
    res = run_bass_kernel_spmd(nc, in_maps, core_ids=list(range(n)))
    return np.concatenate([r["out"] for r in res.results], axis=0).astype(np.float32)
```

```python
import numpy as np
from os import environ as _os_env
import concourse.bass as bass
import concourse.mybir as mybir
from concourse.bass_utils import run_bass_kernel_spmd

F32 = mybir.dt.float32
BF16 = mybir.dt.bfloat16
I32 = mybir.dt.int32
AF = mybir.ActivationFunctionType
ALU = mybir.AluOpType
AX = mybir.AxisListType

SES_MODE = int(_os_env.get("SES", "2"))
SAME_ENGINE_SYNC = SES_MODE != 0
NDMA_SLOTS = int(_os_env.get("NSLOTS", "8"))

SEQ = 4096
DM = 1024
NB = SEQ // 128
NT = SEQ // 512
DEPTH = 2
ALPHA = (2.0 * DEPTH) ** 0.25
LN_EPS = 1e-5
RMS_EPS = 1e-6
N_IN = 4260
C_SB, C_FOX, C_MLA, C_DIL = 0, 768, 1540, 1956
MLA_SCALE = 96.0 ** -0.5
DIL_R = (1, 4, 16)
NEXP = 16
DEXP = 512
NTILE = 11
NSLOT = NTILE * 512
ROUTED = bool(int(_os_env.get("ROUTED", "1")))


class Res:
    __slots__ = ("w", "r", "excl")

    def __init__(self, excl=False):
        self.w = None
        self.r = []
        self.excl = excl


class Sched:
    ENG = ("pe", "act", "dve", "pool", "sp")

    def __init__(self, nc):
        self.nc = nc
        self.ops = {e: [] for e in self.ENG}
        self.cnt = {e: 0 for e in self.ENG}
        self.seen = {e: {} for e in self.ENG}
        self.sems = {}
        self.dma_slots = {}
        self.dma_rr = {}
        self.final_waits = []
        self._stack = []
        self.nops = 0

    def sem(self, key):
        if key not in self.sems:
            cm = self.nc.semaphore("s_" + "_".join(str(k) for k in (key if isinstance(key, tuple) else (key,))))
            s = cm.__enter__()
            self._stack.append(cm)
            self.sems[key] = s
        return self.sems[key]

    def _deps(self, eng, reads, writes):
        deps = {}

        def add(t, same_ok=True):
            if t is None:
                return
            k, v = t
            if k == eng and not same_ok:
                return
            if deps.get(k, 0) < v:
                deps[k] = v
        relax = SES_MODE == 2
        for r in reads:
            add(r.w)
            if r.excl:
                for t in r.r:
                    if t[0] != eng:
                        add(t)
        for w in writes:
            add(w.w, same_ok=not relax)
            for t in w.r:
                add(t, same_ok=not relax)
        waits = []
        seen = self.seen[eng]
        for k, v in deps.items():
            if k == eng and (eng == "pe" or not SAME_ENGINE_SYNC):
                continue
            if seen.get(k, 0) >= v:
                continue
            seen[k] = v
            waits.append((k, v))
        return waits

    def _commit(self, ticket, reads, writes):
        for r in reads:
            if len(r.r) > 16:
                m = {}
                for k, v in r.r:
                    if m.get(k, 0) < v:
                        m[k] = v
                r.r = list(m.items())
            r.r.append(ticket)
        for w in writes:
            w.w = ticket
            w.r = []

    def op(self, eng, fn, reads=(), writes=()):
        waits = self._deps(eng, reads, writes)
        self.cnt[eng] += 1
        ticket = (eng, self.cnt[eng])
        self.ops[eng].append((waits, fn, (eng, 1)))
        self._commit(ticket, reads, writes)
        self.nops += 1
        return ticket

    def dma(self, q, out, in_, reads=(), writes=(), final=False, **kw):
        fn = lambda e, out=out, in_=in_, kw=kw: e.dma_start(out=out, in_=in_, **kw)
        return self.dma_fn(q, fn, reads, writes, final)

    def dma_fn(self, q, fn, reads=(), writes=(), final=False):
        waits = self._deps(q, reads, writes)
        if q not in self.dma_slots:
            self.dma_slots[q] = [[("d", q, i), 0] for i in range(NDMA_SLOTS)]
            self.dma_rr[q] = 0
        i = self.dma_rr[q]
        self.dma_rr[q] = (i + 1) % NDMA_SLOTS
        slot = self.dma_slots[q][i]
        key, tot = slot
        if tot > 0 and self.seen[q].get(key, 0) < tot:
            self.seen[q][key] = tot
            waits.append((key, tot))
        slot[1] = tot + 16
        ticket = (key, tot + 16)
        self.ops[q].append((waits, fn, (key, 16)))
        self._commit(ticket, reads, writes)
        self.nops += 1
        if final:
            self.final_waits.append(ticket)
        return ticket

    def flush(self):
        totals = [(e, self.cnt[e]) for e in self.ENG if self.cnt[e] > 0]
        for q, slots in self.dma_slots.items():
            for key, tot in slots:
                if tot > 0:
                    totals.append((key, tot))
        for e in self.ENG:
            self.sem(e)
            for waits, fn, inc in self.ops[e]:
                for k, v in waits:
                    self.sem(k)
                self.sem(inc[0])
        ops = self.ops
        self.ops = {e: [] for e in self.ENG}
        for e in self.ENG:
            for k, v in totals:
                self.seen[e][k] = max(self.seen[e].get(k, 0), v)
        with self.nc.Block() as block:
            def run(engname):
                def body(e):
                    for waits, fn, inc in ops[engname]:
                        for k, v in waits:
                            e.wait_ge(self.sems[k], v)
                        fn(e).then_inc(self.sems[inc[0]], inc[1])
                    for k, v in totals:
                        e.wait_ge(self.sems[k], v)
                return body
            block.sync(run("sp"))
            block.tensor(run("pe"))
            block.scalar(run("act"))
            block.vector(run("dve"))
            block.gpsimd(run("pool"))

    def close(self):
        for cm in reversed(self._stack):
            cm.__exit__(None, None, None)
        self._stack = []


_UID = [0]


class Alloc:
    def __init__(self, nc):
        self.nc = nc
        self.stack = []

    @property
    def n(self):
        _UID[0] += 1
        return _UID[0]

    def sb(self, name, shape, dt):
        cm = self.nc.sbuf_tensor("%s_%d" % (name, self.n), list(shape), dt)
        t = cm.__enter__()
        self.stack.append(cm)
        return t

    def ps(self, name, shape, dt):
        cm = self.nc.psum_tensor("%s_%d" % (name, self.n), list(shape), dt)
        t = cm.__enter__()
        self.stack.append(cm)
        return t

    def close(self):
        for cm in reversed(self.stack):
            cm.__exit__(None, None, None)
        self.stack = []


class Buf:
    def __init__(self, t, excl=False):
        self.t = t
        self.r = Res(excl)


def rot(A, kind, name, shape, dt, n):
    f = A.sb if kind == "sb" else A.ps
    return [Buf(f(name + str(i), shape, dt), excl=(kind == "ps")) for i in range(n)]


def make_ident(A, S, dt):
    b = Buf(A.sb("ident", [128, 128], dt))
    S.op("pool", lambda e: e.memset(b.t[:], 1.0), writes=[b.r])
    S.op("pool", lambda e: e.affine_select(out=b.t[:], in_=b.t[:], pattern=[[-1, 128]], compare_op=ALU.is_equal,
                                           fill=0.0, base=0, channel_multiplier=1), reads=[b.r], writes=[b.r])
    return b


def perm_view(ap2d, r, t0, n):
    if r == 1:
        return ap2d[:, t0:t0 + n]
    sc = SEQ // r
    v = ap2d.rearrange("p (i c) -> p c i", c=r)
    c0, i0 = t0 // sc, t0 % sc
    if i0 + n <= sc:
        return v[:, c0, i0:i0 + n]
    assert i0 == 0 and n % sc == 0
    return v[:, c0:c0 + n // sc, :]


import os as _os
_P1SEC = _os.environ.get("P1SEC", "abcd")
_MSUB = _os.environ.get("MSUB", "qkrvptc")
_MC = _os.environ.get("MC", "1234")


def phase1(nc, S, T, xin, r_xin, l, sc):
    A = Alloc(nc)
    ident = make_ident(A, S, BF16)
    xT = Buf(A.sb("xT", [128, 8, SEQ], BF16))
    xs = rot(A, "sb", "xs", [128, DM], F32, 4)
    xb = rot(A, "sb", "xb", [128, DM], BF16, 3)
    pst = rot(A, "ps", "pst", [128, DM], BF16, 2)
    pj = rot(A, "ps", "pj", [128, 512], F32, 4)
    pv = rot(A, "ps", "pv", [128, 512], F32, 2)
    Win = T["w_in"][l].rearrange("(c p) n -> p c n", p=128)

    for b in range(NB):
        s, c_, p = xs[b % 4], xb[b % 3], pst[b % 2]
        S.dma("sp", s.t[:], xin[b * 128:(b + 1) * 128, :], reads=[r_xin], writes=[s.r])
        S.op("act", lambda e, s=s, c_=c_: e.activation(out=c_.t[:], in_=s.t[:], func=AF.Copy), reads=[s.r], writes=[c_.r])
        for c in range(8):
            S.op("pe", lambda e, c=c, c_=c_, p=p: e.transpose(out=p.t[:, c * 128:(c + 1) * 128], in_=c_.t[:, c * 128:(c + 1) * 128],
                                                              identity=ident.t[:]), reads=[c_.r, ident.r], writes=[p.r])
        S.op("dve", lambda e, b=b, p=p: e.tensor_copy(out=xT.t[:, :, b * 128:(b + 1) * 128],
                                                      in_=p.t[:, :].rearrange("p (c t) -> p c t", c=8)), reads=[p.r], writes=[xT.r])

    wts = rot(A, "sb", "wt", [128, 8, 416], BF16, 2)
    wsw = rot(A, "sb", "wsw", [128, 8, 256], BF16, 2)
    stg = rot(A, "sb", "stg", [128, 512], BF16, 4)
    vst = rot(A, "sb", "vst", [128, NB, 2, 65], BF16, 2)
    tmp1 = rot(A, "sb", "tmp1", [128, 512], F32, 2)
    tmp2 = rot(A, "sb", "tmp2", [128, 512], F32, 2)
    CT = Buf(A.sb("ropeC", [128, SEQ], BF16))
    ST = Buf(A.sb("ropeS", [128, SEQ], BF16))
    for v in vst:
        S.op("pool", lambda e, v=v: e.memset(v.t[:], 1.0), writes=[v.r])
    state = {"w": 0, "pj": 0, "stg": 0, "v": 0, "pv": 0}

    def load_w(col_ranges):
        w = wts[state["w"] % 2]
        state["w"] += 1
        o = 0
        for (c0, n) in col_ranges:
            S.dma("pool", w.t[:, :, o:o + n], Win[:, :, c0:c0 + n], writes=[w.r])
            o += n
        return w

    def nxt(key, lst):
        b = lst[state[key] % len(lst)]
        state[key] += 1
        return b

    def proj_fm(w, o, M, r, j, wtile=None):
        p = nxt("pj", pj)
        wt_ = w if wtile is None else wtile
        for c in range(8):
            S.op("pe", lambda e, c=c, p=p, wt_=wt_: e.matmul(p.t[0:M, :], lhsT=wt_.t[:, c, o:o + M],
                                                             rhs=perm_view(xT.t[:, c, :], r, j * 512, 512),
                                                             start=(c == 0), stop=(c == 7)),
                 reads=[wt_.r, xT.r], writes=[p.r])
        return p

    def store_rows(st, rows, dst, r_dst, j):
        S.dma("sp", dst[:, j * 512:(j + 1) * 512], st.t[rows[0]:rows[1], :], reads=[st.r], writes=[r_dst])

    def v_proj(w, o, r, vt, ncol=128):
        for b4 in range(NB // 4):
            p = nxt("pv", pv)
            for bb in range(4):
                b = b4 * 4 + bb
                for c in range(8):
                    S.op("pe", lambda e, c=c, b=b, bb=bb, p=p: e.matmul(p.t[:, bb * 128:(bb + 1) * 128],
                                                                      lhsT=perm_view(xT.t[:, c, :], r, b * 128, 128),
                                                                      rhs=w.t[:, c, o:o + ncol], start=(c == 0), stop=(c == 7)),
                         reads=[w.r, xT.r], writes=[p.r])
            S.op("dve", lambda e, b4=b4, p=p: e.tensor_copy(
                out=vt.t[:, b4 * 4:(b4 + 1) * 4, :, 0:64],
                in_=p.t[:, :].rearrange("p (b h d) -> p b h d", b=4, h=2)), reads=[p.r], writes=[vt.r])

    def scale_q(w):
        S.op("dve", lambda e: e.tensor_scalar(out=w.t[:, :, 0:128], in0=w.t[:, :, 0:128], scalar1=0.125, scalar2=None,
                                              op0=ALU.mult), reads=[w.r], writes=[w.r])

    for kind, cbase, hbase, pbase in (("sb", C_SB, 0, 0), ("fox", C_FOX, 4, 2)):
        for hp in range(2):
            w = load_w([(cbase + hp * 128, 128), (cbase + 256 + hp * 128, 128), (cbase + 512 + hp * 128, 128)])
            scale_q(w)
            for qk, dst, rd in ((0, sc["QS"], sc["r_QS"]), (1, sc["KS"], sc["r_KS"])):
                for j in range(NT):
                    p = proj_fm(w, qk * 128, 128, 1, j)
                    st = nxt("stg", stg)
                    S.op("act", lambda e, p=p, st=st: e.activation(out=st.t[:], in_=p.t[:], func=AF.Copy), reads=[p.r], writes=[st.r])
                    for hh in range(2):
                        h = hbase + hp * 2 + hh
                        store_rows(st, (hh * 64, hh * 64 + 64), dst[h][0:64, :], rd[h], j)
            vt = nxt("v", vst)
            v_proj(w, 256, 1, vt)
            S.dma("sp", sc["VS"][pbase + hp], vt.t[:, :, :, :].rearrange("p b h d -> p (b h d)"), reads=[vt.r], writes=[sc["r_VS"][pbase + hp]])

    S.flush()
    if "b" not in _P1SEC:
        A.close()
        return
    A2 = Alloc(nc)
    wf = Buf(A2.sb("wf", [128, 8, 4], BF16))
    S.dma("pool", wf.t[:], Win[:, :, C_FOX + 768:C_FOX + 772], writes=[wf.r])
    bfg = Buf(A2.sb("bfg", [4, 1], F32))
    S.dma("sp", bfg.t[:], T["b_forget"][l].rearrange("(h o) -> h o", o=1), writes=[bfg.r])
    S.op("dve", lambda e: e.tensor_scalar(out=bfg.t[:], in0=bfg.t[:], scalar1=-1.0, scalar2=None, op0=ALU.mult), reads=[bfg.r], writes=[bfg.r])
    nlf = Buf(A2.sb("nlf", [4, SEQ], F32))
    ncum = Buf(A2.sb("ncum", [4, SEQ], F32))
    ones4 = Buf(A2.sb("ones4", [4, 512], F32))
    S.op("pool", lambda e: e.memset(ones4.t[:], 1.0), writes=[ones4.r])
    for j in range(NT):
        p = proj_fm(wf, 0, 4, 1, j)
        S.op("act", lambda e, p=p, j=j: e.activation(out=nlf.t[:, j * 512:(j + 1) * 512], in_=p.t[0:4, :], func=AF.Exp,
                                                     bias=bfg.t[:, 0:1], scale=-1.0), reads=[p.r, bfg.r], writes=[nlf.r])
    S.op("act", lambda e: e.activation(out=nlf.t[:], in_=nlf.t[:], func=AF.Ln, bias=1.0), reads=[nlf.r], writes=[nlf.r])
    for j in range(NT):
        sl = slice(j * 512, (j + 1) * 512)
        init = 0.0 if j == 0 else ncum.t[:, j * 512 - 1:j * 512]
        S.op("dve", lambda e, sl=sl, init=init: e.tensor_tensor_scan(out=ncum.t[:, sl], data0=ones4.t[:, :], data1=nlf.t[:, sl],
                                                                     initial=init, op0=ALU.mult, op1=ALU.add),
             reads=[ones4.r, nlf.r, ncum.r], writes=[ncum.r])
    class _Alias:
        def __init__(self, t, r):
            self.t, self.r = t, r
    nlf_b = nlf.t[:, :].bitcast(BF16)
    parts = [_Alias(nlf_b[:, 0:SEQ], nlf.r), _Alias(nlf_b[:, SEQ:2 * SEQ], nlf.r), Buf(A2.sb("cpart2", [4, SEQ], BF16))]
    for i in range(3):
        S.op("dve", lambda e, i=i: e.tensor_copy(out=parts[i].t[:], in_=ncum.t[:]), reads=[ncum.r], writes=[parts[i].r])
        if i < 2:
            S.op("dve", lambda e, i=i: e.tensor_tensor(out=ncum.t[:], in0=ncum.t[:], in1=parts[i].t[:], op=ALU.subtract),
                 reads=[ncum.r, parts[i].r], writes=[ncum.r])
    ones3 = Buf(A2.sb("ones3", [35, SEQ], BF16))
    S.op("pool", lambda e: e.memset(ones3.t[0:3, :], 1.0), writes=[ones3.r])
    S.op("pool", lambda e: e.memset(ones3.t[32:35, :], -1.0), writes=[ones3.r])
    for h in range(4):
        H = 4 + h
        S.dma("sp", sc["KS"][H][64:67, :], ones3.t[32:35, :], reads=[ones3.r], writes=[sc["r_KS"][H]])
        S.dma("sp", sc["QS"][H][67:70, :], ones3.t[0:3, :], reads=[ones3.r], writes=[sc["r_QS"][H]])
        for i in range(3):
            S.dma("sp", sc["KS"][H][67 + i:68 + i, :], parts[i].t[h:h + 1, :], reads=[parts[i].r], writes=[sc["r_KS"][H]])
            S.dma("sp", sc["QS"][H][64 + i:65 + i, :], parts[i].t[h:h + 1, :], reads=[parts[i].r], writes=[sc["r_QS"][H]])
    S.flush()
    A2.close()

    if "c" not in _P1SEC:
        A.close()
        return
    w = load_w([(C_MLA, 416)])
    wkrs = nxt("w", wsw) if False else wsw[0]
    if "p" in _MSUB:
        S.op("pool", lambda e: e.tensor_copy(out=wkrs.t[:, :, 0:16], in_=w.t[:, :, 400:416]), reads=[w.r], writes=[wkrs.r])
        S.op("pool", lambda e: e.tensor_copy(out=wkrs.t[:, :, 16:32], in_=w.t[:, :, 384:400]), reads=[w.r], writes=[wkrs.r])
    A3 = Alloc(nc)
    wuq = Buf(A3.sb("wuq", [128, 2, 384], BF16))
    wuqs = Buf(A3.sb("wuqs", [128, 2, 384], BF16))
    wukv = Buf(A3.sb("wukv", [128, 512], BF16))
    S.dma("pool", wuq.t[:], T["w_uq"][l].rearrange("(c p) n -> p c n", p=128), writes=[wuq.r])
    S.dma("pool", wukv.t[:], T["w_ukv"][l], writes=[wukv.r])
    S.op("pool", lambda e: e.tensor_copy(out=wuqs.t[:], in_=wuq.t[:]), reads=[wuq.r], writes=[wuqs.r])
    for c2 in (range(2) if "p" in _MSUB else []):
        v4o = wuqs.t[:, c2, :].rearrange("p (h d) -> p h d", h=4)
        v4i = wuq.t[:, c2, :].rearrange("p (h d) -> p h d", h=4)
        S.op("pool", lambda e, v4o=v4o, v4i=v4i: e.tensor_copy(out=v4o[:, :, 64:80], in_=v4i[:, :, 80:96]), reads=[wuq.r, wuqs.r], writes=[wuqs.r])
        S.op("pool", lambda e, v4o=v4o, v4i=v4i: e.tensor_copy(out=v4o[:, :, 80:96], in_=v4i[:, :, 64:80]), reads=[wuq.r, wuqs.r], writes=[wuqs.r])
    gcq = Buf(A3.sb("gcq", [128, 2], F32))
    gckv = Buf(A3.sb("gckv", [128, 1], F32))
    for c2 in range(2):
        S.dma("sp", gcq.t[:, c2:c2 + 1], T["g_cq"][l][c2 * 128:(c2 + 1) * 128].rearrange("(p o) -> p o", o=1), writes=[gcq.r])
    S.dma("sp", gckv.t[:], T["g_ckv"][l].rearrange("(p o) -> p o", o=1), writes=[gckv.r])
    for tb, nm in (((CT, "rope32c"), (ST, "rope32s")) if "t" in _MSUB else []):
        S.dma("pool", tb.t[0:32, :], T[nm], writes=[tb.r])
        S.dma("pool", tb.t[64:96, :], T[nm], writes=[tb.r])
    onesq = Buf(A3.sb("onesq", [128, 128], BF16))
    oneskv = Buf(A3.sb("oneskv", [128, 128], BF16))
    epst = Buf(A3.sb("epst", [128, 1], F32))
    S.op("pool", lambda e: e.memset(epst.t[:], RMS_EPS), writes=[epst.r])
    S.op("pool", lambda e: e.memset(onesq.t[:], 1.0 / 256.0), writes=[onesq.r])
    S.op("pool", lambda e: e.memset(oneskv.t[:], 1.0 / 128.0), writes=[oneskv.r])
    cqg = rot(A3, "sb", "cqg", [128, 2, 512], BF16, 2)
    cq2 = rot(A3, "sb", "cq2", [128, 2, 512], BF16, 2)
    ckg = rot(A3, "sb", "ckg", [128, 512], BF16, 2)
    ck2 = rot(A3, "sb", "ck2", [128, 512], BF16, 2)
    rq = rot(A3, "sb", "rq", [128, 512], F32, 2)
    rkv = rot(A3, "sb", "rkv", [128, 512], F32, 2)
    rtok = rot(A3, "sb", "rtok", [128, 1], F32, 2)
    vstm = Buf(A3.sb("vstm", [128, NB, 4, 65], BF16))
    S.op("pool", lambda e: e.memset(vstm.t[:], 1.0), writes=[vstm.r])
    wukv_v = wukv.t[:, :].rearrange("p (h x) -> p h x", h=4)[:, :, 64:128]
    for j in (range(NT) if "c" in _MSUB else []):
        tc = slice(j * 512, (j + 1) * 512)
        a, a2, kg, k2, rq_, rkv_ = cqg[j % 2], cq2[j % 2], ckg[j % 2], ck2[j % 2], rq[j % 2], rkv[j % 2]
        for c2 in (range(2) if "1" in _MC else []):
            p = proj_fm(w, c2 * 128, 128, 1, j)
            S.op("dve", lambda e, p=p, c2=c2, a=a: e.tensor_scalar(out=a.t[:, c2, :], in0=p.t[:], scalar1=gcq.t[:, c2:c2 + 1], scalar2=None,
                                                                  op0=ALU.mult), reads=[p.r, gcq.r], writes=[a.r])
            S.op("act", lambda e, p=p, c2=c2, a2=a2: e.activation(out=a2.t[:, c2, :], in_=p.t[:], func=AF.Square), reads=[p.r], writes=[a2.r])
        if "2" in _MC:
            p = proj_fm(w, 256, 128, 1, j)
            if "5" not in _MC:
                S.op("dve", lambda e, p=p, kg=kg: e.tensor_scalar(out=kg.t[:], in0=p.t[:], scalar1=gckv.t[:, 0:1], scalar2=None, op0=ALU.mult),
                     reads=[p.r, gckv.r], writes=[kg.r])
            if "6" not in _MC:
                S.op("act", lambda e, p=p, k2=k2: e.activation(out=k2.t[:], in_=p.t[:], func=AF.Square), reads=[p.r], writes=[k2.r])
        if "3" in _MC:
            p = nxt("pj", pj)
            for c2 in range(2):
                S.op("pe", lambda e, p=p, c2=c2, a2=a2: e.matmul(p.t[:], lhsT=onesq.t[:], rhs=a2.t[:, c2, :], start=(c2 == 0), stop=(c2 == 1)),
                     reads=[onesq.r, a2.r], writes=[p.r])
            S.op("act", lambda e, p=p, rq_=rq_: e.activation(out=rq_.t[:], in_=p.t[:], func=AF.Sqrt, bias=epst.t[:, 0:1]), reads=[p.r, epst.r], writes=[rq_.r])
            S.op("dve", lambda e, rq_=rq_: e.reciprocal(out=rq_.t[:], in_=rq_.t[:]), reads=[rq_.r], writes=[rq_.r])
        if "4" in _MC:
            p = nxt("pj", pj)
            S.op("pe", lambda e, p=p, k2=k2: e.matmul(p.t[:], lhsT=oneskv.t[:], rhs=k2.t[:], start=True, stop=True), reads=[oneskv.r, k2.r], writes=[p.r])
            S.op("act", lambda e, p=p, rkv_=rkv_: e.activation(out=rkv_.t[:], in_=p.t[:], func=AF.Sqrt, bias=epst.t[:, 0:1]), reads=[p.r, epst.r], writes=[rkv_.r])
            S.op("dve", lambda e, rkv_=rkv_: e.reciprocal(out=rkv_.t[:], in_=rkv_.t[:]), reads=[rkv_.r], writes=[rkv_.r])
        for h in (range(4) if "q" in _MSUB else []):
            H = 8 + h
            pa, pb = nxt("pj", pj), nxt("pj", pj)
            for pp, ww in ((pa, wuq), (pb, wuqs)):
                for c2 in range(2):
                    S.op("pe", lambda e, pp=pp, ww=ww, c2=c2, h=h, a=a: e.matmul(pp.t[0:96, :], lhsT=ww.t[:, c2, h * 96:(h + 1) * 96], rhs=a.t[:, c2, :],
                                                                             start=(c2 == 0), stop=(c2 == 1)), reads=[ww.r, a.r], writes=[pp.r])
            st = nxt("stg", stg)
            t1, t2 = tmp1[h % 2], tmp2[h % 2]
            S.op("dve", lambda e, pa=pa, st=st, rq_=rq_: e.scalar_tensor_tensor(out=st.t[0:64, :], in0=pa.t[0:64, :], scalar=MLA_SCALE, in1=rq_.t[0:64, :],
                                                                             op0=ALU.mult, op1=ALU.mult), reads=[pa.r, rq_.r], writes=[st.r])
            S.op("dve", lambda e, pa=pa, t1=t1, tc=tc: e.tensor_tensor(out=t1.t[64:96, :], in0=pa.t[64:96, :], in1=CT.t[64:96, tc], op=ALU.mult),
                 reads=[pa.r, CT.r], writes=[t1.r])
            S.op("dve", lambda e, pb=pb, t2=t2, tc=tc: e.tensor_tensor(out=t2.t[64:96, :], in0=pb.t[64:96, :], in1=ST.t[64:96, tc], op=ALU.mult),
                 reads=[pb.r, ST.r], writes=[t2.r])
            S.op("pool", lambda e, t1=t1, t2=t2: e.tensor_tensor(out=t1.t[64:96, :], in0=t1.t[64:96, :], in1=t2.t[64:96, :], op=ALU.add),
                 reads=[t1.r, t2.r], writes=[t1.r])
            S.op("dve", lambda e, t1=t1, st=st, rq_=rq_: e.scalar_tensor_tensor(out=st.t[64:96, :], in0=t1.t[64:96, :], scalar=MLA_SCALE, in1=rq_.t[64:96, :],
                                                                             op0=ALU.mult, op1=ALU.mult), reads=[t1.r, rq_.r, st.r], writes=[st.r])
            store_rows(st, (0, 96), sc["QS"][H][0:96, :], sc["r_QS"][H], j)
        for h in (range(4) if "k" in _MSUB else []):
            H = 8 + h
            p = nxt("pj", pj)
            S.op("pe", lambda e, p=p, h=h, kg=kg: e.matmul(p.t[0:64, :], lhsT=wukv.t[:, h * 128:h * 128 + 64], rhs=kg.t[:], start=True, stop=True),
                 reads=[wukv.r, kg.r], writes=[p.r])
            st = nxt("stg", stg)
            S.op("dve", lambda e, p=p, st=st, rkv_=rkv_: e.tensor_tensor(out=st.t[0:64, :], in0=p.t[0:64, :], in1=rkv_.t[0:64, :], op=ALU.mult),
                 reads=[p.r, rkv_.r], writes=[st.r])
            store_rows(st, (0, 64), sc["KS"][H][0:64, :], sc["r_KS"][H], j)
        if "r" not in _MSUB:
            continue
        pa = proj_fm(w, 384, 32, 1, j)
        pb = proj_fm(wkrs, 0, 32, 1, j)
        t1, t2 = tmp1[0], tmp2[0]
        st = nxt("stg", stg)
        S.op("dve", lambda e, pa=pa, t1=t1, tc=tc: e.tensor_tensor(out=t1.t[0:32, :], in0=pa.t[0:32, :], in1=CT.t[0:32, tc], op=ALU.mult),
             reads=[pa.r, CT.r], writes=[t1.r])
        S.op("dve", lambda e, pb=pb, t2=t2, tc=tc: e.tensor_tensor(out=t2.t[0:32, :], in0=pb.t[0:32, :], in1=ST.t[0:32, tc], op=ALU.mult),
             reads=[pb.r, ST.r], writes=[t2.r])
        S.op("pool", lambda e, t1=t1, t2=t2, st=st: e.tensor_tensor(out=st.t[0:32, :], in0=t1.t[0:32, :], in1=t2.t[0:32, :], op=ALU.add),
             reads=[t1.r, t2.r], writes=[st.r])
        for h in range(4):
            store_rows(st, (0, 32), sc["KS"][8 + h][64:96, :], sc["r_KS"][8 + h], j)
        for bb in (range(4) if "v" in _MSUB else []):
            b = j * 4 + bb
            p = nxt("pv", pv)
            S.op("pe", lambda e, p=p, bb=bb, kg=kg: e.matmul(p.t[:, 0:256], lhsT=kg.t[:, bb * 128:(bb + 1) * 128], rhs=wukv_v, start=True, stop=True),
                 reads=[wukv.r, kg.r], writes=[p.r])
            S.op("pe", lambda e, p=p, bb=bb, k2=k2: e.matmul(p.t[:, 256:257], lhsT=k2.t[:, bb * 128:(bb + 1) * 128], rhs=oneskv.t[:, 0:1], start=True, stop=True),
                 reads=[oneskv.r, k2.r], writes=[p.r])
            rt = rtok[b % 2]
            S.op("act", lambda e, p=p, rt=rt: e.activation(out=rt.t[:], in_=p.t[:, 256:257], func=AF.Sqrt, bias=epst.t[:, 0:1]), reads=[p.r, epst.r], writes=[rt.r])
            S.op("dve", lambda e, rt=rt: e.reciprocal(out=rt.t[:], in_=rt.t[:]), reads=[rt.r], writes=[rt.r])
            S.op("dve", lambda e, p=p, b=b, rt=rt: e.tensor_scalar(out=vstm.t[:, b, :, 0:64], in0=p.t[:, 0:256].rearrange("p (h d) -> p h d", h=4),
                                                                  scalar1=rt.t[:, 0:1], scalar2=None, op0=ALU.mult), reads=[p.r, rt.r], writes=[vstm.r])
    for hp in range(2):
        S.dma("sp", sc["VS"][4 + hp].rearrange("p (b h d) -> p b h d", b=NB, h=2), vstm.t[:, :, 2 * hp:2 * hp + 2, :], reads=[vstm.r], writes=[sc["r_VS"][4 + hp]])

    S.flush()
    A3.close()
    if "d" not in _P1SEC:
        A.close()
        return
    for tb, nm in ((CT, "rope64c"), (ST, "rope64s")):
        S.dma("pool", tb.t[0:64, :], T[nm], writes=[tb.r])
        S.dma("pool", tb.t[64:128, :], T[nm], writes=[tb.r])
    for g in range(3):
        r = DIL_R[g]
        for hp in range(2):
            o = g * 256 + hp * 128
            w = load_w([(C_DIL + o, 128), (C_DIL + 768 + o, 128), (C_DIL + 1536 + o, 128)])
            scale_q(w)
            ws = wsw[(g * 2 + hp) % 2]
            for c in range(8):
                vo = ws.t[:, c, :].rearrange("p (h f d) -> p h f d", h=4, f=2)
                vi = w.t[:, c, 0:256].rearrange("p (h f d) -> p h f d", h=4, f=2)
                S.op("pool", lambda e, vo=vo, vi=vi: e.tensor_copy(out=vo[:, :, 0, :], in_=vi[:, :, 1, :]), reads=[w.r], writes=[ws.r])
                S.op("pool", lambda e, vo=vo, vi=vi: e.tensor_copy(out=vo[:, :, 1, :], in_=vi[:, :, 0, :]), reads=[w.r], writes=[ws.r])
            for qk, dst, rd in ((0, sc["QD"], sc["r_QD"]), (1, sc["KD"], sc["r_KD"])):
                for j in range(NT):
                    pa = proj_fm(w, qk * 128, 128, r, j)
                    pb = proj_fm(ws, qk * 128, 128, r, j)
                    t1, t2 = tmp1[j % 2], tmp2[j % 2]
                    st = nxt("stg", stg)
                    cv = perm_view(CT.t[:, :], r, j * 512, 512)
                    sv = perm_view(ST.t[:, :], r, j * 512, 512)
                    shp = None if len(cv.shape) == 2 else cv.shape

                    def v3(ap):
                        return ap if shp is None else ap.rearrange("p (a b) -> p a b", a=shp[1])
                    S.op("dve", lambda e, pa=pa, t1=t1, cv=cv, v3=v3: e.tensor_tensor(out=v3(t1.t[:]), in0=v3(pa.t[:]), in1=cv, op=ALU.mult),
                         reads=[pa.r, CT.r], writes=[t1.r])
                    S.op("dve", lambda e, pb=pb, t2=t2, sv=sv, v3=v3: e.tensor_tensor(out=v3(t2.t[:]), in0=v3(pb.t[:]), in1=sv, op=ALU.mult),
                         reads=[pb.r, ST.r], writes=[t2.r])
                    S.op("pool", lambda e, t1=t1, t2=t2, st=st: e.tensor_tensor(out=st.t[:], in0=t1.t[:], in1=t2.t[:], op=ALU.add),
                         reads=[t1.r, t2.r], writes=[st.r])
                    for hh in range(2):
                        store_rows(st, (hh * 64, hh * 64 + 64), dst[g][hp * 2 + hh], rd[g][hp * 2 + hh], j)
            vt = nxt("v", vst)
            v_proj(w, 256, r, vt)
            S.dma("sp", sc["VD"][g][hp], vt.t[:, :, :, :].rearrange("p b h d -> p (b h d)"), reads=[vt.r], writes=[sc["r_VD"][g][hp]])
    S.flush()
    A.close()


def phase2(nc, S, T, l, sc, heads=None, after_sb=None):
    A = Alloc(nc)
    negtri = Buf(A.sb("negtri", [128, 128], BF16))
    S.op("pool", lambda e: e.memset(negtri.t[:], -1.0), writes=[negtri.r])
    S.op("pool", lambda e: e.affine_select(out=negtri.t[:], in_=negtri.t[:], pattern=[[-1, 128]], compare_op=ALU.is_ge, fill=0.0, base=0,
                                           channel_multiplier=1), reads=[negtri.r], writes=[negtri.r])
    ones = Buf(A.sb("ones", [128, 128], BF16))
    S.op("pool", lambda e: e.memset(ones.t[:], 1.0), writes=[ones.r])
    wn = Buf(A.sb("wn", [65, 64], BF16))
    wnsb = Buf(A.sb("wnsb", [65, 64], BF16))
    for t_, v_ in ((wn, RMS_EPS), (wnsb, 0.0)):
        S.op("pool", lambda e, t_=t_: e.memset(t_.t[:], 1.0 / 64.0), writes=[t_.r])
        S.op("pool", lambda e, t_=t_, v_=v_: e.memset(t_.t[64:65, :], v_), reads=[t_.r], writes=[t_.r])
    gh = Buf(A.sb("gh", [64, 16], F32))
    for h_ in range(16):
        S.dma("sp", gh.t[:, h_:h_ + 1], T["g_head"][l][h_].rearrange("(d o) -> d o", o=1), writes=[gh.r])
    eps2 = Buf(A.sb("eps2", [64, 2], F32))
    S.op("pool", lambda e: e.memset(eps2.t[:, 0:1], RMS_EPS), writes=[eps2.r])
    S.op("pool", lambda e: e.memset(eps2.t[:, 1:2], 0.0), reads=[eps2.r], writes=[eps2.r])

    Qt = rot(A, "sb", "Qt", [128, SEQ], BF16, 4)
    Kt = rot(A, "sb", "Kt", [128, SEQ], BF16, 4)
    Vt = rot(A, "sb", "Vt", [128, NB, 2, 65], BF16, 2)
    pz = rot(A, "ps", "pz", [128, 512], F32, 3)
    po = rot(A, "ps", "po", [128, 512], F32, 2)
    pc = rot(A, "ps", "pc", [128, 512], F32, 2)
    pss = rot(A, "ps", "pss", [128, 512], F32, 1)
    Pb = rot(A, "sb", "Pb", [128, 512], BF16, 8)
    eb = rot(A, "sb", "eb", [128, 512], F32, 2)
    spb = rot(A, "sb", "spb", [128, 512], BF16, 3)
    lw = rot(A, "sb", "lw", [128, 512], F32, 4)
    Rsb = Buf(A.sb("Rsb", [128, 512], F32))
    sqb = rot(A, "sb", "sqb", [65, 512], BF16, 2)
    osb = rot(A, "sb", "osb", [65, 512], F32, 3)
    def mk_mask(name, n, conds):
        m = Buf(A.sb(name, [128, n], BF16))
        S.op("pool", lambda e: e.memset(m.t[:], 1.0), writes=[m.r])
        for (step, base, cm) in conds:
            S.op("pool", lambda e, step=step, base=base, cm=cm: e.affine_select(out=m.t[:], in_=m.t[:], pattern=[[step, n]], compare_op=ALU.is_ge, fill=0.0,
                                                                                base=base, channel_multiplier=cm), reads=[m.r], writes=[m.r])
        return m
    maskS = [mk_mask("ms%d" % o, 512, [(1, -128 * o - 1, -1)]) for o in (3, 2, 1, 0)][::-1]
    maskC = [mk_mask("mc%d" % o, 512, [(1, -128 * o, -1)]) for o in range(4)]
    maskD = {512: {o: mk_mask("md%d" % (o + 1), 512, [(1, -128 * o, -1), (-1, 128 + 128 * o, 1)]) for o in range(-1, 4)},
             256: {o: mk_mask("me%d" % o, 256, [(1, -128 * o, -1), (-1, 128 + 128 * o, 1)]) for o in range(0, 2)}}
    stb = rot(A, "sb", "stb", [64, 512], F32, 2)
    yb = rot(A, "sb", "yb", [64, 512], BF16, 2)
    acc = rot(A, "sb", "acc", [65, SEQ], F32, 2)
    st = {"fin": 0, "ld": 0, "vld": 0, "o": 0}

    def finish(src_ap, r_src, h, t0, n, is_sb, in_sbuf=False):
        i = st["fin"]
        st["fin"] += 1
        sq, s_, y, ps_ = sqb[i % 2], stb[i % 2], yb[i % 2], pss[0]
        if in_sbuf:
            o_ap, r_o = src_ap, r_src
        else:
            ob = osb[i % 3]
            S.op("act", lambda e: e.activation(out=ob.t[:, 0:n], in_=src_ap, func=AF.Copy), reads=[r_src], writes=[ob.r])
            o_ap, r_o = ob.t[:, 0:n], ob.r
        S.op("dve", lambda e: e.tensor_tensor(out=sq.t[:, 0:n], in0=o_ap, in1=o_ap, op=ALU.mult), reads=[r_o], writes=[sq.r])
        wn_ = wnsb if is_sb else wn
        S.op("pe", lambda e: e.matmul(ps_.t[0:64, 0:n], lhsT=wn_.t[:, :], rhs=sq.t[:, 0:n], start=True, stop=True), reads=[wn_.r, sq.r], writes=[ps_.r])
        S.op("act", lambda e: e.activation(out=s_.t[:, 0:n], in_=ps_.t[0:64, 0:n], func=AF.Ln, bias=(eps2.t[:, 0:1] if is_sb else eps2.t[:, 1:2])),
             reads=[ps_.r, eps2.r], writes=[s_.r])
        S.op("act", lambda e: e.activation(out=s_.t[:, 0:n], in_=s_.t[:, 0:n], func=AF.Exp, scale=-0.5), reads=[s_.r], writes=[s_.r])
        S.op("dve", lambda e: e.scalar_tensor_tensor(out=y.t[:, 0:n], in0=o_ap[0:64], scalar=gh.t[:, h:h + 1], in1=s_.t[:, 0:n],
                                                     op0=ALU.mult, op1=ALU.mult), reads=[r_o, gh.r, s_.r], writes=[y.r])
        S.dma("sp", sc["OnT"][h // 2, (h % 2) * 64:(h % 2) * 64 + 64, t0:t0 + n], y.t[:, 0:n], reads=[y.r], writes=[sc["r_OnT"]])

    def load_qk(qsrc, r_q, ksrc, r_k, kd):
        i = st["ld"]
        st["ld"] += 1
        q, k = Qt[i % 4], Kt[i % 4]
        S.dma("sp", q.t[0:kd, :], qsrc, reads=[r_q], writes=[q.r])
        S.dma("sp", k.t[0:kd, :], ksrc, reads=[r_k], writes=[k.r])
        return q, k

    def load_v(vsrc, r_v):
        i = st["vld"]
        st["vld"] += 1
        v = Vt[i % 2]
        S.dma("sp", v.t[:, :, :, :].rearrange("p b h d -> p (b h d)"), vsrc, reads=[r_v], writes=[v.r])
        return v

    def make_stages(steps, q, k, kd, v, hh, kind, done_cb, u=0):
        n_ = len(steps)
        ctx = [dict() for _ in range(n_)]
        zsel = [[pz[0], pz[1]], [pz[2], pc[0]]][u]

        def s1(i):
            sp_ = steps[i]
            z = zsel[i % 2]
            q0, n, kb = sp_["q0"], sp_["n"], sp_["kb"]
            S.op("pe", lambda e: e.matmul(z.t[:, 0:n], lhsT=k.t[0:kd, kb * 128:(kb + 1) * 128], rhs=q.t[0:kd, q0:q0 + n], start=True, stop=True),
                 reads=[k.r, q.r], writes=[z.r])
            if kind == "sb":
                e_, s_ = eb[i % 2], spb[i % 3]
                S.op("act", lambda e: e.activation(out=e_.t[:, 0:n], in_=z.t[:, 0:n], func=AF.Exp), reads=[z.r], writes=[e_.r])
                S.op("act", lambda e: e.activation(out=s_.t[:, 0:n], in_=e_.t[:, 0:n], func=AF.Ln, bias=1.0), reads=[e_.r], writes=[s_.r])
                if sp_["mask"] is not None:
                    mk = sp_["mask"]
                    S.op("pool", lambda e: e.tensor_tensor(out=s_.t[:, 0:n], in0=s_.t[:, 0:n], in1=mk.t[:, 0:n], op=ALU.mult), reads=[s_.r, mk.r], writes=[s_.r])
                ctx[i]["sp"] = s_
            else:
                p_ = Pb[u * 4 + i % 4]
                if kind == "fox" and sp_["mask"] is not None:
                    l_ = lw[u * 2 + i % 2]
                    S.op("dve", lambda e: e.tensor_scalar(out=l_.t[:, 0:n], in0=z.t[:, 0:n], scalar1=60.0, scalar2=None, op0=ALU.min), reads=[z.r], writes=[l_.r])
                    S.op("act", lambda e: e.activation(out=p_.t[:, 0:n], in_=l_.t[:, 0:n], func=AF.Exp), reads=[l_.r], writes=[p_.r])
                else:
                    S.op("act", lambda e: e.activation(out=p_.t[:, 0:n], in_=z.t[:, 0:n], func=AF.Exp), reads=[z.r], writes=[p_.r])
                if sp_["mask"] is not None:
                    mk = sp_["mask"]
                    S.op("dve", lambda e: e.tensor_tensor(out=p_.t[:, 0:n], in0=p_.t[:, 0:n], in1=mk.t[:, 0:n], op=ALU.mult), reads=[p_.r, mk.r], writes=[p_.r])
                ctx[i]["P"] = p_

        def s2(i):
            if kind != "sb":
                return
            sp_ = steps[i]
            q0, n, kb = sp_["q0"], sp_["n"], sp_["kb"]
            s_ = ctx[i]["sp"]
            c_, rc, l_, p_ = pc[i % 2], pz[2], lw[i % 2], Pb[i % 4]
            S.op("pe", lambda e: e.matmul(c_.t[:, 0:n], lhsT=k.t[0:kd, kb * 128:(kb + 1) * 128], rhs=q.t[0:kd, q0:q0 + n], start=True, stop=False),
                 reads=[k.r, q.r], writes=[c_.r])
            S.op("pe", lambda e: e.matmul(c_.t[:, 0:n], lhsT=negtri.t[:], rhs=s_.t[:, 0:n], start=False, stop=True), reads=[negtri.r, s_.r], writes=[c_.r])
            S.op("pe", lambda e: e.matmul(rc.t[:, 0:n], lhsT=ones.t[:], rhs=s_.t[:, 0:n], start=True, stop=True), reads=[ones.r, s_.r], writes=[rc.r])
            if sp_["first"]:
                S.op("dve", lambda e: e.tensor_copy(out=l_.t[:, 0:n], in_=c_.t[:, 0:n]), reads=[c_.r], writes=[l_.r])
                S.op("dve", lambda e: e.tensor_copy(out=Rsb.t[:, 0:n], in_=rc.t[:, 0:n]), reads=[rc.r], writes=[Rsb.r])
            else:
                S.op("dve", lambda e: e.tensor_tensor(out=l_.t[:, 0:n], in0=c_.t[:, 0:n], in1=Rsb.t[:, 0:n], op=ALU.subtract), reads=[c_.r, Rsb.r], writes=[l_.r])
                S.op("dve", lambda e: e.tensor_tensor(out=Rsb.t[:, 0:n], in0=rc.t[:, 0:n], in1=Rsb.t[:, 0:n], op=ALU.add), reads=[rc.r, Rsb.r], writes=[Rsb.r])
            S.op("act", lambda e: e.activation(out=p_.t[:, 0:n], in_=l_.t[:, 0:n], func=AF.Exp), reads=[l_.r], writes=[p_.r])
            if sp_["mask"] is not None:
                mk = sp_["mask"]
                S.op("pool", lambda e: e.tensor_tensor(out=p_.t[:, 0:n], in0=p_.t[:, 0:n], in1=mk.t[:, 0:n], op=ALU.mult), reads=[p_.r, mk.r], writes=[p_.r])
            ctx[i]["P"] = p_

        def s3(i):
            sp_ = steps[i]
            n, kb = sp_["n"], sp_["kb"]
            if sp_["first"] and u == 0:
                st["o"] += 1
            o_ = po[st["o"] % 2] if u == 0 else pc[1]
            p_ = ctx[i]["P"]
            S.op("pe", lambda e: e.matmul(o_.t[0:65, 0:n], lhsT=v.t[:, kb, hh, :], rhs=p_.t[:, 0:n], start=sp_["first"], stop=sp_["last"]),
                 reads=[v.r, p_.r], writes=[o_.r])
            if sp_["last"]:
                done_cb(o_, sp_)

        return n_, s1, s2, s3

    def drive(stage_sets):
        nmax = max(ss[0] for ss in stage_sets)
        for i in range(nmax + 2):
            for n_, s1, s2, s3 in stage_sets:
                if i < n_:
                    s1(i)
            for n_, s1, s2, s3 in stage_sets:
                if 0 <= i - 1 < n_:
                    s2(i - 1)
            for n_, s1, s2, s3 in stage_sets:
                if 0 <= i - 2 < n_:
                    s3(i - 2)

    def run_steps(steps, q, k, kd, v, hh, kind, done_cb):
        drive([make_stages(steps, q, k, kd, v, hh, kind, done_cb, 0)])

    def causal_steps(strict, descending):
        steps = []
        for qt in range(NT):
            q0 = qt * 512
            kbs = list(range(0, 4 * qt + 4))
            if descending:
                kbs = kbs[::-1]
            for ii, kb in enumerate(kbs):
                o = kb - 4 * qt
                steps.append(dict(q0=q0, n=512, kb=kb, mask=((maskS if strict else maskC)[o] if o >= 0 else None), first=(ii == 0), last=(ii == len(kbs) - 1)))
        return steps

    def dil_steps(r):
        sc_ = SEQ // r
        n = min(512, sc_)
        steps = []
        for q0 in range(0, SEQ, n):
            cs = (q0 // sc_) * sc_
            k_lo = max(cs, q0 - 128)
            kbs = list(range(k_lo // 128, (q0 + n) // 128))
            for ii, kb in enumerate(kbs):
                steps.append(dict(q0=q0, n=n, kb=kb, mask=maskD[n][kb - q0 // 128], first=(ii == 0), last=(ii == len(kbs) - 1)))
        return steps

    hsel = (lambda h: True) if heads is None else (lambda h: h in heads)
    groups = []
    for kind, hbase, pbase, kd in (("sb", 0, 0, 64), ("fox", 4, 2, 70), ("mla", 8, 4, 96)):
        for hp in range(2):
            js = [(kind, hbase + hp * 2 + hh, pbase + hp, hh, kd) for hh in range(2) if hsel(hbase + hp * 2 + hh)]
            if kind == "sb":
                groups += [[j] for j in js]
            elif js:
                groups.append(js)
    step_cache = {"sb": causal_steps(True, True), "fox": causal_steps(False, False)}
    step_cache["mla"] = step_cache["fox"]
    loaded = {}
    vcur = {}

    def prefetch(group):
        for job in group:
            kind, h, pr_, hh, kd = job
            if pr_ not in vcur:
                vcur.clear()
                vcur[pr_] = load_v(sc["VS"][pr_], sc["r_VS"][pr_])
            loaded[h] = load_qk(sc["QS"][h][0:kd, :], sc["r_QS"][h], sc["KS"][h][0:kd, :], sc["r_KS"][h], kd) + (vcur[pr_],)
    if groups:
        prefetch(groups[0])
    for gi, group in enumerate(groups):
        if after_sb is not None and group[0][0] != "sb":
            after_sb()
            after_sb = None
        cur = [loaded.pop(job[1]) for job in group]
        if gi + 1 < len(groups):
            prefetch(groups[gi + 1])
        sets = []
        for u, (job, (q, k, v)) in enumerate(zip(group, cur)):
            kind, h, pr_, hh, kd = job

            def done(o_, sp_, h=h, kind=kind):
                finish(o_.t[0:65, 0:sp_["n"]], o_.r, h, sp_["q0"], sp_["n"], kind == "sb")
            sets.append(make_stages(step_cache[kind], q, k, kd, v, hh, kind, done, u))
        drive(sets)
    if after_sb is not None:
        after_sb()
    dgroups = [(hp, g) for hp in range(2) if (hsel(12 + 2 * hp) or hsel(13 + 2 * hp)) for g in range(3)]
    dsteps = {g: dil_steps(DIL_R[g]) for g in range(3)}
    dl = {}

    def dprefetch(grp):
        hp, g = grp
        v = load_v(sc["VD"][g][hp], sc["r_VD"][g][hp])
        dl[grp] = [load_qk(sc["QD"][g][hp * 2 + hh], sc["r_QD"][g][hp * 2 + hh], sc["KD"][g][hp * 2 + hh], sc["r_KD"][g][hp * 2 + hh], 64) + (v,)
                   for hh in range(2)]
    if dgroups:
        dprefetch(dgroups[0])
    for gi, grp in enumerate(dgroups):
        hp, g = grp
        r = DIL_R[g]
        cur = dl.pop(grp)
        if gi + 1 < len(dgroups):
            dprefetch(dgroups[gi + 1])
        sets = []
        for hh in range(2):
            q, k, v = cur[hh]
            a_ = acc[hh]

            def done(o_, sp_, a_=a_, r=r, g=g):
                n, q0 = sp_["n"], sp_["q0"]
                dst = perm_view(a_.t[:, :], r, q0, n)
                if g == 0:
                    S.op("act", lambda e: e.activation(out=dst, in_=o_.t[0:65, 0:n], func=AF.Copy), reads=[o_.r], writes=[a_.r])
                else:
                    S.op("dve", lambda e: e.tensor_tensor(out=dst, in0=o_.t[0:65, 0:n], in1=dst, op=ALU.add), reads=[o_.r, a_.r], writes=[a_.r])
            sets.append(make_stages(dsteps[g], q, k, 64, v, hh, "dil", done, hh))
        drive(sets)
        if g == 2:
            for h2 in range(2):
                for qt in range(NT):
                    finish(acc[h2].t[:, qt * 512:(qt + 1) * 512], acc[h2].r, 12 + hp * 2 + h2, qt * 512, 512, False, in_sbuf=True)
    S.flush()
    A.close()


def layernorm_block(S, y, g_b, b_b, small, out):
    st6, mv, rs = small["st6"], small["mv"], small["rs"]
    for hf in range(2):
        S.op("dve", lambda e, hf=hf: e.bn_stats(out=st6.t[:, hf, :], in_=y.t[:, hf * 512:(hf + 1) * 512]), reads=[y.r], writes=[st6.r])
    S.op("dve", lambda e: e.bn_aggr(out=mv.t[:], in_=st6.t[:, :, :].rearrange("p a b -> p (a b)")), reads=[st6.r], writes=[mv.r])
    S.op("act", lambda e: e.activation(out=rs.t[:], in_=mv.t[:, 1:2], func=AF.Ln, bias=small["eps"].t[:, 0:1]), reads=[mv.r, small["eps"].r], writes=[rs.r])
    S.op("act", lambda e: e.activation(out=rs.t[:], in_=rs.t[:], func=AF.Exp, scale=-0.5), reads=[rs.r], writes=[rs.r])
    S.op("dve", lambda e: e.scalar_tensor_tensor(out=y.t[:], in0=y.t[:], scalar=mv.t[:, 0:1], in1=g_b.t[:], op0=ALU.subtract, op1=ALU.mult),
         reads=[y.r, mv.r, g_b.r], writes=[y.r])
    S.op("dve", lambda e: e.scalar_tensor_tensor(out=out.t[:], in0=y.t[:], scalar=rs.t[:, 0:1], in1=b_b.t[:], op0=ALU.mult, op1=ALU.add),
         reads=[y.r, rs.r, b_b.r], writes=[out.r])


def bcast_row(S, A, name, src1d, n):
    b = Buf(A.sb(name, [128, n], F32))
    S.dma("sp", b.t[:], src1d.rearrange("(o n) -> o n", o=1).partition_broadcast(128), writes=[b.r])
    return b


def phase3(nc, S, T, xin, r_xin, l, sc):
    A = Alloc(nc)
    identf = make_ident(A, S, F32)
    wout = Buf(A.sb("wout", [128, 8, DM], BF16))
    S.dma("pool", wout.t[:], T["w_out"][l].rearrange("(c p) n -> p c n", p=128), writes=[wout.r])
    wr = Buf(A.sb("wr", [128, 8, 20], F32))
    S.dma("sp", wr.t[:, :, 0:4], T["w_group"][l].rearrange("(c p) n -> p c n", p=128), writes=[wr.r])
    S.dma("sp", wr.t[:, :, 4:20], T["w_expert"][l].rearrange("(c p) n -> p c n", p=128), writes=[wr.r])
    brt = Buf(A.sb("brt", [128, 20], F32))
    S.dma("sp", brt.t[:, 0:4], T["b_group"][l].rearrange("(o n) -> o n", o=1).partition_broadcast(128), writes=[brt.r])
    S.dma("sp", brt.t[:, 4:20], T["b_expert"][l].rearrange("(o n) -> o n", o=1).partition_broadcast(128), writes=[brt.r])
    g_b = bcast_row(S, A, "ln1g", T["ln1_g"][l], DM)
    b_b = bcast_row(S, A, "ln1b", T["ln1_b"][l], DM)
    on = rot(A, "sb", "on", [128, 8, 512], BF16, 2)
    xs = rot(A, "sb", "xs3", [128, DM], F32, 4)
    y = rot(A, "sb", "y3", [128, DM], F32, 3)
    x1 = rot(A, "sb", "x1o", [128, DM], F32, 6)
    xtf = rot(A, "sb", "xtf", [128, 8, 128], F32, 3)
    xtb = rot(A, "sb", "xtb", [128, 8, 512], BF16, 2)
    gate = Buf(A.sb("gate", [128, NB, 16], F32))
    lgall = Buf(A.sb("lgall", [128, NB, 20], F32))
    if ROUTED:
        x1b = Buf(A.sb("x1b", [128, NB, DM], BF16))
        gohall = Buf(A.sb("gohall", [128, NB, 4], F32))
        S.op("pool", lambda e: e.memset(x1b.t[:, 0:4, :], 0.0), writes=[x1b.r])
        S.op("pool", lambda e: e.memset(gate.t[:], 0.0), writes=[gate.r])
        for k in (range(NTILE) if int(_os_env.get("ZF", "1")) else []):
            S.dma("sp", sc["XS"][k * 512:(k + 1) * 512, :].rearrange("(p r) n -> p (r n)", p=128), x1b.t[:, 0:4, :].rearrange("p b n -> p (b n)"),
                  reads=[x1b.r], writes=[sc["r_XS"]])
        for k in (range(NTILE) if int(_os_env.get("ZF", "1")) else []):
            S.dma("sp", sc["GS"][k * 512:(k + 1) * 512, :].rearrange("(p r) n -> p (r n)", p=128), gate.t[:, 0:4, :].rearrange("p b n -> p (b n)"),
                  reads=[gate.r], writes=[sc["r_GS"]])
    ph = rot(A, "ps", "ph", [128, DM], F32, 2)
    ptr = rot(A, "ps", "ptr", [128, DM], F32, 1)
    plg = rot(A, "ps", "plg", [128, 512], F32, 2)
    epsl = Buf(A.sb("epsl", [128, 1], F32))
    S.op("pool", lambda e: e.memset(epsl.t[:], LN_EPS), writes=[epsl.r])
    small = [dict(st6=Buf(A.sb("st6", [128, 2, 6], F32)), mv=Buf(A.sb("mv", [128, 2], F32)), rs=Buf(A.sb("rs", [128, 1], F32)), eps=epsl) for _ in range(3)]
    pend = []
    pend2 = []

    def ld_on(j):
        S.dma("sp", on[j % 2].t[:], sc["OnT"][:, :, j * 512:(j + 1) * 512].rearrange("c p t -> p c t"), reads=[sc["r_OnT"]], writes=[on[j % 2].r])

    def ld_x(b):
        S.dma("sp", xs[b % 4].t[:], xin[b * 128:(b + 1) * 128, :], reads=[r_xin], writes=[xs[b % 4].r])
    for j in range(NT):
        o_ = on[j % 2]
        if j == 0:
            ld_on(0)
        if j + 1 < NT:
            ld_on(j + 1)
        xb_ = xtb[j % 2]
        for bb in range(4):
            b = j * 4 + bb
            s_, y_, x1_, xf_, p_, sm = xs[b % 4], y[b % 3], x1[b % 6], xtf[b % 3], ph[b % 2], small[b % 3]
            if b == 0:
                ld_x(0)
                ld_x(1)
            if b + 2 < NB:
                ld_x(b + 2)
            for hf in range(2):
                for c in range(8):
                    S.op("pe", lambda e, hf=hf, c=c, bb=bb, o_=o_, p_=p_: e.matmul(p_.t[:, hf * 512:(hf + 1) * 512], lhsT=o_.t[:, c, bb * 128:(bb + 1) * 128],
                                                                            rhs=wout.t[:, c, hf * 512:(hf + 1) * 512], start=(c == 0), stop=(c == 7)),
                         reads=[o_.r, wout.r], writes=[p_.r])
            S.op("dve", lambda e, s_=s_, y_=y_, p_=p_: e.scalar_tensor_tensor(out=y_.t[:], in0=s_.t[:], scalar=ALPHA, in1=p_.t[:], op0=ALU.mult, op1=ALU.add),
                 reads=[s_.r, p_.r], writes=[y_.r])
            layernorm_block(S, y_, g_b, b_b, sm, x1_)
            S.dma("sp", sc["X1"][b * 128:(b + 1) * 128, :], x1_.t[:], reads=[x1_.r], writes=[sc["r_X1"]])
            if ROUTED:
                S.op("act", lambda e, b=b, x1_=x1_: e.activation(out=x1b.t[:, b, :], in_=x1_.t[:], func=AF.Copy), reads=[x1_.r], writes=[x1b.r])
            def stage_b(b=b, bb=bb, x1_=x1_, xf_=xf_, xb_=xb_):
                pt = ptr[0]
                for c in range(8):
                    S.op("pe", lambda e, c=c: e.transpose(out=pt.t[:, c * 128:(c + 1) * 128], in_=x1_.t[:, c * 128:(c + 1) * 128], identity=identf.t[:]),
                         reads=[x1_.r, identf.r], writes=[pt.r])
                S.op("act", lambda e: e.activation(out=xf_.t[:, :, :], in_=pt.t[:, :].rearrange("p (c t) -> p c t", c=8), func=AF.Copy),
                     reads=[pt.r], writes=[xf_.r])
                if not ROUTED:
                    S.op("dve", lambda e: e.tensor_copy(out=xb_.t[:, :, bb * 128:(bb + 1) * 128], in_=pt.t[:, :].rearrange("p (c t) -> p c t", c=8)),
                         reads=[pt.r], writes=[xb_.r])
                def stage_c():
                    pl = plg[b % 2]
                    for c in range(8):
                        S.op("pe", lambda e, c=c: e.matmul(pl.t[:, 0:20], lhsT=xf_.t[:, c, :], rhs=wr.t[:, c, :], start=(c == 0), stop=(c == 7)),
                             reads=[xf_.r, wr.r], writes=[pl.r])
                    S.op("dve", lambda e: e.tensor_tensor(out=lgall.t[:, b, :], in0=pl.t[:, 0:20], in1=brt.t[:], op=ALU.add), reads=[pl.r, brt.r], writes=[lgall.r])
                if int(_os_env.get("INL", "0")):
                    stage_c()
                else:
                    pend2.append(stage_c)
            pend.append(stage_b)
            if len(pend2) > int(_os_env.get("LAG2", "1")):
                pend2.pop(0)()
            if len(pend) > 3:
                pend.pop(0)()
            if (not ROUTED) and bb == 3:
                while pend:
                    pend.pop(0)()
                while pend2:
                    pend2.pop(0)()
        if not ROUTED:
            S.dma("sp", sc["X1T"][:, :, j * 512:(j + 1) * 512], xb_.t[:], reads=[xb_.r], writes=[sc["r_X1T"]])
    while pend:
        pend.pop(0)()
        while len(pend2) > 1:
            pend2.pop(0)()
    while pend2:
        pend2.pop(0)()
    def GT(name, shape):
        return Buf(A.sb("gv_" + name, shape, F32))
    B3 = [128, NB, 4]
    gl = lgall.t[:, :, 0:4]
    el = lgall.t[:, :, 4:20].rearrange("p b (g x) -> p b g x", g=4)
    m_, goh, tmp, se = GT("m", [128, NB]), (gohall if ROUTED else GT("goh", B3)), GT("tmp", B3), GT("se", [128, NB])
    t44, es, m1, oh1, es2, m2, oh2 = GT("t44", [128, NB, 4, 4]), GT("es", B3), GT("m1", [128, NB]), GT("oh1", B3), GT("es2", B3), GT("m2", [128, NB]), GT("oh2", B3)
    d_, p1, p2, gi = GT("d", [128, NB]), GT("p1", [128, NB]), GT("p2", [128, NB]), GT("gi", B3)

    def bc(t2):
        return t2.t[:, :].unsqueeze(2).to_broadcast(B3)

    def D(fn, reads, writes):
        S.op("dve", fn, reads=[x.r for x in reads], writes=[x.r for x in writes])
    D(lambda e: e.tensor_reduce(out=m_.t[:], in_=gl, axis=AX.X, op=ALU.max), [lgall], [m_])
    D(lambda e: e.tensor_tensor(out=goh.t[:], in0=gl, in1=bc(m_), op=ALU.is_equal), [lgall, m_], [goh])
    D(lambda e: e.tensor_tensor(out=tmp.t[:], in0=gl, in1=bc(m_), op=ALU.subtract), [lgall, m_], [tmp])
    S.op("act", lambda e: e.activation(out=tmp.t[:], in_=tmp.t[:], func=AF.Exp), reads=[tmp.r], writes=[tmp.r])
    D(lambda e: e.tensor_reduce(out=se.t[:], in_=tmp.t[:], axis=AX.X, op=ALU.add), [tmp], [se])
    D(lambda e: e.reciprocal(out=se.t[:], in_=se.t[:]), [se], [se])
    D(lambda e: e.tensor_tensor(out=t44.t[:], in0=el, in1=goh.t[:, :, :].unsqueeze(3).to_broadcast([128, NB, 4, 4]), op=ALU.mult), [lgall, goh], [t44])
    D(lambda e: e.tensor_reduce(out=es.t[:], in_=t44.t[:, :, :, :].rearrange("p b g x -> p b x g"), axis=AX.X, op=ALU.add), [t44], [es])
    D(lambda e: e.tensor_reduce(out=m1.t[:], in_=es.t[:], axis=AX.X, op=ALU.max), [es], [m1])
    D(lambda e: e.tensor_tensor(out=oh1.t[:], in0=es.t[:], in1=bc(m1), op=ALU.is_equal), [es, m1], [oh1])
    D(lambda e: e.scalar_tensor_tensor(out=es2.t[:], in0=oh1.t[:], scalar=-1e30, in1=es.t[:], op0=ALU.mult, op1=ALU.add), [oh1, es], [es2])
    D(lambda e: e.tensor_reduce(out=m2.t[:], in_=es2.t[:], axis=AX.X, op=ALU.max), [es2], [m2])
    D(lambda e: e.tensor_tensor(out=oh2.t[:], in0=es2.t[:], in1=bc(m2), op=ALU.is_equal), [es2, m2], [oh2])
    D(lambda e: e.tensor_tensor(out=d_.t[:], in0=m2.t[:], in1=m1.t[:], op=ALU.subtract), [m1, m2], [d_])
    S.op("act", lambda e: e.activation(out=d_.t[:], in_=d_.t[:], func=AF.Exp), reads=[d_.r], writes=[d_.r])
    D(lambda e: e.tensor_scalar(out=p1.t[:], in0=d_.t[:], scalar1=1.0, scalar2=None, op0=ALU.add), [d_], [p1])
    D(lambda e: e.reciprocal(out=p1.t[:], in_=p1.t[:]), [p1], [p1])
    D(lambda e: e.tensor_tensor(out=p2.t[:], in0=d_.t[:], in1=p1.t[:], op=ALU.mult), [d_, p1], [p2])
    D(lambda e: e.tensor_tensor(out=gi.t[:], in0=oh1.t[:], in1=bc(p1), op=ALU.mult), [oh1, p1], [gi])
    D(lambda e: e.tensor_tensor(out=oh2.t[:], in0=oh2.t[:], in1=bc(p2), op=ALU.mult), [oh2, p2], [oh2])
    D(lambda e: e.tensor_tensor(out=gi.t[:], in0=gi.t[:], in1=oh2.t[:], op=ALU.add), [gi, oh2], [gi])
    D(lambda e: e.tensor_tensor(out=gi.t[:], in0=gi.t[:], in1=bc(se), op=ALU.mult), [gi, se], [gi])
    D(lambda e: e.tensor_tensor(out=gate.t[:, :, :].rearrange("p b (g x) -> p b g x", g=4), in0=goh.t[:, :, :].unsqueeze(3).to_broadcast([128, NB, 4, 4]),
                                in1=gi.t[:, :, :].unsqueeze(2).to_broadcast([128, NB, 4, 4]), op=ALU.mult), [goh, gi], [gate])
    if not ROUTED:
        S.dma("sp", sc["GATE"], gate.t[:, :, :].rearrange("p b e -> p (b e)"), reads=[gate.r], writes=[sc["r_GATE"]])
    else:
        route_epilogue(S, A, sc, x1b, gate, gohall, plg, l)
    S.flush()
    A.close()


def route_epilogue(S, A, sc, x1b, gate, gohall, plg, l):
    def T_(name, shape, dt=F32):
        return Buf(A.sb(name, shape, dt))
    onesf = T_("onesf", [128, 128])
    tris = T_("tris", [128, 128])
    S.op("pool", lambda e: e.memset(onesf.t[:], 1.0), writes=[onesf.r])
    S.op("pool", lambda e: e.memset(tris.t[:], 1.0), writes=[tris.r])
    S.op("pool", lambda e: e.affine_select(out=tris.t[:], in_=tris.t[:], pattern=[[1, 128]], compare_op=ALU.is_ge, fill=0.0, base=-1,
                                           channel_multiplier=-1), reads=[tris.r], writes=[tris.r])
    pt, pr = plg[0], plg[1]
    for b in range(NB):
        S.op("pe", lambda e, b=b: e.matmul(pt.t[:, b * 4:(b + 1) * 4], lhsT=onesf.t[:], rhs=gohall.t[:, b, :], start=True, stop=True),
             reads=[onesf.r, gohall.r], writes=[pt.r])
        S.op("pe", lambda e, b=b: e.matmul(pr.t[:, b * 4:(b + 1) * 4], lhsT=tris.t[:], rhs=gohall.t[:, b, :], start=True, stop=True),
             reads=[tris.r, gohall.r], writes=[pr.r])
    totb = T_("totb", [128, NB, 4])
    cum = T_("cumb", [128, NB, 4])
    ones32 = T_("ones32", [128, NB])
    S.op("pool", lambda e: e.memset(ones32.t[:], 1.0), writes=[ones32.r])
    S.op("dve", lambda e: e.tensor_copy(out=totb.t[:, :, :], in_=pt.t[:, 0:NB * 4].rearrange("p (b g) -> p b g", g=4)), reads=[pt.r], writes=[totb.r])
    for g in range(4):
        S.op("dve", lambda e, g=g: e.tensor_tensor_scan(out=cum.t[:, :, g], data0=ones32.t[:, :], data1=totb.t[:, :, g], initial=0.0,
                                                        op0=ALU.mult, op1=ALU.add), reads=[ones32.r, totb.r, cum.r], writes=[cum.r])
    boffx = T_("boffx", [128, NB, 4])
    S.op("dve", lambda e: e.tensor_tensor(out=boffx.t[:], in0=cum.t[:], in1=totb.t[:], op=ALU.subtract), reads=[cum.r, totb.r], writes=[boffx.r])
    thr_i = T_("thri", [128, 16], I32)
    thr = T_("thr", [128, 16])
    S.op("pool", lambda e: e.iota(thr_i.t[:], pattern=[[512, 16]], base=0, channel_multiplier=0), writes=[thr_i.r])
    S.op("dve", lambda e: e.tensor_copy(out=thr.t[:], in_=thr_i.t[:]), reads=[thr_i.r], writes=[thr.r])
    cmp = T_("cmp", [128, 4, 8])
    ntl = T_("ntl", [128, 4])
    S.op("dve", lambda e: e.tensor_tensor(out=cmp.t[:], in0=cum.t[:, NB - 1, :].unsqueeze(2).to_broadcast([128, 4, 8]),
                                          in1=thr.t[:, 0:8].unsqueeze(1).to_broadcast([128, 4, 8]), op=ALU.is_gt), reads=[cum.r, thr.r], writes=[cmp.r])
    S.op("dve", lambda e: e.tensor_reduce(out=ntl.t[:], in_=cmp.t[:], axis=AX.X, op=ALU.add), reads=[cmp.r], writes=[ntl.r])
    S.op("dve", lambda e: e.tensor_scalar(out=ntl.t[:], in0=ntl.t[:], scalar1=512.0, scalar2=None, op0=ALU.mult), reads=[ntl.r], writes=[ntl.r])
    pst = T_("pst", [128, 4])
    pen = T_("pen", [128, 4])
    S.op("pool", lambda e: e.memset(pst.t[:], 0.0), writes=[pst.r])
    for g in range(1, 4):
        S.op("dve", lambda e, g=g: e.tensor_tensor(out=pst.t[:, g:g + 1], in0=pst.t[:, g - 1:g], in1=ntl.t[:, g - 1:g], op=ALU.add),
             reads=[pst.r, ntl.r], writes=[pst.r])
    S.op("dve", lambda e: e.tensor_tensor(out=pen.t[:], in0=pst.t[:], in1=ntl.t[:], op=ALU.add), reads=[pst.r, ntl.r], writes=[pen.r])
    v = T_("vdest", [128, NB, 4])
    S.op("dve", lambda e: e.tensor_tensor(out=v.t[:], in0=pr.t[:, 0:NB * 4].rearrange("p (b g) -> p b g", g=4), in1=boffx.t[:], op=ALU.add),
         reads=[pr.r, boffx.r], writes=[v.r])
    S.op("dve", lambda e: e.tensor_tensor(out=v.t[:], in0=v.t[:], in1=pst.t[:, :].unsqueeze(1).to_broadcast([128, NB, 4]), op=ALU.add),
         reads=[v.r, pst.r], writes=[v.r])
    S.op("dve", lambda e: e.tensor_tensor(out=v.t[:], in0=v.t[:], in1=gohall.t[:], op=ALU.mult), reads=[v.r, gohall.r], writes=[v.r])
    destf = T_("destf", [128, NB])
    desti = T_("desti", [128, NB], I32)
    S.op("dve", lambda e: e.tensor_reduce(out=destf.t[:], in_=v.t[:], axis=AX.X, op=ALU.add), reads=[v.r], writes=[destf.r])
    S.op("dve", lambda e: e.tensor_copy(out=desti.t[:], in_=destf.t[:]), reads=[destf.r], writes=[desti.r])
    S.dma("sp", sc["DEST"], desti.t[:], reads=[desti.r], writes=[sc["r_DEST"]])
    cmp2 = T_("cmp2", [128, NTILE, 4])
    gk = T_("gk", [128, NTILE])
    S.op("dve", lambda e: e.tensor_tensor(out=cmp2.t[:], in0=pen.t[:, :].unsqueeze(1).to_broadcast([128, NTILE, 4]),
                                          in1=thr.t[:, 0:NTILE].unsqueeze(2).to_broadcast([128, NTILE, 4]), op=ALU.is_le), reads=[pen.r, thr.r], writes=[cmp2.r])
    S.op("dve", lambda e: e.tensor_reduce(out=gk.t[:], in_=cmp2.t[:], axis=AX.X, op=ALU.add), reads=[cmp2.r], writes=[gk.r])
    S.op("dve", lambda e: e.tensor_scalar(out=gk.t[:], in0=gk.t[:], scalar1=3.0, scalar2=1024.0, op0=ALU.min, op1=ALU.mult), reads=[gk.r], writes=[gk.r])
    S.op("dve", lambda e: e.tensor_scalar(out=gk.t[:], in0=gk.t[:], scalar1=float(l * 4096), scalar2=None, op0=ALU.add), reads=[gk.r], writes=[gk.r])
    cw_i = T_("cwi", [128, 8], I32)
    cw = T_("cw", [128, 8])
    S.op("pool", lambda e: e.iota(cw_i.t[:], pattern=[[256, 4], [1, 2]], base=0, channel_multiplier=2), writes=[cw_i.r])
    S.op("dve", lambda e: e.tensor_copy(out=cw.t[:], in_=cw_i.t[:]), reads=[cw_i.r], writes=[cw.r])
    idxf = T_("idxf", [128, NTILE, 8])
    idxi = T_("idxi", [128, NTILE, 8], I32)
    S.op("dve", lambda e: e.tensor_tensor(out=idxf.t[:], in0=cw.t[:, :].unsqueeze(1).to_broadcast([128, NTILE, 8]),
                                          in1=gk.t[:, :].unsqueeze(2).to_broadcast([128, NTILE, 8]), op=ALU.add), reads=[cw.r, gk.r], writes=[idxf.r])
    S.op("dve", lambda e: e.tensor_copy(out=idxi.t[:], in_=idxf.t[:]), reads=[idxf.r], writes=[idxi.r])
    S.dma("sp", sc["IDXW"], idxi.t[:, :, :].rearrange("p k j -> p (k j)"), reads=[idxi.r], writes=[sc["r_IDXW"]])
    for b in range(NB):
        S.dma_fn("pool", lambda e, b=b: e.indirect_dma_start(out=sc["XS"][:, :], out_offset=bass.IndirectOffsetOnAxis(ap=desti.t[:, b:b + 1], axis=0),
                                                            in_=x1b.t[:, b, :], in_offset=None), reads=[desti.r, x1b.r], writes=[sc["r_XS"]])
        S.dma_fn("pool", lambda e, b=b: e.indirect_dma_start(out=sc["GS"][:, :], out_offset=bass.IndirectOffsetOnAxis(ap=desti.t[:, b:b + 1], axis=0),
                                                            in_=gate.t[:, b, :], in_offset=None), reads=[desti.r, gate.r], writes=[sc["r_GS"]])


def phase4(nc, S, T, l, sc, xout, r_xout, final, TG=1024):
    A = Alloc(nc)
    g_b = bcast_row(S, A, "ln2g", T["ln2_g"][l], DM)
    b_b = bcast_row(S, A, "ln2b", T["ln2_b"][l], DM)
    gate = Buf(A.sb("gate4", [128, NB, 16], F32))
    S.dma("sp", gate.t[:, :, :].rearrange("p b e -> p (b e)"), sc["GATE"], reads=[sc["r_GATE"]], writes=[gate.r])
    xT = Buf(A.sb("x1T", [128, 8, TG], BF16))
    accb = Buf(A.sb("accm", [128, TG // 128, DM], F32))
    w1 = rot(A, "sb", "w1", [128, 8, DEXP], BF16, 2)
    w3 = rot(A, "sb", "w3", [128, 8, DEXP], BF16, 2)
    w2 = rot(A, "sb", "w2", [128, 4, DM], BF16, 2)
    sa = rot(A, "sb", "sa", [128, 512], F32, 2)
    hT = rot(A, "sb", "hT", [128, 4, 512], BF16, 2)
    xs = rot(A, "sb", "xs4", [128, DM], F32, 2)
    yo = rot(A, "sb", "yo4", [128, DM], F32, 2)
    epsl = Buf(A.sb("epsl4", [128, 1], F32))
    S.op("pool", lambda e: e.memset(epsl.t[:], LN_EPS), writes=[epsl.r])
    small = [dict(st6=Buf(A.sb("st6b", [128, 2, 6], F32)), mv=Buf(A.sb("mvb", [128, 2], F32)), rs=Buf(A.sb("rsb", [128, 1], F32)), eps=epsl) for _ in range(2)]
    pa = rot(A, "ps", "pa", [128, 512], F32, 2)
    pb = rot(A, "ps", "pb", [128, 512], F32, 2)
    py = rot(A, "ps", "py", [128, 512], F32, 3)
    cnt = {"y": 0, "ab": 0, "w": 0}
    W1 = T["w1"][l]
    W3 = T["w3"][l]
    W2 = T["w2"][l]
    for gi in range(SEQ // TG):
        t0 = gi * TG
        S.dma("sp", xT.t[:], sc["X1T"][:, :, t0:t0 + TG], reads=[sc["r_X1T"]], writes=[xT.r])
        for ex in range(NEXP):
            i = cnt["w"]
            cnt["w"] += 1
            a1, a3, a2 = w1[i % 2], w3[i % 2], w2[i % 2]
            S.dma("pool", a1.t[:], W1[ex].rearrange("(c p) n -> p c n", p=128), writes=[a1.r])
            S.dma("pool", a3.t[:], W3[ex].rearrange("(c p) n -> p c n", p=128), writes=[a3.r])
            S.dma("pool", a2.t[:], W2[ex].rearrange("(c p) n -> p c n", p=128), writes=[a2.r])
            for tt in range(TG // 512):
                tc = slice(tt * 512, (tt + 1) * 512)
                h_ = hT[(ex * (TG // 512) + tt) % 2]
                for jc in range(4):
                    k_ = cnt["ab"]
                    cnt["ab"] += 1
                    pa_, pb_, sa_ = pa[k_ % 2], pb[k_ % 2], sa[k_ % 2]
                    for c in range(8):
                        S.op("pe", lambda e, c=c, jc=jc, pa_=pa_, a1=a1, tc=tc: e.matmul(pa_.t[:], lhsT=a1.t[:, c, jc * 128:(jc + 1) * 128], rhs=xT.t[:, c, tc],
                                                                                start=(c == 0), stop=(c == 7)), reads=[a1.r, xT.r], writes=[pa_.r])
                    for c in range(8):
                        S.op("pe", lambda e, c=c, jc=jc, pb_=pb_, a3=a3, tc=tc: e.matmul(pb_.t[:], lhsT=a3.t[:, c, jc * 128:(jc + 1) * 128], rhs=xT.t[:, c, tc],
                                                                                start=(c == 0), stop=(c == 7)), reads=[a3.r, xT.r], writes=[pb_.r])
                    S.op("act", lambda e, pa_=pa_, sa_=sa_: e.activation(out=sa_.t[:], in_=pa_.t[:], func=AF.Silu), reads=[pa_.r], writes=[sa_.r])
                    S.op("dve", lambda e, pb_=pb_, sa_=sa_, h_=h_, jc=jc: e.tensor_tensor(out=h_.t[:, jc, :], in0=pb_.t[:], in1=sa_.t[:], op=ALU.mult),
                         reads=[pb_.r, sa_.r], writes=[h_.r])
                for tb in range(4):
                    blk = tt * 4 + tb
                    gb = (t0 // 128) + blk
                    for hf in range(2):
                        y_ = py[cnt["y"] % 3]
                        cnt["y"] += 1
                        for jc in range(4):
                            S.op("pe", lambda e, jc=jc, tb=tb, hf=hf, y_=y_, h_=h_, a2=a2: e.matmul(y_.t[:], lhsT=h_.t[:, jc, tb * 128:(tb + 1) * 128],
                                                                                           rhs=a2.t[:, jc, hf * 512:(hf + 1) * 512], start=(jc == 0), stop=(jc == 3)),
                                 reads=[h_.r, a2.r], writes=[y_.r])
                        dst = accb.t[:, blk, hf * 512:(hf + 1) * 512]
                        if ex == 0:
                            S.op("dve", lambda e, y_=y_, dst=dst, gb=gb, ex=ex: e.tensor_scalar(out=dst, in0=y_.t[:], scalar1=gate.t[:, gb, ex:ex + 1], scalar2=None, op0=ALU.mult),
                                 reads=[y_.r, gate.r], writes=[accb.r])
                        else:
                            S.op("dve", lambda e, y_=y_, dst=dst, gb=gb, ex=ex: e.scalar_tensor_tensor(out=dst, in0=y_.t[:], scalar=gate.t[:, gb, ex:ex + 1], in1=dst,
                                                                                              op0=ALU.mult, op1=ALU.add), reads=[y_.r, gate.r, accb.r], writes=[accb.r])
        for blk in range(TG // 128):
            gb = (t0 // 128) + blk
            s_, y_, sm = xs[blk % 2], yo[blk % 2], small[blk % 2]
            S.dma("sp", s_.t[:], sc["X1"][gb * 128:(gb + 1) * 128, :], reads=[sc["r_X1"]], writes=[s_.r])
            S.op("dve", lambda e, s_=s_, blk=blk: e.scalar_tensor_tensor(out=s_.t[:], in0=s_.t[:], scalar=ALPHA, in1=accb.t[:, blk, :], op0=ALU.mult, op1=ALU.add),
                 reads=[s_.r, accb.r], writes=[s_.r])
            layernorm_block(S, s_, g_b, b_b, sm, y_)
            S.dma("sp", xout[gb * 128:(gb + 1) * 128, :], y_.t[:], reads=[y_.r], writes=[r_xout], final=final)
    S.flush()
    A.close()


def phase4r(nc, S, T, l, sc, xout, r_xout, final):
    A = Alloc(nc)
    ident = make_ident(A, S, BF16)
    g_b = bcast_row(S, A, "ln2g", T["ln2_g"][l], DM)
    b_b = bcast_row(S, A, "ln2b", T["ln2_b"][l], DM)
    dest = Buf(A.sb("dest4", [128, NB], I32))
    idxw = Buf(A.sb("idxw4", [128, NTILE * 8], I32))
    S.dma("sp", dest.t[:], sc["DEST"], reads=[sc["r_DEST"]], writes=[dest.r])
    S.dma("sp", idxw.t[:], sc["IDXW"], reads=[sc["r_IDXW"]], writes=[idxw.r])
    xs = rot(A, "sb", "xs4r", [128, 4, DM], BF16, 2)
    gs = rot(A, "sb", "gs4r", [128, 4, 16], F32, 2)
    gsel = rot(A, "sb", "gsel", [128, 4, 4], F32, 2)
    xT = rot(A, "sb", "xT4r", [128, 8, 512], BF16, 2)
    accs = rot(A, "sb", "acc4r", [128, 4, DM], F32, 2)
    w1 = rot(A, "sb", "w1r", [128, 8 * DEXP], BF16, 3)
    w3 = rot(A, "sb", "w3r", [128, 8 * DEXP], BF16, 3)
    w2 = rot(A, "sb", "w2r", [128, 4 * DM], BF16, 3)
    sa = rot(A, "sb", "sar", [128, 512], F32, 2)
    hT = rot(A, "sb", "hTr", [128, 4, 512], BF16, 2)
    mt = rot(A, "sb", "mt4", [128, DM], F32, 4)
    xo = rot(A, "sb", "xo4", [128, DM], F32, 4)
    yo = rot(A, "sb", "yo4r", [128, DM], F32, 4)
    epsl = Buf(A.sb("epsl4r", [128, 1], F32))
    S.op("pool", lambda e: e.memset(epsl.t[:], LN_EPS), writes=[epsl.r])
    small = [dict(st6=Buf(A.sb("st6r", [128, 2, 6], F32)), mv=Buf(A.sb("mvr", [128, 2], F32)), rs=Buf(A.sb("rsr", [128, 1], F32)), eps=epsl) for _ in range(4)]
    ptp = rot(A, "ps", "ptp", [128, DM], BF16, 1)
    pa = rot(A, "ps", "par", [128, 512], F32, 2)
    pb = rot(A, "ps", "pbr", [128, 512], F32, 2)
    py = rot(A, "ps", "pyr", [128, 512], F32, 3)
    Wv = [sc["WB"][i][:, :] for i in range(3)]
    cnt = {"y": 0, "ab": 0}
    steps = [(k, j) for k in range(NTILE) for j in range(4)]
    st = {}

    def front(si):
        k, j = steps[si]
        if j == 0:
            x_, g_, gl_, xT_, ac_ = xs[k % 2], gs[k % 2], gsel[k % 2], xT[k % 2], accs[k % 2]
            S.dma("sp", x_.t[:], sc["XS"][k * 512:(k + 1) * 512, :].rearrange("(b p) n -> p b n", p=128), reads=[sc["r_XS"]], writes=[x_.r])
            S.dma("sp", g_.t[:], sc["GS"][k * 512:(k + 1) * 512, :].rearrange("(b p) n -> p b n", p=128), reads=[sc["r_GS"]], writes=[g_.r])
            S.op("dve", lambda e: e.tensor_reduce(out=gl_.t[:], in_=g_.t[:, :, :].rearrange("p b (g j) -> p b j g", g=4), axis=AX.X, op=ALU.add),
                 reads=[g_.r], writes=[gl_.r])
            for blk in range(4):
                p = ptp[0]
                for c in range(8):
                    S.op("pe", lambda e, c=c, blk=blk: e.transpose(out=p.t[:, c * 128:(c + 1) * 128],
                                                                   in_=x_.t[:, blk, :].rearrange("p (pp c) -> p c pp", c=8)[:, c, :], identity=ident.t[:]),
                         reads=[x_.r, ident.r], writes=[p.r])
                S.op("act", lambda e, blk=blk: e.activation(out=xT_.t[:, :, blk * 128:(blk + 1) * 128], in_=p.t[:, :].rearrange("p (c t) -> p c t", c=8), func=AF.Copy),
                     reads=[p.r], writes=[xT_.r])
        xT_, ac_, gl_ = xT[k % 2], accs[k % 2], gsel[k % 2]
        a1, a3, a2, h_ = w1[si % 3], w3[si % 3], w2[si % 3], hT[si % 2]
        for wt_, src in ((a1, Wv[0]), (a3, Wv[1]), (a2, Wv[2])):
            for half in range(2):
                col = k * 8 + j * 2 + half
                S.dma_fn("pool", lambda e, wt_=wt_, src=src, half=half, col=col: e.indirect_dma_start(
                    out=wt_.t[:, half * 2048:(half + 1) * 2048], out_offset=None, in_=src,
                    in_offset=bass.IndirectOffsetOnAxis(ap=idxw.t[:, col:col + 1], axis=0)), reads=[idxw.r, sc["r_WB"][l]], writes=[wt_.r])
        w1v = a1.t[:, :].rearrange("p (c pp q) -> p c q pp", c=8, q=4)
        w3v = a3.t[:, :].rearrange("p (c pp q) -> p c q pp", c=8, q=4)
        for jc in range(4):
            k_ = cnt["ab"]
            cnt["ab"] += 1
            pa_, pb_, sa_ = pa[k_ % 2], pb[k_ % 2], sa[k_ % 2]
            for c in range(8):
                S.op("pe", lambda e, c=c, jc=jc, pa_=pa_: e.matmul(pa_.t[:], lhsT=w1v[:, c, jc, :], rhs=xT_.t[:, c, :], start=(c == 0), stop=(c == 7)),
                     reads=[a1.r, xT_.r], writes=[pa_.r])
            for c in range(8):
                S.op("pe", lambda e, c=c, jc=jc, pb_=pb_: e.matmul(pb_.t[:], lhsT=w3v[:, c, jc, :], rhs=xT_.t[:, c, :], start=(c == 0), stop=(c == 7)),
                     reads=[a3.r, xT_.r], writes=[pb_.r])
            S.op("act", lambda e, pa_=pa_, sa_=sa_: e.activation(out=sa_.t[:], in_=pa_.t[:], func=AF.Silu), reads=[pa_.r], writes=[sa_.r])
            S.op("dve", lambda e, pb_=pb_, sa_=sa_, jc=jc: e.tensor_tensor(out=h_.t[:, jc, :], in0=pb_.t[:], in1=sa_.t[:], op=ALU.mult),
                 reads=[pb_.r, sa_.r], writes=[h_.r])

    def back(si):
        k, j = steps[si]
        ac_, gl_, a2, h_ = accs[k % 2], gsel[k % 2], w2[si % 3], hT[si % 2]
        w2v = a2.t[:, :].rearrange("p (c n) -> p c n", c=4)
        for blk in range(4):
            for hf in range(2):
                y_ = py[cnt["y"] % 3]
                cnt["y"] += 1
                for jc in range(4):
                    S.op("pe", lambda e, jc=jc, blk=blk, hf=hf, y_=y_: e.matmul(y_.t[:], lhsT=h_.t[:, jc, blk * 128:(blk + 1) * 128],
                                                                               rhs=w2v[:, jc, hf * 512:(hf + 1) * 512], start=(jc == 0), stop=(jc == 3)),
                         reads=[h_.r, a2.r], writes=[y_.r])
                dst = ac_.t[:, blk, hf * 512:(hf + 1) * 512]
                if j == 0:
                    S.op("dve", lambda e, y_=y_, dst=dst, blk=blk: e.tensor_scalar(out=dst, in0=y_.t[:], scalar1=gl_.t[:, blk, j:j + 1], scalar2=None, op0=ALU.mult),
                         reads=[y_.r, gl_.r], writes=[ac_.r])
                else:
                    S.op("dve", lambda e, y_=y_, dst=dst, blk=blk: e.scalar_tensor_tensor(out=dst, in0=y_.t[:], scalar=gl_.t[:, blk, j:j + 1], in1=dst,
                                                                                         op0=ALU.mult, op1=ALU.add), reads=[y_.r, gl_.r, ac_.r], writes=[ac_.r])
        if j == 3:
            S.dma("sp", sc["YS"][k * 512:(k + 1) * 512, :].rearrange("(b p) n -> p b n", p=128), ac_.t[:], reads=[ac_.r], writes=[sc["r_YS"]])

    for si in range(len(steps) + 1):
        if si < len(steps):
            front(si)
        if si >= 1:
            back(si - 1)
    def comb_load(b):
        m_, x_ = mt[b % 4], xo[b % 4]
        S.dma_fn("pool", lambda e: e.indirect_dma_start(out=m_.t[:], out_offset=None, in_=sc["YS"][:, :],
                                                        in_offset=bass.IndirectOffsetOnAxis(ap=dest.t[:, b:b + 1], axis=0)),
                 reads=[dest.r, sc["r_YS"]], writes=[m_.r])
        S.dma("sp", x_.t[:], sc["X1"][b * 128:(b + 1) * 128, :], reads=[sc["r_X1"]], writes=[x_.r])
    for b in range(min(3, NB)):
        comb_load(b)
    for b in range(NB):
        m_, x_, y_, sm = mt[b % 4], xo[b % 4], yo[b % 4], small[b % 4]
        S.op("dve", lambda e, m_=m_, x_=x_: e.scalar_tensor_tensor(out=x_.t[:], in0=x_.t[:], scalar=ALPHA, in1=m_.t[:], op0=ALU.mult, op1=ALU.add),
             reads=[x_.r, m_.r], writes=[x_.r])
        layernorm_block(S, x_, g_b, b_b, sm, y_)
        if b + 3 < NB:
            comb_load(b + 3)
        S.dma("sp", xout[b * 128:(b + 1) * 128, :], y_.t[:], reads=[y_.r], writes=[r_xout], final=final)
    S.flush()
    A.close()


def rope_tables():
    pos = np.arange(SEQ, dtype=np.float32)
    out = {}
    for dim, nm in ((64, "rope64"), (32, "rope32")):
        half = dim // 2
        inv = (10000.0 ** (-np.arange(half, dtype=np.float32) / half)).astype(np.float32)
        ang = pos[None, :] * inv[:, None]
        c = np.cos(ang).astype(np.float32)
        s = np.sin(ang).astype(np.float32)
        out[nm + "c"] = np.ascontiguousarray(np.concatenate([c, c], 0))
        out[nm + "s"] = np.ascontiguousarray(np.concatenate([-s, s], 0))
    return out


W_SPECS = [("w_in", [DEPTH, DM, N_IN]), ("b_forget", [DEPTH, 4]), ("g_cq", [DEPTH, 256]), ("g_ckv", [DEPTH, 128]), ("w_uq", [DEPTH, 256, 384]),
           ("w_ukv", [DEPTH, 128, 512]), ("g_head", [DEPTH, 16, 64]), ("w_out", [DEPTH, DM, DM]), ("ln1_g", [DEPTH, DM]), ("ln1_b", [DEPTH, DM]),
           ("w_group", [DEPTH, DM, 4]), ("b_group", [DEPTH, 4]), ("w_expert", [DEPTH, DM, 16]), ("b_expert", [DEPTH, 16]),
           ("w1", [DEPTH, NEXP, DM, DEXP]), ("w3", [DEPTH, NEXP, DM, DEXP]), ("w2", [DEPTH, NEXP, DEXP, DM]), ("ln2_g", [DEPTH, DM]), ("ln2_b", [DEPTH, DM])]


def build_program(nseq=2, layers=(0, 1), phases=(1, 2, 3, 4), debug=False, heads=None, TG=1024):
    nc = bass.Bass("TRN2", target_bir_lowering=False)
    T = {}
    T["x"] = nc.dram_tensor("x", [nseq, SEQ, DM], F32, kind="ExternalInput").ap()
    for nm, shp in W_SPECS:
        T[nm] = nc.dram_tensor(nm, shp, F32, kind="ExternalInput").ap()
    for nm, rows in (("rope64c", 64), ("rope64s", 64), ("rope32c", 32), ("rope32s", 32)):
        T[nm] = nc.dram_tensor(nm, [rows, SEQ], F32, kind="ExternalInput").ap()
    out = nc.dram_tensor("out", [nseq, SEQ, DM], F32, kind="ExternalOutput").ap()
    dk = "ExternalOutput" if debug else "Internal"

    def scratch(name, shape, dt):
        return nc.dram_tensor(name, shape, dt, kind=dk).ap()
    sc = {}
    qs = scratch("QS", [12, 96, SEQ], BF16)
    ks = scratch("KS", [12, 96, SEQ], BF16)
    sc["QS"] = [qs[h] for h in range(12)]
    sc["KS"] = [ks[h] for h in range(12)]
    vs = scratch("VS", [6, 128, NB * 2 * 65], BF16)
    sc["VS"] = [vs[p] for p in range(6)]
    qd = scratch("QD", [3, 4, 64, SEQ], BF16)
    kd = scratch("KD", [3, 4, 64, SEQ], BF16)
    sc["QD"] = [[qd[g, h] for h in range(4)] for g in range(3)]
    sc["KD"] = [[kd[g, h] for h in range(4)] for g in range(3)]
    vd = scratch("VD", [3, 2, 128, NB * 2 * 65], BF16)
    sc["VD"] = [[vd[g, p] for p in range(2)] for g in range(3)]
    sc["OnT"] = scratch("OnT", [8, 128, SEQ], BF16)
    sc["X1"] = scratch("X1", [SEQ, DM], F32)
    sc["X1T"] = scratch("X1T", [128, 8, SEQ], BF16)
    sc["GATE"] = scratch("GATE", [128, NB * 16], F32)
    sc["XS"] = scratch("XS", [NSLOT, DM], BF16)
    sc["GS"] = scratch("GS", [NSLOT, 16], F32)
    sc["YS"] = scratch("YS", [NSLOT, DM], F32)
    sc["DEST"] = scratch("DEST", [128, NB], I32)
    sc["IDXW"] = scratch("IDXW", [128, NTILE * 8], I32)
    sc["WB"] = [scratch("WB%d" % i, [DEPTH * 4096, 2048], BF16) for i in range(3)]
    sc["r_WB"] = [Res() for _ in range(DEPTH)]
    xmid = scratch("XMID", [SEQ, DM], F32)
    sc["r_QS"] = [Res() for _ in range(12)]
    sc["r_KS"] = [Res() for _ in range(12)]
    sc["r_VS"] = [Res() for _ in range(6)]
    sc["r_QD"] = [[Res() for _ in range(4)] for _ in range(3)]
    sc["r_KD"] = [[Res() for _ in range(4)] for _ in range(3)]
    sc["r_VD"] = [[Res() for _ in range(2)] for _ in range(3)]
    for k in ("OnT", "X1", "X1T", "GATE", "XS", "GS", "YS", "DEST", "IDXW"):
        sc["r_" + k] = Res()
    r_xmid = Res()
    r_x = Res()
    r_out = Res()
    S = Sched(nc)
    for s in range(nseq):
        for li, l in enumerate(layers):
            xin, r_xin = (T["x"][s], r_x) if li == 0 else (xmid, r_xmid)
            last = li == len(layers) - 1
            xo, r_xo = (out[s], r_out) if last else (xmid, r_xmid)
            if 1 in phases:
                phase1(nc, S, T, xin, r_xin, l, sc)
            if 2 in phases:
                cv = (lambda l=l: convert_expert_weights(S, T, sc, l)) if (ROUTED and s == 0) else None
                phase2(nc, S, T, l, sc, heads=heads, after_sb=cv)
            if 3 in phases:
                phase3(nc, S, T, xin, r_xin, l, sc)
            if 4 in phases:
                if ROUTED:
                    phase4r(nc, S, T, l, sc, xo, r_xo, final=last)
                else:
                    phase4(nc, S, T, l, sc, xo, r_xo, final=last, TG=TG)
    S.close()
    return nc, S


def convert_expert_weights(S, T, sc, l):
    for i, nm in enumerate(("w1", "w3", "w2")):
        src = T[nm][l].rearrange("e k n -> (e k n)").rearrange("(r x) -> r x", x=2048)
        for ch in range(8):
            S.dma("pool", sc["WB"][i][l * 4096 + ch * 512:l * 4096 + (ch + 1) * 512, :], src[ch * 512:(ch + 1) * 512, :], writes=[sc["r_WB"][l]])


_CACHE = {}


def kernel(**inputs):
    n = 8
    nseq = 2
    x = np.ascontiguousarray(np.asarray(inputs["x"], dtype=np.float32))
    tabs = rope_tables()
    if "nc" not in _CACHE:
        _CACHE["nc"] = build_program(nseq=nseq)[0]
    nc = _CACHE["nc"]
    base = {nm: np.ascontiguousarray(np.asarray(inputs[nm], dtype=np.float32)) for nm, _ in W_SPECS}
    base.update(tabs)
    in_maps = []
    for c in range(n):
        m = dict(base)
        m["x"] = x[c * nseq:(c + 1) * nseq]
        in_maps.append(m)
    res = run_bass_kernel_spmd(nc, in_maps, core_ids=list(range(n)))
    return np.concatenate([r["out"] for r in res.results], axis=0).astype(np.float32)
```

```python
import numpy as np
from os import environ as _os_env
import concourse.bass as bass
import concourse.mybir as mybir
from concourse.bass_utils import run_bass_kernel_spmd

F32 = mybir.dt.float32
BF16 = mybir.dt.bfloat16
I32 = mybir.dt.int32
AF = mybir.ActivationFunctionType
ALU = mybir.AluOpType
AX = mybir.AxisListType

SES_MODE = int(_os_env.get("SES", "2"))
SAME_ENGINE_SYNC = SES_MODE != 0
NDMA_SLOTS = int(_os_env.get("NSLOTS", "8"))

SEQ = 4096
DM = 1024
NB = SEQ // 128
NT = SEQ // 512
DEPTH = 2
ALPHA = (2.0 * DEPTH) ** 0.25
LN_EPS = 1e-5
RMS_EPS = 1e-6
N_IN = 4260
C_SB, C_FOX, C_MLA, C_DIL = 0, 768, 1540, 1956
MLA_SCALE = 96.0 ** -0.5
DIL_R = (1, 4, 16)
NEXP = 16
DEXP = 512
NTILE = 11
NSLOT = NTILE * 512
ROUTED = bool(int(_os_env.get("ROUTED", "1")))
TRIM = bool(int(_os_env.get("TRIM", "1")))


class Res:
    __slots__ = ("w", "r", "excl")

    def __init__(self, excl=False):
        self.w = None
        self.r = []
        self.excl = excl


class Sched:
    ENG = ("pe", "act", "dve", "pool", "sp")

    def __init__(self, nc):
        self.nc = nc
        self.ops = {e: [] for e in self.ENG}
        self.cnt = {e: 0 for e in self.ENG}
        self.seen = {e: {} for e in self.ENG}
        self.sems = {}
        self.dma_slots = {}
        self.dma_rr = {}
        self.final_waits = []
        self._stack = []
        self.nops = 0

    def sem(self, key):
        if key not in self.sems:
            cm = self.nc.semaphore("s_" + "_".join(str(k) for k in (key if isinstance(key, tuple) else (key,))))
            s = cm.__enter__()
            self._stack.append(cm)
            self.sems[key] = s
        return self.sems[key]

    def _deps(self, eng, reads, writes):
        deps = {}

        def add(t, same_ok=True):
            if t is None:
                return
            k, v = t
            if k == eng and not same_ok:
                return
            if deps.get(k, 0) < v:
                deps[k] = v
        relax = SES_MODE == 2
        for r in reads:
            add(r.w)
            if r.excl:
                for t in r.r:
                    if t[0] != eng:
                        add(t)
        for w in writes:
            add(w.w, same_ok=not relax)
            for t in w.r:
                add(t, same_ok=not relax)
        waits = []
        seen = self.seen[eng]
        for k, v in deps.items():
            if k == eng and (eng == "pe" or not SAME_ENGINE_SYNC):
                continue
            if seen.get(k, 0) >= v:
                continue
            seen[k] = v
            waits.append((k, v))
        return waits

    def _commit(self, ticket, reads, writes):
        for r in reads:
            if len(r.r) > 16:
                m = {}
                for k, v in r.r:
                    if m.get(k, 0) < v:
                        m[k] = v
                r.r = list(m.items())
            r.r.append(ticket)
        for w in writes:
            w.w = ticket
            w.r = []

    def op(self, eng, fn, reads=(), writes=()):
        waits = self._deps(eng, reads, writes)
        self.cnt[eng] += 1
        ticket = (eng, self.cnt[eng])
        self.ops[eng].append((waits, fn, (eng, 1)))
        self._commit(ticket, reads, writes)
        self.nops += 1
        return ticket

    def dma(self, q, out, in_, reads=(), writes=(), final=False, **kw):
        fn = lambda e, out=out, in_=in_, kw=kw: e.dma_start(out=out, in_=in_, **kw)
        return self.dma_fn(q, fn, reads, writes, final)

    def dma_fn(self, q, fn, reads=(), writes=(), final=False):
        waits = self._deps(q, reads, writes)
        if q not in self.dma_slots:
            self.dma_slots[q] = [[("d", q, i), 0] for i in range(NDMA_SLOTS)]
            self.dma_rr[q] = 0
        i = self.dma_rr[q]
        self.dma_rr[q] = (i + 1) % NDMA_SLOTS
        slot = self.dma_slots[q][i]
        key, tot = slot
        if tot > 0 and self.seen[q].get(key, 0) < tot:
            self.seen[q][key] = tot
            waits.append((key, tot))
        slot[1] = tot + 16
        ticket = (key, tot + 16)
        self.ops[q].append((waits, fn, (key, 16)))
        self._commit(ticket, reads, writes)
        self.nops += 1
        if final:
            self.final_waits.append(ticket)
        return ticket

    def flush(self):
        totals = [(e, self.cnt[e]) for e in self.ENG if self.cnt[e] > 0]
        for q, slots in self.dma_slots.items():
            for key, tot in slots:
                if tot > 0:
                    totals.append((key, tot))
        for e in self.ENG:
            self.sem(e)
            for waits, fn, inc in self.ops[e]:
                for k, v in waits:
                    self.sem(k)
                self.sem(inc[0])
        ops = self.ops
        self.ops = {e: [] for e in self.ENG}
        for e in self.ENG:
            for k, v in totals:
                self.seen[e][k] = max(self.seen[e].get(k, 0), v)
        with self.nc.Block() as block:
            def run(engname):
                def body(e):
                    for waits, fn, inc in ops[engname]:
                        for k, v in waits:
                            e.wait_ge(self.sems[k], v)
                        fn(e).then_inc(self.sems[inc[0]], inc[1])
                    for k, v in totals:
                        e.wait_ge(self.sems[k], v)
                return body
            block.sync(run("sp"))
            block.tensor(run("pe"))
            block.scalar(run("act"))
            block.vector(run("dve"))
            block.gpsimd(run("pool"))

    def close(self):
        for cm in reversed(self._stack):
            cm.__exit__(None, None, None)
        self._stack = []


_UID = [0]


class Alloc:
    def __init__(self, nc):
        self.nc = nc
        self.stack = []

    @property
    def n(self):
        _UID[0] += 1
        return _UID[0]

    def sb(self, name, shape, dt):
        cm = self.nc.sbuf_tensor("%s_%d" % (name, self.n), list(shape), dt)
        t = cm.__enter__()
        self.stack.append(cm)
        return t

    def ps(self, name, shape, dt):
        cm = self.nc.psum_tensor("%s_%d" % (name, self.n), list(shape), dt)
        t = cm.__enter__()
        self.stack.append(cm)
        return t

    def close(self):
        for cm in reversed(self.stack):
            cm.__exit__(None, None, None)
        self.stack = []


class Buf:
    def __init__(self, t, excl=False):
        self.t = t
        self.r = Res(excl)


def rot(A, kind, name, shape, dt, n):
    f = A.sb if kind == "sb" else A.ps
    return [Buf(f(name + str(i), shape, dt), excl=(kind == "ps")) for i in range(n)]


def make_ident(A, S, dt):
    b = Buf(A.sb("ident", [128, 128], dt))
    S.op("pool", lambda e: e.memset(b.t[:], 1.0), writes=[b.r])
    S.op("pool", lambda e: e.affine_select(out=b.t[:], in_=b.t[:], pattern=[[-1, 128]], compare_op=ALU.is_equal,
                                           fill=0.0, base=0, channel_multiplier=1), reads=[b.r], writes=[b.r])
    return b


def perm_view(ap2d, r, t0, n):
    if r == 1:
        return ap2d[:, t0:t0 + n]
    sc = SEQ // r
    v = ap2d.rearrange("p (i c) -> p c i", c=r)
    c0, i0 = t0 // sc, t0 % sc
    if i0 + n <= sc:
        return v[:, c0, i0:i0 + n]
    assert i0 == 0 and n % sc == 0
    return v[:, c0:c0 + n // sc, :]


import os as _os
_P1SEC = _os.environ.get("P1SEC", "abcd")
_MSUB = _os.environ.get("MSUB", "qkrvptc")
_MC = _os.environ.get("MC", "1234")


def phase1(nc, S, T, xin, r_xin, l, sc):
    A = Alloc(nc)
    ident = make_ident(A, S, BF16)
    xT = Buf(A.sb("xT", [128, 8, SEQ], BF16))
    xs = rot(A, "sb", "xs", [128, DM], F32, 4)
    xb = rot(A, "sb", "xb", [128, DM], BF16, 3)
    pst = rot(A, "ps", "pst", [128, DM], BF16, 2)
    pj = rot(A, "ps", "pj", [128, 512], F32, 4)
    pv = rot(A, "ps", "pv", [128, 512], F32, 2)
    Win = T["w_in"][l].rearrange("(c p) n -> p c n", p=128)

    for b in range(NB):
        s, c_, p = xs[b % 4], xb[b % 3], pst[b % 2]
        S.dma("sp", s.t[:], xin[b * 128:(b + 1) * 128, :], reads=[r_xin], writes=[s.r])
        S.op("act", lambda e, s=s, c_=c_: e.activation(out=c_.t[:], in_=s.t[:], func=AF.Copy), reads=[s.r], writes=[c_.r])
        for c in range(8):
            S.op("pe", lambda e, c=c, c_=c_, p=p: e.transpose(out=p.t[:, c * 128:(c + 1) * 128], in_=c_.t[:, c * 128:(c + 1) * 128],
                                                              identity=ident.t[:]), reads=[c_.r, ident.r], writes=[p.r])
        S.op("dve", lambda e, b=b, p=p: e.tensor_copy(out=xT.t[:, :, b * 128:(b + 1) * 128],
                                                      in_=p.t[:, :].rearrange("p (c t) -> p c t", c=8)), reads=[p.r], writes=[xT.r])

    wts = rot(A, "sb", "wt", [128, 8, 416], BF16, 2)
    wsw = rot(A, "sb", "wsw", [128, 8, 256], BF16, 2)
    stg = rot(A, "sb", "stg", [128, 512], BF16, 4)
    vst = rot(A, "sb", "vst", [128, NB, 2, 65], BF16, 2)
    tmp1 = rot(A, "sb", "tmp1", [128, 512], F32, 2)
    tmp2 = rot(A, "sb", "tmp2", [128, 512], F32, 2)
    CT = Buf(A.sb("ropeC", [128, SEQ], BF16))
    ST = Buf(A.sb("ropeS", [128, SEQ], BF16))
    for v in vst:
        S.op("pool", lambda e, v=v: e.memset(v.t[:], 1.0), writes=[v.r])
    state = {"w": 0, "pj": 0, "stg": 0, "v": 0, "pv": 0}

    def load_w(col_ranges):
        w = wts[state["w"] % 2]
        state["w"] += 1
        o = 0
        for (c0, n) in col_ranges:
            S.dma("pool", w.t[:, :, o:o + n], Win[:, :, c0:c0 + n], writes=[w.r])
            o += n
        return w

    def nxt(key, lst):
        b = lst[state[key] % len(lst)]
        state[key] += 1
        return b

    def proj_fm(w, o, M, r, j, wtile=None):
        p = nxt("pj", pj)
        wt_ = w if wtile is None else wtile
        for c in range(8):
            S.op("pe", lambda e, c=c, p=p, wt_=wt_: e.matmul(p.t[0:M, :], lhsT=wt_.t[:, c, o:o + M],
                                                             rhs=perm_view(xT.t[:, c, :], r, j * 512, 512),
                                                             start=(c == 0), stop=(c == 7)),
                 reads=[wt_.r, xT.r], writes=[p.r])
        return p

    def store_rows(st, rows, dst, r_dst, j):
        S.dma("sp", dst[:, j * 512:(j + 1) * 512], st.t[rows[0]:rows[1], :], reads=[st.r], writes=[r_dst])

    def v_proj(w, o, r, vt, ncol=128):
        for b4 in range(NB // 4):
            p = nxt("pv", pv)
            for bb in range(4):
                b = b4 * 4 + bb
                for c in range(8):
                    S.op("pe", lambda e, c=c, b=b, bb=bb, p=p: e.matmul(p.t[:, bb * 128:(bb + 1) * 128],
                                                                      lhsT=perm_view(xT.t[:, c, :], r, b * 128, 128),
                                                                      rhs=w.t[:, c, o:o + ncol], start=(c == 0), stop=(c == 7)),
                         reads=[w.r, xT.r], writes=[p.r])
            S.op("dve", lambda e, b4=b4, p=p: e.tensor_copy(
                out=vt.t[:, b4 * 4:(b4 + 1) * 4, :, 0:64],
                in_=p.t[:, :].rearrange("p (b h d) -> p b h d", b=4, h=2)), reads=[p.r], writes=[vt.r])

    def scale_q(w):
        S.op("dve", lambda e: e.tensor_scalar(out=w.t[:, :, 0:128], in0=w.t[:, :, 0:128], scalar1=0.125, scalar2=None,
                                              op0=ALU.mult), reads=[w.r], writes=[w.r])

    for kind, cbase, hbase, pbase in (("sb", C_SB, 0, 0), ("fox", C_FOX, 4, 2)):
        for hp in range(2):
            w = load_w([(cbase + hp * 128, 128), (cbase + 256 + hp * 128, 128), (cbase + 512 + hp * 128, 128)])
            scale_q(w)
            for qk, dst, rd in ((0, sc["QS"], sc["r_QS"]), (1, sc["KS"], sc["r_KS"])):
                for j in range(NT):
                    p = proj_fm(w, qk * 128, 128, 1, j)
                    st = nxt("stg", stg)
                    S.op("act", lambda e, p=p, st=st: e.activation(out=st.t[:], in_=p.t[:], func=AF.Copy), reads=[p.r], writes=[st.r])
                    for hh in range(2):
                        h = hbase + hp * 2 + hh
                        store_rows(st, (hh * 64, hh * 64 + 64), dst[h][0:64, :], rd[h], j)
            vt = nxt("v", vst)
            v_proj(w, 256, 1, vt)
            S.dma("sp", sc["VS"][pbase + hp], vt.t[:, :, :, :].rearrange("p b h d -> p (b h d)"), reads=[vt.r], writes=[sc["r_VS"][pbase + hp]])

    S.flush()
    if "b" not in _P1SEC:
        A.close()
        return
    A2 = Alloc(nc)
    wf = Buf(A2.sb("wf", [128, 8, 4], BF16))
    S.dma("pool", wf.t[:], Win[:, :, C_FOX + 768:C_FOX + 772], writes=[wf.r])
    bfg = Buf(A2.sb("bfg", [4, 1], F32))
    S.dma("sp", bfg.t[:], T["b_forget"][l].rearrange("(h o) -> h o", o=1), writes=[bfg.r])
    S.op("dve", lambda e: e.tensor_scalar(out=bfg.t[:], in0=bfg.t[:], scalar1=-1.0, scalar2=None, op0=ALU.mult), reads=[bfg.r], writes=[bfg.r])
    nlf = Buf(A2.sb("nlf", [4, SEQ], F32))
    ncum = Buf(A2.sb("ncum", [4, SEQ], F32))
    ones4 = Buf(A2.sb("ones4", [4, 512], F32))
    S.op("pool", lambda e: e.memset(ones4.t[:], 1.0), writes=[ones4.r])
    for j in range(NT):
        p = proj_fm(wf, 0, 4, 1, j)
        S.op("act", lambda e, p=p, j=j: e.activation(out=nlf.t[:, j * 512:(j + 1) * 512], in_=p.t[0:4, :], func=AF.Exp,
                                                     bias=bfg.t[:, 0:1], scale=-1.0), reads=[p.r, bfg.r], writes=[nlf.r])
    S.op("act", lambda e: e.activation(out=nlf.t[:], in_=nlf.t[:], func=AF.Ln, bias=1.0), reads=[nlf.r], writes=[nlf.r])
    for j in range(NT):
        sl = slice(j * 512, (j + 1) * 512)
        init = 0.0 if j == 0 else ncum.t[:, j * 512 - 1:j * 512]
        S.op("dve", lambda e, sl=sl, init=init: e.tensor_tensor_scan(out=ncum.t[:, sl], data0=ones4.t[:, :], data1=nlf.t[:, sl],
                                                                     initial=init, op0=ALU.mult, op1=ALU.add),
             reads=[ones4.r, nlf.r, ncum.r], writes=[ncum.r])
    class _Alias:
        def __init__(self, t, r):
            self.t, self.r = t, r
    nlf_b = nlf.t[:, :].bitcast(BF16)
    parts = [_Alias(nlf_b[:, 0:SEQ], nlf.r), _Alias(nlf_b[:, SEQ:2 * SEQ], nlf.r), Buf(A2.sb("cpart2", [4, SEQ], BF16))]
    for i in range(3):
        S.op("dve", lambda e, i=i: e.tensor_copy(out=parts[i].t[:], in_=ncum.t[:]), reads=[ncum.r], writes=[parts[i].r])
        if i < 2:
            S.op("dve", lambda e, i=i: e.tensor_tensor(out=ncum.t[:], in0=ncum.t[:], in1=parts[i].t[:], op=ALU.subtract),
                 reads=[ncum.r, parts[i].r], writes=[ncum.r])
    ones3 = Buf(A2.sb("ones3", [35, SEQ], BF16))
    S.op("pool", lambda e: e.memset(ones3.t[0:3, :], 1.0), writes=[ones3.r])
    S.op("pool", lambda e: e.memset(ones3.t[32:35, :], -1.0), writes=[ones3.r])
    for h in range(4):
        H = 4 + h
        S.dma("sp", sc["KS"][H][64:67, :], ones3.t[32:35, :], reads=[ones3.r], writes=[sc["r_KS"][H]])
        S.dma("sp", sc["QS"][H][67:70, :], ones3.t[0:3, :], reads=[ones3.r], writes=[sc["r_QS"][H]])
        for i in range(3):
            S.dma("sp", sc["KS"][H][67 + i:68 + i, :], parts[i].t[h:h + 1, :], reads=[parts[i].r], writes=[sc["r_KS"][H]])
            S.dma("sp", sc["QS"][H][64 + i:65 + i, :], parts[i].t[h:h + 1, :], reads=[parts[i].r], writes=[sc["r_QS"][H]])
    S.flush()
    A2.close()

    if "c" not in _P1SEC:
        A.close()
        return
    w = load_w([(C_MLA, 416)])
    wkrs = nxt("w", wsw) if False else wsw[0]
    if "p" in _MSUB:
        S.op("pool", lambda e: e.tensor_copy(out=wkrs.t[:, :, 0:16], in_=w.t[:, :, 400:416]), reads=[w.r], writes=[wkrs.r])
        S.op("pool", lambda e: e.tensor_copy(out=wkrs.t[:, :, 16:32], in_=w.t[:, :, 384:400]), reads=[w.r], writes=[wkrs.r])
    A3 = Alloc(nc)
    wuq = Buf(A3.sb("wuq", [128, 2, 384], BF16))
    wuqs = Buf(A3.sb("wuqs", [128, 2, 384], BF16))
    wukv = Buf(A3.sb("wukv", [128, 512], BF16))
    S.dma("pool", wuq.t[:], T["w_uq"][l].rearrange("(c p) n -> p c n", p=128), writes=[wuq.r])
    S.dma("pool", wukv.t[:], T["w_ukv"][l], writes=[wukv.r])
    S.op("pool", lambda e: e.tensor_copy(out=wuqs.t[:], in_=wuq.t[:]), reads=[wuq.r], writes=[wuqs.r])
    for c2 in (range(2) if "p" in _MSUB else []):
        v4o = wuqs.t[:, c2, :].rearrange("p (h d) -> p h d", h=4)
        v4i = wuq.t[:, c2, :].rearrange("p (h d) -> p h d", h=4)
        S.op("pool", lambda e, v4o=v4o, v4i=v4i: e.tensor_copy(out=v4o[:, :, 64:80], in_=v4i[:, :, 80:96]), reads=[wuq.r, wuqs.r], writes=[wuqs.r])
        S.op("pool", lambda e, v4o=v4o, v4i=v4i: e.tensor_copy(out=v4o[:, :, 80:96], in_=v4i[:, :, 64:80]), reads=[wuq.r, wuqs.r], writes=[wuqs.r])
    gcq = Buf(A3.sb("gcq", [128, 2], F32))
    gckv = Buf(A3.sb("gckv", [128, 1], F32))
    for c2 in range(2):
        S.dma("sp", gcq.t[:, c2:c2 + 1], T["g_cq"][l][c2 * 128:(c2 + 1) * 128].rearrange("(p o) -> p o", o=1), writes=[gcq.r])
    S.dma("sp", gckv.t[:], T["g_ckv"][l].rearrange("(p o) -> p o", o=1), writes=[gckv.r])
    for tb, nm in (((CT, "rope32c"), (ST, "rope32s")) if "t" in _MSUB else []):
        S.dma("pool", tb.t[0:32, :], T[nm], writes=[tb.r])
        S.dma("pool", tb.t[64:96, :], T[nm], writes=[tb.r])
    onesq = Buf(A3.sb("onesq", [128, 128], BF16))
    oneskv = Buf(A3.sb("oneskv", [128, 128], BF16))
    epst = Buf(A3.sb("epst", [128, 1], F32))
    S.op("pool", lambda e: e.memset(epst.t[:], RMS_EPS), writes=[epst.r])
    S.op("pool", lambda e: e.memset(onesq.t[:], 1.0 / 256.0), writes=[onesq.r])
    S.op("pool", lambda e: e.memset(oneskv.t[:], 1.0 / 128.0), writes=[oneskv.r])
    cqg = rot(A3, "sb", "cqg", [128, 2, 512], BF16, 2)
    cq2 = rot(A3, "sb", "cq2", [128, 2, 512], BF16, 2)
    ckg = rot(A3, "sb", "ckg", [128, 512], BF16, 2)
    ck2 = rot(A3, "sb", "ck2", [128, 512], BF16, 2)
    rq = rot(A3, "sb", "rq", [128, 512], F32, 2)
    rkv = rot(A3, "sb", "rkv", [128, 512], F32, 2)
    rtok = rot(A3, "sb", "rtok", [128, 1], F32, 2)
    vstm = Buf(A3.sb("vstm", [128, NB, 4, 65], BF16))
    S.op("pool", lambda e: e.memset(vstm.t[:], 1.0), writes=[vstm.r])
    wukv_v = wukv.t[:, :].rearrange("p (h x) -> p h x", h=4)[:, :, 64:128]
    for j in (range(NT) if "c" in _MSUB else []):
        tc = slice(j * 512, (j + 1) * 512)
        a, a2, kg, k2, rq_, rkv_ = cqg[j % 2], cq2[j % 2], ckg[j % 2], ck2[j % 2], rq[j % 2], rkv[j % 2]
        for c2 in (range(2) if "1" in _MC else []):
            p = proj_fm(w, c2 * 128, 128, 1, j)
            S.op("dve", lambda e, p=p, c2=c2, a=a: e.tensor_scalar(out=a.t[:, c2, :], in0=p.t[:], scalar1=gcq.t[:, c2:c2 + 1], scalar2=None,
                                                                  op0=ALU.mult), reads=[p.r, gcq.r], writes=[a.r])
            S.op("act", lambda e, p=p, c2=c2, a2=a2: e.activation(out=a2.t[:, c2, :], in_=p.t[:], func=AF.Square), reads=[p.r], writes=[a2.r])
        if "2" in _MC:
            p = proj_fm(w, 256, 128, 1, j)
            if "5" not in _MC:
                S.op("dve", lambda e, p=p, kg=kg: e.tensor_scalar(out=kg.t[:], in0=p.t[:], scalar1=gckv.t[:, 0:1], scalar2=None, op0=ALU.mult),
                     reads=[p.r, gckv.r], writes=[kg.r])
            if "6" not in _MC:
                S.op("act", lambda e, p=p, k2=k2: e.activation(out=k2.t[:], in_=p.t[:], func=AF.Square), reads=[p.r], writes=[k2.r])
        if "3" in _MC:
            p = nxt("pj", pj)
            for c2 in range(2):
                S.op("pe", lambda e, p=p, c2=c2, a2=a2: e.matmul(p.t[:], lhsT=onesq.t[:], rhs=a2.t[:, c2, :], start=(c2 == 0), stop=(c2 == 1)),
                     reads=[onesq.r, a2.r], writes=[p.r])
            S.op("act", lambda e, p=p, rq_=rq_: e.activation(out=rq_.t[:], in_=p.t[:], func=AF.Sqrt, bias=epst.t[:, 0:1]), reads=[p.r, epst.r], writes=[rq_.r])
            S.op("dve", lambda e, rq_=rq_: e.reciprocal(out=rq_.t[:], in_=rq_.t[:]), reads=[rq_.r], writes=[rq_.r])
        if "4" in _MC:
            p = nxt("pj", pj)
            S.op("pe", lambda e, p=p, k2=k2: e.matmul(p.t[:], lhsT=oneskv.t[:], rhs=k2.t[:], start=True, stop=True), reads=[oneskv.r, k2.r], writes=[p.r])
            S.op("act", lambda e, p=p, rkv_=rkv_: e.activation(out=rkv_.t[:], in_=p.t[:], func=AF.Sqrt, bias=epst.t[:, 0:1]), reads=[p.r, epst.r], writes=[rkv_.r])
            S.op("dve", lambda e, rkv_=rkv_: e.reciprocal(out=rkv_.t[:], in_=rkv_.t[:]), reads=[rkv_.r], writes=[rkv_.r])
        for h in (range(4) if "q" in _MSUB else []):
            H = 8 + h
            pa, pb = nxt("pj", pj), nxt("pj", pj)
            for pp, ww in ((pa, wuq), (pb, wuqs)):
                for c2 in range(2):
                    S.op("pe", lambda e, pp=pp, ww=ww, c2=c2, h=h, a=a: e.matmul(pp.t[0:96, :], lhsT=ww.t[:, c2, h * 96:(h + 1) * 96], rhs=a.t[:, c2, :],
                                                                             start=(c2 == 0), stop=(c2 == 1)), reads=[ww.r, a.r], writes=[pp.r])
            st = nxt("stg", stg)
            t1, t2 = tmp1[h % 2], tmp2[h % 2]
            S.op("dve", lambda e, pa=pa, st=st, rq_=rq_: e.scalar_tensor_tensor(out=st.t[0:64, :], in0=pa.t[0:64, :], scalar=MLA_SCALE, in1=rq_.t[0:64, :],
                                                                             op0=ALU.mult, op1=ALU.mult), reads=[pa.r, rq_.r], writes=[st.r])
            S.op("dve", lambda e, pa=pa, t1=t1, tc=tc: e.tensor_tensor(out=t1.t[64:96, :], in0=pa.t[64:96, :], in1=CT.t[64:96, tc], op=ALU.mult),
                 reads=[pa.r, CT.r], writes=[t1.r])
            S.op("dve", lambda e, pb=pb, t2=t2, tc=tc: e.tensor_tensor(out=t2.t[64:96, :], in0=pb.t[64:96, :], in1=ST.t[64:96, tc], op=ALU.mult),
                 reads=[pb.r, ST.r], writes=[t2.r])
            S.op("pool", lambda e, t1=t1, t2=t2: e.tensor_tensor(out=t1.t[64:96, :], in0=t1.t[64:96, :], in1=t2.t[64:96, :], op=ALU.add),
                 reads=[t1.r, t2.r], writes=[t1.r])
            S.op("dve", lambda e, t1=t1, st=st, rq_=rq_: e.scalar_tensor_tensor(out=st.t[64:96, :], in0=t1.t[64:96, :], scalar=MLA_SCALE, in1=rq_.t[64:96, :],
                                                                             op0=ALU.mult, op1=ALU.mult), reads=[t1.r, rq_.r, st.r], writes=[st.r])
            store_rows(st, (0, 96), sc["QS"][H][0:96, :], sc["r_QS"][H], j)
        for h in (range(4) if "k" in _MSUB else []):
            H = 8 + h
            p = nxt("pj", pj)
            S.op("pe", lambda e, p=p, h=h, kg=kg: e.matmul(p.t[0:64, :], lhsT=wukv.t[:, h * 128:h * 128 + 64], rhs=kg.t[:], start=True, stop=True),
                 reads=[wukv.r, kg.r], writes=[p.r])
            st = nxt("stg", stg)
            S.op("dve", lambda e, p=p, st=st, rkv_=rkv_: e.tensor_tensor(out=st.t[0:64, :], in0=p.t[0:64, :], in1=rkv_.t[0:64, :], op=ALU.mult),
                 reads=[p.r, rkv_.r], writes=[st.r])
            store_rows(st, (0, 64), sc["KS"][H][0:64, :], sc["r_KS"][H], j)
        if "r" not in _MSUB:
            continue
        pa = proj_fm(w, 384, 32, 1, j)
        pb = proj_fm(wkrs, 0, 32, 1, j)
        t1, t2 = tmp1[0], tmp2[0]
        st = nxt("stg", stg)
        S.op("dve", lambda e, pa=pa, t1=t1, tc=tc: e.tensor_tensor(out=t1.t[0:32, :], in0=pa.t[0:32, :], in1=CT.t[0:32, tc], op=ALU.mult),
             reads=[pa.r, CT.r], writes=[t1.r])
        S.op("dve", lambda e, pb=pb, t2=t2, tc=tc: e.tensor_tensor(out=t2.t[0:32, :], in0=pb.t[0:32, :], in1=ST.t[0:32, tc], op=ALU.mult),
             reads=[pb.r, ST.r], writes=[t2.r])
        S.op("pool", lambda e, t1=t1, t2=t2, st=st: e.tensor_tensor(out=st.t[0:32, :], in0=t1.t[0:32, :], in1=t2.t[0:32, :], op=ALU.add),
             reads=[t1.r, t2.r], writes=[st.r])
        for h in range(4):
            store_rows(st, (0, 32), sc["KS"][8 + h][64:96, :], sc["r_KS"][8 + h], j)
        for bb in (range(4) if "v" in _MSUB else []):
            b = j * 4 + bb
            p = nxt("pv", pv)
            S.op("pe", lambda e, p=p, bb=bb, kg=kg: e.matmul(p.t[:, 0:256], lhsT=kg.t[:, bb * 128:(bb + 1) * 128], rhs=wukv_v, start=True, stop=True),
                 reads=[wukv.r, kg.r], writes=[p.r])
            S.op("pe", lambda e, p=p, bb=bb, k2=k2: e.matmul(p.t[:, 256:257], lhsT=k2.t[:, bb * 128:(bb + 1) * 128], rhs=oneskv.t[:, 0:1], start=True, stop=True),
                 reads=[oneskv.r, k2.r], writes=[p.r])
            rt = rtok[b % 2]
            S.op("act", lambda e, p=p, rt=rt: e.activation(out=rt.t[:], in_=p.t[:, 256:257], func=AF.Sqrt, bias=epst.t[:, 0:1]), reads=[p.r, epst.r], writes=[rt.r])
            S.op("dve", lambda e, rt=rt: e.reciprocal(out=rt.t[:], in_=rt.t[:]), reads=[rt.r], writes=[rt.r])
            S.op("dve", lambda e, p=p, b=b, rt=rt: e.tensor_scalar(out=vstm.t[:, b, :, 0:64], in0=p.t[:, 0:256].rearrange("p (h d) -> p h d", h=4),
                                                                  scalar1=rt.t[:, 0:1], scalar2=None, op0=ALU.mult), reads=[p.r, rt.r], writes=[vstm.r])
    for hp in range(2):
        S.dma("sp", sc["VS"][4 + hp].rearrange("p (b h d) -> p b h d", b=NB, h=2), vstm.t[:, :, 2 * hp:2 * hp + 2, :], reads=[vstm.r], writes=[sc["r_VS"][4 + hp]])

    S.flush()
    A3.close()
    if "d" not in _P1SEC:
        A.close()
        return
    for tb, nm in ((CT, "rope64c"), (ST, "rope64s")):
        S.dma("pool", tb.t[0:64, :], T[nm], writes=[tb.r])
        S.dma("pool", tb.t[64:128, :], T[nm], writes=[tb.r])
    for g in range(3):
        r = DIL_R[g]
        for hp in range(2):
            o = g * 256 + hp * 128
            w = load_w([(C_DIL + o, 128), (C_DIL + 768 + o, 128), (C_DIL + 1536 + o, 128)])
            scale_q(w)
            ws = wsw[(g * 2 + hp) % 2]
            for c in range(8):
                vo = ws.t[:, c, :].rearrange("p (h f d) -> p h f d", h=4, f=2)
                vi = w.t[:, c, 0:256].rearrange("p (h f d) -> p h f d", h=4, f=2)
                S.op("pool", lambda e, vo=vo, vi=vi: e.tensor_copy(out=vo[:, :, 0, :], in_=vi[:, :, 1, :]), reads=[w.r], writes=[ws.r])
                S.op("pool", lambda e, vo=vo, vi=vi: e.tensor_copy(out=vo[:, :, 1, :], in_=vi[:, :, 0, :]), reads=[w.r], writes=[ws.r])
            for qk, dst, rd in ((0, sc["QD"], sc["r_QD"]), (1, sc["KD"], sc["r_KD"])):
                for j in range(NT):
                    pa = proj_fm(w, qk * 128, 128, r, j)
                    pb = proj_fm(ws, qk * 128, 128, r, j)
                    t1, t2 = tmp1[j % 2], tmp2[j % 2]
                    st = nxt("stg", stg)
                    cv = perm_view(CT.t[:, :], r, j * 512, 512)
                    sv = perm_view(ST.t[:, :], r, j * 512, 512)
                    shp = None if len(cv.shape) == 2 else cv.shape

                    def v3(ap):
                        return ap if shp is None else ap.rearrange("p (a b) -> p a b", a=shp[1])
                    S.op("dve", lambda e, pa=pa, t1=t1, cv=cv, v3=v3: e.tensor_tensor(out=v3(t1.t[:]), in0=v3(pa.t[:]), in1=cv, op=ALU.mult),
                         reads=[pa.r, CT.r], writes=[t1.r])
                    S.op("dve", lambda e, pb=pb, t2=t2, sv=sv, v3=v3: e.tensor_tensor(out=v3(t2.t[:]), in0=v3(pb.t[:]), in1=sv, op=ALU.mult),
                         reads=[pb.r, ST.r], writes=[t2.r])
                    S.op("pool", lambda e, t1=t1, t2=t2, st=st: e.tensor_tensor(out=st.t[:], in0=t1.t[:], in1=t2.t[:], op=ALU.add),
                         reads=[t1.r, t2.r], writes=[st.r])
                    for hh in range(2):
                        store_rows(st, (hh * 64, hh * 64 + 64), dst[g][hp * 2 + hh], rd[g][hp * 2 + hh], j)
            vt = nxt("v", vst)
            v_proj(w, 256, r, vt)
            S.dma("sp", sc["VD"][g][hp], vt.t[:, :, :, :].rearrange("p b h d -> p (b h d)"), reads=[vt.r], writes=[sc["r_VD"][g][hp]])
    S.flush()
    A.close()


def phase2(nc, S, T, l, sc, heads=None, after_sb=None):
    A = Alloc(nc)
    negtri = Buf(A.sb("negtri", [128, 128], BF16))
    S.op("pool", lambda e: e.memset(negtri.t[:], -1.0), writes=[negtri.r])
    S.op("pool", lambda e: e.affine_select(out=negtri.t[:], in_=negtri.t[:], pattern=[[-1, 128]], compare_op=ALU.is_ge, fill=0.0, base=0,
                                           channel_multiplier=1), reads=[negtri.r], writes=[negtri.r])
    ones = Buf(A.sb("ones", [128, 128], BF16))
    S.op("pool", lambda e: e.memset(ones.t[:], 1.0), writes=[ones.r])
    wn = Buf(A.sb("wn", [65, 64], BF16))
    wnsb = Buf(A.sb("wnsb", [65, 64], BF16))
    for t_, v_ in ((wn, RMS_EPS), (wnsb, 0.0)):
        S.op("pool", lambda e, t_=t_: e.memset(t_.t[:], 1.0 / 64.0), writes=[t_.r])
        S.op("pool", lambda e, t_=t_, v_=v_: e.memset(t_.t[64:65, :], v_), reads=[t_.r], writes=[t_.r])
    gh = Buf(A.sb("gh", [64, 16], F32))
    for h_ in range(16):
        S.dma("sp", gh.t[:, h_:h_ + 1], T["g_head"][l][h_].rearrange("(d o) -> d o", o=1), writes=[gh.r])
    eps2 = Buf(A.sb("eps2", [64, 2], F32))
    S.op("pool", lambda e: e.memset(eps2.t[:, 0:1], RMS_EPS), writes=[eps2.r])
    S.op("pool", lambda e: e.memset(eps2.t[:, 1:2], 0.0), reads=[eps2.r], writes=[eps2.r])

    Qt = rot(A, "sb", "Qt", [128, SEQ], BF16, 4)
    Kt = rot(A, "sb", "Kt", [128, SEQ], BF16, 4)
    Vt = rot(A, "sb", "Vt", [128, NB, 2, 65], BF16, 2)
    pz = rot(A, "ps", "pz", [128, 512], F32, 3)
    po = rot(A, "ps", "po", [128, 512], F32, 2)
    pc = rot(A, "ps", "pc", [128, 512], F32, 2)
    pss = rot(A, "ps", "pss", [128, 512], F32, 1)
    Pb = rot(A, "sb", "Pb", [128, 512], BF16, 8)
    eb = rot(A, "sb", "eb", [128, 512], F32, 2)
    spb = rot(A, "sb", "spb", [128, 512], BF16, 3)
    lw = rot(A, "sb", "lw", [128, 512], F32, 4)
    Rsb = Buf(A.sb("Rsb", [128, 512], F32))
    sqb = rot(A, "sb", "sqb", [65, 512], BF16, 2)
    osb = rot(A, "sb", "osb", [65, 512], F32, 3)
    def mk_mask(name, n, conds):
        m = Buf(A.sb(name, [128, n], BF16))
        S.op("pool", lambda e: e.memset(m.t[:], 1.0), writes=[m.r])
        for (step, base, cm) in conds:
            S.op("pool", lambda e, step=step, base=base, cm=cm: e.affine_select(out=m.t[:], in_=m.t[:], pattern=[[step, n]], compare_op=ALU.is_ge, fill=0.0,
                                                                                base=base, channel_multiplier=cm), reads=[m.r], writes=[m.r])
        return m
    maskS = [mk_mask("ms%d" % o, 512, [(1, -128 * o - 1, -1)]) for o in (3, 2, 1, 0)][::-1]
    maskC = [mk_mask("mc%d" % o, 512, [(1, -128 * o, -1)]) for o in range(4)]
    maskD = {512: {o: mk_mask("md%d" % (o + 1), 512, [(1, -128 * o, -1), (-1, 128 + 128 * o, 1)]) for o in range(-1, 4)},
             256: {o: mk_mask("me%d" % o, 256, [(1, -128 * o, -1), (-1, 128 + 128 * o, 1)]) for o in range(0, 2)}}
    stb = rot(A, "sb", "stb", [64, 512], F32, 2)
    yb = rot(A, "sb", "yb", [64, 512], BF16, 2)
    acc = rot(A, "sb", "acc", [65, SEQ], F32, 2)
    st = {"fin": 0, "ld": 0, "vld": 0, "o": 0}

    def finish(src_ap, r_src, h, t0, n, is_sb, in_sbuf=False):
        i = st["fin"]
        st["fin"] += 1
        sq, s_, y, ps_ = sqb[i % 2], stb[i % 2], yb[i % 2], pss[0]
        if in_sbuf:
            o_ap, r_o = src_ap, r_src
        else:
            ob = osb[i % 3]
            S.op("act", lambda e: e.activation(out=ob.t[:, 0:n], in_=src_ap, func=AF.Copy), reads=[r_src], writes=[ob.r])
            o_ap, r_o = ob.t[:, 0:n], ob.r
        S.op("dve", lambda e: e.tensor_tensor(out=sq.t[:, 0:n], in0=o_ap, in1=o_ap, op=ALU.mult), reads=[r_o], writes=[sq.r])
        wn_ = wnsb if is_sb else wn
        S.op("pe", lambda e: e.matmul(ps_.t[0:64, 0:n], lhsT=wn_.t[:, :], rhs=sq.t[:, 0:n], start=True, stop=True), reads=[wn_.r, sq.r], writes=[ps_.r])
        S.op("act", lambda e: e.activation(out=s_.t[:, 0:n], in_=ps_.t[0:64, 0:n], func=AF.Ln, bias=(eps2.t[:, 0:1] if is_sb else eps2.t[:, 1:2])),
             reads=[ps_.r, eps2.r], writes=[s_.r])
        S.op("act", lambda e: e.activation(out=s_.t[:, 0:n], in_=s_.t[:, 0:n], func=AF.Exp, scale=-0.5), reads=[s_.r], writes=[s_.r])
        S.op("dve", lambda e: e.scalar_tensor_tensor(out=y.t[:, 0:n], in0=o_ap[0:64], scalar=gh.t[:, h:h + 1], in1=s_.t[:, 0:n],
                                                     op0=ALU.mult, op1=ALU.mult), reads=[r_o, gh.r, s_.r], writes=[y.r])
        S.dma("sp", sc["OnT"][h // 2, (h % 2) * 64:(h % 2) * 64 + 64, t0:t0 + n], y.t[:, 0:n], reads=[y.r], writes=[sc["r_OnT"]])

    def load_qk(qsrc, r_q, ksrc, r_k, kd):
        i = st["ld"]
        st["ld"] += 1
        q, k = Qt[i % 4], Kt[i % 4]
        S.dma("sp", q.t[0:kd, :], qsrc, reads=[r_q], writes=[q.r])
        S.dma("sp", k.t[0:kd, :], ksrc, reads=[r_k], writes=[k.r])
        return q, k

    def load_v(vsrc, r_v):
        i = st["vld"]
        st["vld"] += 1
        v = Vt[i % 2]
        S.dma("sp", v.t[:, :, :, :].rearrange("p b h d -> p (b h d)"), vsrc, reads=[r_v], writes=[v.r])
        return v

    def make_stages(steps, q, k, kd, v, hh, kind, done_cb, u=0):
        n_ = len(steps)
        ctx = [dict() for _ in range(n_)]
        zsel = [[pz[0], pz[1]], [pz[2], pc[0]]][u]

        def s1(i):
            sp_ = steps[i]
            z = zsel[i % 2]
            q0, n, kb = sp_["q0"], sp_["n"], sp_["kb"]
            S.op("pe", lambda e: e.matmul(z.t[:, 0:n], lhsT=k.t[0:kd, kb * 128:(kb + 1) * 128], rhs=q.t[0:kd, q0:q0 + n], start=True, stop=True),
                 reads=[k.r, q.r], writes=[z.r])
            if kind == "sb":
                e_, s_ = eb[i % 2], spb[i % 3]
                S.op("act", lambda e: e.activation(out=e_.t[:, 0:n], in_=z.t[:, 0:n], func=AF.Exp), reads=[z.r], writes=[e_.r])
                S.op("act", lambda e: e.activation(out=s_.t[:, 0:n], in_=e_.t[:, 0:n], func=AF.Ln, bias=1.0), reads=[e_.r], writes=[s_.r])
                if sp_["mask"] is not None:
                    mk = sp_["mask"]
                    S.op("pool", lambda e: e.tensor_tensor(out=s_.t[:, 0:n], in0=s_.t[:, 0:n], in1=mk.t[:, 0:n], op=ALU.mult), reads=[s_.r, mk.r], writes=[s_.r])
                ctx[i]["sp"] = s_
            else:
                p_ = Pb[u * 4 + i % 4]
                if kind == "fox" and sp_["mask"] is not None:
                    l_ = lw[u * 2 + i % 2]
                    S.op("dve", lambda e: e.tensor_scalar(out=l_.t[:, 0:n], in0=z.t[:, 0:n], scalar1=60.0, scalar2=None, op0=ALU.min), reads=[z.r], writes=[l_.r])
                    S.op("act", lambda e: e.activation(out=p_.t[:, 0:n], in_=l_.t[:, 0:n], func=AF.Exp), reads=[l_.r], writes=[p_.r])
                else:
                    S.op("act", lambda e: e.activation(out=p_.t[:, 0:n], in_=z.t[:, 0:n], func=AF.Exp), reads=[z.r], writes=[p_.r])
                if sp_["mask"] is not None:
                    mk = sp_["mask"]
                    S.op("dve", lambda e: e.tensor_tensor(out=p_.t[:, 0:n], in0=p_.t[:, 0:n], in1=mk.t[:, 0:n], op=ALU.mult), reads=[p_.r, mk.r], writes=[p_.r])
                ctx[i]["P"] = p_

        def s2(i):
            if kind != "sb":
                return
            sp_ = steps[i]
            q0, n, kb = sp_["q0"], sp_["n"], sp_["kb"]
            s_ = ctx[i]["sp"]
            c_, rc, l_, p_ = pc[i % 2], pz[2], lw[i % 2], Pb[i % 4]
            S.op("pe", lambda e: e.matmul(c_.t[:, 0:n], lhsT=k.t[0:kd, kb * 128:(kb + 1) * 128], rhs=q.t[0:kd, q0:q0 + n], start=True, stop=False),
                 reads=[k.r, q.r], writes=[c_.r])
            S.op("pe", lambda e: e.matmul(c_.t[:, 0:n], lhsT=negtri.t[:], rhs=s_.t[:, 0:n], start=False, stop=True), reads=[negtri.r, s_.r], writes=[c_.r])
            S.op("pe", lambda e: e.matmul(rc.t[:, 0:n], lhsT=ones.t[:], rhs=s_.t[:, 0:n], start=True, stop=True), reads=[ones.r, s_.r], writes=[rc.r])
            oc = sp_["oc"]
            if sp_["first"]:
                S.op("pool", lambda e: e.memset(Rsb.t[:], 0.0), writes=[Rsb.r])
            S.op("dve", lambda e: e.tensor_tensor(out=l_.t[:, 0:n], in0=c_.t[:, 0:n], in1=Rsb.t[:, oc:oc + n], op=ALU.subtract), reads=[c_.r, Rsb.r], writes=[l_.r])
            S.op("dve", lambda e: e.tensor_tensor(out=Rsb.t[:, oc:oc + n], in0=rc.t[:, 0:n], in1=Rsb.t[:, oc:oc + n], op=ALU.add), reads=[rc.r, Rsb.r], writes=[Rsb.r])
            S.op("act", lambda e: e.activation(out=p_.t[:, 0:n], in_=l_.t[:, 0:n], func=AF.Exp), reads=[l_.r], writes=[p_.r])
            if sp_["mask"] is not None:
                mk = sp_["mask"]
                S.op("pool", lambda e: e.tensor_tensor(out=p_.t[:, 0:n], in0=p_.t[:, 0:n], in1=mk.t[:, 0:n], op=ALU.mult), reads=[p_.r, mk.r], writes=[p_.r])
            ctx[i]["P"] = p_

        def s3(i):
            sp_ = steps[i]
            n, kb = sp_["n"], sp_["kb"]
            if sp_["first"] and u == 0:
                st["o"] += 1
            o_ = po[st["o"] % 2] if u == 0 else pc[1]
            p_ = ctx[i]["P"]
            oc = sp_["oc"]
            S.op("pe", lambda e: e.matmul(o_.t[0:65, oc:oc + n], lhsT=v.t[:, kb, hh, :], rhs=p_.t[:, 0:n], start=sp_["first"], stop=sp_["last"],
                                          skip_group_check=True), reads=[v.r, p_.r], writes=[o_.r])
            if sp_["last"]:
                done_cb(o_, sp_)

        return n_, s1, s2, s3

    def drive(stage_sets):
        nmax = max(ss[0] for ss in stage_sets)
        for i in range(nmax + 2):
            for n_, s1, s2, s3 in stage_sets:
                if i < n_:
                    s1(i)
            for n_, s1, s2, s3 in stage_sets:
                if 0 <= i - 1 < n_:
                    s2(i - 1)
            for n_, s1, s2, s3 in stage_sets:
                if 0 <= i - 2 < n_:
                    s3(i - 2)

    def run_steps(steps, q, k, kd, v, hh, kind, done_cb):
        drive([make_stages(steps, q, k, kd, v, hh, kind, done_cb, 0)])

    def causal_steps(strict, descending):
        steps = []
        for qt in range(NT):
            q0 = qt * 512
            kbs = list(range(0, 4 * qt + 4))
            if descending:
                kbs = kbs[::-1]
            for ii, kb in enumerate(kbs):
                o = kb - 4 * qt
                if o >= 0 and TRIM:
                    steps.append(dict(q0=q0 + 128 * o, n=512 - 128 * o, oc=128 * o, kb=kb, mask=(maskS if strict else maskC)[0],
                                      first=(ii == 0), last=(ii == len(kbs) - 1), tq0=q0, tn=512))
                else:
                    steps.append(dict(q0=q0, n=512, oc=0, kb=kb, mask=((maskS if strict else maskC)[o] if o >= 0 else None),
                                      first=(ii == 0), last=(ii == len(kbs) - 1), tq0=q0, tn=512))
        return steps

    def dil_steps(r):
        sc_ = SEQ // r
        n = min(512, sc_)
        steps = []
        for q0 in range(0, SEQ, n):
            cs = (q0 // sc_) * sc_
            k_lo = max(cs, q0 - 128)
            kbs = list(range(k_lo // 128, (q0 + n) // 128))
            for ii, kb in enumerate(kbs):
                steps.append(dict(q0=q0, n=n, oc=0, kb=kb, mask=maskD[n][kb - q0 // 128], first=(ii == 0), last=(ii == len(kbs) - 1), tq0=q0, tn=n))
        return steps

    hsel = (lambda h: True) if heads is None else (lambda h: h in heads)
    groups = []
    for kind, hbase, pbase, kd in (("sb", 0, 0, 64), ("fox", 4, 2, 70), ("mla", 8, 4, 96)):
        for hp in range(2):
            js = [(kind, hbase + hp * 2 + hh, pbase + hp, hh, kd) for hh in range(2) if hsel(hbase + hp * 2 + hh)]
            if kind == "sb":
                groups += [[j] for j in js]
            elif js:
                groups.append(js)
    step_cache = {"sb": causal_steps(True, True), "fox": causal_steps(False, False)}
    step_cache["mla"] = step_cache["fox"]
    loaded = {}
    vcur = {}

    def prefetch(group):
        for job in group:
            kind, h, pr_, hh, kd = job
            if pr_ not in vcur:
                vcur.clear()
                vcur[pr_] = load_v(sc["VS"][pr_], sc["r_VS"][pr_])
            loaded[h] = load_qk(sc["QS"][h][0:kd, :], sc["r_QS"][h], sc["KS"][h][0:kd, :], sc["r_KS"][h], kd) + (vcur[pr_],)
    if groups:
        prefetch(groups[0])
    for gi, group in enumerate(groups):
        if after_sb is not None and group[0][0] != "sb":
            after_sb()
            after_sb = None
        cur = [loaded.pop(job[1]) for job in group]
        if gi + 1 < len(groups):
            prefetch(groups[gi + 1])
        sets = []
        for u, (job, (q, k, v)) in enumerate(zip(group, cur)):
            kind, h, pr_, hh, kd = job

            def done(o_, sp_, h=h, kind=kind):
                finish(o_.t[0:65, 0:sp_["tn"]], o_.r, h, sp_["tq0"], sp_["tn"], kind == "sb")
            sets.append(make_stages(step_cache[kind], q, k, kd, v, hh, kind, done, u))
        drive(sets)
    if after_sb is not None:
        after_sb()
    dgroups = [(hp, g) for hp in range(2) if (hsel(12 + 2 * hp) or hsel(13 + 2 * hp)) for g in range(3)]
    dsteps = {g: dil_steps(DIL_R[g]) for g in range(3)}
    dl = {}

    def dprefetch(grp):
        hp, g = grp
        v = load_v(sc["VD"][g][hp], sc["r_VD"][g][hp])
        dl[grp] = [load_qk(sc["QD"][g][hp * 2 + hh], sc["r_QD"][g][hp * 2 + hh], sc["KD"][g][hp * 2 + hh], sc["r_KD"][g][hp * 2 + hh], 64) + (v,)
                   for hh in range(2)]
    if dgroups:
        dprefetch(dgroups[0])
    for gi, grp in enumerate(dgroups):
        hp, g = grp
        r = DIL_R[g]
        cur = dl.pop(grp)
        if gi + 1 < len(dgroups):
            dprefetch(dgroups[gi + 1])
        sets = []
        for hh in range(2):
            q, k, v = cur[hh]
            a_ = acc[hh]

            def done(o_, sp_, a_=a_, r=r, g=g):
                n, q0 = sp_["tn"], sp_["tq0"]
                dst = perm_view(a_.t[:, :], r, q0, n)
                if g == 0:
                    S.op("act", lambda e: e.activation(out=dst, in_=o_.t[0:65, 0:n], func=AF.Copy), reads=[o_.r], writes=[a_.r])
                else:
                    S.op("dve", lambda e: e.tensor_tensor(out=dst, in0=o_.t[0:65, 0:n], in1=dst, op=ALU.add), reads=[o_.r, a_.r], writes=[a_.r])
            sets.append(make_stages(dsteps[g], q, k, 64, v, hh, "dil", done, hh))
        drive(sets)
        if g == 2:
            for h2 in range(2):
                for qt in range(NT):
                    finish(acc[h2].t[:, qt * 512:(qt + 1) * 512], acc[h2].r, 12 + hp * 2 + h2, qt * 512, 512, False, in_sbuf=True)
    S.flush()
    A.close()


def layernorm_block(S, y, g_b, b_b, small, out):
    st6, mv, rs = small["st6"], small["mv"], small["rs"]
    for hf in range(2):
        S.op("dve", lambda e, hf=hf: e.bn_stats(out=st6.t[:, hf, :], in_=y.t[:, hf * 512:(hf + 1) * 512]), reads=[y.r], writes=[st6.r])
    S.op("dve", lambda e: e.bn_aggr(out=mv.t[:], in_=st6.t[:, :, :].rearrange("p a b -> p (a b)")), reads=[st6.r], writes=[mv.r])
    S.op("act", lambda e: e.activation(out=rs.t[:], in_=mv.t[:, 1:2], func=AF.Ln, bias=small["eps"].t[:, 0:1]), reads=[mv.r, small["eps"].r], writes=[rs.r])
    S.op("act", lambda e: e.activation(out=rs.t[:], in_=rs.t[:], func=AF.Exp, scale=-0.5), reads=[rs.r], writes=[rs.r])
    S.op("dve", lambda e: e.scalar_tensor_tensor(out=y.t[:], in0=y.t[:], scalar=mv.t[:, 0:1], in1=g_b.t[:], op0=ALU.subtract, op1=ALU.mult),
         reads=[y.r, mv.r, g_b.r], writes=[y.r])
    S.op("dve", lambda e: e.scalar_tensor_tensor(out=out.t[:], in0=y.t[:], scalar=rs.t[:, 0:1], in1=b_b.t[:], op0=ALU.mult, op1=ALU.add),
         reads=[y.r, rs.r, b_b.r], writes=[out.r])


def bcast_row(S, A, name, src1d, n):
    b = Buf(A.sb(name, [128, n], F32))
    S.dma("sp", b.t[:], src1d.rearrange("(o n) -> o n", o=1).partition_broadcast(128), writes=[b.r])
    return b


def phase3(nc, S, T, xin, r_xin, l, sc):
    A = Alloc(nc)
    identf = make_ident(A, S, F32)
    wout = Buf(A.sb("wout", [128, 8, DM], BF16))
    S.dma("pool", wout.t[:], T["w_out"][l].rearrange("(c p) n -> p c n", p=128), writes=[wout.r])
    wr = Buf(A.sb("wr", [128, 8, 20], F32))
    S.dma("sp", wr.t[:, :, 0:4], T["w_group"][l].rearrange("(c p) n -> p c n", p=128), writes=[wr.r])
    S.dma("sp", wr.t[:, :, 4:20], T["w_expert"][l].rearrange("(c p) n -> p c n", p=128), writes=[wr.r])
    brt = Buf(A.sb("brt", [128, 20], F32))
    S.dma("sp", brt.t[:, 0:4], T["b_group"][l].rearrange("(o n) -> o n", o=1).partition_broadcast(128), writes=[brt.r])
    S.dma("sp", brt.t[:, 4:20], T["b_expert"][l].rearrange("(o n) -> o n", o=1).partition_broadcast(128), writes=[brt.r])
    g_b = bcast_row(S, A, "ln1g", T["ln1_g"][l], DM)
    b_b = bcast_row(S, A, "ln1b", T["ln1_b"][l], DM)
    on = rot(A, "sb", "on", [128, 8, 512], BF16, 2)
    xs = rot(A, "sb", "xs3", [128, DM], F32, 4)
    y = rot(A, "sb", "y3", [128, DM], F32, 3)
    x1 = rot(A, "sb", "x1o", [128, DM], F32, 6)
    xtf = rot(A, "sb", "xtf", [128, 8, 128], F32, 3)
    xtb = rot(A, "sb", "xtb", [128, 8, 512], BF16, 2)
    gate = Buf(A.sb("gate", [128, NB, 16], F32))
    lgall = Buf(A.sb("lgall", [128, NB, 20], F32))
    if ROUTED:
        x1b = Buf(A.sb("x1b", [128, NB, DM], BF16))
        gohall = Buf(A.sb("gohall", [128, NB, 4], F32))
        S.op("pool", lambda e: e.memset(x1b.t[:, 0:4, :], 0.0), writes=[x1b.r])
        S.op("pool", lambda e: e.memset(gate.t[:], 0.0), writes=[gate.r])
        for k in (range(NTILE) if int(_os_env.get("ZF", "1")) else []):
            S.dma("sp", sc["XS"][k * 512:(k + 1) * 512, :].rearrange("(p r) n -> p (r n)", p=128), x1b.t[:, 0:4, :].rearrange("p b n -> p (b n)"),
                  reads=[x1b.r], writes=[sc["r_XS"]])
        for k in (range(NTILE) if int(_os_env.get("ZF", "1")) else []):
            S.dma("sp", sc["GS"][k * 512:(k + 1) * 512, :].rearrange("(p r) n -> p (r n)", p=128), gate.t[:, 0:4, :].rearrange("p b n -> p (b n)"),
                  reads=[gate.r], writes=[sc["r_GS"]])
    ph = rot(A, "ps", "ph", [128, DM], F32, 2)
    ptr = rot(A, "ps", "ptr", [128, DM], F32, 1)
    plg = rot(A, "ps", "plg", [128, 512], F32, 2)
    epsl = Buf(A.sb("epsl", [128, 1], F32))
    S.op("pool", lambda e: e.memset(epsl.t[:], LN_EPS), writes=[epsl.r])
    small = [dict(st6=Buf(A.sb("st6", [128, 2, 6], F32)), mv=Buf(A.sb("mv", [128, 2], F32)), rs=Buf(A.sb("rs", [128, 1], F32)), eps=epsl) for _ in range(3)]
    pend = []
    pend2 = []

    def ld_on(j):
        S.dma("sp", on[j % 2].t[:], sc["OnT"][:, :, j * 512:(j + 1) * 512].rearrange("c p t -> p c t"), reads=[sc["r_OnT"]], writes=[on[j % 2].r])

    def ld_x(b):
        S.dma("sp", xs[b % 4].t[:], xin[b * 128:(b + 1) * 128, :], reads=[r_xin], writes=[xs[b % 4].r])
    for j in range(NT):
        o_ = on[j % 2]
        if j == 0:
            ld_on(0)
        if j + 1 < NT:
            ld_on(j + 1)
        xb_ = xtb[j % 2]
        for bb in range(4):
            b = j * 4 + bb
            s_, y_, x1_, xf_, p_, sm = xs[b % 4], y[b % 3], x1[b % 6], xtf[b % 3], ph[b % 2], small[b % 3]
            if b == 0:
                ld_x(0)
                ld_x(1)
            if b + 2 < NB:
                ld_x(b + 2)
            for hf in range(2):
                for c in range(8):
                    S.op("pe", lambda e, hf=hf, c=c, bb=bb, o_=o_, p_=p_: e.matmul(p_.t[:, hf * 512:(hf + 1) * 512], lhsT=o_.t[:, c, bb * 128:(bb + 1) * 128],
                                                                            rhs=wout.t[:, c, hf * 512:(hf + 1) * 512], start=(c == 0), stop=(c == 7)),
                         reads=[o_.r, wout.r], writes=[p_.r])
            S.op("dve", lambda e, s_=s_, y_=y_, p_=p_: e.scalar_tensor_tensor(out=y_.t[:], in0=s_.t[:], scalar=ALPHA, in1=p_.t[:], op0=ALU.mult, op1=ALU.add),
                 reads=[s_.r, p_.r], writes=[y_.r])
            layernorm_block(S, y_, g_b, b_b, sm, x1_)
            S.dma("sp", sc["X1"][b * 128:(b + 1) * 128, :], x1_.t[:], reads=[x1_.r], writes=[sc["r_X1"]])
            if ROUTED:
                S.op("act", lambda e, b=b, x1_=x1_: e.activation(out=x1b.t[:, b, :], in_=x1_.t[:], func=AF.Copy), reads=[x1_.r], writes=[x1b.r])
            def stage_b(b=b, bb=bb, x1_=x1_, xf_=xf_, xb_=xb_):
                pt = ptr[0]
                for c in range(8):
                    S.op("pe", lambda e, c=c: e.transpose(out=pt.t[:, c * 128:(c + 1) * 128], in_=x1_.t[:, c * 128:(c + 1) * 128], identity=identf.t[:]),
                         reads=[x1_.r, identf.r], writes=[pt.r])
                S.op("act", lambda e: e.activation(out=xf_.t[:, :, :], in_=pt.t[:, :].rearrange("p (c t) -> p c t", c=8), func=AF.Copy),
                     reads=[pt.r], writes=[xf_.r])
                if not ROUTED:
                    S.op("dve", lambda e: e.tensor_copy(out=xb_.t[:, :, bb * 128:(bb + 1) * 128], in_=pt.t[:, :].rearrange("p (c t) -> p c t", c=8)),
                         reads=[pt.r], writes=[xb_.r])
                def stage_c():
                    pl = plg[b % 2]
                    for c in range(8):
                        S.op("pe", lambda e, c=c: e.matmul(pl.t[:, 0:20], lhsT=xf_.t[:, c, :], rhs=wr.t[:, c, :], start=(c == 0), stop=(c == 7)),
                             reads=[xf_.r, wr.r], writes=[pl.r])
                    S.op("dve", lambda e: e.tensor_tensor(out=lgall.t[:, b, :], in0=pl.t[:, 0:20], in1=brt.t[:], op=ALU.add), reads=[pl.r, brt.r], writes=[lgall.r])
                if int(_os_env.get("INL", "0")):
                    stage_c()
                else:
                    pend2.append(stage_c)
            pend.append(stage_b)
            if len(pend2) > int(_os_env.get("LAG2", "1")):
                pend2.pop(0)()
            if len(pend) > 3:
                pend.pop(0)()
            if (not ROUTED) and bb == 3:
                while pend:
                    pend.pop(0)()
                while pend2:
                    pend2.pop(0)()
        if not ROUTED:
            S.dma("sp", sc["X1T"][:, :, j * 512:(j + 1) * 512], xb_.t[:], reads=[xb_.r], writes=[sc["r_X1T"]])
    while pend:
        pend.pop(0)()
        while len(pend2) > 1:
            pend2.pop(0)()
    while pend2:
        pend2.pop(0)()
    def GT(name, shape):
        return Buf(A.sb("gv_" + name, shape, F32))
    B3 = [128, NB, 4]
    gl = lgall.t[:, :, 0:4]
    el = lgall.t[:, :, 4:20].rearrange("p b (g x) -> p b g x", g=4)
    m_, goh, tmp, se = GT("m", [128, NB]), (gohall if ROUTED else GT("goh", B3)), GT("tmp", B3), GT("se", [128, NB])
    t44, es, m1, oh1, es2, m2, oh2 = GT("t44", [128, NB, 4, 4]), GT("es", B3), GT("m1", [128, NB]), GT("oh1", B3), GT("es2", B3), GT("m2", [128, NB]), GT("oh2", B3)
    d_, p1, p2, gi = GT("d", [128, NB]), GT("p1", [128, NB]), GT("p2", [128, NB]), GT("gi", B3)

    def bc(t2):
        return t2.t[:, :].unsqueeze(2).to_broadcast(B3)

    def D(fn, reads, writes):
        S.op("dve", fn, reads=[x.r for x in reads], writes=[x.r for x in writes])
    D(lambda e: e.tensor_reduce(out=m_.t[:], in_=gl, axis=AX.X, op=ALU.max), [lgall], [m_])
    D(lambda e: e.tensor_tensor(out=goh.t[:], in0=gl, in1=bc(m_), op=ALU.is_equal), [lgall, m_], [goh])
    D(lambda e: e.tensor_tensor(out=tmp.t[:], in0=gl, in1=bc(m_), op=ALU.subtract), [lgall, m_], [tmp])
    S.op("act", lambda e: e.activation(out=tmp.t[:], in_=tmp.t[:], func=AF.Exp), reads=[tmp.r], writes=[tmp.r])
    D(lambda e: e.tensor_reduce(out=se.t[:], in_=tmp.t[:], axis=AX.X, op=ALU.add), [tmp], [se])
    D(lambda e: e.reciprocal(out=se.t[:], in_=se.t[:]), [se], [se])
    D(lambda e: e.tensor_tensor(out=t44.t[:], in0=el, in1=goh.t[:, :, :].unsqueeze(3).to_broadcast([128, NB, 4, 4]), op=ALU.mult), [lgall, goh], [t44])
    D(lambda e: e.tensor_reduce(out=es.t[:], in_=t44.t[:, :, :, :].rearrange("p b g x -> p b x g"), axis=AX.X, op=ALU.add), [t44], [es])
    D(lambda e: e.tensor_reduce(out=m1.t[:], in_=es.t[:], axis=AX.X, op=ALU.max), [es], [m1])
    D(lambda e: e.tensor_tensor(out=oh1.t[:], in0=es.t[:], in1=bc(m1), op=ALU.is_equal), [es, m1], [oh1])
    D(lambda e: e.scalar_tensor_tensor(out=es2.t[:], in0=oh1.t[:], scalar=-1e30, in1=es.t[:], op0=ALU.mult, op1=ALU.add), [oh1, es], [es2])
    D(lambda e: e.tensor_reduce(out=m2.t[:], in_=es2.t[:], axis=AX.X, op=ALU.max), [es2], [m2])
    D(lambda e: e.tensor_tensor(out=oh2.t[:], in0=es2.t[:], in1=bc(m2), op=ALU.is_equal), [es2, m2], [oh2])
    D(lambda e: e.tensor_tensor(out=d_.t[:], in0=m2.t[:], in1=m1.t[:], op=ALU.subtract), [m1, m2], [d_])
    S.op("act", lambda e: e.activation(out=d_.t[:], in_=d_.t[:], func=AF.Exp), reads=[d_.r], writes=[d_.r])
    D(lambda e: e.tensor_scalar(out=p1.t[:], in0=d_.t[:], scalar1=1.0, scalar2=None, op0=ALU.add), [d_], [p1])
    D(lambda e: e.reciprocal(out=p1.t[:], in_=p1.t[:]), [p1], [p1])
    D(lambda e: e.tensor_tensor(out=p2.t[:], in0=d_.t[:], in1=p1.t[:], op=ALU.mult), [d_, p1], [p2])
    D(lambda e: e.tensor_tensor(out=gi.t[:], in0=oh1.t[:], in1=bc(p1), op=ALU.mult), [oh1, p1], [gi])
    D(lambda e: e.tensor_tensor(out=oh2.t[:], in0=oh2.t[:], in1=bc(p2), op=ALU.mult), [oh2, p2], [oh2])
    D(lambda e: e.tensor_tensor(out=gi.t[:], in0=gi.t[:], in1=oh2.t[:], op=ALU.add), [gi, oh2], [gi])
    D(lambda e: e.tensor_tensor(out=gi.t[:], in0=gi.t[:], in1=bc(se), op=ALU.mult), [gi, se], [gi])
    D(lambda e: e.tensor_tensor(out=gate.t[:, :, :].rearrange("p b (g x) -> p b g x", g=4), in0=goh.t[:, :, :].unsqueeze(3).to_broadcast([128, NB, 4, 4]),
                                in1=gi.t[:, :, :].unsqueeze(2).to_broadcast([128, NB, 4, 4]), op=ALU.mult), [goh, gi], [gate])
    if not ROUTED:
        S.dma("sp", sc["GATE"], gate.t[:, :, :].rearrange("p b e -> p (b e)"), reads=[gate.r], writes=[sc["r_GATE"]])
    else:
        route_epilogue(S, A, sc, x1b, gate, gohall, plg, l)
    S.flush()
    A.close()


def route_epilogue(S, A, sc, x1b, gate, gohall, plg, l):
    def T_(name, shape, dt=F32):
        return Buf(A.sb(name, shape, dt))
    onesf = T_("onesf", [128, 128])
    tris = T_("tris", [128, 128])
    S.op("pool", lambda e: e.memset(onesf.t[:], 1.0), writes=[onesf.r])
    S.op("pool", lambda e: e.memset(tris.t[:], 1.0), writes=[tris.r])
    S.op("pool", lambda e: e.affine_select(out=tris.t[:], in_=tris.t[:], pattern=[[1, 128]], compare_op=ALU.is_ge, fill=0.0, base=-1,
                                           channel_multiplier=-1), reads=[tris.r], writes=[tris.r])
    pt, pr = plg[0], plg[1]
    for b in range(NB):
        S.op("pe", lambda e, b=b: e.matmul(pt.t[:, b * 4:(b + 1) * 4], lhsT=onesf.t[:], rhs=gohall.t[:, b, :], start=True, stop=True),
             reads=[onesf.r, gohall.r], writes=[pt.r])
        S.op("pe", lambda e, b=b: e.matmul(pr.t[:, b * 4:(b + 1) * 4], lhsT=tris.t[:], rhs=gohall.t[:, b, :], start=True, stop=True),
             reads=[tris.r, gohall.r], writes=[pr.r])
    totb = T_("totb", [128, NB, 4])
    cum = T_("cumb", [128, NB, 4])
    ones32 = T_("ones32", [128, NB])
    S.op("pool", lambda e: e.memset(ones32.t[:], 1.0), writes=[ones32.r])
    S.op("dve", lambda e: e.tensor_copy(out=totb.t[:, :, :], in_=pt.t[:, 0:NB * 4].rearrange("p (b g) -> p b g", g=4)), reads=[pt.r], writes=[totb.r])
    for g in range(4):
        S.op("dve", lambda e, g=g: e.tensor_tensor_scan(out=cum.t[:, :, g], data0=ones32.t[:, :], data1=totb.t[:, :, g], initial=0.0,
                                                        op0=ALU.mult, op1=ALU.add), reads=[ones32.r, totb.r, cum.r], writes=[cum.r])
    boffx = T_("boffx", [128, NB, 4])
    S.op("dve", lambda e: e.tensor_tensor(out=boffx.t[:], in0=cum.t[:], in1=totb.t[:], op=ALU.subtract), reads=[cum.r, totb.r], writes=[boffx.r])
    thr_i = T_("thri", [128, 16], I32)
    thr = T_("thr", [128, 16])
    S.op("pool", lambda e: e.iota(thr_i.t[:], pattern=[[512, 16]], base=0, channel_multiplier=0), writes=[thr_i.r])
    S.op("dve", lambda e: e.tensor_copy(out=thr.t[:], in_=thr_i.t[:]), reads=[thr_i.r], writes=[thr.r])
    cmp = T_("cmp", [128, 4, 8])
    ntl = T_("ntl", [128, 4])
    S.op("dve", lambda e: e.tensor_tensor(out=cmp.t[:], in0=cum.t[:, NB - 1, :].unsqueeze(2).to_broadcast([128, 4, 8]),
                                          in1=thr.t[:, 0:8].unsqueeze(1).to_broadcast([128, 4, 8]), op=ALU.is_gt), reads=[cum.r, thr.r], writes=[cmp.r])
    S.op("dve", lambda e: e.tensor_reduce(out=ntl.t[:], in_=cmp.t[:], axis=AX.X, op=ALU.add), reads=[cmp.r], writes=[ntl.r])
    S.op("dve", lambda e: e.tensor_scalar(out=ntl.t[:], in0=ntl.t[:], scalar1=512.0, scalar2=None, op0=ALU.mult), reads=[ntl.r], writes=[ntl.r])
    pst = T_("pst", [128, 4])
    pen = T_("pen", [128, 4])
    S.op("pool", lambda e: e.memset(pst.t[:], 0.0), writes=[pst.r])
    for g in range(1, 4):
        S.op("dve", lambda e, g=g: e.tensor_tensor(out=pst.t[:, g:g + 1], in0=pst.t[:, g - 1:g], in1=ntl.t[:, g - 1:g], op=ALU.add),
             reads=[pst.r, ntl.r], writes=[pst.r])
    S.op("dve", lambda e: e.tensor_tensor(out=pen.t[:], in0=pst.t[:], in1=ntl.t[:], op=ALU.add), reads=[pst.r, ntl.r], writes=[pen.r])
    v = T_("vdest", [128, NB, 4])
    S.op("dve", lambda e: e.tensor_tensor(out=v.t[:], in0=pr.t[:, 0:NB * 4].rearrange("p (b g) -> p b g", g=4), in1=boffx.t[:], op=ALU.add),
         reads=[pr.r, boffx.r], writes=[v.r])
    S.op("dve", lambda e: e.tensor_tensor(out=v.t[:], in0=v.t[:], in1=pst.t[:, :].unsqueeze(1).to_broadcast([128, NB, 4]), op=ALU.add),
         reads=[v.r, pst.r], writes=[v.r])
    S.op("dve", lambda e: e.tensor_tensor(out=v.t[:], in0=v.t[:], in1=gohall.t[:], op=ALU.mult), reads=[v.r, gohall.r], writes=[v.r])
    destf = T_("destf", [128, NB])
    desti = T_("desti", [128, NB], I32)
    S.op("dve", lambda e: e.tensor_reduce(out=destf.t[:], in_=v.t[:], axis=AX.X, op=ALU.add), reads=[v.r], writes=[destf.r])
    S.op("dve", lambda e: e.tensor_copy(out=desti.t[:], in_=destf.t[:]), reads=[destf.r], writes=[desti.r])
    S.dma("sp", sc["DEST"], desti.t[:], reads=[desti.r], writes=[sc["r_DEST"]])
    cmp2 = T_("cmp2", [128, NTILE, 4])
    gk = T_("gk", [128, NTILE])
    S.op("dve", lambda e: e.tensor_tensor(out=cmp2.t[:], in0=pen.t[:, :].unsqueeze(1).to_broadcast([128, NTILE, 4]),
                                          in1=thr.t[:, 0:NTILE].unsqueeze(2).to_broadcast([128, NTILE, 4]), op=ALU.is_le), reads=[pen.r, thr.r], writes=[cmp2.r])
    S.op("dve", lambda e: e.tensor_reduce(out=gk.t[:], in_=cmp2.t[:], axis=AX.X, op=ALU.add), reads=[cmp2.r], writes=[gk.r])
    S.op("dve", lambda e: e.tensor_scalar(out=gk.t[:], in0=gk.t[:], scalar1=3.0, scalar2=1024.0, op0=ALU.min, op1=ALU.mult), reads=[gk.r], writes=[gk.r])
    S.op("dve", lambda e: e.tensor_scalar(out=gk.t[:], in0=gk.t[:], scalar1=float(l * 4096), scalar2=None, op0=ALU.add), reads=[gk.r], writes=[gk.r])
    cw_i = T_("cwi", [128, 8], I32)
    cw = T_("cw", [128, 8])
    S.op("pool", lambda e: e.iota(cw_i.t[:], pattern=[[256, 4], [1, 2]], base=0, channel_multiplier=2), writes=[cw_i.r])
    S.op("dve", lambda e: e.tensor_copy(out=cw.t[:], in_=cw_i.t[:]), reads=[cw_i.r], writes=[cw.r])
    idxf = T_("idxf", [128, NTILE, 8])
    idxi = T_("idxi", [128, NTILE, 8], I32)
    S.op("dve", lambda e: e.tensor_tensor(out=idxf.t[:], in0=cw.t[:, :].unsqueeze(1).to_broadcast([128, NTILE, 8]),
                                          in1=gk.t[:, :].unsqueeze(2).to_broadcast([128, NTILE, 8]), op=ALU.add), reads=[cw.r, gk.r], writes=[idxf.r])
    S.op("dve", lambda e: e.tensor_copy(out=idxi.t[:], in_=idxf.t[:]), reads=[idxf.r], writes=[idxi.r])
    S.dma("sp", sc["IDXW"], idxi.t[:, :, :].rearrange("p k j -> p (k j)"), reads=[idxi.r], writes=[sc["r_IDXW"]])
    for b in range(NB):
        S.dma_fn("pool", lambda e, b=b: e.indirect_dma_start(out=sc["XS"][:, :], out_offset=bass.IndirectOffsetOnAxis(ap=desti.t[:, b:b + 1], axis=0),
                                                            in_=x1b.t[:, b, :], in_offset=None), reads=[desti.r, x1b.r], writes=[sc["r_XS"]])
        S.dma_fn("pool", lambda e, b=b: e.indirect_dma_start(out=sc["GS"][:, :], out_offset=bass.IndirectOffsetOnAxis(ap=desti.t[:, b:b + 1], axis=0),
                                                            in_=gate.t[:, b, :], in_offset=None), reads=[desti.r, gate.r], writes=[sc["r_GS"]])


def phase4(nc, S, T, l, sc, xout, r_xout, final, TG=1024):
    A = Alloc(nc)
    g_b = bcast_row(S, A, "ln2g", T["ln2_g"][l], DM)
    b_b = bcast_row(S, A, "ln2b", T["ln2_b"][l], DM)
    gate = Buf(A.sb("gate4", [128, NB, 16], F32))
    S.dma("sp", gate.t[:, :, :].rearrange("p b e -> p (b e)"), sc["GATE"], reads=[sc["r_GATE"]], writes=[gate.r])
    xT = Buf(A.sb("x1T", [128, 8, TG], BF16))
    accb = Buf(A.sb("accm", [128, TG // 128, DM], F32))
    w1 = rot(A, "sb", "w1", [128, 8, DEXP], BF16, 2)
    w3 = rot(A, "sb", "w3", [128, 8, DEXP], BF16, 2)
    w2 = rot(A, "sb", "w2", [128, 4, DM], BF16, 2)
    sa = rot(A, "sb", "sa", [128, 512], F32, 2)
    hT = rot(A, "sb", "hT", [128, 4, 512], BF16, 2)
    xs = rot(A, "sb", "xs4", [128, DM], F32, 2)
    yo = rot(A, "sb", "yo4", [128, DM], F32, 2)
    epsl = Buf(A.sb("epsl4", [128, 1], F32))
    S.op("pool", lambda e: e.memset(epsl.t[:], LN_EPS), writes=[epsl.r])
    small = [dict(st6=Buf(A.sb("st6b", [128, 2, 6], F32)), mv=Buf(A.sb("mvb", [128, 2], F32)), rs=Buf(A.sb("rsb", [128, 1], F32)), eps=epsl) for _ in range(2)]
    pa = rot(A, "ps", "pa", [128, 512], F32, 2)
    pb = rot(A, "ps", "pb", [128, 512], F32, 2)
    py = rot(A, "ps", "py", [128, 512], F32, 3)
    cnt = {"y": 0, "ab": 0, "w": 0}
    W1 = T["w1"][l]
    W3 = T["w3"][l]
    W2 = T["w2"][l]
    for gi in range(SEQ // TG):
        t0 = gi * TG
        S.dma("sp", xT.t[:], sc["X1T"][:, :, t0:t0 + TG], reads=[sc["r_X1T"]], writes=[xT.r])
        for ex in range(NEXP):
            i = cnt["w"]
            cnt["w"] += 1
            a1, a3, a2 = w1[i % 2], w3[i % 2], w2[i % 2]
            S.dma("pool", a1.t[:], W1[ex].rearrange("(c p) n -> p c n", p=128), writes=[a1.r])
            S.dma("pool", a3.t[:], W3[ex].rearrange("(c p) n -> p c n", p=128), writes=[a3.r])
            S.dma("pool", a2.t[:], W2[ex].rearrange("(c p) n -> p c n", p=128), writes=[a2.r])
            for tt in range(TG // 512):
                tc = slice(tt * 512, (tt + 1) * 512)
                h_ = hT[(ex * (TG // 512) + tt) % 2]
                for jc in range(4):
                    k_ = cnt["ab"]
                    cnt["ab"] += 1
                    pa_, pb_, sa_ = pa[k_ % 2], pb[k_ % 2], sa[k_ % 2]
                    for c in range(8):
                        S.op("pe", lambda e, c=c, jc=jc, pa_=pa_, a1=a1, tc=tc: e.matmul(pa_.t[:], lhsT=a1.t[:, c, jc * 128:(jc + 1) * 128], rhs=xT.t[:, c, tc],
                                                                                start=(c == 0), stop=(c == 7)), reads=[a1.r, xT.r], writes=[pa_.r])
                    for c in range(8):
                        S.op("pe", lambda e, c=c, jc=jc, pb_=pb_, a3=a3, tc=tc: e.matmul(pb_.t[:], lhsT=a3.t[:, c, jc * 128:(jc + 1) * 128], rhs=xT.t[:, c, tc],
                                                                                start=(c == 0), stop=(c == 7)), reads=[a3.r, xT.r], writes=[pb_.r])
                    S.op("act", lambda e, pa_=pa_, sa_=sa_: e.activation(out=sa_.t[:], in_=pa_.t[:], func=AF.Silu), reads=[pa_.r], writes=[sa_.r])
                    S.op("dve", lambda e, pb_=pb_, sa_=sa_, h_=h_, jc=jc: e.tensor_tensor(out=h_.t[:, jc, :], in0=pb_.t[:], in1=sa_.t[:], op=ALU.mult),
                         reads=[pb_.r, sa_.r], writes=[h_.r])
                for tb in range(4):
                    blk = tt * 4 + tb
                    gb = (t0 // 128) + blk
                    for hf in range(2):
                        y_ = py[cnt["y"] % 3]
                        cnt["y"] += 1
                        for jc in range(4):
                            S.op("pe", lambda e, jc=jc, tb=tb, hf=hf, y_=y_, h_=h_, a2=a2: e.matmul(y_.t[:], lhsT=h_.t[:, jc, tb * 128:(tb + 1) * 128],
                                                                                           rhs=a2.t[:, jc, hf * 512:(hf + 1) * 512], start=(jc == 0), stop=(jc == 3)),
                                 reads=[h_.r, a2.r], writes=[y_.r])
                        dst = accb.t[:, blk, hf * 512:(hf + 1) * 512]
                        if ex == 0:
                            S.op("dve", lambda e, y_=y_, dst=dst, gb=gb, ex=ex: e.tensor_scalar(out=dst, in0=y_.t[:], scalar1=gate.t[:, gb, ex:ex + 1], scalar2=None, op0=ALU.mult),
                                 reads=[y_.r, gate.r], writes=[accb.r])
                        else:
                            S.op("dve", lambda e, y_=y_, dst=dst, gb=gb, ex=ex: e.scalar_tensor_tensor(out=dst, in0=y_.t[:], scalar=gate.t[:, gb, ex:ex + 1], in1=dst,
                                                                                              op0=ALU.mult, op1=ALU.add), reads=[y_.r, gate.r, accb.r], writes=[accb.r])
        for blk in range(TG // 128):
            gb = (t0 // 128) + blk
            s_, y_, sm = xs[blk % 2], yo[blk % 2], small[blk % 2]
            S.dma("sp", s_.t[:], sc["X1"][gb * 128:(gb + 1) * 128, :], reads=[sc["r_X1"]], writes=[s_.r])
            S.op("dve", lambda e, s_=s_, blk=blk: e.scalar_tensor_tensor(out=s_.t[:], in0=s_.t[:], scalar=ALPHA, in1=accb.t[:, blk, :], op0=ALU.mult, op1=ALU.add),
                 reads=[s_.r, accb.r], writes=[s_.r])
            layernorm_block(S, s_, g_b, b_b, sm, y_)
            S.dma("sp", xout[gb * 128:(gb + 1) * 128, :], y_.t[:], reads=[y_.r], writes=[r_xout], final=final)
    S.flush()
    A.close()


def phase4r(nc, S, T, l, sc, xout, r_xout, final):
    A = Alloc(nc)
    ident = make_ident(A, S, BF16)
    g_b = bcast_row(S, A, "ln2g", T["ln2_g"][l], DM)
    b_b = bcast_row(S, A, "ln2b", T["ln2_b"][l], DM)
    dest = Buf(A.sb("dest4", [128, NB], I32))
    idxw = Buf(A.sb("idxw4", [128, NTILE * 8], I32))
    S.dma("sp", dest.t[:], sc["DEST"], reads=[sc["r_DEST"]], writes=[dest.r])
    S.dma("sp", idxw.t[:], sc["IDXW"], reads=[sc["r_IDXW"]], writes=[idxw.r])
    xs = rot(A, "sb", "xs4r", [128, 4, DM], BF16, 2)
    gs = rot(A, "sb", "gs4r", [128, 4, 16], F32, 2)
    gsel = rot(A, "sb", "gsel", [128, 4, 4], F32, 2)
    xT = rot(A, "sb", "xT4r", [128, 8, 512], BF16, 2)
    accs = rot(A, "sb", "acc4r", [128, 4, DM], F32, 2)
    w1 = rot(A, "sb", "w1r", [128, 8 * DEXP], BF16, 3)
    w3 = rot(A, "sb", "w3r", [128, 8 * DEXP], BF16, 3)
    w2 = rot(A, "sb", "w2r", [128, 4 * DM], BF16, 3)
    sa = rot(A, "sb", "sar", [128, 512], F32, 2)
    hT = rot(A, "sb", "hTr", [128, 4, 512], BF16, 2)
    mt = rot(A, "sb", "mt4", [128, DM], F32, 4)
    xo = rot(A, "sb", "xo4", [128, DM], F32, 4)
    yo = rot(A, "sb", "yo4r", [128, DM], F32, 4)
    epsl = Buf(A.sb("epsl4r", [128, 1], F32))
    S.op("pool", lambda e: e.memset(epsl.t[:], LN_EPS), writes=[epsl.r])
    small = [dict(st6=Buf(A.sb("st6r", [128, 2, 6], F32)), mv=Buf(A.sb("mvr", [128, 2], F32)), rs=Buf(A.sb("rsr", [128, 1], F32)), eps=epsl) for _ in range(4)]
    ptp = rot(A, "ps", "ptp", [128, DM], BF16, 1)
    pa = rot(A, "ps", "par", [128, 512], F32, 2)
    pb = rot(A, "ps", "pbr", [128, 512], F32, 2)
    py = rot(A, "ps", "pyr", [128, 512], F32, 3)
    Wv = [sc["WB"][i][:, :] for i in range(3)]
    cnt = {"y": 0, "ab": 0}
    steps = [(k, j) for k in range(NTILE) for j in range(4)]
    st = {}

    def front(si):
        k, j = steps[si]
        if j == 0:
            x_, g_, gl_, xT_, ac_ = xs[k % 2], gs[k % 2], gsel[k % 2], xT[k % 2], accs[k % 2]
            S.dma("sp", x_.t[:], sc["XS"][k * 512:(k + 1) * 512, :].rearrange("(b p) n -> p b n", p=128), reads=[sc["r_XS"]], writes=[x_.r])
            S.dma("sp", g_.t[:], sc["GS"][k * 512:(k + 1) * 512, :].rearrange("(b p) n -> p b n", p=128), reads=[sc["r_GS"]], writes=[g_.r])
            S.op("dve", lambda e: e.tensor_reduce(out=gl_.t[:], in_=g_.t[:, :, :].rearrange("p b (g j) -> p b j g", g=4), axis=AX.X, op=ALU.add),
                 reads=[g_.r], writes=[gl_.r])
            for blk in range(4):
                p = ptp[0]
                for c in range(8):
                    S.op("pe", lambda e, c=c, blk=blk: e.transpose(out=p.t[:, c * 128:(c + 1) * 128],
                                                                   in_=x_.t[:, blk, :].rearrange("p (pp c) -> p c pp", c=8)[:, c, :], identity=ident.t[:]),
                         reads=[x_.r, ident.r], writes=[p.r])
                S.op("act", lambda e, blk=blk: e.activation(out=xT_.t[:, :, blk * 128:(blk + 1) * 128], in_=p.t[:, :].rearrange("p (c t) -> p c t", c=8), func=AF.Copy),
                     reads=[p.r], writes=[xT_.r])
        xT_, ac_, gl_ = xT[k % 2], accs[k % 2], gsel[k % 2]
        a1, a3, a2, h_ = w1[si % 3], w3[si % 3], w2[si % 3], hT[si % 2]
        for wt_, src in ((a1, Wv[0]), (a3, Wv[1]), (a2, Wv[2])):
            for half in range(2):
                col = k * 8 + j * 2 + half
                S.dma_fn("pool", lambda e, wt_=wt_, src=src, half=half, col=col: e.indirect_dma_start(
                    out=wt_.t[:, half * 2048:(half + 1) * 2048], out_offset=None, in_=src,
                    in_offset=bass.IndirectOffsetOnAxis(ap=idxw.t[:, col:col + 1], axis=0)), reads=[idxw.r, sc["r_WB"][l]], writes=[wt_.r])
        w1v = a1.t[:, :].rearrange("p (c pp q) -> p c q pp", c=8, q=4)
        w3v = a3.t[:, :].rearrange("p (c pp q) -> p c q pp", c=8, q=4)
        for jc in range(4):
            k_ = cnt["ab"]
            cnt["ab"] += 1
            pa_, pb_, sa_ = pa[k_ % 2], pb[k_ % 2], sa[k_ % 2]
            for c in range(8):
                S.op("pe", lambda e, c=c, jc=jc, pa_=pa_: e.matmul(pa_.t[:], lhsT=w1v[:, c, jc, :], rhs=xT_.t[:, c, :], start=(c == 0), stop=(c == 7)),
                     reads=[a1.r, xT_.r], writes=[pa_.r])
            for c in range(8):
                S.op("pe", lambda e, c=c, jc=jc, pb_=pb_: e.matmul(pb_.t[:], lhsT=w3v[:, c, jc, :], rhs=xT_.t[:, c, :], start=(c == 0), stop=(c == 7)),
                     reads=[a3.r, xT_.r], writes=[pb_.r])
            S.op("act", lambda e, pa_=pa_, sa_=sa_: e.activation(out=sa_.t[:], in_=pa_.t[:], func=AF.Silu), reads=[pa_.r], writes=[sa_.r])
            S.op("dve", lambda e, pb_=pb_, sa_=sa_, jc=jc: e.tensor_tensor(out=h_.t[:, jc, :], in0=pb_.t[:], in1=sa_.t[:], op=ALU.mult),
                 reads=[pb_.r, sa_.r], writes=[h_.r])

    def back(si):
        k, j = steps[si]
        ac_, gl_, a2, h_ = accs[k % 2], gsel[k % 2], w2[si % 3], hT[si % 2]
        w2v = a2.t[:, :].rearrange("p (c n) -> p c n", c=4)
        for blk in range(4):
            for hf in range(2):
                y_ = py[cnt["y"] % 3]
                cnt["y"] += 1
                for jc in range(4):
                    S.op("pe", lambda e, jc=jc, blk=blk, hf=hf, y_=y_: e.matmul(y_.t[:], lhsT=h_.t[:, jc, blk * 128:(blk + 1) * 128],
                                                                               rhs=w2v[:, jc, hf * 512:(hf + 1) * 512], start=(jc == 0), stop=(jc == 3)),
                         reads=[h_.r, a2.r], writes=[y_.r])
                dst = ac_.t[:, blk, hf * 512:(hf + 1) * 512]
                if j == 0:
                    S.op("dve", lambda e, y_=y_, dst=dst, blk=blk: e.tensor_scalar(out=dst, in0=y_.t[:], scalar1=gl_.t[:, blk, j:j + 1], scalar2=None, op0=ALU.mult),
                         reads=[y_.r, gl_.r], writes=[ac_.r])
                else:
                    S.op("dve", lambda e, y_=y_, dst=dst, blk=blk: e.scalar_tensor_tensor(out=dst, in0=y_.t[:], scalar=gl_.t[:, blk, j:j + 1], in1=dst,
                                                                                         op0=ALU.mult, op1=ALU.add), reads=[y_.r, gl_.r, ac_.r], writes=[ac_.r])
        if j == 3:
            S.dma("sp", sc["YS"][k * 512:(k + 1) * 512, :].rearrange("(b p) n -> p b n", p=128), ac_.t[:], reads=[ac_.r], writes=[sc["r_YS"]])

    for si in range(len(steps) + 1):
        if si < len(steps):
            front(si)
        if si >= 1:
            back(si - 1)
    def comb_load(b):
        m_, x_ = mt[b % 4], xo[b % 4]
        S.dma_fn("pool", lambda e: e.indirect_dma_start(out=m_.t[:], out_offset=None, in_=sc["YS"][:, :],
                                                        in_offset=bass.IndirectOffsetOnAxis(ap=dest.t[:, b:b + 1], axis=0)),
                 reads=[dest.r, sc["r_YS"]], writes=[m_.r])
        S.dma("sp", x_.t[:], sc["X1"][b * 128:(b + 1) * 128, :], reads=[sc["r_X1"]], writes=[x_.r])
    for b in range(min(3, NB)):
        comb_load(b)
    for b in range(NB):
        m_, x_, y_, sm = mt[b % 4], xo[b % 4], yo[b % 4], small[b % 4]
        S.op("dve", lambda e, m_=m_, x_=x_: e.scalar_tensor_tensor(out=x_.t[:], in0=x_.t[:], scalar=ALPHA, in1=m_.t[:], op0=ALU.mult, op1=ALU.add),
             reads=[x_.r, m_.r], writes=[x_.r])
        layernorm_block(S, x_, g_b, b_b, sm, y_)
        if b + 3 < NB:
            comb_load(b + 3)
        S.dma("sp", xout[b * 128:(b + 1) * 128, :], y_.t[:], reads=[y_.r], writes=[r_xout], final=final)
    S.flush()
    A.close()


def rope_tables():
    pos = np.arange(SEQ, dtype=np.float32)
    out = {}
    for dim, nm in ((64, "rope64"), (32, "rope32")):
        half = dim // 2
        inv = (10000.0 ** (-np.arange(half, dtype=np.float32) / half)).astype(np.float32)
        ang = pos[None, :] * inv[:, None]
        c = np.cos(ang).astype(np.float32)
        s = np.sin(ang).astype(np.float32)
        out[nm + "c"] = np.ascontiguousarray(np.concatenate([c, c], 0))
        out[nm + "s"] = np.ascontiguousarray(np.concatenate([-s, s], 0))
    return out


W_SPECS = [("w_in", [DEPTH, DM, N_IN]), ("b_forget", [DEPTH, 4]), ("g_cq", [DEPTH, 256]), ("g_ckv", [DEPTH, 128]), ("w_uq", [DEPTH, 256, 384]),
           ("w_ukv", [DEPTH, 128, 512]), ("g_head", [DEPTH, 16, 64]), ("w_out", [DEPTH, DM, DM]), ("ln1_g", [DEPTH, DM]), ("ln1_b", [DEPTH, DM]),
           ("w_group", [DEPTH, DM, 4]), ("b_group", [DEPTH, 4]), ("w_expert", [DEPTH, DM, 16]), ("b_expert", [DEPTH, 16]),
           ("w1", [DEPTH, NEXP, DM, DEXP]), ("w3", [DEPTH, NEXP, DM, DEXP]), ("w2", [DEPTH, NEXP, DEXP, DM]), ("ln2_g", [DEPTH, DM]), ("ln2_b", [DEPTH, DM])]


def build_program(nseq=2, layers=(0, 1), phases=(1, 2, 3, 4), debug=False, heads=None, TG=1024):
    nc = bass.Bass("TRN2", target_bir_lowering=False)
    T = {}
    T["x"] = nc.dram_tensor("x", [nseq, SEQ, DM], F32, kind="ExternalInput").ap()
    for nm, shp in W_SPECS:
        T[nm] = nc.dram_tensor(nm, shp, F32, kind="ExternalInput").ap()
    for nm, rows in (("rope64c", 64), ("rope64s", 64), ("rope32c", 32), ("rope32s", 32)):
        T[nm] = nc.dram_tensor(nm, [rows, SEQ], F32, kind="ExternalInput").ap()
    out = nc.dram_tensor("out", [nseq, SEQ, DM], F32, kind="ExternalOutput").ap()
    dk = "ExternalOutput" if debug else "Internal"

    def scratch(name, shape, dt):
        return nc.dram_tensor(name, shape, dt, kind=dk).ap()
    sc = {}
    qs = scratch("QS", [12, 96, SEQ], BF16)
    ks = scratch("KS", [12, 96, SEQ], BF16)
    sc["QS"] = [qs[h] for h in range(12)]
    sc["KS"] = [ks[h] for h in range(12)]
    vs = scratch("VS", [6, 128, NB * 2 * 65], BF16)
    sc["VS"] = [vs[p] for p in range(6)]
    qd = scratch("QD", [3, 4, 64, SEQ], BF16)
    kd = scratch("KD", [3, 4, 64, SEQ], BF16)
    sc["QD"] = [[qd[g, h] for h in range(4)] for g in range(3)]
    sc["KD"] = [[kd[g, h] for h in range(4)] for g in range(3)]
    vd = scratch("VD", [3, 2, 128, NB * 2 * 65], BF16)
    sc["VD"] = [[vd[g, p] for p in range(2)] for g in range(3)]
    sc["OnT"] = scratch("OnT", [8, 128, SEQ], BF16)
    sc["X1"] = scratch("X1", [SEQ, DM], F32)
    sc["X1T"] = scratch("X1T", [128, 8, SEQ], BF16)
    sc["GATE"] = scratch("GATE", [128, NB * 16], F32)
    sc["XS"] = scratch("XS", [NSLOT, DM], BF16)
    sc["GS"] = scratch("GS", [NSLOT, 16], F32)
    sc["YS"] = scratch("YS", [NSLOT, DM], F32)
    sc["DEST"] = scratch("DEST", [128, NB], I32)
    sc["IDXW"] = scratch("IDXW", [128, NTILE * 8], I32)
    sc["WB"] = [scratch("WB%d" % i, [DEPTH * 4096, 2048], BF16) for i in range(3)]
    sc["r_WB"] = [Res() for _ in range(DEPTH)]
    xmid = scratch("XMID", [SEQ, DM], F32)
    sc["r_QS"] = [Res() for _ in range(12)]
    sc["r_KS"] = [Res() for _ in range(12)]
    sc["r_VS"] = [Res() for _ in range(6)]
    sc["r_QD"] = [[Res() for _ in range(4)] for _ in range(3)]
    sc["r_KD"] = [[Res() for _ in range(4)] for _ in range(3)]
    sc["r_VD"] = [[Res() for _ in range(2)] for _ in range(3)]
    for k in ("OnT", "X1", "X1T", "GATE", "XS", "GS", "YS", "DEST", "IDXW"):
        sc["r_" + k] = Res()
    r_xmid = Res()
    r_x = Res()
    r_out = Res()
    S = Sched(nc)
    for s in range(nseq):
        for li, l in enumerate(layers):
            xin, r_xin = (T["x"][s], r_x) if li == 0 else (xmid, r_xmid)
            last = li == len(layers) - 1
            xo, r_xo = (out[s], r_out) if last else (xmid, r_xmid)
            if 1 in phases:
                phase1(nc, S, T, xin, r_xin, l, sc)
            if 2 in phases:
                cv = (lambda l=l: convert_expert_weights(S, T, sc, l)) if (ROUTED and s == 0) else None
                phase2(nc, S, T, l, sc, heads=heads, after_sb=cv)
            if 3 in phases:
                phase3(nc, S, T, xin, r_xin, l, sc)
            if 4 in phases:
                if ROUTED:
                    phase4r(nc, S, T, l, sc, xo, r_xo, final=last)
                else:
                    phase4(nc, S, T, l, sc, xo, r_xo, final=last, TG=TG)
    S.close()
    return nc, S


def convert_expert_weights(S, T, sc, l):
    for i, nm in enumerate(("w1", "w3", "w2")):
        src = T[nm][l].rearrange("e k n -> (e k n)").rearrange("(r x) -> r x", x=2048)
        for ch in range(8):
            S.dma("pool", sc["WB"][i][l * 4096 + ch * 512:l * 4096 + (ch + 1) * 512, :], src[ch * 512:(ch + 1) * 512, :], writes=[sc["r_WB"][l]])


_CACHE = {}


def kernel(**inputs):
    n = 8
    nseq = 2
    x = np.ascontiguousarray(np.asarray(inputs["x"], dtype=np.float32))
    tabs = rope_tables()
    if "nc" not in _CACHE:
        _CACHE["nc"] = build_program(nseq=nseq)[0]
    nc = _CACHE["nc"]
    base = {nm: np.ascontiguousarray(np.asarray(inputs[nm], dtype=np.float32)) for nm, _ in W_SPECS}
    base.update(tabs)
    in_maps = []
    for c in range(n):
        m = dict(base)
        m["x"] = x[c * nseq:(c + 1) * nseq]
        in_maps.append(m)
    res = run_bass_kernel_spmd(nc, in_maps, core_ids=list(range(n)))
    return np.concatenate([r["out"] for r in res.results], axis=0).astype(np.float32)
```

```python
import numpy as np
from os import environ as _os_env
import concourse.bass as bass
import concourse.mybir as mybir
from concourse.bass_utils import run_bass_kernel_spmd

F32 = mybir.dt.float32
BF16 = mybir.dt.bfloat16
I32 = mybir.dt.int32
AF = mybir.ActivationFunctionType
ALU = mybir.AluOpType
AX = mybir.AxisListType

SES_MODE = int(_os_env.get("SES", "2"))
SAME_ENGINE_SYNC = SES_MODE != 0
NDMA_SLOTS = int(_os_env.get("NSLOTS", "8"))

SEQ = 4096
DM = 1024
NB = SEQ // 128
NT = SEQ // 512
DEPTH = 2
ALPHA = (2.0 * DEPTH) ** 0.25
LN_EPS = 1e-5
RMS_EPS = 1e-6
N_IN = 4260
C_SB, C_FOX, C_MLA, C_DIL = 0, 768, 1540, 1956
MLA_SCALE = 96.0 ** -0.5
DIL_R = (1, 4, 16)
NEXP = 16
DEXP = 512
NTILE = 11
NSLOT = NTILE * 512
ROUTED = bool(int(_os_env.get("ROUTED", "1")))
TRIM = bool(int(_os_env.get("TRIM", "1")))


class Res:
    __slots__ = ("w", "r", "excl")

    def __init__(self, excl=False):
        self.w = None
        self.r = []
        self.excl = excl


class Sched:
    ENG = ("pe", "act", "dve", "pool", "sp")

    def __init__(self, nc):
        self.nc = nc
        self.ops = {e: [] for e in self.ENG}
        self.cnt = {e: 0 for e in self.ENG}
        self.seen = {e: {} for e in self.ENG}
        self.sems = {}
        self.dma_slots = {}
        self.dma_rr = {}
        self.final_waits = []
        self._stack = []
        self.nops = 0

    def sem(self, key):
        if key not in self.sems:
            cm = self.nc.semaphore("s_" + "_".join(str(k) for k in (key if isinstance(key, tuple) else (key,))))
            s = cm.__enter__()
            self._stack.append(cm)
            self.sems[key] = s
        return self.sems[key]

    def _deps(self, eng, reads, writes):
        deps = {}

        def add(t, same_ok=True):
            if t is None:
                return
            k, v = t
            if k == eng and not same_ok:
                return
            if deps.get(k, 0) < v:
                deps[k] = v
        relax = SES_MODE == 2
        for r in reads:
            add(r.w)
            if r.excl:
                for t in r.r:
                    if t[0] != eng:
                        add(t)
        for w in writes:
            add(w.w, same_ok=not relax)
            for t in w.r:
                add(t, same_ok=not relax)
        waits = []
        seen = self.seen[eng]
        for k, v in deps.items():
            if k == eng and (eng == "pe" or not SAME_ENGINE_SYNC):
                continue
            if seen.get(k, 0) >= v:
                continue
            seen[k] = v
            waits.append((k, v))
        return waits

    def _commit(self, ticket, reads, writes):
        for r in reads:
            if len(r.r) > 16:
                m = {}
                for k, v in r.r:
                    if m.get(k, 0) < v:
                        m[k] = v
                r.r = list(m.items())
            r.r.append(ticket)
        for w in writes:
            w.w = ticket
            w.r = []

    def op(self, eng, fn, reads=(), writes=()):
        waits = self._deps(eng, reads, writes)
        self.cnt[eng] += 1
        ticket = (eng, self.cnt[eng])
        self.ops[eng].append((waits, fn, (eng, 1)))
        self._commit(ticket, reads, writes)
        self.nops += 1
        return ticket

    def dma(self, q, out, in_, reads=(), writes=(), final=False, **kw):
        fn = lambda e, out=out, in_=in_, kw=kw: e.dma_start(out=out, in_=in_, **kw)
        return self.dma_fn(q, fn, reads, writes, final)

    def dma_fn(self, q, fn, reads=(), writes=(), final=False):
        waits = self._deps(q, reads, writes)
        if q not in self.dma_slots:
            self.dma_slots[q] = [[("d", q, i), 0] for i in range(NDMA_SLOTS)]
            self.dma_rr[q] = 0
        i = self.dma_rr[q]
        self.dma_rr[q] = (i + 1) % NDMA_SLOTS
        slot = self.dma_slots[q][i]
        key, tot = slot
        if tot > 0 and self.seen[q].get(key, 0) < tot:
            self.seen[q][key] = tot
            waits.append((key, tot))
        slot[1] = tot + 16
        ticket = (key, tot + 16)
        self.ops[q].append((waits, fn, (key, 16)))
        self._commit(ticket, reads, writes)
        self.nops += 1
        if final:
            self.final_waits.append(ticket)
        return ticket

    def flush(self):
        totals = [(e, self.cnt[e]) for e in self.ENG if self.cnt[e] > 0]
        for q, slots in self.dma_slots.items():
            for key, tot in slots:
                if tot > 0:
                    totals.append((key, tot))
        for e in self.ENG:
            self.sem(e)
            for waits, fn, inc in self.ops[e]:
                for k, v in waits:
                    self.sem(k)
                self.sem(inc[0])
        ops = self.ops
        self.ops = {e: [] for e in self.ENG}
        for e in self.ENG:
            for k, v in totals:
                self.seen[e][k] = max(self.seen[e].get(k, 0), v)
        with self.nc.Block() as block:
            def run(engname):
                def body(e):
                    for waits, fn, inc in ops[engname]:
                        for k, v in waits:
                            e.wait_ge(self.sems[k], v)
                        fn(e).then_inc(self.sems[inc[0]], inc[1])
                    for k, v in totals:
                        e.wait_ge(self.sems[k], v)
                return body
            block.sync(run("sp"))
            block.tensor(run("pe"))
            block.scalar(run("act"))
            block.vector(run("dve"))
            block.gpsimd(run("pool"))

    def close(self):
        for cm in reversed(self._stack):
            cm.__exit__(None, None, None)
        self._stack = []


_UID = [0]


class Alloc:
    def __init__(self, nc):
        self.nc = nc
        self.stack = []

    @property
    def n(self):
        _UID[0] += 1
        return _UID[0]

    def sb(self, name, shape, dt):
        cm = self.nc.sbuf_tensor("%s_%d" % (name, self.n), list(shape), dt)
        t = cm.__enter__()
        self.stack.append(cm)
        return t

    def ps(self, name, shape, dt):
        cm = self.nc.psum_tensor("%s_%d" % (name, self.n), list(shape), dt)
        t = cm.__enter__()
        self.stack.append(cm)
        return t

    def close(self):
        for cm in reversed(self.stack):
            cm.__exit__(None, None, None)
        self.stack = []


class Buf:
    def __init__(self, t, excl=False):
        self.t = t
        self.r = Res(excl)


def rot(A, kind, name, shape, dt, n):
    f = A.sb if kind == "sb" else A.ps
    return [Buf(f(name + str(i), shape, dt), excl=(kind == "ps")) for i in range(n)]


def make_ident(A, S, dt):
    b = Buf(A.sb("ident", [128, 128], dt))
    S.op("pool", lambda e: e.memset(b.t[:], 1.0), writes=[b.r])
    S.op("pool", lambda e: e.affine_select(out=b.t[:], in_=b.t[:], pattern=[[-1, 128]], compare_op=ALU.is_equal,
                                           fill=0.0, base=0, channel_multiplier=1), reads=[b.r], writes=[b.r])
    return b


def perm_view(ap2d, r, t0, n):
    if r == 1:
        return ap2d[:, t0:t0 + n]
    sc = SEQ // r
    v = ap2d.rearrange("p (i c) -> p c i", c=r)
    c0, i0 = t0 // sc, t0 % sc
    if i0 + n <= sc:
        return v[:, c0, i0:i0 + n]
    assert i0 == 0 and n % sc == 0
    return v[:, c0:c0 + n // sc, :]


import os as _os
_P1SEC = _os.environ.get("P1SEC", "abcd")
_MSUB = _os.environ.get("MSUB", "qkrvptc")
_MC = _os.environ.get("MC", "1234")


def phase1(nc, S, T, xin, r_xin, l, sc):
    A = Alloc(nc)
    ident = make_ident(A, S, BF16)
    xT = Buf(A.sb("xT", [128, 8, SEQ], BF16))
    xs = rot(A, "sb", "xs", [128, DM], F32, 4)
    xb = rot(A, "sb", "xb", [128, DM], BF16, 3)
    pst = rot(A, "ps", "pst", [128, DM], BF16, 2)
    pj = rot(A, "ps", "pj", [128, 512], F32, 4)
    pv = rot(A, "ps", "pv", [128, 512], F32, 2)
    Win = T["w_in"][l].rearrange("(c p) n -> p c n", p=128)

    for b in range(NB):
        s, c_, p = xs[b % 4], xb[b % 3], pst[b % 2]
        S.dma("sp", s.t[:], xin[b * 128:(b + 1) * 128, :], reads=[r_xin], writes=[s.r])
        S.op("act", lambda e, s=s, c_=c_: e.activation(out=c_.t[:], in_=s.t[:], func=AF.Copy), reads=[s.r], writes=[c_.r])
        for c in range(8):
            S.op("pe", lambda e, c=c, c_=c_, p=p: e.transpose(out=p.t[:, c * 128:(c + 1) * 128], in_=c_.t[:, c * 128:(c + 1) * 128],
                                                              identity=ident.t[:]), reads=[c_.r, ident.r], writes=[p.r])
        S.op("dve", lambda e, b=b, p=p: e.tensor_copy(out=xT.t[:, :, b * 128:(b + 1) * 128],
                                                      in_=p.t[:, :].rearrange("p (c t) -> p c t", c=8)), reads=[p.r], writes=[xT.r])

    wts = rot(A, "sb", "wt", [128, 8, 416], BF16, 2)
    wsw = rot(A, "sb", "wsw", [128, 8, 256], BF16, 2)
    stg = rot(A, "sb", "stg", [128, 512], BF16, 4)
    vst = rot(A, "sb", "vst", [128, NB, 2, 65], BF16, 2)
    tmp1 = rot(A, "sb", "tmp1", [128, 512], F32, 2)
    tmp2 = rot(A, "sb", "tmp2", [128, 512], F32, 2)
    CT = Buf(A.sb("ropeC", [128, SEQ], BF16))
    ST = Buf(A.sb("ropeS", [128, SEQ], BF16))
    for v in vst:
        S.op("pool", lambda e, v=v: e.memset(v.t[:], 1.0), writes=[v.r])
    state = {"w": 0, "pj": 0, "stg": 0, "v": 0, "pv": 0}

    def load_w(col_ranges):
        w = wts[state["w"] % 2]
        state["w"] += 1
        o = 0
        for (c0, n) in col_ranges:
            S.dma("pool", w.t[:, :, o:o + n], Win[:, :, c0:c0 + n], writes=[w.r])
            o += n
        return w

    def nxt(key, lst):
        b = lst[state[key] % len(lst)]
        state[key] += 1
        return b

    def proj_fm(w, o, M, r, j, wtile=None):
        p = nxt("pj", pj)
        wt_ = w if wtile is None else wtile
        for c in range(8):
            S.op("pe", lambda e, c=c, p=p, wt_=wt_: e.matmul(p.t[0:M, :], lhsT=wt_.t[:, c, o:o + M],
                                                             rhs=perm_view(xT.t[:, c, :], r, j * 512, 512),
                                                             start=(c == 0), stop=(c == 7)),
                 reads=[wt_.r, xT.r], writes=[p.r])
        return p

    def store_rows(st, rows, dst, r_dst, j):
        S.dma("sp", dst[:, j * 512:(j + 1) * 512], st.t[rows[0]:rows[1], :], reads=[st.r], writes=[r_dst])

    def v_proj(w, o, r, vt, ncol=128):
        for b4 in range(NB // 4):
            p = nxt("pv", pv)
            for bb in range(4):
                b = b4 * 4 + bb
                for c in range(8):
                    S.op("pe", lambda e, c=c, b=b, bb=bb, p=p: e.matmul(p.t[:, bb * 128:(bb + 1) * 128],
                                                                      lhsT=perm_view(xT.t[:, c, :], r, b * 128, 128),
                                                                      rhs=w.t[:, c, o:o + ncol], start=(c == 0), stop=(c == 7)),
                         reads=[w.r, xT.r], writes=[p.r])
            S.op("dve", lambda e, b4=b4, p=p: e.tensor_copy(
                out=vt.t[:, b4 * 4:(b4 + 1) * 4, :, 0:64],
                in_=p.t[:, :].rearrange("p (b h d) -> p b h d", b=4, h=2)), reads=[p.r], writes=[vt.r])

    def scale_q(w):
        S.op("dve", lambda e: e.tensor_scalar(out=w.t[:, :, 0:128], in0=w.t[:, :, 0:128], scalar1=0.125, scalar2=None,
                                              op0=ALU.mult), reads=[w.r], writes=[w.r])

    for kind, cbase, hbase, pbase in (("sb", C_SB, 0, 0), ("fox", C_FOX, 4, 2)):
        for hp in range(2):
            w = load_w([(cbase + hp * 128, 128), (cbase + 256 + hp * 128, 128), (cbase + 512 + hp * 128, 128)])
            scale_q(w)
            for qk, dst, rd in ((0, sc["QS"], sc["r_QS"]), (1, sc["KS"], sc["r_KS"])):
                for j in range(NT):
                    p = proj_fm(w, qk * 128, 128, 1, j)
                    st = nxt("stg", stg)
                    S.op("act", lambda e, p=p, st=st: e.activation(out=st.t[:], in_=p.t[:], func=AF.Copy), reads=[p.r], writes=[st.r])
                    for hh in range(2):
                        h = hbase + hp * 2 + hh
                        store_rows(st, (hh * 64, hh * 64 + 64), dst[h][0:64, :], rd[h], j)
            vt = nxt("v", vst)
            v_proj(w, 256, 1, vt)
            S.dma("sp", sc["VS"][pbase + hp], vt.t[:, :, :, :].rearrange("p b h d -> p (b h d)"), reads=[vt.r], writes=[sc["r_VS"][pbase + hp]])

    S.flush()
    if "b" not in _P1SEC:
        A.close()
        return
    A2 = Alloc(nc)
    wf = Buf(A2.sb("wf", [128, 8, 4], BF16))
    S.dma("pool", wf.t[:], Win[:, :, C_FOX + 768:C_FOX + 772], writes=[wf.r])
    bfg = Buf(A2.sb("bfg", [4, 1], F32))
    S.dma("sp", bfg.t[:], T["b_forget"][l].rearrange("(h o) -> h o", o=1), writes=[bfg.r])
    S.op("dve", lambda e: e.tensor_scalar(out=bfg.t[:], in0=bfg.t[:], scalar1=-1.0, scalar2=None, op0=ALU.mult), reads=[bfg.r], writes=[bfg.r])
    nlf = Buf(A2.sb("nlf", [4, SEQ], F32))
    ncum = Buf(A2.sb("ncum", [4, SEQ], F32))
    ones4 = Buf(A2.sb("ones4", [4, 512], F32))
    S.op("pool", lambda e: e.memset(ones4.t[:], 1.0), writes=[ones4.r])
    for j in range(NT):
        p = proj_fm(wf, 0, 4, 1, j)
        S.op("act", lambda e, p=p, j=j: e.activation(out=nlf.t[:, j * 512:(j + 1) * 512], in_=p.t[0:4, :], func=AF.Exp,
                                                     bias=bfg.t[:, 0:1], scale=-1.0), reads=[p.r, bfg.r], writes=[nlf.r])
    S.op("act", lambda e: e.activation(out=nlf.t[:], in_=nlf.t[:], func=AF.Ln, bias=1.0), reads=[nlf.r], writes=[nlf.r])
    for j in range(NT):
        sl = slice(j * 512, (j + 1) * 512)
        init = 0.0 if j == 0 else ncum.t[:, j * 512 - 1:j * 512]
        S.op("dve", lambda e, sl=sl, init=init: e.tensor_tensor_scan(out=ncum.t[:, sl], data0=ones4.t[:, :], data1=nlf.t[:, sl],
                                                                     initial=init, op0=ALU.mult, op1=ALU.add),
             reads=[ones4.r, nlf.r, ncum.r], writes=[ncum.r])
    class _Alias:
        def __init__(self, t, r):
            self.t, self.r = t, r
    nlf_b = nlf.t[:, :].bitcast(BF16)
    parts = [_Alias(nlf_b[:, 0:SEQ], nlf.r), _Alias(nlf_b[:, SEQ:2 * SEQ], nlf.r), Buf(A2.sb("cpart2", [4, SEQ], BF16))]
    for i in range(3):
        S.op("dve", lambda e, i=i: e.tensor_copy(out=parts[i].t[:], in_=ncum.t[:]), reads=[ncum.r], writes=[parts[i].r])
        if i < 2:
            S.op("dve", lambda e, i=i: e.tensor_tensor(out=ncum.t[:], in0=ncum.t[:], in1=parts[i].t[:], op=ALU.subtract),
                 reads=[ncum.r, parts[i].r], writes=[ncum.r])
    ones3 = Buf(A2.sb("ones3", [35, SEQ], BF16))
    S.op("pool", lambda e: e.memset(ones3.t[0:3, :], 1.0), writes=[ones3.r])
    S.op("pool", lambda e: e.memset(ones3.t[32:35, :], -1.0), writes=[ones3.r])
    for h in range(4):
        H = 4 + h
        S.dma("sp", sc["KS"][H][64:67, :], ones3.t[32:35, :], reads=[ones3.r], writes=[sc["r_KS"][H]])
        S.dma("sp", sc["QS"][H][67:70, :], ones3.t[0:3, :], reads=[ones3.r], writes=[sc["r_QS"][H]])
        for i in range(3):
            S.dma("sp", sc["KS"][H][67 + i:68 + i, :], parts[i].t[h:h + 1, :], reads=[parts[i].r], writes=[sc["r_KS"][H]])
            S.dma("sp", sc["QS"][H][64 + i:65 + i, :], parts[i].t[h:h + 1, :], reads=[parts[i].r], writes=[sc["r_QS"][H]])
    S.flush()
    A2.close()

    if "c" not in _P1SEC:
        A.close()
        return
    w = load_w([(C_MLA, 416)])
    wkrs = nxt("w", wsw) if False else wsw[0]
    if "p" in _MSUB:
        S.op("pool", lambda e: e.tensor_copy(out=wkrs.t[:, :, 0:16], in_=w.t[:, :, 400:416]), reads=[w.r], writes=[wkrs.r])
        S.op("pool", lambda e: e.tensor_copy(out=wkrs.t[:, :, 16:32], in_=w.t[:, :, 384:400]), reads=[w.r], writes=[wkrs.r])
    A3 = Alloc(nc)
    wuq = Buf(A3.sb("wuq", [128, 2, 384], BF16))
    wuqs = Buf(A3.sb("wuqs", [128, 2, 384], BF16))
    wukv = Buf(A3.sb("wukv", [128, 512], BF16))
    S.dma("pool", wuq.t[:], T["w_uq"][l].rearrange("(c p) n -> p c n", p=128), writes=[wuq.r])
    S.dma("pool", wukv.t[:], T["w_ukv"][l], writes=[wukv.r])
    S.op("pool", lambda e: e.tensor_copy(out=wuqs.t[:], in_=wuq.t[:]), reads=[wuq.r], writes=[wuqs.r])
    for c2 in (range(2) if "p" in _MSUB else []):
        v4o = wuqs.t[:, c2, :].rearrange("p (h d) -> p h d", h=4)
        v4i = wuq.t[:, c2, :].rearrange("p (h d) -> p h d", h=4)
        S.op("pool", lambda e, v4o=v4o, v4i=v4i: e.tensor_copy(out=v4o[:, :, 64:80], in_=v4i[:, :, 80:96]), reads=[wuq.r, wuqs.r], writes=[wuqs.r])
        S.op("pool", lambda e, v4o=v4o, v4i=v4i: e.tensor_copy(out=v4o[:, :, 80:96], in_=v4i[:, :, 64:80]), reads=[wuq.r, wuqs.r], writes=[wuqs.r])
    gcq = Buf(A3.sb("gcq", [128, 2], F32))
    gckv = Buf(A3.sb("gckv", [128, 1], F32))
    for c2 in range(2):
        S.dma("sp", gcq.t[:, c2:c2 + 1], T["g_cq"][l][c2 * 128:(c2 + 1) * 128].rearrange("(p o) -> p o", o=1), writes=[gcq.r])
    S.dma("sp", gckv.t[:], T["g_ckv"][l].rearrange("(p o) -> p o", o=1), writes=[gckv.r])
    for tb, nm in (((CT, "rope32c"), (ST, "rope32s")) if "t" in _MSUB else []):
        S.dma("pool", tb.t[0:32, :], T[nm], writes=[tb.r])
        S.dma("pool", tb.t[64:96, :], T[nm], writes=[tb.r])
    onesq = Buf(A3.sb("onesq", [128, 128], BF16))
    oneskv = Buf(A3.sb("oneskv", [128, 128], BF16))
    epst = Buf(A3.sb("epst", [128, 1], F32))
    S.op("pool", lambda e: e.memset(epst.t[:], RMS_EPS), writes=[epst.r])
    S.op("pool", lambda e: e.memset(onesq.t[:], 1.0 / 256.0), writes=[onesq.r])
    S.op("pool", lambda e: e.memset(oneskv.t[:], 1.0 / 128.0), writes=[oneskv.r])
    cqg = rot(A3, "sb", "cqg", [128, 2, 512], BF16, 2)
    cq2 = rot(A3, "sb", "cq2", [128, 2, 512], BF16, 2)
    ckg = rot(A3, "sb", "ckg", [128, 512], BF16, 2)
    ck2 = rot(A3, "sb", "ck2", [128, 512], BF16, 2)
    rq = rot(A3, "sb", "rq", [128, 512], F32, 2)
    rkv = rot(A3, "sb", "rkv", [128, 512], F32, 2)
    rtok = rot(A3, "sb", "rtok", [128, 1], F32, 2)
    vstm = Buf(A3.sb("vstm", [128, NB, 4, 65], BF16))
    S.op("pool", lambda e: e.memset(vstm.t[:], 1.0), writes=[vstm.r])
    wukv_v = wukv.t[:, :].rearrange("p (h x) -> p h x", h=4)[:, :, 64:128]
    for j in (range(NT) if "c" in _MSUB else []):
        tc = slice(j * 512, (j + 1) * 512)
        a, a2, kg, k2, rq_, rkv_ = cqg[j % 2], cq2[j % 2], ckg[j % 2], ck2[j % 2], rq[j % 2], rkv[j % 2]
        for c2 in (range(2) if "1" in _MC else []):
            p = proj_fm(w, c2 * 128, 128, 1, j)
            S.op("dve", lambda e, p=p, c2=c2, a=a: e.tensor_scalar(out=a.t[:, c2, :], in0=p.t[:], scalar1=gcq.t[:, c2:c2 + 1], scalar2=None,
                                                                  op0=ALU.mult), reads=[p.r, gcq.r], writes=[a.r])
            S.op("act", lambda e, p=p, c2=c2, a2=a2: e.activation(out=a2.t[:, c2, :], in_=p.t[:], func=AF.Square), reads=[p.r], writes=[a2.r])
        if "2" in _MC:
            p = proj_fm(w, 256, 128, 1, j)
            if "5" not in _MC:
                S.op("dve", lambda e, p=p, kg=kg: e.tensor_scalar(out=kg.t[:], in0=p.t[:], scalar1=gckv.t[:, 0:1], scalar2=None, op0=ALU.mult),
                     reads=[p.r, gckv.r], writes=[kg.r])
            if "6" not in _MC:
                S.op("act", lambda e, p=p, k2=k2: e.activation(out=k2.t[:], in_=p.t[:], func=AF.Square), reads=[p.r], writes=[k2.r])
        if "3" in _MC:
            p = nxt("pj", pj)
            for c2 in range(2):
                S.op("pe", lambda e, p=p, c2=c2, a2=a2: e.matmul(p.t[:], lhsT=onesq.t[:], rhs=a2.t[:, c2, :], start=(c2 == 0), stop=(c2 == 1)),
                     reads=[onesq.r, a2.r], writes=[p.r])
            S.op("act", lambda e, p=p, rq_=rq_: e.activation(out=rq_.t[:], in_=p.t[:], func=AF.Sqrt, bias=epst.t[:, 0:1]), reads=[p.r, epst.r], writes=[rq_.r])
            S.op("dve", lambda e, rq_=rq_: e.reciprocal(out=rq_.t[:], in_=rq_.t[:]), reads=[rq_.r], writes=[rq_.r])
        if "4" in _MC:
            p = nxt("pj", pj)
            S.op("pe", lambda e, p=p, k2=k2: e.matmul(p.t[:], lhsT=oneskv.t[:], rhs=k2.t[:], start=True, stop=True), reads=[oneskv.r, k2.r], writes=[p.r])
            S.op("act", lambda e, p=p, rkv_=rkv_: e.activation(out=rkv_.t[:], in_=p.t[:], func=AF.Sqrt, bias=epst.t[:, 0:1]), reads=[p.r, epst.r], writes=[rkv_.r])
            S.op("dve", lambda e, rkv_=rkv_: e.reciprocal(out=rkv_.t[:], in_=rkv_.t[:]), reads=[rkv_.r], writes=[rkv_.r])
        for h in (range(4) if "q" in _MSUB else []):
            H = 8 + h
            pa, pb = nxt("pj", pj), nxt("pj", pj)
            for pp, ww in ((pa, wuq), (pb, wuqs)):
                for c2 in range(2):
                    S.op("pe", lambda e, pp=pp, ww=ww, c2=c2, h=h, a=a: e.matmul(pp.t[0:96, :], lhsT=ww.t[:, c2, h * 96:(h + 1) * 96], rhs=a.t[:, c2, :],
                                                                             start=(c2 == 0), stop=(c2 == 1)), reads=[ww.r, a.r], writes=[pp.r])
            st = nxt("stg", stg)
            t1, t2 = tmp1[h % 2], tmp2[h % 2]
            S.op("dve", lambda e, pa=pa, st=st, rq_=rq_: e.scalar_tensor_tensor(out=st.t[0:64, :], in0=pa.t[0:64, :], scalar=MLA_SCALE, in1=rq_.t[0:64, :],
                                                                             op0=ALU.mult, op1=ALU.mult), reads=[pa.r, rq_.r], writes=[st.r])
            S.op("dve", lambda e, pa=pa, t1=t1, tc=tc: e.tensor_tensor(out=t1.t[64:96, :], in0=pa.t[64:96, :], in1=CT.t[64:96, tc], op=ALU.mult),
                 reads=[pa.r, CT.r], writes=[t1.r])
            S.op("dve", lambda e, pb=pb, t2=t2, tc=tc: e.tensor_tensor(out=t2.t[64:96, :], in0=pb.t[64:96, :], in1=ST.t[64:96, tc], op=ALU.mult),
                 reads=[pb.r, ST.r], writes=[t2.r])
            S.op("pool", lambda e, t1=t1, t2=t2: e.tensor_tensor(out=t1.t[64:96, :], in0=t1.t[64:96, :], in1=t2.t[64:96, :], op=ALU.add),
                 reads=[t1.r, t2.r], writes=[t1.r])
            S.op("dve", lambda e, t1=t1, st=st, rq_=rq_: e.scalar_tensor_tensor(out=st.t[64:96, :], in0=t1.t[64:96, :], scalar=MLA_SCALE, in1=rq_.t[64:96, :],
                                                                             op0=ALU.mult, op1=ALU.mult), reads=[t1.r, rq_.r, st.r], writes=[st.r])
            store_rows(st, (0, 96), sc["QS"][H][0:96, :], sc["r_QS"][H], j)
        for h in (range(4) if "k" in _MSUB else []):
            H = 8 + h
            p = nxt("pj", pj)
            S.op("pe", lambda e, p=p, h=h, kg=kg: e.matmul(p.t[0:64, :], lhsT=wukv.t[:, h * 128:h * 128 + 64], rhs=kg.t[:], start=True, stop=True),
                 reads=[wukv.r, kg.r], writes=[p.r])
            st = nxt("stg", stg)
            S.op("dve", lambda e, p=p, st=st, rkv_=rkv_: e.tensor_tensor(out=st.t[0:64, :], in0=p.t[0:64, :], in1=rkv_.t[0:64, :], op=ALU.mult),
                 reads=[p.r, rkv_.r], writes=[st.r])
            store_rows(st, (0, 64), sc["KS"][H][0:64, :], sc["r_KS"][H], j)
        if "r" not in _MSUB:
            continue
        pa = proj_fm(w, 384, 32, 1, j)
        pb = proj_fm(wkrs, 0, 32, 1, j)
        t1, t2 = tmp1[0], tmp2[0]
        st = nxt("stg", stg)
        S.op("dve", lambda e, pa=pa, t1=t1, tc=tc: e.tensor_tensor(out=t1.t[0:32, :], in0=pa.t[0:32, :], in1=CT.t[0:32, tc], op=ALU.mult),
             reads=[pa.r, CT.r], writes=[t1.r])
        S.op("dve", lambda e, pb=pb, t2=t2, tc=tc: e.tensor_tensor(out=t2.t[0:32, :], in0=pb.t[0:32, :], in1=ST.t[0:32, tc], op=ALU.mult),
             reads=[pb.r, ST.r], writes=[t2.r])
        S.op("pool", lambda e, t1=t1, t2=t2, st=st: e.tensor_tensor(out=st.t[0:32, :], in0=t1.t[0:32, :], in1=t2.t[0:32, :], op=ALU.add),
             reads=[t1.r, t2.r], writes=[st.r])
        for h in range(4):
            store_rows(st, (0, 32), sc["KS"][8 + h][64:96, :], sc["r_KS"][8 + h], j)
        for bb in (range(4) if "v" in _MSUB else []):
            b = j * 4 + bb
            p = nxt("pv", pv)
            S.op("pe", lambda e, p=p, bb=bb, kg=kg: e.matmul(p.t[:, 0:256], lhsT=kg.t[:, bb * 128:(bb + 1) * 128], rhs=wukv_v, start=True, stop=True),
                 reads=[wukv.r, kg.r], writes=[p.r])
            S.op("pe", lambda e, p=p, bb=bb, k2=k2: e.matmul(p.t[:, 256:257], lhsT=k2.t[:, bb * 128:(bb + 1) * 128], rhs=oneskv.t[:, 0:1], start=True, stop=True),
                 reads=[oneskv.r, k2.r], writes=[p.r])
            rt = rtok[b % 2]
            S.op("act", lambda e, p=p, rt=rt: e.activation(out=rt.t[:], in_=p.t[:, 256:257], func=AF.Sqrt, bias=epst.t[:, 0:1]), reads=[p.r, epst.r], writes=[rt.r])
            S.op("dve", lambda e, rt=rt: e.reciprocal(out=rt.t[:], in_=rt.t[:]), reads=[rt.r], writes=[rt.r])
            S.op("dve", lambda e, p=p, b=b, rt=rt: e.tensor_scalar(out=vstm.t[:, b, :, 0:64], in0=p.t[:, 0:256].rearrange("p (h d) -> p h d", h=4),
                                                                  scalar1=rt.t[:, 0:1], scalar2=None, op0=ALU.mult), reads=[p.r, rt.r], writes=[vstm.r])
    for hp in range(2):
        S.dma("sp", sc["VS"][4 + hp].rearrange("p (b h d) -> p b h d", b=NB, h=2), vstm.t[:, :, 2 * hp:2 * hp + 2, :], reads=[vstm.r], writes=[sc["r_VS"][4 + hp]])

    S.flush()
    A3.close()
    if "d" not in _P1SEC:
        A.close()
        return
    for tb, nm in ((CT, "rope64c"), (ST, "rope64s")):
        S.dma("pool", tb.t[0:64, :], T[nm], writes=[tb.r])
        S.dma("pool", tb.t[64:128, :], T[nm], writes=[tb.r])
    for g in range(3):
        r = DIL_R[g]
        for hp in range(2):
            o = g * 256 + hp * 128
            w = load_w([(C_DIL + o, 128), (C_DIL + 768 + o, 128), (C_DIL + 1536 + o, 128)])
            scale_q(w)
            ws = wsw[(g * 2 + hp) % 2]
            for c in range(8):
                vo = ws.t[:, c, :].rearrange("p (h f d) -> p h f d", h=4, f=2)
                vi = w.t[:, c, 0:256].rearrange("p (h f d) -> p h f d", h=4, f=2)
                S.op("pool", lambda e, vo=vo, vi=vi: e.tensor_copy(out=vo[:, :, 0, :], in_=vi[:, :, 1, :]), reads=[w.r], writes=[ws.r])
                S.op("pool", lambda e, vo=vo, vi=vi: e.tensor_copy(out=vo[:, :, 1, :], in_=vi[:, :, 0, :]), reads=[w.r], writes=[ws.r])
            for qk, dst, rd in ((0, sc["QD"], sc["r_QD"]), (1, sc["KD"], sc["r_KD"])):
                for j in range(NT):
                    pa = proj_fm(w, qk * 128, 128, r, j)
                    pb = proj_fm(ws, qk * 128, 128, r, j)
                    t1, t2 = tmp1[j % 2], tmp2[j % 2]
                    st = nxt("stg", stg)
                    cv = perm_view(CT.t[:, :], r, j * 512, 512)
                    sv = perm_view(ST.t[:, :], r, j * 512, 512)
                    shp = None if len(cv.shape) == 2 else cv.shape

                    def v3(ap):
                        return ap if shp is None else ap.rearrange("p (a b) -> p a b", a=shp[1])
                    S.op("dve", lambda e, pa=pa, t1=t1, cv=cv, v3=v3: e.tensor_tensor(out=v3(t1.t[:]), in0=v3(pa.t[:]), in1=cv, op=ALU.mult),
                         reads=[pa.r, CT.r], writes=[t1.r])
                    S.op("dve", lambda e, pb=pb, t2=t2, sv=sv, v3=v3: e.tensor_tensor(out=v3(t2.t[:]), in0=v3(pb.t[:]), in1=sv, op=ALU.mult),
                         reads=[pb.r, ST.r], writes=[t2.r])
                    S.op("pool", lambda e, t1=t1, t2=t2, st=st: e.tensor_tensor(out=st.t[:], in0=t1.t[:], in1=t2.t[:], op=ALU.add),
                         reads=[t1.r, t2.r], writes=[st.r])
                    for hh in range(2):
                        store_rows(st, (hh * 64, hh * 64 + 64), dst[g][hp * 2 + hh], rd[g][hp * 2 + hh], j)
            vt = nxt("v", vst)
            v_proj(w, 256, r, vt)
            S.dma("sp", sc["VD"][g][hp], vt.t[:, :, :, :].rearrange("p b h d -> p (b h d)"), reads=[vt.r], writes=[sc["r_VD"][g][hp]])
    S.flush()
    A.close()


def phase2(nc, S, T, l, sc, heads=None, after_sb=None):
    A = Alloc(nc)
    negtri = Buf(A.sb("negtri", [128, 128], BF16))
    S.op("pool", lambda e: e.memset(negtri.t[:], -1.0), writes=[negtri.r])
    S.op("pool", lambda e: e.affine_select(out=negtri.t[:], in_=negtri.t[:], pattern=[[-1, 128]], compare_op=ALU.is_ge, fill=0.0, base=0,
                                           channel_multiplier=1), reads=[negtri.r], writes=[negtri.r])
    ones = Buf(A.sb("ones", [128, 128], BF16))
    S.op("pool", lambda e: e.memset(ones.t[:], 1.0), writes=[ones.r])
    wn = Buf(A.sb("wn", [65, 64], BF16))
    wnsb = Buf(A.sb("wnsb", [65, 64], BF16))
    for t_, v_ in ((wn, RMS_EPS), (wnsb, 0.0)):
        S.op("pool", lambda e, t_=t_: e.memset(t_.t[:], 1.0 / 64.0), writes=[t_.r])
        S.op("pool", lambda e, t_=t_, v_=v_: e.memset(t_.t[64:65, :], v_), reads=[t_.r], writes=[t_.r])
    gh = Buf(A.sb("gh", [64, 16], F32))
    for h_ in range(16):
        S.dma("sp", gh.t[:, h_:h_ + 1], T["g_head"][l][h_].rearrange("(d o) -> d o", o=1), writes=[gh.r])
    eps2 = Buf(A.sb("eps2", [64, 2], F32))
    S.op("pool", lambda e: e.memset(eps2.t[:, 0:1], RMS_EPS), writes=[eps2.r])
    S.op("pool", lambda e: e.memset(eps2.t[:, 1:2], 0.0), reads=[eps2.r], writes=[eps2.r])

    Qt = rot(A, "sb", "Qt", [128, SEQ], BF16, 4)
    Kt = rot(A, "sb", "Kt", [128, SEQ], BF16, 4)
    Vt = rot(A, "sb", "Vt", [128, NB, 2, 65], BF16, 2)
    pz = rot(A, "ps", "pz", [128, 512], F32, 3)
    po = rot(A, "ps", "po", [128, 512], F32, 2)
    pc = rot(A, "ps", "pc", [128, 512], F32, 2)
    pss = rot(A, "ps", "pss", [128, 512], F32, 1)
    Pb = rot(A, "sb", "Pb", [128, 512], BF16, 8)
    eb = rot(A, "sb", "eb", [128, 512], F32, 2)
    spb = rot(A, "sb", "spb", [128, 512], BF16, 3)
    lw = rot(A, "sb", "lw", [128, 512], F32, 4)
    Rsb = Buf(A.sb("Rsb", [128, 512], F32))
    sqb = rot(A, "sb", "sqb", [65, 512], BF16, 2)
    osb = rot(A, "sb", "osb", [65, 512], F32, 3)
    def mk_mask(name, n, conds):
        m = Buf(A.sb(name, [128, n], BF16))
        S.op("pool", lambda e: e.memset(m.t[:], 1.0), writes=[m.r])
        for (step, base, cm) in conds:
            S.op("pool", lambda e, step=step, base=base, cm=cm: e.affine_select(out=m.t[:], in_=m.t[:], pattern=[[step, n]], compare_op=ALU.is_ge, fill=0.0,
                                                                                base=base, channel_multiplier=cm), reads=[m.r], writes=[m.r])
        return m
    maskS = [mk_mask("ms%d" % o, 512, [(1, -128 * o - 1, -1)]) for o in (3, 2, 1, 0)][::-1]
    maskC = [mk_mask("mc%d" % o, 512, [(1, -128 * o, -1)]) for o in range(4)]
    maskT = [mk_mask("mt0", 256, [(1, 0, -1), (-1, 128, 1)]), mk_mask("mt1", 128, [(-1, 0, 1)])]
    maskD = {512: {o: mk_mask("md%d" % (o + 1), 512, [(1, -128 * o, -1), (-1, 128 + 128 * o, 1)]) for o in range(-1, 4)},
             256: {o: mk_mask("me%d" % o, 256, [(1, -128 * o, -1), (-1, 128 + 128 * o, 1)]) for o in range(0, 2)}}
    stb = rot(A, "sb", "stb", [64, 512], F32, 2)
    yb = rot(A, "sb", "yb", [64, 512], BF16, 2)
    acc = rot(A, "sb", "acc", [65, SEQ], F32, 2)
    st = {"fin": 0, "ld": 0, "vld": 0, "o": 0}

    def finish(src_ap, r_src, h, t0, n, is_sb, in_sbuf=False):
        i = st["fin"]
        st["fin"] += 1
        sq, s_, y, ps_ = sqb[i % 2], stb[i % 2], yb[i % 2], pss[0]
        if in_sbuf:
            o_ap, r_o = src_ap, r_src
        else:
            ob = osb[i % 3]
            S.op("act", lambda e: e.activation(out=ob.t[:, 0:n], in_=src_ap, func=AF.Copy), reads=[r_src], writes=[ob.r])
            o_ap, r_o = ob.t[:, 0:n], ob.r
        S.op("dve", lambda e: e.tensor_tensor(out=sq.t[:, 0:n], in0=o_ap, in1=o_ap, op=ALU.mult), reads=[r_o], writes=[sq.r])
        wn_ = wnsb if is_sb else wn
        S.op("pe", lambda e: e.matmul(ps_.t[0:64, 0:n], lhsT=wn_.t[:, :], rhs=sq.t[:, 0:n], start=True, stop=True), reads=[wn_.r, sq.r], writes=[ps_.r])
        S.op("act", lambda e: e.activation(out=s_.t[:, 0:n], in_=ps_.t[0:64, 0:n], func=AF.Ln, bias=(eps2.t[:, 0:1] if is_sb else eps2.t[:, 1:2])),
             reads=[ps_.r, eps2.r], writes=[s_.r])
        S.op("act", lambda e: e.activation(out=s_.t[:, 0:n], in_=s_.t[:, 0:n], func=AF.Exp, scale=-0.5), reads=[s_.r], writes=[s_.r])
        S.op("dve", lambda e: e.scalar_tensor_tensor(out=y.t[:, 0:n], in0=o_ap[0:64], scalar=gh.t[:, h:h + 1], in1=s_.t[:, 0:n],
                                                     op0=ALU.mult, op1=ALU.mult), reads=[r_o, gh.r, s_.r], writes=[y.r])
        S.dma("sp", sc["OnT"][h // 2, (h % 2) * 64:(h % 2) * 64 + 64, t0:t0 + n], y.t[:, 0:n], reads=[y.r], writes=[sc["r_OnT"]])

    def load_qk(qsrc, r_q, ksrc, r_k, kd):
        i = st["ld"]
        st["ld"] += 1
        q, k = Qt[i % 4], Kt[i % 4]
        S.dma("sp", q.t[0:kd, :], qsrc, reads=[r_q], writes=[q.r])
        S.dma("sp", k.t[0:kd, :], ksrc, reads=[r_k], writes=[k.r])
        return q, k

    def load_v(vsrc, r_v):
        i = st["vld"]
        st["vld"] += 1
        v = Vt[i % 2]
        S.dma("sp", v.t[:, :, :, :].rearrange("p b h d -> p (b h d)"), vsrc, reads=[r_v], writes=[v.r])
        return v

    def make_stages(steps, q, k, kd, v, hh, kind, done_cb, u=0):
        n_ = len(steps)
        ctx = [dict() for _ in range(n_)]
        zsel = [[pz[0], pz[1]], [pz[2], pc[0]]][u]

        def s1(i):
            sp_ = steps[i]
            z = zsel[i % 2]
            q0, n, kb = sp_["q0"], sp_["n"], sp_["kb"]
            S.op("pe", lambda e: e.matmul(z.t[:, 0:n], lhsT=k.t[0:kd, kb * 128:(kb + 1) * 128], rhs=q.t[0:kd, q0:q0 + n], start=True, stop=True),
                 reads=[k.r, q.r], writes=[z.r])
            if kind == "sb":
                e_, s_ = eb[i % 2], spb[i % 3]
                S.op("act", lambda e: e.activation(out=e_.t[:, 0:n], in_=z.t[:, 0:n], func=AF.Exp), reads=[z.r], writes=[e_.r])
                S.op("act", lambda e: e.activation(out=s_.t[:, 0:n], in_=e_.t[:, 0:n], func=AF.Ln, bias=1.0), reads=[e_.r], writes=[s_.r])
                if sp_["mask"] is not None:
                    mk = sp_["mask"]
                    S.op("pool", lambda e: e.tensor_tensor(out=s_.t[:, 0:n], in0=s_.t[:, 0:n], in1=mk.t[:, 0:n], op=ALU.mult), reads=[s_.r, mk.r], writes=[s_.r])
                ctx[i]["sp"] = s_
            else:
                p_ = Pb[u * 4 + i % 4]
                if kind == "fox" and sp_["mask"] is not None:
                    l_ = lw[u * 2 + i % 2]
                    S.op("dve", lambda e: e.tensor_scalar(out=l_.t[:, 0:n], in0=z.t[:, 0:n], scalar1=60.0, scalar2=None, op0=ALU.min), reads=[z.r], writes=[l_.r])
                    S.op("act", lambda e: e.activation(out=p_.t[:, 0:n], in_=l_.t[:, 0:n], func=AF.Exp), reads=[l_.r], writes=[p_.r])
                else:
                    S.op("act", lambda e: e.activation(out=p_.t[:, 0:n], in_=z.t[:, 0:n], func=AF.Exp), reads=[z.r], writes=[p_.r])
                if sp_["mask"] is not None:
                    mk = sp_["mask"]
                    S.op("dve", lambda e: e.tensor_tensor(out=p_.t[:, 0:n], in0=p_.t[:, 0:n], in1=mk.t[:, 0:n], op=ALU.mult), reads=[p_.r, mk.r], writes=[p_.r])
                ctx[i]["P"] = p_

        def s2(i):
            if kind != "sb":
                return
            sp_ = steps[i]
            q0, n, kb = sp_["q0"], sp_["n"], sp_["kb"]
            s_ = ctx[i]["sp"]
            c_, rc, l_, p_ = pc[i % 2], pz[2], lw[i % 2], Pb[i % 4]
            S.op("pe", lambda e: e.matmul(c_.t[:, 0:n], lhsT=k.t[0:kd, kb * 128:(kb + 1) * 128], rhs=q.t[0:kd, q0:q0 + n], start=True, stop=False),
                 reads=[k.r, q.r], writes=[c_.r])
            S.op("pe", lambda e: e.matmul(c_.t[:, 0:n], lhsT=negtri.t[:], rhs=s_.t[:, 0:n], start=False, stop=True), reads=[negtri.r, s_.r], writes=[c_.r])
            S.op("pe", lambda e: e.matmul(rc.t[:, 0:n], lhsT=ones.t[:], rhs=s_.t[:, 0:n], start=True, stop=True), reads=[ones.r, s_.r], writes=[rc.r])
            oc = sp_["oc"]
            if sp_["first"]:
                S.op("pool", lambda e: e.memset(Rsb.t[:], 0.0), writes=[Rsb.r])
            S.op("dve", lambda e: e.tensor_tensor(out=l_.t[:, 0:n], in0=c_.t[:, 0:n], in1=Rsb.t[:, oc:oc + n], op=ALU.subtract), reads=[c_.r, Rsb.r], writes=[l_.r])
            S.op("dve", lambda e: e.tensor_tensor(out=Rsb.t[:, oc:oc + n], in0=rc.t[:, 0:n], in1=Rsb.t[:, oc:oc + n], op=ALU.add), reads=[rc.r, Rsb.r], writes=[Rsb.r])
            S.op("act", lambda e: e.activation(out=p_.t[:, 0:n], in_=l_.t[:, 0:n], func=AF.Exp), reads=[l_.r], writes=[p_.r])
            if sp_["mask"] is not None:
                mk = sp_["mask"]
                S.op("pool", lambda e: e.tensor_tensor(out=p_.t[:, 0:n], in0=p_.t[:, 0:n], in1=mk.t[:, 0:n], op=ALU.mult), reads=[p_.r, mk.r], writes=[p_.r])
            ctx[i]["P"] = p_

        def s3(i):
            sp_ = steps[i]
            n, kb = sp_["n"], sp_["kb"]
            if sp_["first"] and u == 0:
                st["o"] += 1
            o_ = po[st["o"] % 2] if u == 0 else pc[1]
            p_ = ctx[i]["P"]
            oc = sp_["oc"]
            S.op("pe", lambda e: e.matmul(o_.t[0:65, oc:oc + n], lhsT=v.t[:, kb, hh, :], rhs=p_.t[:, 0:n], start=sp_["first"], stop=sp_["last"],
                                          skip_group_check=True), reads=[v.r, p_.r], writes=[o_.r])
            if sp_["last"]:
                done_cb(o_, sp_)

        return n_, s1, s2, s3

    def drive(stage_sets):
        nmax = max(ss[0] for ss in stage_sets)
        for i in range(nmax + 2):
            for n_, s1, s2, s3 in stage_sets:
                if i < n_:
                    s1(i)
            for n_, s1, s2, s3 in stage_sets:
                if 0 <= i - 1 < n_:
                    s2(i - 1)
            for n_, s1, s2, s3 in stage_sets:
                if 0 <= i - 2 < n_:
                    s3(i - 2)

    def run_steps(steps, q, k, kd, v, hh, kind, done_cb):
        drive([make_stages(steps, q, k, kd, v, hh, kind, done_cb, 0)])

    def causal_steps(strict, descending):
        steps = []
        for qt in range(NT):
            q0 = qt * 512
            kbs = list(range(0, 4 * qt + 4))
            if descending:
                kbs = kbs[::-1]
            for ii, kb in enumerate(kbs):
                o = kb - 4 * qt
                if o >= 0 and TRIM:
                    steps.append(dict(q0=q0 + 128 * o, n=512 - 128 * o, oc=128 * o, kb=kb, mask=(maskS if strict else maskC)[0],
                                      first=(ii == 0), last=(ii == len(kbs) - 1), tq0=q0, tn=512))
                else:
                    steps.append(dict(q0=q0, n=512, oc=0, kb=kb, mask=((maskS if strict else maskC)[o] if o >= 0 else None),
                                      first=(ii == 0), last=(ii == len(kbs) - 1), tq0=q0, tn=512))
        return steps

    def dil_steps(r):
        sc_ = SEQ // r
        n = min(512, sc_)
        steps = []
        for q0 in range(0, SEQ, n):
            cs = (q0 // sc_) * sc_
            k_lo = max(cs, q0 - 128)
            kbs = list(range(k_lo // 128, (q0 + n) // 128))
            for ii, kb in enumerate(kbs):
                k0 = 128 * kb
                c0, c1 = max(0, k0 - q0), min(n, k0 + 256 - q0)
                if TRIM:
                    d = q0 + c0 - k0
                    mk = maskT[0] if d == 0 else maskT[1]
                    steps.append(dict(q0=q0 + c0, n=c1 - c0, oc=c0, kb=kb, mask=mk, first=(ii == 0), last=(ii == len(kbs) - 1), tq0=q0, tn=n))
                else:
                    steps.append(dict(q0=q0, n=n, oc=0, kb=kb, mask=maskD[n][kb - q0 // 128], first=(ii == 0), last=(ii == len(kbs) - 1), tq0=q0, tn=n))
        return steps

    hsel = (lambda h: True) if heads is None else (lambda h: h in heads)
    groups = []
    for kind, hbase, pbase, kd in (("sb", 0, 0, 64), ("fox", 4, 2, 70), ("mla", 8, 4, 96)):
        for hp in range(2):
            js = [(kind, hbase + hp * 2 + hh, pbase + hp, hh, kd) for hh in range(2) if hsel(hbase + hp * 2 + hh)]
            if kind == "sb":
                groups += [[j] for j in js]
            elif js:
                groups.append(js)
    step_cache = {"sb": causal_steps(True, True), "fox": causal_steps(False, False)}
    step_cache["mla"] = step_cache["fox"]
    loaded = {}
    vcur = {}

    def prefetch(group):
        for job in group:
            kind, h, pr_, hh, kd = job
            if pr_ not in vcur:
                vcur.clear()
                vcur[pr_] = load_v(sc["VS"][pr_], sc["r_VS"][pr_])
            loaded[h] = load_qk(sc["QS"][h][0:kd, :], sc["r_QS"][h], sc["KS"][h][0:kd, :], sc["r_KS"][h], kd) + (vcur[pr_],)
    if groups:
        prefetch(groups[0])
    for gi, group in enumerate(groups):
        if after_sb is not None and group[0][0] != "sb":
            after_sb()
            after_sb = None
        cur = [loaded.pop(job[1]) for job in group]
        if gi + 1 < len(groups):
            prefetch(groups[gi + 1])
        sets = []
        for u, (job, (q, k, v)) in enumerate(zip(group, cur)):
            kind, h, pr_, hh, kd = job

            def done(o_, sp_, h=h, kind=kind):
                finish(o_.t[0:65, 0:sp_["tn"]], o_.r, h, sp_["tq0"], sp_["tn"], kind == "sb")
            sets.append(make_stages(step_cache[kind], q, k, kd, v, hh, kind, done, u))
        drive(sets)
    if after_sb is not None:
        after_sb()
    dgroups = [(hp, g) for hp in range(2) if (hsel(12 + 2 * hp) or hsel(13 + 2 * hp)) for g in range(3)]
    dsteps = {g: dil_steps(DIL_R[g]) for g in range(3)}
    dl = {}

    def dprefetch(grp):
        hp, g = grp
        v = load_v(sc["VD"][g][hp], sc["r_VD"][g][hp])
        dl[grp] = [load_qk(sc["QD"][g][hp * 2 + hh], sc["r_QD"][g][hp * 2 + hh], sc["KD"][g][hp * 2 + hh], sc["r_KD"][g][hp * 2 + hh], 64) + (v,)
                   for hh in range(2)]
    if dgroups:
        dprefetch(dgroups[0])
    for gi, grp in enumerate(dgroups):
        hp, g = grp
        r = DIL_R[g]
        cur = dl.pop(grp)
        if gi + 1 < len(dgroups):
            dprefetch(dgroups[gi + 1])
        sets = []
        for hh in range(2):
            q, k, v = cur[hh]
            a_ = acc[hh]

            def done(o_, sp_, a_=a_, r=r, g=g):
                n, q0 = sp_["tn"], sp_["tq0"]
                dst = perm_view(a_.t[:, :], r, q0, n)
                if g == 0:
                    S.op("act", lambda e: e.activation(out=dst, in_=o_.t[0:65, 0:n], func=AF.Copy), reads=[o_.r], writes=[a_.r])
                else:
                    S.op("dve", lambda e: e.tensor_tensor(out=dst, in0=o_.t[0:65, 0:n], in1=dst, op=ALU.add), reads=[o_.r, a_.r], writes=[a_.r])
            sets.append(make_stages(dsteps[g], q, k, 64, v, hh, "dil", done, hh))
        drive(sets)
        if g == 2:
            for h2 in range(2):
                for qt in range(NT):
                    finish(acc[h2].t[:, qt * 512:(qt + 1) * 512], acc[h2].r, 12 + hp * 2 + h2, qt * 512, 512, False, in_sbuf=True)
    S.flush()
    A.close()


def layernorm_block(S, y, g_b, b_b, small, out):
    st6, mv, rs = small["st6"], small["mv"], small["rs"]
    for hf in range(2):
        S.op("dve", lambda e, hf=hf: e.bn_stats(out=st6.t[:, hf, :], in_=y.t[:, hf * 512:(hf + 1) * 512]), reads=[y.r], writes=[st6.r])
    S.op("dve", lambda e: e.bn_aggr(out=mv.t[:], in_=st6.t[:, :, :].rearrange("p a b -> p (a b)")), reads=[st6.r], writes=[mv.r])
    S.op("act", lambda e: e.activation(out=rs.t[:], in_=mv.t[:, 1:2], func=AF.Ln, bias=small["eps"].t[:, 0:1]), reads=[mv.r, small["eps"].r], writes=[rs.r])
    S.op("act", lambda e: e.activation(out=rs.t[:], in_=rs.t[:], func=AF.Exp, scale=-0.5), reads=[rs.r], writes=[rs.r])
    S.op("dve", lambda e: e.scalar_tensor_tensor(out=y.t[:], in0=y.t[:], scalar=mv.t[:, 0:1], in1=g_b.t[:], op0=ALU.subtract, op1=ALU.mult),
         reads=[y.r, mv.r, g_b.r], writes=[y.r])
    S.op("dve", lambda e: e.scalar_tensor_tensor(out=out.t[:], in0=y.t[:], scalar=rs.t[:, 0:1], in1=b_b.t[:], op0=ALU.mult, op1=ALU.add),
         reads=[y.r, rs.r, b_b.r], writes=[out.r])


def bcast_row(S, A, name, src1d, n):
    b = Buf(A.sb(name, [128, n], F32))
    S.dma("sp", b.t[:], src1d.rearrange("(o n) -> o n", o=1).partition_broadcast(128), writes=[b.r])
    return b


def phase3(nc, S, T, xin, r_xin, l, sc):
    A = Alloc(nc)
    identf = make_ident(A, S, F32)
    wout = Buf(A.sb("wout", [128, 8, DM], BF16))
    S.dma("pool", wout.t[:], T["w_out"][l].rearrange("(c p) n -> p c n", p=128), writes=[wout.r])
    wr = Buf(A.sb("wr", [128, 8, 20], F32))
    S.dma("sp", wr.t[:, :, 0:4], T["w_group"][l].rearrange("(c p) n -> p c n", p=128), writes=[wr.r])
    S.dma("sp", wr.t[:, :, 4:20], T["w_expert"][l].rearrange("(c p) n -> p c n", p=128), writes=[wr.r])
    brt = Buf(A.sb("brt", [128, 20], F32))
    S.dma("sp", brt.t[:, 0:4], T["b_group"][l].rearrange("(o n) -> o n", o=1).partition_broadcast(128), writes=[brt.r])
    S.dma("sp", brt.t[:, 4:20], T["b_expert"][l].rearrange("(o n) -> o n", o=1).partition_broadcast(128), writes=[brt.r])
    g_b = bcast_row(S, A, "ln1g", T["ln1_g"][l], DM)
    b_b = bcast_row(S, A, "ln1b", T["ln1_b"][l], DM)
    on = rot(A, "sb", "on", [128, 8, 512], BF16, 2)
    xs = rot(A, "sb", "xs3", [128, DM], F32, 4)
    y = rot(A, "sb", "y3", [128, DM], F32, 3)
    x1 = rot(A, "sb", "x1o", [128, DM], F32, 6)
    xtf = rot(A, "sb", "xtf", [128, 8, 128], F32, 3)
    xtb = rot(A, "sb", "xtb", [128, 8, 512], BF16, 2)
    gate = Buf(A.sb("gate", [128, NB, 16], F32))
    lgall = Buf(A.sb("lgall", [128, NB, 20], F32))
    if ROUTED:
        x1b = Buf(A.sb("x1b", [128, NB, DM], BF16))
        gohall = Buf(A.sb("gohall", [128, NB, 4], F32))
        S.op("pool", lambda e: e.memset(x1b.t[:, 0:4, :], 0.0), writes=[x1b.r])
        S.op("pool", lambda e: e.memset(gate.t[:], 0.0), writes=[gate.r])
        for k in (range(NTILE) if int(_os_env.get("ZF", "1")) else []):
            S.dma("sp", sc["XS"][k * 512:(k + 1) * 512, :].rearrange("(p r) n -> p (r n)", p=128), x1b.t[:, 0:4, :].rearrange("p b n -> p (b n)"),
                  reads=[x1b.r], writes=[sc["r_XS"]])
        for k in (range(NTILE) if int(_os_env.get("ZF", "1")) else []):
            S.dma("sp", sc["GS"][k * 512:(k + 1) * 512, :].rearrange("(p r) n -> p (r n)", p=128), gate.t[:, 0:4, :].rearrange("p b n -> p (b n)"),
                  reads=[gate.r], writes=[sc["r_GS"]])
    ph = rot(A, "ps", "ph", [128, DM], F32, 2)
    ptr = rot(A, "ps", "ptr", [128, DM], F32, 1)
    plg = rot(A, "ps", "plg", [128, 512], F32, 2)
    epsl = Buf(A.sb("epsl", [128, 1], F32))
    S.op("pool", lambda e: e.memset(epsl.t[:], LN_EPS), writes=[epsl.r])
    small = [dict(st6=Buf(A.sb("st6", [128, 2, 6], F32)), mv=Buf(A.sb("mv", [128, 2], F32)), rs=Buf(A.sb("rs", [128, 1], F32)), eps=epsl) for _ in range(3)]
    pend = []
    pend2 = []

    def ld_on(j):
        S.dma("sp", on[j % 2].t[:], sc["OnT"][:, :, j * 512:(j + 1) * 512].rearrange("c p t -> p c t"), reads=[sc["r_OnT"]], writes=[on[j % 2].r])

    def ld_x(b):
        S.dma("sp", xs[b % 4].t[:], xin[b * 128:(b + 1) * 128, :], reads=[r_xin], writes=[xs[b % 4].r])
    for j in range(NT):
        o_ = on[j % 2]
        if j == 0:
            ld_on(0)
        if j + 1 < NT:
            ld_on(j + 1)
        xb_ = xtb[j % 2]
        for bb in range(4):
            b = j * 4 + bb
            s_, y_, x1_, xf_, p_, sm = xs[b % 4], y[b % 3], x1[b % 6], xtf[b % 3], ph[b % 2], small[b % 3]
            if b == 0:
                ld_x(0)
                ld_x(1)
            if b + 2 < NB:
                ld_x(b + 2)
            for hf in range(2):
                for c in range(8):
                    S.op("pe", lambda e, hf=hf, c=c, bb=bb, o_=o_, p_=p_: e.matmul(p_.t[:, hf * 512:(hf + 1) * 512], lhsT=o_.t[:, c, bb * 128:(bb + 1) * 128],
                                                                            rhs=wout.t[:, c, hf * 512:(hf + 1) * 512], start=(c == 0), stop=(c == 7)),
                         reads=[o_.r, wout.r], writes=[p_.r])
            S.op("dve", lambda e, s_=s_, y_=y_, p_=p_: e.scalar_tensor_tensor(out=y_.t[:], in0=s_.t[:], scalar=ALPHA, in1=p_.t[:], op0=ALU.mult, op1=ALU.add),
                 reads=[s_.r, p_.r], writes=[y_.r])
            layernorm_block(S, y_, g_b, b_b, sm, x1_)
            S.dma("sp", sc["X1"][b * 128:(b + 1) * 128, :], x1_.t[:], reads=[x1_.r], writes=[sc["r_X1"]])
            if ROUTED:
                S.op("act", lambda e, b=b, x1_=x1_: e.activation(out=x1b.t[:, b, :], in_=x1_.t[:], func=AF.Copy), reads=[x1_.r], writes=[x1b.r])
            def stage_b(b=b, bb=bb, x1_=x1_, xf_=xf_, xb_=xb_):
                pt = ptr[0]
                for c in range(8):
                    S.op("pe", lambda e, c=c: e.transpose(out=pt.t[:, c * 128:(c + 1) * 128], in_=x1_.t[:, c * 128:(c + 1) * 128], identity=identf.t[:]),
                         reads=[x1_.r, identf.r], writes=[pt.r])
                S.op("act", lambda e: e.activation(out=xf_.t[:, :, :], in_=pt.t[:, :].rearrange("p (c t) -> p c t", c=8), func=AF.Copy),
                     reads=[pt.r], writes=[xf_.r])
                if not ROUTED:
                    S.op("dve", lambda e: e.tensor_copy(out=xb_.t[:, :, bb * 128:(bb + 1) * 128], in_=pt.t[:, :].rearrange("p (c t) -> p c t", c=8)),
                         reads=[pt.r], writes=[xb_.r])
                def stage_c():
                    pl = plg[b % 2]
                    for c in range(8):
                        S.op("pe", lambda e, c=c: e.matmul(pl.t[:, 0:20], lhsT=xf_.t[:, c, :], rhs=wr.t[:, c, :], start=(c == 0), stop=(c == 7)),
                             reads=[xf_.r, wr.r], writes=[pl.r])
                    S.op("dve", lambda e: e.tensor_tensor(out=lgall.t[:, b, :], in0=pl.t[:, 0:20], in1=brt.t[:], op=ALU.add), reads=[pl.r, brt.r], writes=[lgall.r])
                if int(_os_env.get("INL", "0")):
                    stage_c()
                else:
                    pend2.append(stage_c)
            pend.append(stage_b)
            if len(pend2) > int(_os_env.get("LAG2", "1")):
                pend2.pop(0)()
            if len(pend) > 3:
                pend.pop(0)()
            if (not ROUTED) and bb == 3:
                while pend:
                    pend.pop(0)()
                while pend2:
                    pend2.pop(0)()
        if not ROUTED:
            S.dma("sp", sc["X1T"][:, :, j * 512:(j + 1) * 512], xb_.t[:], reads=[xb_.r], writes=[sc["r_X1T"]])
    while pend:
        pend.pop(0)()
        while len(pend2) > 1:
            pend2.pop(0)()
    while pend2:
        pend2.pop(0)()
    def GT(name, shape):
        return Buf(A.sb("gv_" + name, shape, F32))
    B3 = [128, NB, 4]
    gl = lgall.t[:, :, 0:4]
    el = lgall.t[:, :, 4:20].rearrange("p b (g x) -> p b g x", g=4)
    m_, goh, tmp, se = GT("m", [128, NB]), (gohall if ROUTED else GT("goh", B3)), GT("tmp", B3), GT("se", [128, NB])
    t44, es, m1, oh1, es2, m2, oh2 = GT("t44", [128, NB, 4, 4]), GT("es", B3), GT("m1", [128, NB]), GT("oh1", B3), GT("es2", B3), GT("m2", [128, NB]), GT("oh2", B3)
    d_, p1, p2, gi = GT("d", [128, NB]), GT("p1", [128, NB]), GT("p2", [128, NB]), GT("gi", B3)

    def bc(t2):
        return t2.t[:, :].unsqueeze(2).to_broadcast(B3)

    def D(fn, reads, writes):
        S.op("dve", fn, reads=[x.r for x in reads], writes=[x.r for x in writes])
    D(lambda e: e.tensor_reduce(out=m_.t[:], in_=gl, axis=AX.X, op=ALU.max), [lgall], [m_])
    D(lambda e: e.tensor_tensor(out=goh.t[:], in0=gl, in1=bc(m_), op=ALU.is_equal), [lgall, m_], [goh])
    D(lambda e: e.tensor_tensor(out=tmp.t[:], in0=gl, in1=bc(m_), op=ALU.subtract), [lgall, m_], [tmp])
    S.op("act", lambda e: e.activation(out=tmp.t[:], in_=tmp.t[:], func=AF.Exp), reads=[tmp.r], writes=[tmp.r])
    D(lambda e: e.tensor_reduce(out=se.t[:], in_=tmp.t[:], axis=AX.X, op=ALU.add), [tmp], [se])
    D(lambda e: e.reciprocal(out=se.t[:], in_=se.t[:]), [se], [se])
    D(lambda e: e.tensor_tensor(out=t44.t[:], in0=el, in1=goh.t[:, :, :].unsqueeze(3).to_broadcast([128, NB, 4, 4]), op=ALU.mult), [lgall, goh], [t44])
    D(lambda e: e.tensor_reduce(out=es.t[:], in_=t44.t[:, :, :, :].rearrange("p b g x -> p b x g"), axis=AX.X, op=ALU.add), [t44], [es])
    D(lambda e: e.tensor_reduce(out=m1.t[:], in_=es.t[:], axis=AX.X, op=ALU.max), [es], [m1])
    D(lambda e: e.tensor_tensor(out=oh1.t[:], in0=es.t[:], in1=bc(m1), op=ALU.is_equal), [es, m1], [oh1])
    D(lambda e: e.scalar_tensor_tensor(out=es2.t[:], in0=oh1.t[:], scalar=-1e30, in1=es.t[:], op0=ALU.mult, op1=ALU.add), [oh1, es], [es2])
    D(lambda e: e.tensor_reduce(out=m2.t[:], in_=es2.t[:], axis=AX.X, op=ALU.max), [es2], [m2])
    D(lambda e: e.tensor_tensor(out=oh2.t[:], in0=es2.t[:], in1=bc(m2), op=ALU.is_equal), [es2, m2], [oh2])
    D(lambda e: e.tensor_tensor(out=d_.t[:], in0=m2.t[:], in1=m1.t[:], op=ALU.subtract), [m1, m2], [d_])
    S.op("act", lambda e: e.activation(out=d_.t[:], in_=d_.t[:], func=AF.Exp), reads=[d_.r], writes=[d_.r])
    D(lambda e: e.tensor_scalar(out=p1.t[:], in0=d_.t[:], scalar1=1.0, scalar2=None, op0=ALU.add), [d_], [p1])
    D(lambda e: e.reciprocal(out=p1.t[:], in_=p1.t[:]), [p1], [p1])
    D(lambda e: e.tensor_tensor(out=p2.t[:], in0=d_.t[:], in1=p1.t[:], op=ALU.mult), [d_, p1], [p2])
    D(lambda e: e.tensor_tensor(out=gi.t[:], in0=oh1.t[:], in1=bc(p1), op=ALU.mult), [oh1, p1], [gi])
    D(lambda e: e.tensor_tensor(out=oh2.t[:], in0=oh2.t[:], in1=bc(p2), op=ALU.mult), [oh2, p2], [oh2])
    D(lambda e: e.tensor_tensor(out=gi.t[:], in0=gi.t[:], in1=oh2.t[:], op=ALU.add), [gi, oh2], [gi])
    D(lambda e: e.tensor_tensor(out=gi.t[:], in0=gi.t[:], in1=bc(se), op=ALU.mult), [gi, se], [gi])
    D(lambda e: e.tensor_tensor(out=gate.t[:, :, :].rearrange("p b (g x) -> p b g x", g=4), in0=goh.t[:, :, :].unsqueeze(3).to_broadcast([128, NB, 4, 4]),
                                in1=gi.t[:, :, :].unsqueeze(2).to_broadcast([128, NB, 4, 4]), op=ALU.mult), [goh, gi], [gate])
    if not ROUTED:
        S.dma("sp", sc["GATE"], gate.t[:, :, :].rearrange("p b e -> p (b e)"), reads=[gate.r], writes=[sc["r_GATE"]])
    else:
        route_epilogue(S, A, sc, x1b, gate, gohall, plg, l)
    S.flush()
    A.close()


def route_epilogue(S, A, sc, x1b, gate, gohall, plg, l):
    def T_(name, shape, dt=F32):
        return Buf(A.sb(name, shape, dt))
    onesf = T_("onesf", [128, 128])
    tris = T_("tris", [128, 128])
    S.op("pool", lambda e: e.memset(onesf.t[:], 1.0), writes=[onesf.r])
    S.op("pool", lambda e: e.memset(tris.t[:], 1.0), writes=[tris.r])
    S.op("pool", lambda e: e.affine_select(out=tris.t[:], in_=tris.t[:], pattern=[[1, 128]], compare_op=ALU.is_ge, fill=0.0, base=-1,
                                           channel_multiplier=-1), reads=[tris.r], writes=[tris.r])
    pt, pr = plg[0], plg[1]
    for b in range(NB):
        S.op("pe", lambda e, b=b: e.matmul(pt.t[:, b * 4:(b + 1) * 4], lhsT=onesf.t[:], rhs=gohall.t[:, b, :], start=True, stop=True),
             reads=[onesf.r, gohall.r], writes=[pt.r])
        S.op("pe", lambda e, b=b: e.matmul(pr.t[:, b * 4:(b + 1) * 4], lhsT=tris.t[:], rhs=gohall.t[:, b, :], start=True, stop=True),
             reads=[tris.r, gohall.r], writes=[pr.r])
    totb = T_("totb", [128, NB, 4])
    cum = T_("cumb", [128, NB, 4])
    ones32 = T_("ones32", [128, NB])
    S.op("pool", lambda e: e.memset(ones32.t[:], 1.0), writes=[ones32.r])
    S.op("dve", lambda e: e.tensor_copy(out=totb.t[:, :, :], in_=pt.t[:, 0:NB * 4].rearrange("p (b g) -> p b g", g=4)), reads=[pt.r], writes=[totb.r])
    for g in range(4):
        S.op("dve", lambda e, g=g: e.tensor_tensor_scan(out=cum.t[:, :, g], data0=ones32.t[:, :], data1=totb.t[:, :, g], initial=0.0,
                                                        op0=ALU.mult, op1=ALU.add), reads=[ones32.r, totb.r, cum.r], writes=[cum.r])
    boffx = T_("boffx", [128, NB, 4])
    S.op("dve", lambda e: e.tensor_tensor(out=boffx.t[:], in0=cum.t[:], in1=totb.t[:], op=ALU.subtract), reads=[cum.r, totb.r], writes=[boffx.r])
    thr_i = T_("thri", [128, 16], I32)
    thr = T_("thr", [128, 16])
    S.op("pool", lambda e: e.iota(thr_i.t[:], pattern=[[512, 16]], base=0, channel_multiplier=0), writes=[thr_i.r])
    S.op("dve", lambda e: e.tensor_copy(out=thr.t[:], in_=thr_i.t[:]), reads=[thr_i.r], writes=[thr.r])
    cmp = T_("cmp", [128, 4, 8])
    ntl = T_("ntl", [128, 4])
    S.op("dve", lambda e: e.tensor_tensor(out=cmp.t[:], in0=cum.t[:, NB - 1, :].unsqueeze(2).to_broadcast([128, 4, 8]),
                                          in1=thr.t[:, 0:8].unsqueeze(1).to_broadcast([128, 4, 8]), op=ALU.is_gt), reads=[cum.r, thr.r], writes=[cmp.r])
    S.op("dve", lambda e: e.tensor_reduce(out=ntl.t[:], in_=cmp.t[:], axis=AX.X, op=ALU.add), reads=[cmp.r], writes=[ntl.r])
    S.op("dve", lambda e: e.tensor_scalar(out=ntl.t[:], in0=ntl.t[:], scalar1=512.0, scalar2=None, op0=ALU.mult), reads=[ntl.r], writes=[ntl.r])
    pst = T_("pst", [128, 4])
    pen = T_("pen", [128, 4])
    S.op("pool", lambda e: e.memset(pst.t[:], 0.0), writes=[pst.r])
    for g in range(1, 4):
        S.op("dve", lambda e, g=g: e.tensor_tensor(out=pst.t[:, g:g + 1], in0=pst.t[:, g - 1:g], in1=ntl.t[:, g - 1:g], op=ALU.add),
             reads=[pst.r, ntl.r], writes=[pst.r])
    S.op("dve", lambda e: e.tensor_tensor(out=pen.t[:], in0=pst.t[:], in1=ntl.t[:], op=ALU.add), reads=[pst.r, ntl.r], writes=[pen.r])
    v = T_("vdest", [128, NB, 4])
    S.op("dve", lambda e: e.tensor_tensor(out=v.t[:], in0=pr.t[:, 0:NB * 4].rearrange("p (b g) -> p b g", g=4), in1=boffx.t[:], op=ALU.add),
         reads=[pr.r, boffx.r], writes=[v.r])
    S.op("dve", lambda e: e.tensor_tensor(out=v.t[:], in0=v.t[:], in1=pst.t[:, :].unsqueeze(1).to_broadcast([128, NB, 4]), op=ALU.add),
         reads=[v.r, pst.r], writes=[v.r])
    S.op("dve", lambda e: e.tensor_tensor(out=v.t[:], in0=v.t[:], in1=gohall.t[:], op=ALU.mult), reads=[v.r, gohall.r], writes=[v.r])
    destf = T_("destf", [128, NB])
    desti = T_("desti", [128, NB], I32)
    S.op("dve", lambda e: e.tensor_reduce(out=destf.t[:], in_=v.t[:], axis=AX.X, op=ALU.add), reads=[v.r], writes=[destf.r])
    S.op("dve", lambda e: e.tensor_copy(out=desti.t[:], in_=destf.t[:]), reads=[destf.r], writes=[desti.r])
    S.dma("sp", sc["DEST"], desti.t[:], reads=[desti.r], writes=[sc["r_DEST"]])
    cmp2 = T_("cmp2", [128, NTILE, 4])
    gk = T_("gk", [128, NTILE])
    S.op("dve", lambda e: e.tensor_tensor(out=cmp2.t[:], in0=pen.t[:, :].unsqueeze(1).to_broadcast([128, NTILE, 4]),
                                          in1=thr.t[:, 0:NTILE].unsqueeze(2).to_broadcast([128, NTILE, 4]), op=ALU.is_le), reads=[pen.r, thr.r], writes=[cmp2.r])
    S.op("dve", lambda e: e.tensor_reduce(out=gk.t[:], in_=cmp2.t[:], axis=AX.X, op=ALU.add), reads=[cmp2.r], writes=[gk.r])
    S.op("dve", lambda e: e.tensor_scalar(out=gk.t[:], in0=gk.t[:], scalar1=3.0, scalar2=1024.0, op0=ALU.min, op1=ALU.mult), reads=[gk.r], writes=[gk.r])
    S.op("dve", lambda e: e.tensor_scalar(out=gk.t[:], in0=gk.t[:], scalar1=float(l * 4096), scalar2=None, op0=ALU.add), reads=[gk.r], writes=[gk.r])
    cw_i = T_("cwi", [128, 8], I32)
    cw = T_("cw", [128, 8])
    S.op("pool", lambda e: e.iota(cw_i.t[:], pattern=[[256, 4], [1, 2]], base=0, channel_multiplier=2), writes=[cw_i.r])
    S.op("dve", lambda e: e.tensor_copy(out=cw.t[:], in_=cw_i.t[:]), reads=[cw_i.r], writes=[cw.r])
    idxf = T_("idxf", [128, NTILE, 8])
    idxi = T_("idxi", [128, NTILE, 8], I32)
    S.op("dve", lambda e: e.tensor_tensor(out=idxf.t[:], in0=cw.t[:, :].unsqueeze(1).to_broadcast([128, NTILE, 8]),
                                          in1=gk.t[:, :].unsqueeze(2).to_broadcast([128, NTILE, 8]), op=ALU.add), reads=[cw.r, gk.r], writes=[idxf.r])
    S.op("dve", lambda e: e.tensor_copy(out=idxi.t[:], in_=idxf.t[:]), reads=[idxf.r], writes=[idxi.r])
    S.dma("sp", sc["IDXW"], idxi.t[:, :, :].rearrange("p k j -> p (k j)"), reads=[idxi.r], writes=[sc["r_IDXW"]])
    for b in range(NB):
        S.dma_fn("pool", lambda e, b=b: e.indirect_dma_start(out=sc["XS"][:, :], out_offset=bass.IndirectOffsetOnAxis(ap=desti.t[:, b:b + 1], axis=0),
                                                            in_=x1b.t[:, b, :], in_offset=None), reads=[desti.r, x1b.r], writes=[sc["r_XS"]])
        S.dma_fn("pool", lambda e, b=b: e.indirect_dma_start(out=sc["GS"][:, :], out_offset=bass.IndirectOffsetOnAxis(ap=desti.t[:, b:b + 1], axis=0),
                                                            in_=gate.t[:, b, :], in_offset=None), reads=[desti.r, gate.r], writes=[sc["r_GS"]])


def phase4(nc, S, T, l, sc, xout, r_xout, final, TG=1024):
    A = Alloc(nc)
    g_b = bcast_row(S, A, "ln2g", T["ln2_g"][l], DM)
    b_b = bcast_row(S, A, "ln2b", T["ln2_b"][l], DM)
    gate = Buf(A.sb("gate4", [128, NB, 16], F32))
    S.dma("sp", gate.t[:, :, :].rearrange("p b e -> p (b e)"), sc["GATE"], reads=[sc["r_GATE"]], writes=[gate.r])
    xT = Buf(A.sb("x1T", [128, 8, TG], BF16))
    accb = Buf(A.sb("accm", [128, TG // 128, DM], F32))
    w1 = rot(A, "sb", "w1", [128, 8, DEXP], BF16, 2)
    w3 = rot(A, "sb", "w3", [128, 8, DEXP], BF16, 2)
    w2 = rot(A, "sb", "w2", [128, 4, DM], BF16, 2)
    sa = rot(A, "sb", "sa", [128, 512], F32, 2)
    hT = rot(A, "sb", "hT", [128, 4, 512], BF16, 2)
    xs = rot(A, "sb", "xs4", [128, DM], F32, 2)
    yo = rot(A, "sb", "yo4", [128, DM], F32, 2)
    epsl = Buf(A.sb("epsl4", [128, 1], F32))
    S.op("pool", lambda e: e.memset(epsl.t[:], LN_EPS), writes=[epsl.r])
    small = [dict(st6=Buf(A.sb("st6b", [128, 2, 6], F32)), mv=Buf(A.sb("mvb", [128, 2], F32)), rs=Buf(A.sb("rsb", [128, 1], F32)), eps=epsl) for _ in range(2)]
    pa = rot(A, "ps", "pa", [128, 512], F32, 2)
    pb = rot(A, "ps", "pb", [128, 512], F32, 2)
    py = rot(A, "ps", "py", [128, 512], F32, 3)
    cnt = {"y": 0, "ab": 0, "w": 0}
    W1 = T["w1"][l]
    W3 = T["w3"][l]
    W2 = T["w2"][l]
    for gi in range(SEQ // TG):
        t0 = gi * TG
        S.dma("sp", xT.t[:], sc["X1T"][:, :, t0:t0 + TG], reads=[sc["r_X1T"]], writes=[xT.r])
        for ex in range(NEXP):
            i = cnt["w"]
            cnt["w"] += 1
            a1, a3, a2 = w1[i % 2], w3[i % 2], w2[i % 2]
            S.dma("pool", a1.t[:], W1[ex].rearrange("(c p) n -> p c n", p=128), writes=[a1.r])
            S.dma("pool", a3.t[:], W3[ex].rearrange("(c p) n -> p c n", p=128), writes=[a3.r])
            S.dma("pool", a2.t[:], W2[ex].rearrange("(c p) n -> p c n", p=128), writes=[a2.r])
            for tt in range(TG // 512):
                tc = slice(tt * 512, (tt + 1) * 512)
                h_ = hT[(ex * (TG // 512) + tt) % 2]
                for jc in range(4):
                    k_ = cnt["ab"]
                    cnt["ab"] += 1
                    pa_, pb_, sa_ = pa[k_ % 2], pb[k_ % 2], sa[k_ % 2]
                    for c in range(8):
                        S.op("pe", lambda e, c=c, jc=jc, pa_=pa_, a1=a1, tc=tc: e.matmul(pa_.t[:], lhsT=a1.t[:, c, jc * 128:(jc + 1) * 128], rhs=xT.t[:, c, tc],
                                                                                start=(c == 0), stop=(c == 7)), reads=[a1.r, xT.r], writes=[pa_.r])
                    for c in range(8):
                        S.op("pe", lambda e, c=c, jc=jc, pb_=pb_, a3=a3, tc=tc: e.matmul(pb_.t[:], lhsT=a3.t[:, c, jc * 128:(jc + 1) * 128], rhs=xT.t[:, c, tc],
                                                                                start=(c == 0), stop=(c == 7)), reads=[a3.r, xT.r], writes=[pb_.r])
                    S.op("act", lambda e, pa_=pa_, sa_=sa_: e.activation(out=sa_.t[:], in_=pa_.t[:], func=AF.Silu), reads=[pa_.r], writes=[sa_.r])
                    S.op("dve", lambda e, pb_=pb_, sa_=sa_, h_=h_, jc=jc: e.tensor_tensor(out=h_.t[:, jc, :], in0=pb_.t[:], in1=sa_.t[:], op=ALU.mult),
                         reads=[pb_.r, sa_.r], writes=[h_.r])
                for tb in range(4):
                    blk = tt * 4 + tb
                    gb = (t0 // 128) + blk
                    for hf in range(2):
                        y_ = py[cnt["y"] % 3]
                        cnt["y"] += 1
                        for jc in range(4):
                            S.op("pe", lambda e, jc=jc, tb=tb, hf=hf, y_=y_, h_=h_, a2=a2: e.matmul(y_.t[:], lhsT=h_.t[:, jc, tb * 128:(tb + 1) * 128],
                                                                                           rhs=a2.t[:, jc, hf * 512:(hf + 1) * 512], start=(jc == 0), stop=(jc == 3)),
                                 reads=[h_.r, a2.r], writes=[y_.r])
                        dst = accb.t[:, blk, hf * 512:(hf + 1) * 512]
                        if ex == 0:
                            S.op("dve", lambda e, y_=y_, dst=dst, gb=gb, ex=ex: e.tensor_scalar(out=dst, in0=y_.t[:], scalar1=gate.t[:, gb, ex:ex + 1], scalar2=None, op0=ALU.mult),
                                 reads=[y_.r, gate.r], writes=[accb.r])
                        else:
                            S.op("dve", lambda e, y_=y_, dst=dst, gb=gb, ex=ex: e.scalar_tensor_tensor(out=dst, in0=y_.t[:], scalar=gate.t[:, gb, ex:ex + 1], in1=dst,
                                                                                              op0=ALU.mult, op1=ALU.add), reads=[y_.r, gate.r, accb.r], writes=[accb.r])
        for blk in range(TG // 128):
            gb = (t0 // 128) + blk
            s_, y_, sm = xs[blk % 2], yo[blk % 2], small[blk % 2]
            S.dma("sp", s_.t[:], sc["X1"][gb * 128:(gb + 1) * 128, :], reads=[sc["r_X1"]], writes=[s_.r])
            S.op("dve", lambda e, s_=s_, blk=blk: e.scalar_tensor_tensor(out=s_.t[:], in0=s_.t[:], scalar=ALPHA, in1=accb.t[:, blk, :], op0=ALU.mult, op1=ALU.add),
                 reads=[s_.r, accb.r], writes=[s_.r])
            layernorm_block(S, s_, g_b, b_b, sm, y_)
            S.dma("sp", xout[gb * 128:(gb + 1) * 128, :], y_.t[:], reads=[y_.r], writes=[r_xout], final=final)
    S.flush()
    A.close()


def phase4r(nc, S, T, l, sc, xout, r_xout, final):
    A = Alloc(nc)
    ident = make_ident(A, S, BF16)
    g_b = bcast_row(S, A, "ln2g", T["ln2_g"][l], DM)
    b_b = bcast_row(S, A, "ln2b", T["ln2_b"][l], DM)
    dest = Buf(A.sb("dest4", [128, NB], I32))
    idxw = Buf(A.sb("idxw4", [128, NTILE * 8], I32))
    S.dma("sp", dest.t[:], sc["DEST"], reads=[sc["r_DEST"]], writes=[dest.r])
    S.dma("sp", idxw.t[:], sc["IDXW"], reads=[sc["r_IDXW"]], writes=[idxw.r])
    xs = rot(A, "sb", "xs4r", [128, 4, DM], BF16, 2)
    gs = rot(A, "sb", "gs4r", [128, 4, 16], F32, 2)
    gsel = rot(A, "sb", "gsel", [128, 4, 4], F32, 2)
    xT = rot(A, "sb", "xT4r", [128, 8, 512], BF16, 2)
    accs = rot(A, "sb", "acc4r", [128, 4, DM], F32, 2)
    w1 = rot(A, "sb", "w1r", [128, 8 * DEXP], BF16, 3)
    w3 = rot(A, "sb", "w3r", [128, 8 * DEXP], BF16, 3)
    w2 = rot(A, "sb", "w2r", [128, 4 * DM], BF16, 3)
    sa = rot(A, "sb", "sar", [128, 512], F32, 2)
    hT = rot(A, "sb", "hTr", [128, 4, 512], BF16, 2)
    mt = rot(A, "sb", "mt4", [128, DM], F32, 4)
    xo = rot(A, "sb", "xo4", [128, DM], F32, 4)
    yo = rot(A, "sb", "yo4r", [128, DM], F32, 4)
    epsl = Buf(A.sb("epsl4r", [128, 1], F32))
    S.op("pool", lambda e: e.memset(epsl.t[:], LN_EPS), writes=[epsl.r])
    small = [dict(st6=Buf(A.sb("st6r", [128, 2, 6], F32)), mv=Buf(A.sb("mvr", [128, 2], F32)), rs=Buf(A.sb("rsr", [128, 1], F32)), eps=epsl) for _ in range(4)]
    ptp = rot(A, "ps", "ptp", [128, DM], BF16, 1)
    pa = rot(A, "ps", "par", [128, 512], F32, 2)
    pb = rot(A, "ps", "pbr", [128, 512], F32, 2)
    py = rot(A, "ps", "pyr", [128, 512], F32, 3)
    Wv = [sc["WB"][i][:, :] for i in range(3)]
    cnt = {"y": 0, "ab": 0}
    steps = [(k, j) for k in range(NTILE) for j in range(4)]
    st = {}

    def front(si):
        k, j = steps[si]
        if j == 0:
            x_, g_, gl_, xT_, ac_ = xs[k % 2], gs[k % 2], gsel[k % 2], xT[k % 2], accs[k % 2]
            S.dma("sp", x_.t[:], sc["XS"][k * 512:(k + 1) * 512, :].rearrange("(b p) n -> p b n", p=128), reads=[sc["r_XS"]], writes=[x_.r])
            S.dma("sp", g_.t[:], sc["GS"][k * 512:(k + 1) * 512, :].rearrange("(b p) n -> p b n", p=128), reads=[sc["r_GS"]], writes=[g_.r])
            S.op("dve", lambda e: e.tensor_reduce(out=gl_.t[:], in_=g_.t[:, :, :].rearrange("p b (g j) -> p b j g", g=4), axis=AX.X, op=ALU.add),
                 reads=[g_.r], writes=[gl_.r])
            for blk in range(4):
                p = ptp[0]
                for c in range(8):
                    S.op("pe", lambda e, c=c, blk=blk: e.transpose(out=p.t[:, c * 128:(c + 1) * 128],
                                                                   in_=x_.t[:, blk, :].rearrange("p (pp c) -> p c pp", c=8)[:, c, :], identity=ident.t[:]),
                         reads=[x_.r, ident.r], writes=[p.r])
                S.op("act", lambda e, blk=blk: e.activation(out=xT_.t[:, :, blk * 128:(blk + 1) * 128], in_=p.t[:, :].rearrange("p (c t) -> p c t", c=8), func=AF.Copy),
                     reads=[p.r], writes=[xT_.r])
        xT_, ac_, gl_ = xT[k % 2], accs[k % 2], gsel[k % 2]
        a1, a3, a2, h_ = w1[si % 3], w3[si % 3], w2[si % 3], hT[si % 2]
        for wt_, src in ((a1, Wv[0]), (a3, Wv[1]), (a2, Wv[2])):
            for half in range(2):
                col = k * 8 + j * 2 + half
                S.dma_fn("pool", lambda e, wt_=wt_, src=src, half=half, col=col: e.indirect_dma_start(
                    out=wt_.t[:, half * 2048:(half + 1) * 2048], out_offset=None, in_=src,
                    in_offset=bass.IndirectOffsetOnAxis(ap=idxw.t[:, col:col + 1], axis=0)), reads=[idxw.r, sc["r_WB"][l]], writes=[wt_.r])
        w1v = a1.t[:, :].rearrange("p (c pp q) -> p c q pp", c=8, q=4)
        w3v = a3.t[:, :].rearrange("p (c pp q) -> p c q pp", c=8, q=4)
        for jc in range(4):
            k_ = cnt["ab"]
            cnt["ab"] += 1
            pa_, pb_, sa_ = pa[k_ % 2], pb[k_ % 2], sa[k_ % 2]
            for c in range(8):
                S.op("pe", lambda e, c=c, jc=jc, pa_=pa_: e.matmul(pa_.t[:], lhsT=w1v[:, c, jc, :], rhs=xT_.t[:, c, :], start=(c == 0), stop=(c == 7)),
                     reads=[a1.r, xT_.r], writes=[pa_.r])
            for c in range(8):
                S.op("pe", lambda e, c=c, jc=jc, pb_=pb_: e.matmul(pb_.t[:], lhsT=w3v[:, c, jc, :], rhs=xT_.t[:, c, :], start=(c == 0), stop=(c == 7)),
                     reads=[a3.r, xT_.r], writes=[pb_.r])
            S.op("act", lambda e, pa_=pa_, sa_=sa_: e.activation(out=sa_.t[:], in_=pa_.t[:], func=AF.Silu), reads=[pa_.r], writes=[sa_.r])
            S.op("dve", lambda e, pb_=pb_, sa_=sa_, jc=jc: e.tensor_tensor(out=h_.t[:, jc, :], in0=pb_.t[:], in1=sa_.t[:], op=ALU.mult),
                 reads=[pb_.r, sa_.r], writes=[h_.r])

    def back(si):
        k, j = steps[si]
        ac_, gl_, a2, h_ = accs[k % 2], gsel[k % 2], w2[si % 3], hT[si % 2]
        w2v = a2.t[:, :].rearrange("p (c n) -> p c n", c=4)
        for blk in range(4):
            for hf in range(2):
                y_ = py[cnt["y"] % 3]
                cnt["y"] += 1
                for jc in range(4):
                    S.op("pe", lambda e, jc=jc, blk=blk, hf=hf, y_=y_: e.matmul(y_.t[:], lhsT=h_.t[:, jc, blk * 128:(blk + 1) * 128],
                                                                               rhs=w2v[:, jc, hf * 512:(hf + 1) * 512], start=(jc == 0), stop=(jc == 3)),
                         reads=[h_.r, a2.r], writes=[y_.r])
                dst = ac_.t[:, blk, hf * 512:(hf + 1) * 512]
                if j == 0:
                    S.op("dve", lambda e, y_=y_, dst=dst, blk=blk: e.tensor_scalar(out=dst, in0=y_.t[:], scalar1=gl_.t[:, blk, j:j + 1], scalar2=None, op0=ALU.mult),
                         reads=[y_.r, gl_.r], writes=[ac_.r])
                else:
                    S.op("dve", lambda e, y_=y_, dst=dst, blk=blk: e.scalar_tensor_tensor(out=dst, in0=y_.t[:], scalar=gl_.t[:, blk, j:j + 1], in1=dst,
                                                                                         op0=ALU.mult, op1=ALU.add), reads=[y_.r, gl_.r, ac_.r], writes=[ac_.r])
        if j == 3:
            S.dma("sp", sc["YS"][k * 512:(k + 1) * 512, :].rearrange("(b p) n -> p b n", p=128), ac_.t[:], reads=[ac_.r], writes=[sc["r_YS"]])

    for si in range(len(steps) + 1):
        if si < len(steps):
            front(si)
        if si >= 1:
            back(si - 1)
    def comb_load(b):
        m_, x_ = mt[b % 4], xo[b % 4]
        S.dma_fn("pool", lambda e: e.indirect_dma_start(out=m_.t[:], out_offset=None, in_=sc["YS"][:, :],
                                                        in_offset=bass.IndirectOffsetOnAxis(ap=dest.t[:, b:b + 1], axis=0)),
                 reads=[dest.r, sc["r_YS"]], writes=[m_.r])
        S.dma("sp", x_.t[:], sc["X1"][b * 128:(b + 1) * 128, :], reads=[sc["r_X1"]], writes=[x_.r])
    for b in range(min(3, NB)):
        comb_load(b)
    for b in range(NB):
        m_, x_, y_, sm = mt[b % 4], xo[b % 4], yo[b % 4], small[b % 4]
        S.op("dve", lambda e, m_=m_, x_=x_: e.scalar_tensor_tensor(out=x_.t[:], in0=x_.t[:], scalar=ALPHA, in1=m_.t[:], op0=ALU.mult, op1=ALU.add),
             reads=[x_.r, m_.r], writes=[x_.r])
        layernorm_block(S, x_, g_b, b_b, sm, y_)
        if b + 3 < NB:
            comb_load(b + 3)
        S.dma("sp", xout[b * 128:(b + 1) * 128, :], y_.t[:], reads=[y_.r], writes=[r_xout], final=final)
    S.flush()
    A.close()


def rope_tables():
    pos = np.arange(SEQ, dtype=np.float32)
    out = {}
    for dim, nm in ((64, "rope64"), (32, "rope32")):
        half = dim // 2
        inv = (10000.0 ** (-np.arange(half, dtype=np.float32) / half)).astype(np.float32)
        ang = pos[None, :] * inv[:, None]
        c = np.cos(ang).astype(np.float32)
        s = np.sin(ang).astype(np.float32)
        out[nm + "c"] = np.ascontiguousarray(np.concatenate([c, c], 0))
        out[nm + "s"] = np.ascontiguousarray(np.concatenate([-s, s], 0))
    return out


W_SPECS = [("w_in", [DEPTH, DM, N_IN]), ("b_forget", [DEPTH, 4]), ("g_cq", [DEPTH, 256]), ("g_ckv", [DEPTH, 128]), ("w_uq", [DEPTH, 256, 384]),
           ("w_ukv", [DEPTH, 128, 512]), ("g_head", [DEPTH, 16, 64]), ("w_out", [DEPTH, DM, DM]), ("ln1_g", [DEPTH, DM]), ("ln1_b", [DEPTH, DM]),
           ("w_group", [DEPTH, DM, 4]), ("b_group", [DEPTH, 4]), ("w_expert", [DEPTH, DM, 16]), ("b_expert", [DEPTH, 16]),
           ("w1", [DEPTH, NEXP, DM, DEXP]), ("w3", [DEPTH, NEXP, DM, DEXP]), ("w2", [DEPTH, NEXP, DEXP, DM]), ("ln2_g", [DEPTH, DM]), ("ln2_b", [DEPTH, DM])]


def build_program(nseq=2, layers=(0, 1), phases=(1, 2, 3, 4), debug=False, heads=None, TG=1024):
    nc = bass.Bass("TRN2", target_bir_lowering=False)
    T = {}
    T["x"] = nc.dram_tensor("x", [nseq, SEQ, DM], F32, kind="ExternalInput").ap()
    for nm, shp in W_SPECS:
        T[nm] = nc.dram_tensor(nm, shp, F32, kind="ExternalInput").ap()
    for nm, rows in (("rope64c", 64), ("rope64s", 64), ("rope32c", 32), ("rope32s", 32)):
        T[nm] = nc.dram_tensor(nm, [rows, SEQ], F32, kind="ExternalInput").ap()
    out = nc.dram_tensor("out", [nseq, SEQ, DM], F32, kind="ExternalOutput").ap()
    dk = "ExternalOutput" if debug else "Internal"

    def scratch(name, shape, dt):
        return nc.dram_tensor(name, shape, dt, kind=dk).ap()
    sc = {}
    qs = scratch("QS", [12, 96, SEQ], BF16)
    ks = scratch("KS", [12, 96, SEQ], BF16)
    sc["QS"] = [qs[h] for h in range(12)]
    sc["KS"] = [ks[h] for h in range(12)]
    vs = scratch("VS", [6, 128, NB * 2 * 65], BF16)
    sc["VS"] = [vs[p] for p in range(6)]
    qd = scratch("QD", [3, 4, 64, SEQ], BF16)
    kd = scratch("KD", [3, 4, 64, SEQ], BF16)
    sc["QD"] = [[qd[g, h] for h in range(4)] for g in range(3)]
    sc["KD"] = [[kd[g, h] for h in range(4)] for g in range(3)]
    vd = scratch("VD", [3, 2, 128, NB * 2 * 65], BF16)
    sc["VD"] = [[vd[g, p] for p in range(2)] for g in range(3)]
    sc["OnT"] = scratch("OnT", [8, 128, SEQ], BF16)
    sc["X1"] = scratch("X1", [SEQ, DM], F32)
    sc["X1T"] = scratch("X1T", [128, 8, SEQ], BF16)
    sc["GATE"] = scratch("GATE", [128, NB * 16], F32)
    sc["XS"] = scratch("XS", [NSLOT, DM], BF16)
    sc["GS"] = scratch("GS", [NSLOT, 16], F32)
    sc["YS"] = scratch("YS", [NSLOT, DM], F32)
    sc["DEST"] = scratch("DEST", [128, NB], I32)
    sc["IDXW"] = scratch("IDXW", [128, NTILE * 8], I32)
    sc["WB"] = [scratch("WB%d" % i, [DEPTH * 4096, 2048], BF16) for i in range(3)]
    sc["r_WB"] = [Res() for _ in range(DEPTH)]
    xmid = scratch("XMID", [SEQ, DM], F32)
    sc["r_QS"] = [Res() for _ in range(12)]
    sc["r_KS"] = [Res() for _ in range(12)]
    sc["r_VS"] = [Res() for _ in range(6)]
    sc["r_QD"] = [[Res() for _ in range(4)] for _ in range(3)]
    sc["r_KD"] = [[Res() for _ in range(4)] for _ in range(3)]
    sc["r_VD"] = [[Res() for _ in range(2)] for _ in range(3)]
    for k in ("OnT", "X1", "X1T", "GATE", "XS", "GS", "YS", "DEST", "IDXW"):
        sc["r_" + k] = Res()
    r_xmid = Res()
    r_x = Res()
    r_out = Res()
    S = Sched(nc)
    for s in range(nseq):
        for li, l in enumerate(layers):
            xin, r_xin = (T["x"][s], r_x) if li == 0 else (xmid, r_xmid)
            last = li == len(layers) - 1
            xo, r_xo = (out[s], r_out) if last else (xmid, r_xmid)
            if 1 in phases:
                phase1(nc, S, T, xin, r_xin, l, sc)
            if 2 in phases:
                cv = (lambda l=l: convert_expert_weights(S, T, sc, l)) if (ROUTED and s == 0) else None
                phase2(nc, S, T, l, sc, heads=heads, after_sb=cv)
            if 3 in phases:
                phase3(nc, S, T, xin, r_xin, l, sc)
            if 4 in phases:
                if ROUTED:
                    phase4r(nc, S, T, l, sc, xo, r_xo, final=last)
                else:
                    phase4(nc, S, T, l, sc, xo, r_xo, final=last, TG=TG)
    S.close()
    return nc, S


def convert_expert_weights(S, T, sc, l):
    for i, nm in enumerate(("w1", "w3", "w2")):
        src = T[nm][l].rearrange("e k n -> (e k n)").rearrange("(r x) -> r x", x=2048)
        for ch in range(8):
            S.dma("pool", sc["WB"][i][l * 4096 + ch * 512:l * 4096 + (ch + 1) * 512, :], src[ch * 512:(ch + 1) * 512, :], writes=[sc["r_WB"][l]])


_CACHE = {}


def kernel(**inputs):
    n = 8
    nseq = 2
    x = np.ascontiguousarray(np.asarray(inputs["x"], dtype=np.float32))
    tabs = rope_tables()
    if "nc" not in _CACHE:
        _CACHE["nc"] = build_program(nseq=nseq)[0]
    nc = _CACHE["nc"]
    base = {nm: np.ascontiguousarray(np.asarray(inputs[nm], dtype=np.float32)) for nm, _ in W_SPECS}
    base.update(tabs)
    in_maps = []
    for c in range(n):
        m = dict(base)
        m["x"] = x[c * nseq:(c + 1) * nseq]
        in_maps.append(m)
    res = run_bass_kernel_spmd(nc, in_maps, core_ids=list(range(n)))
    return np.concatenate([r["out"] for r in res.results], axis=0).astype(np.float32)
```
